# Optimizing a Trainium2 kernel written in Bass

```python
import math
import jax, jax.numpy as jnp
from jax import lax
import numpy as np

D_MODEL = 1024
BATCH = 8
SEQ = 4096
DEPTH = 2

D_MIX = D_MODEL
A_HEADS = 4
A_HEAD_DIM = 64
A_WIDTH = A_HEADS * A_HEAD_DIM
CHUNK = 128
B_HEADS = 8
B_HEAD_DIM = 64
B_WIDTH = B_HEADS * B_HEAD_DIM
DILATED_PAIRS = ((128, 1), (512, 4), (2048, 16))
ROT_DIM = B_HEAD_DIM // 4
ROPE_THETA = 500000.0
C_WIDTH = D_MIX - A_WIDTH - B_WIDTH
SSM_GROUP = 16
SSM_GROUPS = C_WIDTH // SSM_GROUP
SSM_STATE = 64
DT_MIN = 0.001
DT_MAX = 0.1
PROJ_WIDTH = 2 * A_WIDTH + 3 * B_WIDTH + C_WIDTH
N_EXPERTS = 16
N_EXPERT_GROUPS = 4
EXPERTS_PER_GROUP = N_EXPERTS // N_EXPERT_GROUPS
TOP_K = 2
D_EXPERT = D_MODEL // 2
MOE_BLOCK = 128
ALPHA = (2.0 * DEPTH) ** 0.25
BETA = (8.0 * DEPTH) ** -0.25
LN_EPS = 1e-5
NEG_INF = -1e30

kernel_name = 'hybrid_gmlp_dilated_s5_moe_deepnorm'


def layer_norm(x):
    x32 = x.astype(jnp.float32)
    mu = x32.mean(-1, keepdims=True)
    var = jnp.square(x32 - mu).mean(-1, keepdims=True)
    return (x32 - mu) * lax.rsqrt(var + LN_EPS)


def partial_rope(x, positions):
    half = ROT_DIM // 2
    freqs = ROPE_THETA ** (-jnp.arange(half, dtype=jnp.float32) * 2.0 / ROT_DIM)
    ang = positions.astype(jnp.float32)[..., None] * freqs
    cos = jnp.cos(ang)[:, :, None, :]
    sin = jnp.sin(ang)[:, :, None, :]
    x1 = x[..., :half]
    x2 = x[..., half:ROT_DIM]
    return jnp.concatenate([x1 * cos - x2 * sin, x2 * cos + x1 * sin, x[..., ROT_DIM:]], axis=-1)


def chunked_spatial_gating(uv, ln_g, ln_b, w_s, b_s):
    u, v = jnp.split(jax.nn.gelu(uv), 2, axis=-1)
    v = layer_norm(v) * ln_g + ln_b
    bsz, t, _ = v.shape
    vh = v.reshape(bsz, t // CHUNK, CHUNK, A_HEADS, A_HEAD_DIM)
    causal = jnp.tril(jnp.ones((CHUNK, CHUNK), dtype=bool))
    w = jnp.where(causal[None], w_s, 0.0)
    sv = jnp.einsum('hts,bnshe->bnthe', w, vh) + b_s.T[:, :, None]
    return u * sv.reshape(bsz, t, A_WIDTH)


def dilated_branch(q, k, v, window, dilation):
    bsz, t, h, e = q.shape
    blk = window // dilation
    seg = blk * dilation
    tp = -(-t // seg) * seg
    nb = tp // seg
    pad = ((0, 0), (0, tp - t), (0, 0), (0, 0))

    def split(z):
        return jnp.pad(z, pad).reshape(bsz, nb, blk, dilation, h, e)

    def with_prev(z):
        prev = jnp.pad(z[:, :-1], ((0, 0), (1, 0), (0, 0), (0, 0), (0, 0), (0, 0)))
        return jnp.concatenate([prev, z], axis=2)

    qb = split(q)
    kb = with_prev(split(k))
    vb = with_prev(split(v))
    s = jnp.einsum('bnqrhe,bnkrhe->bnrhqk', qb, kb) * (e ** -0.5)
    qi = jnp.arange(blk)[:, None]
    kk = jnp.arange(2 * blk)[None, :]
    band = (kk >= qi) & (kk <= qi + blk)
    valid = band[None] & ((jnp.arange(nb)[:, None, None] > 0) | (kk >= blk)[None])
    s = jnp.where(valid[None, :, None, None], s, NEG_INF)
    m = s.max(-1)
    p = jnp.exp(s - m[..., None])
    den = p.sum(-1)
    o = jnp.einsum('bnrhqk,bnkrhe->bnqrhe', p, vb) / jnp.moveaxis(den, -1, 2)[..., None]
    o = o.reshape(bsz, tp, h, e)[:, :t]
    m = jnp.moveaxis(m, -1, 2).reshape(bsz, tp, h)[:, :t]
    den = jnp.moveaxis(den, -1, 2).reshape(bsz, tp, h)[:, :t]
    return o, m, den


def dilated_attention(q, k, v):
    outs = [dilated_branch(q, k, v, w, d) for (w, d) in DILATED_PAIRS]
    o_all = jnp.stack([o for o, _, _ in outs])
    m_all = jnp.stack([m for _, m, _ in outs])
    d_all = jnp.stack([dn for _, _, dn in outs])
    wts = d_all * jnp.exp(m_all - m_all.max(0))
    return jnp.einsum('gbth,gbthe->bthe', wts, o_all) / wts.sum(0)[..., None]


def s5_mixer(u, lam_re, lam_im, log_dt, b_re, b_im, c_re, c_im, d_skip, glu_w, glu_b):
    f32 = jnp.float32
    u = u.astype(f32)
    bsz, t, _ = u.shape
    ug = u.reshape(bsz, t, SSM_GROUPS, SSM_GROUP)
    lam_re = lam_re.astype(f32)
    lam_im = lam_im.astype(f32)
    dt = jnp.exp(log_dt.astype(f32))[:, None]
    mag = jnp.exp(lam_re * dt)
    ab_re = mag * jnp.cos(lam_im * dt)
    ab_im = mag * jnp.sin(lam_im * dt)
    nr = ab_re - 1.0
    ni = ab_im
    mod2 = lam_re * lam_re + lam_im * lam_im
    f_re = (nr * lam_re + ni * lam_im) / mod2
    f_im = (ni * lam_re - nr * lam_im) / mod2
    b_re = b_re.astype(f32)
    b_im = b_im.astype(f32)
    bb_re = f_re[..., None] * b_re - f_im[..., None] * b_im
    bb_im = f_re[..., None] * b_im + f_im[..., None] * b_re
    bu_re = jnp.einsum('btgc,gnc->btgn', ug, bb_re)
    bu_im = jnp.einsum('btgc,gnc->btgn', ug, bb_im)
    a_re = jnp.broadcast_to(ab_re, bu_re.shape)
    a_im = jnp.broadcast_to(ab_im, bu_im.shape)

    def combine(e1, e2):
        a1r, a1i, b1r, b1i = e1
        a2r, a2i, b2r, b2i = e2
        return (a2r * a1r - a2i * a1i,
                a2r * a1i + a2i * a1r,
                a2r * b1r - a2i * b1i + b2r,
                a2r * b1i + a2i * b1r + b2i)

    _, _, x_re, x_im = lax.associative_scan(combine, (a_re, a_im, bu_re, bu_im), axis=1)
    y = (jnp.einsum('btgn,gcn->btgc', x_re, c_re.astype(f32))
         - jnp.einsum('btgn,gcn->btgc', x_im, c_im.astype(f32))
         + d_skip.astype(f32) * ug)
    y = jax.nn.gelu(y.reshape(bsz, t, C_WIDTH))
    return y * jax.nn.sigmoid(y @ glu_w + glu_b)


def grouped_moe(h, router_w, router_bias, w_gate, w_up, w_down):
    bsz, t, d = h.shape
    n_tok = bsz * t
    ht = h.reshape(n_tok, d)
    scores = jax.nn.sigmoid(ht.astype(jnp.float32) @ router_w.astype(jnp.float32))
    sel = scores + router_bias.astype(jnp.float32)
    sel_g = sel.reshape(n_tok, N_EXPERT_GROUPS, EXPERTS_PER_GROUP)
    group_score = lax.top_k(sel_g, 2)[0].sum(-1)
    g_idx = jnp.argmax(group_score, axis=-1)
    sel_in = jnp.take_along_axis(sel_g, g_idx[:, None, None], axis=1)[:, 0]
    _, loc = lax.top_k(sel_in, TOP_K)
    e_idx = g_idx[:, None] * EXPERTS_PER_GROUP + loc
    gate = jnp.take_along_axis(scores, e_idx, axis=1)
    gate = gate / gate.sum(-1, keepdims=True)

    n_asg = n_tok * TOP_K
    flat_e = e_idx.reshape(n_asg)
    flat_tok = jnp.repeat(jnp.arange(n_tok, dtype=jnp.int32), TOP_K)
    flat_w = gate.reshape(n_asg)
    order = jnp.argsort(flat_e)
    se = flat_e[order]
    counts = jnp.bincount(flat_e, length=N_EXPERTS)
    starts = jnp.cumsum(counts) - counts
    pcounts = (counts + MOE_BLOCK - 1) // MOE_BLOCK * MOE_BLOCK
    pends = jnp.cumsum(pcounts)
    pstarts = pends - pcounts
    dest = pstarts[se] + (jnp.arange(n_asg) - starts[se])
    cap = n_asg + N_EXPERTS * MOE_BLOCK
    n_blk = cap // MOE_BLOCK
    slot_tok = jnp.zeros((cap,), jnp.int32).at[dest].set(flat_tok[order])
    slot_w = jnp.zeros((cap,), jnp.float32).at[dest].set(flat_w[order])
    blk_e = jnp.minimum(jnp.searchsorted(pends, jnp.arange(n_blk) * MOE_BLOCK, side='right'),
                        N_EXPERTS - 1)

    def run_block(args):
        tok, w, e = args
        rows = ht[tok]
        hid = jax.nn.silu(rows @ w_gate[e]) * (rows @ w_up[e])
        return ((hid @ w_down[e]) * w[:, None]).astype(jnp.float32)

    out = lax.map(run_block, (slot_tok.reshape(n_blk, MOE_BLOCK),
                              slot_w.reshape(n_blk, MOE_BLOCK), blk_e))
    y = jnp.zeros((n_tok, d), jnp.float32).at[slot_tok].add(out.reshape(cap, d))
    return y.reshape(bsz, t, d)


def setup_inputs(seed: int = 0) -> dict:
    key = jax.random.key(seed)
    ks = jax.random.split(key, 32)

    def nrm(k, shape, scale):
        return jax.random.normal(k, shape, jnp.float32) * scale

    n = jnp.arange(SSM_STATE, dtype=jnp.float32)
    return {
        'x': nrm(ks[0], (BATCH, SEQ, D_MODEL), 1.0),
        'c': nrm(ks[1], (BATCH, D_MODEL), 1.0),
        'positions': jnp.broadcast_to(jnp.arange(SEQ, dtype=jnp.int32)[None], (BATCH, SEQ)),
        'ada_w': nrm(ks[2], (DEPTH, D_MODEL, 6 * D_MODEL), 0.1 * D_MODEL ** -0.5),
        'ada_b': nrm(ks[3], (DEPTH, 6 * D_MODEL), 0.01),
        'w_in': nrm(ks[4], (DEPTH, D_MODEL, PROJ_WIDTH), D_MODEL ** -0.5),
        'gm_ln_g': 1.0 + nrm(ks[5], (DEPTH, A_WIDTH), 0.01),
        'gm_ln_b': nrm(ks[6], (DEPTH, A_WIDTH), 0.01),
        'gm_ws': nrm(ks[7], (DEPTH, A_HEADS, CHUNK, CHUNK), 0.5 * CHUNK ** -0.5),
        'gm_bs': 1.0 + nrm(ks[8], (DEPTH, A_HEADS, CHUNK), 0.01),
        'ssm_lam_re': -0.5 + nrm(ks[9], (DEPTH, SSM_GROUPS, SSM_STATE), 0.01),
        'ssm_lam_im': math.pi * n + nrm(ks[10], (DEPTH, SSM_GROUPS, SSM_STATE), 0.01),
        'ssm_log_dt': jax.random.uniform(ks[11], (DEPTH, SSM_GROUPS), jnp.float32,
                                         math.log(DT_MIN), math.log(DT_MAX)),
        'ssm_b_re': nrm(ks[12], (DEPTH, SSM_GROUPS, SSM_STATE, SSM_GROUP), (2 * SSM_GROUP) ** -0.5),
        'ssm_b_im': nrm(ks[13], (DEPTH, SSM_GROUPS, SSM_STATE, SSM_GROUP), (2 * SSM_GROUP) ** -0.5),
        'ssm_c_re': nrm(ks[14], (DEPTH, SSM_GROUPS, SSM_GROUP, SSM_STATE), SSM_STATE ** -0.5),
        'ssm_c_im': nrm(ks[15], (DEPTH, SSM_GROUPS, SSM_GROUP, SSM_STATE), SSM_STATE ** -0.5),
        'ssm_d': nrm(ks[16], (DEPTH, SSM_GROUPS, SSM_GROUP), 1.0),
        'glu_w': nrm(ks[17], (DEPTH, C_WIDTH, C_WIDTH), C_WIDTH ** -0.5),
        'glu_b': nrm(ks[18], (DEPTH, C_WIDTH), 0.01),
        'w_out': nrm(ks[19], (DEPTH, D_MIX, D_MODEL), BETA * D_MIX ** -0.5),
        'ln1_g': 1.0 + nrm(ks[20], (DEPTH, D_MODEL), 0.01),
        'ln1_b': nrm(ks[21], (DEPTH, D_MODEL), 0.01),
        'router_w': nrm(ks[22], (D_MODEL, N_EXPERTS), D_MODEL ** -0.5),
        'router_bias': nrm(ks[23], (N_EXPERTS,), 0.01),
        'exp_w_gate': nrm(ks[24], (DEPTH, N_EXPERTS, D_MODEL, D_EXPERT), D_MODEL ** -0.5),
        'exp_w_up': nrm(ks[25], (DEPTH, N_EXPERTS, D_MODEL, D_EXPERT), D_MODEL ** -0.5),
        'exp_w_down': nrm(ks[26], (DEPTH, N_EXPERTS, D_EXPERT, D_MODEL), BETA * D_EXPERT ** -0.5),
        'ln2_g': 1.0 + nrm(ks[27], (DEPTH, D_MODEL), 0.01),
        'ln2_b': nrm(ks[28], (DEPTH, D_MODEL), 0.01),
    }


def reference(x, c, positions, ada_w, ada_b, w_in, gm_ln_g, gm_ln_b, gm_ws, gm_bs,
              ssm_lam_re, ssm_lam_im, ssm_log_dt, ssm_b_re, ssm_b_im, ssm_c_re, ssm_c_im,
              ssm_d, glu_w, glu_b, w_out, ln1_g, ln1_b, router_w, router_bias,
              exp_w_gate, exp_w_up, exp_w_down, ln2_g, ln2_b):
    bsz, t, _ = x.shape
    cond = jax.nn.silu(c.astype(jnp.float32))
    for l in range(DEPTH):
        mod = cond @ ada_w[l] + ada_b[l]
        sh1, sc1, g1, sh2, sc2, g2 = [m[:, None, :] for m in jnp.split(mod, 6, axis=-1)]

        h = layer_norm(x) * (1.0 + sc1) + sh1
        proj = h @ w_in[l]
        a_uv, qkv, s_in = jnp.split(proj, [2 * A_WIDTH, 2 * A_WIDTH + 3 * B_WIDTH], axis=-1)
        a_out = chunked_spatial_gating(a_uv, gm_ln_g[l], gm_ln_b[l], gm_ws[l], gm_bs[l])
        qkv = qkv.reshape(bsz, t, 3, B_HEADS, B_HEAD_DIM)
        q = partial_rope(qkv[:, :, 0], positions)
        k = partial_rope(qkv[:, :, 1], positions)
        b_out = dilated_attention(q, k, qkv[:, :, 2]).reshape(bsz, t, B_WIDTH)
        c_out = s5_mixer(s_in, ssm_lam_re[l], ssm_lam_im[l], ssm_log_dt[l], ssm_b_re[l],
                         ssm_b_im[l], ssm_c_re[l], ssm_c_im[l], ssm_d[l], glu_w[l], glu_b[l])
        mix = jnp.concatenate([a_out, b_out, c_out], axis=-1) @ w_out[l]
        x = layer_norm(ALPHA * x + (1.0 + g1) * mix) * ln1_g[l] + ln1_b[l]

        h2 = layer_norm(x) * (1.0 + sc2) + sh2
        ffn = grouped_moe(h2, router_w, router_bias, exp_w_gate[l], exp_w_up[l], exp_w_down[l])
        x = layer_norm(ALPHA * x + (1.0 + g2) * ffn) * ln2_g[l] + ln2_b[l]
    return x
```

```python
import numpy as np
import concourse.bass as bass
import concourse.mybir as mybir

F32 = mybir.dt.float32
BF16 = mybir.dt.bfloat16
I32 = mybir.dt.int32
U32 = mybir.dt.uint32
AF = mybir.ActivationFunctionType
ALU = mybir.AluOpType
AX = mybir.AxisListType


RELAXED = ()
ALLOW_RELAX = True


class Buf:
    __slots__ = ("w", "r", "name")

    def __init__(self, name=""):
        self.w = None
        self.r = []
        self.name = name


class Ctx:
    def __init__(self, nc, strict_same=False):
        self.nc = nc
        self.strict_same = strict_same
        self.relaxed = set(RELAXED)
        self.engs = {"pe": nc.tensor, "act": nc.scalar, "dve": nc.vector, "pool": nc.gpsimd, "sp": nc.sync}
        self.sem = {}
        self.cnt = {}
        for e in ("pe", "act", "dve", "pool"):
            self.sem[e] = nc.alloc_semaphore("s_" + e)
            self.cnt[e] = 0
        self.dq = {}
        for q, n in (("sp", 10), ("act", 4), ("pool", 8)):
            self.dq[q] = {"sems": [nc.alloc_semaphore(f"d_{q}{i}") for i in range(n)], "vals": [0] * n, "k": 0}
        self.waited = {}
        self.nbuf = 0
        self.out_events = []

    def buf(self, name=""):
        return Buf(name)

    def _wait(self, eng, ev):
        sem, val = ev
        key = (eng, id(sem))
        if self.waited.get(key, 0) >= val:
            return
        self.engs[eng].wait_ge(sem, val)
        self.waited[key] = val

    def _deps(self, eng, reads, writes, relax=()):
        own = self.sem.get(eng)
        rl = set(id(b) for b in relax) if ALLOW_RELAX else set()

        def chk(b, ev):
            if ev[0] is own and (eng == "pe" or id(b) in rl):
                return
            self._wait(eng, ev)
        for b in reads:
            if b.w is not None:
                chk(b, b.w)
        for b in writes:
            if b.w is not None:
                chk(b, b.w)
            for ev in b.r:
                chk(b, ev)

    def _commit(self, ev, reads, writes):
        for b in writes:
            b.w = ev
            b.r = []
        for b in reads:
            b.r.append(ev)
            if len(b.r) > 24:
                b.r = b.r[-24:]

    def op(self, eng, fn, reads=(), writes=(), signal=True, relax=()):
        self._deps(eng, reads, writes, relax)
        inst = fn(self.engs[eng])
        if signal:
            self.cnt[eng] += 1
            inst.then_inc(self.sem[eng], 1)
            ev = (self.sem[eng], self.cnt[eng])
        else:
            ev = (self.sem[eng], self.cnt[eng] + 1)
        self._commit(ev, reads, writes)
        return ev

    def dma(self, q, out, in_, reads=(), writes=(), fn=None, **kw):
        d = self.dq[q]
        i = d["k"] % len(d["sems"])
        d["k"] += 1
        sem = d["sems"][i]
        self._deps(q, reads, writes)
        if d["vals"][i] > 0:
            self._wait(q, (sem, d["vals"][i]))
        if fn is None:
            inst = self.engs[q].dma_start(out=out, in_=in_, **kw)
        else:
            inst = fn(self.engs[q])
        d["vals"][i] += 16
        inst.then_inc(sem, 16)
        ev = (sem, d["vals"][i])
        self._commit(ev, reads, writes)
        return ev

    def barrier(self):
        evs = [(self.sem[e], self.cnt[e]) for e in self.sem if self.cnt[e] > 0]
        for q, d in self.dq.items():
            for sem, v in zip(d["sems"], d["vals"]):
                if v > 0:
                    evs.append((sem, v))
        for eng in ("pe", "act", "dve", "pool", "sp"):
            own = self.sem.get(eng)
            for ev in evs:
                self._wait(eng, ev)

    def finish(self, bufs):
        for b in bufs:
            if b.w is not None:
                self._wait("sp", b.w)
            for ev in b.r:
                self._wait("sp", ev)

from concourse.bass_utils import run_bass_kernel_spmd
import math
import contextlib

T = 4096
D = 1024
NCH = 32
PW = 2304
EPS = 1e-5
ALPHA = (2.0 * 2) ** 0.25
TWO_PI = 2.0 * math.pi
ROPE_THETA = 500000.0


class Tl:
    def __init__(self, h, name=""):
        self.h = h
        self.b = Buf(name)

    def __getitem__(self, k):
        return self.h[k]


class KB:
    def __init__(self, nc, nlayers=2, dbg=(), stop_after=None):
        self.nc = nc
        self.cx = Ctx(nc)
        self.dbg = set(dbg)
        self.stop_after = stop_after
        self.nlayers = nlayers
        self.outs = []
        self.stk = [contextlib.ExitStack()]
        self.nps = 0

    def inp(self, name, shape, dt=F32):
        self.in_names = getattr(self, "in_names", set())
        self.in_names.add(name)
        return Tl(self.nc.dram_tensor(name, list(shape), dt, kind="ExternalInput").ap(), name)

    def outp(self, name, shape, dt=F32):
        t = Tl(self.nc.dram_tensor(name, list(shape), dt, kind="ExternalOutput").ap(), name)
        self.outs.append(t)
        return t

    def scratch(self, name, shape, dt):
        return Tl(self.nc.dram_tensor(name, list(shape), dt, kind="Internal").ap(), name)

    def sb(self, name, shape, dt):
        self.nsb = getattr(self, "nsb", 0) + 1
        h = self.stk[-1].enter_context(self.nc.sbuf_tensor(f"{name}_{self.nsb}", list(shape), dt))
        return Tl(h, name)

    def push(self):
        self.stk.append(contextlib.ExitStack())

    def pop(self):
        self.cx.barrier()
        self.stk.pop().close()

    def ps(self, name, shape, dt=F32):
        return Tl(self.nc.alloc_psum_tensor(name, list(shape), dt), name)

    def _rw(self, r, w):
        return [t.b for t in r], [t.b for t in w]

    def V(self, fn, r=(), w=(), x=()):
        r, w = self._rw(r, w)
        return self.cx.op("dve", fn, r, w, relax=[t.b for t in x])

    def S(self, fn, r=(), w=(), x=()):
        r, w = self._rw(r, w)
        return self.cx.op("act", fn, r, w, relax=[t.b for t in x])

    def G(self, fn, r=(), w=(), x=()):
        r, w = self._rw(r, w)
        return self.cx.op("pool", fn, r, w, relax=[t.b for t in x])

    def P(self, fn, r=(), w=(), signal=True):
        r, w = self._rw(r, w)
        return self.cx.op("pe", fn, r, w, signal=signal)

    def dma(self, q, out, in_, r=(), w=(), **kw):
        r, w = self._rw(r, w)
        return self.cx.dma(q, out, in_, r, w, **kw)

    def mm(self, out, lhsT, rhs, r, w, start, stop, signal=None):
        if signal is None:
            signal = stop
        return self.P(lambda e: e.matmul(out, lhsT, rhs, start=start, stop=stop), r, w, signal=signal)

    def tr(self, out, in_, ident, r, w, signal=True):
        return self.P(lambda e: e.transpose(out, in_, ident), r, w, signal=signal)

    def declare_io(self):
        i = self.inp
        self.x_in = i("x", [T, D])
        self.ccol = i("ccol", [128, 8])
        self.pos = i("pos", [128, NCH], I32)
        self.ada_w = i("ada_w", [2, D, 6 * D])
        self.ada_b = i("ada_b", [2, 6 * D])
        self.w_in = i("w_in", [2, D, PW])
        self.gm_ln_g = i("gm_ln_g", [2, 256])
        self.gm_ln_b = i("gm_ln_b", [2, 256])
        self.gm_ws = i("gm_ws", [2, 4, 128, 128])
        self.gm_bsT = i("gm_bsT", [2, 128, 4])
        self.out = self.outp("out", [T, D])
        self.mixtok = self.scratch("mixtok", [T, 1024], BF16)
        self.vd = self.scratch("vd", [T, 520], BF16)

    def consts(self):
        nc = self.nc
        self.identb = self.sb("identb", [128, 128], BF16)
        self.identf = self.sb("identf", [128, 128], F32)
        self.onesf = self.sb("onesf", [128, 128], F32)
        self.eps_t = self.sb("eps_t", [128, 1], F32)
        self.G(lambda e: e.memset(self.onesf[:, :], 1.0), w=[self.onesf])
        self.G(lambda e: e.memset(self.eps_t[:, :], EPS), w=[self.eps_t])
        self.mhalf = self.sb("mhalf", [128, 1], F32)
        self.G(lambda e: e.memset(self.mhalf[:, :], -0.5), w=[self.mhalf])
        self.G(lambda e: e.affine_select(out=self.identf[:, :], in_=self.onesf[:, :], pattern=[[-1, 128]],
                                         compare_op=ALU.is_equal, fill=0.0, base=0, channel_multiplier=1),
               r=[self.onesf], w=[self.identf])
        self.G(lambda e: e.tensor_copy(out=self.identb[:, :], in_=self.identf[:, :]), r=[self.identf], w=[self.identb])
        self.posf = self.sb("posf", [128, NCH], F32)
        self.posi = self.sb("posi", [128, NCH], I32)
        self.dma("sp", self.posi[:, :], self.pos[:, :], r=[self.pos], w=[self.posi])
        self.V(lambda e: e.tensor_copy(out=self.posf[:, :], in_=self.posi[:, :]), r=[self.posi], w=[self.posf])
        self.cs = self.sb("cs", [128, NCH, 8], F32)
        self.sn = self.sb("sn", [128, NCH, 8], F32)
        self.push()
        ang = self.sb("ang", [128, NCH, 8], F32)
        tmp = self.sb("angt", [128, NCH, 8], F32)
        tmi = self.sb("angi", [128, NCH, 8], I32)
        for j in range(8):
            fr = ROPE_THETA ** (-(j * 2.0) / 16.0)
            self.V(lambda e, j=j, fr=fr: e.tensor_scalar(out=ang[:, :, j], in0=self.posf[:, :], scalar1=float(fr), scalar2=None, op0=ALU.mult),
                   r=[self.posf], w=[ang])
        self.sincos(ang, self.sn, self.cs, tmp, tmi, [128, NCH * 8])
        self.pop()

    def _flat(self, t):
        ap = t[:]
        if len(ap.shape) == 2:
            return ap
        names = " ".join(f"a{i}" for i in range(len(ap.shape) - 1))
        return ap.rearrange(f"p {names} -> p ({names})")

    def range_reduce(self, src, dst, tmp, tmi, shift):
        s, d, t, ti = self._flat(src), self._flat(dst), self._flat(tmp), self._flat(tmi)
        self.V(lambda e: e.tensor_scalar(out=t, in0=s, scalar1=float(shift), scalar2=float(1.0 / TWO_PI), op0=ALU.add, op1=ALU.mult),
               r=[src], w=[tmp])
        self.V(lambda e: e.tensor_copy(out=ti, in_=t), r=[tmp], w=[tmi])
        self.V(lambda e: e.tensor_copy(out=t, in_=ti), r=[tmi], w=[tmp])
        self.V(lambda e: e.tensor_scalar(out=t, in0=t, scalar1=float(-TWO_PI), scalar2=float(shift), op0=ALU.mult, op1=ALU.add),
               r=[tmp], w=[tmp])
        self.V(lambda e: e.tensor_tensor(out=d, in0=t, in1=s, op=ALU.add), r=[tmp, src], w=[dst])
        self.V(lambda e: e.tensor_scalar(out=t, in0=d, scalar1=float(math.pi), scalar2=float(-TWO_PI), op0=ALU.is_gt, op1=ALU.mult),
               r=[dst], w=[tmp])
        self.V(lambda e: e.tensor_tensor(out=d, in0=d, in1=t, op=ALU.add), r=[tmp, dst], w=[dst])
        self.V(lambda e: e.tensor_scalar(out=t, in0=d, scalar1=float(-math.pi), scalar2=float(TWO_PI), op0=ALU.is_lt, op1=ALU.mult),
               r=[dst], w=[tmp])
        self.V(lambda e: e.tensor_tensor(out=d, in0=d, in1=t, op=ALU.add), r=[tmp, dst], w=[dst])
        self.V(lambda e: e.tensor_scalar(out=d, in0=d, scalar1=float(math.pi), scalar2=float(-math.pi), op0=ALU.min, op1=ALU.max),
               r=[dst], w=[dst])

    def sincos(self, ang, sn, cs, tmp, tmi, shape):
        self.range_reduce(ang, sn, tmp, tmi, 0.0)
        self.S(lambda e: e.activation(out=self._flat(sn), in_=self._flat(sn), func=AF.Sin), r=[sn], w=[sn])
        self.range_reduce(ang, cs, tmp, tmi, math.pi / 2)
        self.S(lambda e: e.activation(out=self._flat(cs), in_=self._flat(cs), func=AF.Sin), r=[cs], w=[cs])

    def setup(self):
        self.bank = [self.ps(f"bank{i}", [128, 512], F32) for i in range(8)]
        self.consts()

    def layer_alloc(self):
        self.modp = self.sb("modp", [128, 4, 8], F32)

        self.wsT = self.sb("wsT", [128, 4, 128], BF16)
        self.gbs = self.sb("gbs", [128, 4], F32)
        self.glng = self.sb("glng", [128, 256], F32)
        self.glnb = self.sb("glnb", [128, 256], F32)

    def prep_alloc(self):
        self.adaw = [self.sb(f"adaw{i}", [128, 8, 512], F32) for i in range(2)]
        self.adab = [self.sb(f"adab{i}", [128, 512], F32) for i in range(2)]
        self.modc = [self.sb(f"modc{i}", [128, 512], F32) for i in range(2)]
        self.wtmp = self.sb("wtmp", [128, 4, 128], F32)
        ccs = self.sb("ccs", [128, 8], F32)
        self.dma("sp", ccs[:, :], self.ccol[:, :], r=[self.ccol], w=[ccs])
        self.S(lambda e: e.activation(out=ccs[:, :], in_=ccs[:, :], func=AF.Silu), r=[ccs], w=[ccs])
        self.condrep = self.sb("condrep", [128, 8, 128], F32)
        self.V(lambda e: e.tensor_copy(out=self.condrep[:, :, :], in_=ccs[:, :].unsqueeze(2).to_broadcast([128, 8, 128])),
               r=[ccs], w=[self.condrep])


    def load_win(self, l):
        self.win = self.sb("win", [128, 8, PW], BF16)
        for k in range(8):
            self.dma("pool", self.win[:, k, :], self.w_in[l, k * 128:(k + 1) * 128, :], r=[self.w_in], w=[self.win])

    def layer_prep(self, l, part=0):
        self.push()
        self.prep_alloc()
        pb = self.bank[7]
        pt = self.bank[6]
        for n in range(12):
            if (part == 0) != (n // 2 in (0, 1)):
                continue
            aw = self.adaw[n % 2]
            ab = self.adab[n % 2]
            mc = self.modc[n % 2]
            self.dma("sp", aw[:, :, :], self.ada_w[l, :, n * 512:(n + 1) * 512].rearrange("(k p) n -> p k n", p=128),
                     r=[self.ada_w], w=[aw])
            self.dma("sp", ab[:, :], self.ada_b[l, n * 512:(n + 1) * 512].partition_broadcast(128), r=[self.ada_b], w=[ab])
            for k in range(8):
                self.mm(pb[:, :], self.condrep[:, k, :], aw[:, k, :], r=[self.condrep, aw], w=[pb], start=(k == 0), stop=(k == 7))
            which, half = n // 2, n % 2
            if which in (2, 5, 3, 4):
                tgt = self.opg if which in (2, 5) else self.opg2
                gi = {2: 0, 5: 1, 3: 0, 4: 1}[which]
                dst = tgt[:, gi, half * 512:(half + 1) * 512]
                self.V(lambda e, dst=dst: e.tensor_tensor(out=dst, in0=pb[:, :], in1=ab[:, :], op=ALU.add), r=[pb, ab], w=[tgt])
                if which != 3:
                    self.V(lambda e, dst=dst: e.tensor_scalar(out=dst, in0=dst, scalar1=1.0, scalar2=None, op0=ALU.add), r=[tgt], w=[tgt])
            else:
                slot = {0: 0, 1: 1}[which]
                self.V(lambda e: e.tensor_tensor(out=mc[:, :], in0=pb[:, :], in1=ab[:, :], op=ALU.add), r=[pb, ab], w=[mc])
                for b4 in range(4):
                    self.tr(pt[:, b4 * 128:(b4 + 1) * 128], mc[:, b4 * 128:(b4 + 1) * 128], self.identf[:, :], r=[mc, self.identf], w=[pt])
                addc = 1.0 if slot in (1, 3) else 0.0
                for b4 in range(4):
                    self.V(lambda e, b4=b4: e.tensor_scalar(out=self.modp[:, slot, half * 4 + b4:half * 4 + b4 + 1],
                                                            in0=pt[:, b4 * 128:b4 * 128 + 1], scalar1=float(addc), scalar2=None, op0=ALU.add),
                           r=[pt], w=[self.modp])
        if part == 0:
            self.dma("sp", self.wtmp[:, :, :], self.gm_ws[l].rearrange("h t s -> t h s"), r=[self.gm_ws], w=[self.wtmp])
            self.G(lambda e: e.affine_select(out=self.wtmp[:, :, :], in_=self.wtmp[:, :, :], pattern=[[0, 4], [-1, 128]],
                                             compare_op=ALU.is_ge, fill=0.0, base=0, channel_multiplier=1), r=[self.wtmp], w=[self.wtmp])
            for h in range(4):
                self.tr(pt[:, h * 128:(h + 1) * 128], self.wtmp[:, h, :], self.identf[:, :], r=[self.wtmp, self.identf], w=[pt])
            self.V(lambda e: e.tensor_copy(out=self.wsT[:, :, :], in_=pt[:, :].rearrange("p (h t) -> p h t", h=4)), r=[pt], w=[self.wsT])
            self.dma("sp", self.gbs[:, :], self.gm_bsT[l], r=[self.gm_bsT], w=[self.gbs])
            self.dma("sp", self.glng[:, :], self.gm_ln_g[l].partition_broadcast(128), r=[self.gm_ln_g], w=[self.glng])

            self.dma("sp", self.glnb[:, :], self.gm_ln_b[l].partition_broadcast(128), r=[self.gm_ln_b], w=[self.glnb])
        self.pop()

    def ln_stats(self, src, src_ap_fn, n, st, act=False):
        if act:
            return self.ln_stats_act(src, src_ap_fn, n, st)
        nchk = (n + 511) // 512
        w = n // nchk
        for i in range(nchk):
            self.V(lambda e, i=i: e.bn_stats(out=st[:, 8 + i * 6:8 + (i + 1) * 6], in_=src_ap_fn(i * w, (i + 1) * w)), r=[src], w=[st])
        self.V(lambda e: e.bn_aggr(out=st[:, 0:2], in_=st[:, 8:8 + 6 * nchk]), r=[st], w=[st])
        self.V(lambda e: e.tensor_scalar(out=st[:, 2:3], in0=st[:, 1:2], scalar1=float(EPS), scalar2=None, op0=ALU.add), r=[st], w=[st])
        self.G(lambda e: e.tensor_tensor(out=st[:, 3:4], in0=st[:, 2:3], in1=self.mhalf[:, 0:1], op=ALU.pow), r=[st, self.mhalf], w=[st])
        self.V(lambda e: e.tensor_scalar(out=st[:, 4:5], in0=st[:, 0:1], scalar1=-1.0, scalar2=st[:, 3:4], op0=ALU.mult, op1=ALU.mult), r=[st], w=[st])

    def ln_stats_act(self, src, src_ap_fn, n, st):
        nchk = (n + 511) // 512
        w = n // nchk
        for i in range(nchk):
            self.V(lambda e, i=i: e.bn_stats(out=st[:, 8 + i * 6:8 + (i + 1) * 6], in_=src_ap_fn(i * w, (i + 1) * w)), r=[src], w=[st])
        self.V(lambda e: e.bn_aggr(out=st[:, 0:2], in_=st[:, 8:8 + 6 * nchk]), r=[st], w=[st])
        self.S(lambda e: e.activation(out=st[:, 2:3], in_=st[:, 1:2], func=AF.Sqrt, bias=self.eps_t[:, 0:1], scale=1.0), r=[st, self.eps_t], w=[st])
        self.V(lambda e: e.reciprocal(out=st[:, 3:4], in_=st[:, 2:3]), r=[st], w=[st])
        self.V(lambda e: e.tensor_scalar(out=st[:, 4:5], in0=st[:, 0:1], scalar1=-1.0, scalar2=st[:, 3:4], op0=ALU.mult, op1=ALU.mult), r=[st], w=[st])

    def qk_alloc(self):
        self.QT = self.sb("QT", [128, 4, T], BF16)
        self.KT = self.sb("KT", [128, 4, T], BF16)

    def p1_alloc(self):
        s = self.sb
        self.xt = [s(f"xt{i}", [128, D], F32) for i in range(2)]
        self.xn = [s(f"xn{i}", [128, D], BF16) for i in range(2)]
        self.st = [s(f"st{i}", [128, 24], F32) for i in range(2)]
        self.st2 = [s(f"stb{i}", [128, 24], F32) for i in range(2)]
        self.hT = [s(f"hT{i}", [128, 8, 128], BF16) for i in range(2)]
        self.gl = [s(f"gl{i}", [128, 512], F32) for i in range(1)] * 2
        self.vnb = [s(f"vnb{i}", [128, 256], F32) for i in range(1)] * 2
        self.vb = [s(f"vb{i}", [128, 256], BF16) for i in range(1)] * 2
        self.aout = [s(f"aout{i}", [128, 256], BF16) for i in range(1)] * 2
        self.qf = [s(f"qf{i}", [128, 2, 512], F32) for i in range(1)] * 2
        self.qb = [s(f"qb{i}", [128, 2, 512], BF16) for i in range(1)] * 2
        self.rt = [s(f"rt{i}", [128, 4, 8, 8], F32) for i in range(1)] * 2
        self.vp = [s(f"vp{i}", [128, 8, 65], BF16) for i in range(1)] * 2
        for i in range(1):
            self.G(lambda e, i=i: e.memset(self.vp[i][:, :, :], 1.0), w=[self.vp[i]])

    def p1_chunk(self, l, j, x_src):
        i2 = j % 2
        xt, xn, st, st2, hT = self.xt[i2], self.xn[i2], self.st[i2], self.st2[i2], self.hT[i2]
        gl, vnb, vb, aout, qf, qb, rt, vp = self.gl[i2], self.vnb[i2], self.vb[i2], self.aout[i2], self.qf[i2], self.qb[i2], self.rt[i2], self.vp[i2]
        B = self.bank
        rows = slice(j * 128, (j + 1) * 128)
        if j == 0:
            self.dma("sp", xt[:, :], x_src[rows, :], r=[x_src], w=[xt])
        if j + 1 < NCH:
            xtn = self.xt[(j + 1) % 2]
            self.dma("sp", xtn[:, :], x_src[(j + 1) * 128:(j + 2) * 128, :], r=[x_src], w=[xtn])
        self.ln_stats(xt, lambda a, b: xt[:, a:b], D, st)
        self.S(lambda e: e.activation(out=xn[:, :], in_=xt[:, :], func=AF.Identity, bias=st[:, 4:5], scale=st[:, 3:4]), r=[xt, st], w=[xn])
        pT = B[0]
        pTb = pT[:, :].bitcast(BF16).rearrange("p (k t) -> p k t", k=8)
        for k in range(8):
            self.tr(pTb[:, k, :], xn[:, k * 128:(k + 1) * 128], self.identb[:, :], r=[xn, self.identb], w=[pT], signal=(k == 7))
        for k in range(8):
            self.V(lambda e, k=k: e.tensor_scalar(out=hT[:, k, :], in0=pTb[:, k, :], scalar1=self.modp[:, 1, k:k + 1], scalar2=self.modp[:, 0, k:k + 1],
                                                  op0=ALU.mult, op1=ALU.add), r=[pT, self.modp], w=[hT], x=[hT])
        for bi, c0 in ((1, 0), (2, 512), (3, 1024), (4, 1536)):
            for k in range(8):
                self.mm(B[bi][:, :], hT[:, k, :], self.win[:, k, c0:c0 + 512], r=[hT, self.win], w=[B[bi]], start=(k == 0), stop=(k == 7))
        self.S(lambda e: e.activation(out=gl[:, :], in_=B[1][:, :], func=AF.Gelu_apprx_tanh), r=[B[1]], w=[gl])
        self.ln_stats(gl, lambda a, b: gl[:, 256 + a:256 + b], 256, st2)
        self.V(lambda e: e.tensor_scalar(out=vnb[:, :], in0=gl[:, 256:512], scalar1=st2[:, 3:4], scalar2=st2[:, 4:5], op0=ALU.mult, op1=ALU.add),
               r=[gl, st2], w=[vnb])
        self.V(lambda e: e.tensor_tensor(out=vnb[:, :], in0=vnb[:, :], in1=self.glng[:, :], op=ALU.mult), r=[vnb, self.glng], w=[vnb], x=[vnb])
        self.V(lambda e: e.tensor_tensor(out=vb[:, :], in0=vnb[:, :], in1=self.glnb[:, :], op=ALU.add), r=[vnb, self.glnb], w=[vb], x=[vnb])
        psv = B[5]
        for h in range(4):
            self.mm(psv[:, h * 64:(h + 1) * 64], self.wsT[:, h, :], vb[:, h * 64:(h + 1) * 64], r=[self.wsT, vb], w=[psv], start=True, stop=True,
                    signal=(h == 3))
        for h in range(4):
            self.V(lambda e, h=h: e.scalar_tensor_tensor(out=aout[:, h * 64:(h + 1) * 64], in0=psv[:, h * 64:(h + 1) * 64], scalar=self.gbs[:, h:h + 1],
                                                         in1=gl[:, h * 64:(h + 1) * 64], op0=ALU.add, op1=ALU.mult), r=[psv, self.gbs, gl], w=[aout], x=[aout])
        self.dma("sp", self.mixtok[rows, 0:256], aout[:, :], r=[aout], w=[self.mixtok])
        self.S(lambda e: e.copy(out=qf[:, 0, :], in_=B[2][:, :]), r=[B[2]], w=[qf])
        self.S(lambda e: e.copy(out=qf[:, 1, :], in_=B[3][:, :]), r=[B[3]], w=[qf], x=[qf])
        q4 = qf[:, :, :].rearrange("p a (h e) -> p (a h) e", e=64)
        o4 = qb[:, :, :].rearrange("p a (h e) -> p (a h) e", e=64)
        for a in range(2):
            xa1, xa2 = q4[:, a * 8:(a + 1) * 8, 0:8], q4[:, a * 8:(a + 1) * 8, 8:16]
            cb = self.cs[:, j, :].unsqueeze(1).to_broadcast([128, 8, 8])
            sb_ = self.sn[:, j, :].unsqueeze(1).to_broadcast([128, 8, 8])
            oa = o4[:, a * 8:(a + 1) * 8, :]
            self.G(lambda e, xa1=xa1, cb=cb: e.tensor_tensor(out=rt[:, 0, :, :], in0=xa1, in1=cb, op=ALU.mult), r=[qf, self.cs], w=[rt])
            self.G(lambda e, xa2=xa2, sb_=sb_: e.tensor_tensor(out=rt[:, 1, :, :], in0=xa2, in1=sb_, op=ALU.mult), r=[qf, self.sn], w=[rt])
            self.G(lambda e, xa2=xa2, cb=cb: e.tensor_tensor(out=rt[:, 2, :, :], in0=xa2, in1=cb, op=ALU.mult), r=[qf, self.cs], w=[rt])
            self.G(lambda e, xa1=xa1, sb_=sb_: e.tensor_tensor(out=rt[:, 3, :, :], in0=xa1, in1=sb_, op=ALU.mult), r=[qf, self.sn], w=[rt])
            self.G(lambda e, oa=oa: e.tensor_tensor(out=oa[:, :, 0:8], in0=rt[:, 0, :, :], in1=rt[:, 1, :, :], op=ALU.subtract), r=[rt], w=[qb])
            self.G(lambda e, oa=oa: e.tensor_tensor(out=oa[:, :, 8:16], in0=rt[:, 2, :, :], in1=rt[:, 3, :, :], op=ALU.add), r=[rt], w=[qb])
            self.G(lambda e, oa=oa, a=a: e.tensor_copy(out=oa[:, :, 16:64], in_=q4[:, a * 8:(a + 1) * 8, 16:64]), r=[qf], w=[qb])
        pq = B[6]
        pqb = pq[:, :].bitcast(BF16).rearrange("p (a k t) -> p a k t", a=2, k=4)
        for a in range(2):
            for k in range(4):
                self.tr(pqb[:, a, k, :], qb[:, a, k * 128:(k + 1) * 128], self.identb[:, :], r=[qb, self.identb], w=[pq], signal=(a == 1 and k == 3))
        self.S(lambda e: e.copy(out=self.QT[:, :, j * 128:(j + 1) * 128], in_=pqb[:, 0, :, :]), r=[pq], w=[self.QT])
        self.S(lambda e: e.copy(out=self.KT[:, :, j * 128:(j + 1) * 128], in_=pqb[:, 1, :, :]), r=[pq], w=[self.KT])
        self.S(lambda e: e.copy(out=vp[:, :, 0:64], in_=B[4][:, :].rearrange("p (h e) -> p h e", e=64)), r=[B[4]], w=[vp])
        self.dma("sp", self.vd[rows, :], vp[:, :, :].rearrange("p h e -> p (h e)"), r=[vp], w=[self.vd])

    def finish(self):
        bufs = [t.b for t in self.outs]
        self.cx.finish(bufs)

    def p2_alloc(self):
        s = self.sb
        self.vbr = [s(f"vbr{i}", [128, 32, 520], BF16) for i in range(2)]
        self.negm = s("negm", [128, 256], BF16)
        negf = s("negf", [128, 256], F32)
        zf = s("zf", [128, 256], F32)
        self.G(lambda e: e.memset(zf[:, :], 0.0), w=[zf])
        self.G(lambda e: e.affine_select(out=negf[:, 0:128], in_=zf[:, 0:128], pattern=[[-1, 128]], compare_op=ALU.is_ge, fill=-30000.0,
                                         base=0, channel_multiplier=1), r=[zf], w=[negf])
        self.G(lambda e: e.affine_select(out=negf[:, 128:256], in_=zf[:, 128:256], pattern=[[1, 128]], compare_op=ALU.is_ge, fill=-30000.0,
                                         base=0, channel_multiplier=-1), r=[zf], w=[negf])
        self.G(lambda e: e.tensor_copy(out=self.negm[:, :], in_=negf[:, :]), r=[negf], w=[self.negm])
        self.m01 = s("m01", [128, 256], BF16)
        onef = s("onef2", [128, 256], F32)
        self.G(lambda e: e.memset(onef[:, :], 1.0), w=[onef])
        self.G(lambda e: e.affine_select(out=onef[:, 0:128], in_=onef[:, 0:128], pattern=[[-1, 128]], compare_op=ALU.is_ge, fill=0.0,
                                         base=0, channel_multiplier=1), r=[onef], w=[onef])
        self.G(lambda e: e.affine_select(out=onef[:, 128:256], in_=onef[:, 128:256], pattern=[[1, 128]], compare_op=ALU.is_ge, fill=0.0,
                                         base=0, channel_multiplier=-1), r=[onef], w=[onef])
        self.G(lambda e: e.tensor_copy(out=self.m01[:, :], in_=onef[:, :]), r=[onef], w=[self.m01])
        self.pexp = [s(f"pexp{i}", [128, 256], BF16) for i in range(6)]
        self.osb = [s(f"osb{i}", [128, 520], F32) for i in range(2)]

    def p2_attention(self):
        B = self.bank
        hb = 0
        self._hb = 0
        dils = (1, 4, 16)

        def load_vb(bi):
            d = dils[bi]
            vb_ = self.vbr[bi % 2]
            src = self.vd[:, :].rearrange("(n l r) c -> l n r c", l=128, r=d)
            for n in range(T // (128 * d)):
                self.dma("sp", vb_[:, n * d:(n + 1) * d, :], src[:, n, :, :], r=[self.vd], w=[vb_])
        load_vb(0)
        load_vb(1)
        for bi, d in enumerate(dils):
            seg = 128 * d
            nseg = T // seg
            vb = self.vbr[bi % 2]
            if bi == 1:
                load_vb(2)
            odst = self.obr[bi][:, :].rearrange("(n l r) c -> l n r c", l=128, r=d)
            blk = 0
            for n in range(nseg):
                for r_ in range(d):
                    cols = slice(n * seg + r_, (n + 1) * seg, d)
                    pcols = slice((n - 1) * seg + r_, n * seg, d)
                    bcur = n * d + r_
                    bprev = (n - 1) * d + r_
                    po = [B[6], B[7]]
                    osb = self.osb[blk % 2]
                    def scores(h):
                        nonlocal hb
                        hp, p0 = h // 2, (h % 2) * 64
                        ps = B[hb % 6]
                        o0 = 0
                        pe_ = self.pexp[hb % 6]
                        hb += 1
                        c0 = 0 if n > 0 else 128
                        if n > 0:
                            self.mm(ps[:, o0:o0 + 128], self.KT[p0:p0 + 64, hp, pcols], self.QT[p0:p0 + 64, hp, cols], r=[self.KT, self.QT], w=[ps],
                                    start=True, stop=True, signal=False)
                        self.mm(ps[:, o0 + 128:o0 + 256], self.KT[p0:p0 + 64, hp, cols], self.QT[p0:p0 + 64, hp, cols], r=[self.KT, self.QT], w=[ps],
                                start=True, stop=True)
                        self.S(lambda e, pe_=pe_, ps=ps, c0=c0, o0=o0: e.activation(out=pe_[:, c0:256], in_=ps[:, o0 + c0:o0 + 256], func=AF.Exp, scale=0.125),
                               r=[ps], w=[pe_])
                        mk = self.V if (h % 2 == 0) else self.G
                        mk(lambda e, pe_=pe_, c0=c0: e.tensor_tensor(out=pe_[:, c0:256], in0=pe_[:, c0:256], in1=self.m01[:, c0:256], op=ALU.mult),
                           r=[pe_, self.m01], w=[pe_])
                        return pe_

                    def pv(h, pe_):
                        pob = po[h // 4]
                        oc = slice((h % 4) * 65, (h % 4) * 65 + 65)
                        if n > 0:
                            self.mm(pob[:, oc], pe_[:, 0:128], vb[:, bprev, h * 65:(h + 1) * 65], r=[pe_, vb], w=[pob], start=True, stop=False)
                        self.mm(pob[:, oc], pe_[:, 128:256], vb[:, bcur, h * 65:(h + 1) * 65], r=[pe_, vb], w=[pob], start=(n == 0), stop=True)
                    pend = []
                    for h in range(8):
                        pend.append((h, scores(h)))
                        if len(pend) > 4:
                            pv(*pend.pop(0))
                    while pend:
                        pv(*pend.pop(0))
                    self.V(lambda e, osb=osb, po=po: e.tensor_copy(out=osb[:, 0:260], in_=po[0][:, 0:260]), r=[po[0]], w=[osb])
                    self.V(lambda e, osb=osb, po=po: e.tensor_copy(out=osb[:, 260:520], in_=po[1][:, 0:260]), r=[po[1]], w=[osb])
                    self.dma("sp", odst[:, n, r_, :], osb[:, :], r=[osb], w=[self.obr[bi]])
                    blk += 1

    def s5_io(self):
        i = self.inp
        self.lam_re = i("lam_re", [2, 1024])
        self.lam_im = i("lam_im", [2, 1024])
        self.log_dt = i("log_dt", [2, 16])
        self.ssm_bT = i("ssm_bT", [2, 2, 128, 2, 64])
        self.ssm_cT = i("ssm_cT", [2, 16, 128, 16])
        self.ssm_dT = i("ssm_dT", [2, 128, 2])
        self.glu_w = i("glu_w", [2, 256, 256])
        self.glu_bT = i("glu_bT", [2, 128, 2])
        self.coutT = self.scratch("coutT", [256, T], BF16)
        self.obr = [self.scratch(f"obr{i}", [T, 520], F32) for i in range(3)]

    def s5_alloc(self):
        s = self.sb
        self.Bblk = s("Bblk", [128, 2, 8, 2, 64], BF16)
        self.Cblk = s("Cblk", [128, 16, 128], BF16)
        self.Pre = s("Pre", [128, 16, 64], F32)
        self.PsT = s("PsT", [128, 16, 2, 64], F32)
        self.Qre = s("Qre", [128, 16, 64], F32)
        self.QsT = s("QsT", [128, 16, 2, 64], F32)
        self.glubh = s("glubh", [128, 2], F32)
        self.TriT = s("TriT", [128, 128], BF16)
        self.ones1 = s("ones1", [1, 128], BF16)
        self.dTt = s("dTt", [128, 2], F32)
        self.gluw = s("gluw", [128, 2, 256], BF16)
        self.glub = s("glub", [128, 2], F32)

    def s5_prep(self, l):
        s = self.sb
        V, S, G = self.V, self.S, self.G
        self.push()
        lre = s("lre", [128, 16, 64], F32)
        lim = s("lim", [128, 16, 64], F32)
        ldt = s("ldt", [128, 16], F32)
        self.dma("sp", lre[:, :, :].rearrange("p g n -> p (g n)"), self.lam_re[l].partition_broadcast(128), r=[self.lam_re], w=[lre])
        self.dma("sp", lim[:, :, :].rearrange("p g n -> p (g n)"), self.lam_im[l].partition_broadcast(128), r=[self.lam_im], w=[lim])
        self.dma("sp", ldt[:, :], self.log_dt[l].partition_broadcast(128), r=[self.log_dt], w=[ldt])
        S(lambda e: e.activation(out=ldt[:, :], in_=ldt[:, :], func=AF.Exp), r=[ldt], w=[ldt])
        dtb = ldt[:, :].unsqueeze(2).to_broadcast([128, 16, 64])
        lrd = s("lrd", [128, 16, 64], F32)
        lid = s("lid", [128, 16, 64], F32)
        V(lambda e: e.tensor_tensor(out=lrd[:, :, :], in0=lre[:, :, :], in1=dtb, op=ALU.mult), r=[lre, ldt], w=[lrd])
        V(lambda e: e.tensor_tensor(out=lid[:, :, :], in0=lim[:, :, :], in1=dtb, op=ALU.mult), r=[lim, ldt], w=[lid])
        sp1 = s("sp1", [128, 1], F32)
        G(lambda e: e.iota(sp1[:, :], pattern=[[0, 1]], base=1, channel_multiplier=1, allow_small_or_imprecise_dtypes=True), w=[sp1])
        E = s("E", [128, 16, 64], F32)
        An = s("An", [128, 16, 64], F32)
        sA = s("sA", [128, 16, 64], F32)
        cA = s("cA", [128, 16, 64], F32)
        tmp = s("s5tmp", [128, 16, 64], F32)
        tmi = s("s5tmi", [128, 16, 64], I32)
        qm = s("qm", [128, 16, 64], F32)
        pm = s("pm", [128, 16, 64], F32)
        V(lambda e: e.tensor_scalar(out=E[:, :, :], in0=lrd[:, :, :], scalar1=sp1[:, 0:1], scalar2=None, op0=ALU.mult), r=[lrd, sp1], w=[E])
        V(lambda e: e.tensor_scalar(out=An[:, :, :], in0=lid[:, :, :], scalar1=sp1[:, 0:1], scalar2=None, op0=ALU.mult), r=[lid, sp1], w=[An])
        self.sincos(An, sA, cA, tmp, tmi, None)
        S(lambda e: e.activation(out=qm[:, :, :], in_=E[:, :, :], func=AF.Exp), r=[E], w=[qm])
        S(lambda e: e.activation(out=pm[:, :, :], in_=E[:, :, :], func=AF.Exp, scale=-1.0), r=[E], w=[pm])
        V(lambda e: e.tensor_tensor(out=self.Qre[:, :, :], in0=qm[:, :, :], in1=cA[:, :, :], op=ALU.mult), r=[qm, cA], w=[self.Qre])
        V(lambda e: e.tensor_tensor(out=self.QsT[:, :, 1, :], in0=qm[:, :, :], in1=sA[:, :, :], op=ALU.mult), r=[qm, sA], w=[self.QsT])
        V(lambda e: e.tensor_scalar(out=self.QsT[:, :, 0, :], in0=self.QsT[:, :, 1, :], scalar1=-1.0, scalar2=None, op0=ALU.mult), r=[self.QsT], w=[self.QsT])
        V(lambda e: e.tensor_tensor(out=self.Pre[:, :, :], in0=pm[:, :, :], in1=cA[:, :, :], op=ALU.mult), r=[pm, cA], w=[self.Pre])
        V(lambda e: e.tensor_tensor(out=self.PsT[:, :, 0, :], in0=pm[:, :, :], in1=sA[:, :, :], op=ALU.mult), r=[pm, sA], w=[self.PsT])
        V(lambda e: e.tensor_scalar(out=self.PsT[:, :, 1, :], in0=self.PsT[:, :, 0, :], scalar1=-1.0, scalar2=None, op0=ALU.mult), r=[self.PsT], w=[self.PsT])
        self.sincos(lid, sA, cA, tmp, tmi, None)
        S(lambda e: e.activation(out=qm[:, :, :], in_=lrd[:, :, :], func=AF.Exp), r=[lrd], w=[qm])
        nr, ni = E, An
        V(lambda e: e.tensor_tensor(out=nr[:, :, :], in0=qm[:, :, :], in1=cA[:, :, :], op=ALU.mult), r=[qm, cA], w=[nr])
        V(lambda e: e.tensor_scalar(out=nr[:, :, :], in0=nr[:, :, :], scalar1=-1.0, scalar2=None, op0=ALU.add), r=[nr], w=[nr])
        V(lambda e: e.tensor_tensor(out=ni[:, :, :], in0=qm[:, :, :], in1=sA[:, :, :], op=ALU.mult), r=[qm, sA], w=[ni])
        m2 = pm
        V(lambda e: e.tensor_tensor(out=m2[:, :, :], in0=lre[:, :, :], in1=lre[:, :, :], op=ALU.mult), r=[lre], w=[m2])
        V(lambda e: e.tensor_tensor(out=tmp[:, :, :], in0=lim[:, :, :], in1=lim[:, :, :], op=ALU.mult), r=[lim], w=[tmp])
        V(lambda e: e.tensor_tensor(out=m2[:, :, :], in0=m2[:, :, :], in1=tmp[:, :, :], op=ALU.add), r=[m2, tmp], w=[m2])
        V(lambda e: e.reciprocal(out=m2[:, :, :], in_=m2[:, :, :]), r=[m2], w=[m2])
        fre, fim = sA, cA
        t1 = s("ft1", [128, 16, 64], F32)
        t2 = s("ft2", [128, 16, 64], F32)
        V(lambda e: e.tensor_tensor(out=t1[:, :, :], in0=nr[:, :, :], in1=lre[:, :, :], op=ALU.mult), r=[nr, lre], w=[t1])
        V(lambda e: e.tensor_tensor(out=t2[:, :, :], in0=ni[:, :, :], in1=lim[:, :, :], op=ALU.mult), r=[ni, lim], w=[t2])
        V(lambda e: e.tensor_tensor(out=t1[:, :, :], in0=t1[:, :, :], in1=t2[:, :, :], op=ALU.add), r=[t1, t2], w=[t1])
        V(lambda e: e.tensor_tensor(out=fre[:, :, :], in0=t1[:, :, :], in1=m2[:, :, :], op=ALU.mult), r=[t1, m2], w=[fre])
        V(lambda e: e.tensor_tensor(out=t1[:, :, :], in0=ni[:, :, :], in1=lre[:, :, :], op=ALU.mult), r=[ni, lre], w=[t1])
        V(lambda e: e.tensor_tensor(out=t2[:, :, :], in0=nr[:, :, :], in1=lim[:, :, :], op=ALU.mult), r=[nr, lim], w=[t2])
        V(lambda e: e.tensor_tensor(out=t1[:, :, :], in0=t1[:, :, :], in1=t2[:, :, :], op=ALU.subtract), r=[t1, t2], w=[t1])
        V(lambda e: e.tensor_tensor(out=fim[:, :, :], in0=t1[:, :, :], in1=m2[:, :, :], op=ALU.mult), r=[t1, m2], w=[fim])
        bT = s("bTt", [128, 2, 2, 64], F32)
        self.dma("sp", bT[:, :, :, :], self.ssm_bT[l].rearrange("r p k n -> p r k n"), r=[self.ssm_bT], w=[bT])
        bm = s("bmask", [128, 8], F32)
        one8 = s("one8", [128, 8], F32)
        G(lambda e: e.memset(one8[:, :], 1.0), w=[one8])
        G(lambda e: e.affine_select(out=bm[:, :], in_=one8[:, :], pattern=[[-16, 8]], compare_op=ALU.is_ge, fill=0.0, base=0, channel_multiplier=1),
          r=[one8], w=[bm])
        G(lambda e: e.affine_select(out=bm[:, :], in_=bm[:, :], pattern=[[16, 8]], compare_op=ALU.is_ge, fill=0.0, base=15, channel_multiplier=-1),
          r=[bm], w=[bm])
        bmb = bm[:, :].unsqueeze(2).to_broadcast([128, 8, 64])
        for kc in range(2):
            fr = fre[:, kc * 8:(kc + 1) * 8, :]
            fi = fim[:, kc * 8:(kc + 1) * 8, :]
            bre = bT[:, 0, kc, :].unsqueeze(1).to_broadcast([128, 8, 64])
            bim = bT[:, 1, kc, :].unsqueeze(1).to_broadcast([128, 8, 64])
            a1, a2 = t1[:, 0:8, :], t2[:, 0:8, :]
            V(lambda e, fr=fr, bre=bre: e.tensor_tensor(out=a1, in0=fr, in1=bre, op=ALU.mult), r=[fre, bT], w=[t1])
            V(lambda e, fi=fi, bim=bim: e.tensor_tensor(out=a2, in0=fi, in1=bim, op=ALU.mult), r=[fim, bT], w=[t2])
            V(lambda e: e.tensor_tensor(out=a1, in0=a1, in1=a2, op=ALU.subtract), r=[t1, t2], w=[t1])
            V(lambda e, kc=kc: e.tensor_tensor(out=self.Bblk[:, kc, :, 0, :], in0=a1, in1=bmb, op=ALU.mult), r=[t1, bm], w=[self.Bblk])
            V(lambda e, fr=fr, bim=bim: e.tensor_tensor(out=a1, in0=fr, in1=bim, op=ALU.mult), r=[fre, bT], w=[t1])
            V(lambda e, fi=fi, bre=bre: e.tensor_tensor(out=a2, in0=fi, in1=bre, op=ALU.mult), r=[fim, bT], w=[t2])
            V(lambda e: e.tensor_tensor(out=a1, in0=a1, in1=a2, op=ALU.add), r=[t1, t2], w=[t1])
            V(lambda e, kc=kc: e.tensor_tensor(out=self.Bblk[:, kc, :, 1, :], in0=a1, in1=bmb, op=ALU.mult), r=[t1, bm], w=[self.Bblk])
        cTs = s("cTs", [128, 16, 16], F32)
        self.dma("sp", cTs[:, :, :], self.ssm_cT[l].rearrange("g p c -> p g c"), r=[self.ssm_cT], w=[cTs])
        sg = s("sgn", [128, 1], F32)
        G(lambda e: e.memset(sg[0:64, :], 1.0), w=[sg])
        G(lambda e: e.memset(sg[64:128, :], -1.0), w=[sg])
        G(lambda e: e.memset(self.Cblk[:, :, :], 0.0), w=[self.Cblk])
        for g in range(16):
            c0 = (g % 8) * 16
            S(lambda e, g=g, c0=c0: e.activation(out=self.Cblk[:, g, c0:c0 + 16], in_=cTs[:, g, :], func=AF.Identity, scale=sg[:, 0:1]),
              r=[cTs, sg, self.Cblk], w=[self.Cblk])
        onesb = s("onesb", [128, 128], F32)
        G(lambda e: e.memset(onesb[:, :], 1.0), w=[onesb])
        G(lambda e: e.affine_select(out=onesb[:, :], in_=onesb[:, :], pattern=[[1, 128]], compare_op=ALU.is_ge, fill=0.0, base=0, channel_multiplier=-1),
          r=[onesb], w=[onesb])
        G(lambda e: e.tensor_copy(out=self.TriT[:, :], in_=onesb[:, :]), r=[onesb], w=[self.TriT])
        G(lambda e: e.memset(self.ones1[:, :], 1.0), w=[self.ones1])
        self.dma("sp", self.dTt[:, :], self.ssm_dT[l], r=[self.ssm_dT], w=[self.dTt])
        self.dma("sp", self.glub[:, :], self.glu_bT[l], r=[self.glu_bT], w=[self.glub])
        V(lambda e: e.tensor_scalar(out=self.glubh[:, :], in0=self.glub[:, :], scalar1=0.5, scalar2=None, op0=ALU.mult), r=[self.glub], w=[self.glubh])
        self.dma("pool", self.gluw[:, :, :], self.glu_w[l].rearrange("(k p) n -> p k n", p=128), r=[self.glu_w], w=[self.gluw])
        self.pop()

    def s5_chunk(self, l, j):
        B = self.bank
        V, S, G = self.V, self.S, self.G
        hT = self.hT[j % 2]
        uT = self.uT[j % 2]
        co = self.co[j % 2]
        ps_s = B[7]
        for cc in range(2):
            for k in range(8):
                self.mm(ps_s[:, cc * 128:(cc + 1) * 128], self.win[:, k, 2048 + cc * 128:2048 + (cc + 1) * 128], hT[:, k, :], r=[self.win, hT], w=[ps_s],
                        start=(k == 0), stop=(k == 7), signal=(k == 7 and cc == 1))
        S(lambda e: e.copy(out=uT[:, :, :], in_=ps_s[:, 0:256].rearrange("p (c t) -> p c t", c=2)), r=[ps_s], w=[uT])
        yps = B[7]
        for h in range(2):
            bu = [B[1], B[2]]
            zz = [B[3], B[4]]
            g0 = h * 8
            for q in range(2):
                self.mm(bu[q][:, :], uT[:, h, :], self.Bblk[:, h, q * 4:(q + 1) * 4, :, :].rearrange("p g r n -> p (g r n)"), r=[uT, self.Bblk], w=[bu[q]],
                        start=True, stop=True)
            t1, t2, vv = self.s5t1, self.s5t2, self.s5v
            for q in range(2):
                gs = slice(g0 + q * 4, g0 + q * 4 + 4)
                bu4 = bu[q][:, :].rearrange("p (g r n) -> p g r n", g=4, r=2)
                pc = self.Pre[:, gs, :].unsqueeze(2).to_broadcast([128, 4, 2, 64])
                V(lambda e, q=q, bu4=bu4, pc=pc: e.tensor_tensor(out=t1[:, q * 4:(q + 1) * 4, :, :], in0=bu4, in1=pc, op=ALU.mult), r=[bu[q], self.Pre], w=[t1], x=[t1])
                V(lambda e, q=q, bu4=bu4, gs=gs: e.tensor_tensor(out=t2[:, q * 4:(q + 1) * 4, :, :], in0=bu4[:, :, ::-1, :], in1=self.PsT[:, gs, :, :], op=ALU.mult),
                  r=[bu[q], self.PsT], w=[t2], x=[t2])
            G(lambda e: e.tensor_tensor(out=vv[:, :], in0=t1[:, :, :, :].rearrange("p g r n -> p (g r n)"), in1=t2[:, :, :, :].rearrange("p g r n -> p (g r n)"), op=ALU.add),
              r=[t1, t2], w=[vv])
            for q in range(2):
                self.mm(zz[q][:, :], self.TriT[:, :], vv[:, q * 512:(q + 1) * 512], r=[self.TriT, vv], w=[zz[q]], start=True, stop=False)
                self.mm(zz[q][:, :], self.ones1[:, :], self.x0row[h][:, q * 512:(q + 1) * 512], r=[self.ones1, self.x0row[h]], w=[zz[q]], start=False, stop=True)
            xs = self.s5x[h]
            for q in range(2):
                gs = slice(g0 + q * 4, g0 + q * 4 + 4)
                z4 = zz[q][:, :].rearrange("p (g r n) -> p g r n", g=4, r=2)
                qc = self.Qre[:, gs, :].unsqueeze(2).to_broadcast([128, 4, 2, 64])
                V(lambda e, q=q, z4=z4, qc=qc: e.tensor_tensor(out=t1[:, q * 4:(q + 1) * 4, :, :], in0=z4, in1=qc, op=ALU.mult), r=[zz[q], self.Qre], w=[t1], x=[t1])
                V(lambda e, q=q, z4=z4, gs=gs: e.tensor_tensor(out=t2[:, q * 4:(q + 1) * 4, :, :], in0=z4[:, :, ::-1, :], in1=self.QsT[:, gs, :, :], op=ALU.mult),
                  r=[zz[q], self.QsT], w=[t2], x=[t2])
            G(lambda e, xs=xs: e.tensor_tensor(out=xs[:, :, :].rearrange("p g m -> p (g m)"), in0=t1[:, :, :, :].rearrange("p g r n -> p (g r n)"),
                                        in1=t2[:, :, :, :].rearrange("p g r n -> p (g r n)"), op=ALU.add), r=[t1, t2], w=[xs])
            self.dma("act", self.x0row[h][0:1, :], xs[127:128, :, :].rearrange("p g m -> p (g m)"), r=[xs], w=[self.x0row[h]])
            pxt = B[0]
            pxb = pxt[:, :].bitcast(BF16).rearrange("p (g t) -> p g t", g=8)
            for g in range(8):
                self.tr(pxb[:, g, :], xs[:, g, :], self.identb[:, :], r=[xs, self.identb], w=[pxt], signal=(g == 7))
            V(lambda e, pxb=pxb: e.tensor_copy(out=self.s5xT[:, :, :], in_=pxb), r=[pxt], w=[self.s5xT])
            for g in range(8):
                self.mm(yps[:, 256 + h * 128:256 + (h + 1) * 128], self.Cblk[:, g0 + g, :], self.s5xT[:, g, :], r=[self.Cblk, self.s5xT], w=[yps],
                        start=(g == 0), stop=(g == 7))
        for cc in range(2):
            V(lambda e, cc=cc: e.scalar_tensor_tensor(out=self.yf[:, cc, :], in0=uT[:, cc, :], scalar=self.dTt[:, cc:cc + 1], in1=yps[:, 256 + cc * 128:256 + (cc + 1) * 128],
                                                      op0=ALU.mult, op1=ALU.add), r=[uT, self.dTt, yps], w=[self.yf], x=[self.yf])
        S(lambda e: e.activation(out=self.yg[:, :, :], in_=self.yf[:, :, :], func=AF.Gelu_apprx_tanh), r=[self.yf], w=[self.yg])
        gps = B[5]
        for c2 in range(2):
            for cc in range(2):
                self.mm(gps[:, c2 * 128:(c2 + 1) * 128], self.gluw[:, cc, c2 * 128:(c2 + 1) * 128], self.yg[:, cc, :], r=[self.gluw, self.yg], w=[gps],
                        start=(cc == 0), stop=(cc == 1))
        for c2 in range(2):
            S(lambda e, c2=c2: e.activation(out=self.sgm[:, c2, :], in_=gps[:, c2 * 128:(c2 + 1) * 128], func=AF.Sigmoid, bias=self.glub[:, c2:c2 + 1], scale=1.0),
              r=[gps, self.glub], w=[self.sgm])
        V(lambda e: e.tensor_tensor(out=co[:, :, :], in0=self.yg[:, :, :], in1=self.sgm[:, :, :], op=ALU.mult), r=[self.yg, self.sgm], w=[co])
        self.dma("sp", self.coutT[:, j * 128:(j + 1) * 128].rearrange("(c p) t -> p c t", p=128), co[:, :, :], r=[co], w=[self.coutT])

    def half(self, bank, lo):
        t = Tl(bank.h, bank.b.name + ("lo" if lo else "hi"))
        return t

    def p1v2_alloc(self):
        s = self.sb
        self.xt = [s(f"xt{i}", [128, D], F32) for i in range(2)]
        self.xn = [s(f"xn{i}", [128, D], BF16) for i in range(2)]
        self.st = [s(f"st{i}", [128, 24], F32) for i in range(2)]
        self.st2 = [s(f"stb{i}", [128, 24], F32) for i in range(2)]
        self.hT = [s(f"hT{i}", [128, 8, 128], BF16) for i in range(2)]
        self.gl = [s(f"gl{i}", [128, 512], F32) for i in range(2)]
        self.qbd = [s(f"qbd{i}", [128, 2, 512], BF16) for i in range(2)]
        self.vp = [s(f"vp{i}", [128, 8, 65], BF16) for i in range(2)]
        for i in range(2):
            self.G(lambda e, i=i: e.memset(self.vp[i][:, :, :], 1.0), w=[self.vp[i]])
        self.uT = [s(f"uT{i}", [128, 2, 128], BF16) for i in range(2)]
        self.vnb = s("vnb", [128, 256], F32)
        self.vb = s("vb", [128, 256], BF16)
        self.aout = s("aout", [128, 256], BF16)
        self.qb = s("qb", [128, 2, 512], BF16)
        self.rt = s("rt", [128, 4, 8, 8], F32)
        self.uD = [s(f"uD{i}", [128, 2, 128], F32) for i in range(3)]
        self.p1t = [s(f"p1t{i}", [128, 4, 2, 64], BF16) for i in range(2)]
        self.p2t = [s(f"p2t{i}", [128, 4, 2, 64], BF16) for i in range(2)]
        self.q1 = [s(f"q1_{i}", [128, 4, 2, 64], F32) for i in range(2)]
        self.q2 = [s(f"q2_{i}", [128, 4, 2, 64], F32) for i in range(2)]
        self.qv = [s(f"qv{i}", [128, 512], BF16) for i in range(2)]
        self.qx = [s(f"qx{i}", [128, 4, 128], BF16) for i in range(2)]
        self.qxT = [s(f"qxT{i}", [128, 4, 128], BF16) for i in range(3)]
        self.x0q = [s(f"x0q{i}", [1, 512], BF16) for i in range(4)]
        for i in range(4):
            self.G(lambda e, i=i: e.memset(self.x0q[i][:, :], 0.0), w=[self.x0q[i]])
        self.yf = s("yf2", [128, 2, 128], F32)
        self.yg = s("yg2", [128, 2, 128], BF16)
        self.sgm = s("sgm2", [128, 2, 128], F32)
        self.co = [s(f"co2_{i}", [128, 2, 128], BF16) for i in range(2)]
        B = self.bank
        self.b5lo, self.b5hi = Tl(B[5].h, "b5lo"), Tl(B[5].h, "b5hi")
        self.b6lo, self.b6hi = Tl(B[6].h, "b6lo"), Tl(B[6].h, "b6hi")
        self.b7lo, self.b7hi = Tl(B[7].h, "b7lo"), Tl(B[7].h, "b7hi")

    def p1_front_a(self, l, j, x_src):
        if j >= NCH:
            return
        i2 = j % 2
        xt, xn, st, hT = self.xt[i2], self.xn[i2], self.st[i2], self.hT[i2]
        B = self.bank
        V, S, G = self.V, self.S, self.G
        if j == 0:
            self.dma("sp", xt[:, :], x_src[0:128, :], r=[x_src], w=[xt])
        if j + 1 < NCH:
            xtn = self.xt[(j + 1) % 2]
            self.dma("sp", xtn[:, :], x_src[(j + 1) * 128:(j + 2) * 128, :], r=[x_src], w=[xtn])
        self.ln_stats(xt, lambda a, b: xt[:, a:b], D, st)
        S(lambda e: e.activation(out=xn[:, :], in_=xt[:, :], func=AF.Identity, bias=st[:, 4:5], scale=st[:, 3:4]), r=[xt, st], w=[xn])
        pT = B[0]
        pTb = pT[:, :].bitcast(BF16).rearrange("p (k t) -> p k t", k=8)
        for k in range(8):
            self.tr(pTb[:, k, :], xn[:, k * 128:(k + 1) * 128], self.identb[:, :], r=[xn, self.identb], w=[pT], signal=(k == 7))
        for k in range(8):
            S(lambda e, k=k: e.activation(out=hT[:, k, :], in_=pTb[:, k, :], func=AF.Identity, scale=self.modp[:, 1, k:k + 1], bias=self.modp[:, 0, k:k + 1]),
              r=[pT, self.modp], w=[hT], x=[hT])

    def p1_front_s(self, l, j):
        if j >= NCH:
            return
        i2 = j % 2
        hT, uT = self.hT[i2], self.uT[i2]
        S = self.S
        ps_s = self.b6lo
        for cc in range(2):
            for k in range(8):
                self.mm(ps_s[:, cc * 128:(cc + 1) * 128], self.win[:, k, 2048 + cc * 128:2048 + (cc + 1) * 128], hT[:, k, :], r=[self.win, hT], w=[ps_s],
                        start=(k == 0), stop=(k == 7), signal=(k == 7 and cc == 1))
        S(lambda e: e.copy(out=uT[:, :, :], in_=ps_s[:, 0:256].rearrange("p (c t) -> p c t", c=2)), r=[ps_s], w=[uT])
        uD = self.uD[j % 3]
        for cc in range(2):
            S(lambda e, cc=cc: e.activation(out=uD[:, cc, :], in_=ps_s[:, cc * 128:(cc + 1) * 128], func=AF.Identity, scale=self.dTt[:, cc:cc + 1]), r=[ps_s, self.dTt], w=[uD], x=[uD])

    def p1_front_p(self, l, j, which):
        if j >= NCH:
            return
        i2 = j % 2
        hT, gl, qf, vp = self.hT[i2], self.gl[i2], self.qbd[i2], self.vp[i2]
        B = self.bank
        S = self.S

        def proj(bank, c0):
            for k in range(8):
                self.mm(bank[:, :], hT[:, k, :], self.win[:, k, c0:c0 + 512], r=[hT, self.win], w=[bank], start=(k == 0), stop=(k == 7))
        if which == 0:
            proj(B[1], 0)
            S(lambda e: e.activation(out=gl[:, :], in_=B[1][:, :], func=AF.Gelu_apprx_tanh), r=[B[1]], w=[gl])
        elif which == 1:
            proj(B[2], 512)
            S(lambda e: e.copy(out=qf[:, 0, :], in_=B[2][:, :]), r=[B[2]], w=[qf])
        elif which == 2:
            proj(B[1], 1024)
            S(lambda e: e.copy(out=qf[:, 1, :], in_=B[1][:, :]), r=[B[1]], w=[qf], x=[qf])
        else:
            proj(B[2], 1536)
            S(lambda e: e.copy(out=vp[:, :, 0:64], in_=B[2][:, :].rearrange("p (h e) -> p h e", e=64)), r=[B[2]], w=[vp])

    def p1_front(self, l, j, x_src):
        self.p1_front_a(l, j, x_src)
        self.p1_front_s(l, j)
        for w_ in range(4):
            self.p1_front_p(l, j, w_)

    def p1_back(self, l, j):
        if j < 0:
            return
        i2 = j % 2
        st2 = self.st2[i2]
        gl, qf, vp, uT = self.gl[i2], self.qbd[i2], self.vp[i2], self.uT[i2]
        vnb, vb, aout, qb, rt = self.vnb, self.vb, self.aout, self.qbd[i2], self.rt
        B = self.bank
        V, S, G = self.V, self.S, self.G
        rows = slice(j * 128, (j + 1) * 128)
        self.ln_stats(gl, lambda a, b: gl[:, 256 + a:256 + b], 256, st2)
        V(lambda e: e.tensor_scalar(out=vnb[:, :], in0=gl[:, 256:512], scalar1=st2[:, 3:4], scalar2=st2[:, 4:5], op0=ALU.mult, op1=ALU.add),
          r=[gl, st2], w=[vnb])
        V(lambda e: e.tensor_tensor(out=vnb[:, :], in0=vnb[:, :], in1=self.glng[:, :], op=ALU.mult), r=[vnb, self.glng], w=[vnb], x=[vnb])
        V(lambda e: e.tensor_tensor(out=vb[:, :], in0=vnb[:, :], in1=self.glnb[:, :], op=ALU.add), r=[vnb, self.glnb], w=[vb], x=[vnb])
        psv = self.b5lo
        for h in range(4):
            self.mm(psv[:, h * 64:(h + 1) * 64], self.wsT[:, h, :], vb[:, h * 64:(h + 1) * 64], r=[self.wsT, vb], w=[psv], start=True, stop=True,
                    signal=(h == 3))
        for h in range(4):
            V(lambda e, h=h: e.scalar_tensor_tensor(out=aout[:, h * 64:(h + 1) * 64], in0=psv[:, h * 64:(h + 1) * 64], scalar=self.gbs[:, h:h + 1],
                                                    in1=gl[:, h * 64:(h + 1) * 64], op0=ALU.add, op1=ALU.mult), r=[psv, self.gbs, gl], w=[aout], x=[aout])
        self.dma("sp", self.mixtok[rows, 0:256], aout[:, :], r=[aout], w=[self.mixtok])
        q4 = qf[:, :, :].rearrange("p a (h e) -> p (a h) e", e=64)
        o4 = qb[:, :, :].rearrange("p a (h e) -> p (a h) e", e=64)
        for a in range(2):
            xa1, xa2 = q4[:, a * 8:(a + 1) * 8, 0:8], q4[:, a * 8:(a + 1) * 8, 8:16]
            cb = self.cs[:, j, :].unsqueeze(1).to_broadcast([128, 8, 8])
            sb_ = self.sn[:, j, :].unsqueeze(1).to_broadcast([128, 8, 8])
            oa = o4[:, a * 8:(a + 1) * 8, :]
            G(lambda e, xa1=xa1, cb=cb: e.tensor_tensor(out=rt[:, 0, :, :], in0=xa1, in1=cb, op=ALU.mult), r=[qf, self.cs], w=[rt])
            G(lambda e, xa2=xa2, sb_=sb_: e.tensor_tensor(out=rt[:, 1, :, :], in0=xa2, in1=sb_, op=ALU.mult), r=[qf, self.sn], w=[rt])
            G(lambda e, xa2=xa2, cb=cb: e.tensor_tensor(out=rt[:, 2, :, :], in0=xa2, in1=cb, op=ALU.mult), r=[qf, self.cs], w=[rt])
            G(lambda e, xa1=xa1, sb_=sb_: e.tensor_tensor(out=rt[:, 3, :, :], in0=xa1, in1=sb_, op=ALU.mult), r=[qf, self.sn], w=[rt])
            G(lambda e, oa=oa: e.tensor_tensor(out=oa[:, :, 0:8], in0=rt[:, 0, :, :], in1=rt[:, 1, :, :], op=ALU.subtract), r=[rt], w=[qb])
            G(lambda e, oa=oa: e.tensor_tensor(out=oa[:, :, 8:16], in0=rt[:, 2, :, :], in1=rt[:, 3, :, :], op=ALU.add), r=[rt], w=[qb])
        pq = B[7]
        pqb = pq[:, :].bitcast(BF16).rearrange("p (a k t) -> p a k t", a=2, k=4)
        for a in range(2):
            for k in range(4):
                self.tr(pqb[:, a, k, :], qb[:, a, k * 128:(k + 1) * 128], self.identb[:, :], r=[qb, self.identb], w=[pq], signal=(a == 1 and k == 3))
        S(lambda e: e.copy(out=self.QT[:, :, j * 128:(j + 1) * 128], in_=pqb[:, 0, :, :]), r=[pq], w=[self.QT])
        S(lambda e: e.copy(out=self.KT[:, :, j * 128:(j + 1) * 128], in_=pqb[:, 1, :, :]), r=[pq], w=[self.KT])
        self.dma("sp", self.vd[rows, :], vp[:, :, :].rearrange("p h e -> p (h e)"), r=[vp], w=[self.vd])

    def s5A(self, i):
        if i < 0 or i >= 4 * NCH:
            return
        j, q = divmod(i, 4)
        kc = q // 2
        gs = slice(q * 4, q * 4 + 4)
        uT = self.uT[j % 2]
        bu = self.bank[3]
        t1, t2 = self.p1t[i % 2], self.p2t[i % 2]
        self.mm(bu[:, :], uT[:, kc, :], self.Bblk[:, kc, (q % 2) * 4:(q % 2) * 4 + 4, :, :].rearrange("p g r n -> p (g r n)"), r=[uT, self.Bblk], w=[bu],
                start=True, stop=True)
        bu4 = bu[:, :].rearrange("p (g r n) -> p g r n", g=4, r=2)
        pc = self.Pre[:, gs, :].unsqueeze(2).to_broadcast([128, 4, 2, 64])
        self.V(lambda e: e.tensor_tensor(out=t1[:, :, :, :], in0=bu4, in1=pc, op=ALU.mult), r=[bu, self.Pre], w=[t1])
        self.V(lambda e: e.tensor_tensor(out=t2[:, :, :, :], in0=bu4[:, :, ::-1, :], in1=self.PsT[:, gs, :, :], op=ALU.mult), r=[bu, self.PsT], w=[t2])

    def s5B(self, i):
        if i < 0 or i >= 4 * NCH:
            return
        j, q = divmod(i, 4)
        gs = slice(q * 4, q * 4 + 4)
        zz = self.bank[4]
        t1, t2 = self.p1t[i % 2], self.p2t[i % 2]
        u1, u2, xs = self.q1[i % 2], self.q2[i % 2], self.qx[i % 2]
        f = lambda t: t[:, :, :, :].rearrange("p g r n -> p (g r n)")
        self.mm(zz[:, :], self.TriT[:, :], f(t1), r=[self.TriT, t1], w=[zz], start=True, stop=False)
        self.mm(zz[:, :], self.TriT[:, :], f(t2), r=[self.TriT, t2], w=[zz], start=False, stop=False)
        self.mm(zz[:, :], self.ones1[:, :], self.x0q[q][:, :], r=[self.ones1, self.x0q[q]], w=[zz], start=False, stop=True)
        z4 = zz[:, :].rearrange("p (g r n) -> p g r n", g=4, r=2)
        qc = self.Qre[:, gs, :].unsqueeze(2).to_broadcast([128, 4, 2, 64])
        self.V(lambda e: e.tensor_tensor(out=u1[:, :, :, :], in0=z4, in1=qc, op=ALU.mult), r=[zz, self.Qre], w=[u1])
        self.V(lambda e: e.tensor_tensor(out=u2[:, :, :, :], in0=z4[:, :, ::-1, :], in1=self.QsT[:, gs, :, :], op=ALU.mult), r=[zz, self.QsT], w=[u2])
        self.G(lambda e: e.tensor_tensor(out=xs[:, :, :].rearrange("p g m -> p (g m)"), in0=f(u1), in1=f(u2), op=ALU.add), r=[u1, u2], w=[xs])
        self.dma("pool", self.x0q[q][0:1, :], xs[127:128, :, :].rearrange("p g m -> p (g m)"), r=[xs], w=[self.x0q[q]])

    def s5C(self, i):
        if i < 0 or i >= 4 * NCH:
            return
        xs, xT = self.qx[i % 2], self.qxT[i % 3]
        pxt = self.b6hi
        pxb = pxt[:, 256:512].bitcast(BF16).rearrange("p (g t) -> p g t", g=4)
        for g in range(4):
            self.tr(pxb[:, g, :], xs[:, g, :], self.identb[:, :], r=[xs, self.identb], w=[pxt], signal=(g == 3))
        self.S(lambda e: e.copy(out=xT[:, :, :], in_=pxb), r=[pxt], w=[xT])

    def s5D(self, i):
        if i < 0 or i >= 4 * NCH:
            return
        j, q = divmod(i, 4)
        kc = q // 2
        xT = self.qxT[i % 3]
        yps = self.b5hi
        for g in range(4):
            self.mm(yps[:, 256 + kc * 128:256 + (kc + 1) * 128], self.Cblk[:, q * 4 + g, :], xT[:, g, :], r=[self.Cblk, xT], w=[yps],
                    start=(q % 2 == 0 and g == 0), stop=(q % 2 == 1 and g == 3))

    def s5_step(self, i):
        self.s5A(i + 2)
        self.s5B(i + 1)
        self.s5C(i)
        if i % 2 == 0:
            self.s5D(i - 2)
            self.s5D(i - 1)

    def s5_tail(self, j):
        if j < 0 or j >= NCH:
            return
        V, S, G = self.V, self.S, self.G
        i2 = j % 2
        uT = self.uT[i2]
        yps = self.b5hi
        co = self.co[i2]
        for cc in range(2):
            V(lambda e, cc=cc: e.scalar_tensor_tensor(out=self.yf[:, cc, :], in0=self.uD[j % 3][:, cc, :], scalar=1.0, in1=yps[:, 256 + cc * 128:256 + (cc + 1) * 128],
                                                      op0=ALU.mult, op1=ALU.add), r=[self.uD[j % 3], yps], w=[self.yf], x=[self.yf])
        S(lambda e: e.activation(out=self.yg[:, :, :], in_=self.yf[:, :, :], func=AF.Gelu_apprx_tanh), r=[self.yf], w=[self.yg])
        gps = self.b5lo
        for c2 in range(2):
            for cc in range(2):
                self.mm(gps[:, c2 * 128:(c2 + 1) * 128], self.gluw[:, cc, c2 * 128:(c2 + 1) * 128], self.yg[:, cc, :], r=[self.gluw, self.yg], w=[gps],
                        start=(cc == 0), stop=(cc == 1))
        for c2 in range(2):
            S(lambda e, c2=c2: e.activation(out=self.sgm[:, c2, :], in_=gps[:, c2 * 128:(c2 + 1) * 128], func=AF.Tanh, bias=self.glubh[:, c2:c2 + 1], scale=0.5),
              r=[gps, self.glubh], w=[self.sgm])
        V(lambda e: e.scalar_tensor_tensor(out=self.sgm[:, :, :], in0=self.sgm[:, :, :], scalar=1.0, in1=self.yg[:, :, :], op0=ALU.add, op1=ALU.mult),
          r=[self.sgm, self.yg], w=[self.sgm])
        V(lambda e: e.tensor_scalar(out=co[:, :, :], in0=self.sgm[:, :, :], scalar1=0.5, scalar2=None, op0=ALU.mult), r=[self.sgm], w=[co])
        self.dma("sp", self.coutT[:, j * 128:(j + 1) * 128].rearrange("(c p) t -> p c t", p=128), co[:, :, :], r=[co], w=[self.coutT])

    def p1_all(self, l, x_src):
        self.p1_front(l, 0, x_src)
        self.s5A(0); self.s5A(1); self.s5B(0)
        for j in range(NCH):
            self.p1_front(l, j + 1, x_src)
            self.p1_back(l, j)
            for q in range(4):
                self.s5_step(4 * j + q)
                if q == 0:
                    self.s5_tail(j - 1)
        self.s5_step(4 * NCH)
        self.s5_tail(NCH - 1)
        assert (4 * NCH) % 2 == 0

    CAP = 896
    NSLOT = 16 * 896 + 128
    GROUPS = ((0, 4), (512, 3))

    def p3_io(self):
        i = self.inp
        self.w_out = i("w_out", [2, D, D])
        self.ln1_g = i("ln1_g", [2, D]); self.ln1_b = i("ln1_b", [2, D])
        self.ln2_g = i("ln2_g", [2, D]); self.ln2_b = i("ln2_b", [2, D])
        self.router_w = i("router_w", [D, 16])
        self.router_bias = i("router_bias", [16])
        self.x1d = self.scratch("x1d", [T, D], F32)
        self.xmid = self.scratch("xmid", [T, D], F32)
        self.h2slots = self.scratch("h2slots", [self.NSLOT, D], BF16)
        self.oslots = self.scratch("oslots", [self.NSLOT, D], F32)

    def route_alloc(self):
        s = self.sb
        self.slotA = s("slotA", [128, NCH], I32)
        self.slotB = s("slotB", [128, NCH], I32)
        self.gAB = s("gAB", [128, 2, NCH], F32)

    def p3_alloc(self, l):
        s = self.sb
        self.wout = s("wout", [128, 8, D], BF16)
        for k in range(8):
            self.dma("pool", self.wout[:, k, :], self.w_out[l, k * 128:(k + 1) * 128, :], r=[self.w_out], w=[self.wout])
        self.lng = s("lng", [128, D], F32); self.lnb = s("lnb", [128, D], F32)
        self.dma("sp", self.lng[:, :], self.ln1_g[l].partition_broadcast(128), r=[self.ln1_g], w=[self.lng])
        self.dma("sp", self.lnb[:, :], self.ln1_b[l].partition_broadcast(128), r=[self.ln1_b], w=[self.lnb])
        self.rw = s("rw", [128, 8, 16], F32)
        self.dma("sp", self.rw[:, :, :], self.router_w[:, :].rearrange("(k p) e -> p k e", p=128), r=[self.router_w], w=[self.rw])
        self.rbias = s("rbias", [128, 16], F32)
        self.dma("sp", self.rbias[:, :], self.router_bias[:].partition_broadcast(128), r=[self.router_bias], w=[self.rbias])
        self.o3 = [s(f"o3_{i}", [128, 3, 520], F32) for i in range(2)]
        self.rec = s("rec", [128, 8], F32)
        self.mt = [s(f"mt{i}", [128, D], BF16) for i in range(2)]
        self.mixT = [s(f"mixT{i}", [128, 8, 128], BF16) for i in range(2)]
        self.xres = [s(f"xres{i}", [128, D], F32) for i in range(2)]
        self.yy = s("yy", [128, D], F32)
        self.x1t = [s(f"x1t{i}", [128, D], F32) for i in range(2)]
        self.cT = [s(f"cT{i}", [128, 2, 128], BF16) for i in range(2)]
        self.st3b = s("st3b", [128, 24], F32)
        self.h2f = s("h2f", [128, D], F32)
        self.h2all = s("h2all", [128, NCH, D], BF16)
        self.h2c = [Tl(self.h2all.h, f"h2c{j}") for j in range(NCH)]
        self.scall = s("scall", [128, NCH, 16], F32)
        self.h2T = s("h2T", [128, 8, 128], F32)
        self.st3 = s("st3", [128, 24], F32)
        self.trs = s("trs", [128, 128], F32)
        self.eoff = s("eoff", [128, 16], F32)
        self.trashp = s("trashp", [128, 1], F32)
        self.ones16 = s("ones16", [128, 16], F32)
        G = self.G
        G(lambda e: e.memset(self.ones16[:, :], 1.0), w=[self.ones16])
        G(lambda e: e.affine_select(out=self.trs[:, :], in_=self.onesf[:, :], pattern=[[1, 128]], compare_op=ALU.is_ge, fill=0.0, base=-1, channel_multiplier=-1),
          r=[self.onesf], w=[self.trs])
        G(lambda e: e.iota(self.eoff[:, :], pattern=[[self.CAP, 16]], base=0, channel_multiplier=0, allow_small_or_imprecise_dtypes=True), w=[self.eoff])
        G(lambda e: e.iota(self.trashp[:, :], pattern=[[0, 1]], base=16 * self.CAP, channel_multiplier=1, allow_small_or_imprecise_dtypes=True), w=[self.trashp])

    def p3_L1(self, k):
        if k >= NCH:
            return
        rws = slice(k * 128, (k + 1) * 128)
        o3_, mt_ = self.o3[k % 2], self.mt[k % 2]
        for bi in range(3):
            self.dma("sp", o3_[:, bi, :], self.obr[bi][rws, :], r=[self.obr[bi]], w=[o3_])
        self.dma("sp", mt_[:, 0:256], self.mixtok[rws, 0:256], r=[self.mixtok], w=[mt_])

    def p3_L2(self, k, x_src):
        if k >= NCH:
            return
        rws = slice(k * 128, (k + 1) * 128)
        self.dma("sp", self.cT[k % 2][:, :, :], self.coutT[:, rws].rearrange("(c p) t -> p c t", p=128), r=[self.coutT], w=[self.cT[k % 2]])
        self.dma("sp", self.xres[k % 2][:, :], x_src[rws, :], r=[x_src], w=[self.xres[k % 2]])

    def p3_S1(self, j):
        if j >= NCH or j < 0:
            return
        B = self.bank
        V, S, G = self.V, self.S, self.G
        o3, rec, mt = self.o3[j % 2], self.rec, self.mt[j % 2]
        mixT = self.mixT[j % 2]
        V(lambda e: e.tensor_tensor(out=o3[:, 0, :], in0=o3[:, 0, :], in1=o3[:, 1, :], op=ALU.add), r=[o3], w=[o3], x=[o3])
        V(lambda e: e.tensor_tensor(out=o3[:, 0, :], in0=o3[:, 0, :], in1=o3[:, 2, :], op=ALU.add), r=[o3], w=[o3], x=[o3])
        o8 = o3[:, 0, :].rearrange("p (h e) -> p h e", e=65)
        V(lambda e: e.reciprocal(out=rec[:, :], in_=o8[:, :, 64]), r=[o3], w=[rec])
        V(lambda e: e.tensor_tensor(out=mt[:, 256:768].rearrange("p (h e) -> p h e", e=64), in0=o8[:, :, 0:64], in1=rec[:, :].unsqueeze(2).to_broadcast([128, 8, 64]),
                                    op=ALU.mult), r=[o3, rec], w=[mt])
        pT = B[0]
        pTb = pT[:, :].bitcast(BF16).rearrange("p (k t) -> p k t", k=8)
        for k in range(6):
            self.tr(pTb[:, k, :], mt[:, k * 128:(k + 1) * 128], self.identb[:, :], r=[mt, self.identb], w=[pT], signal=(k == 5))
        S(lambda e: e.copy(out=mixT[:, 0:6, :], in_=pTb[:, 0:6, :]), r=[pT], w=[mixT])

    def p3_S2(self, j):
        if j >= NCH or j < 0:
            return
        B = self.bank
        V, S, G = self.V, self.S, self.G
        rows = slice(j * 128, (j + 1) * 128)
        mixT, cT, xres = self.mixT[j % 2], self.cT[j % 2], self.xres[j % 2]
        wb = [B[1], B[2]] if j % 2 == 0 else [B[6], B[7]]
        for nb in range(2):
            for k in range(8):
                lhs = mixT[:, k, :] if k < 6 else cT[:, k - 6, :]
                self.mm(wb[nb][:, :], lhs, self.wout[:, k, nb * 512:(nb + 1) * 512], r=[mixT, cT, self.wout], w=[wb[nb]], start=(k == 0), stop=(k == 7))
        yy = self.yy
        for nb in range(2):
            cs_ = slice(nb * 512, (nb + 1) * 512)
            V(lambda e, nb=nb, cs_=cs_: e.tensor_tensor(out=yy[:, cs_], in0=wb[nb][:, :], in1=self.opg[:, 0, cs_], op=ALU.mult), r=[wb[nb], self.opg], w=[yy], x=[yy])
        V(lambda e: e.scalar_tensor_tensor(out=yy[:, :], in0=xres[:, :], scalar=float(ALPHA), in1=yy[:, :], op0=ALU.mult, op1=ALU.add), r=[xres, yy], w=[yy], x=[yy])
        self.ln_stats(yy, lambda a, b: yy[:, a:b], D, self.st3, act=True)
        x1 = self.x1t[j % 2]
        S(lambda e: e.activation(out=x1[:, :], in_=yy[:, :], func=AF.Identity, bias=self.st3[:, 4:5], scale=self.st3[:, 3:4]), r=[yy, self.st3], w=[x1])
        V(lambda e: e.tensor_tensor(out=x1[:, :], in0=x1[:, :], in1=self.lng[:, :], op=ALU.mult), r=[x1, self.lng], w=[x1])
        V(lambda e: e.tensor_tensor(out=x1[:, :], in0=x1[:, :], in1=self.lnb[:, :], op=ALU.add), r=[x1, self.lnb], w=[x1], x=[x1])
        self.dma("sp", self.x1d[rows, :], x1[:, :], r=[x1], w=[self.x1d])

    def p3_S3(self, j):
        if j >= NCH or j < 0:
            return
        B = self.bank
        V, S, G = self.V, self.S, self.G
        x1 = self.x1t[j % 2]
        self.ln_stats(x1, lambda a, b: x1[:, a:b], D, self.st3b, act=True)
        h2f = self.h2f
        S(lambda e: e.activation(out=h2f[:, :], in_=x1[:, :], func=AF.Identity, bias=self.st3b[:, 4:5], scale=self.st3b[:, 3:4]), r=[x1, self.st3b], w=[h2f])
        V(lambda e: e.tensor_tensor(out=h2f[:, :], in0=h2f[:, :], in1=self.opg2[:, 1, :], op=ALU.mult), r=[h2f, self.opg2], w=[h2f], x=[h2f])
        V(lambda e: e.tensor_tensor(out=h2f[:, :], in0=h2f[:, :], in1=self.opg2[:, 0, :], op=ALU.add), r=[h2f, self.opg2], w=[h2f], x=[h2f])
        S(lambda e: e.copy(out=self.h2all[:, j, :], in_=h2f[:, :]), r=[h2f], w=[self.h2c[j]])
        for half in range(2):
            pt = B[3 + half]
            for k in range(4):
                self.tr(pt[:, k * 128:(k + 1) * 128], h2f[:, (half * 4 + k) * 128:(half * 4 + k + 1) * 128], self.identf[:, :], r=[h2f, self.identf], w=[pt], signal=(k == 3))
            S(lambda e, half=half, pt=pt: e.copy(out=self.h2T[:, half * 4:(half + 1) * 4, :], in_=pt[:, :].rearrange("p (k t) -> p k t", k=4)), r=[pt], w=[self.h2T])
        lg = B[5]
        for k in range(8):
            self.mm(lg[:, 0:16], self.h2T[:, k, :], self.rw[:, k, :], r=[self.h2T, self.rw], w=[lg], start=(k == 0), stop=(k == 7))
        S(lambda e: e.copy(out=self.scall[:, j, :], in_=lg[:, 0:16]), r=[lg], w=[self.scall])

    def p3_all(self, l, x_src):
        self.p3_L1(0)
        for j in range(-2, NCH):
            self.p3_L1(j + 3)
            self.p3_L2(j + 2, x_src)
            self.p3_S1(j + 2)
            self.p3_S2(j + 1)
            self.p3_S3(j)
            if j >= 0 and (j + 1) % self.RSEG == 0:
                self.p3_route(j + 1 - self.RSEG)

    RSEG = 8

    def p3_route_alloc(self):
        s = self.sb
        NJ = self.RSEG
        mk = lambda n: s(n, [128, NJ, 16], F32)
        t = {}
        t["big"] = [mk(f"r_{i}") for i in range(14)]
        t["g4"] = [s(f"r4_{i}", [128, NJ, 4], F32) for i in range(4)]
        t["g1"] = [s(f"r1_{i}", [128, NJ], F32) for i in range(3)]
        t["mask_e0"] = mk("mask_e0")
        t["mask_j0"] = s("mask_j0", [128, 16, NJ], F32)
        G = self.G
        G(lambda e: e.memset(t["mask_e0"][:, :, :], 1.0), w=[t["mask_e0"]])
        G(lambda e: e.memset(t["mask_e0"][:, :, 0:1], 0.0), w=[t["mask_e0"]])
        G(lambda e: e.memset(t["mask_j0"][:, :, :], 1.0), w=[t["mask_j0"]])
        G(lambda e: e.memset(t["mask_j0"][:, :, 0:1], 0.0), w=[t["mask_j0"]])
        self.carry = s("carry", [128, 16], F32)
        G(lambda e: e.memset(self.carry[:, :], 0.0), w=[self.carry])
        self._rt = t

    def p3_route(self, j0):
        B = self.bank
        V, S, G = self.V, self.S, self.G
        NJ = self.RSEG
        t = self._rt
        sel, eq, msk, top2, chosen, gw, tmp, cum, pos, valid, slotv, baseT, totT, sc = t["big"]
        m1, m2, gs, gsel = t["g4"]
        gmax, gsum, t32 = t["g1"]
        mask_e0, mask_j0 = t["mask_e0"], t["mask_j0"]
        f2 = lambda t_: t_[:, :, :].rearrange("p j e -> p (j e)")
        S(lambda e: e.activation(out=f2(sc), in_=self.scall[:, j0:j0 + NJ, :].rearrange("p j e -> p (j e)"), func=AF.Sigmoid), r=[self.scall], w=[sc])
        g4 = lambda t: t[:, :, :].rearrange("p j (g i) -> p (j g) i", i=4)
        b4 = lambda t: t[:, :, :].rearrange("p j g -> p (j g)").unsqueeze(2).to_broadcast([128, NJ * 4, 4])
        V(lambda e: e.tensor_tensor(out=sel[:, :, :], in0=sc[:, :, :], in1=self.rbias[:, :].unsqueeze(1).to_broadcast([128, NJ, 16]), op=ALU.add), r=[sc, self.rbias], w=[sel])
        V(lambda e: e.tensor_reduce(out=m1[:, :, :].rearrange("p j g -> p (j g)"), in_=g4(sel), axis=AX.X, op=ALU.max), r=[sel], w=[m1])
        V(lambda e: e.tensor_tensor(out=g4(eq), in0=g4(sel), in1=b4(m1), op=ALU.is_equal), r=[sel, m1], w=[eq])
        V(lambda e: e.scalar_tensor_tensor(out=f2(msk), in0=f2(eq), scalar=-1e9, in1=f2(sel), op0=ALU.mult, op1=ALU.add), r=[eq, sel], w=[msk])
        V(lambda e: e.tensor_reduce(out=m2[:, :, :].rearrange("p j g -> p (j g)"), in_=g4(msk), axis=AX.X, op=ALU.max), r=[msk], w=[m2])
        V(lambda e: e.tensor_tensor(out=gs[:, :, :], in0=m1[:, :, :], in1=m2[:, :, :], op=ALU.add), r=[m1, m2], w=[gs])
        V(lambda e: e.tensor_reduce(out=gmax[:, :], in_=gs[:, :, :], axis=AX.X, op=ALU.max), r=[gs], w=[gmax])
        V(lambda e: e.tensor_tensor(out=gsel[:, :, :], in0=gs[:, :, :], in1=gmax[:, :].unsqueeze(2).to_broadcast([128, NJ, 4]), op=ALU.is_equal), r=[gs, gmax], w=[gsel])
        V(lambda e: e.tensor_tensor(out=g4(top2), in0=g4(sel), in1=b4(m2), op=ALU.is_ge), r=[sel, m2], w=[top2])
        V(lambda e: e.tensor_tensor(out=g4(chosen), in0=g4(top2), in1=b4(gsel), op=ALU.mult), r=[top2, gsel], w=[chosen])
        V(lambda e: e.tensor_tensor(out=gw[:, :, :], in0=chosen[:, :, :], in1=sc[:, :, :], op=ALU.mult), r=[chosen, sc], w=[gw])
        V(lambda e: e.tensor_reduce(out=gsum[:, :], in_=gw[:, :, :], axis=AX.X, op=ALU.add), r=[gw], w=[gsum])
        V(lambda e: e.reciprocal(out=gsum[:, :], in_=gsum[:, :]), r=[gsum], w=[gsum])
        V(lambda e: e.tensor_tensor(out=gw[:, :, :], in0=gw[:, :, :], in1=gsum[:, :].unsqueeze(2).to_broadcast([128, NJ, 16]), op=ALU.mult), r=[gw, gsum], w=[gw])
        cbk = B[5]
        W = NJ * 16
        self.mm(cbk[:, 128:128 + W], self.trs[:, :], f2(chosen), r=[self.trs, chosen], w=[cbk], start=True, stop=True)
        self.mm(cbk[:, 256:256 + W], self.onesf[:, :], f2(chosen), r=[self.onesf, chosen], w=[cbk], start=True, stop=True)
        V(lambda e: e.tensor_copy(out=f2(totT).rearrange("p (e j) -> p e j", e=16),
                                  in_=cbk[:, 256:256 + W].rearrange("p (j e) -> p e j", e=16)), r=[cbk], w=[totT])
        V(lambda e: e.tensor_tensor_scan(out=f2(baseT), data0=mask_j0[:, :, :].rearrange("p e j -> p (e j)"), data1=f2(totT), initial=0.0, op0=ALU.mult, op1=ALU.add),
          r=[mask_j0, totT], w=[baseT])
        V(lambda e: e.tensor_tensor(out=f2(baseT), in0=f2(baseT), in1=f2(totT), op=ALU.subtract), r=[baseT, totT], w=[baseT])
        bT3 = f2(baseT).rearrange("p (e j) -> p e j", e=16)
        tT3 = f2(totT).rearrange("p (e j) -> p e j", e=16)
        V(lambda e: e.tensor_tensor(out=bT3, in0=bT3, in1=self.carry[:, :].unsqueeze(2).to_broadcast([128, 16, NJ]), op=ALU.add), r=[baseT, self.carry], w=[baseT])
        V(lambda e: e.tensor_tensor(out=self.carry[:, :], in0=bT3[:, :, NJ - 1], in1=tT3[:, :, NJ - 1], op=ALU.add), r=[baseT, totT], w=[self.carry])
        V(lambda e: e.tensor_tensor(out=pos[:, :, :], in0=cbk[:, 128:128 + W].rearrange("p (j e) -> p j e", e=16),
                                    in1=f2(baseT).rearrange("p (e j) -> p j e", e=16), op=ALU.add), r=[cbk, baseT], w=[pos])
        V(lambda e: e.tensor_scalar(out=f2(valid), in0=f2(pos), scalar1=float(self.CAP), scalar2=None, op0=ALU.is_lt), r=[pos], w=[valid])
        V(lambda e: e.tensor_tensor(out=slotv[:, :, :], in0=pos[:, :, :], in1=self.eoff[:, :].unsqueeze(1).to_broadcast([128, NJ, 16]), op=ALU.add), r=[pos, self.eoff], w=[slotv])
        V(lambda e: e.tensor_scalar(out=f2(slotv), in0=f2(slotv), scalar1=self.trashp[:, 0:1], scalar2=None, op0=ALU.subtract), r=[slotv, self.trashp], w=[slotv])
        V(lambda e: e.tensor_tensor(out=f2(slotv), in0=f2(slotv), in1=f2(valid), op=ALU.mult), r=[slotv, valid], w=[slotv])
        V(lambda e: e.tensor_scalar(out=f2(slotv), in0=f2(slotv), scalar1=self.trashp[:, 0:1], scalar2=None, op0=ALU.add), r=[slotv, self.trashp], w=[slotv])
        V(lambda e: e.tensor_tensor(out=f2(gw), in0=f2(gw), in1=f2(valid), op=ALU.mult), r=[gw, valid], w=[gw])
        V(lambda e: e.tensor_tensor_scan(out=f2(cum), data0=f2(mask_e0), data1=f2(chosen), initial=0.0, op0=ALU.mult, op1=ALU.add), r=[mask_e0, chosen], w=[cum])
        for which, dsti in ((1.0, self.slotA), (2.0, self.slotB)):
            wi = int(which) - 1
            V(lambda e, which=which: e.tensor_scalar(out=f2(tmp), in0=f2(cum), scalar1=float(which), scalar2=None, op0=ALU.is_equal), r=[cum], w=[tmp])
            V(lambda e: e.tensor_tensor(out=f2(tmp), in0=f2(tmp), in1=f2(chosen), op=ALU.mult), r=[tmp, chosen], w=[tmp])
            V(lambda e: e.tensor_tensor(out=f2(eq), in0=f2(tmp), in1=f2(gw), op=ALU.mult), r=[tmp, gw], w=[eq])
            V(lambda e, wi=wi: e.tensor_reduce(out=self.gAB[:, wi, j0:j0 + NJ], in_=eq[:, :, :], axis=AX.X, op=ALU.add), r=[eq], w=[self.gAB])
            V(lambda e: e.tensor_tensor(out=f2(tmp), in0=f2(tmp), in1=f2(slotv), op=ALU.mult), r=[tmp, slotv], w=[tmp])
            V(lambda e: e.tensor_reduce(out=t32[:, :], in_=tmp[:, :, :], axis=AX.X, op=ALU.add), r=[tmp], w=[t32])
            V(lambda e: e.tensor_scalar(out=t32[:, :], in0=t32[:, :], scalar1=0.0, scalar2=float(self.NSLOT - 1), op0=ALU.max, op1=ALU.min), r=[t32], w=[t32])
            V(lambda e, dsti=dsti: e.tensor_copy(out=dsti[:, j0:j0 + NJ], in_=t32[:, :]), r=[t32], w=[dsti])
        for j in range(j0, j0 + NJ):
            for dsti in (self.slotA, self.slotB):
                self.cx.dma("pool", None, None, reads=[self.h2c[j].b, dsti.b], writes=[],
                            fn=lambda e, dsti=dsti, j=j: e.indirect_dma_start(out=self.h2slots[:, :], out_offset=bass.IndirectOffsetOnAxis(ap=dsti[:, j:j + 1], axis=0),
                                                                              in_=self.h2all[:, j, :], in_offset=None))

    def p4_io(self):
        i = self.inp
        self.w_gate = i("exp_w_gate", [2, 16, D, 512])
        self.w_up = i("exp_w_up", [2, 16, D, 512])
        self.w_down = i("exp_w_down", [2, 16, 512, D])

    def zero_slots(self):
        self.push()
        zt = self.sb("zt", [128, D], F32)
        self.G(lambda e: e.memset(zt[:, :], 0.0), w=[zt])
        self.dma("sp", self.oslots[16 * self.CAP:16 * self.CAP + 128, :], zt[:, :], r=[zt], w=[self.oslots])
        self.pop()

    def p4_experts(self, l):
        B = self.bank
        V, S, G = self.V, self.S, self.G
        s = self.sb
        wg = [s(f"wg{i}", [128, 8, 512], BF16) for i in range(2)]
        wu = [s(f"wu{i}", [128, 8, 512], BF16) for i in range(2)]
        wd = [s(f"wd{i}", [128, 4, D], BF16) for i in range(2)]
        rt = [s(f"rtok{i}", [128, 4, D], BF16) for i in range(2)]
        rT = s("rT", [128, 8, 512], BF16)
        sil = [s(f"sil{i}", [128, 512], BF16) for i in range(2)]
        hidT = s("hidT", [128, 4, 512], BF16)
        osb = [s(f"eosb{i}", [128, D], F32) for i in range(2)]

        def load_w(e):
            i = e % 2
            self.dma("pool", wg[i][:, :, :], self.w_gate[l, e].rearrange("(k p) f -> p k f", p=128), r=[self.w_gate], w=[wg[i]])
            self.dma("pool", wu[i][:, :, :], self.w_up[l, e].rearrange("(k p) f -> p k f", p=128), r=[self.w_up], w=[wu[i]])
            self.dma("pool", wd[i][:, :, :], self.w_down[l, e].rearrange("(k p) f -> p k f", p=128), r=[self.w_down], w=[wd[i]])

        glist = [(e, off, nb) for e in range(16) for (off, nb) in self.GROUPS]
        load_w(0)
        ob = 0

        def load_rows(gi):
            e, off, nb = glist[gi]
            r0 = e * self.CAP + off
            rtk = rt[gi % 2]
            self.dma("sp", rtk[:, 0:nb, :], self.h2slots[r0:r0 + nb * 128, :].rearrange("(b p) d -> p b d", p=128), r=[self.h2slots], w=[rtk])
        load_rows(0)
        for gi, (e, off, nb) in enumerate(glist):
            if off == 0 and e + 1 < 16:
                load_w(e + 1)
            i = e % 2
            r0 = e * self.CAP + off
            N = nb * 128
            rtk = rt[gi % 2]
            if gi + 1 < len(glist):
                load_rows(gi + 1)
            for blk in range(nb):
                pT = B[blk % 2]
                pTb = pT[:, :].bitcast(BF16).rearrange("p (k t) -> p k t", k=8)
                for k in range(8):
                    self.tr(pTb[:, k, :], rtk[:, blk, k * 128:(k + 1) * 128], self.identb[:, :], r=[rtk, self.identb], w=[pT], signal=(k == 7))
                if blk % 2 == 0:
                    V(lambda e_, blk=blk, pTb=pTb: e_.tensor_copy(out=rT[:, :, blk * 128:(blk + 1) * 128], in_=pTb), r=[pT], w=[rT])
                else:
                    S(lambda e_, blk=blk, pTb=pTb: e_.copy(out=rT[:, :, blk * 128:(blk + 1) * 128], in_=pTb), r=[pT], w=[rT])
            for fc in range(4):
                pg, pu = B[2 + 2 * (fc % 2)], B[3 + 2 * (fc % 2)]
                for k in range(8):
                    self.mm(pg[:, 0:N], wg[i][:, k, fc * 128:(fc + 1) * 128], rT[:, k, 0:N], r=[wg[i], rT], w=[pg], start=(k == 0), stop=(k == 7))
                for k in range(8):
                    self.mm(pu[:, 0:N], wu[i][:, k, fc * 128:(fc + 1) * 128], rT[:, k, 0:N], r=[wu[i], rT], w=[pu], start=(k == 0), stop=(k == 7))
                sl = sil[fc % 2]
                S(lambda e_, sl=sl, pg=pg, N=N: e_.activation(out=sl[:, 0:N], in_=pg[:, 0:N], func=AF.Silu), r=[pg], w=[sl])
                V(lambda e_, sl=sl, pu=pu, fc=fc, N=N: e_.tensor_tensor(out=hidT[:, fc, 0:N], in0=pu[:, 0:N], in1=sl[:, 0:N], op=ALU.mult), r=[pu, sl], w=[hidT])
            for blk in range(nb):
                o = osb[ob % 2]
                ob += 1
                for half in range(2):
                    pd = B[6 + half]
                    for fc in range(4):
                        self.mm(pd[:, :], hidT[:, fc, blk * 128:(blk + 1) * 128], wd[i][:, fc, half * 512:(half + 1) * 512], r=[hidT, wd[i]], w=[pd],
                                start=(fc == 0), stop=(fc == 3))
                    V(lambda e_, o=o, pd=pd, half=half: e_.tensor_tensor(out=o[:, half * 512:(half + 1) * 512], in0=pd[:, :],
                                                                         in1=self.opg[:, 1, half * 512:(half + 1) * 512], op=ALU.mult), r=[pd, self.opg], w=[o], x=[o])
                self.dma("sp", self.oslots[r0 + blk * 128:r0 + (blk + 1) * 128, :], o[:, :], r=[o], w=[])

    def p5_alloc(self, l):
        s = self.sb
        self.lng2 = s("lng2", [128, D], F32); self.lnb2 = s("lnb2", [128, D], F32)
        self.dma("sp", self.lng2[:, :], self.ln2_g[l].partition_broadcast(128), r=[self.ln2_g], w=[self.lng2])
        self.dma("sp", self.lnb2[:, :], self.ln2_b[l].partition_broadcast(128), r=[self.ln2_b], w=[self.lnb2])
        self.rA = [s(f"rA{i}", [128, D], F32) for i in range(2)]
        self.rB = [s(f"rB{i}", [128, D], F32) for i in range(2)]
        self.x1r = [s(f"x1r{i}", [128, D], F32) for i in range(2)]
        self.x2t = [s(f"x2t{i}", [128, D], F32) for i in range(2)]
        self.st5 = s("st5", [128, 24], F32)
        self.y5 = [s(f"y5_{i}", [128, D], F32) for i in range(2)]

    def p5_loads(self, jj):
        if jj >= NCH:
            return
        rA_, rB_, x1r_ = self.rA[jj % 2], self.rB[jj % 2], self.x1r[jj % 2]
        self.cx.dma("pool", None, None, reads=[self.oslots.b, self.slotA.b], writes=[rA_.b],
                    fn=lambda e: e.indirect_dma_start(out=rA_[:, :], out_offset=None, in_=self.oslots[:, :],
                                                      in_offset=bass.IndirectOffsetOnAxis(ap=self.slotA[:, jj:jj + 1], axis=0)))
        self.cx.dma("pool", None, None, reads=[self.oslots.b, self.slotB.b], writes=[rB_.b],
                    fn=lambda e: e.indirect_dma_start(out=rB_[:, :], out_offset=None, in_=self.oslots[:, :],
                                                      in_offset=bass.IndirectOffsetOnAxis(ap=self.slotB[:, jj:jj + 1], axis=0)))
        self.dma("sp", x1r_[:, :], self.x1d[jj * 128:(jj + 1) * 128, :], r=[self.x1d], w=[x1r_])

    def p5_S1(self, j):
        if j >= NCH:
            return
        V, S, G = self.V, self.S, self.G
        rA, rB, x1r, y5 = self.rA[j % 2], self.rB[j % 2], self.x1r[j % 2], self.y5[j % 2]
        S(lambda e: e.activation(out=rA[:, :], in_=rA[:, :], func=AF.Identity, scale=self.gAB[:, 0, j:j + 1]), r=[rA, self.gAB], w=[rA])
        V(lambda e: e.scalar_tensor_tensor(out=rA[:, :], in0=rB[:, :], scalar=self.gAB[:, 1, j:j + 1], in1=rA[:, :], op0=ALU.mult, op1=ALU.add), r=[rB, rA, self.gAB], w=[rA])
        V(lambda e: e.scalar_tensor_tensor(out=y5[:, :], in0=x1r[:, :], scalar=float(ALPHA), in1=rA[:, :], op0=ALU.mult, op1=ALU.add), r=[x1r, rA], w=[y5], x=[rA])

    def p5_S2(self, j, dst):
        V, S, G = self.V, self.S, self.G
        rows = slice(j * 128, (j + 1) * 128)
        y5, x2 = self.y5[j % 2], self.x2t[j % 2]
        self.ln_stats(y5, lambda a, b: y5[:, a:b], D, self.st5, act=True)
        S(lambda e: e.activation(out=x2[:, :], in_=y5[:, :], func=AF.Identity, bias=self.st5[:, 4:5], scale=self.st5[:, 3:4]), r=[y5, self.st5], w=[x2])
        V(lambda e: e.tensor_tensor(out=x2[:, 0:512], in0=x2[:, 0:512], in1=self.lng2[:, 0:512], op=ALU.mult), r=[x2, self.lng2], w=[x2])
        G(lambda e: e.tensor_tensor(out=x2[:, 512:1024], in0=x2[:, 512:1024], in1=self.lng2[:, 512:1024], op=ALU.mult), r=[x2, self.lng2], w=[x2])
        V(lambda e: e.tensor_tensor(out=x2[:, 0:512], in0=x2[:, 0:512], in1=self.lnb2[:, 0:512], op=ALU.add), r=[x2, self.lnb2], w=[x2])
        G(lambda e: e.tensor_tensor(out=x2[:, 512:1024], in0=x2[:, 512:1024], in1=self.lnb2[:, 512:1024], op=ALU.add), r=[x2, self.lnb2], w=[x2])
        self.dma("sp", dst[rows, :], x2[:, :], r=[x2], w=[dst])

    def p5_all(self, l, dst):
        self.p5_loads(0); self.p5_loads(1)
        self.p5_S1(0)
        for j in range(NCH):
            self.p5_loads(j + 2)
            self.p5_S1(j + 1)
            self.p5_S2(j, dst)

    def build(self):
        self.declare_io(); self.s5_io(); self.p3_io(); self.p4_io()
        self.setup()
        x_src = self.x_in
        for l in range(self.nlayers):
            dst = self.out if l == self.nlayers - 1 else self.xmid
            self.push()
            self.layer_alloc(); self.route_alloc(); self.layer_prep(l, 0)
            self.zero_slots()
            self.push(); self.qk_alloc()
            self.push(); self.load_win(l); self.s5_alloc(); self.s5_prep(l); self.p1v2_alloc()
            self.p1_all(l, x_src)
            self.pop()
            self.push(); self.p2_alloc(); self.p2_attention(); self.pop()
            self.pop()
            self.push()
            self.opg = self.sb("opg", [128, 2, 1024], F32)
            self.opg2 = self.sb("opg2", [128, 2, 1024], F32)
            self.layer_prep(l, 1)
            self.push(); self.p3_alloc(l)
            self.p3_route_alloc()
            self.p3_all(l, x_src)
            self.pop()
            self.push(); self.p4_experts(l); self.pop()
            self.push(); self.p5_alloc(l)
            self.p5_all(l, dst)
            self.pop()
            self.pop()
            self.pop()
            x_src = dst
        if getattr(self, "dbg_hook", None):
            self.dbg_hook(self)
        self.finish()


def make_inputs(inp, b):
    c = np.ascontiguousarray
    f = lambda k: np.asarray(inp[k])
    br, bi = f("ssm_b_re"), f("ssm_b_im")
    def bl(a):
        L = a.shape[0]
        return a.reshape(L, 2, 8, 64, 16).transpose(0, 2, 4, 1, 3).reshape(L, 128, 2, 64)
    bT = np.stack([bl(br), bl(bi)], axis=1)
    cr, ci = f("ssm_c_re"), f("ssm_c_im")
    cT = np.concatenate([cr.transpose(0, 1, 3, 2), ci.transpose(0, 1, 3, 2)], axis=2)
    L = br.shape[0]
    d = {
        "x": c(f("x")[b]), "ccol": c(f("c")[b].reshape(8, 128).T), "pos": c(f("positions")[b].reshape(32, 128).T),
        "ada_w": f("ada_w"), "ada_b": f("ada_b"), "w_in": f("w_in"), "gm_ln_g": f("gm_ln_g"), "gm_ln_b": f("gm_ln_b"),
        "gm_ws": f("gm_ws"), "gm_bsT": c(f("gm_bs").transpose(0, 2, 1)),
        "lam_re": c(f("ssm_lam_re").reshape(L, 1024)), "lam_im": c(f("ssm_lam_im").reshape(L, 1024)), "log_dt": f("ssm_log_dt"),
        "ssm_bT": c(bT), "ssm_cT": c(cT), "ssm_dT": c(f("ssm_d").reshape(L, 2, 128).transpose(0, 2, 1)),
        "glu_w": f("glu_w"), "glu_bT": c(f("glu_b").reshape(L, 2, 128).transpose(0, 2, 1)),
        "w_out": f("w_out"), "ln1_g": f("ln1_g"), "ln1_b": f("ln1_b"), "ln2_g": f("ln2_g"), "ln2_b": f("ln2_b"),
        "router_w": f("router_w"), "router_bias": f("router_bias"),
        "exp_w_gate": f("exp_w_gate"), "exp_w_up": f("exp_w_up"), "exp_w_down": f("exp_w_down"),
    }
    return d


_CACHE = {}


def kernel(**inputs):
    n = 8
    if "nc" not in _CACHE:
        nc = bass.Bass("TRN2", target_bir_lowering=False)
        kb = KB(nc)
        kb.build()
        _CACHE["nc"] = nc
        _CACHE["names"] = kb.in_names
    nc = _CACHE["nc"]
    names = _CACHE["names"]
    in_maps = []
    for b in range(n):
        im = make_inputs(inputs, b)
        in_maps.append({k: v for k, v in im.items() if k in names})
    res = run_bass_kernel_spmd(nc, in_maps, core_ids=list(range(n)))
    out = np.stack([np.asarray(r["out"]) for r in res.results], axis=0)
    return out.astype(np.float32)
```

```python
import numpy as np
import concourse.bass as bass
import concourse.mybir as mybir

F32 = mybir.dt.float32
BF16 = mybir.dt.bfloat16
I32 = mybir.dt.int32
U32 = mybir.dt.uint32
AF = mybir.ActivationFunctionType
ALU = mybir.AluOpType
AX = mybir.AxisListType


RELAXED = ()
ALLOW_RELAX = True


class Buf:
    __slots__ = ("w", "r", "name")

    def __init__(self, name=""):
        self.w = None
        self.r = []
        self.name = name


class Ctx:
    def __init__(self, nc, strict_same=False):
        self.nc = nc
        self.strict_same = strict_same
        self.relaxed = set(RELAXED)
        self.engs = {"pe": nc.tensor, "act": nc.scalar, "dve": nc.vector, "pool": nc.gpsimd, "sp": nc.sync}
        self.sem = {}
        self.cnt = {}
        for e in ("pe", "act", "dve", "pool"):
            self.sem[e] = nc.alloc_semaphore("s_" + e)
            self.cnt[e] = 0
        self.dq = {}
        for q, n in (("sp", 10), ("act", 4), ("pool", 8)):
            self.dq[q] = {"sems": [nc.alloc_semaphore(f"d_{q}{i}") for i in range(n)], "vals": [0] * n, "k": 0}
        self.waited = {}
        self.nbuf = 0
        self.out_events = []

    def buf(self, name=""):
        return Buf(name)

    def _wait(self, eng, ev):
        sem, val = ev
        key = (eng, id(sem))
        if self.waited.get(key, 0) >= val:
            return
        self.engs[eng].wait_ge(sem, val)
        self.waited[key] = val

    def _deps(self, eng, reads, writes, relax=()):
        own = self.sem.get(eng)
        rl = set(id(b) for b in relax) if ALLOW_RELAX else set()

        def chk(b, ev):
            if ev[0] is own and (eng == "pe" or id(b) in rl):
                return
            self._wait(eng, ev)
        for b in reads:
            if b.w is not None:
                chk(b, b.w)
        for b in writes:
            if b.w is not None:
                chk(b, b.w)
            for ev in b.r:
                chk(b, ev)

    def _commit(self, ev, reads, writes):
        for b in writes:
            b.w = ev
            b.r = []
        for b in reads:
            b.r.append(ev)
            if len(b.r) > 24:
                b.r = b.r[-24:]

    def op(self, eng, fn, reads=(), writes=(), signal=True, relax=()):
        self._deps(eng, reads, writes, relax)
        inst = fn(self.engs[eng])
        if signal:
            self.cnt[eng] += 1
            inst.then_inc(self.sem[eng], 1)
            ev = (self.sem[eng], self.cnt[eng])
        else:
            ev = (self.sem[eng], self.cnt[eng] + 1)
        self._commit(ev, reads, writes)
        return ev

    def dma(self, q, out, in_, reads=(), writes=(), fn=None, **kw):
        d = self.dq[q]
        i = d["k"] % len(d["sems"])
        d["k"] += 1
        sem = d["sems"][i]
        self._deps(q, reads, writes)
        if d["vals"][i] > 0:
            self._wait(q, (sem, d["vals"][i]))
        if fn is None:
            inst = self.engs[q].dma_start(out=out, in_=in_, **kw)
        else:
            inst = fn(self.engs[q])
        d["vals"][i] += 16
        inst.then_inc(sem, 16)
        ev = (sem, d["vals"][i])
        self._commit(ev, reads, writes)
        return ev

    def barrier(self):
        evs = [(self.sem[e], self.cnt[e]) for e in self.sem if self.cnt[e] > 0]
        for q, d in self.dq.items():
            for sem, v in zip(d["sems"], d["vals"]):
                if v > 0:
                    evs.append((sem, v))
        for eng in ("pe", "act", "dve", "pool", "sp"):
            own = self.sem.get(eng)
            for ev in evs:
                self._wait(eng, ev)

    def finish(self, bufs):
        for b in bufs:
            if b.w is not None:
                self._wait("sp", b.w)
            for ev in b.r:
                self._wait("sp", ev)

from concourse.bass_utils import run_bass_kernel_spmd
import math
import contextlib

T = 4096
D = 1024
NCH = 32
PW = 2304
EPS = 1e-5
ALPHA = (2.0 * 2) ** 0.25
TWO_PI = 2.0 * math.pi
ROPE_THETA = 500000.0


class Tl:
    def __init__(self, h, name=""):
        self.h = h
        self.b = Buf(name)

    def __getitem__(self, k):
        return self.h[k]


class KB:
    def __init__(self, nc, nlayers=2, dbg=(), stop_after=None):
        self.nc = nc
        self.cx = Ctx(nc)
        self.dbg = set(dbg)
        self.stop_after = stop_after
        self.nlayers = nlayers
        self.outs = []
        self.stk = [contextlib.ExitStack()]
        self.nps = 0

    def inp(self, name, shape, dt=F32):
        self.in_names = getattr(self, "in_names", set())
        self.in_names.add(name)
        return Tl(self.nc.dram_tensor(name, list(shape), dt, kind="ExternalInput").ap(), name)

    def outp(self, name, shape, dt=F32):
        t = Tl(self.nc.dram_tensor(name, list(shape), dt, kind="ExternalOutput").ap(), name)
        self.outs.append(t)
        return t

    def scratch(self, name, shape, dt):
        return Tl(self.nc.dram_tensor(name, list(shape), dt, kind="Internal").ap(), name)

    def sb(self, name, shape, dt):
        self.nsb = getattr(self, "nsb", 0) + 1
        h = self.stk[-1].enter_context(self.nc.sbuf_tensor(f"{name}_{self.nsb}", list(shape), dt))
        return Tl(h, name)

    def push(self):
        self.stk.append(contextlib.ExitStack())

    def pop(self):
        self.cx.barrier()
        self.stk.pop().close()

    def ps(self, name, shape, dt=F32):
        return Tl(self.nc.alloc_psum_tensor(name, list(shape), dt), name)

    def _rw(self, r, w):
        return [t.b for t in r], [t.b for t in w]

    def V(self, fn, r=(), w=(), x=()):
        r, w = self._rw(r, w)
        return self.cx.op("dve", fn, r, w, relax=[t.b for t in x])

    def S(self, fn, r=(), w=(), x=()):
        r, w = self._rw(r, w)
        return self.cx.op("act", fn, r, w, relax=[t.b for t in x])

    def G(self, fn, r=(), w=(), x=()):
        r, w = self._rw(r, w)
        return self.cx.op("pool", fn, r, w, relax=[t.b for t in x])

    def P(self, fn, r=(), w=(), signal=True):
        r, w = self._rw(r, w)
        return self.cx.op("pe", fn, r, w, signal=signal)

    def dma(self, q, out, in_, r=(), w=(), **kw):
        r, w = self._rw(r, w)
        return self.cx.dma(q, out, in_, r, w, **kw)

    def mm(self, out, lhsT, rhs, r, w, start, stop, signal=None):
        if signal is None:
            signal = stop
        return self.P(lambda e: e.matmul(out, lhsT, rhs, start=start, stop=stop), r, w, signal=signal)

    def tr(self, out, in_, ident, r, w, signal=True):
        return self.P(lambda e: e.transpose(out, in_, ident), r, w, signal=signal)

    def declare_io(self):
        i = self.inp
        self.x_in = i("x", [T, D])
        self.ccol = i("ccol", [128, 8])
        self.pos = i("pos", [128, NCH], I32)
        self.ada_w = i("ada_w", [2, D, 6 * D])
        self.ada_b = i("ada_b", [2, 6 * D])
        self.w_in = i("w_in", [2, D, PW])
        self.gm_ln_g = i("gm_ln_g", [2, 256])
        self.gm_ln_b = i("gm_ln_b", [2, 256])
        self.gm_ws = i("gm_ws", [2, 4, 128, 128])
        self.gm_bsT = i("gm_bsT", [2, 128, 4])
        self.out = self.outp("out", [T, D])
        self.mixtok = self.scratch("mixtok", [T, 1024], BF16)
        self.vd = self.scratch("vd", [T, 520], BF16)

    def consts(self):
        nc = self.nc
        self.identb = self.sb("identb", [128, 128], BF16)
        self.identf = self.sb("identf", [128, 128], F32)
        self.onesf = self.sb("onesf", [128, 128], F32)
        self.eps_t = self.sb("eps_t", [128, 1], F32)
        self.G(lambda e: e.memset(self.onesf[:, :], 1.0), w=[self.onesf])
        self.G(lambda e: e.memset(self.eps_t[:, :], EPS), w=[self.eps_t])
        self.mhalf = self.sb("mhalf", [128, 1], F32)
        self.G(lambda e: e.memset(self.mhalf[:, :], -0.5), w=[self.mhalf])
        self.G(lambda e: e.affine_select(out=self.identf[:, :], in_=self.onesf[:, :], pattern=[[-1, 128]],
                                         compare_op=ALU.is_equal, fill=0.0, base=0, channel_multiplier=1),
               r=[self.onesf], w=[self.identf])
        self.G(lambda e: e.tensor_copy(out=self.identb[:, :], in_=self.identf[:, :]), r=[self.identf], w=[self.identb])
        self.posf = self.sb("posf", [128, NCH], F32)
        self.posi = self.sb("posi", [128, NCH], I32)
        self.dma("sp", self.posi[:, :], self.pos[:, :], r=[self.pos], w=[self.posi])
        self.V(lambda e: e.tensor_copy(out=self.posf[:, :], in_=self.posi[:, :]), r=[self.posi], w=[self.posf])
        self.cs = self.sb("cs", [128, NCH, 8], F32)
        self.sn = self.sb("sn", [128, NCH, 8], F32)
        self.push()
        ang = self.sb("ang", [128, NCH, 8], F32)
        tmp = self.sb("angt", [128, NCH, 8], F32)
        tmi = self.sb("angi", [128, NCH, 8], I32)
        for j in range(8):
            fr = ROPE_THETA ** (-(j * 2.0) / 16.0)
            self.V(lambda e, j=j, fr=fr: e.tensor_scalar(out=ang[:, :, j], in0=self.posf[:, :], scalar1=float(fr), scalar2=None, op0=ALU.mult),
                   r=[self.posf], w=[ang])
        self.sincos(ang, self.sn, self.cs, tmp, tmi, [128, NCH * 8])
        self.pop()

    def _flat(self, t):
        ap = t[:]
        if len(ap.shape) == 2:
            return ap
        names = " ".join(f"a{i}" for i in range(len(ap.shape) - 1))
        return ap.rearrange(f"p {names} -> p ({names})")

    def range_reduce(self, src, dst, tmp, tmi, shift):
        s, d, t, ti = self._flat(src), self._flat(dst), self._flat(tmp), self._flat(tmi)
        self.V(lambda e: e.tensor_scalar(out=t, in0=s, scalar1=float(shift), scalar2=float(1.0 / TWO_PI), op0=ALU.add, op1=ALU.mult),
               r=[src], w=[tmp])
        self.V(lambda e: e.tensor_copy(out=ti, in_=t), r=[tmp], w=[tmi])
        self.V(lambda e: e.tensor_copy(out=t, in_=ti), r=[tmi], w=[tmp])
        self.V(lambda e: e.tensor_scalar(out=t, in0=t, scalar1=float(-TWO_PI), scalar2=float(shift), op0=ALU.mult, op1=ALU.add),
               r=[tmp], w=[tmp])
        self.V(lambda e: e.tensor_tensor(out=d, in0=t, in1=s, op=ALU.add), r=[tmp, src], w=[dst])
        self.V(lambda e: e.tensor_scalar(out=t, in0=d, scalar1=float(math.pi), scalar2=float(-TWO_PI), op0=ALU.is_gt, op1=ALU.mult),
               r=[dst], w=[tmp])
        self.V(lambda e: e.tensor_tensor(out=d, in0=d, in1=t, op=ALU.add), r=[tmp, dst], w=[dst])
        self.V(lambda e: e.tensor_scalar(out=t, in0=d, scalar1=float(-math.pi), scalar2=float(TWO_PI), op0=ALU.is_lt, op1=ALU.mult),
               r=[dst], w=[tmp])
        self.V(lambda e: e.tensor_tensor(out=d, in0=d, in1=t, op=ALU.add), r=[tmp, dst], w=[dst])
        self.V(lambda e: e.tensor_scalar(out=d, in0=d, scalar1=float(math.pi), scalar2=float(-math.pi), op0=ALU.min, op1=ALU.max),
               r=[dst], w=[dst])

    def sincos(self, ang, sn, cs, tmp, tmi, shape):
        self.range_reduce(ang, sn, tmp, tmi, 0.0)
        self.S(lambda e: e.activation(out=self._flat(sn), in_=self._flat(sn), func=AF.Sin), r=[sn], w=[sn])
        self.range_reduce(ang, cs, tmp, tmi, math.pi / 2)
        self.S(lambda e: e.activation(out=self._flat(cs), in_=self._flat(cs), func=AF.Sin), r=[cs], w=[cs])

    def setup(self):
        self.bank = [self.ps(f"bank{i}", [128, 512], F32) for i in range(8)]
        self.consts()

    def layer_alloc(self):
        self.modp = self.sb("modp", [128, 4, 8], F32)

        self.wsT = self.sb("wsT", [128, 4, 128], BF16)
        self.gbs = self.sb("gbs", [128, 4], F32)
        self.glng = self.sb("glng", [128, 256], F32)
        self.glnb = self.sb("glnb", [128, 256], F32)

    def prep_alloc(self):
        self.adaw = [self.sb(f"adaw{i}", [128, 8, 512], F32) for i in range(2)]
        self.adab = [self.sb(f"adab{i}", [128, 512], F32) for i in range(2)]
        self.modc = [self.sb(f"modc{i}", [128, 512], F32) for i in range(2)]
        self.wtmp = self.sb("wtmp", [128, 4, 128], F32)
        ccs = self.sb("ccs", [128, 8], F32)
        self.dma("sp", ccs[:, :], self.ccol[:, :], r=[self.ccol], w=[ccs])
        self.S(lambda e: e.activation(out=ccs[:, :], in_=ccs[:, :], func=AF.Silu), r=[ccs], w=[ccs])
        self.condrep = self.sb("condrep", [128, 8, 128], F32)
        self.V(lambda e: e.tensor_copy(out=self.condrep[:, :, :], in_=ccs[:, :].unsqueeze(2).to_broadcast([128, 8, 128])),
               r=[ccs], w=[self.condrep])


    def load_win(self, l):
        self.win = self.sb("win", [128, 8, PW], BF16)
        for k in range(8):
            self.dma("pool", self.win[:, k, :], self.w_in[l, k * 128:(k + 1) * 128, :], r=[self.w_in], w=[self.win])

    def layer_prep(self, l, part=0):
        self.push()
        self.prep_alloc()
        pb = self.bank[7]
        pt = self.bank[6]
        for n in range(12):
            if (part == 0) != (n // 2 in (0, 1)):
                continue
            aw = self.adaw[n % 2]
            ab = self.adab[n % 2]
            mc = self.modc[n % 2]
            self.dma("sp", aw[:, :, :], self.ada_w[l, :, n * 512:(n + 1) * 512].rearrange("(k p) n -> p k n", p=128),
                     r=[self.ada_w], w=[aw])
            self.dma("sp", ab[:, :], self.ada_b[l, n * 512:(n + 1) * 512].partition_broadcast(128), r=[self.ada_b], w=[ab])
            for k in range(8):
                self.mm(pb[:, :], self.condrep[:, k, :], aw[:, k, :], r=[self.condrep, aw], w=[pb], start=(k == 0), stop=(k == 7))
            which, half = n // 2, n % 2
            if which in (2, 5, 3, 4):
                tgt = self.opg if which in (2, 5) else self.opg2
                gi = {2: 0, 5: 1, 3: 0, 4: 1}[which]
                dst = tgt[:, gi, half * 512:(half + 1) * 512]
                self.V(lambda e, dst=dst: e.tensor_tensor(out=dst, in0=pb[:, :], in1=ab[:, :], op=ALU.add), r=[pb, ab], w=[tgt])
                if which != 3:
                    self.V(lambda e, dst=dst: e.tensor_scalar(out=dst, in0=dst, scalar1=1.0, scalar2=None, op0=ALU.add), r=[tgt], w=[tgt])
            else:
                slot = {0: 0, 1: 1}[which]
                self.V(lambda e: e.tensor_tensor(out=mc[:, :], in0=pb[:, :], in1=ab[:, :], op=ALU.add), r=[pb, ab], w=[mc])
                for b4 in range(4):
                    self.tr(pt[:, b4 * 128:(b4 + 1) * 128], mc[:, b4 * 128:(b4 + 1) * 128], self.identf[:, :], r=[mc, self.identf], w=[pt])
                addc = 1.0 if slot in (1, 3) else 0.0
                for b4 in range(4):
                    self.V(lambda e, b4=b4: e.tensor_scalar(out=self.modp[:, slot, half * 4 + b4:half * 4 + b4 + 1],
                                                            in0=pt[:, b4 * 128:b4 * 128 + 1], scalar1=float(addc), scalar2=None, op0=ALU.add),
                           r=[pt], w=[self.modp])
        if part == 0:
            self.dma("sp", self.wtmp[:, :, :], self.gm_ws[l].rearrange("h t s -> t h s"), r=[self.gm_ws], w=[self.wtmp])
            self.G(lambda e: e.affine_select(out=self.wtmp[:, :, :], in_=self.wtmp[:, :, :], pattern=[[0, 4], [-1, 128]],
                                             compare_op=ALU.is_ge, fill=0.0, base=0, channel_multiplier=1), r=[self.wtmp], w=[self.wtmp])
            for h in range(4):
                self.tr(pt[:, h * 128:(h + 1) * 128], self.wtmp[:, h, :], self.identf[:, :], r=[self.wtmp, self.identf], w=[pt])
            self.V(lambda e: e.tensor_copy(out=self.wsT[:, :, :], in_=pt[:, :].rearrange("p (h t) -> p h t", h=4)), r=[pt], w=[self.wsT])
            self.dma("sp", self.gbs[:, :], self.gm_bsT[l], r=[self.gm_bsT], w=[self.gbs])
            self.dma("sp", self.glng[:, :], self.gm_ln_g[l].partition_broadcast(128), r=[self.gm_ln_g], w=[self.glng])

            self.dma("sp", self.glnb[:, :], self.gm_ln_b[l].partition_broadcast(128), r=[self.gm_ln_b], w=[self.glnb])
        self.pop()

    def ln_stats(self, src, src_ap_fn, n, st, act=False):
        if act:
            return self.ln_stats_act(src, src_ap_fn, n, st)
        nchk = (n + 511) // 512
        w = n // nchk
        for i in range(nchk):
            self.V(lambda e, i=i: e.bn_stats(out=st[:, 8 + i * 6:8 + (i + 1) * 6], in_=src_ap_fn(i * w, (i + 1) * w)), r=[src], w=[st])
        self.V(lambda e: e.bn_aggr(out=st[:, 0:2], in_=st[:, 8:8 + 6 * nchk]), r=[st], w=[st])
        self.V(lambda e: e.tensor_scalar(out=st[:, 2:3], in0=st[:, 1:2], scalar1=float(EPS), scalar2=None, op0=ALU.add), r=[st], w=[st])
        self.G(lambda e: e.tensor_tensor(out=st[:, 3:4], in0=st[:, 2:3], in1=self.mhalf[:, 0:1], op=ALU.pow), r=[st, self.mhalf], w=[st])
        self.V(lambda e: e.tensor_scalar(out=st[:, 4:5], in0=st[:, 0:1], scalar1=-1.0, scalar2=st[:, 3:4], op0=ALU.mult, op1=ALU.mult), r=[st], w=[st])

    def ln_stats_act(self, src, src_ap_fn, n, st):
        nchk = (n + 511) // 512
        w = n // nchk
        for i in range(nchk):
            self.V(lambda e, i=i: e.bn_stats(out=st[:, 8 + i * 6:8 + (i + 1) * 6], in_=src_ap_fn(i * w, (i + 1) * w)), r=[src], w=[st])
        self.V(lambda e: e.bn_aggr(out=st[:, 0:2], in_=st[:, 8:8 + 6 * nchk]), r=[st], w=[st])
        self.S(lambda e: e.activation(out=st[:, 2:3], in_=st[:, 1:2], func=AF.Sqrt, bias=self.eps_t[:, 0:1], scale=1.0), r=[st, self.eps_t], w=[st])
        self.V(lambda e: e.reciprocal(out=st[:, 3:4], in_=st[:, 2:3]), r=[st], w=[st])
        self.V(lambda e: e.tensor_scalar(out=st[:, 4:5], in0=st[:, 0:1], scalar1=-1.0, scalar2=st[:, 3:4], op0=ALU.mult, op1=ALU.mult), r=[st], w=[st])

    def qk_alloc(self):
        self.QT = self.sb("QT", [128, 4, T], BF16)
        self.KT = self.sb("KT", [128, 4, T], BF16)

    def p1_alloc(self):
        s = self.sb
        self.xt = [s(f"xt{i}", [128, D], F32) for i in range(2)]
        self.xn = [s(f"xn{i}", [128, D], BF16) for i in range(2)]
        self.st = [s(f"st{i}", [128, 24], F32) for i in range(2)]
        self.st2 = [s(f"stb{i}", [128, 24], F32) for i in range(2)]
        self.hT = [s(f"hT{i}", [128, 8, 128], BF16) for i in range(2)]
        self.gl = [s(f"gl{i}", [128, 512], F32) for i in range(1)] * 2
        self.vnb = [s(f"vnb{i}", [128, 256], F32) for i in range(1)] * 2
        self.vb = [s(f"vb{i}", [128, 256], BF16) for i in range(1)] * 2
        self.aout = [s(f"aout{i}", [128, 256], BF16) for i in range(1)] * 2
        self.qf = [s(f"qf{i}", [128, 2, 512], F32) for i in range(1)] * 2
        self.qb = [s(f"qb{i}", [128, 2, 512], BF16) for i in range(1)] * 2
        self.rt = [s(f"rt{i}", [128, 4, 8, 8], F32) for i in range(1)] * 2
        self.vp = [s(f"vp{i}", [128, 8, 65], BF16) for i in range(1)] * 2
        for i in range(1):
            self.G(lambda e, i=i: e.memset(self.vp[i][:, :, :], 1.0), w=[self.vp[i]])

    def p1_chunk(self, l, j, x_src):
        i2 = j % 2
        xt, xn, st, st2, hT = self.xt[i2], self.xn[i2], self.st[i2], self.st2[i2], self.hT[i2]
        gl, vnb, vb, aout, qf, qb, rt, vp = self.gl[i2], self.vnb[i2], self.vb[i2], self.aout[i2], self.qf[i2], self.qb[i2], self.rt[i2], self.vp[i2]
        B = self.bank
        rows = slice(j * 128, (j + 1) * 128)
        if j == 0:
            self.dma("sp", xt[:, :], x_src[rows, :], r=[x_src], w=[xt])
        if j + 1 < NCH:
            xtn = self.xt[(j + 1) % 2]
            self.dma("sp", xtn[:, :], x_src[(j + 1) * 128:(j + 2) * 128, :], r=[x_src], w=[xtn])
        self.ln_stats(xt, lambda a, b: xt[:, a:b], D, st)
        self.S(lambda e: e.activation(out=xn[:, :], in_=xt[:, :], func=AF.Identity, bias=st[:, 4:5], scale=st[:, 3:4]), r=[xt, st], w=[xn])
        pT = B[0]
        pTb = pT[:, :].bitcast(BF16).rearrange("p (k t) -> p k t", k=8)
        for k in range(8):
            self.tr(pTb[:, k, :], xn[:, k * 128:(k + 1) * 128], self.identb[:, :], r=[xn, self.identb], w=[pT], signal=(k == 7))
        for k in range(8):
            self.V(lambda e, k=k: e.tensor_scalar(out=hT[:, k, :], in0=pTb[:, k, :], scalar1=self.modp[:, 1, k:k + 1], scalar2=self.modp[:, 0, k:k + 1],
                                                  op0=ALU.mult, op1=ALU.add), r=[pT, self.modp], w=[hT], x=[hT])
        for bi, c0 in ((1, 0), (2, 512), (3, 1024), (4, 1536)):
            for k in range(8):
                self.mm(B[bi][:, :], hT[:, k, :], self.win[:, k, c0:c0 + 512], r=[hT, self.win], w=[B[bi]], start=(k == 0), stop=(k == 7))
        self.S(lambda e: e.activation(out=gl[:, :], in_=B[1][:, :], func=AF.Gelu_apprx_tanh), r=[B[1]], w=[gl])
        self.ln_stats(gl, lambda a, b: gl[:, 256 + a:256 + b], 256, st2)
        self.V(lambda e: e.tensor_scalar(out=vnb[:, :], in0=gl[:, 256:512], scalar1=st2[:, 3:4], scalar2=st2[:, 4:5], op0=ALU.mult, op1=ALU.add),
               r=[gl, st2], w=[vnb])
        self.V(lambda e: e.tensor_tensor(out=vnb[:, :], in0=vnb[:, :], in1=self.glng[:, :], op=ALU.mult), r=[vnb, self.glng], w=[vnb], x=[vnb])
        self.V(lambda e: e.tensor_tensor(out=vb[:, :], in0=vnb[:, :], in1=self.glnb[:, :], op=ALU.add), r=[vnb, self.glnb], w=[vb], x=[vnb])
        psv = B[5]
        for h in range(4):
            self.mm(psv[:, h * 64:(h + 1) * 64], self.wsT[:, h, :], vb[:, h * 64:(h + 1) * 64], r=[self.wsT, vb], w=[psv], start=True, stop=True,
                    signal=(h == 3))
        for h in range(4):
            self.V(lambda e, h=h: e.scalar_tensor_tensor(out=aout[:, h * 64:(h + 1) * 64], in0=psv[:, h * 64:(h + 1) * 64], scalar=self.gbs[:, h:h + 1],
                                                         in1=gl[:, h * 64:(h + 1) * 64], op0=ALU.add, op1=ALU.mult), r=[psv, self.gbs, gl], w=[aout], x=[aout])
        self.dma("sp", self.mixtok[rows, 0:256], aout[:, :], r=[aout], w=[self.mixtok])
        self.S(lambda e: e.copy(out=qf[:, 0, :], in_=B[2][:, :]), r=[B[2]], w=[qf])
        self.S(lambda e: e.copy(out=qf[:, 1, :], in_=B[3][:, :]), r=[B[3]], w=[qf], x=[qf])
        q4 = qf[:, :, :].rearrange("p a (h e) -> p (a h) e", e=64)
        o4 = qb[:, :, :].rearrange("p a (h e) -> p (a h) e", e=64)
        for a in range(2):
            xa1, xa2 = q4[:, a * 8:(a + 1) * 8, 0:8], q4[:, a * 8:(a + 1) * 8, 8:16]
            cb = self.cs[:, j, :].unsqueeze(1).to_broadcast([128, 8, 8])
            sb_ = self.sn[:, j, :].unsqueeze(1).to_broadcast([128, 8, 8])
            oa = o4[:, a * 8:(a + 1) * 8, :]
            self.G(lambda e, xa1=xa1, cb=cb: e.tensor_tensor(out=rt[:, 0, :, :], in0=xa1, in1=cb, op=ALU.mult), r=[qf, self.cs], w=[rt])
            self.G(lambda e, xa2=xa2, sb_=sb_: e.tensor_tensor(out=rt[:, 1, :, :], in0=xa2, in1=sb_, op=ALU.mult), r=[qf, self.sn], w=[rt])
            self.G(lambda e, xa2=xa2, cb=cb: e.tensor_tensor(out=rt[:, 2, :, :], in0=xa2, in1=cb, op=ALU.mult), r=[qf, self.cs], w=[rt])
            self.G(lambda e, xa1=xa1, sb_=sb_: e.tensor_tensor(out=rt[:, 3, :, :], in0=xa1, in1=sb_, op=ALU.mult), r=[qf, self.sn], w=[rt])
            self.G(lambda e, oa=oa: e.tensor_tensor(out=oa[:, :, 0:8], in0=rt[:, 0, :, :], in1=rt[:, 1, :, :], op=ALU.subtract), r=[rt], w=[qb])
            self.G(lambda e, oa=oa: e.tensor_tensor(out=oa[:, :, 8:16], in0=rt[:, 2, :, :], in1=rt[:, 3, :, :], op=ALU.add), r=[rt], w=[qb])
            self.G(lambda e, oa=oa, a=a: e.tensor_copy(out=oa[:, :, 16:64], in_=q4[:, a * 8:(a + 1) * 8, 16:64]), r=[qf], w=[qb])
        pq = B[6]
        pqb = pq[:, :].bitcast(BF16).rearrange("p (a k t) -> p a k t", a=2, k=4)
        for a in range(2):
            for k in range(4):
                self.tr(pqb[:, a, k, :], qb[:, a, k * 128:(k + 1) * 128], self.identb[:, :], r=[qb, self.identb], w=[pq], signal=(a == 1 and k == 3))
        self.S(lambda e: e.copy(out=self.QT[:, :, j * 128:(j + 1) * 128], in_=pqb[:, 0, :, :]), r=[pq], w=[self.QT])
        self.S(lambda e: e.copy(out=self.KT[:, :, j * 128:(j + 1) * 128], in_=pqb[:, 1, :, :]), r=[pq], w=[self.KT])
        self.S(lambda e: e.copy(out=vp[:, :, 0:64], in_=B[4][:, :].rearrange("p (h e) -> p h e", e=64)), r=[B[4]], w=[vp])
        self.dma("sp", self.vd[rows, :], vp[:, :, :].rearrange("p h e -> p (h e)"), r=[vp], w=[self.vd])

    def finish(self):
        bufs = [t.b for t in self.outs]
        self.cx.finish(bufs)

    def p2_alloc(self):
        s = self.sb
        self.vbr = [s(f"vbr{i}", [128, 32, 520], BF16) for i in range(2)]
        self.negm = s("negm", [128, 256], BF16)
        negf = s("negf", [128, 256], F32)
        zf = s("zf", [128, 256], F32)
        self.G(lambda e: e.memset(zf[:, :], 0.0), w=[zf])
        self.G(lambda e: e.affine_select(out=negf[:, 0:128], in_=zf[:, 0:128], pattern=[[-1, 128]], compare_op=ALU.is_ge, fill=-30000.0,
                                         base=0, channel_multiplier=1), r=[zf], w=[negf])
        self.G(lambda e: e.affine_select(out=negf[:, 128:256], in_=zf[:, 128:256], pattern=[[1, 128]], compare_op=ALU.is_ge, fill=-30000.0,
                                         base=0, channel_multiplier=-1), r=[zf], w=[negf])
        self.G(lambda e: e.tensor_copy(out=self.negm[:, :], in_=negf[:, :]), r=[negf], w=[self.negm])
        self.m01 = s("m01", [128, 256], BF16)
        onef = s("onef2", [128, 256], F32)
        self.G(lambda e: e.memset(onef[:, :], 1.0), w=[onef])
        self.G(lambda e: e.affine_select(out=onef[:, 0:128], in_=onef[:, 0:128], pattern=[[-1, 128]], compare_op=ALU.is_ge, fill=0.0,
                                         base=0, channel_multiplier=1), r=[onef], w=[onef])
        self.G(lambda e: e.affine_select(out=onef[:, 128:256], in_=onef[:, 128:256], pattern=[[1, 128]], compare_op=ALU.is_ge, fill=0.0,
                                         base=0, channel_multiplier=-1), r=[onef], w=[onef])
        self.G(lambda e: e.tensor_copy(out=self.m01[:, :], in_=onef[:, :]), r=[onef], w=[self.m01])
        self.pexp = [s(f"pexp{i}", [128, 256], BF16) for i in range(6)]
        self.osb = [s(f"osb{i}", [128, 520], F32) for i in range(2)]

    def p2_attention(self):
        B = self.bank
        hb = 0
        self._hb = 0
        dils = (1, 4, 16)

        def load_vb(bi):
            d = dils[bi]
            vb_ = self.vbr[bi % 2]
            src = self.vd[:, :].rearrange("(n l r) c -> l n r c", l=128, r=d)
            for n in range(T // (128 * d)):
                self.dma("sp", vb_[:, n * d:(n + 1) * d, :], src[:, n, :, :], r=[self.vd], w=[vb_])
        load_vb(0)
        load_vb(1)
        for bi, d in enumerate(dils):
            seg = 128 * d
            nseg = T // seg
            vb = self.vbr[bi % 2]
            if bi == 1:
                load_vb(2)
            odst = self.obr[bi][:, :].rearrange("(n l r) c -> l n r c", l=128, r=d)
            blk = 0
            for n in range(nseg):
                for r_ in range(d):
                    cols = slice(n * seg + r_, (n + 1) * seg, d)
                    pcols = slice((n - 1) * seg + r_, n * seg, d)
                    bcur = n * d + r_
                    bprev = (n - 1) * d + r_
                    po = [B[6], B[7]]
                    osb = self.osb[blk % 2]
                    def scores(h):
                        nonlocal hb
                        hp, p0 = h // 2, (h % 2) * 64
                        ps = B[hb % 6]
                        o0 = 0
                        pe_ = self.pexp[hb % 6]
                        hb += 1
                        c0 = 0 if n > 0 else 128
                        if n > 0:
                            self.mm(ps[:, o0:o0 + 128], self.KT[p0:p0 + 64, hp, pcols], self.QT[p0:p0 + 64, hp, cols], r=[self.KT, self.QT], w=[ps],
                                    start=True, stop=True, signal=False)
                        self.mm(ps[:, o0 + 128:o0 + 256], self.KT[p0:p0 + 64, hp, cols], self.QT[p0:p0 + 64, hp, cols], r=[self.KT, self.QT], w=[ps],
                                start=True, stop=True)
                        self.S(lambda e, pe_=pe_, ps=ps, c0=c0, o0=o0: e.activation(out=pe_[:, c0:256], in_=ps[:, o0 + c0:o0 + 256], func=AF.Exp, scale=0.125),
                               r=[ps], w=[pe_])
                        mk = self.V if (h % 2 == 0) else self.G
                        mk(lambda e, pe_=pe_, c0=c0: e.tensor_tensor(out=pe_[:, c0:256], in0=pe_[:, c0:256], in1=self.m01[:, c0:256], op=ALU.mult),
                           r=[pe_, self.m01], w=[pe_])
                        return pe_

                    def pv(h, pe_):
                        pob = po[h // 4]
                        oc = slice((h % 4) * 65, (h % 4) * 65 + 65)
                        if n > 0:
                            self.mm(pob[:, oc], pe_[:, 0:128], vb[:, bprev, h * 65:(h + 1) * 65], r=[pe_, vb], w=[pob], start=True, stop=False)
                        self.mm(pob[:, oc], pe_[:, 128:256], vb[:, bcur, h * 65:(h + 1) * 65], r=[pe_, vb], w=[pob], start=(n == 0), stop=True)
                    pend = []
                    for h in range(8):
                        pend.append((h, scores(h)))
                        if len(pend) > 4:
                            pv(*pend.pop(0))
                    while pend:
                        pv(*pend.pop(0))
                    self.V(lambda e, osb=osb, po=po: e.tensor_copy(out=osb[:, 0:260], in_=po[0][:, 0:260]), r=[po[0]], w=[osb])
                    self.V(lambda e, osb=osb, po=po: e.tensor_copy(out=osb[:, 260:520], in_=po[1][:, 0:260]), r=[po[1]], w=[osb])
                    self.dma("sp", odst[:, n, r_, :], osb[:, :], r=[osb], w=[self.obr[bi]])
                    blk += 1

    def s5_io(self):
        i = self.inp
        self.lam_re = i("lam_re", [2, 1024])
        self.lam_im = i("lam_im", [2, 1024])
        self.log_dt = i("log_dt", [2, 16])
        self.ssm_bT = i("ssm_bT", [2, 2, 128, 2, 64])
        self.ssm_cT = i("ssm_cT", [2, 16, 128, 16])
        self.ssm_dT = i("ssm_dT", [2, 128, 2])
        self.glu_w = i("glu_w", [2, 256, 256])
        self.glu_bT = i("glu_bT", [2, 128, 2])
        self.coutT = self.scratch("coutT", [256, T], BF16)
        self.obr = [self.scratch(f"obr{i}", [T, 520], F32) for i in range(3)]

    def s5_alloc(self):
        s = self.sb
        self.Bblk = s("Bblk", [128, 2, 8, 2, 64], BF16)
        self.Cblk = s("Cblk", [128, 16, 128], BF16)
        self.Pre = s("Pre", [128, 16, 64], F32)
        self.PsT = s("PsT", [128, 16, 2, 64], F32)
        self.Qre = s("Qre", [128, 16, 64], F32)
        self.QsT = s("QsT", [128, 16, 2, 64], F32)
        self.glubh = s("glubh", [128, 2], F32)
        self.TriT = s("TriT", [128, 128], BF16)
        self.ones1 = s("ones1", [1, 128], BF16)
        self.dTt = s("dTt", [128, 2], F32)
        self.gluw = s("gluw", [128, 2, 256], BF16)
        self.glub = s("glub", [128, 2], F32)

    def s5_prep(self, l):
        s = self.sb
        V, S, G = self.V, self.S, self.G
        self.push()
        lre = s("lre", [128, 16, 64], F32)
        lim = s("lim", [128, 16, 64], F32)
        ldt = s("ldt", [128, 16], F32)
        self.dma("sp", lre[:, :, :].rearrange("p g n -> p (g n)"), self.lam_re[l].partition_broadcast(128), r=[self.lam_re], w=[lre])
        self.dma("sp", lim[:, :, :].rearrange("p g n -> p (g n)"), self.lam_im[l].partition_broadcast(128), r=[self.lam_im], w=[lim])
        self.dma("sp", ldt[:, :], self.log_dt[l].partition_broadcast(128), r=[self.log_dt], w=[ldt])
        S(lambda e: e.activation(out=ldt[:, :], in_=ldt[:, :], func=AF.Exp), r=[ldt], w=[ldt])
        dtb = ldt[:, :].unsqueeze(2).to_broadcast([128, 16, 64])
        lrd = s("lrd", [128, 16, 64], F32)
        lid = s("lid", [128, 16, 64], F32)
        V(lambda e: e.tensor_tensor(out=lrd[:, :, :], in0=lre[:, :, :], in1=dtb, op=ALU.mult), r=[lre, ldt], w=[lrd])
        V(lambda e: e.tensor_tensor(out=lid[:, :, :], in0=lim[:, :, :], in1=dtb, op=ALU.mult), r=[lim, ldt], w=[lid])
        sp1 = s("sp1", [128, 1], F32)
        G(lambda e: e.iota(sp1[:, :], pattern=[[0, 1]], base=1, channel_multiplier=1, allow_small_or_imprecise_dtypes=True), w=[sp1])
        E = s("E", [128, 16, 64], F32)
        An = s("An", [128, 16, 64], F32)
        sA = s("sA", [128, 16, 64], F32)
        cA = s("cA", [128, 16, 64], F32)
        tmp = s("s5tmp", [128, 16, 64], F32)
        tmi = s("s5tmi", [128, 16, 64], I32)
        qm = s("qm", [128, 16, 64], F32)
        pm = s("pm", [128, 16, 64], F32)
        V(lambda e: e.tensor_scalar(out=E[:, :, :], in0=lrd[:, :, :], scalar1=sp1[:, 0:1], scalar2=None, op0=ALU.mult), r=[lrd, sp1], w=[E])
        V(lambda e: e.tensor_scalar(out=An[:, :, :], in0=lid[:, :, :], scalar1=sp1[:, 0:1], scalar2=None, op0=ALU.mult), r=[lid, sp1], w=[An])
        self.sincos(An, sA, cA, tmp, tmi, None)
        S(lambda e: e.activation(out=qm[:, :, :], in_=E[:, :, :], func=AF.Exp), r=[E], w=[qm])
        S(lambda e: e.activation(out=pm[:, :, :], in_=E[:, :, :], func=AF.Exp, scale=-1.0), r=[E], w=[pm])
        V(lambda e: e.tensor_tensor(out=self.Qre[:, :, :], in0=qm[:, :, :], in1=cA[:, :, :], op=ALU.mult), r=[qm, cA], w=[self.Qre])
        V(lambda e: e.tensor_tensor(out=self.QsT[:, :, 1, :], in0=qm[:, :, :], in1=sA[:, :, :], op=ALU.mult), r=[qm, sA], w=[self.QsT])
        V(lambda e: e.tensor_scalar(out=self.QsT[:, :, 0, :], in0=self.QsT[:, :, 1, :], scalar1=-1.0, scalar2=None, op0=ALU.mult), r=[self.QsT], w=[self.QsT])
        V(lambda e: e.tensor_tensor(out=self.Pre[:, :, :], in0=pm[:, :, :], in1=cA[:, :, :], op=ALU.mult), r=[pm, cA], w=[self.Pre])
        V(lambda e: e.tensor_tensor(out=self.PsT[:, :, 0, :], in0=pm[:, :, :], in1=sA[:, :, :], op=ALU.mult), r=[pm, sA], w=[self.PsT])
        V(lambda e: e.tensor_scalar(out=self.PsT[:, :, 1, :], in0=self.PsT[:, :, 0, :], scalar1=-1.0, scalar2=None, op0=ALU.mult), r=[self.PsT], w=[self.PsT])
        self.sincos(lid, sA, cA, tmp, tmi, None)
        S(lambda e: e.activation(out=qm[:, :, :], in_=lrd[:, :, :], func=AF.Exp), r=[lrd], w=[qm])
        nr, ni = E, An
        V(lambda e: e.tensor_tensor(out=nr[:, :, :], in0=qm[:, :, :], in1=cA[:, :, :], op=ALU.mult), r=[qm, cA], w=[nr])
        V(lambda e: e.tensor_scalar(out=nr[:, :, :], in0=nr[:, :, :], scalar1=-1.0, scalar2=None, op0=ALU.add), r=[nr], w=[nr])
        V(lambda e: e.tensor_tensor(out=ni[:, :, :], in0=qm[:, :, :], in1=sA[:, :, :], op=ALU.mult), r=[qm, sA], w=[ni])
        m2 = pm
        V(lambda e: e.tensor_tensor(out=m2[:, :, :], in0=lre[:, :, :], in1=lre[:, :, :], op=ALU.mult), r=[lre], w=[m2])
        V(lambda e: e.tensor_tensor(out=tmp[:, :, :], in0=lim[:, :, :], in1=lim[:, :, :], op=ALU.mult), r=[lim], w=[tmp])
        V(lambda e: e.tensor_tensor(out=m2[:, :, :], in0=m2[:, :, :], in1=tmp[:, :, :], op=ALU.add), r=[m2, tmp], w=[m2])
        V(lambda e: e.reciprocal(out=m2[:, :, :], in_=m2[:, :, :]), r=[m2], w=[m2])
        fre, fim = sA, cA
        t1 = s("ft1", [128, 16, 64], F32)
        t2 = s("ft2", [128, 16, 64], F32)
        V(lambda e: e.tensor_tensor(out=t1[:, :, :], in0=nr[:, :, :], in1=lre[:, :, :], op=ALU.mult), r=[nr, lre], w=[t1])
        V(lambda e: e.tensor_tensor(out=t2[:, :, :], in0=ni[:, :, :], in1=lim[:, :, :], op=ALU.mult), r=[ni, lim], w=[t2])
        V(lambda e: e.tensor_tensor(out=t1[:, :, :], in0=t1[:, :, :], in1=t2[:, :, :], op=ALU.add), r=[t1, t2], w=[t1])
        V(lambda e: e.tensor_tensor(out=fre[:, :, :], in0=t1[:, :, :], in1=m2[:, :, :], op=ALU.mult), r=[t1, m2], w=[fre])
        V(lambda e: e.tensor_tensor(out=t1[:, :, :], in0=ni[:, :, :], in1=lre[:, :, :], op=ALU.mult), r=[ni, lre], w=[t1])
        V(lambda e: e.tensor_tensor(out=t2[:, :, :], in0=nr[:, :, :], in1=lim[:, :, :], op=ALU.mult), r=[nr, lim], w=[t2])
        V(lambda e: e.tensor_tensor(out=t1[:, :, :], in0=t1[:, :, :], in1=t2[:, :, :], op=ALU.subtract), r=[t1, t2], w=[t1])
        V(lambda e: e.tensor_tensor(out=fim[:, :, :], in0=t1[:, :, :], in1=m2[:, :, :], op=ALU.mult), r=[t1, m2], w=[fim])
        bT = s("bTt", [128, 2, 2, 64], F32)
        self.dma("sp", bT[:, :, :, :], self.ssm_bT[l].rearrange("r p k n -> p r k n"), r=[self.ssm_bT], w=[bT])
        bm = s("bmask", [128, 8], F32)
        one8 = s("one8", [128, 8], F32)
        G(lambda e: e.memset(one8[:, :], 1.0), w=[one8])
        G(lambda e: e.affine_select(out=bm[:, :], in_=one8[:, :], pattern=[[-16, 8]], compare_op=ALU.is_ge, fill=0.0, base=0, channel_multiplier=1),
          r=[one8], w=[bm])
        G(lambda e: e.affine_select(out=bm[:, :], in_=bm[:, :], pattern=[[16, 8]], compare_op=ALU.is_ge, fill=0.0, base=15, channel_multiplier=-1),
          r=[bm], w=[bm])
        bmb = bm[:, :].unsqueeze(2).to_broadcast([128, 8, 64])
        for kc in range(2):
            fr = fre[:, kc * 8:(kc + 1) * 8, :]
            fi = fim[:, kc * 8:(kc + 1) * 8, :]
            bre = bT[:, 0, kc, :].unsqueeze(1).to_broadcast([128, 8, 64])
            bim = bT[:, 1, kc, :].unsqueeze(1).to_broadcast([128, 8, 64])
            a1, a2 = t1[:, 0:8, :], t2[:, 0:8, :]
            V(lambda e, fr=fr, bre=bre: e.tensor_tensor(out=a1, in0=fr, in1=bre, op=ALU.mult), r=[fre, bT], w=[t1])
            V(lambda e, fi=fi, bim=bim: e.tensor_tensor(out=a2, in0=fi, in1=bim, op=ALU.mult), r=[fim, bT], w=[t2])
            V(lambda e: e.tensor_tensor(out=a1, in0=a1, in1=a2, op=ALU.subtract), r=[t1, t2], w=[t1])
            V(lambda e, kc=kc: e.tensor_tensor(out=self.Bblk[:, kc, :, 0, :], in0=a1, in1=bmb, op=ALU.mult), r=[t1, bm], w=[self.Bblk])
            V(lambda e, fr=fr, bim=bim: e.tensor_tensor(out=a1, in0=fr, in1=bim, op=ALU.mult), r=[fre, bT], w=[t1])
            V(lambda e, fi=fi, bre=bre: e.tensor_tensor(out=a2, in0=fi, in1=bre, op=ALU.mult), r=[fim, bT], w=[t2])
            V(lambda e: e.tensor_tensor(out=a1, in0=a1, in1=a2, op=ALU.add), r=[t1, t2], w=[t1])
            V(lambda e, kc=kc: e.tensor_tensor(out=self.Bblk[:, kc, :, 1, :], in0=a1, in1=bmb, op=ALU.mult), r=[t1, bm], w=[self.Bblk])
        cTs = s("cTs", [128, 16, 16], F32)
        self.dma("sp", cTs[:, :, :], self.ssm_cT[l].rearrange("g p c -> p g c"), r=[self.ssm_cT], w=[cTs])
        sg = s("sgn", [128, 1], F32)
        G(lambda e: e.memset(sg[0:64, :], 1.0), w=[sg])
        G(lambda e: e.memset(sg[64:128, :], -1.0), w=[sg])
        G(lambda e: e.memset(self.Cblk[:, :, :], 0.0), w=[self.Cblk])
        for g in range(16):
            c0 = (g % 8) * 16
            S(lambda e, g=g, c0=c0: e.activation(out=self.Cblk[:, g, c0:c0 + 16], in_=cTs[:, g, :], func=AF.Identity, scale=sg[:, 0:1]),
              r=[cTs, sg, self.Cblk], w=[self.Cblk])
        onesb = s("onesb", [128, 128], F32)
        G(lambda e: e.memset(onesb[:, :], 1.0), w=[onesb])
        G(lambda e: e.affine_select(out=onesb[:, :], in_=onesb[:, :], pattern=[[1, 128]], compare_op=ALU.is_ge, fill=0.0, base=0, channel_multiplier=-1),
          r=[onesb], w=[onesb])
        G(lambda e: e.tensor_copy(out=self.TriT[:, :], in_=onesb[:, :]), r=[onesb], w=[self.TriT])
        G(lambda e: e.memset(self.ones1[:, :], 1.0), w=[self.ones1])
        self.dma("sp", self.dTt[:, :], self.ssm_dT[l], r=[self.ssm_dT], w=[self.dTt])
        self.dma("sp", self.glub[:, :], self.glu_bT[l], r=[self.glu_bT], w=[self.glub])
        V(lambda e: e.tensor_scalar(out=self.glubh[:, :], in0=self.glub[:, :], scalar1=0.5, scalar2=None, op0=ALU.mult), r=[self.glub], w=[self.glubh])
        self.dma("pool", self.gluw[:, :, :], self.glu_w[l].rearrange("(k p) n -> p k n", p=128), r=[self.glu_w], w=[self.gluw])
        self.pop()

    def s5_chunk(self, l, j):
        B = self.bank
        V, S, G = self.V, self.S, self.G
        hT = self.hT[j % 2]
        uT = self.uT[j % 2]
        co = self.co[j % 2]
        ps_s = B[7]
        for cc in range(2):
            for k in range(8):
                self.mm(ps_s[:, cc * 128:(cc + 1) * 128], self.win[:, k, 2048 + cc * 128:2048 + (cc + 1) * 128], hT[:, k, :], r=[self.win, hT], w=[ps_s],
                        start=(k == 0), stop=(k == 7), signal=(k == 7 and cc == 1))
        S(lambda e: e.copy(out=uT[:, :, :], in_=ps_s[:, 0:256].rearrange("p (c t) -> p c t", c=2)), r=[ps_s], w=[uT])
        yps = B[7]
        for h in range(2):
            bu = [B[1], B[2]]
            zz = [B[3], B[4]]
            g0 = h * 8
            for q in range(2):
                self.mm(bu[q][:, :], uT[:, h, :], self.Bblk[:, h, q * 4:(q + 1) * 4, :, :].rearrange("p g r n -> p (g r n)"), r=[uT, self.Bblk], w=[bu[q]],
                        start=True, stop=True)
            t1, t2, vv = self.s5t1, self.s5t2, self.s5v
            for q in range(2):
                gs = slice(g0 + q * 4, g0 + q * 4 + 4)
                bu4 = bu[q][:, :].rearrange("p (g r n) -> p g r n", g=4, r=2)
                pc = self.Pre[:, gs, :].unsqueeze(2).to_broadcast([128, 4, 2, 64])
                V(lambda e, q=q, bu4=bu4, pc=pc: e.tensor_tensor(out=t1[:, q * 4:(q + 1) * 4, :, :], in0=bu4, in1=pc, op=ALU.mult), r=[bu[q], self.Pre], w=[t1], x=[t1])
                V(lambda e, q=q, bu4=bu4, gs=gs: e.tensor_tensor(out=t2[:, q * 4:(q + 1) * 4, :, :], in0=bu4[:, :, ::-1, :], in1=self.PsT[:, gs, :, :], op=ALU.mult),
                  r=[bu[q], self.PsT], w=[t2], x=[t2])
            G(lambda e: e.tensor_tensor(out=vv[:, :], in0=t1[:, :, :, :].rearrange("p g r n -> p (g r n)"), in1=t2[:, :, :, :].rearrange("p g r n -> p (g r n)"), op=ALU.add),
              r=[t1, t2], w=[vv])
            for q in range(2):
                self.mm(zz[q][:, :], self.TriT[:, :], vv[:, q * 512:(q + 1) * 512], r=[self.TriT, vv], w=[zz[q]], start=True, stop=False)
                self.mm(zz[q][:, :], self.ones1[:, :], self.x0row[h][:, q * 512:(q + 1) * 512], r=[self.ones1, self.x0row[h]], w=[zz[q]], start=False, stop=True)
            xs = self.s5x[h]
            for q in range(2):
                gs = slice(g0 + q * 4, g0 + q * 4 + 4)
                z4 = zz[q][:, :].rearrange("p (g r n) -> p g r n", g=4, r=2)
                qc = self.Qre[:, gs, :].unsqueeze(2).to_broadcast([128, 4, 2, 64])
                V(lambda e, q=q, z4=z4, qc=qc: e.tensor_tensor(out=t1[:, q * 4:(q + 1) * 4, :, :], in0=z4, in1=qc, op=ALU.mult), r=[zz[q], self.Qre], w=[t1], x=[t1])
                V(lambda e, q=q, z4=z4, gs=gs: e.tensor_tensor(out=t2[:, q * 4:(q + 1) * 4, :, :], in0=z4[:, :, ::-1, :], in1=self.QsT[:, gs, :, :], op=ALU.mult),
                  r=[zz[q], self.QsT], w=[t2], x=[t2])
            G(lambda e, xs=xs: e.tensor_tensor(out=xs[:, :, :].rearrange("p g m -> p (g m)"), in0=t1[:, :, :, :].rearrange("p g r n -> p (g r n)"),
                                        in1=t2[:, :, :, :].rearrange("p g r n -> p (g r n)"), op=ALU.add), r=[t1, t2], w=[xs])
            self.dma("act", self.x0row[h][0:1, :], xs[127:128, :, :].rearrange("p g m -> p (g m)"), r=[xs], w=[self.x0row[h]])
            pxt = B[0]
            pxb = pxt[:, :].bitcast(BF16).rearrange("p (g t) -> p g t", g=8)
            for g in range(8):
                self.tr(pxb[:, g, :], xs[:, g, :], self.identb[:, :], r=[xs, self.identb], w=[pxt], signal=(g == 7))
            V(lambda e, pxb=pxb: e.tensor_copy(out=self.s5xT[:, :, :], in_=pxb), r=[pxt], w=[self.s5xT])
            for g in range(8):
                self.mm(yps[:, 256 + h * 128:256 + (h + 1) * 128], self.Cblk[:, g0 + g, :], self.s5xT[:, g, :], r=[self.Cblk, self.s5xT], w=[yps],
                        start=(g == 0), stop=(g == 7))
        for cc in range(2):
            V(lambda e, cc=cc: e.scalar_tensor_tensor(out=self.yf[:, cc, :], in0=uT[:, cc, :], scalar=self.dTt[:, cc:cc + 1], in1=yps[:, 256 + cc * 128:256 + (cc + 1) * 128],
                                                      op0=ALU.mult, op1=ALU.add), r=[uT, self.dTt, yps], w=[self.yf], x=[self.yf])
        S(lambda e: e.activation(out=self.yg[:, :, :], in_=self.yf[:, :, :], func=AF.Gelu_apprx_tanh), r=[self.yf], w=[self.yg])
        gps = B[5]
        for c2 in range(2):
            for cc in range(2):
                self.mm(gps[:, c2 * 128:(c2 + 1) * 128], self.gluw[:, cc, c2 * 128:(c2 + 1) * 128], self.yg[:, cc, :], r=[self.gluw, self.yg], w=[gps],
                        start=(cc == 0), stop=(cc == 1))
        for c2 in range(2):
            S(lambda e, c2=c2: e.activation(out=self.sgm[:, c2, :], in_=gps[:, c2 * 128:(c2 + 1) * 128], func=AF.Sigmoid, bias=self.glub[:, c2:c2 + 1], scale=1.0),
              r=[gps, self.glub], w=[self.sgm])
        V(lambda e: e.tensor_tensor(out=co[:, :, :], in0=self.yg[:, :, :], in1=self.sgm[:, :, :], op=ALU.mult), r=[self.yg, self.sgm], w=[co])
        self.dma("sp", self.coutT[:, j * 128:(j + 1) * 128].rearrange("(c p) t -> p c t", p=128), co[:, :, :], r=[co], w=[self.coutT])

    def half(self, bank, lo):
        t = Tl(bank.h, bank.b.name + ("lo" if lo else "hi"))
        return t

    def p1v2_alloc(self):
        s = self.sb
        self.xt = [s(f"xt{i}", [128, D], F32) for i in range(2)]
        self.xn = [s(f"xn{i}", [128, D], BF16) for i in range(2)]
        self.st = [s(f"st{i}", [128, 24], F32) for i in range(2)]
        self.st2 = [s(f"stb{i}", [128, 24], F32) for i in range(2)]
        self.hT = [s(f"hT{i}", [128, 8, 128], BF16) for i in range(2)]
        self.gl = [s(f"gl{i}", [128, 512], F32) for i in range(2)]
        self.qbd = [s(f"qbd{i}", [128, 2, 512], BF16) for i in range(2)]
        self.vp = [s(f"vp{i}", [128, 8, 65], BF16) for i in range(2)]
        for i in range(2):
            self.G(lambda e, i=i: e.memset(self.vp[i][:, :, :], 1.0), w=[self.vp[i]])
        self.uT = [s(f"uT{i}", [128, 2, 128], BF16) for i in range(2)]
        self.vnb = s("vnb", [128, 256], F32)
        self.vb = s("vb", [128, 256], BF16)
        self.aout = s("aout", [128, 256], BF16)
        self.qb = s("qb", [128, 2, 512], BF16)
        self.rt = s("rt", [128, 4, 8, 8], F32)
        self.uD = [s(f"uD{i}", [128, 2, 128], F32) for i in range(3)]
        self.p1t = [s(f"p1t{i}", [128, 4, 2, 64], BF16) for i in range(2)]
        self.p2t = [s(f"p2t{i}", [128, 4, 2, 64], BF16) for i in range(2)]
        self.q1 = [s(f"q1_{i}", [128, 4, 2, 64], F32) for i in range(2)]
        self.q2 = [s(f"q2_{i}", [128, 4, 2, 64], F32) for i in range(2)]
        self.qv = [s(f"qv{i}", [128, 512], BF16) for i in range(2)]
        self.qx = [s(f"qx{i}", [128, 4, 128], BF16) for i in range(2)]
        self.qxT = [s(f"qxT{i}", [128, 4, 128], BF16) for i in range(3)]
        self.x0q = [s(f"x0q{i}", [1, 512], BF16) for i in range(4)]
        for i in range(4):
            self.G(lambda e, i=i: e.memset(self.x0q[i][:, :], 0.0), w=[self.x0q[i]])
        self.yf = s("yf2", [128, 2, 128], F32)
        self.yg = s("yg2", [128, 2, 128], BF16)
        self.sgm = s("sgm2", [128, 2, 128], F32)
        self.co = [s(f"co2_{i}", [128, 2, 128], BF16) for i in range(2)]
        B = self.bank
        self.b5lo, self.b5hi = Tl(B[5].h, "b5lo"), Tl(B[5].h, "b5hi")
        self.b6lo, self.b6hi = Tl(B[6].h, "b6lo"), Tl(B[6].h, "b6hi")
        self.b7lo, self.b7hi = Tl(B[7].h, "b7lo"), Tl(B[7].h, "b7hi")

    def p1_front_ln(self, l, j, x_src):
        if j >= NCH:
            return
        i2 = j % 2
        xt, xn, st = self.xt[i2], self.xn[i2], self.st[i2]
        S = self.S
        if j == 0:
            self.dma("sp", xt[:, :], x_src[0:128, :], r=[x_src], w=[xt])
            xt1 = self.xt[1]
            self.dma("sp", xt1[:, :], x_src[128:256, :], r=[x_src], w=[xt1])
        self.ln_stats(xt, lambda a, b: xt[:, a:b], D, st)
        S(lambda e: e.activation(out=xn[:, :], in_=xt[:, :], func=AF.Identity, bias=st[:, 4:5], scale=st[:, 3:4]), r=[xt, st], w=[xn])
        if j + 2 < NCH:
            self.dma("sp", xt[:, :], x_src[(j + 2) * 128:(j + 3) * 128, :], r=[x_src], w=[xt])

    def p1_front_a(self, l, j, x_src):
        if j >= NCH:
            return
        i2 = j % 2
        xn, hT = self.xn[i2], self.hT[i2]
        B = self.bank
        S = self.S
        pT = B[0]
        pTb = pT[:, :].bitcast(BF16).rearrange("p (k t) -> p k t", k=8)
        for k in range(8):
            self.tr(pTb[:, k, :], xn[:, k * 128:(k + 1) * 128], self.identb[:, :], r=[xn, self.identb], w=[pT], signal=(k == 7))
        for k in range(8):
            S(lambda e, k=k: e.activation(out=hT[:, k, :], in_=pTb[:, k, :], func=AF.Identity, scale=self.modp[:, 1, k:k + 1], bias=self.modp[:, 0, k:k + 1]),
              r=[pT, self.modp], w=[hT], x=[hT])

    def p1_front_s(self, l, j):
        if j >= NCH:
            return
        i2 = j % 2
        hT, uT = self.hT[i2], self.uT[i2]
        S = self.S
        ps_s = self.b6lo
        for cc in range(2):
            for k in range(8):
                self.mm(ps_s[:, cc * 128:(cc + 1) * 128], self.win[:, k, 2048 + cc * 128:2048 + (cc + 1) * 128], hT[:, k, :], r=[self.win, hT], w=[ps_s],
                        start=(k == 0), stop=(k == 7), signal=(k == 7 and cc == 1))
        S(lambda e: e.copy(out=uT[:, :, :], in_=ps_s[:, 0:256].rearrange("p (c t) -> p c t", c=2)), r=[ps_s], w=[uT])
        uD = self.uD[j % 3]
        for cc in range(2):
            S(lambda e, cc=cc: e.activation(out=uD[:, cc, :], in_=ps_s[:, cc * 128:(cc + 1) * 128], func=AF.Identity, scale=self.dTt[:, cc:cc + 1]), r=[ps_s, self.dTt], w=[uD], x=[uD])

    def p1_front_p(self, l, j, which):
        if j >= NCH:
            return
        i2 = j % 2
        hT, gl, qf, vp = self.hT[i2], self.gl[i2], self.qbd[i2], self.vp[i2]
        B = self.bank
        S = self.S

        def proj(bank, c0):
            for k in range(8):
                self.mm(bank[:, :], hT[:, k, :], self.win[:, k, c0:c0 + 512], r=[hT, self.win], w=[bank], start=(k == 0), stop=(k == 7))
        if which == 0:
            proj(B[1], 0)
            S(lambda e: e.activation(out=gl[:, :], in_=B[1][:, :], func=AF.Gelu_apprx_tanh), r=[B[1]], w=[gl])
        elif which == 1:
            proj(B[2], 512)
            S(lambda e: e.copy(out=qf[:, 0, :], in_=B[2][:, :]), r=[B[2]], w=[qf])
        elif which == 2:
            proj(B[1], 1024)
            S(lambda e: e.copy(out=qf[:, 1, :], in_=B[1][:, :]), r=[B[1]], w=[qf], x=[qf])
        else:
            proj(B[2], 1536)
            S(lambda e: e.copy(out=vp[:, :, 0:64], in_=B[2][:, :].rearrange("p (h e) -> p h e", e=64)), r=[B[2]], w=[vp])

    def p1_front(self, l, j, x_src):
        self.p1_front_a(l, j, x_src)
        self.p1_front_s(l, j)
        for w_ in range(4):
            self.p1_front_p(l, j, w_)

    def p1_back(self, l, j):
        if j < 0:
            return
        i2 = j % 2
        st2 = self.st2[i2]
        gl, qf, vp, uT = self.gl[i2], self.qbd[i2], self.vp[i2], self.uT[i2]
        vnb, vb, aout, qb, rt = self.vnb, self.vb, self.aout, self.qbd[i2], self.rt
        B = self.bank
        V, S, G = self.V, self.S, self.G
        rows = slice(j * 128, (j + 1) * 128)
        self.ln_stats(gl, lambda a, b: gl[:, 256 + a:256 + b], 256, st2)
        V(lambda e: e.tensor_scalar(out=vnb[:, :], in0=gl[:, 256:512], scalar1=st2[:, 3:4], scalar2=st2[:, 4:5], op0=ALU.mult, op1=ALU.add),
          r=[gl, st2], w=[vnb])
        V(lambda e: e.tensor_tensor(out=vnb[:, :], in0=vnb[:, :], in1=self.glng[:, :], op=ALU.mult), r=[vnb, self.glng], w=[vnb], x=[vnb])
        V(lambda e: e.tensor_tensor(out=vb[:, :], in0=vnb[:, :], in1=self.glnb[:, :], op=ALU.add), r=[vnb, self.glnb], w=[vb], x=[vnb])
        psv = self.b5lo
        for h in range(4):
            self.mm(psv[:, h * 64:(h + 1) * 64], self.wsT[:, h, :], vb[:, h * 64:(h + 1) * 64], r=[self.wsT, vb], w=[psv], start=True, stop=True,
                    signal=(h == 3))
        for h in range(4):
            V(lambda e, h=h: e.scalar_tensor_tensor(out=aout[:, h * 64:(h + 1) * 64], in0=psv[:, h * 64:(h + 1) * 64], scalar=self.gbs[:, h:h + 1],
                                                    in1=gl[:, h * 64:(h + 1) * 64], op0=ALU.add, op1=ALU.mult), r=[psv, self.gbs, gl], w=[aout], x=[aout])
        self.dma("sp", self.mixtok[rows, 0:256], aout[:, :], r=[aout], w=[self.mixtok])
        q4 = qf[:, :, :].rearrange("p a (h e) -> p (a h) e", e=64)
        o4 = qb[:, :, :].rearrange("p a (h e) -> p (a h) e", e=64)
        for a in range(2):
            xa1, xa2 = q4[:, a * 8:(a + 1) * 8, 0:8], q4[:, a * 8:(a + 1) * 8, 8:16]
            cb = self.cs[:, j, :].unsqueeze(1).to_broadcast([128, 8, 8])
            sb_ = self.sn[:, j, :].unsqueeze(1).to_broadcast([128, 8, 8])
            oa = o4[:, a * 8:(a + 1) * 8, :]
            G(lambda e, xa1=xa1, cb=cb: e.tensor_tensor(out=rt[:, 0, :, :], in0=xa1, in1=cb, op=ALU.mult), r=[qf, self.cs], w=[rt])
            G(lambda e, xa2=xa2, sb_=sb_: e.tensor_tensor(out=rt[:, 1, :, :], in0=xa2, in1=sb_, op=ALU.mult), r=[qf, self.sn], w=[rt])
            G(lambda e, xa2=xa2, cb=cb: e.tensor_tensor(out=rt[:, 2, :, :], in0=xa2, in1=cb, op=ALU.mult), r=[qf, self.cs], w=[rt])
            G(lambda e, xa1=xa1, sb_=sb_: e.tensor_tensor(out=rt[:, 3, :, :], in0=xa1, in1=sb_, op=ALU.mult), r=[qf, self.sn], w=[rt])
            G(lambda e, oa=oa: e.tensor_tensor(out=oa[:, :, 0:8], in0=rt[:, 0, :, :], in1=rt[:, 1, :, :], op=ALU.subtract), r=[rt], w=[qb])
            G(lambda e, oa=oa: e.tensor_tensor(out=oa[:, :, 8:16], in0=rt[:, 2, :, :], in1=rt[:, 3, :, :], op=ALU.add), r=[rt], w=[qb])
        pq = B[7]
        pqb = pq[:, :].bitcast(BF16).rearrange("p (a k t) -> p a k t", a=2, k=4)
        for a in range(2):
            for k in range(4):
                self.tr(pqb[:, a, k, :], qb[:, a, k * 128:(k + 1) * 128], self.identb[:, :], r=[qb, self.identb], w=[pq], signal=(a == 1 and k == 3))
        S(lambda e: e.copy(out=self.QT[:, :, j * 128:(j + 1) * 128], in_=pqb[:, 0, :, :]), r=[pq], w=[self.QT])
        S(lambda e: e.copy(out=self.KT[:, :, j * 128:(j + 1) * 128], in_=pqb[:, 1, :, :]), r=[pq], w=[self.KT])
        self.dma("sp", self.vd[rows, :], vp[:, :, :].rearrange("p h e -> p (h e)"), r=[vp], w=[self.vd])

    def s5A(self, i):
        if i < 0 or i >= 4 * NCH:
            return
        j, q = divmod(i, 4)
        kc = q // 2
        gs = slice(q * 4, q * 4 + 4)
        uT = self.uT[j % 2]
        bu = self.bank[3]
        t1, t2 = self.p1t[i % 2], self.p2t[i % 2]
        self.mm(bu[:, :], uT[:, kc, :], self.Bblk[:, kc, (q % 2) * 4:(q % 2) * 4 + 4, :, :].rearrange("p g r n -> p (g r n)"), r=[uT, self.Bblk], w=[bu],
                start=True, stop=True)
        bu4 = bu[:, :].rearrange("p (g r n) -> p g r n", g=4, r=2)
        pc = self.Pre[:, gs, :].unsqueeze(2).to_broadcast([128, 4, 2, 64])
        self.V(lambda e: e.tensor_tensor(out=t1[:, :, :, :], in0=bu4, in1=pc, op=ALU.mult), r=[bu, self.Pre], w=[t1])
        self.V(lambda e: e.tensor_tensor(out=t2[:, :, :, :], in0=bu4[:, :, ::-1, :], in1=self.PsT[:, gs, :, :], op=ALU.mult), r=[bu, self.PsT], w=[t2])

    def s5B(self, i):
        if i < 0 or i >= 4 * NCH:
            return
        j, q = divmod(i, 4)
        gs = slice(q * 4, q * 4 + 4)
        zz = self.bank[4]
        t1, t2 = self.p1t[i % 2], self.p2t[i % 2]
        u1, u2, xs = self.q1[i % 2], self.q2[i % 2], self.qx[i % 2]
        f = lambda t: t[:, :, :, :].rearrange("p g r n -> p (g r n)")
        self.mm(zz[:, :], self.TriT[:, :], f(t1), r=[self.TriT, t1], w=[zz], start=True, stop=False)
        self.mm(zz[:, :], self.TriT[:, :], f(t2), r=[self.TriT, t2], w=[zz], start=False, stop=False)
        self.mm(zz[:, :], self.ones1[:, :], self.x0q[q][:, :], r=[self.ones1, self.x0q[q]], w=[zz], start=False, stop=True)
        z4 = zz[:, :].rearrange("p (g r n) -> p g r n", g=4, r=2)
        qc = self.Qre[:, gs, :].unsqueeze(2).to_broadcast([128, 4, 2, 64])
        self.V(lambda e: e.tensor_tensor(out=u1[:, :, :, :], in0=z4, in1=qc, op=ALU.mult), r=[zz, self.Qre], w=[u1])
        self.V(lambda e: e.tensor_tensor(out=u2[:, :, :, :], in0=z4[:, :, ::-1, :], in1=self.QsT[:, gs, :, :], op=ALU.mult), r=[zz, self.QsT], w=[u2])
        self.G(lambda e: e.tensor_tensor(out=xs[:, :, :].rearrange("p g m -> p (g m)"), in0=f(u1), in1=f(u2), op=ALU.add), r=[u1, u2], w=[xs])
        self.dma("pool", self.x0q[q][0:1, :], xs[127:128, :, :].rearrange("p g m -> p (g m)"), r=[xs], w=[self.x0q[q]])

    def s5C(self, i):
        if i < 0 or i >= 4 * NCH:
            return
        xs, xT = self.qx[i % 2], self.qxT[i % 3]
        pxt = self.b6hi
        pxb = pxt[:, 256:512].bitcast(BF16).rearrange("p (g t) -> p g t", g=4)
        for g in range(4):
            self.tr(pxb[:, g, :], xs[:, g, :], self.identb[:, :], r=[xs, self.identb], w=[pxt], signal=(g == 3))
        self.S(lambda e: e.copy(out=xT[:, :, :], in_=pxb), r=[pxt], w=[xT])

    def s5D(self, i):
        if i < 0 or i >= 4 * NCH:
            return
        j, q = divmod(i, 4)
        kc = q // 2
        xT = self.qxT[i % 3]
        yps = self.b5hi
        for g in range(4):
            self.mm(yps[:, 256 + kc * 128:256 + (kc + 1) * 128], self.Cblk[:, q * 4 + g, :], xT[:, g, :], r=[self.Cblk, xT], w=[yps],
                    start=(q % 2 == 0 and g == 0), stop=(q % 2 == 1 and g == 3))

    def s5_step(self, i):
        self.s5A(i + 2)
        self.s5B(i + 1)
        self.s5C(i)
        if i % 2 == 0:
            self.s5D(i - 2)
            self.s5D(i - 1)

    def s5_tail(self, j):
        if j < 0 or j >= NCH:
            return
        V, S, G = self.V, self.S, self.G
        i2 = j % 2
        uT = self.uT[i2]
        yps = self.b5hi
        co = self.co[i2]
        for cc in range(2):
            V(lambda e, cc=cc: e.scalar_tensor_tensor(out=self.yf[:, cc, :], in0=self.uD[j % 3][:, cc, :], scalar=1.0, in1=yps[:, 256 + cc * 128:256 + (cc + 1) * 128],
                                                      op0=ALU.mult, op1=ALU.add), r=[self.uD[j % 3], yps], w=[self.yf], x=[self.yf])
        S(lambda e: e.activation(out=self.yg[:, :, :], in_=self.yf[:, :, :], func=AF.Gelu_apprx_tanh), r=[self.yf], w=[self.yg])
        gps = self.b5lo
        for c2 in range(2):
            for cc in range(2):
                self.mm(gps[:, c2 * 128:(c2 + 1) * 128], self.gluw[:, cc, c2 * 128:(c2 + 1) * 128], self.yg[:, cc, :], r=[self.gluw, self.yg], w=[gps],
                        start=(cc == 0), stop=(cc == 1))
        for c2 in range(2):
            S(lambda e, c2=c2: e.activation(out=self.sgm[:, c2, :], in_=gps[:, c2 * 128:(c2 + 1) * 128], func=AF.Tanh, bias=self.glubh[:, c2:c2 + 1], scale=0.5),
              r=[gps, self.glubh], w=[self.sgm])
        V(lambda e: e.scalar_tensor_tensor(out=self.sgm[:, :, :], in0=self.sgm[:, :, :], scalar=1.0, in1=self.yg[:, :, :], op0=ALU.add, op1=ALU.mult),
          r=[self.sgm, self.yg], w=[self.sgm])
        V(lambda e: e.tensor_scalar(out=co[:, :, :], in0=self.sgm[:, :, :], scalar1=0.5, scalar2=None, op0=ALU.mult), r=[self.sgm], w=[co])
        self.dma("sp", self.coutT[:, j * 128:(j + 1) * 128].rearrange("(c p) t -> p c t", p=128), co[:, :, :], r=[co], w=[self.coutT])

    def p1_all(self, l, x_src):
        self.p1_front_ln(l, 0, x_src)
        self.p1_front_ln(l, 1, x_src)
        self.p1_front(l, 0, x_src)
        self.s5A(0); self.s5A(1); self.s5B(0)
        for j in range(NCH):
            self.p1_front_ln(l, j + 2, x_src)
            self.p1_front(l, j + 1, x_src)
            self.p1_back(l, j)
            for q in range(4):
                self.s5_step(4 * j + q)
                if q == 0:
                    self.s5_tail(j - 1)
        self.s5_step(4 * NCH)
        self.s5_tail(NCH - 1)
        assert (4 * NCH) % 2 == 0

    CAP = 896
    NSLOT = 16 * 896 + 128
    GROUPS = ((0, 4), (512, 3))

    def p3_io(self):
        i = self.inp
        self.w_out = i("w_out", [2, D, D])
        self.ln1_g = i("ln1_g", [2, D]); self.ln1_b = i("ln1_b", [2, D])
        self.ln2_g = i("ln2_g", [2, D]); self.ln2_b = i("ln2_b", [2, D])
        self.router_w = i("router_w", [D, 16])
        self.router_bias = i("router_bias", [16])
        self.x1d = self.scratch("x1d", [T, D], F32)
        self.xmid = self.scratch("xmid", [T, D], F32)
        self.h2slots = self.scratch("h2slots", [self.NSLOT, D], BF16)
        self.oslots = self.scratch("oslots", [self.NSLOT, D], F32)

    def route_alloc(self):
        s = self.sb
        self.slotA = s("slotA", [128, NCH], I32)
        self.slotB = s("slotB", [128, NCH], I32)
        self.gAB = s("gAB", [128, 2, NCH], F32)

    def p3_alloc(self, l):
        s = self.sb
        self.wout = s("wout", [128, 8, D], BF16)
        for k in range(8):
            self.dma("pool", self.wout[:, k, :], self.w_out[l, k * 128:(k + 1) * 128, :], r=[self.w_out], w=[self.wout])
        self.lng = s("lng", [128, D], F32); self.lnb = s("lnb", [128, D], F32)
        self.dma("sp", self.lng[:, :], self.ln1_g[l].partition_broadcast(128), r=[self.ln1_g], w=[self.lng])
        self.dma("sp", self.lnb[:, :], self.ln1_b[l].partition_broadcast(128), r=[self.ln1_b], w=[self.lnb])
        self.rw = s("rw", [128, 8, 16], F32)
        self.dma("sp", self.rw[:, :, :], self.router_w[:, :].rearrange("(k p) e -> p k e", p=128), r=[self.router_w], w=[self.rw])
        self.rbias = s("rbias", [128, 16], F32)
        self.dma("sp", self.rbias[:, :], self.router_bias[:].partition_broadcast(128), r=[self.router_bias], w=[self.rbias])
        self.o3 = [s(f"o3_{i}", [128, 3, 520], F32) for i in range(2)]
        self.rec = s("rec", [128, 8], F32)
        self.mt = [s(f"mt{i}", [128, D], BF16) for i in range(2)]
        self.mixT = [s(f"mixT{i}", [128, 8, 128], BF16) for i in range(2)]
        self.xres = [s(f"xres{i}", [128, D], F32) for i in range(2)]
        self.yy = s("yy", [128, D], F32)
        self.x1t = [s(f"x1t{i}", [128, D], F32) for i in range(2)]
        self.cT = [s(f"cT{i}", [128, 2, 128], BF16) for i in range(2)]
        self.st3b = s("st3b", [128, 24], F32)
        self.h2f = s("h2f", [128, D], F32)
        self.h2all = s("h2all", [128, NCH, D], BF16)
        self.h2c = [Tl(self.h2all.h, f"h2c{j}") for j in range(NCH)]
        self.scall = s("scall", [128, NCH, 16], F32)
        self.h2T = s("h2T", [128, 8, 128], F32)
        self.st3 = s("st3", [128, 24], F32)
        self.trs = s("trs", [128, 128], F32)
        self.eoff = s("eoff", [128, 16], F32)
        self.trashp = s("trashp", [128, 1], F32)
        self.ones16 = s("ones16", [128, 16], F32)
        G = self.G
        G(lambda e: e.memset(self.ones16[:, :], 1.0), w=[self.ones16])
        G(lambda e: e.affine_select(out=self.trs[:, :], in_=self.onesf[:, :], pattern=[[1, 128]], compare_op=ALU.is_ge, fill=0.0, base=-1, channel_multiplier=-1),
          r=[self.onesf], w=[self.trs])
        G(lambda e: e.iota(self.eoff[:, :], pattern=[[self.CAP, 16]], base=0, channel_multiplier=0, allow_small_or_imprecise_dtypes=True), w=[self.eoff])
        G(lambda e: e.iota(self.trashp[:, :], pattern=[[0, 1]], base=16 * self.CAP, channel_multiplier=1, allow_small_or_imprecise_dtypes=True), w=[self.trashp])

    def p3_L1(self, k):
        if k >= NCH:
            return
        rws = slice(k * 128, (k + 1) * 128)
        o3_, mt_ = self.o3[k % 2], self.mt[k % 2]
        for bi in range(3):
            self.dma("sp", o3_[:, bi, :], self.obr[bi][rws, :], r=[self.obr[bi]], w=[o3_])
        self.dma("sp", mt_[:, 0:256], self.mixtok[rws, 0:256], r=[self.mixtok], w=[mt_])

    def p3_L2(self, k, x_src):
        if k >= NCH:
            return
        rws = slice(k * 128, (k + 1) * 128)
        self.dma("sp", self.cT[k % 2][:, :, :], self.coutT[:, rws].rearrange("(c p) t -> p c t", p=128), r=[self.coutT], w=[self.cT[k % 2]])
        self.dma("sp", self.xres[k % 2][:, :], x_src[rws, :], r=[x_src], w=[self.xres[k % 2]])

    def p3_S1(self, j):
        if j >= NCH or j < 0:
            return
        B = self.bank
        V, S, G = self.V, self.S, self.G
        o3, rec, mt = self.o3[j % 2], self.rec, self.mt[j % 2]
        mixT = self.mixT[j % 2]
        V(lambda e: e.tensor_tensor(out=o3[:, 0, :], in0=o3[:, 0, :], in1=o3[:, 1, :], op=ALU.add), r=[o3], w=[o3], x=[o3])
        V(lambda e: e.tensor_tensor(out=o3[:, 0, :], in0=o3[:, 0, :], in1=o3[:, 2, :], op=ALU.add), r=[o3], w=[o3], x=[o3])
        o8 = o3[:, 0, :].rearrange("p (h e) -> p h e", e=65)
        V(lambda e: e.reciprocal(out=rec[:, :], in_=o8[:, :, 64]), r=[o3], w=[rec])
        V(lambda e: e.tensor_tensor(out=mt[:, 256:768].rearrange("p (h e) -> p h e", e=64), in0=o8[:, :, 0:64], in1=rec[:, :].unsqueeze(2).to_broadcast([128, 8, 64]),
                                    op=ALU.mult), r=[o3, rec], w=[mt])
        pT = B[0]
        pTb = pT[:, :].bitcast(BF16).rearrange("p (k t) -> p k t", k=8)
        for k in range(6):
            self.tr(pTb[:, k, :], mt[:, k * 128:(k + 1) * 128], self.identb[:, :], r=[mt, self.identb], w=[pT], signal=(k == 5))
        S(lambda e: e.copy(out=mixT[:, 0:6, :], in_=pTb[:, 0:6, :]), r=[pT], w=[mixT])

    def p3_S2(self, j):
        if j >= NCH or j < 0:
            return
        B = self.bank
        V, S, G = self.V, self.S, self.G
        rows = slice(j * 128, (j + 1) * 128)
        mixT, cT, xres = self.mixT[j % 2], self.cT[j % 2], self.xres[j % 2]
        wb = [B[1], B[2]] if j % 2 == 0 else [B[6], B[7]]
        for nb in range(2):
            for k in range(8):
                lhs = mixT[:, k, :] if k < 6 else cT[:, k - 6, :]
                self.mm(wb[nb][:, :], lhs, self.wout[:, k, nb * 512:(nb + 1) * 512], r=[mixT, cT, self.wout], w=[wb[nb]], start=(k == 0), stop=(k == 7))
        yy = self.yy
        for nb in range(2):
            cs_ = slice(nb * 512, (nb + 1) * 512)
            V(lambda e, nb=nb, cs_=cs_: e.tensor_tensor(out=yy[:, cs_], in0=wb[nb][:, :], in1=self.opg[:, 0, cs_], op=ALU.mult), r=[wb[nb], self.opg], w=[yy], x=[yy])
        V(lambda e: e.scalar_tensor_tensor(out=yy[:, :], in0=xres[:, :], scalar=float(ALPHA), in1=yy[:, :], op0=ALU.mult, op1=ALU.add), r=[xres, yy], w=[yy], x=[yy])
        self.ln_stats(yy, lambda a, b: yy[:, a:b], D, self.st3, act=True)
        x1 = self.x1t[j % 2]
        S(lambda e: e.activation(out=x1[:, :], in_=yy[:, :], func=AF.Identity, bias=self.st3[:, 4:5], scale=self.st3[:, 3:4]), r=[yy, self.st3], w=[x1])
        V(lambda e: e.tensor_tensor(out=x1[:, :], in0=x1[:, :], in1=self.lng[:, :], op=ALU.mult), r=[x1, self.lng], w=[x1])
        V(lambda e: e.tensor_tensor(out=x1[:, :], in0=x1[:, :], in1=self.lnb[:, :], op=ALU.add), r=[x1, self.lnb], w=[x1], x=[x1])
        self.dma("sp", self.x1d[rows, :], x1[:, :], r=[x1], w=[self.x1d])

    def p3_S3(self, j):
        if j >= NCH or j < 0:
            return
        B = self.bank
        V, S, G = self.V, self.S, self.G
        x1 = self.x1t[j % 2]
        self.ln_stats(x1, lambda a, b: x1[:, a:b], D, self.st3b, act=True)
        h2f = self.h2f
        S(lambda e: e.activation(out=h2f[:, :], in_=x1[:, :], func=AF.Identity, bias=self.st3b[:, 4:5], scale=self.st3b[:, 3:4]), r=[x1, self.st3b], w=[h2f])
        V(lambda e: e.tensor_tensor(out=h2f[:, :], in0=h2f[:, :], in1=self.opg2[:, 1, :], op=ALU.mult), r=[h2f, self.opg2], w=[h2f], x=[h2f])
        V(lambda e: e.tensor_tensor(out=h2f[:, :], in0=h2f[:, :], in1=self.opg2[:, 0, :], op=ALU.add), r=[h2f, self.opg2], w=[h2f], x=[h2f])
        S(lambda e: e.copy(out=self.h2all[:, j, :], in_=h2f[:, :]), r=[h2f], w=[self.h2c[j]])
        for half in range(2):
            pt = B[3 + half]
            for k in range(4):
                self.tr(pt[:, k * 128:(k + 1) * 128], h2f[:, (half * 4 + k) * 128:(half * 4 + k + 1) * 128], self.identf[:, :], r=[h2f, self.identf], w=[pt], signal=(k == 3))
            S(lambda e, half=half, pt=pt: e.copy(out=self.h2T[:, half * 4:(half + 1) * 4, :], in_=pt[:, :].rearrange("p (k t) -> p k t", k=4)), r=[pt], w=[self.h2T])
        lg = B[5]
        for k in range(8):
            self.mm(lg[:, 0:16], self.h2T[:, k, :], self.rw[:, k, :], r=[self.h2T, self.rw], w=[lg], start=(k == 0), stop=(k == 7))
        S(lambda e: e.copy(out=self.scall[:, j, :], in_=lg[:, 0:16]), r=[lg], w=[self.scall])

    def p3_all(self, l, x_src):
        self.p3_L1(0)
        for j in range(-2, NCH):
            self.p3_L1(j + 3)
            self.p3_L2(j + 2, x_src)
            self.p3_S1(j + 2)
            self.p3_S2(j + 1)
            self.p3_S3(j)
            if j >= 0 and (j + 1) % self.RSEG == 0:
                self.p3_route(j + 1 - self.RSEG)

    RSEG = 8

    def p3_route_alloc(self):
        s = self.sb
        NJ = self.RSEG
        mk = lambda n: s(n, [128, NJ, 16], F32)
        t = {}
        t["big"] = [mk(f"r_{i}") for i in range(14)]
        t["g4"] = [s(f"r4_{i}", [128, NJ, 4], F32) for i in range(4)]
        t["g1"] = [s(f"r1_{i}", [128, NJ], F32) for i in range(3)]
        t["mask_e0"] = mk("mask_e0")
        t["mask_j0"] = s("mask_j0", [128, 16, NJ], F32)
        G = self.G
        G(lambda e: e.memset(t["mask_e0"][:, :, :], 1.0), w=[t["mask_e0"]])
        G(lambda e: e.memset(t["mask_e0"][:, :, 0:1], 0.0), w=[t["mask_e0"]])
        G(lambda e: e.memset(t["mask_j0"][:, :, :], 1.0), w=[t["mask_j0"]])
        G(lambda e: e.memset(t["mask_j0"][:, :, 0:1], 0.0), w=[t["mask_j0"]])
        self.carry = s("carry", [128, 16], F32)
        G(lambda e: e.memset(self.carry[:, :], 0.0), w=[self.carry])
        self._rt = t

    def p3_route(self, j0):
        B = self.bank
        V, S, G = self.V, self.S, self.G
        NJ = self.RSEG
        t = self._rt
        sel, eq, msk, top2, chosen, gw, tmp, cum, pos, valid, slotv, baseT, totT, sc = t["big"]
        m1, m2, gs, gsel = t["g4"]
        gmax, gsum, t32 = t["g1"]
        mask_e0, mask_j0 = t["mask_e0"], t["mask_j0"]
        f2 = lambda t_: t_[:, :, :].rearrange("p j e -> p (j e)")
        S(lambda e: e.activation(out=f2(sc), in_=self.scall[:, j0:j0 + NJ, :].rearrange("p j e -> p (j e)"), func=AF.Sigmoid), r=[self.scall], w=[sc])
        g4 = lambda t: t[:, :, :].rearrange("p j (g i) -> p (j g) i", i=4)
        b4 = lambda t: t[:, :, :].rearrange("p j g -> p (j g)").unsqueeze(2).to_broadcast([128, NJ * 4, 4])
        V(lambda e: e.tensor_tensor(out=sel[:, :, :], in0=sc[:, :, :], in1=self.rbias[:, :].unsqueeze(1).to_broadcast([128, NJ, 16]), op=ALU.add), r=[sc, self.rbias], w=[sel])
        V(lambda e: e.tensor_reduce(out=m1[:, :, :].rearrange("p j g -> p (j g)"), in_=g4(sel), axis=AX.X, op=ALU.max), r=[sel], w=[m1])
        V(lambda e: e.tensor_tensor(out=g4(eq), in0=g4(sel), in1=b4(m1), op=ALU.is_equal), r=[sel, m1], w=[eq])
        V(lambda e: e.scalar_tensor_tensor(out=f2(msk), in0=f2(eq), scalar=-1e9, in1=f2(sel), op0=ALU.mult, op1=ALU.add), r=[eq, sel], w=[msk])
        V(lambda e: e.tensor_reduce(out=m2[:, :, :].rearrange("p j g -> p (j g)"), in_=g4(msk), axis=AX.X, op=ALU.max), r=[msk], w=[m2])
        V(lambda e: e.tensor_tensor(out=gs[:, :, :], in0=m1[:, :, :], in1=m2[:, :, :], op=ALU.add), r=[m1, m2], w=[gs])
        V(lambda e: e.tensor_reduce(out=gmax[:, :], in_=gs[:, :, :], axis=AX.X, op=ALU.max), r=[gs], w=[gmax])
        V(lambda e: e.tensor_tensor(out=gsel[:, :, :], in0=gs[:, :, :], in1=gmax[:, :].unsqueeze(2).to_broadcast([128, NJ, 4]), op=ALU.is_equal), r=[gs, gmax], w=[gsel])
        V(lambda e: e.tensor_tensor(out=g4(top2), in0=g4(sel), in1=b4(m2), op=ALU.is_ge), r=[sel, m2], w=[top2])
        V(lambda e: e.tensor_tensor(out=g4(chosen), in0=g4(top2), in1=b4(gsel), op=ALU.mult), r=[top2, gsel], w=[chosen])
        V(lambda e: e.tensor_tensor(out=gw[:, :, :], in0=chosen[:, :, :], in1=sc[:, :, :], op=ALU.mult), r=[chosen, sc], w=[gw])
        V(lambda e: e.tensor_reduce(out=gsum[:, :], in_=gw[:, :, :], axis=AX.X, op=ALU.add), r=[gw], w=[gsum])
        V(lambda e: e.reciprocal(out=gsum[:, :], in_=gsum[:, :]), r=[gsum], w=[gsum])
        V(lambda e: e.tensor_tensor(out=gw[:, :, :], in0=gw[:, :, :], in1=gsum[:, :].unsqueeze(2).to_broadcast([128, NJ, 16]), op=ALU.mult), r=[gw, gsum], w=[gw])
        cbk = B[5]
        W = NJ * 16
        self.mm(cbk[:, 128:128 + W], self.trs[:, :], f2(chosen), r=[self.trs, chosen], w=[cbk], start=True, stop=True)
        self.mm(cbk[:, 256:256 + W], self.onesf[:, :], f2(chosen), r=[self.onesf, chosen], w=[cbk], start=True, stop=True)
        V(lambda e: e.tensor_copy(out=f2(totT).rearrange("p (e j) -> p e j", e=16),
                                  in_=cbk[:, 256:256 + W].rearrange("p (j e) -> p e j", e=16)), r=[cbk], w=[totT])
        V(lambda e: e.tensor_tensor_scan(out=f2(baseT), data0=mask_j0[:, :, :].rearrange("p e j -> p (e j)"), data1=f2(totT), initial=0.0, op0=ALU.mult, op1=ALU.add),
          r=[mask_j0, totT], w=[baseT])
        V(lambda e: e.tensor_tensor(out=f2(baseT), in0=f2(baseT), in1=f2(totT), op=ALU.subtract), r=[baseT, totT], w=[baseT])
        bT3 = f2(baseT).rearrange("p (e j) -> p e j", e=16)
        tT3 = f2(totT).rearrange("p (e j) -> p e j", e=16)
        V(lambda e: e.tensor_tensor(out=bT3, in0=bT3, in1=self.carry[:, :].unsqueeze(2).to_broadcast([128, 16, NJ]), op=ALU.add), r=[baseT, self.carry], w=[baseT])
        V(lambda e: e.tensor_tensor(out=self.carry[:, :], in0=bT3[:, :, NJ - 1], in1=tT3[:, :, NJ - 1], op=ALU.add), r=[baseT, totT], w=[self.carry])
        V(lambda e: e.tensor_tensor(out=pos[:, :, :], in0=cbk[:, 128:128 + W].rearrange("p (j e) -> p j e", e=16),
                                    in1=f2(baseT).rearrange("p (e j) -> p j e", e=16), op=ALU.add), r=[cbk, baseT], w=[pos])
        V(lambda e: e.tensor_scalar(out=f2(valid), in0=f2(pos), scalar1=float(self.CAP), scalar2=None, op0=ALU.is_lt), r=[pos], w=[valid])
        V(lambda e: e.tensor_tensor(out=slotv[:, :, :], in0=pos[:, :, :], in1=self.eoff[:, :].unsqueeze(1).to_broadcast([128, NJ, 16]), op=ALU.add), r=[pos, self.eoff], w=[slotv])
        V(lambda e: e.tensor_scalar(out=f2(slotv), in0=f2(slotv), scalar1=self.trashp[:, 0:1], scalar2=None, op0=ALU.subtract), r=[slotv, self.trashp], w=[slotv])
        V(lambda e: e.tensor_tensor(out=f2(slotv), in0=f2(slotv), in1=f2(valid), op=ALU.mult), r=[slotv, valid], w=[slotv])
        V(lambda e: e.tensor_scalar(out=f2(slotv), in0=f2(slotv), scalar1=self.trashp[:, 0:1], scalar2=None, op0=ALU.add), r=[slotv, self.trashp], w=[slotv])
        V(lambda e: e.tensor_tensor(out=f2(gw), in0=f2(gw), in1=f2(valid), op=ALU.mult), r=[gw, valid], w=[gw])
        V(lambda e: e.tensor_tensor_scan(out=f2(cum), data0=f2(mask_e0), data1=f2(chosen), initial=0.0, op0=ALU.mult, op1=ALU.add), r=[mask_e0, chosen], w=[cum])
        for which, dsti in ((1.0, self.slotA), (2.0, self.slotB)):
            wi = int(which) - 1
            V(lambda e, which=which: e.tensor_scalar(out=f2(tmp), in0=f2(cum), scalar1=float(which), scalar2=None, op0=ALU.is_equal), r=[cum], w=[tmp])
            V(lambda e: e.tensor_tensor(out=f2(tmp), in0=f2(tmp), in1=f2(chosen), op=ALU.mult), r=[tmp, chosen], w=[tmp])
            V(lambda e: e.tensor_tensor(out=f2(eq), in0=f2(tmp), in1=f2(gw), op=ALU.mult), r=[tmp, gw], w=[eq])
            V(lambda e, wi=wi: e.tensor_reduce(out=self.gAB[:, wi, j0:j0 + NJ], in_=eq[:, :, :], axis=AX.X, op=ALU.add), r=[eq], w=[self.gAB])
            V(lambda e: e.tensor_tensor(out=f2(tmp), in0=f2(tmp), in1=f2(slotv), op=ALU.mult), r=[tmp, slotv], w=[tmp])
            V(lambda e: e.tensor_reduce(out=t32[:, :], in_=tmp[:, :, :], axis=AX.X, op=ALU.add), r=[tmp], w=[t32])
            V(lambda e: e.tensor_scalar(out=t32[:, :], in0=t32[:, :], scalar1=0.0, scalar2=float(self.NSLOT - 1), op0=ALU.max, op1=ALU.min), r=[t32], w=[t32])
            V(lambda e, dsti=dsti: e.tensor_copy(out=dsti[:, j0:j0 + NJ], in_=t32[:, :]), r=[t32], w=[dsti])
        for j in range(j0, j0 + NJ):
            for dsti in (self.slotA, self.slotB):
                self.cx.dma("pool", None, None, reads=[self.h2c[j].b, dsti.b], writes=[],
                            fn=lambda e, dsti=dsti, j=j: e.indirect_dma_start(out=self.h2slots[:, :], out_offset=bass.IndirectOffsetOnAxis(ap=dsti[:, j:j + 1], axis=0),
                                                                              in_=self.h2all[:, j, :], in_offset=None))

    def p4_io(self):
        i = self.inp
        self.w_gate = i("exp_w_gate", [2, 16, D, 512])
        self.w_up = i("exp_w_up", [2, 16, D, 512])
        self.w_down = i("exp_w_down", [2, 16, 512, D])

    def zero_slots(self):
        self.push()
        zt = self.sb("zt", [128, D], F32)
        self.G(lambda e: e.memset(zt[:, :], 0.0), w=[zt])
        self.dma("sp", self.oslots[16 * self.CAP:16 * self.CAP + 128, :], zt[:, :], r=[zt], w=[self.oslots])
        self.pop()

    def p4_experts(self, l):
        B = self.bank
        V, S, G = self.V, self.S, self.G
        s = self.sb
        wg = [s(f"wg{i}", [128, 8, 512], BF16) for i in range(2)]
        wu = [s(f"wu{i}", [128, 8, 512], BF16) for i in range(2)]
        wd = [s(f"wd{i}", [128, 4, D], BF16) for i in range(2)]
        rt = [s(f"rtok{i}", [128, 4, D], BF16) for i in range(2)]
        rT = s("rT", [128, 8, 512], BF16)
        sil = [s(f"sil{i}", [128, 512], BF16) for i in range(2)]
        hidT = s("hidT", [128, 4, 512], BF16)
        osb = [s(f"eosb{i}", [128, D], F32) for i in range(2)]

        def load_w(e):
            i = e % 2
            self.dma("pool", wg[i][:, :, :], self.w_gate[l, e].rearrange("(k p) f -> p k f", p=128), r=[self.w_gate], w=[wg[i]])
            self.dma("pool", wu[i][:, :, :], self.w_up[l, e].rearrange("(k p) f -> p k f", p=128), r=[self.w_up], w=[wu[i]])
            self.dma("pool", wd[i][:, :, :], self.w_down[l, e].rearrange("(k p) f -> p k f", p=128), r=[self.w_down], w=[wd[i]])

        glist = [(e, off, nb) for e in range(16) for (off, nb) in self.GROUPS]
        load_w(0)
        ob = 0

        def load_rows(gi):
            e, off, nb = glist[gi]
            r0 = e * self.CAP + off
            rtk = rt[gi % 2]
            self.dma("sp", rtk[:, 0:nb, :], self.h2slots[r0:r0 + nb * 128, :].rearrange("(b p) d -> p b d", p=128), r=[self.h2slots], w=[rtk])
        load_rows(0)
        for gi, (e, off, nb) in enumerate(glist):
            if off == 0 and e + 1 < 16:
                load_w(e + 1)
            i = e % 2
            r0 = e * self.CAP + off
            N = nb * 128
            rtk = rt[gi % 2]
            if gi + 1 < len(glist):
                load_rows(gi + 1)
            for blk in range(nb):
                pT = B[blk % 2]
                pTb = pT[:, :].bitcast(BF16).rearrange("p (k t) -> p k t", k=8)
                for k in range(8):
                    self.tr(pTb[:, k, :], rtk[:, blk, k * 128:(k + 1) * 128], self.identb[:, :], r=[rtk, self.identb], w=[pT], signal=(k == 7))
                if blk % 2 == 0:
                    V(lambda e_, blk=blk, pTb=pTb: e_.tensor_copy(out=rT[:, :, blk * 128:(blk + 1) * 128], in_=pTb), r=[pT], w=[rT])
                else:
                    S(lambda e_, blk=blk, pTb=pTb: e_.copy(out=rT[:, :, blk * 128:(blk + 1) * 128], in_=pTb), r=[pT], w=[rT])
            for fc in range(4):
                pg, pu = B[2 + 2 * (fc % 2)], B[3 + 2 * (fc % 2)]
                for k in range(8):
                    self.mm(pg[:, 0:N], wg[i][:, k, fc * 128:(fc + 1) * 128], rT[:, k, 0:N], r=[wg[i], rT], w=[pg], start=(k == 0), stop=(k == 7))
                for k in range(8):
                    self.mm(pu[:, 0:N], wu[i][:, k, fc * 128:(fc + 1) * 128], rT[:, k, 0:N], r=[wu[i], rT], w=[pu], start=(k == 0), stop=(k == 7))
                sl = sil[fc % 2]
                S(lambda e_, sl=sl, pg=pg, N=N: e_.activation(out=sl[:, 0:N], in_=pg[:, 0:N], func=AF.Silu), r=[pg], w=[sl])
                V(lambda e_, sl=sl, pu=pu, fc=fc, N=N: e_.tensor_tensor(out=hidT[:, fc, 0:N], in0=pu[:, 0:N], in1=sl[:, 0:N], op=ALU.mult), r=[pu, sl], w=[hidT])
            for blk in range(nb):
                o = osb[ob % 2]
                ob += 1
                for half in range(2):
                    pd = B[6 + half]
                    for fc in range(4):
                        self.mm(pd[:, :], hidT[:, fc, blk * 128:(blk + 1) * 128], wd[i][:, fc, half * 512:(half + 1) * 512], r=[hidT, wd[i]], w=[pd],
                                start=(fc == 0), stop=(fc == 3))
                    V(lambda e_, o=o, pd=pd, half=half: e_.tensor_tensor(out=o[:, half * 512:(half + 1) * 512], in0=pd[:, :],
                                                                         in1=self.opg[:, 1, half * 512:(half + 1) * 512], op=ALU.mult), r=[pd, self.opg], w=[o], x=[o])
                self.dma("sp", self.oslots[r0 + blk * 128:r0 + (blk + 1) * 128, :], o[:, :], r=[o], w=[])

    def p5_alloc(self, l):
        s = self.sb
        self.lng2 = s("lng2", [128, D], F32); self.lnb2 = s("lnb2", [128, D], F32)
        self.dma("sp", self.lng2[:, :], self.ln2_g[l].partition_broadcast(128), r=[self.ln2_g], w=[self.lng2])
        self.dma("sp", self.lnb2[:, :], self.ln2_b[l].partition_broadcast(128), r=[self.ln2_b], w=[self.lnb2])
        self.rA = [s(f"rA{i}", [128, D], F32) for i in range(2)]
        self.rB = [s(f"rB{i}", [128, D], F32) for i in range(2)]
        self.x1r = [s(f"x1r{i}", [128, D], F32) for i in range(2)]
        self.x2t = [s(f"x2t{i}", [128, D], F32) for i in range(2)]
        self.st5 = s("st5", [128, 24], F32)
        self.y5 = [s(f"y5_{i}", [128, D], F32) for i in range(2)]

    def p5_loads(self, jj):
        if jj >= NCH:
            return
        rA_, rB_, x1r_ = self.rA[jj % 2], self.rB[jj % 2], self.x1r[jj % 2]
        self.cx.dma("pool", None, None, reads=[self.oslots.b, self.slotA.b], writes=[rA_.b],
                    fn=lambda e: e.indirect_dma_start(out=rA_[:, :], out_offset=None, in_=self.oslots[:, :],
                                                      in_offset=bass.IndirectOffsetOnAxis(ap=self.slotA[:, jj:jj + 1], axis=0)))
        self.cx.dma("pool", None, None, reads=[self.oslots.b, self.slotB.b], writes=[rB_.b],
                    fn=lambda e: e.indirect_dma_start(out=rB_[:, :], out_offset=None, in_=self.oslots[:, :],
                                                      in_offset=bass.IndirectOffsetOnAxis(ap=self.slotB[:, jj:jj + 1], axis=0)))
        self.dma("sp", x1r_[:, :], self.x1d[jj * 128:(jj + 1) * 128, :], r=[self.x1d], w=[x1r_])

    def p5_S1(self, j):
        if j >= NCH:
            return
        V, S, G = self.V, self.S, self.G
        rA, rB, x1r, y5 = self.rA[j % 2], self.rB[j % 2], self.x1r[j % 2], self.y5[j % 2]
        S(lambda e: e.activation(out=rA[:, :], in_=rA[:, :], func=AF.Identity, scale=self.gAB[:, 0, j:j + 1]), r=[rA, self.gAB], w=[rA])
        V(lambda e: e.scalar_tensor_tensor(out=rA[:, :], in0=rB[:, :], scalar=self.gAB[:, 1, j:j + 1], in1=rA[:, :], op0=ALU.mult, op1=ALU.add), r=[rB, rA, self.gAB], w=[rA])
        V(lambda e: e.scalar_tensor_tensor(out=y5[:, :], in0=x1r[:, :], scalar=float(ALPHA), in1=rA[:, :], op0=ALU.mult, op1=ALU.add), r=[x1r, rA], w=[y5], x=[rA])

    def p5_S2(self, j, dst):
        V, S, G = self.V, self.S, self.G
        rows = slice(j * 128, (j + 1) * 128)
        y5, x2 = self.y5[j % 2], self.x2t[j % 2]
        self.ln_stats(y5, lambda a, b: y5[:, a:b], D, self.st5, act=True)
        S(lambda e: e.activation(out=x2[:, :], in_=y5[:, :], func=AF.Identity, bias=self.st5[:, 4:5], scale=self.st5[:, 3:4]), r=[y5, self.st5], w=[x2])
        V(lambda e: e.tensor_tensor(out=x2[:, 0:512], in0=x2[:, 0:512], in1=self.lng2[:, 0:512], op=ALU.mult), r=[x2, self.lng2], w=[x2])
        G(lambda e: e.tensor_tensor(out=x2[:, 512:1024], in0=x2[:, 512:1024], in1=self.lng2[:, 512:1024], op=ALU.mult), r=[x2, self.lng2], w=[x2])
        V(lambda e: e.tensor_tensor(out=x2[:, 0:512], in0=x2[:, 0:512], in1=self.lnb2[:, 0:512], op=ALU.add), r=[x2, self.lnb2], w=[x2])
        G(lambda e: e.tensor_tensor(out=x2[:, 512:1024], in0=x2[:, 512:1024], in1=self.lnb2[:, 512:1024], op=ALU.add), r=[x2, self.lnb2], w=[x2])
        self.dma("sp", dst[rows, :], x2[:, :], r=[x2], w=[dst])

    def p5_all(self, l, dst):
        self.p5_loads(0); self.p5_loads(1)
        self.p5_S1(0)
        for j in range(NCH):
            self.p5_loads(j + 2)
            self.p5_S1(j + 1)
            self.p5_S2(j, dst)

    def build(self):
        self.declare_io(); self.s5_io(); self.p3_io(); self.p4_io()
        self.setup()
        x_src = self.x_in
        for l in range(self.nlayers):
            dst = self.out if l == self.nlayers - 1 else self.xmid
            self.push()
            self.layer_alloc(); self.route_alloc(); self.layer_prep(l, 0)
            self.zero_slots()
            self.push(); self.qk_alloc()
            self.push(); self.s5_alloc(); self.s5_prep(l); self.load_win(l); self.p1v2_alloc()
            self.p1_all(l, x_src)
            self.pop()
            self.push(); self.p2_alloc(); self.p2_attention(); self.pop()
            self.pop()
            self.push()
            self.opg = self.sb("opg", [128, 2, 1024], F32)
            self.opg2 = self.sb("opg2", [128, 2, 1024], F32)
            self.layer_prep(l, 1)
            self.push(); self.p3_alloc(l)
            self.p3_route_alloc()
            self.p3_all(l, x_src)
            self.pop()
            self.push(); self.p4_experts(l); self.pop()
            self.push(); self.p5_alloc(l)
            self.p5_all(l, dst)
            self.pop()
            self.pop()
            self.pop()
            x_src = dst
        if getattr(self, "dbg_hook", None):
            self.dbg_hook(self)
        self.finish()


def make_inputs(inp, b):
    c = np.ascontiguousarray
    f = lambda k: np.asarray(inp[k])
    br, bi = f("ssm_b_re"), f("ssm_b_im")
    def bl(a):
        L = a.shape[0]
        return a.reshape(L, 2, 8, 64, 16).transpose(0, 2, 4, 1, 3).reshape(L, 128, 2, 64)
    bT = np.stack([bl(br), bl(bi)], axis=1)
    cr, ci = f("ssm_c_re"), f("ssm_c_im")
    cT = np.concatenate([cr.transpose(0, 1, 3, 2), ci.transpose(0, 1, 3, 2)], axis=2)
    L = br.shape[0]
    d = {
        "x": c(f("x")[b]), "ccol": c(f("c")[b].reshape(8, 128).T), "pos": c(f("positions")[b].reshape(32, 128).T),
        "ada_w": f("ada_w"), "ada_b": f("ada_b"), "w_in": f("w_in"), "gm_ln_g": f("gm_ln_g"), "gm_ln_b": f("gm_ln_b"),
        "gm_ws": f("gm_ws"), "gm_bsT": c(f("gm_bs").transpose(0, 2, 1)),
        "lam_re": c(f("ssm_lam_re").reshape(L, 1024)), "lam_im": c(f("ssm_lam_im").reshape(L, 1024)), "log_dt": f("ssm_log_dt"),
        "ssm_bT": c(bT), "ssm_cT": c(cT), "ssm_dT": c(f("ssm_d").reshape(L, 2, 128).transpose(0, 2, 1)),
        "glu_w": f("glu_w"), "glu_bT": c(f("glu_b").reshape(L, 2, 128).transpose(0, 2, 1)),
        "w_out": f("w_out"), "ln1_g": f("ln1_g"), "ln1_b": f("ln1_b"), "ln2_g": f("ln2_g"), "ln2_b": f("ln2_b"),
        "router_w": f("router_w"), "router_bias": f("router_bias"),
        "exp_w_gate": f("exp_w_gate"), "exp_w_up": f("exp_w_up"), "exp_w_down": f("exp_w_down"),
    }
    return d


_CACHE = {}


def kernel(**inputs):
    n = 8
    if "nc" not in _CACHE:
        nc = bass.Bass("TRN2", target_bir_lowering=False)
        kb = KB(nc)
        kb.build()
        _CACHE["nc"] = nc
        _CACHE["names"] = kb.in_names
    nc = _CACHE["nc"]
    names = _CACHE["names"]
    in_maps = []
    for b in range(n):
        im = make_inputs(inputs, b)
        in_maps.append({k: v for k, v in im.items() if k in names})
    res = run_bass_kernel_spmd(nc, in_maps, core_ids=list(range(n)))
    out = np.stack([np.asarray(r["out"]) for r in res.results], axis=0)
    return out.astype(np.float32)
```

```python
import numpy as np
import concourse.bass as bass
import concourse.mybir as mybir

F32 = mybir.dt.float32
BF16 = mybir.dt.bfloat16
I32 = mybir.dt.int32
U32 = mybir.dt.uint32
AF = mybir.ActivationFunctionType
ALU = mybir.AluOpType
AX = mybir.AxisListType


RELAXED = ()
ALLOW_RELAX = True


class Buf:
    __slots__ = ("w", "r", "name")

    def __init__(self, name=""):
        self.w = None
        self.r = []
        self.name = name


class Ctx:
    def __init__(self, nc, strict_same=False):
        self.nc = nc
        self.strict_same = strict_same
        self.relaxed = set(RELAXED)
        self.engs = {"pe": nc.tensor, "act": nc.scalar, "dve": nc.vector, "pool": nc.gpsimd, "sp": nc.sync}
        self.sem = {}
        self.cnt = {}
        for e in ("pe", "act", "dve", "pool"):
            self.sem[e] = nc.alloc_semaphore("s_" + e)
            self.cnt[e] = 0
        self.dq = {}
        for q, n in (("sp", 10), ("act", 4), ("pool", 8)):
            self.dq[q] = {"sems": [nc.alloc_semaphore(f"d_{q}{i}") for i in range(n)], "vals": [0] * n, "k": 0}
        self.waited = {}
        self.nbuf = 0
        self.out_events = []

    def buf(self, name=""):
        return Buf(name)

    def _wait(self, eng, ev):
        sem, val = ev
        key = (eng, id(sem))
        if self.waited.get(key, 0) >= val:
            return
        self.engs[eng].wait_ge(sem, val)
        self.waited[key] = val

    def _deps(self, eng, reads, writes, relax=()):
        own = self.sem.get(eng)
        rl = set(id(b) for b in relax) if ALLOW_RELAX else set()

        def chk(b, ev):
            if ev[0] is own and (eng == "pe" or id(b) in rl):
                return
            self._wait(eng, ev)
        for b in reads:
            if b.w is not None:
                chk(b, b.w)
        for b in writes:
            if b.w is not None:
                chk(b, b.w)
            for ev in b.r:
                chk(b, ev)

    def _commit(self, ev, reads, writes):
        for b in writes:
            b.w = ev
            b.r = []
        for b in reads:
            b.r.append(ev)
            if len(b.r) > 24:
                b.r = b.r[-24:]

    def op(self, eng, fn, reads=(), writes=(), signal=True, relax=()):
        self._deps(eng, reads, writes, relax)
        inst = fn(self.engs[eng])
        if signal:
            self.cnt[eng] += 1
            inst.then_inc(self.sem[eng], 1)
            ev = (self.sem[eng], self.cnt[eng])
        else:
            ev = (self.sem[eng], self.cnt[eng] + 1)
        self._commit(ev, reads, writes)
        return ev

    def dma(self, q, out, in_, reads=(), writes=(), fn=None, **kw):
        d = self.dq[q]
        i = d["k"] % len(d["sems"])
        d["k"] += 1
        sem = d["sems"][i]
        self._deps(q, reads, writes)
        if d["vals"][i] > 0:
            self._wait(q, (sem, d["vals"][i]))
        if fn is None:
            inst = self.engs[q].dma_start(out=out, in_=in_, **kw)
        else:
            inst = fn(self.engs[q])
        d["vals"][i] += 16
        inst.then_inc(sem, 16)
        ev = (sem, d["vals"][i])
        self._commit(ev, reads, writes)
        return ev

    def barrier(self):
        evs = [(self.sem[e], self.cnt[e]) for e in self.sem if self.cnt[e] > 0]
        for q, d in self.dq.items():
            for sem, v in zip(d["sems"], d["vals"]):
                if v > 0:
                    evs.append((sem, v))
        for eng in ("pe", "act", "dve", "pool", "sp"):
            own = self.sem.get(eng)
            for ev in evs:
                self._wait(eng, ev)

    def finish(self, bufs):
        for b in bufs:
            if b.w is not None:
                self._wait("sp", b.w)
            for ev in b.r:
                self._wait("sp", ev)

from concourse.bass_utils import run_bass_kernel_spmd
import math
import contextlib

T = 4096
D = 1024
NCH = 32
PW = 2304
EPS = 1e-5
ALPHA = (2.0 * 2) ** 0.25
TWO_PI = 2.0 * math.pi
ROPE_THETA = 500000.0


class Tl:
    def __init__(self, h, name=""):
        self.h = h
        self.b = Buf(name)

    def __getitem__(self, k):
        return self.h[k]


class KB:
    def __init__(self, nc, nlayers=2, dbg=(), stop_after=None):
        self.nc = nc
        self.cx = Ctx(nc)
        self.dbg = set(dbg)
        self.stop_after = stop_after
        self.nlayers = nlayers
        self.outs = []
        self.stk = [contextlib.ExitStack()]
        self.nps = 0

    def inp(self, name, shape, dt=F32):
        self.in_names = getattr(self, "in_names", set())
        self.in_names.add(name)
        return Tl(self.nc.dram_tensor(name, list(shape), dt, kind="ExternalInput").ap(), name)

    def outp(self, name, shape, dt=F32):
        t = Tl(self.nc.dram_tensor(name, list(shape), dt, kind="ExternalOutput").ap(), name)
        self.outs.append(t)
        return t

    def scratch(self, name, shape, dt):
        return Tl(self.nc.dram_tensor(name, list(shape), dt, kind="Internal").ap(), name)

    def sb(self, name, shape, dt):
        self.nsb = getattr(self, "nsb", 0) + 1
        h = self.stk[-1].enter_context(self.nc.sbuf_tensor(f"{name}_{self.nsb}", list(shape), dt))
        return Tl(h, name)

    def push(self):
        self.stk.append(contextlib.ExitStack())

    def pop(self):
        self.cx.barrier()
        self.stk.pop().close()

    def ps(self, name, shape, dt=F32):
        return Tl(self.nc.alloc_psum_tensor(name, list(shape), dt), name)

    def _rw(self, r, w):
        return [t.b for t in r], [t.b for t in w]

    def V(self, fn, r=(), w=(), x=()):
        r, w = self._rw(r, w)
        return self.cx.op("dve", fn, r, w, relax=[t.b for t in x])

    def S(self, fn, r=(), w=(), x=()):
        r, w = self._rw(r, w)
        return self.cx.op("act", fn, r, w, relax=[t.b for t in x])

    def G(self, fn, r=(), w=(), x=()):
        r, w = self._rw(r, w)
        return self.cx.op("pool", fn, r, w, relax=[t.b for t in x])

    def P(self, fn, r=(), w=(), signal=True):
        r, w = self._rw(r, w)
        return self.cx.op("pe", fn, r, w, signal=signal)

    def dma(self, q, out, in_, r=(), w=(), **kw):
        r, w = self._rw(r, w)
        return self.cx.dma(q, out, in_, r, w, **kw)

    def mm(self, out, lhsT, rhs, r, w, start, stop, signal=None):
        if signal is None:
            signal = stop
        return self.P(lambda e: e.matmul(out, lhsT, rhs, start=start, stop=stop), r, w, signal=signal)

    def tr(self, out, in_, ident, r, w, signal=True):
        return self.P(lambda e: e.transpose(out, in_, ident), r, w, signal=signal)

    def declare_io(self):
        i = self.inp
        self.x_in = i("x", [T, D])
        self.ccol = i("ccol", [128, 8])
        self.pos = i("pos", [128, NCH], I32)
        self.ada_w = i("ada_w", [2, D, 6 * D])
        self.ada_b = i("ada_b", [2, 6 * D])
        self.w_in = i("w_in", [2, D, PW])
        self.gm_ln_g = i("gm_ln_g", [2, 256])
        self.gm_ln_b = i("gm_ln_b", [2, 256])
        self.gm_ws = i("gm_ws", [2, 4, 128, 128])
        self.gm_bsT = i("gm_bsT", [2, 128, 4])
        self.out = self.outp("out", [T, D])
        self.mixtok = self.scratch("mixtok", [T, 1024], BF16)
        self.vd = self.scratch("vd", [T, 520], BF16)

    def consts(self):
        nc = self.nc
        self.identb = self.sb("identb", [128, 128], BF16)
        self.identf = self.sb("identf", [128, 128], F32)
        self.onesf = self.sb("onesf", [128, 128], F32)
        self.eps_t = self.sb("eps_t", [128, 1], F32)
        self.G(lambda e: e.memset(self.onesf[:, :], 1.0), w=[self.onesf])
        self.G(lambda e: e.memset(self.eps_t[:, :], EPS), w=[self.eps_t])
        self.mhalf = self.sb("mhalf", [128, 1], F32)
        self.G(lambda e: e.memset(self.mhalf[:, :], -0.5), w=[self.mhalf])
        self.G(lambda e: e.affine_select(out=self.identf[:, :], in_=self.onesf[:, :], pattern=[[-1, 128]],
                                         compare_op=ALU.is_equal, fill=0.0, base=0, channel_multiplier=1),
               r=[self.onesf], w=[self.identf])
        self.G(lambda e: e.tensor_copy(out=self.identb[:, :], in_=self.identf[:, :]), r=[self.identf], w=[self.identb])
        self.posf = self.sb("posf", [128, NCH], F32)
        self.posi = self.sb("posi", [128, NCH], I32)
        self.dma("sp", self.posi[:, :], self.pos[:, :], r=[self.pos], w=[self.posi])
        self.V(lambda e: e.tensor_copy(out=self.posf[:, :], in_=self.posi[:, :]), r=[self.posi], w=[self.posf])
        self.cs = self.sb("cs", [128, NCH, 8], F32)
        self.sn = self.sb("sn", [128, NCH, 8], F32)
        self.push()
        ang = self.sb("ang", [128, NCH, 8], F32)
        tmp = self.sb("angt", [128, NCH, 8], F32)
        tmi = self.sb("angi", [128, NCH, 8], I32)
        for j in range(8):
            fr = ROPE_THETA ** (-(j * 2.0) / 16.0)
            self.V(lambda e, j=j, fr=fr: e.tensor_scalar(out=ang[:, :, j], in0=self.posf[:, :], scalar1=float(fr), scalar2=None, op0=ALU.mult),
                   r=[self.posf], w=[ang])
        self.sincos(ang, self.sn, self.cs, tmp, tmi, [128, NCH * 8])
        self.pop()

    def _flat(self, t):
        ap = t[:]
        if len(ap.shape) == 2:
            return ap
        names = " ".join(f"a{i}" for i in range(len(ap.shape) - 1))
        return ap.rearrange(f"p {names} -> p ({names})")

    def range_reduce(self, src, dst, tmp, tmi, shift):
        s, d, t, ti = self._flat(src), self._flat(dst), self._flat(tmp), self._flat(tmi)
        self.V(lambda e: e.tensor_scalar(out=t, in0=s, scalar1=float(shift), scalar2=float(1.0 / TWO_PI), op0=ALU.add, op1=ALU.mult),
               r=[src], w=[tmp])
        self.V(lambda e: e.tensor_copy(out=ti, in_=t), r=[tmp], w=[tmi])
        self.V(lambda e: e.tensor_copy(out=t, in_=ti), r=[tmi], w=[tmp])
        self.V(lambda e: e.tensor_scalar(out=t, in0=t, scalar1=float(-TWO_PI), scalar2=float(shift), op0=ALU.mult, op1=ALU.add),
               r=[tmp], w=[tmp])
        self.V(lambda e: e.tensor_tensor(out=d, in0=t, in1=s, op=ALU.add), r=[tmp, src], w=[dst])
        self.V(lambda e: e.tensor_scalar(out=t, in0=d, scalar1=float(math.pi), scalar2=float(-TWO_PI), op0=ALU.is_gt, op1=ALU.mult),
               r=[dst], w=[tmp])
        self.V(lambda e: e.tensor_tensor(out=d, in0=d, in1=t, op=ALU.add), r=[tmp, dst], w=[dst])
        self.V(lambda e: e.tensor_scalar(out=t, in0=d, scalar1=float(-math.pi), scalar2=float(TWO_PI), op0=ALU.is_lt, op1=ALU.mult),
               r=[dst], w=[tmp])
        self.V(lambda e: e.tensor_tensor(out=d, in0=d, in1=t, op=ALU.add), r=[tmp, dst], w=[dst])
        self.V(lambda e: e.tensor_scalar(out=d, in0=d, scalar1=float(math.pi), scalar2=float(-math.pi), op0=ALU.min, op1=ALU.max),
               r=[dst], w=[dst])

    def sincos(self, ang, sn, cs, tmp, tmi, shape):
        self.range_reduce(ang, sn, tmp, tmi, 0.0)
        self.S(lambda e: e.activation(out=self._flat(sn), in_=self._flat(sn), func=AF.Sin), r=[sn], w=[sn])
        self.range_reduce(ang, cs, tmp, tmi, math.pi / 2)
        self.S(lambda e: e.activation(out=self._flat(cs), in_=self._flat(cs), func=AF.Sin), r=[cs], w=[cs])

    def setup(self):
        self.bank = [self.ps(f"bank{i}", [128, 512], F32) for i in range(8)]
        self.consts()

    def layer_alloc(self):
        self.modp = self.sb("modp", [128, 4, 8], F32)

        self.wsT = self.sb("wsT", [128, 4, 128], BF16)
        self.gbs = self.sb("gbs", [128, 4], F32)
        self.glng = self.sb("glng", [128, 256], F32)
        self.glnb = self.sb("glnb", [128, 256], F32)

    def prep_alloc(self):
        self.adaw = [self.sb(f"adaw{i}", [128, 8, 512], F32) for i in range(2)]
        self.adab = [self.sb(f"adab{i}", [128, 512], F32) for i in range(2)]
        self.modc = [self.sb(f"modc{i}", [128, 512], F32) for i in range(2)]
        self.wtmp = self.sb("wtmp", [128, 4, 128], F32)
        ccs = self.sb("ccs", [128, 8], F32)
        self.dma("sp", ccs[:, :], self.ccol[:, :], r=[self.ccol], w=[ccs])
        self.S(lambda e: e.activation(out=ccs[:, :], in_=ccs[:, :], func=AF.Silu), r=[ccs], w=[ccs])
        self.condrep = self.sb("condrep", [128, 8, 128], F32)
        self.V(lambda e: e.tensor_copy(out=self.condrep[:, :, :], in_=ccs[:, :].unsqueeze(2).to_broadcast([128, 8, 128])),
               r=[ccs], w=[self.condrep])


    def load_win(self, l):
        self.win = self.sb("win", [128, 8, PW], BF16)
        for k in range(8):
            self.dma("pool", self.win[:, k, :], self.w_in[l, k * 128:(k + 1) * 128, :], r=[self.w_in], w=[self.win])

    def layer_prep(self, l, part=0):
        self.push()
        self.prep_alloc()
        pb = self.bank[7]
        pt = self.bank[6]
        for n in range(12):
            if (part == 0) != (n // 2 in (0, 1)):
                continue
            aw = self.adaw[n % 2]
            ab = self.adab[n % 2]
            mc = self.modc[n % 2]
            self.dma("sp", aw[:, :, :], self.ada_w[l, :, n * 512:(n + 1) * 512].rearrange("(k p) n -> p k n", p=128),
                     r=[self.ada_w], w=[aw])
            self.dma("sp", ab[:, :], self.ada_b[l, n * 512:(n + 1) * 512].partition_broadcast(128), r=[self.ada_b], w=[ab])
            for k in range(8):
                self.mm(pb[:, :], self.condrep[:, k, :], aw[:, k, :], r=[self.condrep, aw], w=[pb], start=(k == 0), stop=(k == 7))
            which, half = n // 2, n % 2
            if which in (2, 5, 3, 4):
                tgt = self.opg if which in (2, 5) else self.opg2
                gi = {2: 0, 5: 1, 3: 0, 4: 1}[which]
                dst = tgt[:, gi, half * 512:(half + 1) * 512]
                self.V(lambda e, dst=dst: e.tensor_tensor(out=dst, in0=pb[:, :], in1=ab[:, :], op=ALU.add), r=[pb, ab], w=[tgt])
                if which != 3:
                    self.V(lambda e, dst=dst: e.tensor_scalar(out=dst, in0=dst, scalar1=1.0, scalar2=None, op0=ALU.add), r=[tgt], w=[tgt])
            else:
                slot = {0: 0, 1: 1}[which]
                self.V(lambda e: e.tensor_tensor(out=mc[:, :], in0=pb[:, :], in1=ab[:, :], op=ALU.add), r=[pb, ab], w=[mc])
                for b4 in range(4):
                    self.tr(pt[:, b4 * 128:(b4 + 1) * 128], mc[:, b4 * 128:(b4 + 1) * 128], self.identf[:, :], r=[mc, self.identf], w=[pt])
                addc = 1.0 if slot in (1, 3) else 0.0
                for b4 in range(4):
                    self.V(lambda e, b4=b4: e.tensor_scalar(out=self.modp[:, slot, half * 4 + b4:half * 4 + b4 + 1],
                                                            in0=pt[:, b4 * 128:b4 * 128 + 1], scalar1=float(addc), scalar2=None, op0=ALU.add),
                           r=[pt], w=[self.modp])
        if part == 0:
            self.dma("sp", self.wtmp[:, :, :], self.gm_ws[l].rearrange("h t s -> t h s"), r=[self.gm_ws], w=[self.wtmp])
            self.G(lambda e: e.affine_select(out=self.wtmp[:, :, :], in_=self.wtmp[:, :, :], pattern=[[0, 4], [-1, 128]],
                                             compare_op=ALU.is_ge, fill=0.0, base=0, channel_multiplier=1), r=[self.wtmp], w=[self.wtmp])
            for h in range(4):
                self.tr(pt[:, h * 128:(h + 1) * 128], self.wtmp[:, h, :], self.identf[:, :], r=[self.wtmp, self.identf], w=[pt])
            self.V(lambda e: e.tensor_copy(out=self.wsT[:, :, :], in_=pt[:, :].rearrange("p (h t) -> p h t", h=4)), r=[pt], w=[self.wsT])
            self.dma("sp", self.gbs[:, :], self.gm_bsT[l], r=[self.gm_bsT], w=[self.gbs])
            self.dma("sp", self.glng[:, :], self.gm_ln_g[l].partition_broadcast(128), r=[self.gm_ln_g], w=[self.glng])

            self.dma("sp", self.glnb[:, :], self.gm_ln_b[l].partition_broadcast(128), r=[self.gm_ln_b], w=[self.glnb])
        self.pop()

    def ln_stats(self, src, src_ap_fn, n, st, act=False):
        if act:
            return self.ln_stats_act(src, src_ap_fn, n, st)
        nchk = (n + 511) // 512
        w = n // nchk
        for i in range(nchk):
            self.V(lambda e, i=i: e.bn_stats(out=st[:, 8 + i * 6:8 + (i + 1) * 6], in_=src_ap_fn(i * w, (i + 1) * w)), r=[src], w=[st])
        self.V(lambda e: e.bn_aggr(out=st[:, 0:2], in_=st[:, 8:8 + 6 * nchk]), r=[st], w=[st])
        self.V(lambda e: e.tensor_scalar(out=st[:, 2:3], in0=st[:, 1:2], scalar1=float(EPS), scalar2=None, op0=ALU.add), r=[st], w=[st])
        self.G(lambda e: e.tensor_tensor(out=st[:, 3:4], in0=st[:, 2:3], in1=self.mhalf[:, 0:1], op=ALU.pow), r=[st, self.mhalf], w=[st])
        self.V(lambda e: e.tensor_scalar(out=st[:, 4:5], in0=st[:, 0:1], scalar1=-1.0, scalar2=st[:, 3:4], op0=ALU.mult, op1=ALU.mult), r=[st], w=[st])

    def ln_stats_act(self, src, src_ap_fn, n, st):
        nchk = (n + 511) // 512
        w = n // nchk
        for i in range(nchk):
            self.V(lambda e, i=i: e.bn_stats(out=st[:, 8 + i * 6:8 + (i + 1) * 6], in_=src_ap_fn(i * w, (i + 1) * w)), r=[src], w=[st])
        self.V(lambda e: e.bn_aggr(out=st[:, 0:2], in_=st[:, 8:8 + 6 * nchk]), r=[st], w=[st])
        self.S(lambda e: e.activation(out=st[:, 2:3], in_=st[:, 1:2], func=AF.Sqrt, bias=self.eps_t[:, 0:1], scale=1.0), r=[st, self.eps_t], w=[st])
        self.V(lambda e: e.reciprocal(out=st[:, 3:4], in_=st[:, 2:3]), r=[st], w=[st])
        self.V(lambda e: e.tensor_scalar(out=st[:, 4:5], in0=st[:, 0:1], scalar1=-1.0, scalar2=st[:, 3:4], op0=ALU.mult, op1=ALU.mult), r=[st], w=[st])

    def qk_alloc(self):
        self.QT = self.sb("QT", [128, 4, T], BF16)
        self.KT = self.sb("KT", [128, 4, T], BF16)

    def p1_alloc(self):
        s = self.sb
        self.xt = [s(f"xt{i}", [128, D], F32) for i in range(2)]
        self.xn = [s(f"xn{i}", [128, D], BF16) for i in range(2)]
        self.st = [s(f"st{i}", [128, 24], F32) for i in range(2)]
        self.st2 = [s(f"stb{i}", [128, 24], F32) for i in range(2)]
        self.hT = [s(f"hT{i}", [128, 8, 128], BF16) for i in range(2)]
        self.gl = [s(f"gl{i}", [128, 512], F32) for i in range(1)] * 2
        self.vnb = [s(f"vnb{i}", [128, 256], F32) for i in range(1)] * 2
        self.vb = [s(f"vb{i}", [128, 256], BF16) for i in range(1)] * 2
        self.aout = [s(f"aout{i}", [128, 256], BF16) for i in range(1)] * 2
        self.qf = [s(f"qf{i}", [128, 2, 512], F32) for i in range(1)] * 2
        self.qb = [s(f"qb{i}", [128, 2, 512], BF16) for i in range(1)] * 2
        self.rt = [s(f"rt{i}", [128, 4, 8, 8], F32) for i in range(1)] * 2
        self.vp = [s(f"vp{i}", [128, 8, 65], BF16) for i in range(1)] * 2
        for i in range(1):
            self.G(lambda e, i=i: e.memset(self.vp[i][:, :, :], 1.0), w=[self.vp[i]])

    def p1_chunk(self, l, j, x_src):
        i2 = j % 2
        xt, xn, st, st2, hT = self.xt[i2], self.xn[i2], self.st[i2], self.st2[i2], self.hT[i2]
        gl, vnb, vb, aout, qf, qb, rt, vp = self.gl[i2], self.vnb[i2], self.vb[i2], self.aout[i2], self.qf[i2], self.qb[i2], self.rt[i2], self.vp[i2]
        B = self.bank
        rows = slice(j * 128, (j + 1) * 128)
        if j == 0:
            self.dma("sp", xt[:, :], x_src[rows, :], r=[x_src], w=[xt])
        if j + 1 < NCH:
            xtn = self.xt[(j + 1) % 2]
            self.dma("sp", xtn[:, :], x_src[(j + 1) * 128:(j + 2) * 128, :], r=[x_src], w=[xtn])
        self.ln_stats(xt, lambda a, b: xt[:, a:b], D, st)
        self.S(lambda e: e.activation(out=xn[:, :], in_=xt[:, :], func=AF.Identity, bias=st[:, 4:5], scale=st[:, 3:4]), r=[xt, st], w=[xn])
        pT = B[0]
        pTb = pT[:, :].bitcast(BF16).rearrange("p (k t) -> p k t", k=8)
        for k in range(8):
            self.tr(pTb[:, k, :], xn[:, k * 128:(k + 1) * 128], self.identb[:, :], r=[xn, self.identb], w=[pT], signal=(k == 7))
        for k in range(8):
            self.V(lambda e, k=k: e.tensor_scalar(out=hT[:, k, :], in0=pTb[:, k, :], scalar1=self.modp[:, 1, k:k + 1], scalar2=self.modp[:, 0, k:k + 1],
                                                  op0=ALU.mult, op1=ALU.add), r=[pT, self.modp], w=[hT], x=[hT])
        for bi, c0 in ((1, 0), (2, 512), (3, 1024), (4, 1536)):
            for k in range(8):
                self.mm(B[bi][:, :], hT[:, k, :], self.win[:, k, c0:c0 + 512], r=[hT, self.win], w=[B[bi]], start=(k == 0), stop=(k == 7))
        self.S(lambda e: e.activation(out=gl[:, :], in_=B[1][:, :], func=AF.Gelu_apprx_tanh), r=[B[1]], w=[gl])
        self.ln_stats(gl, lambda a, b: gl[:, 256 + a:256 + b], 256, st2)
        self.V(lambda e: e.tensor_scalar(out=vnb[:, :], in0=gl[:, 256:512], scalar1=st2[:, 3:4], scalar2=st2[:, 4:5], op0=ALU.mult, op1=ALU.add),
               r=[gl, st2], w=[vnb])
        self.V(lambda e: e.tensor_tensor(out=vnb[:, :], in0=vnb[:, :], in1=self.glng[:, :], op=ALU.mult), r=[vnb, self.glng], w=[vnb], x=[vnb])
        self.V(lambda e: e.tensor_tensor(out=vb[:, :], in0=vnb[:, :], in1=self.glnb[:, :], op=ALU.add), r=[vnb, self.glnb], w=[vb], x=[vnb])
        psv = B[5]
        for h in range(4):
            self.mm(psv[:, h * 64:(h + 1) * 64], self.wsT[:, h, :], vb[:, h * 64:(h + 1) * 64], r=[self.wsT, vb], w=[psv], start=True, stop=True,
                    signal=(h == 3))
        for h in range(4):
            self.V(lambda e, h=h: e.scalar_tensor_tensor(out=aout[:, h * 64:(h + 1) * 64], in0=psv[:, h * 64:(h + 1) * 64], scalar=self.gbs[:, h:h + 1],
                                                         in1=gl[:, h * 64:(h + 1) * 64], op0=ALU.add, op1=ALU.mult), r=[psv, self.gbs, gl], w=[aout], x=[aout])
        self.dma("sp", self.mixtok[rows, 0:256], aout[:, :], r=[aout], w=[self.mixtok])
        self.S(lambda e: e.copy(out=qf[:, 0, :], in_=B[2][:, :]), r=[B[2]], w=[qf])
        self.S(lambda e: e.copy(out=qf[:, 1, :], in_=B[3][:, :]), r=[B[3]], w=[qf], x=[qf])
        q4 = qf[:, :, :].rearrange("p a (h e) -> p (a h) e", e=64)
        o4 = qb[:, :, :].rearrange("p a (h e) -> p (a h) e", e=64)
        for a in range(2):
            xa1, xa2 = q4[:, a * 8:(a + 1) * 8, 0:8], q4[:, a * 8:(a + 1) * 8, 8:16]
            cb = self.cs[:, j, :].unsqueeze(1).to_broadcast([128, 8, 8])
            sb_ = self.sn[:, j, :].unsqueeze(1).to_broadcast([128, 8, 8])
            oa = o4[:, a * 8:(a + 1) * 8, :]
            self.G(lambda e, xa1=xa1, cb=cb: e.tensor_tensor(out=rt[:, 0, :, :], in0=xa1, in1=cb, op=ALU.mult), r=[qf, self.cs], w=[rt])
            self.G(lambda e, xa2=xa2, sb_=sb_: e.tensor_tensor(out=rt[:, 1, :, :], in0=xa2, in1=sb_, op=ALU.mult), r=[qf, self.sn], w=[rt])
            self.G(lambda e, xa2=xa2, cb=cb: e.tensor_tensor(out=rt[:, 2, :, :], in0=xa2, in1=cb, op=ALU.mult), r=[qf, self.cs], w=[rt])
            self.G(lambda e, xa1=xa1, sb_=sb_: e.tensor_tensor(out=rt[:, 3, :, :], in0=xa1, in1=sb_, op=ALU.mult), r=[qf, self.sn], w=[rt])
            self.G(lambda e, oa=oa: e.tensor_tensor(out=oa[:, :, 0:8], in0=rt[:, 0, :, :], in1=rt[:, 1, :, :], op=ALU.subtract), r=[rt], w=[qb])
            self.G(lambda e, oa=oa: e.tensor_tensor(out=oa[:, :, 8:16], in0=rt[:, 2, :, :], in1=rt[:, 3, :, :], op=ALU.add), r=[rt], w=[qb])
            self.G(lambda e, oa=oa, a=a: e.tensor_copy(out=oa[:, :, 16:64], in_=q4[:, a * 8:(a + 1) * 8, 16:64]), r=[qf], w=[qb])
        pq = B[6]
        pqb = pq[:, :].bitcast(BF16).rearrange("p (a k t) -> p a k t", a=2, k=4)
        for a in range(2):
            for k in range(4):
                self.tr(pqb[:, a, k, :], qb[:, a, k * 128:(k + 1) * 128], self.identb[:, :], r=[qb, self.identb], w=[pq], signal=(a == 1 and k == 3))
        self.S(lambda e: e.copy(out=self.QT[:, :, j * 128:(j + 1) * 128], in_=pqb[:, 0, :, :]), r=[pq], w=[self.QT])
        self.S(lambda e: e.copy(out=self.KT[:, :, j * 128:(j + 1) * 128], in_=pqb[:, 1, :, :]), r=[pq], w=[self.KT])
        self.S(lambda e: e.copy(out=vp[:, :, 0:64], in_=B[4][:, :].rearrange("p (h e) -> p h e", e=64)), r=[B[4]], w=[vp])
        self.dma("sp", self.vd[rows, :], vp[:, :, :].rearrange("p h e -> p (h e)"), r=[vp], w=[self.vd])

    def finish(self):
        bufs = [t.b for t in self.outs]
        self.cx.finish(bufs)

    def p2_alloc(self):
        s = self.sb
        self.vbr = [s(f"vbr{i}", [128, 32, 520], BF16) for i in range(2)]
        self.negm = s("negm", [128, 256], BF16)
        negf = s("negf", [128, 256], F32)
        zf = s("zf", [128, 256], F32)
        self.G(lambda e: e.memset(zf[:, :], 0.0), w=[zf])
        self.G(lambda e: e.affine_select(out=negf[:, 0:128], in_=zf[:, 0:128], pattern=[[-1, 128]], compare_op=ALU.is_ge, fill=-30000.0,
                                         base=0, channel_multiplier=1), r=[zf], w=[negf])
        self.G(lambda e: e.affine_select(out=negf[:, 128:256], in_=zf[:, 128:256], pattern=[[1, 128]], compare_op=ALU.is_ge, fill=-30000.0,
                                         base=0, channel_multiplier=-1), r=[zf], w=[negf])
        self.G(lambda e: e.tensor_copy(out=self.negm[:, :], in_=negf[:, :]), r=[negf], w=[self.negm])
        self.m01 = s("m01", [128, 256], BF16)
        onef = s("onef2", [128, 256], F32)
        self.G(lambda e: e.memset(onef[:, :], 1.0), w=[onef])
        self.G(lambda e: e.affine_select(out=onef[:, 0:128], in_=onef[:, 0:128], pattern=[[-1, 128]], compare_op=ALU.is_ge, fill=0.0,
                                         base=0, channel_multiplier=1), r=[onef], w=[onef])
        self.G(lambda e: e.affine_select(out=onef[:, 128:256], in_=onef[:, 128:256], pattern=[[1, 128]], compare_op=ALU.is_ge, fill=0.0,
                                         base=0, channel_multiplier=-1), r=[onef], w=[onef])
        self.G(lambda e: e.tensor_copy(out=self.m01[:, :], in_=onef[:, :]), r=[onef], w=[self.m01])
        self.pexp = [s(f"pexp{i}", [128, 256], BF16) for i in range(6)]
        self.osb = [s(f"osb{i}", [128, 520], F32) for i in range(2)]

    def p2_attention(self):
        B = self.bank
        hb = 0
        self._hb = 0
        dils = (1, 4, 16)

        def load_vb(bi):
            d = dils[bi]
            vb_ = self.vbr[bi % 2]
            src = self.vd[:, :].rearrange("(n l r) c -> l n r c", l=128, r=d)
            for n in range(T // (128 * d)):
                self.dma("sp", vb_[:, n * d:(n + 1) * d, :], src[:, n, :, :], r=[self.vd], w=[vb_])
        load_vb(0)
        load_vb(1)
        for bi, d in enumerate(dils):
            seg = 128 * d
            nseg = T // seg
            vb = self.vbr[bi % 2]
            if bi == 1:
                load_vb(2)
            odst = self.obr[bi][:, :].rearrange("(n l r) c -> l n r c", l=128, r=d)
            blk = 0
            for n in range(nseg):
                for r_ in range(d):
                    cols = slice(n * seg + r_, (n + 1) * seg, d)
                    pcols = slice((n - 1) * seg + r_, n * seg, d)
                    bcur = n * d + r_
                    bprev = (n - 1) * d + r_
                    po = [B[6], B[7]]
                    osb = self.osb[blk % 2]
                    def scores(h):
                        nonlocal hb
                        hp, p0 = h // 2, (h % 2) * 64
                        ps = B[hb % 6]
                        o0 = 0
                        pe_ = self.pexp[hb % 6]
                        hb += 1
                        c0 = 0 if n > 0 else 128
                        if n > 0:
                            self.mm(ps[:, o0:o0 + 128], self.KT[p0:p0 + 64, hp, pcols], self.QT[p0:p0 + 64, hp, cols], r=[self.KT, self.QT], w=[ps],
                                    start=True, stop=True, signal=False)
                        self.mm(ps[:, o0 + 128:o0 + 256], self.KT[p0:p0 + 64, hp, cols], self.QT[p0:p0 + 64, hp, cols], r=[self.KT, self.QT], w=[ps],
                                start=True, stop=True)
                        self.S(lambda e, pe_=pe_, ps=ps, c0=c0, o0=o0: e.activation(out=pe_[:, c0:256], in_=ps[:, o0 + c0:o0 + 256], func=AF.Exp, scale=0.125),
                               r=[ps], w=[pe_])
                        mk = self.V if (h % 2 == 0) else self.G
                        mk(lambda e, pe_=pe_, c0=c0: e.tensor_tensor(out=pe_[:, c0:256], in0=pe_[:, c0:256], in1=self.m01[:, c0:256], op=ALU.mult),
                           r=[pe_, self.m01], w=[pe_])
                        return pe_

                    def pv(h, pe_):
                        pob = po[h // 4]
                        oc = slice((h % 4) * 65, (h % 4) * 65 + 65)
                        if n > 0:
                            self.mm(pob[:, oc], pe_[:, 0:128], vb[:, bprev, h * 65:(h + 1) * 65], r=[pe_, vb], w=[pob], start=True, stop=False)
                        self.mm(pob[:, oc], pe_[:, 128:256], vb[:, bcur, h * 65:(h + 1) * 65], r=[pe_, vb], w=[pob], start=(n == 0), stop=True)
                    pend = []
                    for h in range(8):
                        pend.append((h, scores(h)))
                        if len(pend) > 4:
                            pv(*pend.pop(0))
                    while pend:
                        pv(*pend.pop(0))
                    self.V(lambda e, osb=osb, po=po: e.tensor_copy(out=osb[:, 0:260], in_=po[0][:, 0:260]), r=[po[0]], w=[osb])
                    self.V(lambda e, osb=osb, po=po: e.tensor_copy(out=osb[:, 260:520], in_=po[1][:, 0:260]), r=[po[1]], w=[osb])
                    self.dma("sp", odst[:, n, r_, :], osb[:, :], r=[osb], w=[self.obr[bi]])
                    blk += 1

    def s5_io(self):
        i = self.inp
        self.lam_re = i("lam_re", [2, 1024])
        self.lam_im = i("lam_im", [2, 1024])
        self.log_dt = i("log_dt", [2, 16])
        self.ssm_bT = i("ssm_bT", [2, 2, 128, 2, 64])
        self.ssm_cT = i("ssm_cT", [2, 16, 128, 16])
        self.ssm_dT = i("ssm_dT", [2, 128, 2])
        self.glu_w = i("glu_w", [2, 256, 256])
        self.glu_bT = i("glu_bT", [2, 128, 2])
        self.coutT = self.scratch("coutT", [256, T], BF16)
        self.obr = [self.scratch(f"obr{i}", [T, 520], F32) for i in range(3)]

    def s5_alloc(self):
        s = self.sb
        self.Bblk = s("Bblk", [128, 2, 8, 2, 64], BF16)
        self.Cblk = s("Cblk", [128, 16, 128], BF16)
        self.Pre = s("Pre", [128, 16, 64], F32)
        self.PsT = s("PsT", [128, 16, 2, 64], F32)
        self.Qre = s("Qre", [128, 16, 64], F32)
        self.QsT = s("QsT", [128, 16, 2, 64], F32)
        self.glubh = s("glubh", [128, 2], F32)
        self.TriT = s("TriT", [128, 128], BF16)
        self.ones1 = s("ones1", [1, 128], BF16)
        self.dTt = s("dTt", [128, 2], F32)
        self.gluw = s("gluw", [128, 2, 256], BF16)
        self.glub = s("glub", [128, 2], F32)

    def s5_prep(self, l):
        s = self.sb
        V, S, G = self.V, self.S, self.G
        self.push()
        lre = s("lre", [128, 16, 64], F32)
        lim = s("lim", [128, 16, 64], F32)
        ldt = s("ldt", [128, 16], F32)
        self.dma("sp", lre[:, :, :].rearrange("p g n -> p (g n)"), self.lam_re[l].partition_broadcast(128), r=[self.lam_re], w=[lre])
        self.dma("sp", lim[:, :, :].rearrange("p g n -> p (g n)"), self.lam_im[l].partition_broadcast(128), r=[self.lam_im], w=[lim])
        self.dma("sp", ldt[:, :], self.log_dt[l].partition_broadcast(128), r=[self.log_dt], w=[ldt])
        S(lambda e: e.activation(out=ldt[:, :], in_=ldt[:, :], func=AF.Exp), r=[ldt], w=[ldt])
        dtb = ldt[:, :].unsqueeze(2).to_broadcast([128, 16, 64])
        lrd = s("lrd", [128, 16, 64], F32)
        lid = s("lid", [128, 16, 64], F32)
        V(lambda e: e.tensor_tensor(out=lrd[:, :, :], in0=lre[:, :, :], in1=dtb, op=ALU.mult), r=[lre, ldt], w=[lrd])
        V(lambda e: e.tensor_tensor(out=lid[:, :, :], in0=lim[:, :, :], in1=dtb, op=ALU.mult), r=[lim, ldt], w=[lid])
        sp1 = s("sp1", [128, 1], F32)
        G(lambda e: e.iota(sp1[:, :], pattern=[[0, 1]], base=1, channel_multiplier=1, allow_small_or_imprecise_dtypes=True), w=[sp1])
        E = s("E", [128, 16, 64], F32)
        An = s("An", [128, 16, 64], F32)
        sA = s("sA", [128, 16, 64], F32)
        cA = s("cA", [128, 16, 64], F32)
        tmp = s("s5tmp", [128, 16, 64], F32)
        tmi = s("s5tmi", [128, 16, 64], I32)
        qm = s("qm", [128, 16, 64], F32)
        pm = s("pm", [128, 16, 64], F32)
        V(lambda e: e.tensor_scalar(out=E[:, :, :], in0=lrd[:, :, :], scalar1=sp1[:, 0:1], scalar2=None, op0=ALU.mult), r=[lrd, sp1], w=[E])
        V(lambda e: e.tensor_scalar(out=An[:, :, :], in0=lid[:, :, :], scalar1=sp1[:, 0:1], scalar2=None, op0=ALU.mult), r=[lid, sp1], w=[An])
        self.sincos(An, sA, cA, tmp, tmi, None)
        S(lambda e: e.activation(out=qm[:, :, :], in_=E[:, :, :], func=AF.Exp), r=[E], w=[qm])
        S(lambda e: e.activation(out=pm[:, :, :], in_=E[:, :, :], func=AF.Exp, scale=-1.0), r=[E], w=[pm])
        V(lambda e: e.tensor_tensor(out=self.Qre[:, :, :], in0=qm[:, :, :], in1=cA[:, :, :], op=ALU.mult), r=[qm, cA], w=[self.Qre])
        V(lambda e: e.tensor_tensor(out=self.QsT[:, :, 1, :], in0=qm[:, :, :], in1=sA[:, :, :], op=ALU.mult), r=[qm, sA], w=[self.QsT])
        V(lambda e: e.tensor_scalar(out=self.QsT[:, :, 0, :], in0=self.QsT[:, :, 1, :], scalar1=-1.0, scalar2=None, op0=ALU.mult), r=[self.QsT], w=[self.QsT])
        V(lambda e: e.tensor_tensor(out=self.Pre[:, :, :], in0=pm[:, :, :], in1=cA[:, :, :], op=ALU.mult), r=[pm, cA], w=[self.Pre])
        V(lambda e: e.tensor_tensor(out=self.PsT[:, :, 0, :], in0=pm[:, :, :], in1=sA[:, :, :], op=ALU.mult), r=[pm, sA], w=[self.PsT])
        V(lambda e: e.tensor_scalar(out=self.PsT[:, :, 1, :], in0=self.PsT[:, :, 0, :], scalar1=-1.0, scalar2=None, op0=ALU.mult), r=[self.PsT], w=[self.PsT])
        self.sincos(lid, sA, cA, tmp, tmi, None)
        S(lambda e: e.activation(out=qm[:, :, :], in_=lrd[:, :, :], func=AF.Exp), r=[lrd], w=[qm])
        nr, ni = E, An
        V(lambda e: e.tensor_tensor(out=nr[:, :, :], in0=qm[:, :, :], in1=cA[:, :, :], op=ALU.mult), r=[qm, cA], w=[nr])
        V(lambda e: e.tensor_scalar(out=nr[:, :, :], in0=nr[:, :, :], scalar1=-1.0, scalar2=None, op0=ALU.add), r=[nr], w=[nr])
        V(lambda e: e.tensor_tensor(out=ni[:, :, :], in0=qm[:, :, :], in1=sA[:, :, :], op=ALU.mult), r=[qm, sA], w=[ni])
        m2 = pm
        V(lambda e: e.tensor_tensor(out=m2[:, :, :], in0=lre[:, :, :], in1=lre[:, :, :], op=ALU.mult), r=[lre], w=[m2])
        V(lambda e: e.tensor_tensor(out=tmp[:, :, :], in0=lim[:, :, :], in1=lim[:, :, :], op=ALU.mult), r=[lim], w=[tmp])
        V(lambda e: e.tensor_tensor(out=m2[:, :, :], in0=m2[:, :, :], in1=tmp[:, :, :], op=ALU.add), r=[m2, tmp], w=[m2])
        V(lambda e: e.reciprocal(out=m2[:, :, :], in_=m2[:, :, :]), r=[m2], w=[m2])
        fre, fim = sA, cA
        t1 = s("ft1", [128, 16, 64], F32)
        t2 = s("ft2", [128, 16, 64], F32)
        V(lambda e: e.tensor_tensor(out=t1[:, :, :], in0=nr[:, :, :], in1=lre[:, :, :], op=ALU.mult), r=[nr, lre], w=[t1])
        V(lambda e: e.tensor_tensor(out=t2[:, :, :], in0=ni[:, :, :], in1=lim[:, :, :], op=ALU.mult), r=[ni, lim], w=[t2])
        V(lambda e: e.tensor_tensor(out=t1[:, :, :], in0=t1[:, :, :], in1=t2[:, :, :], op=ALU.add), r=[t1, t2], w=[t1])
        V(lambda e: e.tensor_tensor(out=fre[:, :, :], in0=t1[:, :, :], in1=m2[:, :, :], op=ALU.mult), r=[t1, m2], w=[fre])
        V(lambda e: e.tensor_tensor(out=t1[:, :, :], in0=ni[:, :, :], in1=lre[:, :, :], op=ALU.mult), r=[ni, lre], w=[t1])
        V(lambda e: e.tensor_tensor(out=t2[:, :, :], in0=nr[:, :, :], in1=lim[:, :, :], op=ALU.mult), r=[nr, lim], w=[t2])
        V(lambda e: e.tensor_tensor(out=t1[:, :, :], in0=t1[:, :, :], in1=t2[:, :, :], op=ALU.subtract), r=[t1, t2], w=[t1])
        V(lambda e: e.tensor_tensor(out=fim[:, :, :], in0=t1[:, :, :], in1=m2[:, :, :], op=ALU.mult), r=[t1, m2], w=[fim])
        bT = s("bTt", [128, 2, 2, 64], F32)
        self.dma("sp", bT[:, :, :, :], self.ssm_bT[l].rearrange("r p k n -> p r k n"), r=[self.ssm_bT], w=[bT])
        bm = s("bmask", [128, 8], F32)
        one8 = s("one8", [128, 8], F32)
        G(lambda e: e.memset(one8[:, :], 1.0), w=[one8])
        G(lambda e: e.affine_select(out=bm[:, :], in_=one8[:, :], pattern=[[-16, 8]], compare_op=ALU.is_ge, fill=0.0, base=0, channel_multiplier=1),
          r=[one8], w=[bm])
        G(lambda e: e.affine_select(out=bm[:, :], in_=bm[:, :], pattern=[[16, 8]], compare_op=ALU.is_ge, fill=0.0, base=15, channel_multiplier=-1),
          r=[bm], w=[bm])
        bmb = bm[:, :].unsqueeze(2).to_broadcast([128, 8, 64])
        for kc in range(2):
            fr = fre[:, kc * 8:(kc + 1) * 8, :]
            fi = fim[:, kc * 8:(kc + 1) * 8, :]
            bre = bT[:, 0, kc, :].unsqueeze(1).to_broadcast([128, 8, 64])
            bim = bT[:, 1, kc, :].unsqueeze(1).to_broadcast([128, 8, 64])
            a1, a2 = t1[:, 0:8, :], t2[:, 0:8, :]
            V(lambda e, fr=fr, bre=bre: e.tensor_tensor(out=a1, in0=fr, in1=bre, op=ALU.mult), r=[fre, bT], w=[t1])
            V(lambda e, fi=fi, bim=bim: e.tensor_tensor(out=a2, in0=fi, in1=bim, op=ALU.mult), r=[fim, bT], w=[t2])
            V(lambda e: e.tensor_tensor(out=a1, in0=a1, in1=a2, op=ALU.subtract), r=[t1, t2], w=[t1])
            V(lambda e, kc=kc: e.tensor_tensor(out=self.Bblk[:, kc, :, 0, :], in0=a1, in1=bmb, op=ALU.mult), r=[t1, bm], w=[self.Bblk])
            V(lambda e, fr=fr, bim=bim: e.tensor_tensor(out=a1, in0=fr, in1=bim, op=ALU.mult), r=[fre, bT], w=[t1])
            V(lambda e, fi=fi, bre=bre: e.tensor_tensor(out=a2, in0=fi, in1=bre, op=ALU.mult), r=[fim, bT], w=[t2])
            V(lambda e: e.tensor_tensor(out=a1, in0=a1, in1=a2, op=ALU.add), r=[t1, t2], w=[t1])
            V(lambda e, kc=kc: e.tensor_tensor(out=self.Bblk[:, kc, :, 1, :], in0=a1, in1=bmb, op=ALU.mult), r=[t1, bm], w=[self.Bblk])
        cTs = s("cTs", [128, 16, 16], F32)
        self.dma("sp", cTs[:, :, :], self.ssm_cT[l].rearrange("g p c -> p g c"), r=[self.ssm_cT], w=[cTs])
        sg = s("sgn", [128, 1], F32)
        G(lambda e: e.memset(sg[0:64, :], 1.0), w=[sg])
        G(lambda e: e.memset(sg[64:128, :], -1.0), w=[sg])
        G(lambda e: e.memset(self.Cblk[:, :, :], 0.0), w=[self.Cblk])
        for g in range(16):
            c0 = (g % 8) * 16
            S(lambda e, g=g, c0=c0: e.activation(out=self.Cblk[:, g, c0:c0 + 16], in_=cTs[:, g, :], func=AF.Identity, scale=sg[:, 0:1]),
              r=[cTs, sg, self.Cblk], w=[self.Cblk])
        onesb = s("onesb", [128, 128], F32)
        G(lambda e: e.memset(onesb[:, :], 1.0), w=[onesb])
        G(lambda e: e.affine_select(out=onesb[:, :], in_=onesb[:, :], pattern=[[1, 128]], compare_op=ALU.is_ge, fill=0.0, base=0, channel_multiplier=-1),
          r=[onesb], w=[onesb])
        G(lambda e: e.tensor_copy(out=self.TriT[:, :], in_=onesb[:, :]), r=[onesb], w=[self.TriT])
        G(lambda e: e.memset(self.ones1[:, :], 1.0), w=[self.ones1])
        self.dma("sp", self.dTt[:, :], self.ssm_dT[l], r=[self.ssm_dT], w=[self.dTt])
        self.dma("sp", self.glub[:, :], self.glu_bT[l], r=[self.glu_bT], w=[self.glub])
        V(lambda e: e.tensor_scalar(out=self.glubh[:, :], in0=self.glub[:, :], scalar1=0.5, scalar2=None, op0=ALU.mult), r=[self.glub], w=[self.glubh])
        self.dma("pool", self.gluw[:, :, :], self.glu_w[l].rearrange("(k p) n -> p k n", p=128), r=[self.glu_w], w=[self.gluw])
        self.pop()

    def s5_chunk(self, l, j):
        B = self.bank
        V, S, G = self.V, self.S, self.G
        hT = self.hT[j % 2]
        uT = self.uT[j % 2]
        co = self.co[j % 2]
        ps_s = B[7]
        for cc in range(2):
            for k in range(8):
                self.mm(ps_s[:, cc * 128:(cc + 1) * 128], self.win[:, k, 2048 + cc * 128:2048 + (cc + 1) * 128], hT[:, k, :], r=[self.win, hT], w=[ps_s],
                        start=(k == 0), stop=(k == 7), signal=(k == 7 and cc == 1))
        S(lambda e: e.copy(out=uT[:, :, :], in_=ps_s[:, 0:256].rearrange("p (c t) -> p c t", c=2)), r=[ps_s], w=[uT])
        yps = B[7]
        for h in range(2):
            bu = [B[1], B[2]]
            zz = [B[3], B[4]]
            g0 = h * 8
            for q in range(2):
                self.mm(bu[q][:, :], uT[:, h, :], self.Bblk[:, h, q * 4:(q + 1) * 4, :, :].rearrange("p g r n -> p (g r n)"), r=[uT, self.Bblk], w=[bu[q]],
                        start=True, stop=True)
            t1, t2, vv = self.s5t1, self.s5t2, self.s5v
            for q in range(2):
                gs = slice(g0 + q * 4, g0 + q * 4 + 4)
                bu4 = bu[q][:, :].rearrange("p (g r n) -> p g r n", g=4, r=2)
                pc = self.Pre[:, gs, :].unsqueeze(2).to_broadcast([128, 4, 2, 64])
                V(lambda e, q=q, bu4=bu4, pc=pc: e.tensor_tensor(out=t1[:, q * 4:(q + 1) * 4, :, :], in0=bu4, in1=pc, op=ALU.mult), r=[bu[q], self.Pre], w=[t1], x=[t1])
                V(lambda e, q=q, bu4=bu4, gs=gs: e.tensor_tensor(out=t2[:, q * 4:(q + 1) * 4, :, :], in0=bu4[:, :, ::-1, :], in1=self.PsT[:, gs, :, :], op=ALU.mult),
                  r=[bu[q], self.PsT], w=[t2], x=[t2])
            G(lambda e: e.tensor_tensor(out=vv[:, :], in0=t1[:, :, :, :].rearrange("p g r n -> p (g r n)"), in1=t2[:, :, :, :].rearrange("p g r n -> p (g r n)"), op=ALU.add),
              r=[t1, t2], w=[vv])
            for q in range(2):
                self.mm(zz[q][:, :], self.TriT[:, :], vv[:, q * 512:(q + 1) * 512], r=[self.TriT, vv], w=[zz[q]], start=True, stop=False)
                self.mm(zz[q][:, :], self.ones1[:, :], self.x0row[h][:, q * 512:(q + 1) * 512], r=[self.ones1, self.x0row[h]], w=[zz[q]], start=False, stop=True)
            xs = self.s5x[h]
            for q in range(2):
                gs = slice(g0 + q * 4, g0 + q * 4 + 4)
                z4 = zz[q][:, :].rearrange("p (g r n) -> p g r n", g=4, r=2)
                qc = self.Qre[:, gs, :].unsqueeze(2).to_broadcast([128, 4, 2, 64])
                V(lambda e, q=q, z4=z4, qc=qc: e.tensor_tensor(out=t1[:, q * 4:(q + 1) * 4, :, :], in0=z4, in1=qc, op=ALU.mult), r=[zz[q], self.Qre], w=[t1], x=[t1])
                V(lambda e, q=q, z4=z4, gs=gs: e.tensor_tensor(out=t2[:, q * 4:(q + 1) * 4, :, :], in0=z4[:, :, ::-1, :], in1=self.QsT[:, gs, :, :], op=ALU.mult),
                  r=[zz[q], self.QsT], w=[t2], x=[t2])
            G(lambda e, xs=xs: e.tensor_tensor(out=xs[:, :, :].rearrange("p g m -> p (g m)"), in0=t1[:, :, :, :].rearrange("p g r n -> p (g r n)"),
                                        in1=t2[:, :, :, :].rearrange("p g r n -> p (g r n)"), op=ALU.add), r=[t1, t2], w=[xs])
            self.dma("act", self.x0row[h][0:1, :], xs[127:128, :, :].rearrange("p g m -> p (g m)"), r=[xs], w=[self.x0row[h]])
            pxt = B[0]
            pxb = pxt[:, :].bitcast(BF16).rearrange("p (g t) -> p g t", g=8)
            for g in range(8):
                self.tr(pxb[:, g, :], xs[:, g, :], self.identb[:, :], r=[xs, self.identb], w=[pxt], signal=(g == 7))
            V(lambda e, pxb=pxb: e.tensor_copy(out=self.s5xT[:, :, :], in_=pxb), r=[pxt], w=[self.s5xT])
            for g in range(8):
                self.mm(yps[:, 256 + h * 128:256 + (h + 1) * 128], self.Cblk[:, g0 + g, :], self.s5xT[:, g, :], r=[self.Cblk, self.s5xT], w=[yps],
                        start=(g == 0), stop=(g == 7))
        for cc in range(2):
            V(lambda e, cc=cc: e.scalar_tensor_tensor(out=self.yf[:, cc, :], in0=uT[:, cc, :], scalar=self.dTt[:, cc:cc + 1], in1=yps[:, 256 + cc * 128:256 + (cc + 1) * 128],
                                                      op0=ALU.mult, op1=ALU.add), r=[uT, self.dTt, yps], w=[self.yf], x=[self.yf])
        S(lambda e: e.activation(out=self.yg[:, :, :], in_=self.yf[:, :, :], func=AF.Gelu_apprx_tanh), r=[self.yf], w=[self.yg])
        gps = B[5]
        for c2 in range(2):
            for cc in range(2):
                self.mm(gps[:, c2 * 128:(c2 + 1) * 128], self.gluw[:, cc, c2 * 128:(c2 + 1) * 128], self.yg[:, cc, :], r=[self.gluw, self.yg], w=[gps],
                        start=(cc == 0), stop=(cc == 1))
        for c2 in range(2):
            S(lambda e, c2=c2: e.activation(out=self.sgm[:, c2, :], in_=gps[:, c2 * 128:(c2 + 1) * 128], func=AF.Sigmoid, bias=self.glub[:, c2:c2 + 1], scale=1.0),
              r=[gps, self.glub], w=[self.sgm])
        V(lambda e: e.tensor_tensor(out=co[:, :, :], in0=self.yg[:, :, :], in1=self.sgm[:, :, :], op=ALU.mult), r=[self.yg, self.sgm], w=[co])
        self.dma("sp", self.coutT[:, j * 128:(j + 1) * 128].rearrange("(c p) t -> p c t", p=128), co[:, :, :], r=[co], w=[self.coutT])

    def half(self, bank, lo):
        t = Tl(bank.h, bank.b.name + ("lo" if lo else "hi"))
        return t

    def p1v2_alloc(self):
        s = self.sb
        self.xt = [s(f"xt{i}", [128, D], F32) for i in range(2)]
        self.xn = [s(f"xn{i}", [128, D], BF16) for i in range(2)]
        self.st = [s(f"st{i}", [128, 24], F32) for i in range(2)]
        self.st2 = [s(f"stb{i}", [128, 24], F32) for i in range(2)]
        self.hT = [s(f"hT{i}", [128, 8, 128], BF16) for i in range(2)]
        self.gl = [s(f"gl{i}", [128, 512], F32) for i in range(2)]
        self.qbd = [s(f"qbd{i}", [128, 2, 512], BF16) for i in range(2)]
        self.vp = [s(f"vp{i}", [128, 8, 65], BF16) for i in range(2)]
        for i in range(2):
            self.G(lambda e, i=i: e.memset(self.vp[i][:, :, :], 1.0), w=[self.vp[i]])
        self.uT = [s(f"uT{i}", [128, 2, 128], BF16) for i in range(2)]
        self.vnb = s("vnb", [128, 256], F32)
        self.vb = s("vb", [128, 256], BF16)
        self.aout = s("aout", [128, 256], BF16)
        self.qb = s("qb", [128, 2, 512], BF16)
        self.rt = s("rt", [128, 4, 8, 8], F32)
        self.uD = [s(f"uD{i}", [128, 2, 128], F32) for i in range(3)]
        self.p1t = [s(f"p1t{i}", [128, 4, 2, 64], BF16) for i in range(2)]
        self.p2t = [s(f"p2t{i}", [128, 4, 2, 64], BF16) for i in range(2)]
        self.q1 = [s(f"q1_{i}", [128, 4, 2, 64], F32) for i in range(2)]
        self.q2 = [s(f"q2_{i}", [128, 4, 2, 64], F32) for i in range(2)]
        self.qv = [s(f"qv{i}", [128, 512], BF16) for i in range(2)]
        self.qx = [s(f"qx{i}", [128, 4, 128], BF16) for i in range(2)]
        self.qxT = [s(f"qxT{i}", [128, 4, 128], BF16) for i in range(3)]
        self.x0q = [s(f"x0q{i}", [1, 512], BF16) for i in range(4)]
        for i in range(4):
            self.G(lambda e, i=i: e.memset(self.x0q[i][:, :], 0.0), w=[self.x0q[i]])
        self.yf = s("yf2", [128, 2, 128], F32)
        self.yg = s("yg2", [128, 2, 128], BF16)
        self.sgm = s("sgm2", [128, 2, 128], F32)
        self.co = [s(f"co2_{i}", [128, 2, 128], BF16) for i in range(2)]
        B = self.bank
        self.b5lo, self.b5hi = Tl(B[5].h, "b5lo"), Tl(B[5].h, "b5hi")
        self.b6lo, self.b6hi = Tl(B[6].h, "b6lo"), Tl(B[6].h, "b6hi")
        self.b7lo, self.b7hi = Tl(B[7].h, "b7lo"), Tl(B[7].h, "b7hi")

    def p1_front_ln(self, l, j, x_src):
        if j >= NCH:
            return
        i2 = j % 2
        xt, xn, st = self.xt[i2], self.xn[i2], self.st[i2]
        S = self.S
        if j == 0:
            self.dma("sp", xt[:, :], x_src[0:128, :], r=[x_src], w=[xt])
            xt1 = self.xt[1]
            self.dma("sp", xt1[:, :], x_src[128:256, :], r=[x_src], w=[xt1])
        self.ln_stats(xt, lambda a, b: xt[:, a:b], D, st)
        S(lambda e: e.activation(out=xn[:, :], in_=xt[:, :], func=AF.Identity, bias=st[:, 4:5], scale=st[:, 3:4]), r=[xt, st], w=[xn])
        if j + 2 < NCH:
            self.dma("sp", xt[:, :], x_src[(j + 2) * 128:(j + 3) * 128, :], r=[x_src], w=[xt])

    def p1_front_a(self, l, j, x_src):
        if j >= NCH:
            return
        i2 = j % 2
        xn, hT = self.xn[i2], self.hT[i2]
        B = self.bank
        S = self.S
        pT = B[0]
        pTb = pT[:, :].bitcast(BF16).rearrange("p (k t) -> p k t", k=8)
        for k in range(8):
            self.tr(pTb[:, k, :], xn[:, k * 128:(k + 1) * 128], self.identb[:, :], r=[xn, self.identb], w=[pT], signal=(k == 7))
        for k in range(8):
            S(lambda e, k=k: e.activation(out=hT[:, k, :], in_=pTb[:, k, :], func=AF.Identity, scale=self.modp[:, 1, k:k + 1], bias=self.modp[:, 0, k:k + 1]),
              r=[pT, self.modp], w=[hT], x=[hT])

    def p1_front_s(self, l, j):
        if j >= NCH:
            return
        i2 = j % 2
        hT, uT = self.hT[i2], self.uT[i2]
        S = self.S
        ps_s = self.b6lo
        for cc in range(2):
            for k in range(8):
                self.mm(ps_s[:, cc * 128:(cc + 1) * 128], self.win[:, k, 2048 + cc * 128:2048 + (cc + 1) * 128], hT[:, k, :], r=[self.win, hT], w=[ps_s],
                        start=(k == 0), stop=(k == 7), signal=(k == 7 and cc == 1))
        S(lambda e: e.copy(out=uT[:, :, :], in_=ps_s[:, 0:256].rearrange("p (c t) -> p c t", c=2)), r=[ps_s], w=[uT])
        uD = self.uD[j % 3]
        for cc in range(2):
            S(lambda e, cc=cc: e.activation(out=uD[:, cc, :], in_=ps_s[:, cc * 128:(cc + 1) * 128], func=AF.Identity, scale=self.dTt[:, cc:cc + 1]), r=[ps_s, self.dTt], w=[uD], x=[uD])

    def p1_front_p(self, l, j, which):
        if j >= NCH:
            return
        i2 = j % 2
        hT, gl, qf, vp = self.hT[i2], self.gl[i2], self.qbd[i2], self.vp[i2]
        B = self.bank
        S = self.S

        def proj(bank, c0):
            for k in range(8):
                self.mm(bank[:, :], hT[:, k, :], self.win[:, k, c0:c0 + 512], r=[hT, self.win], w=[bank], start=(k == 0), stop=(k == 7))
        if which == 0:
            proj(B[1], 0)
            S(lambda e: e.activation(out=gl[:, :], in_=B[1][:, :], func=AF.Gelu_apprx_tanh), r=[B[1]], w=[gl])
        elif which == 1:
            proj(B[2], 512)
            S(lambda e: e.copy(out=qf[:, 0, :], in_=B[2][:, :]), r=[B[2]], w=[qf])
        elif which == 2:
            proj(B[1], 1024)
            S(lambda e: e.copy(out=qf[:, 1, :], in_=B[1][:, :]), r=[B[1]], w=[qf], x=[qf])
        else:
            proj(B[2], 1536)
            S(lambda e: e.copy(out=vp[:, :, 0:64], in_=B[2][:, :].rearrange("p (h e) -> p h e", e=64)), r=[B[2]], w=[vp])

    def p1_front(self, l, j, x_src):
        self.p1_front_a(l, j, x_src)
        self.p1_front_s(l, j)
        for w_ in range(4):
            self.p1_front_p(l, j, w_)

    def p1_back(self, l, j):
        if j < 0:
            return
        i2 = j % 2
        st2 = self.st2[i2]
        gl, qf, vp, uT = self.gl[i2], self.qbd[i2], self.vp[i2], self.uT[i2]
        vnb, vb, aout, qb, rt = self.vnb, self.vb, self.aout, self.qbd[i2], self.rt
        B = self.bank
        V, S, G = self.V, self.S, self.G
        rows = slice(j * 128, (j + 1) * 128)
        self.ln_stats(gl, lambda a, b: gl[:, 256 + a:256 + b], 256, st2)
        V(lambda e: e.tensor_scalar(out=vnb[:, :], in0=gl[:, 256:512], scalar1=st2[:, 3:4], scalar2=st2[:, 4:5], op0=ALU.mult, op1=ALU.add),
          r=[gl, st2], w=[vnb])
        V(lambda e: e.tensor_tensor(out=vnb[:, :], in0=vnb[:, :], in1=self.glng[:, :], op=ALU.mult), r=[vnb, self.glng], w=[vnb], x=[vnb])
        V(lambda e: e.tensor_tensor(out=vb[:, :], in0=vnb[:, :], in1=self.glnb[:, :], op=ALU.add), r=[vnb, self.glnb], w=[vb], x=[vnb])
        psv = self.b5lo
        for h in range(4):
            self.mm(psv[:, h * 64:(h + 1) * 64], self.wsT[:, h, :], vb[:, h * 64:(h + 1) * 64], r=[self.wsT, vb], w=[psv], start=True, stop=True,
                    signal=(h == 3))
        for h in range(4):
            V(lambda e, h=h: e.scalar_tensor_tensor(out=aout[:, h * 64:(h + 1) * 64], in0=psv[:, h * 64:(h + 1) * 64], scalar=self.gbs[:, h:h + 1],
                                                    in1=gl[:, h * 64:(h + 1) * 64], op0=ALU.add, op1=ALU.mult), r=[psv, self.gbs, gl], w=[aout], x=[aout])
        self.dma("sp", self.mixtok[rows, 0:256], aout[:, :], r=[aout], w=[self.mixtok])
        q4 = qf[:, :, :].rearrange("p a (h e) -> p (a h) e", e=64)
        o4 = qb[:, :, :].rearrange("p a (h e) -> p (a h) e", e=64)
        for a in range(2):
            xa1, xa2 = q4[:, a * 8:(a + 1) * 8, 0:8], q4[:, a * 8:(a + 1) * 8, 8:16]
            cb = self.cs[:, j, :].unsqueeze(1).to_broadcast([128, 8, 8])
            sb_ = self.sn[:, j, :].unsqueeze(1).to_broadcast([128, 8, 8])
            oa = o4[:, a * 8:(a + 1) * 8, :]
            G(lambda e, xa1=xa1, cb=cb: e.tensor_tensor(out=rt[:, 0, :, :], in0=xa1, in1=cb, op=ALU.mult), r=[qf, self.cs], w=[rt])
            G(lambda e, xa2=xa2, sb_=sb_: e.tensor_tensor(out=rt[:, 1, :, :], in0=xa2, in1=sb_, op=ALU.mult), r=[qf, self.sn], w=[rt])
            G(lambda e, xa2=xa2, cb=cb: e.tensor_tensor(out=rt[:, 2, :, :], in0=xa2, in1=cb, op=ALU.mult), r=[qf, self.cs], w=[rt])
            G(lambda e, xa1=xa1, sb_=sb_: e.tensor_tensor(out=rt[:, 3, :, :], in0=xa1, in1=sb_, op=ALU.mult), r=[qf, self.sn], w=[rt])
            G(lambda e, oa=oa: e.tensor_tensor(out=oa[:, :, 0:8], in0=rt[:, 0, :, :], in1=rt[:, 1, :, :], op=ALU.subtract), r=[rt], w=[qb])
            G(lambda e, oa=oa: e.tensor_tensor(out=oa[:, :, 8:16], in0=rt[:, 2, :, :], in1=rt[:, 3, :, :], op=ALU.add), r=[rt], w=[qb])
        pq = B[7]
        pqb = pq[:, :].bitcast(BF16).rearrange("p (a k t) -> p a k t", a=2, k=4)
        for a in range(2):
            for k in range(4):
                self.tr(pqb[:, a, k, :], qb[:, a, k * 128:(k + 1) * 128], self.identb[:, :], r=[qb, self.identb], w=[pq], signal=(a == 1 and k == 3))
        S(lambda e: e.copy(out=self.QT[:, :, j * 128:(j + 1) * 128], in_=pqb[:, 0, :, :]), r=[pq], w=[self.QT])
        S(lambda e: e.copy(out=self.KT[:, :, j * 128:(j + 1) * 128], in_=pqb[:, 1, :, :]), r=[pq], w=[self.KT])
        self.dma("sp", self.vd[rows, :], vp[:, :, :].rearrange("p h e -> p (h e)"), r=[vp], w=[self.vd])

    def s5A(self, i):
        if i < 0 or i >= 4 * NCH:
            return
        j, q = divmod(i, 4)
        kc = q // 2
        gs = slice(q * 4, q * 4 + 4)
        uT = self.uT[j % 2]
        bu = self.bank[3]
        t1, t2 = self.p1t[i % 2], self.p2t[i % 2]
        self.mm(bu[:, :], uT[:, kc, :], self.Bblk[:, kc, (q % 2) * 4:(q % 2) * 4 + 4, :, :].rearrange("p g r n -> p (g r n)"), r=[uT, self.Bblk], w=[bu],
                start=True, stop=True)
        bu4 = bu[:, :].rearrange("p (g r n) -> p g r n", g=4, r=2)
        pc = self.Pre[:, gs, :].unsqueeze(2).to_broadcast([128, 4, 2, 64])
        self.V(lambda e: e.tensor_tensor(out=t1[:, :, :, :], in0=bu4, in1=pc, op=ALU.mult), r=[bu, self.Pre], w=[t1])
        self.V(lambda e: e.tensor_tensor(out=t2[:, :, :, :], in0=bu4[:, :, ::-1, :], in1=self.PsT[:, gs, :, :], op=ALU.mult), r=[bu, self.PsT], w=[t2])

    def s5B(self, i):
        if i < 0 or i >= 4 * NCH:
            return
        j, q = divmod(i, 4)
        gs = slice(q * 4, q * 4 + 4)
        zz = self.bank[4]
        t1, t2 = self.p1t[i % 2], self.p2t[i % 2]
        u1, u2, xs = self.q1[i % 2], self.q2[i % 2], self.qx[i % 2]
        f = lambda t: t[:, :, :, :].rearrange("p g r n -> p (g r n)")
        self.mm(zz[:, :], self.TriT[:, :], f(t1), r=[self.TriT, t1], w=[zz], start=True, stop=False)
        self.mm(zz[:, :], self.TriT[:, :], f(t2), r=[self.TriT, t2], w=[zz], start=False, stop=False)
        self.mm(zz[:, :], self.ones1[:, :], self.x0q[q][:, :], r=[self.ones1, self.x0q[q]], w=[zz], start=False, stop=True)
        z4 = zz[:, :].rearrange("p (g r n) -> p g r n", g=4, r=2)
        qc = self.Qre[:, gs, :].unsqueeze(2).to_broadcast([128, 4, 2, 64])
        self.V(lambda e: e.tensor_tensor(out=u1[:, :, :, :], in0=z4, in1=qc, op=ALU.mult), r=[zz, self.Qre], w=[u1])
        self.V(lambda e: e.tensor_tensor(out=u2[:, :, :, :], in0=z4[:, :, ::-1, :], in1=self.QsT[:, gs, :, :], op=ALU.mult), r=[zz, self.QsT], w=[u2])
        self.G(lambda e: e.tensor_tensor(out=xs[:, :, :].rearrange("p g m -> p (g m)"), in0=f(u1), in1=f(u2), op=ALU.add), r=[u1, u2], w=[xs])
        self.dma("pool", self.x0q[q][0:1, :], xs[127:128, :, :].rearrange("p g m -> p (g m)"), r=[xs], w=[self.x0q[q]])

    def s5C(self, i):
        if i < 0 or i >= 4 * NCH:
            return
        xs, xT = self.qx[i % 2], self.qxT[i % 3]
        pxt = self.b6hi
        pxb = pxt[:, 256:512].bitcast(BF16).rearrange("p (g t) -> p g t", g=4)
        for g in range(4):
            self.tr(pxb[:, g, :], xs[:, g, :], self.identb[:, :], r=[xs, self.identb], w=[pxt], signal=(g == 3))
        self.S(lambda e: e.copy(out=xT[:, :, :], in_=pxb), r=[pxt], w=[xT])

    def s5D(self, i):
        if i < 0 or i >= 4 * NCH:
            return
        j, q = divmod(i, 4)
        kc = q // 2
        xT = self.qxT[i % 3]
        yps = self.b5hi
        for g in range(4):
            self.mm(yps[:, 256 + kc * 128:256 + (kc + 1) * 128], self.Cblk[:, q * 4 + g, :], xT[:, g, :], r=[self.Cblk, xT], w=[yps],
                    start=(q % 2 == 0 and g == 0), stop=(q % 2 == 1 and g == 3))

    def s5_step(self, i):
        self.s5A(i + 2)
        self.s5B(i + 1)
        self.s5C(i)
        if i % 2 == 0:
            self.s5D(i - 2)
            self.s5D(i - 1)

    def s5_tail(self, j):
        if j < 0 or j >= NCH:
            return
        V, S, G = self.V, self.S, self.G
        i2 = j % 2
        uT = self.uT[i2]
        yps = self.b5hi
        co = self.co[i2]
        for cc in range(2):
            V(lambda e, cc=cc: e.scalar_tensor_tensor(out=self.yf[:, cc, :], in0=self.uD[j % 3][:, cc, :], scalar=1.0, in1=yps[:, 256 + cc * 128:256 + (cc + 1) * 128],
                                                      op0=ALU.mult, op1=ALU.add), r=[self.uD[j % 3], yps], w=[self.yf], x=[self.yf])
        S(lambda e: e.activation(out=self.yg[:, :, :], in_=self.yf[:, :, :], func=AF.Gelu_apprx_tanh), r=[self.yf], w=[self.yg])
        gps = self.b5lo
        for c2 in range(2):
            for cc in range(2):
                self.mm(gps[:, c2 * 128:(c2 + 1) * 128], self.gluw[:, cc, c2 * 128:(c2 + 1) * 128], self.yg[:, cc, :], r=[self.gluw, self.yg], w=[gps],
                        start=(cc == 0), stop=(cc == 1))
        for c2 in range(2):
            S(lambda e, c2=c2: e.activation(out=self.sgm[:, c2, :], in_=gps[:, c2 * 128:(c2 + 1) * 128], func=AF.Tanh, bias=self.glubh[:, c2:c2 + 1], scale=0.5),
              r=[gps, self.glubh], w=[self.sgm])
        V(lambda e: e.scalar_tensor_tensor(out=self.sgm[:, :, :], in0=self.sgm[:, :, :], scalar=1.0, in1=self.yg[:, :, :], op0=ALU.add, op1=ALU.mult),
          r=[self.sgm, self.yg], w=[self.sgm])
        V(lambda e: e.tensor_scalar(out=co[:, :, :], in0=self.sgm[:, :, :], scalar1=0.5, scalar2=None, op0=ALU.mult), r=[self.sgm], w=[co])
        self.dma("sp", self.coutT[:, j * 128:(j + 1) * 128].rearrange("(c p) t -> p c t", p=128), co[:, :, :], r=[co], w=[self.coutT])

    def p1_all(self, l, x_src):
        self.p1_front_ln(l, 0, x_src)
        self.p1_front_ln(l, 1, x_src)
        self.p1_front(l, 0, x_src)
        self.s5A(0); self.s5A(1); self.s5B(0)
        for j in range(NCH):
            self.p1_front_ln(l, j + 2, x_src)
            self.p1_front(l, j + 1, x_src)
            self.p1_back(l, j)
            for q in range(4):
                self.s5_step(4 * j + q)
                if q == 0:
                    self.s5_tail(j - 1)
        self.s5_step(4 * NCH)
        self.s5_tail(NCH - 1)
        assert (4 * NCH) % 2 == 0

    CAP = 896
    NSLOT = 16 * 896 + 128
    GROUPS = ((0, 4), (512, 3))

    def p3_io(self):
        i = self.inp
        self.w_out = i("w_out", [2, D, D])
        self.ln1_g = i("ln1_g", [2, D]); self.ln1_b = i("ln1_b", [2, D])
        self.ln2_g = i("ln2_g", [2, D]); self.ln2_b = i("ln2_b", [2, D])
        self.router_w = i("router_w", [D, 16])
        self.router_bias = i("router_bias", [16])
        self.x1d = self.scratch("x1d", [T, D], F32)
        self.xmid = self.scratch("xmid", [T, D], F32)
        self.h2slots = self.scratch("h2slots", [self.NSLOT, D], BF16)
        self.oslots = self.scratch("oslots", [self.NSLOT, D], F32)

    def route_alloc(self):
        s = self.sb
        self.slotA = s("slotA", [128, NCH], I32)
        self.slotB = s("slotB", [128, NCH], I32)
        self.gAB = s("gAB", [128, 2, NCH], F32)

    def p3_alloc(self, l):
        s = self.sb
        self.wout = s("wout", [128, 8, D], BF16)
        for k in range(8):
            self.dma("pool", self.wout[:, k, :], self.w_out[l, k * 128:(k + 1) * 128, :], r=[self.w_out], w=[self.wout])
        self.lng = s("lng", [128, D], F32); self.lnb = s("lnb", [128, D], F32)
        self.dma("sp", self.lng[:, :], self.ln1_g[l].partition_broadcast(128), r=[self.ln1_g], w=[self.lng])
        self.dma("sp", self.lnb[:, :], self.ln1_b[l].partition_broadcast(128), r=[self.ln1_b], w=[self.lnb])
        self.rw = s("rw", [128, 8, 16], F32)
        self.dma("sp", self.rw[:, :, :], self.router_w[:, :].rearrange("(k p) e -> p k e", p=128), r=[self.router_w], w=[self.rw])
        self.rbias = s("rbias", [128, 16], F32)
        self.dma("sp", self.rbias[:, :], self.router_bias[:].partition_broadcast(128), r=[self.router_bias], w=[self.rbias])
        self.o3 = [s(f"o3_{i}", [128, 3, 520], F32) for i in range(2)]
        self.rec = s("rec", [128, 8], F32)
        self.mt = [s(f"mt{i}", [128, D], BF16) for i in range(2)]
        self.mixT = [s(f"mixT{i}", [128, 8, 128], BF16) for i in range(2)]
        self.xres = [s(f"xres{i}", [128, D], F32) for i in range(2)]
        self.yy = s("yy", [128, D], F32)
        self.x1t = [s(f"x1t{i}", [128, D], F32) for i in range(2)]
        self.cT = [s(f"cT{i}", [128, 2, 128], BF16) for i in range(2)]
        self.st3b = s("st3b", [128, 24], F32)
        self.h2f = s("h2f", [128, D], F32)
        self.h2all = s("h2all", [128, NCH, D], BF16)
        self.h2c = [Tl(self.h2all.h, f"h2c{j}") for j in range(NCH)]
        self.scall = s("scall", [128, NCH, 16], F32)
        self.h2T = s("h2T", [128, 8, 128], F32)
        self.st3 = s("st3", [128, 24], F32)
        self.trs = s("trs", [128, 128], F32)
        self.eoff = s("eoff", [128, 16], F32)
        self.trashp = s("trashp", [128, 1], F32)
        self.ones16 = s("ones16", [128, 16], F32)
        G = self.G
        G(lambda e: e.memset(self.ones16[:, :], 1.0), w=[self.ones16])
        G(lambda e: e.affine_select(out=self.trs[:, :], in_=self.onesf[:, :], pattern=[[1, 128]], compare_op=ALU.is_ge, fill=0.0, base=-1, channel_multiplier=-1),
          r=[self.onesf], w=[self.trs])
        G(lambda e: e.iota(self.eoff[:, :], pattern=[[self.CAP, 16]], base=0, channel_multiplier=0, allow_small_or_imprecise_dtypes=True), w=[self.eoff])
        G(lambda e: e.iota(self.trashp[:, :], pattern=[[0, 1]], base=16 * self.CAP, channel_multiplier=1, allow_small_or_imprecise_dtypes=True), w=[self.trashp])

    def p3_L1(self, k):
        if k >= NCH:
            return
        rws = slice(k * 128, (k + 1) * 128)
        o3_, mt_ = self.o3[k % 2], self.mt[k % 2]
        for bi in range(3):
            self.dma("sp", o3_[:, bi, :], self.obr[bi][rws, :], r=[self.obr[bi]], w=[o3_])
        self.dma("sp", mt_[:, 0:256], self.mixtok[rws, 0:256], r=[self.mixtok], w=[mt_])

    def p3_L2(self, k, x_src):
        if k >= NCH:
            return
        rws = slice(k * 128, (k + 1) * 128)
        self.dma("sp", self.cT[k % 2][:, :, :], self.coutT[:, rws].rearrange("(c p) t -> p c t", p=128), r=[self.coutT], w=[self.cT[k % 2]])
        self.dma("sp", self.xres[k % 2][:, :], x_src[rws, :], r=[x_src], w=[self.xres[k % 2]])

    def p3_S1(self, j):
        if j >= NCH or j < 0:
            return
        B = self.bank
        V, S, G = self.V, self.S, self.G
        o3, rec, mt = self.o3[j % 2], self.rec, self.mt[j % 2]
        mixT = self.mixT[j % 2]
        V(lambda e: e.tensor_tensor(out=o3[:, 0, :], in0=o3[:, 0, :], in1=o3[:, 1, :], op=ALU.add), r=[o3], w=[o3], x=[o3])
        V(lambda e: e.tensor_tensor(out=o3[:, 0, :], in0=o3[:, 0, :], in1=o3[:, 2, :], op=ALU.add), r=[o3], w=[o3], x=[o3])
        o8 = o3[:, 0, :].rearrange("p (h e) -> p h e", e=65)
        V(lambda e: e.reciprocal(out=rec[:, :], in_=o8[:, :, 64]), r=[o3], w=[rec])
        V(lambda e: e.tensor_tensor(out=mt[:, 256:768].rearrange("p (h e) -> p h e", e=64), in0=o8[:, :, 0:64], in1=rec[:, :].unsqueeze(2).to_broadcast([128, 8, 64]),
                                    op=ALU.mult), r=[o3, rec], w=[mt])
        pT = B[0]
        pTb = pT[:, :].bitcast(BF16).rearrange("p (k t) -> p k t", k=8)
        for k in range(6):
            self.tr(pTb[:, k, :], mt[:, k * 128:(k + 1) * 128], self.identb[:, :], r=[mt, self.identb], w=[pT], signal=(k == 5))
        S(lambda e: e.copy(out=mixT[:, 0:6, :], in_=pTb[:, 0:6, :]), r=[pT], w=[mixT])

    def p3_S2(self, j):
        if j >= NCH or j < 0:
            return
        B = self.bank
        V, S, G = self.V, self.S, self.G
        rows = slice(j * 128, (j + 1) * 128)
        mixT, cT, xres = self.mixT[j % 2], self.cT[j % 2], self.xres[j % 2]
        wb = [B[1], B[2]] if j % 2 == 0 else [B[6], B[7]]
        for nb in range(2):
            for k in range(8):
                lhs = mixT[:, k, :] if k < 6 else cT[:, k - 6, :]
                self.mm(wb[nb][:, :], lhs, self.wout[:, k, nb * 512:(nb + 1) * 512], r=[mixT, cT, self.wout], w=[wb[nb]], start=(k == 0), stop=(k == 7))
        yy = self.yy
        for nb in range(2):
            cs_ = slice(nb * 512, (nb + 1) * 512)
            V(lambda e, nb=nb, cs_=cs_: e.tensor_tensor(out=yy[:, cs_], in0=wb[nb][:, :], in1=self.opg[:, 0, cs_], op=ALU.mult), r=[wb[nb], self.opg], w=[yy], x=[yy])
        V(lambda e: e.scalar_tensor_tensor(out=yy[:, :], in0=xres[:, :], scalar=float(ALPHA), in1=yy[:, :], op0=ALU.mult, op1=ALU.add), r=[xres, yy], w=[yy], x=[yy])
        self.ln_stats(yy, lambda a, b: yy[:, a:b], D, self.st3, act=True)
        x1 = self.x1t[j % 2]
        S(lambda e: e.activation(out=x1[:, :], in_=yy[:, :], func=AF.Identity, bias=self.st3[:, 4:5], scale=self.st3[:, 3:4]), r=[yy, self.st3], w=[x1])
        V(lambda e: e.tensor_tensor(out=x1[:, :], in0=x1[:, :], in1=self.lng[:, :], op=ALU.mult), r=[x1, self.lng], w=[x1])
        V(lambda e: e.tensor_tensor(out=x1[:, :], in0=x1[:, :], in1=self.lnb[:, :], op=ALU.add), r=[x1, self.lnb], w=[x1], x=[x1])
        self.dma("sp", self.x1d[rows, :], x1[:, :], r=[x1], w=[self.x1d])

    def p3_S3(self, j):
        if j >= NCH or j < 0:
            return
        B = self.bank
        V, S, G = self.V, self.S, self.G
        x1 = self.x1t[j % 2]
        self.ln_stats(x1, lambda a, b: x1[:, a:b], D, self.st3b, act=True)
        h2f = self.h2f
        S(lambda e: e.activation(out=h2f[:, :], in_=x1[:, :], func=AF.Identity, bias=self.st3b[:, 4:5], scale=self.st3b[:, 3:4]), r=[x1, self.st3b], w=[h2f])
        V(lambda e: e.tensor_tensor(out=h2f[:, :], in0=h2f[:, :], in1=self.opg2[:, 1, :], op=ALU.mult), r=[h2f, self.opg2], w=[h2f], x=[h2f])
        V(lambda e: e.tensor_tensor(out=h2f[:, :], in0=h2f[:, :], in1=self.opg2[:, 0, :], op=ALU.add), r=[h2f, self.opg2], w=[h2f], x=[h2f])
        S(lambda e: e.copy(out=self.h2all[:, j, :], in_=h2f[:, :]), r=[h2f], w=[self.h2c[j]])
        for half in range(2):
            pt = B[3 + half]
            for k in range(4):
                self.tr(pt[:, k * 128:(k + 1) * 128], h2f[:, (half * 4 + k) * 128:(half * 4 + k + 1) * 128], self.identf[:, :], r=[h2f, self.identf], w=[pt], signal=(k == 3))
            S(lambda e, half=half, pt=pt: e.copy(out=self.h2T[:, half * 4:(half + 1) * 4, :], in_=pt[:, :].rearrange("p (k t) -> p k t", k=4)), r=[pt], w=[self.h2T])
        lg = B[5]
        for k in range(8):
            self.mm(lg[:, 0:16], self.h2T[:, k, :], self.rw[:, k, :], r=[self.h2T, self.rw], w=[lg], start=(k == 0), stop=(k == 7))
        S(lambda e: e.copy(out=self.scall[:, j, :], in_=lg[:, 0:16]), r=[lg], w=[self.scall])

    def p3_all(self, l, x_src):
        self.p3_L1(0)
        for j in range(-2, NCH):
            self.p3_L1(j + 3)
            self.p3_L2(j + 2, x_src)
            self.p3_S1(j + 2)
            self.p3_S2(j + 1)
            self.p3_S3(j)
            if j >= 0 and (j + 1) % self.RSEG == 0:
                self.p3_route(j + 1 - self.RSEG)

    RSEG = 8

    def p3_route_alloc(self):
        s = self.sb
        NJ = self.RSEG
        mk = lambda n: s(n, [128, NJ, 16], F32)
        t = {}
        t["big"] = [mk(f"r_{i}") for i in range(14)]
        t["g4"] = [s(f"r4_{i}", [128, NJ, 4], F32) for i in range(4)]
        t["g1"] = [s(f"r1_{i}", [128, NJ], F32) for i in range(3)]
        t["mask_e0"] = mk("mask_e0")
        t["mask_j0"] = s("mask_j0", [128, 16, NJ], F32)
        G = self.G
        G(lambda e: e.memset(t["mask_e0"][:, :, :], 1.0), w=[t["mask_e0"]])
        G(lambda e: e.memset(t["mask_e0"][:, :, 0:1], 0.0), w=[t["mask_e0"]])
        G(lambda e: e.memset(t["mask_j0"][:, :, :], 1.0), w=[t["mask_j0"]])
        G(lambda e: e.memset(t["mask_j0"][:, :, 0:1], 0.0), w=[t["mask_j0"]])
        self.carry = s("carry", [128, 16], F32)
        G(lambda e: e.memset(self.carry[:, :], 0.0), w=[self.carry])
        self._rt = t

    def p3_route(self, j0):
        B = self.bank
        V, S, G = self.V, self.S, self.G
        NJ = self.RSEG
        t = self._rt
        sel, eq, msk, top2, chosen, gw, tmp, cum, pos, valid, slotv, baseT, totT, sc = t["big"]
        m1, m2, gs, gsel = t["g4"]
        gmax, gsum, t32 = t["g1"]
        mask_e0, mask_j0 = t["mask_e0"], t["mask_j0"]
        f2 = lambda t_: t_[:, :, :].rearrange("p j e -> p (j e)")
        S(lambda e: e.activation(out=f2(sc), in_=self.scall[:, j0:j0 + NJ, :].rearrange("p j e -> p (j e)"), func=AF.Sigmoid), r=[self.scall], w=[sc])
        g4 = lambda t: t[:, :, :].rearrange("p j (g i) -> p (j g) i", i=4)
        b4 = lambda t: t[:, :, :].rearrange("p j g -> p (j g)").unsqueeze(2).to_broadcast([128, NJ * 4, 4])
        V(lambda e: e.tensor_tensor(out=sel[:, :, :], in0=sc[:, :, :], in1=self.rbias[:, :].unsqueeze(1).to_broadcast([128, NJ, 16]), op=ALU.add), r=[sc, self.rbias], w=[sel])
        V(lambda e: e.tensor_reduce(out=m1[:, :, :].rearrange("p j g -> p (j g)"), in_=g4(sel), axis=AX.X, op=ALU.max), r=[sel], w=[m1])
        V(lambda e: e.tensor_tensor(out=g4(eq), in0=g4(sel), in1=b4(m1), op=ALU.is_equal), r=[sel, m1], w=[eq])
        V(lambda e: e.scalar_tensor_tensor(out=f2(msk), in0=f2(eq), scalar=-1e9, in1=f2(sel), op0=ALU.mult, op1=ALU.add), r=[eq, sel], w=[msk])
        V(lambda e: e.tensor_reduce(out=m2[:, :, :].rearrange("p j g -> p (j g)"), in_=g4(msk), axis=AX.X, op=ALU.max), r=[msk], w=[m2])
        V(lambda e: e.tensor_tensor(out=gs[:, :, :], in0=m1[:, :, :], in1=m2[:, :, :], op=ALU.add), r=[m1, m2], w=[gs])
        V(lambda e: e.tensor_reduce(out=gmax[:, :], in_=gs[:, :, :], axis=AX.X, op=ALU.max), r=[gs], w=[gmax])
        V(lambda e: e.tensor_tensor(out=gsel[:, :, :], in0=gs[:, :, :], in1=gmax[:, :].unsqueeze(2).to_broadcast([128, NJ, 4]), op=ALU.is_equal), r=[gs, gmax], w=[gsel])
        V(lambda e: e.tensor_tensor(out=g4(top2), in0=g4(sel), in1=b4(m2), op=ALU.is_ge), r=[sel, m2], w=[top2])
        V(lambda e: e.tensor_tensor(out=g4(chosen), in0=g4(top2), in1=b4(gsel), op=ALU.mult), r=[top2, gsel], w=[chosen])
        V(lambda e: e.tensor_tensor(out=gw[:, :, :], in0=chosen[:, :, :], in1=sc[:, :, :], op=ALU.mult), r=[chosen, sc], w=[gw])
        V(lambda e: e.tensor_reduce(out=gsum[:, :], in_=gw[:, :, :], axis=AX.X, op=ALU.add), r=[gw], w=[gsum])
        V(lambda e: e.reciprocal(out=gsum[:, :], in_=gsum[:, :]), r=[gsum], w=[gsum])
        V(lambda e: e.tensor_tensor(out=gw[:, :, :], in0=gw[:, :, :], in1=gsum[:, :].unsqueeze(2).to_broadcast([128, NJ, 16]), op=ALU.mult), r=[gw, gsum], w=[gw])
        cbk = B[5]
        W = NJ * 16
        self.mm(cbk[:, 128:128 + W], self.trs[:, :], f2(chosen), r=[self.trs, chosen], w=[cbk], start=True, stop=True)
        self.mm(cbk[:, 256:256 + W], self.onesf[:, :], f2(chosen), r=[self.onesf, chosen], w=[cbk], start=True, stop=True)
        V(lambda e: e.tensor_copy(out=f2(totT).rearrange("p (e j) -> p e j", e=16),
                                  in_=cbk[:, 256:256 + W].rearrange("p (j e) -> p e j", e=16)), r=[cbk], w=[totT])
        V(lambda e: e.tensor_tensor_scan(out=f2(baseT), data0=mask_j0[:, :, :].rearrange("p e j -> p (e j)"), data1=f2(totT), initial=0.0, op0=ALU.mult, op1=ALU.add),
          r=[mask_j0, totT], w=[baseT])
        V(lambda e: e.tensor_tensor(out=f2(baseT), in0=f2(baseT), in1=f2(totT), op=ALU.subtract), r=[baseT, totT], w=[baseT])
        bT3 = f2(baseT).rearrange("p (e j) -> p e j", e=16)
        tT3 = f2(totT).rearrange("p (e j) -> p e j", e=16)
        V(lambda e: e.tensor_tensor(out=bT3, in0=bT3, in1=self.carry[:, :].unsqueeze(2).to_broadcast([128, 16, NJ]), op=ALU.add), r=[baseT, self.carry], w=[baseT])
        V(lambda e: e.tensor_tensor(out=self.carry[:, :], in0=bT3[:, :, NJ - 1], in1=tT3[:, :, NJ - 1], op=ALU.add), r=[baseT, totT], w=[self.carry])
        V(lambda e: e.tensor_tensor(out=pos[:, :, :], in0=cbk[:, 128:128 + W].rearrange("p (j e) -> p j e", e=16),
                                    in1=f2(baseT).rearrange("p (e j) -> p j e", e=16), op=ALU.add), r=[cbk, baseT], w=[pos])
        V(lambda e: e.tensor_scalar(out=f2(valid), in0=f2(pos), scalar1=float(self.CAP), scalar2=None, op0=ALU.is_lt), r=[pos], w=[valid])
        V(lambda e: e.tensor_tensor(out=slotv[:, :, :], in0=pos[:, :, :], in1=self.eoff[:, :].unsqueeze(1).to_broadcast([128, NJ, 16]), op=ALU.add), r=[pos, self.eoff], w=[slotv])
        V(lambda e: e.tensor_scalar(out=f2(slotv), in0=f2(slotv), scalar1=self.trashp[:, 0:1], scalar2=None, op0=ALU.subtract), r=[slotv, self.trashp], w=[slotv])
        V(lambda e: e.tensor_tensor(out=f2(slotv), in0=f2(slotv), in1=f2(valid), op=ALU.mult), r=[slotv, valid], w=[slotv])
        V(lambda e: e.tensor_scalar(out=f2(slotv), in0=f2(slotv), scalar1=self.trashp[:, 0:1], scalar2=None, op0=ALU.add), r=[slotv, self.trashp], w=[slotv])
        V(lambda e: e.tensor_tensor(out=f2(gw), in0=f2(gw), in1=f2(valid), op=ALU.mult), r=[gw, valid], w=[gw])
        V(lambda e: e.tensor_tensor_scan(out=f2(cum), data0=f2(mask_e0), data1=f2(chosen), initial=0.0, op0=ALU.mult, op1=ALU.add), r=[mask_e0, chosen], w=[cum])
        for which, dsti in ((1.0, self.slotA), (2.0, self.slotB)):
            wi = int(which) - 1
            V(lambda e, which=which: e.tensor_scalar(out=f2(tmp), in0=f2(cum), scalar1=float(which), scalar2=None, op0=ALU.is_equal), r=[cum], w=[tmp])
            V(lambda e: e.tensor_tensor(out=f2(tmp), in0=f2(tmp), in1=f2(chosen), op=ALU.mult), r=[tmp, chosen], w=[tmp])
            V(lambda e: e.tensor_tensor(out=f2(eq), in0=f2(tmp), in1=f2(gw), op=ALU.mult), r=[tmp, gw], w=[eq])
            V(lambda e, wi=wi: e.tensor_reduce(out=self.gAB[:, wi, j0:j0 + NJ], in_=eq[:, :, :], axis=AX.X, op=ALU.add), r=[eq], w=[self.gAB])
            V(lambda e: e.tensor_tensor(out=f2(tmp), in0=f2(tmp), in1=f2(slotv), op=ALU.mult), r=[tmp, slotv], w=[tmp])
            V(lambda e: e.tensor_reduce(out=t32[:, :], in_=tmp[:, :, :], axis=AX.X, op=ALU.add), r=[tmp], w=[t32])
            V(lambda e: e.tensor_scalar(out=t32[:, :], in0=t32[:, :], scalar1=0.0, scalar2=float(self.NSLOT - 1), op0=ALU.max, op1=ALU.min), r=[t32], w=[t32])
            V(lambda e, dsti=dsti: e.tensor_copy(out=dsti[:, j0:j0 + NJ], in_=t32[:, :]), r=[t32], w=[dsti])
        for j in range(j0, j0 + NJ):
            for dsti in (self.slotA, self.slotB):
                self.cx.dma("pool", None, None, reads=[self.h2c[j].b, dsti.b], writes=[],
                            fn=lambda e, dsti=dsti, j=j: e.indirect_dma_start(out=self.h2slots[:, :], out_offset=bass.IndirectOffsetOnAxis(ap=dsti[:, j:j + 1], axis=0),
                                                                              in_=self.h2all[:, j, :], in_offset=None))

    def p4_io(self):
        i = self.inp
        self.w_gate = i("exp_w_gate", [2, 16, D, 512])
        self.w_up = i("exp_w_up", [2, 16, D, 512])
        self.w_down = i("exp_w_down", [2, 16, 512, D])

    def zero_slots(self):
        self.push()
        zt = self.sb("zt", [128, D], F32)
        self.G(lambda e: e.memset(zt[:, :], 0.0), w=[zt])
        self.dma("sp", self.oslots[16 * self.CAP:16 * self.CAP + 128, :], zt[:, :], r=[zt], w=[self.oslots])
        self.pop()

    def p4_experts(self, l):
        B = self.bank
        V, S, G = self.V, self.S, self.G
        s = self.sb
        wg = [s(f"wg{i}", [128, 8, 512], BF16) for i in range(2)]
        wu = [s(f"wu{i}", [128, 8, 512], BF16) for i in range(2)]
        wd = [s(f"wd{i}", [128, 4, D], BF16) for i in range(2)]
        rt = [s(f"rtok{i}", [128, 4, D], BF16) for i in range(2)]
        rT = s("rT", [128, 8, 512], BF16)
        sil = [s(f"sil{i}", [128, 512], BF16) for i in range(2)]
        hidT = s("hidT", [128, 4, 512], BF16)
        osb = [s(f"eosb{i}", [128, D], F32) for i in range(2)]

        def load_w(e):
            i = e % 2
            self.dma("pool", wg[i][:, :, :], self.w_gate[l, e].rearrange("(k p) f -> p k f", p=128), r=[self.w_gate], w=[wg[i]])
            self.dma("pool", wu[i][:, :, :], self.w_up[l, e].rearrange("(k p) f -> p k f", p=128), r=[self.w_up], w=[wu[i]])
            self.dma("pool", wd[i][:, :, :], self.w_down[l, e].rearrange("(k p) f -> p k f", p=128), r=[self.w_down], w=[wd[i]])

        glist = [(e, off, nb) for e in range(16) for (off, nb) in self.GROUPS]
        load_w(0)
        ob = 0

        def load_rows(gi):
            e, off, nb = glist[gi]
            r0 = e * self.CAP + off
            rtk = rt[gi % 2]
            self.dma("sp", rtk[:, 0:nb, :], self.h2slots[r0:r0 + nb * 128, :].rearrange("(b p) d -> p b d", p=128), r=[self.h2slots], w=[rtk])
        load_rows(0)
        for gi, (e, off, nb) in enumerate(glist):
            if off == 0 and e + 1 < 16:
                load_w(e + 1)
            i = e % 2
            r0 = e * self.CAP + off
            N = nb * 128
            rtk = rt[gi % 2]
            if gi + 1 < len(glist):
                load_rows(gi + 1)
            for blk in range(nb):
                pT = B[blk % 2]
                pTb = pT[:, :].bitcast(BF16).rearrange("p (k t) -> p k t", k=8)
                for k in range(8):
                    self.tr(pTb[:, k, :], rtk[:, blk, k * 128:(k + 1) * 128], self.identb[:, :], r=[rtk, self.identb], w=[pT], signal=(k == 7))
                if blk % 2 == 0:
                    V(lambda e_, blk=blk, pTb=pTb: e_.tensor_copy(out=rT[:, :, blk * 128:(blk + 1) * 128], in_=pTb), r=[pT], w=[rT])
                else:
                    S(lambda e_, blk=blk, pTb=pTb: e_.copy(out=rT[:, :, blk * 128:(blk + 1) * 128], in_=pTb), r=[pT], w=[rT])
            for fc in range(4):
                pg, pu = B[2 + 2 * (fc % 2)], B[3 + 2 * (fc % 2)]
                for k in range(8):
                    self.mm(pg[:, 0:N], wg[i][:, k, fc * 128:(fc + 1) * 128], rT[:, k, 0:N], r=[wg[i], rT], w=[pg], start=(k == 0), stop=(k == 7))
                for k in range(8):
                    self.mm(pu[:, 0:N], wu[i][:, k, fc * 128:(fc + 1) * 128], rT[:, k, 0:N], r=[wu[i], rT], w=[pu], start=(k == 0), stop=(k == 7))
                sl = sil[fc % 2]
                S(lambda e_, sl=sl, pg=pg, N=N: e_.activation(out=sl[:, 0:N], in_=pg[:, 0:N], func=AF.Silu), r=[pg], w=[sl])
                V(lambda e_, sl=sl, pu=pu, fc=fc, N=N: e_.tensor_tensor(out=hidT[:, fc, 0:N], in0=pu[:, 0:N], in1=sl[:, 0:N], op=ALU.mult), r=[pu, sl], w=[hidT])
            for blk in range(nb):
                o = osb[ob % 2]
                ob += 1
                for half in range(2):
                    pd = B[6 + half]
                    for fc in range(4):
                        self.mm(pd[:, :], hidT[:, fc, blk * 128:(blk + 1) * 128], wd[i][:, fc, half * 512:(half + 1) * 512], r=[hidT, wd[i]], w=[pd],
                                start=(fc == 0), stop=(fc == 3))
                    V(lambda e_, o=o, pd=pd, half=half: e_.tensor_tensor(out=o[:, half * 512:(half + 1) * 512], in0=pd[:, :],
                                                                         in1=self.opg[:, 1, half * 512:(half + 1) * 512], op=ALU.mult), r=[pd, self.opg], w=[o], x=[o])
                self.dma("sp", self.oslots[r0 + blk * 128:r0 + (blk + 1) * 128, :], o[:, :], r=[o], w=[])

    def p5_alloc(self, l):
        s = self.sb
        self.lng2 = s("lng2", [128, D], F32); self.lnb2 = s("lnb2", [128, D], F32)
        self.dma("sp", self.lng2[:, :], self.ln2_g[l].partition_broadcast(128), r=[self.ln2_g], w=[self.lng2])
        self.dma("sp", self.lnb2[:, :], self.ln2_b[l].partition_broadcast(128), r=[self.ln2_b], w=[self.lnb2])
        self.rA = [s(f"rA{i}", [128, D], F32) for i in range(2)]
        self.rB = [s(f"rB{i}", [128, D], F32) for i in range(2)]
        self.x1r = [s(f"x1r{i}", [128, D], F32) for i in range(2)]
        self.x2t = [s(f"x2t{i}", [128, D], F32) for i in range(2)]
        self.st5 = s("st5", [128, 24], F32)
        self.y5 = [s(f"y5_{i}", [128, D], F32) for i in range(2)]

    def p5_loads(self, jj):
        if jj >= NCH:
            return
        rA_, rB_, x1r_ = self.rA[jj % 2], self.rB[jj % 2], self.x1r[jj % 2]
        self.cx.dma("pool", None, None, reads=[self.oslots.b, self.slotA.b], writes=[rA_.b],
                    fn=lambda e: e.indirect_dma_start(out=rA_[:, :], out_offset=None, in_=self.oslots[:, :],
                                                      in_offset=bass.IndirectOffsetOnAxis(ap=self.slotA[:, jj:jj + 1], axis=0)))
        self.cx.dma("pool", None, None, reads=[self.oslots.b, self.slotB.b], writes=[rB_.b],
                    fn=lambda e: e.indirect_dma_start(out=rB_[:, :], out_offset=None, in_=self.oslots[:, :],
                                                      in_offset=bass.IndirectOffsetOnAxis(ap=self.slotB[:, jj:jj + 1], axis=0)))
        self.dma("sp", x1r_[:, :], self.x1d[jj * 128:(jj + 1) * 128, :], r=[self.x1d], w=[x1r_])

    def p5_S1(self, j):
        if j >= NCH:
            return
        V, S, G = self.V, self.S, self.G
        rA, rB, x1r, y5 = self.rA[j % 2], self.rB[j % 2], self.x1r[j % 2], self.y5[j % 2]
        S(lambda e: e.activation(out=rA[:, :], in_=rA[:, :], func=AF.Identity, scale=self.gAB[:, 0, j:j + 1]), r=[rA, self.gAB], w=[rA])
        V(lambda e: e.scalar_tensor_tensor(out=rA[:, :], in0=rB[:, :], scalar=self.gAB[:, 1, j:j + 1], in1=rA[:, :], op0=ALU.mult, op1=ALU.add), r=[rB, rA, self.gAB], w=[rA])
        V(lambda e: e.scalar_tensor_tensor(out=y5[:, :], in0=x1r[:, :], scalar=float(ALPHA), in1=rA[:, :], op0=ALU.mult, op1=ALU.add), r=[x1r, rA], w=[y5], x=[rA])

    def p5_S2(self, j, dst):
        if j >= NCH or j < 0:
            return
        S = self.S
        y5, x2 = self.y5[j % 2], self.x2t[j % 2]
        self.ln_stats(y5, lambda a, b: y5[:, a:b], D, self.st5, act=True)
        S(lambda e: e.activation(out=x2[:, :], in_=y5[:, :], func=AF.Identity, bias=self.st5[:, 4:5], scale=self.st5[:, 3:4]), r=[y5, self.st5], w=[x2])

    def p5_S3(self, j, dst):
        if j >= NCH or j < 0:
            return
        V, G = self.V, self.G
        rows = slice(j * 128, (j + 1) * 128)
        x2 = self.x2t[j % 2]
        V(lambda e: e.tensor_tensor(out=x2[:, 0:512], in0=x2[:, 0:512], in1=self.lng2[:, 0:512], op=ALU.mult), r=[x2, self.lng2], w=[x2])
        G(lambda e: e.tensor_tensor(out=x2[:, 512:1024], in0=x2[:, 512:1024], in1=self.lng2[:, 512:1024], op=ALU.mult), r=[x2, self.lng2], w=[x2])
        V(lambda e: e.tensor_tensor(out=x2[:, 0:512], in0=x2[:, 0:512], in1=self.lnb2[:, 0:512], op=ALU.add), r=[x2, self.lnb2], w=[x2])
        G(lambda e: e.tensor_tensor(out=x2[:, 512:1024], in0=x2[:, 512:1024], in1=self.lnb2[:, 512:1024], op=ALU.add), r=[x2, self.lnb2], w=[x2])
        self.dma("sp", dst[rows, :], x2[:, :], r=[x2], w=[dst])

    def p5_all(self, l, dst):
        self.p5_loads(0); self.p5_loads(1)
        self.p5_S1(0)
        self.p5_loads(2)
        self.p5_S1(1)
        self.p5_S2(0, dst)
        for j in range(NCH):
            self.p5_loads(j + 3)
            self.p5_S1(j + 2)
            self.p5_S2(j + 1, dst)
            self.p5_S3(j, dst)

    def build(self):
        self.declare_io(); self.s5_io(); self.p3_io(); self.p4_io()
        self.setup()
        x_src = self.x_in
        for l in range(self.nlayers):
            dst = self.out if l == self.nlayers - 1 else self.xmid
            self.push()
            self.layer_alloc(); self.route_alloc(); self.layer_prep(l, 0)
            self.zero_slots()
            self.push(); self.qk_alloc()
            self.push(); self.s5_alloc(); self.s5_prep(l); self.load_win(l); self.p1v2_alloc()
            self.p1_all(l, x_src)
            self.pop()
            self.push(); self.p2_alloc(); self.p2_attention(); self.pop()
            self.pop()
            self.push()
            self.opg = self.sb("opg", [128, 2, 1024], F32)
            self.opg2 = self.sb("opg2", [128, 2, 1024], F32)
            self.layer_prep(l, 1)
            self.push(); self.p3_alloc(l)
            self.p3_route_alloc()
            self.p3_all(l, x_src)
            self.pop()
            self.push(); self.p4_experts(l); self.pop()
            self.push(); self.p5_alloc(l)
            self.p5_all(l, dst)
            self.pop()
            self.pop()
            self.pop()
            x_src = dst
        if getattr(self, "dbg_hook", None):
            self.dbg_hook(self)
        self.finish()


def make_inputs(inp, b):
    c = np.ascontiguousarray
    f = lambda k: np.asarray(inp[k])
    br, bi = f("ssm_b_re"), f("ssm_b_im")
    def bl(a):
        L = a.shape[0]
        return a.reshape(L, 2, 8, 64, 16).transpose(0, 2, 4, 1, 3).reshape(L, 128, 2, 64)
    bT = np.stack([bl(br), bl(bi)], axis=1)
    cr, ci = f("ssm_c_re"), f("ssm_c_im")
    cT = np.concatenate([cr.transpose(0, 1, 3, 2), ci.transpose(0, 1, 3, 2)], axis=2)
    L = br.shape[0]
    d = {
        "x": c(f("x")[b]), "ccol": c(f("c")[b].reshape(8, 128).T), "pos": c(f("positions")[b].reshape(32, 128).T),
        "ada_w": f("ada_w"), "ada_b": f("ada_b"), "w_in": f("w_in"), "gm_ln_g": f("gm_ln_g"), "gm_ln_b": f("gm_ln_b"),
        "gm_ws": f("gm_ws"), "gm_bsT": c(f("gm_bs").transpose(0, 2, 1)),
        "lam_re": c(f("ssm_lam_re").reshape(L, 1024)), "lam_im": c(f("ssm_lam_im").reshape(L, 1024)), "log_dt": f("ssm_log_dt"),
        "ssm_bT": c(bT), "ssm_cT": c(cT), "ssm_dT": c(f("ssm_d").reshape(L, 2, 128).transpose(0, 2, 1)),
        "glu_w": f("glu_w"), "glu_bT": c(f("glu_b").reshape(L, 2, 128).transpose(0, 2, 1)),
        "w_out": f("w_out"), "ln1_g": f("ln1_g"), "ln1_b": f("ln1_b"), "ln2_g": f("ln2_g"), "ln2_b": f("ln2_b"),
        "router_w": f("router_w"), "router_bias": f("router_bias"),
        "exp_w_gate": f("exp_w_gate"), "exp_w_up": f("exp_w_up"), "exp_w_down": f("exp_w_down"),
    }
    return d


_CACHE = {}


def kernel(**inputs):
    n = 8
    if "nc" not in _CACHE:
        nc = bass.Bass("TRN2", target_bir_lowering=False)
        kb = KB(nc)
        kb.build()
        _CACHE["nc"] = nc
        _CACHE["names"] = kb.in_names
    nc = _CACHE["nc"]
    names = _CACHE["names"]
    in_maps = []
    for b in range(n):
        im = make_inputs(inputs, b)
        in_maps.append({k: v for k, v in im.items() if k in names})
    res = run_bass_kernel_spmd(nc, in_maps, core_ids=list(range(n)))
    out = np.stack([np.asarray(r["out"]) for r in res.results], axis=0)
    return out.astype(np.float32)
```

```python
import numpy as np
import concourse.bass as bass
import concourse.mybir as mybir

F32 = mybir.dt.float32
BF16 = mybir.dt.bfloat16
I32 = mybir.dt.int32
U32 = mybir.dt.uint32
AF = mybir.ActivationFunctionType
ALU = mybir.AluOpType
AX = mybir.AxisListType


RELAXED = ()
ALLOW_RELAX = True


class Buf:
    __slots__ = ("w", "r", "name")

    def __init__(self, name=""):
        self.w = None
        self.r = []
        self.name = name


class Ctx:
    def __init__(self, nc, strict_same=False):
        self.nc = nc
        self.strict_same = strict_same
        self.relaxed = set(RELAXED)
        self.engs = {"pe": nc.tensor, "act": nc.scalar, "dve": nc.vector, "pool": nc.gpsimd, "sp": nc.sync}
        self.sem = {}
        self.cnt = {}
        for e in ("pe", "act", "dve", "pool"):
            self.sem[e] = nc.alloc_semaphore("s_" + e)
            self.cnt[e] = 0
        self.dq = {}
        for q, n in (("sp", 10), ("act", 4), ("pool", 8)):
            self.dq[q] = {"sems": [nc.alloc_semaphore(f"d_{q}{i}") for i in range(n)], "vals": [0] * n, "k": 0}
        self.waited = {}
        self.nbuf = 0
        self.out_events = []

    def buf(self, name=""):
        return Buf(name)

    def _wait(self, eng, ev):
        sem, val = ev
        key = (eng, id(sem))
        if self.waited.get(key, 0) >= val:
            return
        self.engs[eng].wait_ge(sem, val)
        self.waited[key] = val

    def _deps(self, eng, reads, writes, relax=()):
        own = self.sem.get(eng)
        rl = set(id(b) for b in relax) if ALLOW_RELAX else set()

        def chk(b, ev):
            if ev[0] is own and (eng == "pe" or id(b) in rl):
                return
            self._wait(eng, ev)
        for b in reads:
            if b.w is not None:
                chk(b, b.w)
        for b in writes:
            if b.w is not None:
                chk(b, b.w)
            for ev in b.r:
                chk(b, ev)

    def _commit(self, ev, reads, writes):
        for b in writes:
            b.w = ev
            b.r = []
        for b in reads:
            b.r.append(ev)
            if len(b.r) > 24:
                b.r = b.r[-24:]

    def op(self, eng, fn, reads=(), writes=(), signal=True, relax=()):
        self._deps(eng, reads, writes, relax)
        inst = fn(self.engs[eng])
        if signal:
            self.cnt[eng] += 1
            inst.then_inc(self.sem[eng], 1)
            ev = (self.sem[eng], self.cnt[eng])
        else:
            ev = (self.sem[eng], self.cnt[eng] + 1)
        self._commit(ev, reads, writes)
        return ev

    def dma(self, q, out, in_, reads=(), writes=(), fn=None, **kw):
        d = self.dq[q]
        i = d["k"] % len(d["sems"])
        d["k"] += 1
        sem = d["sems"][i]
        self._deps(q, reads, writes)
        if d["vals"][i] > 0:
            self._wait(q, (sem, d["vals"][i]))
        if fn is None:
            inst = self.engs[q].dma_start(out=out, in_=in_, **kw)
        else:
            inst = fn(self.engs[q])
        d["vals"][i] += 16
        inst.then_inc(sem, 16)
        ev = (sem, d["vals"][i])
        self._commit(ev, reads, writes)
        return ev

    def barrier(self):
        evs = [(self.sem[e], self.cnt[e]) for e in self.sem if self.cnt[e] > 0]
        for q, d in self.dq.items():
            for sem, v in zip(d["sems"], d["vals"]):
                if v > 0:
                    evs.append((sem, v))
        for eng in ("pe", "act", "dve", "pool", "sp"):
            own = self.sem.get(eng)
            for ev in evs:
                self._wait(eng, ev)

    def finish(self, bufs):
        for b in bufs:
            if b.w is not None:
                self._wait("sp", b.w)
            for ev in b.r:
                self._wait("sp", ev)

from concourse.bass_utils import run_bass_kernel_spmd
import math
import contextlib

T = 4096
D = 1024
NCH = 32
PW = 2304
EPS = 1e-5
ALPHA = (2.0 * 2) ** 0.25
TWO_PI = 2.0 * math.pi
ROPE_THETA = 500000.0


class Tl:
    def __init__(self, h, name=""):
        self.h = h
        self.b = Buf(name)

    def __getitem__(self, k):
        return self.h[k]


class KB:
    def __init__(self, nc, nlayers=2, dbg=(), stop_after=None):
        self.nc = nc
        self.cx = Ctx(nc)
        self.dbg = set(dbg)
        self.stop_after = stop_after
        self.nlayers = nlayers
        self.outs = []
        self.stk = [contextlib.ExitStack()]
        self.nps = 0

    def inp(self, name, shape, dt=F32):
        self.in_names = getattr(self, "in_names", set())
        self.in_names.add(name)
        return Tl(self.nc.dram_tensor(name, list(shape), dt, kind="ExternalInput").ap(), name)

    def outp(self, name, shape, dt=F32):
        t = Tl(self.nc.dram_tensor(name, list(shape), dt, kind="ExternalOutput").ap(), name)
        self.outs.append(t)
        return t

    def scratch(self, name, shape, dt):
        return Tl(self.nc.dram_tensor(name, list(shape), dt, kind="Internal").ap(), name)

    def sb(self, name, shape, dt):
        self.nsb = getattr(self, "nsb", 0) + 1
        h = self.stk[-1].enter_context(self.nc.sbuf_tensor(f"{name}_{self.nsb}", list(shape), dt))
        return Tl(h, name)

    def push(self):
        self.stk.append(contextlib.ExitStack())

    def pop(self):
        self.cx.barrier()
        self.stk.pop().close()

    def ps(self, name, shape, dt=F32):
        return Tl(self.nc.alloc_psum_tensor(name, list(shape), dt), name)

    def _rw(self, r, w):
        return [t.b for t in r], [t.b for t in w]

    def V(self, fn, r=(), w=(), x=()):
        r, w = self._rw(r, w)
        return self.cx.op("dve", fn, r, w, relax=[t.b for t in x])

    def S(self, fn, r=(), w=(), x=()):
        r, w = self._rw(r, w)
        return self.cx.op("act", fn, r, w, relax=[t.b for t in x])

    def G(self, fn, r=(), w=(), x=()):
        r, w = self._rw(r, w)
        return self.cx.op("pool", fn, r, w, relax=[t.b for t in x])

    def P(self, fn, r=(), w=(), signal=True):
        r, w = self._rw(r, w)
        return self.cx.op("pe", fn, r, w, signal=signal)

    def dma(self, q, out, in_, r=(), w=(), **kw):
        r, w = self._rw(r, w)
        return self.cx.dma(q, out, in_, r, w, **kw)

    def mm(self, out, lhsT, rhs, r, w, start, stop, signal=None):
        if signal is None:
            signal = stop
        return self.P(lambda e: e.matmul(out, lhsT, rhs, start=start, stop=stop), r, w, signal=signal)

    def tr(self, out, in_, ident, r, w, signal=True):
        return self.P(lambda e: e.transpose(out, in_, ident), r, w, signal=signal)

    def declare_io(self):
        i = self.inp
        self.x_in = i("x", [T, D])
        self.ccol = i("ccol", [128, 8])
        self.pos = i("pos", [128, NCH], I32)
        self.ada_w = i("ada_w", [2, D, 6 * D])
        self.ada_b = i("ada_b", [2, 6 * D])
        self.w_in = i("w_in", [2, D, PW])
        self.gm_ln_g = i("gm_ln_g", [2, 256])
        self.gm_ln_b = i("gm_ln_b", [2, 256])
        self.gm_ws = i("gm_ws", [2, 4, 128, 128])
        self.gm_bsT = i("gm_bsT", [2, 128, 4])
        self.out = self.outp("out", [T, D])
        self.mixtok = self.scratch("mixtok", [T, 1024], BF16)
        self.vd = self.scratch("vd", [T, 520], BF16)

    def consts(self):
        nc = self.nc
        self.identb = self.sb("identb", [128, 128], BF16)
        self.identf = self.sb("identf", [128, 128], F32)
        self.onesf = self.sb("onesf", [128, 128], F32)
        self.eps_t = self.sb("eps_t", [128, 1], F32)
        self.G(lambda e: e.memset(self.onesf[:, :], 1.0), w=[self.onesf])
        self.G(lambda e: e.memset(self.eps_t[:, :], EPS), w=[self.eps_t])
        self.mhalf = self.sb("mhalf", [128, 1], F32)
        self.G(lambda e: e.memset(self.mhalf[:, :], -0.5), w=[self.mhalf])
        self.G(lambda e: e.affine_select(out=self.identf[:, :], in_=self.onesf[:, :], pattern=[[-1, 128]],
                                         compare_op=ALU.is_equal, fill=0.0, base=0, channel_multiplier=1),
               r=[self.onesf], w=[self.identf])
        self.G(lambda e: e.tensor_copy(out=self.identb[:, :], in_=self.identf[:, :]), r=[self.identf], w=[self.identb])
        self.posf = self.sb("posf", [128, NCH], F32)
        self.posi = self.sb("posi", [128, NCH], I32)
        self.dma("sp", self.posi[:, :], self.pos[:, :], r=[self.pos], w=[self.posi])
        self.V(lambda e: e.tensor_copy(out=self.posf[:, :], in_=self.posi[:, :]), r=[self.posi], w=[self.posf])
        self.cs = self.sb("cs", [128, NCH, 8], F32)
        self.sn = self.sb("sn", [128, NCH, 8], F32)
        self.push()
        ang = self.sb("ang", [128, NCH, 8], F32)
        tmp = self.sb("angt", [128, NCH, 8], F32)
        tmi = self.sb("angi", [128, NCH, 8], I32)
        for j in range(8):
            fr = ROPE_THETA ** (-(j * 2.0) / 16.0)
            self.V(lambda e, j=j, fr=fr: e.tensor_scalar(out=ang[:, :, j], in0=self.posf[:, :], scalar1=float(fr), scalar2=None, op0=ALU.mult),
                   r=[self.posf], w=[ang])
        self.sincos(ang, self.sn, self.cs, tmp, tmi, [128, NCH * 8])
        self.pop()

    def _flat(self, t):
        ap = t[:]
        if len(ap.shape) == 2:
            return ap
        names = " ".join(f"a{i}" for i in range(len(ap.shape) - 1))
        return ap.rearrange(f"p {names} -> p ({names})")

    def range_reduce(self, src, dst, tmp, tmi, shift):
        s, d, t, ti = self._flat(src), self._flat(dst), self._flat(tmp), self._flat(tmi)
        self.V(lambda e: e.tensor_scalar(out=t, in0=s, scalar1=float(shift), scalar2=float(1.0 / TWO_PI), op0=ALU.add, op1=ALU.mult),
               r=[src], w=[tmp])
        self.V(lambda e: e.tensor_copy(out=ti, in_=t), r=[tmp], w=[tmi])
        self.V(lambda e: e.tensor_copy(out=t, in_=ti), r=[tmi], w=[tmp])
        self.V(lambda e: e.tensor_scalar(out=t, in0=t, scalar1=float(-TWO_PI), scalar2=float(shift), op0=ALU.mult, op1=ALU.add),
               r=[tmp], w=[tmp])
        self.V(lambda e: e.tensor_tensor(out=d, in0=t, in1=s, op=ALU.add), r=[tmp, src], w=[dst])
        self.V(lambda e: e.tensor_scalar(out=t, in0=d, scalar1=float(math.pi), scalar2=float(-TWO_PI), op0=ALU.is_gt, op1=ALU.mult),
               r=[dst], w=[tmp])
        self.V(lambda e: e.tensor_tensor(out=d, in0=d, in1=t, op=ALU.add), r=[tmp, dst], w=[dst])
        self.V(lambda e: e.tensor_scalar(out=t, in0=d, scalar1=float(-math.pi), scalar2=float(TWO_PI), op0=ALU.is_lt, op1=ALU.mult),
               r=[dst], w=[tmp])
        self.V(lambda e: e.tensor_tensor(out=d, in0=d, in1=t, op=ALU.add), r=[tmp, dst], w=[dst])
        self.V(lambda e: e.tensor_scalar(out=d, in0=d, scalar1=float(math.pi), scalar2=float(-math.pi), op0=ALU.min, op1=ALU.max),
               r=[dst], w=[dst])

    def sincos(self, ang, sn, cs, tmp, tmi, shape):
        self.range_reduce(ang, sn, tmp, tmi, 0.0)
        self.S(lambda e: e.activation(out=self._flat(sn), in_=self._flat(sn), func=AF.Sin), r=[sn], w=[sn])
        self.range_reduce(ang, cs, tmp, tmi, math.pi / 2)
        self.S(lambda e: e.activation(out=self._flat(cs), in_=self._flat(cs), func=AF.Sin), r=[cs], w=[cs])

    def setup(self):
        self.bank = [self.ps(f"bank{i}", [128, 512], F32) for i in range(8)]
        self.consts()

    def layer_alloc(self):
        self.modp = self.sb("modp", [128, 4, 8], F32)

        self.wsT = self.sb("wsT", [128, 4, 128], BF16)
        self.gbs = self.sb("gbs", [128, 4], F32)
        self.glng = self.sb("glng", [128, 256], F32)
        self.glnb = self.sb("glnb", [128, 256], F32)

    def prep_alloc(self):
        self.adaw = [self.sb(f"adaw{i}", [128, 8, 512], F32) for i in range(2)]
        self.adab = [self.sb(f"adab{i}", [128, 512], F32) for i in range(2)]
        self.modc = [self.sb(f"modc{i}", [128, 512], F32) for i in range(2)]
        self.wtmp = self.sb("wtmp", [128, 4, 128], F32)
        ccs = self.sb("ccs", [128, 8], F32)
        self.dma("sp", ccs[:, :], self.ccol[:, :], r=[self.ccol], w=[ccs])
        self.S(lambda e: e.activation(out=ccs[:, :], in_=ccs[:, :], func=AF.Silu), r=[ccs], w=[ccs])
        self.condrep = self.sb("condrep", [128, 8, 128], F32)
        self.V(lambda e: e.tensor_copy(out=self.condrep[:, :, :], in_=ccs[:, :].unsqueeze(2).to_broadcast([128, 8, 128])),
               r=[ccs], w=[self.condrep])


    def load_win(self, l):
        self.win = self.sb("win", [128, 8, PW], BF16)
        for k in range(8):
            self.dma("pool", self.win[:, k, :], self.w_in[l, k * 128:(k + 1) * 128, :], r=[self.w_in], w=[self.win])

    def layer_prep(self, l, part=0):
        self.push()
        self.prep_alloc()
        pb = self.bank[7]
        pt = self.bank[6]
        for n in range(12):
            if (part == 0) != (n // 2 in (0, 1)):
                continue
            aw = self.adaw[n % 2]
            ab = self.adab[n % 2]
            mc = self.modc[n % 2]
            self.dma("sp", aw[:, :, :], self.ada_w[l, :, n * 512:(n + 1) * 512].rearrange("(k p) n -> p k n", p=128),
                     r=[self.ada_w], w=[aw])
            self.dma("sp", ab[:, :], self.ada_b[l, n * 512:(n + 1) * 512].partition_broadcast(128), r=[self.ada_b], w=[ab])
            for k in range(8):
                self.mm(pb[:, :], self.condrep[:, k, :], aw[:, k, :], r=[self.condrep, aw], w=[pb], start=(k == 0), stop=(k == 7))
            which, half = n // 2, n % 2
            if which in (2, 5, 3, 4):
                tgt = self.opg if which in (2, 5) else self.opg2
                gi = {2: 0, 5: 1, 3: 0, 4: 1}[which]
                dst = tgt[:, gi, half * 512:(half + 1) * 512]
                self.V(lambda e, dst=dst: e.tensor_tensor(out=dst, in0=pb[:, :], in1=ab[:, :], op=ALU.add), r=[pb, ab], w=[tgt])
                if which != 3:
                    self.V(lambda e, dst=dst: e.tensor_scalar(out=dst, in0=dst, scalar1=1.0, scalar2=None, op0=ALU.add), r=[tgt], w=[tgt])
            else:
                slot = {0: 0, 1: 1}[which]
                self.V(lambda e: e.tensor_tensor(out=mc[:, :], in0=pb[:, :], in1=ab[:, :], op=ALU.add), r=[pb, ab], w=[mc])
                for b4 in range(4):
                    self.tr(pt[:, b4 * 128:(b4 + 1) * 128], mc[:, b4 * 128:(b4 + 1) * 128], self.identf[:, :], r=[mc, self.identf], w=[pt])
                addc = 1.0 if slot in (1, 3) else 0.0
                for b4 in range(4):
                    self.V(lambda e, b4=b4: e.tensor_scalar(out=self.modp[:, slot, half * 4 + b4:half * 4 + b4 + 1],
                                                            in0=pt[:, b4 * 128:b4 * 128 + 1], scalar1=float(addc), scalar2=None, op0=ALU.add),
                           r=[pt], w=[self.modp])
        if part == 0:
            self.dma("sp", self.wtmp[:, :, :], self.gm_ws[l].rearrange("h t s -> t h s"), r=[self.gm_ws], w=[self.wtmp])
            self.G(lambda e: e.affine_select(out=self.wtmp[:, :, :], in_=self.wtmp[:, :, :], pattern=[[0, 4], [-1, 128]],
                                             compare_op=ALU.is_ge, fill=0.0, base=0, channel_multiplier=1), r=[self.wtmp], w=[self.wtmp])
            for h in range(4):
                self.tr(pt[:, h * 128:(h + 1) * 128], self.wtmp[:, h, :], self.identf[:, :], r=[self.wtmp, self.identf], w=[pt])
            self.V(lambda e: e.tensor_copy(out=self.wsT[:, :, :], in_=pt[:, :].rearrange("p (h t) -> p h t", h=4)), r=[pt], w=[self.wsT])
            self.dma("sp", self.gbs[:, :], self.gm_bsT[l], r=[self.gm_bsT], w=[self.gbs])
            self.dma("sp", self.glng[:, :], self.gm_ln_g[l].partition_broadcast(128), r=[self.gm_ln_g], w=[self.glng])

            self.dma("sp", self.glnb[:, :], self.gm_ln_b[l].partition_broadcast(128), r=[self.gm_ln_b], w=[self.glnb])
        self.pop()

    def ln_stats(self, src, src_ap_fn, n, st, act=False):
        if act:
            return self.ln_stats_act(src, src_ap_fn, n, st)
        nchk = (n + 511) // 512
        w = n // nchk
        for i in range(nchk):
            self.V(lambda e, i=i: e.bn_stats(out=st[:, 8 + i * 6:8 + (i + 1) * 6], in_=src_ap_fn(i * w, (i + 1) * w)), r=[src], w=[st])
        self.V(lambda e: e.bn_aggr(out=st[:, 0:2], in_=st[:, 8:8 + 6 * nchk]), r=[st], w=[st])
        self.V(lambda e: e.tensor_scalar(out=st[:, 2:3], in0=st[:, 1:2], scalar1=float(EPS), scalar2=None, op0=ALU.add), r=[st], w=[st])
        self.G(lambda e: e.tensor_tensor(out=st[:, 3:4], in0=st[:, 2:3], in1=self.mhalf[:, 0:1], op=ALU.pow), r=[st, self.mhalf], w=[st])
        self.V(lambda e: e.tensor_scalar(out=st[:, 4:5], in0=st[:, 0:1], scalar1=-1.0, scalar2=st[:, 3:4], op0=ALU.mult, op1=ALU.mult), r=[st], w=[st])

    def ln_stats_act(self, src, src_ap_fn, n, st):
        nchk = (n + 511) // 512
        w = n // nchk
        for i in range(nchk):
            self.V(lambda e, i=i: e.bn_stats(out=st[:, 8 + i * 6:8 + (i + 1) * 6], in_=src_ap_fn(i * w, (i + 1) * w)), r=[src], w=[st])
        self.V(lambda e: e.bn_aggr(out=st[:, 0:2], in_=st[:, 8:8 + 6 * nchk]), r=[st], w=[st])
        self.S(lambda e: e.activation(out=st[:, 2:3], in_=st[:, 1:2], func=AF.Sqrt, bias=self.eps_t[:, 0:1], scale=1.0), r=[st, self.eps_t], w=[st])
        self.V(lambda e: e.reciprocal(out=st[:, 3:4], in_=st[:, 2:3]), r=[st], w=[st])
        self.V(lambda e: e.tensor_scalar(out=st[:, 4:5], in0=st[:, 0:1], scalar1=-1.0, scalar2=st[:, 3:4], op0=ALU.mult, op1=ALU.mult), r=[st], w=[st])

    def qk_alloc(self):
        self.QT = self.sb("QT", [128, 4, T], BF16)
        self.KT = self.sb("KT", [128, 4, T], BF16)

    def p1_alloc(self):
        s = self.sb
        self.xt = [s(f"xt{i}", [128, D], F32) for i in range(2)]
        self.xn = [s(f"xn{i}", [128, D], BF16) for i in range(2)]
        self.st = [s(f"st{i}", [128, 24], F32) for i in range(2)]
        self.st2 = [s(f"stb{i}", [128, 24], F32) for i in range(2)]
        self.hT = [s(f"hT{i}", [128, 8, 128], BF16) for i in range(2)]
        self.gl = [s(f"gl{i}", [128, 512], F32) for i in range(1)] * 2
        self.vnb = [s(f"vnb{i}", [128, 256], F32) for i in range(1)] * 2
        self.vb = [s(f"vb{i}", [128, 256], BF16) for i in range(1)] * 2
        self.aout = [s(f"aout{i}", [128, 256], BF16) for i in range(1)] * 2
        self.qf = [s(f"qf{i}", [128, 2, 512], F32) for i in range(1)] * 2
        self.qb = [s(f"qb{i}", [128, 2, 512], BF16) for i in range(1)] * 2
        self.rt = [s(f"rt{i}", [128, 4, 8, 8], F32) for i in range(1)] * 2
        self.vp = [s(f"vp{i}", [128, 8, 65], BF16) for i in range(1)] * 2
        for i in range(1):
            self.G(lambda e, i=i: e.memset(self.vp[i][:, :, :], 1.0), w=[self.vp[i]])

    def p1_chunk(self, l, j, x_src):
        i2 = j % 2
        xt, xn, st, st2, hT = self.xt[i2], self.xn[i2], self.st[i2], self.st2[i2], self.hT[i2]
        gl, vnb, vb, aout, qf, qb, rt, vp = self.gl[i2], self.vnb[i2], self.vb[i2], self.aout[i2], self.qf[i2], self.qb[i2], self.rt[i2], self.vp[i2]
        B = self.bank
        rows = slice(j * 128, (j + 1) * 128)
        if j == 0:
            self.dma("sp", xt[:, :], x_src[rows, :], r=[x_src], w=[xt])
        if j + 1 < NCH:
            xtn = self.xt[(j + 1) % 2]
            self.dma("sp", xtn[:, :], x_src[(j + 1) * 128:(j + 2) * 128, :], r=[x_src], w=[xtn])
        self.ln_stats(xt, lambda a, b: xt[:, a:b], D, st)
        self.S(lambda e: e.activation(out=xn[:, :], in_=xt[:, :], func=AF.Identity, bias=st[:, 4:5], scale=st[:, 3:4]), r=[xt, st], w=[xn])
        pT = B[0]
        pTb = pT[:, :].bitcast(BF16).rearrange("p (k t) -> p k t", k=8)
        for k in range(8):
            self.tr(pTb[:, k, :], xn[:, k * 128:(k + 1) * 128], self.identb[:, :], r=[xn, self.identb], w=[pT], signal=(k == 7))
        for k in range(8):
            self.V(lambda e, k=k: e.tensor_scalar(out=hT[:, k, :], in0=pTb[:, k, :], scalar1=self.modp[:, 1, k:k + 1], scalar2=self.modp[:, 0, k:k + 1],
                                                  op0=ALU.mult, op1=ALU.add), r=[pT, self.modp], w=[hT], x=[hT])
        for bi, c0 in ((1, 0), (2, 512), (3, 1024), (4, 1536)):
            for k in range(8):
                self.mm(B[bi][:, :], hT[:, k, :], self.win[:, k, c0:c0 + 512], r=[hT, self.win], w=[B[bi]], start=(k == 0), stop=(k == 7))
        self.S(lambda e: e.activation(out=gl[:, :], in_=B[1][:, :], func=AF.Gelu_apprx_tanh), r=[B[1]], w=[gl])
        self.ln_stats(gl, lambda a, b: gl[:, 256 + a:256 + b], 256, st2)
        self.V(lambda e: e.tensor_scalar(out=vnb[:, :], in0=gl[:, 256:512], scalar1=st2[:, 3:4], scalar2=st2[:, 4:5], op0=ALU.mult, op1=ALU.add),
               r=[gl, st2], w=[vnb])
        self.V(lambda e: e.tensor_tensor(out=vnb[:, :], in0=vnb[:, :], in1=self.glng[:, :], op=ALU.mult), r=[vnb, self.glng], w=[vnb], x=[vnb])
        self.V(lambda e: e.tensor_tensor(out=vb[:, :], in0=vnb[:, :], in1=self.glnb[:, :], op=ALU.add), r=[vnb, self.glnb], w=[vb], x=[vnb])
        psv = B[5]
        for h in range(4):
            self.mm(psv[:, h * 64:(h + 1) * 64], self.wsT[:, h, :], vb[:, h * 64:(h + 1) * 64], r=[self.wsT, vb], w=[psv], start=True, stop=True,
                    signal=(h == 3))
        for h in range(4):
            self.V(lambda e, h=h: e.scalar_tensor_tensor(out=aout[:, h * 64:(h + 1) * 64], in0=psv[:, h * 64:(h + 1) * 64], scalar=self.gbs[:, h:h + 1],
                                                         in1=gl[:, h * 64:(h + 1) * 64], op0=ALU.add, op1=ALU.mult), r=[psv, self.gbs, gl], w=[aout], x=[aout])
        self.dma("sp", self.mixtok[rows, 0:256], aout[:, :], r=[aout], w=[self.mixtok])
        self.S(lambda e: e.copy(out=qf[:, 0, :], in_=B[2][:, :]), r=[B[2]], w=[qf])
        self.S(lambda e: e.copy(out=qf[:, 1, :], in_=B[3][:, :]), r=[B[3]], w=[qf], x=[qf])
        q4 = qf[:, :, :].rearrange("p a (h e) -> p (a h) e", e=64)
        o4 = qb[:, :, :].rearrange("p a (h e) -> p (a h) e", e=64)
        for a in range(2):
            xa1, xa2 = q4[:, a * 8:(a + 1) * 8, 0:8], q4[:, a * 8:(a + 1) * 8, 8:16]
            cb = self.cs[:, j, :].unsqueeze(1).to_broadcast([128, 8, 8])
            sb_ = self.sn[:, j, :].unsqueeze(1).to_broadcast([128, 8, 8])
            oa = o4[:, a * 8:(a + 1) * 8, :]
            self.G(lambda e, xa1=xa1, cb=cb: e.tensor_tensor(out=rt[:, 0, :, :], in0=xa1, in1=cb, op=ALU.mult), r=[qf, self.cs], w=[rt])
            self.G(lambda e, xa2=xa2, sb_=sb_: e.tensor_tensor(out=rt[:, 1, :, :], in0=xa2, in1=sb_, op=ALU.mult), r=[qf, self.sn], w=[rt])
            self.G(lambda e, xa2=xa2, cb=cb: e.tensor_tensor(out=rt[:, 2, :, :], in0=xa2, in1=cb, op=ALU.mult), r=[qf, self.cs], w=[rt])
            self.G(lambda e, xa1=xa1, sb_=sb_: e.tensor_tensor(out=rt[:, 3, :, :], in0=xa1, in1=sb_, op=ALU.mult), r=[qf, self.sn], w=[rt])
            self.G(lambda e, oa=oa: e.tensor_tensor(out=oa[:, :, 0:8], in0=rt[:, 0, :, :], in1=rt[:, 1, :, :], op=ALU.subtract), r=[rt], w=[qb])
            self.G(lambda e, oa=oa: e.tensor_tensor(out=oa[:, :, 8:16], in0=rt[:, 2, :, :], in1=rt[:, 3, :, :], op=ALU.add), r=[rt], w=[qb])
            self.G(lambda e, oa=oa, a=a: e.tensor_copy(out=oa[:, :, 16:64], in_=q4[:, a * 8:(a + 1) * 8, 16:64]), r=[qf], w=[qb])
        pq = B[6]
        pqb = pq[:, :].bitcast(BF16).rearrange("p (a k t) -> p a k t", a=2, k=4)
        for a in range(2):
            for k in range(4):
                self.tr(pqb[:, a, k, :], qb[:, a, k * 128:(k + 1) * 128], self.identb[:, :], r=[qb, self.identb], w=[pq], signal=(a == 1 and k == 3))
        self.S(lambda e: e.copy(out=self.QT[:, :, j * 128:(j + 1) * 128], in_=pqb[:, 0, :, :]), r=[pq], w=[self.QT])
        self.S(lambda e: e.copy(out=self.KT[:, :, j * 128:(j + 1) * 128], in_=pqb[:, 1, :, :]), r=[pq], w=[self.KT])
        self.S(lambda e: e.copy(out=vp[:, :, 0:64], in_=B[4][:, :].rearrange("p (h e) -> p h e", e=64)), r=[B[4]], w=[vp])
        self.dma("sp", self.vd[rows, :], vp[:, :, :].rearrange("p h e -> p (h e)"), r=[vp], w=[self.vd])

    def finish(self):
        bufs = [t.b for t in self.outs]
        self.cx.finish(bufs)

    def p2_alloc(self):
        s = self.sb
        self.vbr = [s(f"vbr{i}", [128, 32, 520], BF16) for i in range(2)]
        self.negm = s("negm", [128, 256], BF16)
        negf = s("negf", [128, 256], F32)
        zf = s("zf", [128, 256], F32)
        self.G(lambda e: e.memset(zf[:, :], 0.0), w=[zf])
        self.G(lambda e: e.affine_select(out=negf[:, 0:128], in_=zf[:, 0:128], pattern=[[-1, 128]], compare_op=ALU.is_ge, fill=-30000.0,
                                         base=0, channel_multiplier=1), r=[zf], w=[negf])
        self.G(lambda e: e.affine_select(out=negf[:, 128:256], in_=zf[:, 128:256], pattern=[[1, 128]], compare_op=ALU.is_ge, fill=-30000.0,
                                         base=0, channel_multiplier=-1), r=[zf], w=[negf])
        self.G(lambda e: e.tensor_copy(out=self.negm[:, :], in_=negf[:, :]), r=[negf], w=[self.negm])
        self.m01 = s("m01", [128, 256], BF16)
        onef = s("onef2", [128, 256], F32)
        self.G(lambda e: e.memset(onef[:, :], 1.0), w=[onef])
        self.G(lambda e: e.affine_select(out=onef[:, 0:128], in_=onef[:, 0:128], pattern=[[-1, 128]], compare_op=ALU.is_ge, fill=0.0,
                                         base=0, channel_multiplier=1), r=[onef], w=[onef])
        self.G(lambda e: e.affine_select(out=onef[:, 128:256], in_=onef[:, 128:256], pattern=[[1, 128]], compare_op=ALU.is_ge, fill=0.0,
                                         base=0, channel_multiplier=-1), r=[onef], w=[onef])
        self.G(lambda e: e.tensor_copy(out=self.m01[:, :], in_=onef[:, :]), r=[onef], w=[self.m01])
        self.pexp = [s(f"pexp{i}", [128, 256], BF16) for i in range(6)]
        self.osb = [s(f"osb{i}", [128, 520], F32) for i in range(2)]

    def p2_attention(self):
        B = self.bank
        hb = 0
        self._hb = 0
        dils = (1, 4, 16)

        def load_vb(bi):
            d = dils[bi]
            vb_ = self.vbr[bi % 2]
            src = self.vd[:, :].rearrange("(n l r) c -> l n r c", l=128, r=d)
            for n in range(T // (128 * d)):
                self.dma("sp", vb_[:, n * d:(n + 1) * d, :], src[:, n, :, :], r=[self.vd], w=[vb_])
        load_vb(0)
        load_vb(1)
        for bi, d in enumerate(dils):
            seg = 128 * d
            nseg = T // seg
            vb = self.vbr[bi % 2]
            if bi == 1:
                load_vb(2)
            odst = self.obr[bi][:, :].rearrange("(n l r) c -> l n r c", l=128, r=d)
            blk = 0
            for n in range(nseg):
                for r_ in range(d):
                    cols = slice(n * seg + r_, (n + 1) * seg, d)
                    pcols = slice((n - 1) * seg + r_, n * seg, d)
                    bcur = n * d + r_
                    bprev = (n - 1) * d + r_
                    po = [B[6], B[7]]
                    osb = self.osb[blk % 2]
                    def scores(h):
                        nonlocal hb
                        hp, p0 = h // 2, (h % 2) * 64
                        ps = B[hb % 6]
                        o0 = 0
                        pe_ = self.pexp[hb % 6]
                        hb += 1
                        c0 = 0 if n > 0 else 128
                        if n > 0:
                            self.mm(ps[:, o0:o0 + 128], self.KT[p0:p0 + 64, hp, pcols], self.QT[p0:p0 + 64, hp, cols], r=[self.KT, self.QT], w=[ps],
                                    start=True, stop=True, signal=False)
                        self.mm(ps[:, o0 + 128:o0 + 256], self.KT[p0:p0 + 64, hp, cols], self.QT[p0:p0 + 64, hp, cols], r=[self.KT, self.QT], w=[ps],
                                start=True, stop=True)
                        self.S(lambda e, pe_=pe_, ps=ps, c0=c0, o0=o0: e.activation(out=pe_[:, c0:256], in_=ps[:, o0 + c0:o0 + 256], func=AF.Exp, scale=0.125),
                               r=[ps], w=[pe_])
                        mk = self.V if (h % 2 == 0) else self.G
                        mk(lambda e, pe_=pe_, c0=c0: e.tensor_tensor(out=pe_[:, c0:256], in0=pe_[:, c0:256], in1=self.m01[:, c0:256], op=ALU.mult),
                           r=[pe_, self.m01], w=[pe_])
                        return pe_

                    def pv(h, pe_):
                        pob = po[h // 4]
                        oc = slice((h % 4) * 65, (h % 4) * 65 + 65)
                        if n > 0:
                            self.mm(pob[:, oc], pe_[:, 0:128], vb[:, bprev, h * 65:(h + 1) * 65], r=[pe_, vb], w=[pob], start=True, stop=False)
                        self.mm(pob[:, oc], pe_[:, 128:256], vb[:, bcur, h * 65:(h + 1) * 65], r=[pe_, vb], w=[pob], start=(n == 0), stop=True)
                    pend = []
                    for h in range(8):
                        pend.append((h, scores(h)))
                        if len(pend) > 4:
                            pv(*pend.pop(0))
                    while pend:
                        pv(*pend.pop(0))
                    self.V(lambda e, osb=osb, po=po: e.tensor_copy(out=osb[:, 0:260], in_=po[0][:, 0:260]), r=[po[0]], w=[osb])
                    self.V(lambda e, osb=osb, po=po: e.tensor_copy(out=osb[:, 260:520], in_=po[1][:, 0:260]), r=[po[1]], w=[osb])
                    self.dma("sp", odst[:, n, r_, :], osb[:, :], r=[osb], w=[self.obr[bi]])
                    blk += 1

    def s5_io(self):
        i = self.inp
        self.lam_re = i("lam_re", [2, 1024])
        self.lam_im = i("lam_im", [2, 1024])
        self.log_dt = i("log_dt", [2, 16])
        self.ssm_bT = i("ssm_bT", [2, 2, 128, 2, 64])
        self.ssm_cT = i("ssm_cT", [2, 16, 128, 16])
        self.ssm_dT = i("ssm_dT", [2, 128, 2])
        self.glu_w = i("glu_w", [2, 256, 256])
        self.glu_bT = i("glu_bT", [2, 128, 2])
        self.coutT = self.scratch("coutT", [256, T], BF16)
        self.obr = [self.scratch(f"obr{i}", [T, 520], F32) for i in range(3)]

    def s5_alloc(self):
        s = self.sb
        self.Bblk = s("Bblk", [128, 2, 8, 2, 64], BF16)
        self.Cblk = s("Cblk", [128, 16, 128], BF16)
        self.Pre = s("Pre", [128, 16, 64], F32)
        self.PsT = s("PsT", [128, 16, 2, 64], F32)
        self.Qre = s("Qre", [128, 16, 64], F32)
        self.QsT = s("QsT", [128, 16, 2, 64], F32)
        self.glubh = s("glubh", [128, 2], F32)
        self.TriT = s("TriT", [128, 128], BF16)
        self.ones1 = s("ones1", [1, 128], BF16)
        self.dTt = s("dTt", [128, 2], F32)
        self.gluw = s("gluw", [128, 2, 256], BF16)
        self.glub = s("glub", [128, 2], F32)

    def s5_prep(self, l):
        s = self.sb
        V, S, G = self.V, self.S, self.G
        self.push()
        lre = s("lre", [128, 16, 64], F32)
        lim = s("lim", [128, 16, 64], F32)
        ldt = s("ldt", [128, 16], F32)
        self.dma("sp", lre[:, :, :].rearrange("p g n -> p (g n)"), self.lam_re[l].partition_broadcast(128), r=[self.lam_re], w=[lre])
        self.dma("sp", lim[:, :, :].rearrange("p g n -> p (g n)"), self.lam_im[l].partition_broadcast(128), r=[self.lam_im], w=[lim])
        self.dma("sp", ldt[:, :], self.log_dt[l].partition_broadcast(128), r=[self.log_dt], w=[ldt])
        S(lambda e: e.activation(out=ldt[:, :], in_=ldt[:, :], func=AF.Exp), r=[ldt], w=[ldt])
        dtb = ldt[:, :].unsqueeze(2).to_broadcast([128, 16, 64])
        lrd = s("lrd", [128, 16, 64], F32)
        lid = s("lid", [128, 16, 64], F32)
        V(lambda e: e.tensor_tensor(out=lrd[:, :, :], in0=lre[:, :, :], in1=dtb, op=ALU.mult), r=[lre, ldt], w=[lrd])
        V(lambda e: e.tensor_tensor(out=lid[:, :, :], in0=lim[:, :, :], in1=dtb, op=ALU.mult), r=[lim, ldt], w=[lid])
        sp1 = s("sp1", [128, 1], F32)
        G(lambda e: e.iota(sp1[:, :], pattern=[[0, 1]], base=1, channel_multiplier=1, allow_small_or_imprecise_dtypes=True), w=[sp1])
        E = s("E", [128, 16, 64], F32)
        An = s("An", [128, 16, 64], F32)
        sA = s("sA", [128, 16, 64], F32)
        cA = s("cA", [128, 16, 64], F32)
        tmp = s("s5tmp", [128, 16, 64], F32)
        tmi = s("s5tmi", [128, 16, 64], I32)
        qm = s("qm", [128, 16, 64], F32)
        pm = s("pm", [128, 16, 64], F32)
        V(lambda e: e.tensor_scalar(out=E[:, :, :], in0=lrd[:, :, :], scalar1=sp1[:, 0:1], scalar2=None, op0=ALU.mult), r=[lrd, sp1], w=[E])
        V(lambda e: e.tensor_scalar(out=An[:, :, :], in0=lid[:, :, :], scalar1=sp1[:, 0:1], scalar2=None, op0=ALU.mult), r=[lid, sp1], w=[An])
        self.sincos(An, sA, cA, tmp, tmi, None)
        S(lambda e: e.activation(out=qm[:, :, :], in_=E[:, :, :], func=AF.Exp), r=[E], w=[qm])
        S(lambda e: e.activation(out=pm[:, :, :], in_=E[:, :, :], func=AF.Exp, scale=-1.0), r=[E], w=[pm])
        V(lambda e: e.tensor_tensor(out=self.Qre[:, :, :], in0=qm[:, :, :], in1=cA[:, :, :], op=ALU.mult), r=[qm, cA], w=[self.Qre])
        V(lambda e: e.tensor_tensor(out=self.QsT[:, :, 1, :], in0=qm[:, :, :], in1=sA[:, :, :], op=ALU.mult), r=[qm, sA], w=[self.QsT])
        V(lambda e: e.tensor_scalar(out=self.QsT[:, :, 0, :], in0=self.QsT[:, :, 1, :], scalar1=-1.0, scalar2=None, op0=ALU.mult), r=[self.QsT], w=[self.QsT])
        V(lambda e: e.tensor_tensor(out=self.Pre[:, :, :], in0=pm[:, :, :], in1=cA[:, :, :], op=ALU.mult), r=[pm, cA], w=[self.Pre])
        V(lambda e: e.tensor_tensor(out=self.PsT[:, :, 0, :], in0=pm[:, :, :], in1=sA[:, :, :], op=ALU.mult), r=[pm, sA], w=[self.PsT])
        V(lambda e: e.tensor_scalar(out=self.PsT[:, :, 1, :], in0=self.PsT[:, :, 0, :], scalar1=-1.0, scalar2=None, op0=ALU.mult), r=[self.PsT], w=[self.PsT])
        self.sincos(lid, sA, cA, tmp, tmi, None)
        S(lambda e: e.activation(out=qm[:, :, :], in_=lrd[:, :, :], func=AF.Exp), r=[lrd], w=[qm])
        nr, ni = E, An
        V(lambda e: e.tensor_tensor(out=nr[:, :, :], in0=qm[:, :, :], in1=cA[:, :, :], op=ALU.mult), r=[qm, cA], w=[nr])
        V(lambda e: e.tensor_scalar(out=nr[:, :, :], in0=nr[:, :, :], scalar1=-1.0, scalar2=None, op0=ALU.add), r=[nr], w=[nr])
        V(lambda e: e.tensor_tensor(out=ni[:, :, :], in0=qm[:, :, :], in1=sA[:, :, :], op=ALU.mult), r=[qm, sA], w=[ni])
        m2 = pm
        V(lambda e: e.tensor_tensor(out=m2[:, :, :], in0=lre[:, :, :], in1=lre[:, :, :], op=ALU.mult), r=[lre], w=[m2])
        V(lambda e: e.tensor_tensor(out=tmp[:, :, :], in0=lim[:, :, :], in1=lim[:, :, :], op=ALU.mult), r=[lim], w=[tmp])
        V(lambda e: e.tensor_tensor(out=m2[:, :, :], in0=m2[:, :, :], in1=tmp[:, :, :], op=ALU.add), r=[m2, tmp], w=[m2])
        V(lambda e: e.reciprocal(out=m2[:, :, :], in_=m2[:, :, :]), r=[m2], w=[m2])
        fre, fim = sA, cA
        t1 = s("ft1", [128, 16, 64], F32)
        t2 = s("ft2", [128, 16, 64], F32)
        V(lambda e: e.tensor_tensor(out=t1[:, :, :], in0=nr[:, :, :], in1=lre[:, :, :], op=ALU.mult), r=[nr, lre], w=[t1])
        V(lambda e: e.tensor_tensor(out=t2[:, :, :], in0=ni[:, :, :], in1=lim[:, :, :], op=ALU.mult), r=[ni, lim], w=[t2])
        V(lambda e: e.tensor_tensor(out=t1[:, :, :], in0=t1[:, :, :], in1=t2[:, :, :], op=ALU.add), r=[t1, t2], w=[t1])
        V(lambda e: e.tensor_tensor(out=fre[:, :, :], in0=t1[:, :, :], in1=m2[:, :, :], op=ALU.mult), r=[t1, m2], w=[fre])
        V(lambda e: e.tensor_tensor(out=t1[:, :, :], in0=ni[:, :, :], in1=lre[:, :, :], op=ALU.mult), r=[ni, lre], w=[t1])
        V(lambda e: e.tensor_tensor(out=t2[:, :, :], in0=nr[:, :, :], in1=lim[:, :, :], op=ALU.mult), r=[nr, lim], w=[t2])
        V(lambda e: e.tensor_tensor(out=t1[:, :, :], in0=t1[:, :, :], in1=t2[:, :, :], op=ALU.subtract), r=[t1, t2], w=[t1])
        V(lambda e: e.tensor_tensor(out=fim[:, :, :], in0=t1[:, :, :], in1=m2[:, :, :], op=ALU.mult), r=[t1, m2], w=[fim])
        bT = s("bTt", [128, 2, 2, 64], F32)
        self.dma("sp", bT[:, :, :, :], self.ssm_bT[l].rearrange("r p k n -> p r k n"), r=[self.ssm_bT], w=[bT])
        bm = s("bmask", [128, 8], F32)
        one8 = s("one8", [128, 8], F32)
        G(lambda e: e.memset(one8[:, :], 1.0), w=[one8])
        G(lambda e: e.affine_select(out=bm[:, :], in_=one8[:, :], pattern=[[-16, 8]], compare_op=ALU.is_ge, fill=0.0, base=0, channel_multiplier=1),
          r=[one8], w=[bm])
        G(lambda e: e.affine_select(out=bm[:, :], in_=bm[:, :], pattern=[[16, 8]], compare_op=ALU.is_ge, fill=0.0, base=15, channel_multiplier=-1),
          r=[bm], w=[bm])
        bmb = bm[:, :].unsqueeze(2).to_broadcast([128, 8, 64])
        for kc in range(2):
            fr = fre[:, kc * 8:(kc + 1) * 8, :]
            fi = fim[:, kc * 8:(kc + 1) * 8, :]
            bre = bT[:, 0, kc, :].unsqueeze(1).to_broadcast([128, 8, 64])
            bim = bT[:, 1, kc, :].unsqueeze(1).to_broadcast([128, 8, 64])
            a1, a2 = t1[:, 0:8, :], t2[:, 0:8, :]
            V(lambda e, fr=fr, bre=bre: e.tensor_tensor(out=a1, in0=fr, in1=bre, op=ALU.mult), r=[fre, bT], w=[t1])
            V(lambda e, fi=fi, bim=bim: e.tensor_tensor(out=a2, in0=fi, in1=bim, op=ALU.mult), r=[fim, bT], w=[t2])
            V(lambda e: e.tensor_tensor(out=a1, in0=a1, in1=a2, op=ALU.subtract), r=[t1, t2], w=[t1])
            V(lambda e, kc=kc: e.tensor_tensor(out=self.Bblk[:, kc, :, 0, :], in0=a1, in1=bmb, op=ALU.mult), r=[t1, bm], w=[self.Bblk])
            V(lambda e, fr=fr, bim=bim: e.tensor_tensor(out=a1, in0=fr, in1=bim, op=ALU.mult), r=[fre, bT], w=[t1])
            V(lambda e, fi=fi, bre=bre: e.tensor_tensor(out=a2, in0=fi, in1=bre, op=ALU.mult), r=[fim, bT], w=[t2])
            V(lambda e: e.tensor_tensor(out=a1, in0=a1, in1=a2, op=ALU.add), r=[t1, t2], w=[t1])
            V(lambda e, kc=kc: e.tensor_tensor(out=self.Bblk[:, kc, :, 1, :], in0=a1, in1=bmb, op=ALU.mult), r=[t1, bm], w=[self.Bblk])
        cTs = s("cTs", [128, 16, 16], F32)
        self.dma("sp", cTs[:, :, :], self.ssm_cT[l].rearrange("g p c -> p g c"), r=[self.ssm_cT], w=[cTs])
        sg = s("sgn", [128, 1], F32)
        G(lambda e: e.memset(sg[0:64, :], 1.0), w=[sg])
        G(lambda e: e.memset(sg[64:128, :], -1.0), w=[sg])
        G(lambda e: e.memset(self.Cblk[:, :, :], 0.0), w=[self.Cblk])
        for g in range(16):
            c0 = (g % 8) * 16
            S(lambda e, g=g, c0=c0: e.activation(out=self.Cblk[:, g, c0:c0 + 16], in_=cTs[:, g, :], func=AF.Identity, scale=sg[:, 0:1]),
              r=[cTs, sg, self.Cblk], w=[self.Cblk])
        onesb = s("onesb", [128, 128], F32)
        G(lambda e: e.memset(onesb[:, :], 1.0), w=[onesb])
        G(lambda e: e.affine_select(out=onesb[:, :], in_=onesb[:, :], pattern=[[1, 128]], compare_op=ALU.is_ge, fill=0.0, base=0, channel_multiplier=-1),
          r=[onesb], w=[onesb])
        G(lambda e: e.tensor_copy(out=self.TriT[:, :], in_=onesb[:, :]), r=[onesb], w=[self.TriT])
        G(lambda e: e.memset(self.ones1[:, :], 1.0), w=[self.ones1])
        self.dma("sp", self.dTt[:, :], self.ssm_dT[l], r=[self.ssm_dT], w=[self.dTt])
        self.dma("sp", self.glub[:, :], self.glu_bT[l], r=[self.glu_bT], w=[self.glub])
        V(lambda e: e.tensor_scalar(out=self.glubh[:, :], in0=self.glub[:, :], scalar1=0.5, scalar2=None, op0=ALU.mult), r=[self.glub], w=[self.glubh])
        self.dma("pool", self.gluw[:, :, :], self.glu_w[l].rearrange("(k p) n -> p k n", p=128), r=[self.glu_w], w=[self.gluw])
        self.pop()

    def s5_chunk(self, l, j):
        B = self.bank
        V, S, G = self.V, self.S, self.G
        hT = self.hT[j % 2]
        uT = self.uT[j % 2]
        co = self.co[j % 2]
        ps_s = B[7]
        for cc in range(2):
            for k in range(8):
                self.mm(ps_s[:, cc * 128:(cc + 1) * 128], self.win[:, k, 2048 + cc * 128:2048 + (cc + 1) * 128], hT[:, k, :], r=[self.win, hT], w=[ps_s],
                        start=(k == 0), stop=(k == 7), signal=(k == 7 and cc == 1))
        S(lambda e: e.copy(out=uT[:, :, :], in_=ps_s[:, 0:256].rearrange("p (c t) -> p c t", c=2)), r=[ps_s], w=[uT])
        yps = B[7]
        for h in range(2):
            bu = [B[1], B[2]]
            zz = [B[3], B[4]]
            g0 = h * 8
            for q in range(2):
                self.mm(bu[q][:, :], uT[:, h, :], self.Bblk[:, h, q * 4:(q + 1) * 4, :, :].rearrange("p g r n -> p (g r n)"), r=[uT, self.Bblk], w=[bu[q]],
                        start=True, stop=True)
            t1, t2, vv = self.s5t1, self.s5t2, self.s5v
            for q in range(2):
                gs = slice(g0 + q * 4, g0 + q * 4 + 4)
                bu4 = bu[q][:, :].rearrange("p (g r n) -> p g r n", g=4, r=2)
                pc = self.Pre[:, gs, :].unsqueeze(2).to_broadcast([128, 4, 2, 64])
                V(lambda e, q=q, bu4=bu4, pc=pc: e.tensor_tensor(out=t1[:, q * 4:(q + 1) * 4, :, :], in0=bu4, in1=pc, op=ALU.mult), r=[bu[q], self.Pre], w=[t1], x=[t1])
                V(lambda e, q=q, bu4=bu4, gs=gs: e.tensor_tensor(out=t2[:, q * 4:(q + 1) * 4, :, :], in0=bu4[:, :, ::-1, :], in1=self.PsT[:, gs, :, :], op=ALU.mult),
                  r=[bu[q], self.PsT], w=[t2], x=[t2])
            G(lambda e: e.tensor_tensor(out=vv[:, :], in0=t1[:, :, :, :].rearrange("p g r n -> p (g r n)"), in1=t2[:, :, :, :].rearrange("p g r n -> p (g r n)"), op=ALU.add),
              r=[t1, t2], w=[vv])
            for q in range(2):
                self.mm(zz[q][:, :], self.TriT[:, :], vv[:, q * 512:(q + 1) * 512], r=[self.TriT, vv], w=[zz[q]], start=True, stop=False)
                self.mm(zz[q][:, :], self.ones1[:, :], self.x0row[h][:, q * 512:(q + 1) * 512], r=[self.ones1, self.x0row[h]], w=[zz[q]], start=False, stop=True)
            xs = self.s5x[h]
            for q in range(2):
                gs = slice(g0 + q * 4, g0 + q * 4 + 4)
                z4 = zz[q][:, :].rearrange("p (g r n) -> p g r n", g=4, r=2)
                qc = self.Qre[:, gs, :].unsqueeze(2).to_broadcast([128, 4, 2, 64])
                V(lambda e, q=q, z4=z4, qc=qc: e.tensor_tensor(out=t1[:, q * 4:(q + 1) * 4, :, :], in0=z4, in1=qc, op=ALU.mult), r=[zz[q], self.Qre], w=[t1], x=[t1])
                V(lambda e, q=q, z4=z4, gs=gs: e.tensor_tensor(out=t2[:, q * 4:(q + 1) * 4, :, :], in0=z4[:, :, ::-1, :], in1=self.QsT[:, gs, :, :], op=ALU.mult),
                  r=[zz[q], self.QsT], w=[t2], x=[t2])
            G(lambda e, xs=xs: e.tensor_tensor(out=xs[:, :, :].rearrange("p g m -> p (g m)"), in0=t1[:, :, :, :].rearrange("p g r n -> p (g r n)"),
                                        in1=t2[:, :, :, :].rearrange("p g r n -> p (g r n)"), op=ALU.add), r=[t1, t2], w=[xs])
            self.dma("act", self.x0row[h][0:1, :], xs[127:128, :, :].rearrange("p g m -> p (g m)"), r=[xs], w=[self.x0row[h]])
            pxt = B[0]
            pxb = pxt[:, :].bitcast(BF16).rearrange("p (g t) -> p g t", g=8)
            for g in range(8):
                self.tr(pxb[:, g, :], xs[:, g, :], self.identb[:, :], r=[xs, self.identb], w=[pxt], signal=(g == 7))
            V(lambda e, pxb=pxb: e.tensor_copy(out=self.s5xT[:, :, :], in_=pxb), r=[pxt], w=[self.s5xT])
            for g in range(8):
                self.mm(yps[:, 256 + h * 128:256 + (h + 1) * 128], self.Cblk[:, g0 + g, :], self.s5xT[:, g, :], r=[self.Cblk, self.s5xT], w=[yps],
                        start=(g == 0), stop=(g == 7))
        for cc in range(2):
            V(lambda e, cc=cc: e.scalar_tensor_tensor(out=self.yf[:, cc, :], in0=uT[:, cc, :], scalar=self.dTt[:, cc:cc + 1], in1=yps[:, 256 + cc * 128:256 + (cc + 1) * 128],
                                                      op0=ALU.mult, op1=ALU.add), r=[uT, self.dTt, yps], w=[self.yf], x=[self.yf])
        S(lambda e: e.activation(out=self.yg[:, :, :], in_=self.yf[:, :, :], func=AF.Gelu_apprx_tanh), r=[self.yf], w=[self.yg])
        gps = B[5]
        for c2 in range(2):
            for cc in range(2):
                self.mm(gps[:, c2 * 128:(c2 + 1) * 128], self.gluw[:, cc, c2 * 128:(c2 + 1) * 128], self.yg[:, cc, :], r=[self.gluw, self.yg], w=[gps],
                        start=(cc == 0), stop=(cc == 1))
        for c2 in range(2):
            S(lambda e, c2=c2: e.activation(out=self.sgm[:, c2, :], in_=gps[:, c2 * 128:(c2 + 1) * 128], func=AF.Sigmoid, bias=self.glub[:, c2:c2 + 1], scale=1.0),
              r=[gps, self.glub], w=[self.sgm])
        V(lambda e: e.tensor_tensor(out=co[:, :, :], in0=self.yg[:, :, :], in1=self.sgm[:, :, :], op=ALU.mult), r=[self.yg, self.sgm], w=[co])
        self.dma("sp", self.coutT[:, j * 128:(j + 1) * 128].rearrange("(c p) t -> p c t", p=128), co[:, :, :], r=[co], w=[self.coutT])

    def half(self, bank, lo):
        t = Tl(bank.h, bank.b.name + ("lo" if lo else "hi"))
        return t

    def p1v2_alloc(self):
        s = self.sb
        self.xt = [s(f"xt{i}", [128, D], F32) for i in range(2)]
        self.xn = [s(f"xn{i}", [128, D], BF16) for i in range(2)]
        self.st = [s(f"st{i}", [128, 24], F32) for i in range(2)]
        self.st2 = [s(f"stb{i}", [128, 24], F32) for i in range(2)]
        self.hT = [s(f"hT{i}", [128, 8, 128], BF16) for i in range(2)]
        self.gl = [s(f"gl{i}", [128, 512], F32) for i in range(2)]
        self.qbd = [s(f"qbd{i}", [128, 2, 512], BF16) for i in range(2)]
        self.vp = [s(f"vp{i}", [128, 8, 65], BF16) for i in range(2)]
        for i in range(2):
            self.G(lambda e, i=i: e.memset(self.vp[i][:, :, :], 1.0), w=[self.vp[i]])
        self.uT = [s(f"uT{i}", [128, 2, 128], BF16) for i in range(2)]
        self.vnb = s("vnb", [128, 256], F32)
        self.vb = s("vb", [128, 256], BF16)
        self.aout = s("aout", [128, 256], BF16)
        self.qb = s("qb", [128, 2, 512], BF16)
        self.rt = s("rt", [128, 4, 8, 8], F32)
        self.uD = [s(f"uD{i}", [128, 2, 128], F32) for i in range(3)]
        self.p1t = [s(f"p1t{i}", [128, 4, 2, 64], BF16) for i in range(2)]
        self.p2t = [s(f"p2t{i}", [128, 4, 2, 64], BF16) for i in range(2)]
        self.q1 = [s(f"q1_{i}", [128, 4, 2, 64], F32) for i in range(2)]
        self.q2 = [s(f"q2_{i}", [128, 4, 2, 64], F32) for i in range(2)]
        self.qv = [s(f"qv{i}", [128, 512], BF16) for i in range(2)]
        self.qx = [s(f"qx{i}", [128, 4, 128], BF16) for i in range(2)]
        self.qxT = [s(f"qxT{i}", [128, 4, 128], BF16) for i in range(3)]
        self.x0q = [s(f"x0q{i}", [1, 512], BF16) for i in range(4)]
        for i in range(4):
            self.G(lambda e, i=i: e.memset(self.x0q[i][:, :], 0.0), w=[self.x0q[i]])
        self.yf = s("yf2", [128, 2, 128], F32)
        self.yg = s("yg2", [128, 2, 128], BF16)
        self.sgm = s("sgm2", [128, 2, 128], F32)
        self.co = [s(f"co2_{i}", [128, 2, 128], BF16) for i in range(2)]
        B = self.bank
        self.b5lo, self.b5hi = Tl(B[5].h, "b5lo"), Tl(B[5].h, "b5hi")
        self.b6lo, self.b6hi = Tl(B[6].h, "b6lo"), Tl(B[6].h, "b6hi")
        self.b7lo, self.b7hi = Tl(B[7].h, "b7lo"), Tl(B[7].h, "b7hi")

    def p1_front_ln(self, l, j, x_src):
        if j >= NCH:
            return
        i2 = j % 2
        xt, xn, st = self.xt[i2], self.xn[i2], self.st[i2]
        S = self.S
        if j == 0:
            self.dma("sp", xt[:, :], x_src[0:128, :], r=[x_src], w=[xt])
            xt1 = self.xt[1]
            self.dma("sp", xt1[:, :], x_src[128:256, :], r=[x_src], w=[xt1])
        self.ln_stats(xt, lambda a, b: xt[:, a:b], D, st)
        S(lambda e: e.activation(out=xn[:, :], in_=xt[:, :], func=AF.Identity, bias=st[:, 4:5], scale=st[:, 3:4]), r=[xt, st], w=[xn])
        if j + 2 < NCH:
            self.dma("sp", xt[:, :], x_src[(j + 2) * 128:(j + 3) * 128, :], r=[x_src], w=[xt])

    def p1_front_a(self, l, j, x_src):
        if j >= NCH:
            return
        i2 = j % 2
        xn, hT = self.xn[i2], self.hT[i2]
        B = self.bank
        S = self.S
        pT = B[0]
        pTb = pT[:, :].bitcast(BF16).rearrange("p (k t) -> p k t", k=8)
        for k in range(8):
            self.tr(pTb[:, k, :], xn[:, k * 128:(k + 1) * 128], self.identb[:, :], r=[xn, self.identb], w=[pT], signal=(k == 7))
        for k in range(8):
            S(lambda e, k=k: e.activation(out=hT[:, k, :], in_=pTb[:, k, :], func=AF.Identity, scale=self.modp[:, 1, k:k + 1], bias=self.modp[:, 0, k:k + 1]),
              r=[pT, self.modp], w=[hT], x=[hT])

    def p1_front_s(self, l, j):
        if j >= NCH:
            return
        i2 = j % 2
        hT, uT = self.hT[i2], self.uT[i2]
        S = self.S
        ps_s = self.b6lo
        for cc in range(2):
            for k in range(8):
                self.mm(ps_s[:, cc * 128:(cc + 1) * 128], self.win[:, k, 2048 + cc * 128:2048 + (cc + 1) * 128], hT[:, k, :], r=[self.win, hT], w=[ps_s],
                        start=(k == 0), stop=(k == 7), signal=(k == 7 and cc == 1))
        S(lambda e: e.copy(out=uT[:, :, :], in_=ps_s[:, 0:256].rearrange("p (c t) -> p c t", c=2)), r=[ps_s], w=[uT])
        uD = self.uD[j % 3]
        for cc in range(2):
            S(lambda e, cc=cc: e.activation(out=uD[:, cc, :], in_=ps_s[:, cc * 128:(cc + 1) * 128], func=AF.Identity, scale=self.dTt[:, cc:cc + 1]), r=[ps_s, self.dTt], w=[uD], x=[uD])

    def p1_front_p(self, l, j, which):
        if j >= NCH:
            return
        i2 = j % 2
        hT, gl, qf, vp = self.hT[i2], self.gl[i2], self.qbd[i2], self.vp[i2]
        B = self.bank
        S = self.S

        def proj(bank, c0):
            for k in range(8):
                self.mm(bank[:, :], hT[:, k, :], self.win[:, k, c0:c0 + 512], r=[hT, self.win], w=[bank], start=(k == 0), stop=(k == 7))
        if which == 0:
            proj(B[1], 0)
            S(lambda e: e.activation(out=gl[:, :], in_=B[1][:, :], func=AF.Gelu_apprx_tanh), r=[B[1]], w=[gl])
        elif which == 1:
            proj(B[2], 512)
            S(lambda e: e.copy(out=qf[:, 0, :], in_=B[2][:, :]), r=[B[2]], w=[qf])
        elif which == 2:
            proj(B[1], 1024)
            S(lambda e: e.copy(out=qf[:, 1, :], in_=B[1][:, :]), r=[B[1]], w=[qf], x=[qf])
        else:
            proj(B[2], 1536)
            S(lambda e: e.copy(out=vp[:, :, 0:64], in_=B[2][:, :].rearrange("p (h e) -> p h e", e=64)), r=[B[2]], w=[vp])

    def p1_front(self, l, j, x_src):
        self.p1_front_a(l, j, x_src)
        self.p1_front_s(l, j)
        for w_ in range(4):
            self.p1_front_p(l, j, w_)

    def p1_back(self, l, j):
        if j < 0:
            return
        i2 = j % 2
        st2 = self.st2[i2]
        gl, qf, vp, uT = self.gl[i2], self.qbd[i2], self.vp[i2], self.uT[i2]
        vnb, vb, aout, qb, rt = self.vnb, self.vb, self.aout, self.qbd[i2], self.rt
        B = self.bank
        V, S, G = self.V, self.S, self.G
        rows = slice(j * 128, (j + 1) * 128)
        self.ln_stats(gl, lambda a, b: gl[:, 256 + a:256 + b], 256, st2)
        V(lambda e: e.tensor_scalar(out=vnb[:, :], in0=gl[:, 256:512], scalar1=st2[:, 3:4], scalar2=st2[:, 4:5], op0=ALU.mult, op1=ALU.add),
          r=[gl, st2], w=[vnb])
        V(lambda e: e.tensor_tensor(out=vnb[:, :], in0=vnb[:, :], in1=self.glng[:, :], op=ALU.mult), r=[vnb, self.glng], w=[vnb], x=[vnb])
        V(lambda e: e.tensor_tensor(out=vb[:, :], in0=vnb[:, :], in1=self.glnb[:, :], op=ALU.add), r=[vnb, self.glnb], w=[vb], x=[vnb])
        psv = self.b5lo
        for h in range(4):
            self.mm(psv[:, h * 64:(h + 1) * 64], self.wsT[:, h, :], vb[:, h * 64:(h + 1) * 64], r=[self.wsT, vb], w=[psv], start=True, stop=True,
                    signal=(h == 3))
        for h in range(4):
            V(lambda e, h=h: e.scalar_tensor_tensor(out=aout[:, h * 64:(h + 1) * 64], in0=psv[:, h * 64:(h + 1) * 64], scalar=self.gbs[:, h:h + 1],
                                                    in1=gl[:, h * 64:(h + 1) * 64], op0=ALU.add, op1=ALU.mult), r=[psv, self.gbs, gl], w=[aout], x=[aout])
        self.dma("sp", self.mixtok[rows, 0:256], aout[:, :], r=[aout], w=[self.mixtok])
        q4 = qf[:, :, :].rearrange("p a (h e) -> p (a h) e", e=64)
        o4 = qb[:, :, :].rearrange("p a (h e) -> p (a h) e", e=64)
        for a in range(2):
            xa1, xa2 = q4[:, a * 8:(a + 1) * 8, 0:8], q4[:, a * 8:(a + 1) * 8, 8:16]
            cb = self.cs[:, j, :].unsqueeze(1).to_broadcast([128, 8, 8])
            sb_ = self.sn[:, j, :].unsqueeze(1).to_broadcast([128, 8, 8])
            oa = o4[:, a * 8:(a + 1) * 8, :]
            G(lambda e, xa1=xa1, cb=cb: e.tensor_tensor(out=rt[:, 0, :, :], in0=xa1, in1=cb, op=ALU.mult), r=[qf, self.cs], w=[rt])
            G(lambda e, xa2=xa2, sb_=sb_: e.tensor_tensor(out=rt[:, 1, :, :], in0=xa2, in1=sb_, op=ALU.mult), r=[qf, self.sn], w=[rt])
            G(lambda e, xa2=xa2, cb=cb: e.tensor_tensor(out=rt[:, 2, :, :], in0=xa2, in1=cb, op=ALU.mult), r=[qf, self.cs], w=[rt])
            G(lambda e, xa1=xa1, sb_=sb_: e.tensor_tensor(out=rt[:, 3, :, :], in0=xa1, in1=sb_, op=ALU.mult), r=[qf, self.sn], w=[rt])
            G(lambda e, oa=oa: e.tensor_tensor(out=oa[:, :, 0:8], in0=rt[:, 0, :, :], in1=rt[:, 1, :, :], op=ALU.subtract), r=[rt], w=[qb])
            G(lambda e, oa=oa: e.tensor_tensor(out=oa[:, :, 8:16], in0=rt[:, 2, :, :], in1=rt[:, 3, :, :], op=ALU.add), r=[rt], w=[qb])
        pq = B[7]
        pqb = pq[:, :].bitcast(BF16).rearrange("p (a k t) -> p a k t", a=2, k=4)
        for a in range(2):
            for k in range(4):
                self.tr(pqb[:, a, k, :], qb[:, a, k * 128:(k + 1) * 128], self.identb[:, :], r=[qb, self.identb], w=[pq], signal=(a == 1 and k == 3))
        S(lambda e: e.copy(out=self.QT[:, :, j * 128:(j + 1) * 128], in_=pqb[:, 0, :, :]), r=[pq], w=[self.QT])
        S(lambda e: e.copy(out=self.KT[:, :, j * 128:(j + 1) * 128], in_=pqb[:, 1, :, :]), r=[pq], w=[self.KT])
        self.dma("sp", self.vd[rows, :], vp[:, :, :].rearrange("p h e -> p (h e)"), r=[vp], w=[self.vd])

    def s5A(self, i):
        if i < 0 or i >= 4 * NCH:
            return
        j, q = divmod(i, 4)
        kc = q // 2
        gs = slice(q * 4, q * 4 + 4)
        uT = self.uT[j % 2]
        bu = self.bank[3]
        t1, t2 = self.p1t[i % 2], self.p2t[i % 2]
        self.mm(bu[:, :], uT[:, kc, :], self.Bblk[:, kc, (q % 2) * 4:(q % 2) * 4 + 4, :, :].rearrange("p g r n -> p (g r n)"), r=[uT, self.Bblk], w=[bu],
                start=True, stop=True)
        bu4 = bu[:, :].rearrange("p (g r n) -> p g r n", g=4, r=2)
        pc = self.Pre[:, gs, :].unsqueeze(2).to_broadcast([128, 4, 2, 64])
        self.V(lambda e: e.tensor_tensor(out=t1[:, :, :, :], in0=bu4, in1=pc, op=ALU.mult), r=[bu, self.Pre], w=[t1])
        self.V(lambda e: e.tensor_tensor(out=t2[:, :, :, :], in0=bu4[:, :, ::-1, :], in1=self.PsT[:, gs, :, :], op=ALU.mult), r=[bu, self.PsT], w=[t2])

    def s5B(self, i):
        if i < 0 or i >= 4 * NCH:
            return
        j, q = divmod(i, 4)
        gs = slice(q * 4, q * 4 + 4)
        zz = self.bank[4]
        t1, t2 = self.p1t[i % 2], self.p2t[i % 2]
        u1, u2, xs = self.q1[i % 2], self.q2[i % 2], self.qx[i % 2]
        f = lambda t: t[:, :, :, :].rearrange("p g r n -> p (g r n)")
        self.mm(zz[:, :], self.TriT[:, :], f(t1), r=[self.TriT, t1], w=[zz], start=True, stop=False)
        self.mm(zz[:, :], self.TriT[:, :], f(t2), r=[self.TriT, t2], w=[zz], start=False, stop=False)
        self.mm(zz[:, :], self.ones1[:, :], self.x0q[q][:, :], r=[self.ones1, self.x0q[q]], w=[zz], start=False, stop=True)
        z4 = zz[:, :].rearrange("p (g r n) -> p g r n", g=4, r=2)
        qc = self.Qre[:, gs, :].unsqueeze(2).to_broadcast([128, 4, 2, 64])
        self.V(lambda e: e.tensor_tensor(out=u1[:, :, :, :], in0=z4, in1=qc, op=ALU.mult), r=[zz, self.Qre], w=[u1])
        self.V(lambda e: e.tensor_tensor(out=u2[:, :, :, :], in0=z4[:, :, ::-1, :], in1=self.QsT[:, gs, :, :], op=ALU.mult), r=[zz, self.QsT], w=[u2])
        self.G(lambda e: e.tensor_tensor(out=xs[:, :, :].rearrange("p g m -> p (g m)"), in0=f(u1), in1=f(u2), op=ALU.add), r=[u1, u2], w=[xs])
        self.dma("pool", self.x0q[q][0:1, :], xs[127:128, :, :].rearrange("p g m -> p (g m)"), r=[xs], w=[self.x0q[q]])

    def s5C(self, i):
        if i < 0 or i >= 4 * NCH:
            return
        xs, xT = self.qx[i % 2], self.qxT[i % 3]
        pxt = self.b6hi
        pxb = pxt[:, 256:512].bitcast(BF16).rearrange("p (g t) -> p g t", g=4)
        for g in range(4):
            self.tr(pxb[:, g, :], xs[:, g, :], self.identb[:, :], r=[xs, self.identb], w=[pxt], signal=(g == 3))
        self.S(lambda e: e.copy(out=xT[:, :, :], in_=pxb), r=[pxt], w=[xT])

    def s5D(self, i):
        if i < 0 or i >= 4 * NCH:
            return
        j, q = divmod(i, 4)
        kc = q // 2
        xT = self.qxT[i % 3]
        yps = self.b5hi
        for g in range(4):
            self.mm(yps[:, 256 + kc * 128:256 + (kc + 1) * 128], self.Cblk[:, q * 4 + g, :], xT[:, g, :], r=[self.Cblk, xT], w=[yps],
                    start=(q % 2 == 0 and g == 0), stop=(q % 2 == 1 and g == 3))

    def s5_step(self, i):
        self.s5A(i + 2)
        self.s5B(i + 1)
        self.s5C(i)
        if i % 2 == 0:
            self.s5D(i - 2)
            self.s5D(i - 1)

    def s5_tail(self, j):
        if j < 0 or j >= NCH:
            return
        V, S, G = self.V, self.S, self.G
        i2 = j % 2
        uT = self.uT[i2]
        yps = self.b5hi
        co = self.co[i2]
        for cc in range(2):
            V(lambda e, cc=cc: e.scalar_tensor_tensor(out=self.yf[:, cc, :], in0=self.uD[j % 3][:, cc, :], scalar=1.0, in1=yps[:, 256 + cc * 128:256 + (cc + 1) * 128],
                                                      op0=ALU.mult, op1=ALU.add), r=[self.uD[j % 3], yps], w=[self.yf], x=[self.yf])
        S(lambda e: e.activation(out=self.yg[:, :, :], in_=self.yf[:, :, :], func=AF.Gelu_apprx_tanh), r=[self.yf], w=[self.yg])
        gps = self.b5lo
        for c2 in range(2):
            for cc in range(2):
                self.mm(gps[:, c2 * 128:(c2 + 1) * 128], self.gluw[:, cc, c2 * 128:(c2 + 1) * 128], self.yg[:, cc, :], r=[self.gluw, self.yg], w=[gps],
                        start=(cc == 0), stop=(cc == 1))
        for c2 in range(2):
            S(lambda e, c2=c2: e.activation(out=self.sgm[:, c2, :], in_=gps[:, c2 * 128:(c2 + 1) * 128], func=AF.Tanh, bias=self.glubh[:, c2:c2 + 1], scale=0.5),
              r=[gps, self.glubh], w=[self.sgm])
        V(lambda e: e.scalar_tensor_tensor(out=self.sgm[:, :, :], in0=self.sgm[:, :, :], scalar=1.0, in1=self.yg[:, :, :], op0=ALU.add, op1=ALU.mult),
          r=[self.sgm, self.yg], w=[self.sgm])
        V(lambda e: e.tensor_scalar(out=co[:, :, :], in0=self.sgm[:, :, :], scalar1=0.5, scalar2=None, op0=ALU.mult), r=[self.sgm], w=[co])
        self.dma("sp", self.coutT[:, j * 128:(j + 1) * 128].rearrange("(c p) t -> p c t", p=128), co[:, :, :], r=[co], w=[self.coutT])

    def p1_all(self, l, x_src):
        self.p1_front_ln(l, 0, x_src)
        self.p1_front_ln(l, 1, x_src)
        self.p1_front(l, 0, x_src)
        self.s5A(0); self.s5A(1); self.s5B(0)
        for j in range(NCH):
            self.p1_front_ln(l, j + 2, x_src)
            self.p1_front(l, j + 1, x_src)
            self.p1_back(l, j)
            for q in range(4):
                self.s5_step(4 * j + q)
                if q == 0:
                    self.s5_tail(j - 1)
        self.s5_step(4 * NCH)
        self.s5_tail(NCH - 1)
        assert (4 * NCH) % 2 == 0

    CAP = 896
    NSLOT = 16 * 896 + 128
    GROUPS = ((0, 4), (512, 3))

    def p3_io(self):
        i = self.inp
        self.w_out = i("w_out", [2, D, D])
        self.ln1_g = i("ln1_g", [2, D]); self.ln1_b = i("ln1_b", [2, D])
        self.ln2_g = i("ln2_g", [2, D]); self.ln2_b = i("ln2_b", [2, D])
        self.router_w = i("router_w", [D, 16])
        self.router_bias = i("router_bias", [16])
        self.x1d = self.scratch("x1d", [T, D], F32)
        self.xmid = self.scratch("xmid", [T, D], F32)
        self.h2slots = self.scratch("h2slots", [self.NSLOT, D], BF16)
        self.oslots = self.scratch("oslots", [self.NSLOT, D], F32)

    def route_alloc(self):
        s = self.sb
        self.slotA = s("slotA", [128, NCH], I32)
        self.slotB = s("slotB", [128, NCH], I32)
        self.gAB = s("gAB", [128, 2, NCH], F32)

    def p3_alloc(self, l):
        s = self.sb
        self.wout = s("wout", [128, 8, D], BF16)
        for k in range(8):
            self.dma("pool", self.wout[:, k, :], self.w_out[l, k * 128:(k + 1) * 128, :], r=[self.w_out], w=[self.wout])
        self.lng = s("lng", [128, D], F32); self.lnb = s("lnb", [128, D], F32)
        self.dma("sp", self.lng[:, :], self.ln1_g[l].partition_broadcast(128), r=[self.ln1_g], w=[self.lng])
        self.dma("sp", self.lnb[:, :], self.ln1_b[l].partition_broadcast(128), r=[self.ln1_b], w=[self.lnb])
        self.rw = s("rw", [128, 8, 16], F32)
        self.dma("sp", self.rw[:, :, :], self.router_w[:, :].rearrange("(k p) e -> p k e", p=128), r=[self.router_w], w=[self.rw])
        self.rbias = s("rbias", [128, 16], F32)
        self.dma("sp", self.rbias[:, :], self.router_bias[:].partition_broadcast(128), r=[self.router_bias], w=[self.rbias])
        self.o3 = [s(f"o3_{i}", [128, 3, 520], F32) for i in range(2)]
        self.rec = s("rec", [128, 8], F32)
        self.mt = [s(f"mt{i}", [128, D], BF16) for i in range(2)]
        self.mixT = [s(f"mixT{i}", [128, 8, 128], BF16) for i in range(2)]
        self.xres = [s(f"xres{i}", [128, D], F32) for i in range(2)]
        self.yy = s("yy", [128, D], F32)
        self.x1t = [s(f"x1t{i}", [128, D], F32) for i in range(2)]
        self.cT = [s(f"cT{i}", [128, 2, 128], BF16) for i in range(2)]
        self.st3b = s("st3b", [128, 24], F32)
        self.h2f = s("h2f", [128, D], F32)
        self.h2f2 = [self.h2f, s("h2fb", [128, D], F32)]
        self.h2all = s("h2all", [128, NCH, D], BF16)
        self.h2c = [Tl(self.h2all.h, f"h2c{j}") for j in range(NCH)]
        self.scall = s("scall", [128, NCH, 16], F32)
        self.h2T = s("h2T", [128, 8, 128], F32)
        self.st3 = s("st3", [128, 24], F32)
        self.trs = s("trs", [128, 128], F32)
        self.eoff = s("eoff", [128, 16], F32)
        self.trashp = s("trashp", [128, 1], F32)
        self.ones16 = s("ones16", [128, 16], F32)
        G = self.G
        G(lambda e: e.memset(self.ones16[:, :], 1.0), w=[self.ones16])
        G(lambda e: e.affine_select(out=self.trs[:, :], in_=self.onesf[:, :], pattern=[[1, 128]], compare_op=ALU.is_ge, fill=0.0, base=-1, channel_multiplier=-1),
          r=[self.onesf], w=[self.trs])
        G(lambda e: e.iota(self.eoff[:, :], pattern=[[self.CAP, 16]], base=0, channel_multiplier=0, allow_small_or_imprecise_dtypes=True), w=[self.eoff])
        G(lambda e: e.iota(self.trashp[:, :], pattern=[[0, 1]], base=16 * self.CAP, channel_multiplier=1, allow_small_or_imprecise_dtypes=True), w=[self.trashp])

    def p3_L1(self, k):
        if k >= NCH:
            return
        rws = slice(k * 128, (k + 1) * 128)
        o3_, mt_ = self.o3[k % 2], self.mt[k % 2]
        for bi in range(3):
            self.dma("sp", o3_[:, bi, :], self.obr[bi][rws, :], r=[self.obr[bi]], w=[o3_])
        self.dma("sp", mt_[:, 0:256], self.mixtok[rws, 0:256], r=[self.mixtok], w=[mt_])

    def p3_L2(self, k, x_src):
        if k >= NCH:
            return
        rws = slice(k * 128, (k + 1) * 128)
        self.dma("sp", self.cT[k % 2][:, :, :], self.coutT[:, rws].rearrange("(c p) t -> p c t", p=128), r=[self.coutT], w=[self.cT[k % 2]])
        self.dma("sp", self.xres[k % 2][:, :], x_src[rws, :], r=[x_src], w=[self.xres[k % 2]])

    def p3_S1(self, j):
        if j >= NCH or j < 0:
            return
        B = self.bank
        V, S, G = self.V, self.S, self.G
        o3, rec, mt = self.o3[j % 2], self.rec, self.mt[j % 2]
        mixT = self.mixT[j % 2]
        V(lambda e: e.tensor_tensor(out=o3[:, 0, :], in0=o3[:, 0, :], in1=o3[:, 1, :], op=ALU.add), r=[o3], w=[o3], x=[o3])
        V(lambda e: e.tensor_tensor(out=o3[:, 0, :], in0=o3[:, 0, :], in1=o3[:, 2, :], op=ALU.add), r=[o3], w=[o3], x=[o3])
        o8 = o3[:, 0, :].rearrange("p (h e) -> p h e", e=65)
        V(lambda e: e.reciprocal(out=rec[:, :], in_=o8[:, :, 64]), r=[o3], w=[rec])
        V(lambda e: e.tensor_tensor(out=mt[:, 256:768].rearrange("p (h e) -> p h e", e=64), in0=o8[:, :, 0:64], in1=rec[:, :].unsqueeze(2).to_broadcast([128, 8, 64]),
                                    op=ALU.mult), r=[o3, rec], w=[mt])
        pT = B[0]
        pTb = pT[:, :].bitcast(BF16).rearrange("p (k t) -> p k t", k=8)
        for k in range(6):
            self.tr(pTb[:, k, :], mt[:, k * 128:(k + 1) * 128], self.identb[:, :], r=[mt, self.identb], w=[pT], signal=(k == 5))
        S(lambda e: e.copy(out=mixT[:, 0:6, :], in_=pTb[:, 0:6, :]), r=[pT], w=[mixT])

    def p3_S2(self, j):
        if j >= NCH or j < 0:
            return
        B = self.bank
        V, S, G = self.V, self.S, self.G
        rows = slice(j * 128, (j + 1) * 128)
        mixT, cT, xres = self.mixT[j % 2], self.cT[j % 2], self.xres[j % 2]
        wb = [B[1], B[2]] if j % 2 == 0 else [B[6], B[7]]
        for nb in range(2):
            for k in range(8):
                lhs = mixT[:, k, :] if k < 6 else cT[:, k - 6, :]
                self.mm(wb[nb][:, :], lhs, self.wout[:, k, nb * 512:(nb + 1) * 512], r=[mixT, cT, self.wout], w=[wb[nb]], start=(k == 0), stop=(k == 7))
        yy = self.yy
        for nb in range(2):
            cs_ = slice(nb * 512, (nb + 1) * 512)
            V(lambda e, nb=nb, cs_=cs_: e.tensor_tensor(out=yy[:, cs_], in0=wb[nb][:, :], in1=self.opg[:, 0, cs_], op=ALU.mult), r=[wb[nb], self.opg], w=[yy], x=[yy])
        V(lambda e: e.scalar_tensor_tensor(out=yy[:, :], in0=xres[:, :], scalar=float(ALPHA), in1=yy[:, :], op0=ALU.mult, op1=ALU.add), r=[xres, yy], w=[yy], x=[yy])
        self.ln_stats(yy, lambda a, b: yy[:, a:b], D, self.st3, act=True)
        x1 = self.x1t[j % 2]
        S(lambda e: e.activation(out=x1[:, :], in_=yy[:, :], func=AF.Identity, bias=self.st3[:, 4:5], scale=self.st3[:, 3:4]), r=[yy, self.st3], w=[x1])
        V(lambda e: e.tensor_tensor(out=x1[:, :], in0=x1[:, :], in1=self.lng[:, :], op=ALU.mult), r=[x1, self.lng], w=[x1])
        V(lambda e: e.tensor_tensor(out=x1[:, :], in0=x1[:, :], in1=self.lnb[:, :], op=ALU.add), r=[x1, self.lnb], w=[x1], x=[x1])
        self.dma("sp", self.x1d[rows, :], x1[:, :], r=[x1], w=[self.x1d])

    def p3_S3a(self, j):
        if j >= NCH or j < 0:
            return
        S = self.S
        x1 = self.x1t[j % 2]
        h2f = self.h2f2[j % 2]
        self.ln_stats(x1, lambda a, b: x1[:, a:b], D, self.st3b, act=True)
        S(lambda e: e.activation(out=h2f[:, :], in_=x1[:, :], func=AF.Identity, bias=self.st3b[:, 4:5], scale=self.st3b[:, 3:4]), r=[x1, self.st3b], w=[h2f])

    def p3_S3b(self, j):
        if j >= NCH or j < 0:
            return
        B = self.bank
        V, S, G = self.V, self.S, self.G
        h2f = self.h2f2[j % 2]
        V(lambda e: e.tensor_tensor(out=h2f[:, :], in0=h2f[:, :], in1=self.opg2[:, 1, :], op=ALU.mult), r=[h2f, self.opg2], w=[h2f])
        V(lambda e: e.tensor_tensor(out=h2f[:, :], in0=h2f[:, :], in1=self.opg2[:, 0, :], op=ALU.add), r=[h2f, self.opg2], w=[h2f], x=[h2f])
        S(lambda e: e.copy(out=self.h2all[:, j, :], in_=h2f[:, :]), r=[h2f], w=[self.h2c[j]])
        for half in range(2):
            pt = B[3 + half]
            for k in range(4):
                self.tr(pt[:, k * 128:(k + 1) * 128], h2f[:, (half * 4 + k) * 128:(half * 4 + k + 1) * 128], self.identf[:, :], r=[h2f, self.identf], w=[pt], signal=(k == 3))
            S(lambda e, half=half, pt=pt: e.copy(out=self.h2T[:, half * 4:(half + 1) * 4, :], in_=pt[:, :].rearrange("p (k t) -> p k t", k=4)), r=[pt], w=[self.h2T])
        lg = B[5]
        for k in range(8):
            self.mm(lg[:, 0:16], self.h2T[:, k, :], self.rw[:, k, :], r=[self.h2T, self.rw], w=[lg], start=(k == 0), stop=(k == 7))
        S(lambda e: e.copy(out=self.scall[:, j, :], in_=lg[:, 0:16]), r=[lg], w=[self.scall])
        if (j + 1) % self.RSEG == 0:
            self.p3_route(j + 1 - self.RSEG)

    def p3_all(self, l, x_src):
        self.p3_L1(0)
        for j in range(-2, NCH + 1):
            self.p3_L1(j + 3)
            self.p3_L2(j + 2, x_src)
            self.p3_S1(j + 2)
            self.p3_S2(j + 1)
            self.p3_S3a(j)
            self.p3_S3b(j - 1)

    RSEG = 8

    def p3_route_alloc(self):
        s = self.sb
        NJ = self.RSEG
        mk = lambda n: s(n, [128, NJ, 16], F32)
        t = {}
        t["big"] = [mk(f"r_{i}") for i in range(14)]
        t["g4"] = [s(f"r4_{i}", [128, NJ, 4], F32) for i in range(4)]
        t["g1"] = [s(f"r1_{i}", [128, NJ], F32) for i in range(3)]
        t["mask_e0"] = mk("mask_e0")
        t["mask_j0"] = s("mask_j0", [128, 16, NJ], F32)
        G = self.G
        G(lambda e: e.memset(t["mask_e0"][:, :, :], 1.0), w=[t["mask_e0"]])
        G(lambda e: e.memset(t["mask_e0"][:, :, 0:1], 0.0), w=[t["mask_e0"]])
        G(lambda e: e.memset(t["mask_j0"][:, :, :], 1.0), w=[t["mask_j0"]])
        G(lambda e: e.memset(t["mask_j0"][:, :, 0:1], 0.0), w=[t["mask_j0"]])
        self.carry = s("carry", [128, 16], F32)
        G(lambda e: e.memset(self.carry[:, :], 0.0), w=[self.carry])
        self._rt = t

    def p3_route(self, j0):
        B = self.bank
        V, S, G = self.V, self.S, self.G
        NJ = self.RSEG
        t = self._rt
        sel, eq, msk, top2, chosen, gw, tmp, cum, pos, valid, slotv, baseT, totT, sc = t["big"]
        m1, m2, gs, gsel = t["g4"]
        gmax, gsum, t32 = t["g1"]
        mask_e0, mask_j0 = t["mask_e0"], t["mask_j0"]
        f2 = lambda t_: t_[:, :, :].rearrange("p j e -> p (j e)")
        S(lambda e: e.activation(out=f2(sc), in_=self.scall[:, j0:j0 + NJ, :].rearrange("p j e -> p (j e)"), func=AF.Sigmoid), r=[self.scall], w=[sc])
        g4 = lambda t: t[:, :, :].rearrange("p j (g i) -> p (j g) i", i=4)
        b4 = lambda t: t[:, :, :].rearrange("p j g -> p (j g)").unsqueeze(2).to_broadcast([128, NJ * 4, 4])
        V(lambda e: e.tensor_tensor(out=sel[:, :, :], in0=sc[:, :, :], in1=self.rbias[:, :].unsqueeze(1).to_broadcast([128, NJ, 16]), op=ALU.add), r=[sc, self.rbias], w=[sel])
        V(lambda e: e.tensor_reduce(out=m1[:, :, :].rearrange("p j g -> p (j g)"), in_=g4(sel), axis=AX.X, op=ALU.max), r=[sel], w=[m1])
        V(lambda e: e.tensor_tensor(out=g4(eq), in0=g4(sel), in1=b4(m1), op=ALU.is_equal), r=[sel, m1], w=[eq])
        V(lambda e: e.scalar_tensor_tensor(out=f2(msk), in0=f2(eq), scalar=-1e9, in1=f2(sel), op0=ALU.mult, op1=ALU.add), r=[eq, sel], w=[msk])
        V(lambda e: e.tensor_reduce(out=m2[:, :, :].rearrange("p j g -> p (j g)"), in_=g4(msk), axis=AX.X, op=ALU.max), r=[msk], w=[m2])
        V(lambda e: e.tensor_tensor(out=gs[:, :, :], in0=m1[:, :, :], in1=m2[:, :, :], op=ALU.add), r=[m1, m2], w=[gs])
        V(lambda e: e.tensor_reduce(out=gmax[:, :], in_=gs[:, :, :], axis=AX.X, op=ALU.max), r=[gs], w=[gmax])
        V(lambda e: e.tensor_tensor(out=gsel[:, :, :], in0=gs[:, :, :], in1=gmax[:, :].unsqueeze(2).to_broadcast([128, NJ, 4]), op=ALU.is_equal), r=[gs, gmax], w=[gsel])
        V(lambda e: e.tensor_tensor(out=g4(top2), in0=g4(sel), in1=b4(m2), op=ALU.is_ge), r=[sel, m2], w=[top2])
        V(lambda e: e.tensor_tensor(out=g4(chosen), in0=g4(top2), in1=b4(gsel), op=ALU.mult), r=[top2, gsel], w=[chosen])
        V(lambda e: e.tensor_tensor(out=gw[:, :, :], in0=chosen[:, :, :], in1=sc[:, :, :], op=ALU.mult), r=[chosen, sc], w=[gw])
        V(lambda e: e.tensor_reduce(out=gsum[:, :], in_=gw[:, :, :], axis=AX.X, op=ALU.add), r=[gw], w=[gsum])
        V(lambda e: e.reciprocal(out=gsum[:, :], in_=gsum[:, :]), r=[gsum], w=[gsum])
        V(lambda e: e.tensor_tensor(out=gw[:, :, :], in0=gw[:, :, :], in1=gsum[:, :].unsqueeze(2).to_broadcast([128, NJ, 16]), op=ALU.mult), r=[gw, gsum], w=[gw])
        cbk = B[5]
        W = NJ * 16
        self.mm(cbk[:, 128:128 + W], self.trs[:, :], f2(chosen), r=[self.trs, chosen], w=[cbk], start=True, stop=True)
        self.mm(cbk[:, 256:256 + W], self.onesf[:, :], f2(chosen), r=[self.onesf, chosen], w=[cbk], start=True, stop=True)
        V(lambda e: e.tensor_copy(out=f2(totT).rearrange("p (e j) -> p e j", e=16),
                                  in_=cbk[:, 256:256 + W].rearrange("p (j e) -> p e j", e=16)), r=[cbk], w=[totT])
        V(lambda e: e.tensor_tensor_scan(out=f2(baseT), data0=mask_j0[:, :, :].rearrange("p e j -> p (e j)"), data1=f2(totT), initial=0.0, op0=ALU.mult, op1=ALU.add),
          r=[mask_j0, totT], w=[baseT])
        V(lambda e: e.tensor_tensor(out=f2(baseT), in0=f2(baseT), in1=f2(totT), op=ALU.subtract), r=[baseT, totT], w=[baseT])
        bT3 = f2(baseT).rearrange("p (e j) -> p e j", e=16)
        tT3 = f2(totT).rearrange("p (e j) -> p e j", e=16)
        V(lambda e: e.tensor_tensor(out=bT3, in0=bT3, in1=self.carry[:, :].unsqueeze(2).to_broadcast([128, 16, NJ]), op=ALU.add), r=[baseT, self.carry], w=[baseT])
        V(lambda e: e.tensor_tensor(out=self.carry[:, :], in0=bT3[:, :, NJ - 1], in1=tT3[:, :, NJ - 1], op=ALU.add), r=[baseT, totT], w=[self.carry])
        V(lambda e: e.tensor_tensor(out=pos[:, :, :], in0=cbk[:, 128:128 + W].rearrange("p (j e) -> p j e", e=16),
                                    in1=f2(baseT).rearrange("p (e j) -> p j e", e=16), op=ALU.add), r=[cbk, baseT], w=[pos])
        V(lambda e: e.tensor_scalar(out=f2(valid), in0=f2(pos), scalar1=float(self.CAP), scalar2=None, op0=ALU.is_lt), r=[pos], w=[valid])
        V(lambda e: e.tensor_tensor(out=slotv[:, :, :], in0=pos[:, :, :], in1=self.eoff[:, :].unsqueeze(1).to_broadcast([128, NJ, 16]), op=ALU.add), r=[pos, self.eoff], w=[slotv])
        V(lambda e: e.tensor_scalar(out=f2(slotv), in0=f2(slotv), scalar1=self.trashp[:, 0:1], scalar2=None, op0=ALU.subtract), r=[slotv, self.trashp], w=[slotv])
        V(lambda e: e.tensor_tensor(out=f2(slotv), in0=f2(slotv), in1=f2(valid), op=ALU.mult), r=[slotv, valid], w=[slotv])
        V(lambda e: e.tensor_scalar(out=f2(slotv), in0=f2(slotv), scalar1=self.trashp[:, 0:1], scalar2=None, op0=ALU.add), r=[slotv, self.trashp], w=[slotv])
        V(lambda e: e.tensor_tensor(out=f2(gw), in0=f2(gw), in1=f2(valid), op=ALU.mult), r=[gw, valid], w=[gw])
        V(lambda e: e.tensor_tensor_scan(out=f2(cum), data0=f2(mask_e0), data1=f2(chosen), initial=0.0, op0=ALU.mult, op1=ALU.add), r=[mask_e0, chosen], w=[cum])
        for which, dsti in ((1.0, self.slotA), (2.0, self.slotB)):
            wi = int(which) - 1
            V(lambda e, which=which: e.tensor_scalar(out=f2(tmp), in0=f2(cum), scalar1=float(which), scalar2=None, op0=ALU.is_equal), r=[cum], w=[tmp])
            V(lambda e: e.tensor_tensor(out=f2(tmp), in0=f2(tmp), in1=f2(chosen), op=ALU.mult), r=[tmp, chosen], w=[tmp])
            V(lambda e: e.tensor_tensor(out=f2(eq), in0=f2(tmp), in1=f2(gw), op=ALU.mult), r=[tmp, gw], w=[eq])
            V(lambda e, wi=wi: e.tensor_reduce(out=self.gAB[:, wi, j0:j0 + NJ], in_=eq[:, :, :], axis=AX.X, op=ALU.add), r=[eq], w=[self.gAB])
            V(lambda e: e.tensor_tensor(out=f2(tmp), in0=f2(tmp), in1=f2(slotv), op=ALU.mult), r=[tmp, slotv], w=[tmp])
            V(lambda e: e.tensor_reduce(out=t32[:, :], in_=tmp[:, :, :], axis=AX.X, op=ALU.add), r=[tmp], w=[t32])
            V(lambda e: e.tensor_scalar(out=t32[:, :], in0=t32[:, :], scalar1=0.0, scalar2=float(self.NSLOT - 1), op0=ALU.max, op1=ALU.min), r=[t32], w=[t32])
            V(lambda e, dsti=dsti: e.tensor_copy(out=dsti[:, j0:j0 + NJ], in_=t32[:, :]), r=[t32], w=[dsti])
        for j in range(j0, j0 + NJ):
            for dsti in (self.slotA, self.slotB):
                self.cx.dma("pool", None, None, reads=[self.h2c[j].b, dsti.b], writes=[],
                            fn=lambda e, dsti=dsti, j=j: e.indirect_dma_start(out=self.h2slots[:, :], out_offset=bass.IndirectOffsetOnAxis(ap=dsti[:, j:j + 1], axis=0),
                                                                              in_=self.h2all[:, j, :], in_offset=None))

    def p4_io(self):
        i = self.inp
        self.w_gate = i("exp_w_gate", [2, 16, D, 512])
        self.w_up = i("exp_w_up", [2, 16, D, 512])
        self.w_down = i("exp_w_down", [2, 16, 512, D])

    def zero_slots(self):
        self.push()
        zt = self.sb("zt", [128, D], F32)
        self.G(lambda e: e.memset(zt[:, :], 0.0), w=[zt])
        self.dma("sp", self.oslots[16 * self.CAP:16 * self.CAP + 128, :], zt[:, :], r=[zt], w=[self.oslots])
        self.pop()

    def p4_experts(self, l):
        B = self.bank
        V, S, G = self.V, self.S, self.G
        s = self.sb
        wg = [s(f"wg{i}", [128, 8, 512], BF16) for i in range(2)]
        wu = [s(f"wu{i}", [128, 8, 512], BF16) for i in range(2)]
        wd = [s(f"wd{i}", [128, 4, D], BF16) for i in range(2)]
        rt = [s(f"rtok{i}", [128, 4, D], BF16) for i in range(2)]
        rT = s("rT", [128, 8, 512], BF16)
        sil = [s(f"sil{i}", [128, 512], BF16) for i in range(2)]
        hidT = s("hidT", [128, 4, 512], BF16)
        osb = [s(f"eosb{i}", [128, D], F32) for i in range(2)]

        def load_w(e):
            i = e % 2
            self.dma("pool", wg[i][:, :, :], self.w_gate[l, e].rearrange("(k p) f -> p k f", p=128), r=[self.w_gate], w=[wg[i]])
            self.dma("pool", wu[i][:, :, :], self.w_up[l, e].rearrange("(k p) f -> p k f", p=128), r=[self.w_up], w=[wu[i]])
            self.dma("pool", wd[i][:, :, :], self.w_down[l, e].rearrange("(k p) f -> p k f", p=128), r=[self.w_down], w=[wd[i]])

        glist = [(e, off, nb) for e in range(16) for (off, nb) in self.GROUPS]
        load_w(0)
        ob = 0

        def load_rows(gi):
            e, off, nb = glist[gi]
            r0 = e * self.CAP + off
            rtk = rt[gi % 2]
            self.dma("sp", rtk[:, 0:nb, :], self.h2slots[r0:r0 + nb * 128, :].rearrange("(b p) d -> p b d", p=128), r=[self.h2slots], w=[rtk])
        load_rows(0)
        for gi, (e, off, nb) in enumerate(glist):
            if off == 0 and e + 1 < 16:
                load_w(e + 1)
            i = e % 2
            r0 = e * self.CAP + off
            N = nb * 128
            rtk = rt[gi % 2]
            if gi + 1 < len(glist):
                load_rows(gi + 1)
            for blk in range(nb):
                pT = B[blk % 2]
                pTb = pT[:, :].bitcast(BF16).rearrange("p (k t) -> p k t", k=8)
                for k in range(8):
                    self.tr(pTb[:, k, :], rtk[:, blk, k * 128:(k + 1) * 128], self.identb[:, :], r=[rtk, self.identb], w=[pT], signal=(k == 7))
                if blk % 2 == 0:
                    V(lambda e_, blk=blk, pTb=pTb: e_.tensor_copy(out=rT[:, :, blk * 128:(blk + 1) * 128], in_=pTb), r=[pT], w=[rT])
                else:
                    S(lambda e_, blk=blk, pTb=pTb: e_.copy(out=rT[:, :, blk * 128:(blk + 1) * 128], in_=pTb), r=[pT], w=[rT])
            for fc in range(4):
                pg, pu = B[2 + 2 * (fc % 2)], B[3 + 2 * (fc % 2)]
                for k in range(8):
                    self.mm(pg[:, 0:N], wg[i][:, k, fc * 128:(fc + 1) * 128], rT[:, k, 0:N], r=[wg[i], rT], w=[pg], start=(k == 0), stop=(k == 7))
                for k in range(8):
                    self.mm(pu[:, 0:N], wu[i][:, k, fc * 128:(fc + 1) * 128], rT[:, k, 0:N], r=[wu[i], rT], w=[pu], start=(k == 0), stop=(k == 7))
                sl = sil[fc % 2]
                S(lambda e_, sl=sl, pg=pg, N=N: e_.activation(out=sl[:, 0:N], in_=pg[:, 0:N], func=AF.Silu), r=[pg], w=[sl])
                V(lambda e_, sl=sl, pu=pu, fc=fc, N=N: e_.tensor_tensor(out=hidT[:, fc, 0:N], in0=pu[:, 0:N], in1=sl[:, 0:N], op=ALU.mult), r=[pu, sl], w=[hidT])
            for blk in range(nb):
                o = osb[ob % 2]
                ob += 1
                for half in range(2):
                    pd = B[6 + half]
                    for fc in range(4):
                        self.mm(pd[:, :], hidT[:, fc, blk * 128:(blk + 1) * 128], wd[i][:, fc, half * 512:(half + 1) * 512], r=[hidT, wd[i]], w=[pd],
                                start=(fc == 0), stop=(fc == 3))
                    V(lambda e_, o=o, pd=pd, half=half: e_.tensor_tensor(out=o[:, half * 512:(half + 1) * 512], in0=pd[:, :],
                                                                         in1=self.opg[:, 1, half * 512:(half + 1) * 512], op=ALU.mult), r=[pd, self.opg], w=[o], x=[o])
                self.dma("sp", self.oslots[r0 + blk * 128:r0 + (blk + 1) * 128, :], o[:, :], r=[o], w=[])

    def p5_alloc(self, l):
        s = self.sb
        self.lng2 = s("lng2", [128, D], F32); self.lnb2 = s("lnb2", [128, D], F32)
        self.dma("sp", self.lng2[:, :], self.ln2_g[l].partition_broadcast(128), r=[self.ln2_g], w=[self.lng2])
        self.dma("sp", self.lnb2[:, :], self.ln2_b[l].partition_broadcast(128), r=[self.ln2_b], w=[self.lnb2])
        self.rA = [s(f"rA{i}", [128, D], F32) for i in range(2)]
        self.rB = [s(f"rB{i}", [128, D], F32) for i in range(2)]
        self.x1r = [s(f"x1r{i}", [128, D], F32) for i in range(2)]
        self.x2t = [s(f"x2t{i}", [128, D], F32) for i in range(2)]
        self.st5 = s("st5", [128, 24], F32)
        self.y5 = [s(f"y5_{i}", [128, D], F32) for i in range(2)]

    def p5_loads(self, jj):
        if jj >= NCH:
            return
        rA_, rB_, x1r_ = self.rA[jj % 2], self.rB[jj % 2], self.x1r[jj % 2]
        self.cx.dma("pool", None, None, reads=[self.oslots.b, self.slotA.b], writes=[rA_.b],
                    fn=lambda e: e.indirect_dma_start(out=rA_[:, :], out_offset=None, in_=self.oslots[:, :],
                                                      in_offset=bass.IndirectOffsetOnAxis(ap=self.slotA[:, jj:jj + 1], axis=0)))
        self.cx.dma("pool", None, None, reads=[self.oslots.b, self.slotB.b], writes=[rB_.b],
                    fn=lambda e: e.indirect_dma_start(out=rB_[:, :], out_offset=None, in_=self.oslots[:, :],
                                                      in_offset=bass.IndirectOffsetOnAxis(ap=self.slotB[:, jj:jj + 1], axis=0)))
        self.dma("sp", x1r_[:, :], self.x1d[jj * 128:(jj + 1) * 128, :], r=[self.x1d], w=[x1r_])

    def p5_S1(self, j):
        if j >= NCH:
            return
        V, S, G = self.V, self.S, self.G
        rA, rB, x1r, y5 = self.rA[j % 2], self.rB[j % 2], self.x1r[j % 2], self.y5[j % 2]
        S(lambda e: e.activation(out=rA[:, :], in_=rA[:, :], func=AF.Identity, scale=self.gAB[:, 0, j:j + 1]), r=[rA, self.gAB], w=[rA])
        V(lambda e: e.scalar_tensor_tensor(out=rA[:, :], in0=rB[:, :], scalar=self.gAB[:, 1, j:j + 1], in1=rA[:, :], op0=ALU.mult, op1=ALU.add), r=[rB, rA, self.gAB], w=[rA])
        V(lambda e: e.scalar_tensor_tensor(out=y5[:, :], in0=x1r[:, :], scalar=float(ALPHA), in1=rA[:, :], op0=ALU.mult, op1=ALU.add), r=[x1r, rA], w=[y5], x=[rA])

    def p5_S2(self, j, dst):
        if j >= NCH or j < 0:
            return
        S = self.S
        y5, x2 = self.y5[j % 2], self.x2t[j % 2]
        self.ln_stats(y5, lambda a, b: y5[:, a:b], D, self.st5, act=True)
        S(lambda e: e.activation(out=x2[:, :], in_=y5[:, :], func=AF.Identity, bias=self.st5[:, 4:5], scale=self.st5[:, 3:4]), r=[y5, self.st5], w=[x2])

    def p5_S3(self, j, dst):
        if j >= NCH or j < 0:
            return
        V, G = self.V, self.G
        rows = slice(j * 128, (j + 1) * 128)
        x2 = self.x2t[j % 2]
        V(lambda e: e.tensor_tensor(out=x2[:, 0:512], in0=x2[:, 0:512], in1=self.lng2[:, 0:512], op=ALU.mult), r=[x2, self.lng2], w=[x2])
        G(lambda e: e.tensor_tensor(out=x2[:, 512:1024], in0=x2[:, 512:1024], in1=self.lng2[:, 512:1024], op=ALU.mult), r=[x2, self.lng2], w=[x2])
        V(lambda e: e.tensor_tensor(out=x2[:, 0:512], in0=x2[:, 0:512], in1=self.lnb2[:, 0:512], op=ALU.add), r=[x2, self.lnb2], w=[x2])
        G(lambda e: e.tensor_tensor(out=x2[:, 512:1024], in0=x2[:, 512:1024], in1=self.lnb2[:, 512:1024], op=ALU.add), r=[x2, self.lnb2], w=[x2])
        self.dma("sp", dst[rows, :], x2[:, :], r=[x2], w=[dst])

    def p5_all(self, l, dst):
        self.p5_loads(0); self.p5_loads(1)
        self.p5_S1(0)
        self.p5_loads(2)
        self.p5_S1(1)
        self.p5_S2(0, dst)
        for j in range(NCH):
            self.p5_loads(j + 3)
            self.p5_S1(j + 2)
            self.p5_S2(j + 1, dst)
            self.p5_S3(j, dst)

    def build(self):
        self.declare_io(); self.s5_io(); self.p3_io(); self.p4_io()
        self.setup()
        x_src = self.x_in
        for l in range(self.nlayers):
            dst = self.out if l == self.nlayers - 1 else self.xmid
            self.push()
            self.layer_alloc(); self.route_alloc(); self.layer_prep(l, 0)
            self.zero_slots()
            self.push(); self.qk_alloc()
            self.push(); self.s5_alloc(); self.s5_prep(l); self.load_win(l); self.p1v2_alloc()
            self.p1_all(l, x_src)
            self.pop()
            self.push(); self.p2_alloc(); self.p2_attention(); self.pop()
            self.pop()
            self.push()
            self.opg = self.sb("opg", [128, 2, 1024], F32)
            self.opg2 = self.sb("opg2", [128, 2, 1024], F32)
            self.layer_prep(l, 1)
            self.push(); self.p3_alloc(l)
            self.p3_route_alloc()
            self.p3_all(l, x_src)
            self.pop()
            self.push(); self.p4_experts(l); self.pop()
            self.push(); self.p5_alloc(l)
            self.p5_all(l, dst)
            self.pop()
            self.pop()
            self.pop()
            x_src = dst
        if getattr(self, "dbg_hook", None):
            self.dbg_hook(self)
        self.finish()


def make_inputs(inp, b):
    c = np.ascontiguousarray
    f = lambda k: np.asarray(inp[k])
    br, bi = f("ssm_b_re"), f("ssm_b_im")
    def bl(a):
        L = a.shape[0]
        return a.reshape(L, 2, 8, 64, 16).transpose(0, 2, 4, 1, 3).reshape(L, 128, 2, 64)
    bT = np.stack([bl(br), bl(bi)], axis=1)
    cr, ci = f("ssm_c_re"), f("ssm_c_im")
    cT = np.concatenate([cr.transpose(0, 1, 3, 2), ci.transpose(0, 1, 3, 2)], axis=2)
    L = br.shape[0]
    d = {
        "x": c(f("x")[b]), "ccol": c(f("c")[b].reshape(8, 128).T), "pos": c(f("positions")[b].reshape(32, 128).T),
        "ada_w": f("ada_w"), "ada_b": f("ada_b"), "w_in": f("w_in"), "gm_ln_g": f("gm_ln_g"), "gm_ln_b": f("gm_ln_b"),
        "gm_ws": f("gm_ws"), "gm_bsT": c(f("gm_bs").transpose(0, 2, 1)),
        "lam_re": c(f("ssm_lam_re").reshape(L, 1024)), "lam_im": c(f("ssm_lam_im").reshape(L, 1024)), "log_dt": f("ssm_log_dt"),
        "ssm_bT": c(bT), "ssm_cT": c(cT), "ssm_dT": c(f("ssm_d").reshape(L, 2, 128).transpose(0, 2, 1)),
        "glu_w": f("glu_w"), "glu_bT": c(f("glu_b").reshape(L, 2, 128).transpose(0, 2, 1)),
        "w_out": f("w_out"), "ln1_g": f("ln1_g"), "ln1_b": f("ln1_b"), "ln2_g": f("ln2_g"), "ln2_b": f("ln2_b"),
        "router_w": f("router_w"), "router_bias": f("router_bias"),
        "exp_w_gate": f("exp_w_gate"), "exp_w_up": f("exp_w_up"), "exp_w_down": f("exp_w_down"),
    }
    return d


_CACHE = {}


def kernel(**inputs):
    n = 8
    if "nc" not in _CACHE:
        nc = bass.Bass("TRN2", target_bir_lowering=False)
        kb = KB(nc)
        kb.build()
        _CACHE["nc"] = nc
        _CACHE["names"] = kb.in_names
    nc = _CACHE["nc"]
    names = _CACHE["names"]
    in_maps = []
    for b in range(n):
        im = make_inputs(inputs, b)
        in_maps.append({k: v for k, v in im.items() if k in names})
    res = run_bass_kernel_spmd(nc, in_maps, core_ids=list(range(n)))
    out = np.stack([np.asarray(r["out"]) for r in res.results], axis=0)
    return out.astype(np.float32)
```

```python
import numpy as np
import concourse.bass as bass
import concourse.mybir as mybir

F32 = mybir.dt.float32
BF16 = mybir.dt.bfloat16
I32 = mybir.dt.int32
U32 = mybir.dt.uint32
AF = mybir.ActivationFunctionType
ALU = mybir.AluOpType
AX = mybir.AxisListType


RELAXED = ()
ALLOW_RELAX = True


class Buf:
    __slots__ = ("w", "r", "name")

    def __init__(self, name=""):
        self.w = None
        self.r = []
        self.name = name


class Ctx:
    def __init__(self, nc, strict_same=False):
        self.nc = nc
        self.strict_same = strict_same
        self.relaxed = set(RELAXED)
        self.engs = {"pe": nc.tensor, "act": nc.scalar, "dve": nc.vector, "pool": nc.gpsimd, "sp": nc.sync}
        self.sem = {}
        self.cnt = {}
        for e in ("pe", "act", "dve", "pool"):
            self.sem[e] = nc.alloc_semaphore("s_" + e)
            self.cnt[e] = 0
        self.dq = {}
        for q, n in (("sp", 10), ("act", 4), ("pool", 8)):
            self.dq[q] = {"sems": [nc.alloc_semaphore(f"d_{q}{i}") for i in range(n)], "vals": [0] * n, "k": 0}
        self.waited = {}
        self.nbuf = 0
        self.out_events = []

    def buf(self, name=""):
        return Buf(name)

    def _wait(self, eng, ev):
        sem, val = ev
        key = (eng, id(sem))
        if self.waited.get(key, 0) >= val:
            return
        self.engs[eng].wait_ge(sem, val)
        self.waited[key] = val

    def _deps(self, eng, reads, writes, relax=()):
        own = self.sem.get(eng)
        rl = set(id(b) for b in relax) if ALLOW_RELAX else set()

        def chk(b, ev):
            if ev[0] is own and (eng == "pe" or id(b) in rl):
                return
            self._wait(eng, ev)
        for b in reads:
            if b.w is not None:
                chk(b, b.w)
        for b in writes:
            if b.w is not None:
                chk(b, b.w)
            for ev in b.r:
                chk(b, ev)

    def _commit(self, ev, reads, writes):
        for b in writes:
            b.w = ev
            b.r = []
        for b in reads:
            b.r.append(ev)
            if len(b.r) > 24:
                b.r = b.r[-24:]

    def op(self, eng, fn, reads=(), writes=(), signal=True, relax=()):
        self._deps(eng, reads, writes, relax)
        inst = fn(self.engs[eng])
        if signal:
            self.cnt[eng] += 1
            inst.then_inc(self.sem[eng], 1)
            ev = (self.sem[eng], self.cnt[eng])
        else:
            ev = (self.sem[eng], self.cnt[eng] + 1)
        self._commit(ev, reads, writes)
        return ev

    def dma(self, q, out, in_, reads=(), writes=(), fn=None, **kw):
        d = self.dq[q]
        i = d["k"] % len(d["sems"])
        d["k"] += 1
        sem = d["sems"][i]
        self._deps(q, reads, writes)
        if d["vals"][i] > 0:
            self._wait(q, (sem, d["vals"][i]))
        if fn is None:
            inst = self.engs[q].dma_start(out=out, in_=in_, **kw)
        else:
            inst = fn(self.engs[q])
        d["vals"][i] += 16
        inst.then_inc(sem, 16)
        ev = (sem, d["vals"][i])
        self._commit(ev, reads, writes)
        return ev

    def barrier(self):
        evs = [(self.sem[e], self.cnt[e]) for e in self.sem if self.cnt[e] > 0]
        for q, d in self.dq.items():
            for sem, v in zip(d["sems"], d["vals"]):
                if v > 0:
                    evs.append((sem, v))
        for eng in ("pe", "act", "dve", "pool", "sp"):
            own = self.sem.get(eng)
            for ev in evs:
                self._wait(eng, ev)

    def finish(self, bufs):
        for b in bufs:
            if b.w is not None:
                self._wait("sp", b.w)
            for ev in b.r:
                self._wait("sp", ev)

from concourse.bass_utils import run_bass_kernel_spmd
import math
import contextlib

T = 4096
D = 1024
NCH = 32
PW = 2304
EPS = 1e-5
ALPHA = (2.0 * 2) ** 0.25
TWO_PI = 2.0 * math.pi
ROPE_THETA = 500000.0


class Tl:
    def __init__(self, h, name=""):
        self.h = h
        self.b = Buf(name)

    def __getitem__(self, k):
        return self.h[k]


class KB:
    def __init__(self, nc, nlayers=2, dbg=(), stop_after=None):
        self.nc = nc
        self.cx = Ctx(nc)
        self.dbg = set(dbg)
        self.stop_after = stop_after
        self.nlayers = nlayers
        self.outs = []
        self.stk = [contextlib.ExitStack()]
        self.nps = 0

    def inp(self, name, shape, dt=F32):
        self.in_names = getattr(self, "in_names", set())
        self.in_names.add(name)
        return Tl(self.nc.dram_tensor(name, list(shape), dt, kind="ExternalInput").ap(), name)

    def outp(self, name, shape, dt=F32):
        t = Tl(self.nc.dram_tensor(name, list(shape), dt, kind="ExternalOutput").ap(), name)
        self.outs.append(t)
        return t

    def scratch(self, name, shape, dt):
        return Tl(self.nc.dram_tensor(name, list(shape), dt, kind="Internal").ap(), name)

    def sb(self, name, shape, dt):
        self.nsb = getattr(self, "nsb", 0) + 1
        h = self.stk[-1].enter_context(self.nc.sbuf_tensor(f"{name}_{self.nsb}", list(shape), dt))
        return Tl(h, name)

    def push(self):
        self.stk.append(contextlib.ExitStack())

    def pop(self):
        self.cx.barrier()
        self.stk.pop().close()

    def ps(self, name, shape, dt=F32):
        return Tl(self.nc.alloc_psum_tensor(name, list(shape), dt), name)

    def _rw(self, r, w):
        return [t.b for t in r], [t.b for t in w]

    def V(self, fn, r=(), w=(), x=()):
        r, w = self._rw(r, w)
        return self.cx.op("dve", fn, r, w, relax=[t.b for t in x])

    def S(self, fn, r=(), w=(), x=()):
        r, w = self._rw(r, w)
        return self.cx.op("act", fn, r, w, relax=[t.b for t in x])

    def G(self, fn, r=(), w=(), x=()):
        r, w = self._rw(r, w)
        return self.cx.op("pool", fn, r, w, relax=[t.b for t in x])

    def P(self, fn, r=(), w=(), signal=True):
        r, w = self._rw(r, w)
        return self.cx.op("pe", fn, r, w, signal=signal)

    def dma(self, q, out, in_, r=(), w=(), **kw):
        r, w = self._rw(r, w)
        return self.cx.dma(q, out, in_, r, w, **kw)

    def mm(self, out, lhsT, rhs, r, w, start, stop, signal=None):
        if signal is None:
            signal = stop
        return self.P(lambda e: e.matmul(out, lhsT, rhs, start=start, stop=stop), r, w, signal=signal)

    def tr(self, out, in_, ident, r, w, signal=True):
        return self.P(lambda e: e.transpose(out, in_, ident), r, w, signal=signal)

    def declare_io(self):
        i = self.inp
        self.x_in = i("x", [T, D])
        self.ccol = i("ccol", [128, 8])
        self.pos = i("pos", [128, NCH], I32)
        self.ada_w = i("ada_w", [2, D, 6 * D])
        self.ada_b = i("ada_b", [2, 6 * D])
        self.w_in = i("w_in", [2, D, PW])
        self.gm_ln_g = i("gm_ln_g", [2, 256])
        self.gm_ln_b = i("gm_ln_b", [2, 256])
        self.gm_ws = i("gm_ws", [2, 4, 128, 128])
        self.gm_bsT = i("gm_bsT", [2, 128, 4])
        self.out = self.outp("out", [T, D])
        self.mixtok = self.scratch("mixtok", [T, 1024], BF16)
        self.vd = self.scratch("vd", [T, 520], BF16)

    def consts(self):
        nc = self.nc
        self.identb = self.sb("identb", [128, 128], BF16)
        self.identf = self.sb("identf", [128, 128], F32)
        self.onesf = self.sb("onesf", [128, 128], F32)
        self.eps_t = self.sb("eps_t", [128, 1], F32)
        self.G(lambda e: e.memset(self.onesf[:, :], 1.0), w=[self.onesf])
        self.G(lambda e: e.memset(self.eps_t[:, :], EPS), w=[self.eps_t])
        self.mhalf = self.sb("mhalf", [128, 1], F32)
        self.G(lambda e: e.memset(self.mhalf[:, :], -0.5), w=[self.mhalf])
        self.G(lambda e: e.affine_select(out=self.identf[:, :], in_=self.onesf[:, :], pattern=[[-1, 128]],
                                         compare_op=ALU.is_equal, fill=0.0, base=0, channel_multiplier=1),
               r=[self.onesf], w=[self.identf])
        self.G(lambda e: e.tensor_copy(out=self.identb[:, :], in_=self.identf[:, :]), r=[self.identf], w=[self.identb])
        self.posf = self.sb("posf", [128, NCH], F32)
        self.posi = self.sb("posi", [128, NCH], I32)
        self.dma("sp", self.posi[:, :], self.pos[:, :], r=[self.pos], w=[self.posi])
        self.V(lambda e: e.tensor_copy(out=self.posf[:, :], in_=self.posi[:, :]), r=[self.posi], w=[self.posf])
        self.cs = self.sb("cs", [128, NCH, 8], F32)
        self.sn = self.sb("sn", [128, NCH, 8], F32)
        self.push()
        ang = self.sb("ang", [128, NCH, 8], F32)
        tmp = self.sb("angt", [128, NCH, 8], F32)
        tmi = self.sb("angi", [128, NCH, 8], I32)
        for j in range(8):
            fr = ROPE_THETA ** (-(j * 2.0) / 16.0)
            self.V(lambda e, j=j, fr=fr: e.tensor_scalar(out=ang[:, :, j], in0=self.posf[:, :], scalar1=float(fr), scalar2=None, op0=ALU.mult),
                   r=[self.posf], w=[ang])
        self.sincos(ang, self.sn, self.cs, tmp, tmi, [128, NCH * 8])
        self.pop()

    def _flat(self, t):
        ap = t[:]
        if len(ap.shape) == 2:
            return ap
        names = " ".join(f"a{i}" for i in range(len(ap.shape) - 1))
        return ap.rearrange(f"p {names} -> p ({names})")

    def range_reduce(self, src, dst, tmp, tmi, shift):
        s, d, t, ti = self._flat(src), self._flat(dst), self._flat(tmp), self._flat(tmi)
        self.V(lambda e: e.tensor_scalar(out=t, in0=s, scalar1=float(shift), scalar2=float(1.0 / TWO_PI), op0=ALU.add, op1=ALU.mult),
               r=[src], w=[tmp])
        self.V(lambda e: e.tensor_copy(out=ti, in_=t), r=[tmp], w=[tmi])
        self.V(lambda e: e.tensor_copy(out=t, in_=ti), r=[tmi], w=[tmp])
        self.V(lambda e: e.tensor_scalar(out=t, in0=t, scalar1=float(-TWO_PI), scalar2=float(shift), op0=ALU.mult, op1=ALU.add),
               r=[tmp], w=[tmp])
        self.V(lambda e: e.tensor_tensor(out=d, in0=t, in1=s, op=ALU.add), r=[tmp, src], w=[dst])
        self.V(lambda e: e.tensor_scalar(out=t, in0=d, scalar1=float(math.pi), scalar2=float(-TWO_PI), op0=ALU.is_gt, op1=ALU.mult),
               r=[dst], w=[tmp])
        self.V(lambda e: e.tensor_tensor(out=d, in0=d, in1=t, op=ALU.add), r=[tmp, dst], w=[dst])
        self.V(lambda e: e.tensor_scalar(out=t, in0=d, scalar1=float(-math.pi), scalar2=float(TWO_PI), op0=ALU.is_lt, op1=ALU.mult),
               r=[dst], w=[tmp])
        self.V(lambda e: e.tensor_tensor(out=d, in0=d, in1=t, op=ALU.add), r=[tmp, dst], w=[dst])
        self.V(lambda e: e.tensor_scalar(out=d, in0=d, scalar1=float(math.pi), scalar2=float(-math.pi), op0=ALU.min, op1=ALU.max),
               r=[dst], w=[dst])

    def sincos(self, ang, sn, cs, tmp, tmi, shape):
        self.range_reduce(ang, sn, tmp, tmi, 0.0)
        self.S(lambda e: e.activation(out=self._flat(sn), in_=self._flat(sn), func=AF.Sin), r=[sn], w=[sn])
        self.range_reduce(ang, cs, tmp, tmi, math.pi / 2)
        self.S(lambda e: e.activation(out=self._flat(cs), in_=self._flat(cs), func=AF.Sin), r=[cs], w=[cs])

    def setup(self):
        self.bank = [self.ps(f"bank{i}", [128, 512], F32) for i in range(8)]
        self.consts()

    def layer_alloc(self):
        self.modp = self.sb("modp", [128, 4, 8], F32)

        self.wsT = self.sb("wsT", [128, 4, 128], BF16)
        self.gbs = self.sb("gbs", [128, 4], F32)
        self.glng = self.sb("glng", [128, 256], F32)
        self.glnb = self.sb("glnb", [128, 256], F32)

    def prep_alloc(self):
        self.adaw = [self.sb(f"adaw{i}", [128, 8, 512], F32) for i in range(2)]
        self.adab = [self.sb(f"adab{i}", [128, 512], F32) for i in range(2)]
        self.modc = [self.sb(f"modc{i}", [128, 512], F32) for i in range(2)]
        self.wtmp = self.sb("wtmp", [128, 4, 128], F32)
        ccs = self.sb("ccs", [128, 8], F32)
        self.dma("sp", ccs[:, :], self.ccol[:, :], r=[self.ccol], w=[ccs])
        self.S(lambda e: e.activation(out=ccs[:, :], in_=ccs[:, :], func=AF.Silu), r=[ccs], w=[ccs])
        self.condrep = self.sb("condrep", [128, 8, 128], F32)
        self.V(lambda e: e.tensor_copy(out=self.condrep[:, :, :], in_=ccs[:, :].unsqueeze(2).to_broadcast([128, 8, 128])),
               r=[ccs], w=[self.condrep])


    def load_win(self, l):
        self.win = self.sb("win", [128, 8, PW], BF16)
        for k in range(8):
            self.dma("pool", self.win[:, k, :], self.w_in[l, k * 128:(k + 1) * 128, :], r=[self.w_in], w=[self.win])

    def layer_prep(self, l, part=0):
        self.push()
        self.prep_alloc()
        pb = self.bank[7]
        pt = self.bank[6]
        for n in range(12):
            if (part == 0) != (n // 2 in (0, 1)):
                continue
            aw = self.adaw[n % 2]
            ab = self.adab[n % 2]
            mc = self.modc[n % 2]
            self.dma("sp", aw[:, :, :], self.ada_w[l, :, n * 512:(n + 1) * 512].rearrange("(k p) n -> p k n", p=128),
                     r=[self.ada_w], w=[aw])
            self.dma("sp", ab[:, :], self.ada_b[l, n * 512:(n + 1) * 512].partition_broadcast(128), r=[self.ada_b], w=[ab])
            for k in range(8):
                self.mm(pb[:, :], self.condrep[:, k, :], aw[:, k, :], r=[self.condrep, aw], w=[pb], start=(k == 0), stop=(k == 7))
            which, half = n // 2, n % 2
            if which in (2, 5, 3, 4):
                tgt = self.opg if which in (2, 5) else self.opg2
                gi = {2: 0, 5: 1, 3: 0, 4: 1}[which]
                dst = tgt[:, gi, half * 512:(half + 1) * 512]
                self.V(lambda e, dst=dst: e.tensor_tensor(out=dst, in0=pb[:, :], in1=ab[:, :], op=ALU.add), r=[pb, ab], w=[tgt])
                if which != 3:
                    self.V(lambda e, dst=dst: e.tensor_scalar(out=dst, in0=dst, scalar1=1.0, scalar2=None, op0=ALU.add), r=[tgt], w=[tgt])
            else:
                slot = {0: 0, 1: 1}[which]
                self.V(lambda e: e.tensor_tensor(out=mc[:, :], in0=pb[:, :], in1=ab[:, :], op=ALU.add), r=[pb, ab], w=[mc])
                for b4 in range(4):
                    self.tr(pt[:, b4 * 128:(b4 + 1) * 128], mc[:, b4 * 128:(b4 + 1) * 128], self.identf[:, :], r=[mc, self.identf], w=[pt])
                addc = 1.0 if slot in (1, 3) else 0.0
                for b4 in range(4):
                    self.V(lambda e, b4=b4: e.tensor_scalar(out=self.modp[:, slot, half * 4 + b4:half * 4 + b4 + 1],
                                                            in0=pt[:, b4 * 128:b4 * 128 + 1], scalar1=float(addc), scalar2=None, op0=ALU.add),
                           r=[pt], w=[self.modp])
        if part == 0:
            self.dma("sp", self.wtmp[:, :, :], self.gm_ws[l].rearrange("h t s -> t h s"), r=[self.gm_ws], w=[self.wtmp])
            self.G(lambda e: e.affine_select(out=self.wtmp[:, :, :], in_=self.wtmp[:, :, :], pattern=[[0, 4], [-1, 128]],
                                             compare_op=ALU.is_ge, fill=0.0, base=0, channel_multiplier=1), r=[self.wtmp], w=[self.wtmp])
            for h in range(4):
                self.tr(pt[:, h * 128:(h + 1) * 128], self.wtmp[:, h, :], self.identf[:, :], r=[self.wtmp, self.identf], w=[pt])
            self.V(lambda e: e.tensor_copy(out=self.wsT[:, :, :], in_=pt[:, :].rearrange("p (h t) -> p h t", h=4)), r=[pt], w=[self.wsT])
            self.dma("sp", self.gbs[:, :], self.gm_bsT[l], r=[self.gm_bsT], w=[self.gbs])
            self.dma("sp", self.glng[:, :], self.gm_ln_g[l].partition_broadcast(128), r=[self.gm_ln_g], w=[self.glng])

            self.dma("sp", self.glnb[:, :], self.gm_ln_b[l].partition_broadcast(128), r=[self.gm_ln_b], w=[self.glnb])
        self.pop()

    def ln_stats(self, src, src_ap_fn, n, st, act=False):
        if act:
            return self.ln_stats_act(src, src_ap_fn, n, st)
        nchk = (n + 511) // 512
        w = n // nchk
        for i in range(nchk):
            self.V(lambda e, i=i: e.bn_stats(out=st[:, 8 + i * 6:8 + (i + 1) * 6], in_=src_ap_fn(i * w, (i + 1) * w)), r=[src], w=[st])
        self.V(lambda e: e.bn_aggr(out=st[:, 0:2], in_=st[:, 8:8 + 6 * nchk]), r=[st], w=[st])
        self.V(lambda e: e.tensor_scalar(out=st[:, 2:3], in0=st[:, 1:2], scalar1=float(EPS), scalar2=None, op0=ALU.add), r=[st], w=[st])
        self.G(lambda e: e.tensor_tensor(out=st[:, 3:4], in0=st[:, 2:3], in1=self.mhalf[:, 0:1], op=ALU.pow), r=[st, self.mhalf], w=[st])
        self.V(lambda e: e.tensor_scalar(out=st[:, 4:5], in0=st[:, 0:1], scalar1=-1.0, scalar2=st[:, 3:4], op0=ALU.mult, op1=ALU.mult), r=[st], w=[st])

    def ln_stats_act(self, src, src_ap_fn, n, st):
        nchk = (n + 511) // 512
        w = n // nchk
        for i in range(nchk):
            self.V(lambda e, i=i: e.bn_stats(out=st[:, 8 + i * 6:8 + (i + 1) * 6], in_=src_ap_fn(i * w, (i + 1) * w)), r=[src], w=[st])
        self.V(lambda e: e.bn_aggr(out=st[:, 0:2], in_=st[:, 8:8 + 6 * nchk]), r=[st], w=[st])
        self.S(lambda e: e.activation(out=st[:, 2:3], in_=st[:, 1:2], func=AF.Sqrt, bias=self.eps_t[:, 0:1], scale=1.0), r=[st, self.eps_t], w=[st])
        self.V(lambda e: e.reciprocal(out=st[:, 3:4], in_=st[:, 2:3]), r=[st], w=[st])
        self.V(lambda e: e.tensor_scalar(out=st[:, 4:5], in0=st[:, 0:1], scalar1=-1.0, scalar2=st[:, 3:4], op0=ALU.mult, op1=ALU.mult), r=[st], w=[st])

    def qk_alloc(self):
        self.QT = self.sb("QT", [128, 4, T], BF16)
        self.KT = self.sb("KT", [128, 4, T], BF16)

    def p1_alloc(self):
        s = self.sb
        self.xt = [s(f"xt{i}", [128, D], F32) for i in range(2)]
        self.xn = [s(f"xn{i}", [128, D], BF16) for i in range(2)]
        self.st = [s(f"st{i}", [128, 24], F32) for i in range(2)]
        self.st2 = [s(f"stb{i}", [128, 24], F32) for i in range(2)]
        self.hT = [s(f"hT{i}", [128, 8, 128], BF16) for i in range(2)]
        self.gl = [s(f"gl{i}", [128, 512], F32) for i in range(1)] * 2
        self.vnb = [s(f"vnb{i}", [128, 256], F32) for i in range(1)] * 2
        self.vb = [s(f"vb{i}", [128, 256], BF16) for i in range(1)] * 2
        self.aout = [s(f"aout{i}", [128, 256], BF16) for i in range(1)] * 2
        self.qf = [s(f"qf{i}", [128, 2, 512], F32) for i in range(1)] * 2
        self.qb = [s(f"qb{i}", [128, 2, 512], BF16) for i in range(1)] * 2
        self.rt = [s(f"rt{i}", [128, 4, 8, 8], F32) for i in range(1)] * 2
        self.vp = [s(f"vp{i}", [128, 8, 65], BF16) for i in range(1)] * 2
        for i in range(1):
            self.G(lambda e, i=i: e.memset(self.vp[i][:, :, :], 1.0), w=[self.vp[i]])

    def p1_chunk(self, l, j, x_src):
        i2 = j % 2
        xt, xn, st, st2, hT = self.xt[i2], self.xn[i2], self.st[i2], self.st2[i2], self.hT[i2]
        gl, vnb, vb, aout, qf, qb, rt, vp = self.gl[i2], self.vnb[i2], self.vb[i2], self.aout[i2], self.qf[i2], self.qb[i2], self.rt[i2], self.vp[i2]
        B = self.bank
        rows = slice(j * 128, (j + 1) * 128)
        if j == 0:
            self.dma("sp", xt[:, :], x_src[rows, :], r=[x_src], w=[xt])
        if j + 1 < NCH:
            xtn = self.xt[(j + 1) % 2]
            self.dma("sp", xtn[:, :], x_src[(j + 1) * 128:(j + 2) * 128, :], r=[x_src], w=[xtn])
        self.ln_stats(xt, lambda a, b: xt[:, a:b], D, st)
        self.S(lambda e: e.activation(out=xn[:, :], in_=xt[:, :], func=AF.Identity, bias=st[:, 4:5], scale=st[:, 3:4]), r=[xt, st], w=[xn])
        pT = B[0]
        pTb = pT[:, :].bitcast(BF16).rearrange("p (k t) -> p k t", k=8)
        for k in range(8):
            self.tr(pTb[:, k, :], xn[:, k * 128:(k + 1) * 128], self.identb[:, :], r=[xn, self.identb], w=[pT], signal=(k == 7))
        for k in range(8):
            self.V(lambda e, k=k: e.tensor_scalar(out=hT[:, k, :], in0=pTb[:, k, :], scalar1=self.modp[:, 1, k:k + 1], scalar2=self.modp[:, 0, k:k + 1],
                                                  op0=ALU.mult, op1=ALU.add), r=[pT, self.modp], w=[hT], x=[hT])
        for bi, c0 in ((1, 0), (2, 512), (3, 1024), (4, 1536)):
            for k in range(8):
                self.mm(B[bi][:, :], hT[:, k, :], self.win[:, k, c0:c0 + 512], r=[hT, self.win], w=[B[bi]], start=(k == 0), stop=(k == 7))
        self.S(lambda e: e.activation(out=gl[:, :], in_=B[1][:, :], func=AF.Gelu_apprx_tanh), r=[B[1]], w=[gl])
        self.ln_stats(gl, lambda a, b: gl[:, 256 + a:256 + b], 256, st2)
        self.V(lambda e: e.tensor_scalar(out=vnb[:, :], in0=gl[:, 256:512], scalar1=st2[:, 3:4], scalar2=st2[:, 4:5], op0=ALU.mult, op1=ALU.add),
               r=[gl, st2], w=[vnb])
        self.V(lambda e: e.tensor_tensor(out=vnb[:, :], in0=vnb[:, :], in1=self.glng[:, :], op=ALU.mult), r=[vnb, self.glng], w=[vnb], x=[vnb])
        self.V(lambda e: e.tensor_tensor(out=vb[:, :], in0=vnb[:, :], in1=self.glnb[:, :], op=ALU.add), r=[vnb, self.glnb], w=[vb], x=[vnb])
        psv = B[5]
        for h in range(4):
            self.mm(psv[:, h * 64:(h + 1) * 64], self.wsT[:, h, :], vb[:, h * 64:(h + 1) * 64], r=[self.wsT, vb], w=[psv], start=True, stop=True,
                    signal=(h == 3))
        for h in range(4):
            self.V(lambda e, h=h: e.scalar_tensor_tensor(out=aout[:, h * 64:(h + 1) * 64], in0=psv[:, h * 64:(h + 1) * 64], scalar=self.gbs[:, h:h + 1],
                                                         in1=gl[:, h * 64:(h + 1) * 64], op0=ALU.add, op1=ALU.mult), r=[psv, self.gbs, gl], w=[aout], x=[aout])
        self.dma("sp", self.mixtok[rows, 0:256], aout[:, :], r=[aout], w=[self.mixtok])
        self.S(lambda e: e.copy(out=qf[:, 0, :], in_=B[2][:, :]), r=[B[2]], w=[qf])
        self.S(lambda e: e.copy(out=qf[:, 1, :], in_=B[3][:, :]), r=[B[3]], w=[qf], x=[qf])
        q4 = qf[:, :, :].rearrange("p a (h e) -> p (a h) e", e=64)
        o4 = qb[:, :, :].rearrange("p a (h e) -> p (a h) e", e=64)
        for a in range(2):
            xa1, xa2 = q4[:, a * 8:(a + 1) * 8, 0:8], q4[:, a * 8:(a + 1) * 8, 8:16]
            cb = self.cs[:, j, :].unsqueeze(1).to_broadcast([128, 8, 8])
            sb_ = self.sn[:, j, :].unsqueeze(1).to_broadcast([128, 8, 8])
            oa = o4[:, a * 8:(a + 1) * 8, :]
            self.G(lambda e, xa1=xa1, cb=cb: e.tensor_tensor(out=rt[:, 0, :, :], in0=xa1, in1=cb, op=ALU.mult), r=[qf, self.cs], w=[rt])
            self.G(lambda e, xa2=xa2, sb_=sb_: e.tensor_tensor(out=rt[:, 1, :, :], in0=xa2, in1=sb_, op=ALU.mult), r=[qf, self.sn], w=[rt])
            self.G(lambda e, xa2=xa2, cb=cb: e.tensor_tensor(out=rt[:, 2, :, :], in0=xa2, in1=cb, op=ALU.mult), r=[qf, self.cs], w=[rt])
            self.G(lambda e, xa1=xa1, sb_=sb_: e.tensor_tensor(out=rt[:, 3, :, :], in0=xa1, in1=sb_, op=ALU.mult), r=[qf, self.sn], w=[rt])
            self.G(lambda e, oa=oa: e.tensor_tensor(out=oa[:, :, 0:8], in0=rt[:, 0, :, :], in1=rt[:, 1, :, :], op=ALU.subtract), r=[rt], w=[qb])
            self.G(lambda e, oa=oa: e.tensor_tensor(out=oa[:, :, 8:16], in0=rt[:, 2, :, :], in1=rt[:, 3, :, :], op=ALU.add), r=[rt], w=[qb])
            self.G(lambda e, oa=oa, a=a: e.tensor_copy(out=oa[:, :, 16:64], in_=q4[:, a * 8:(a + 1) * 8, 16:64]), r=[qf], w=[qb])
        pq = B[6]
        pqb = pq[:, :].bitcast(BF16).rearrange("p (a k t) -> p a k t", a=2, k=4)
        for a in range(2):
            for k in range(4):
                self.tr(pqb[:, a, k, :], qb[:, a, k * 128:(k + 1) * 128], self.identb[:, :], r=[qb, self.identb], w=[pq], signal=(a == 1 and k == 3))
        self.S(lambda e: e.copy(out=self.QT[:, :, j * 128:(j + 1) * 128], in_=pqb[:, 0, :, :]), r=[pq], w=[self.QT])
        self.S(lambda e: e.copy(out=self.KT[:, :, j * 128:(j + 1) * 128], in_=pqb[:, 1, :, :]), r=[pq], w=[self.KT])
        self.S(lambda e: e.copy(out=vp[:, :, 0:64], in_=B[4][:, :].rearrange("p (h e) -> p h e", e=64)), r=[B[4]], w=[vp])
        self.dma("sp", self.vd[rows, :], vp[:, :, :].rearrange("p h e -> p (h e)"), r=[vp], w=[self.vd])

    def finish(self):
        bufs = [t.b for t in self.outs]
        self.cx.finish(bufs)

    def p2_alloc(self):
        s = self.sb
        self.vbr = [s(f"vbr{i}", [128, 32, 520], BF16) for i in range(2)]
        self.negm = s("negm", [128, 256], BF16)
        negf = s("negf", [128, 256], F32)
        zf = s("zf", [128, 256], F32)
        self.G(lambda e: e.memset(zf[:, :], 0.0), w=[zf])
        self.G(lambda e: e.affine_select(out=negf[:, 0:128], in_=zf[:, 0:128], pattern=[[-1, 128]], compare_op=ALU.is_ge, fill=-30000.0,
                                         base=0, channel_multiplier=1), r=[zf], w=[negf])
        self.G(lambda e: e.affine_select(out=negf[:, 128:256], in_=zf[:, 128:256], pattern=[[1, 128]], compare_op=ALU.is_ge, fill=-30000.0,
                                         base=0, channel_multiplier=-1), r=[zf], w=[negf])
        self.G(lambda e: e.tensor_copy(out=self.negm[:, :], in_=negf[:, :]), r=[negf], w=[self.negm])
        self.m01 = s("m01", [128, 256], BF16)
        onef = s("onef2", [128, 256], F32)
        self.G(lambda e: e.memset(onef[:, :], 1.0), w=[onef])
        self.G(lambda e: e.affine_select(out=onef[:, 0:128], in_=onef[:, 0:128], pattern=[[-1, 128]], compare_op=ALU.is_ge, fill=0.0,
                                         base=0, channel_multiplier=1), r=[onef], w=[onef])
        self.G(lambda e: e.affine_select(out=onef[:, 128:256], in_=onef[:, 128:256], pattern=[[1, 128]], compare_op=ALU.is_ge, fill=0.0,
                                         base=0, channel_multiplier=-1), r=[onef], w=[onef])
        self.G(lambda e: e.tensor_copy(out=self.m01[:, :], in_=onef[:, :]), r=[onef], w=[self.m01])
        self.pexp = [s(f"pexp{i}", [128, 256], BF16) for i in range(6)]
        self.osb = [s(f"osb{i}", [128, 520], F32) for i in range(2)]

    def p2_attention(self):
        B = self.bank
        hb = 0
        self._hb = 0
        dils = (1, 4, 16)

        def load_vb(bi):
            d = dils[bi]
            vb_ = self.vbr[bi % 2]
            src = self.vd[:, :].rearrange("(n l r) c -> l n r c", l=128, r=d)
            for n in range(T // (128 * d)):
                self.dma("sp", vb_[:, n * d:(n + 1) * d, :], src[:, n, :, :], r=[self.vd], w=[vb_])
        load_vb(0)
        load_vb(1)
        for bi, d in enumerate(dils):
            seg = 128 * d
            nseg = T // seg
            vb = self.vbr[bi % 2]
            if bi == 1:
                load_vb(2)
            odst = self.obr[bi][:, :].rearrange("(n l r) c -> l n r c", l=128, r=d)
            blk = 0
            for n in range(nseg):
                for r_ in range(d):
                    cols = slice(n * seg + r_, (n + 1) * seg, d)
                    pcols = slice((n - 1) * seg + r_, n * seg, d)
                    bcur = n * d + r_
                    bprev = (n - 1) * d + r_
                    po = [B[6], B[7]]
                    osb = self.osb[blk % 2]
                    def scores(h):
                        nonlocal hb
                        hp, p0 = h // 2, (h % 2) * 64
                        ps = B[hb % 6]
                        o0 = 0
                        pe_ = self.pexp[hb % 6]
                        hb += 1
                        c0 = 0 if n > 0 else 128
                        if n > 0:
                            self.mm(ps[:, o0:o0 + 128], self.KT[p0:p0 + 64, hp, pcols], self.QT[p0:p0 + 64, hp, cols], r=[self.KT, self.QT], w=[ps],
                                    start=True, stop=True, signal=False)
                        self.mm(ps[:, o0 + 128:o0 + 256], self.KT[p0:p0 + 64, hp, cols], self.QT[p0:p0 + 64, hp, cols], r=[self.KT, self.QT], w=[ps],
                                start=True, stop=True)
                        self.S(lambda e, pe_=pe_, ps=ps, c0=c0, o0=o0: e.activation(out=pe_[:, c0:256], in_=ps[:, o0 + c0:o0 + 256], func=AF.Exp, scale=0.125),
                               r=[ps], w=[pe_])
                        mk = self.V if (h % 2 == 0) else self.G
                        mk(lambda e, pe_=pe_, c0=c0: e.tensor_tensor(out=pe_[:, c0:256], in0=pe_[:, c0:256], in1=self.m01[:, c0:256], op=ALU.mult),
                           r=[pe_, self.m01], w=[pe_])
                        return pe_

                    def pv(h, pe_):
                        pob = po[h // 4]
                        oc = slice((h % 4) * 65, (h % 4) * 65 + 65)
                        if n > 0:
                            self.mm(pob[:, oc], pe_[:, 0:128], vb[:, bprev, h * 65:(h + 1) * 65], r=[pe_, vb], w=[pob], start=True, stop=False)
                        self.mm(pob[:, oc], pe_[:, 128:256], vb[:, bcur, h * 65:(h + 1) * 65], r=[pe_, vb], w=[pob], start=(n == 0), stop=True)
                    pend = []
                    for h in range(8):
                        pend.append((h, scores(h)))
                        if len(pend) > 4:
                            pv(*pend.pop(0))
                    while pend:
                        pv(*pend.pop(0))
                    self.V(lambda e, osb=osb, po=po: e.tensor_copy(out=osb[:, 0:260], in_=po[0][:, 0:260]), r=[po[0]], w=[osb])
                    self.V(lambda e, osb=osb, po=po: e.tensor_copy(out=osb[:, 260:520], in_=po[1][:, 0:260]), r=[po[1]], w=[osb])
                    self.dma("sp", odst[:, n, r_, :], osb[:, :], r=[osb], w=[self.obr[bi]])
                    blk += 1

    def s5_io(self):
        i = self.inp
        self.lam_re = i("lam_re", [2, 1024])
        self.lam_im = i("lam_im", [2, 1024])
        self.log_dt = i("log_dt", [2, 16])
        self.ssm_bT = i("ssm_bT", [2, 2, 128, 2, 64])
        self.ssm_cT = i("ssm_cT", [2, 16, 128, 16])
        self.ssm_dT = i("ssm_dT", [2, 128, 2])
        self.glu_w = i("glu_w", [2, 256, 256])
        self.glu_bT = i("glu_bT", [2, 128, 2])
        self.coutT = self.scratch("coutT", [256, T], BF16)
        self.obr = [self.scratch(f"obr{i}", [T, 520], F32) for i in range(3)]

    def s5_alloc(self):
        s = self.sb
        self.Bblk = s("Bblk", [128, 2, 8, 2, 64], BF16)
        self.Cblk = s("Cblk", [128, 16, 128], BF16)
        self.Pre = s("Pre", [128, 16, 64], F32)
        self.PsT = s("PsT", [128, 16, 2, 64], F32)
        self.Qre = s("Qre", [128, 16, 64], F32)
        self.QsT = s("QsT", [128, 16, 2, 64], F32)
        self.glubh = s("glubh", [128, 2], F32)
        self.TriT = s("TriT", [128, 128], BF16)
        self.ones1 = s("ones1", [1, 128], BF16)
        self.dTt = s("dTt", [128, 2], F32)
        self.gluw = s("gluw", [128, 2, 256], BF16)
        self.glub = s("glub", [128, 2], F32)

    def s5_prep(self, l):
        s = self.sb
        V, S, G = self.V, self.S, self.G
        self.push()
        lre = s("lre", [128, 16, 64], F32)
        lim = s("lim", [128, 16, 64], F32)
        ldt = s("ldt", [128, 16], F32)
        self.dma("sp", lre[:, :, :].rearrange("p g n -> p (g n)"), self.lam_re[l].partition_broadcast(128), r=[self.lam_re], w=[lre])
        self.dma("sp", lim[:, :, :].rearrange("p g n -> p (g n)"), self.lam_im[l].partition_broadcast(128), r=[self.lam_im], w=[lim])
        self.dma("sp", ldt[:, :], self.log_dt[l].partition_broadcast(128), r=[self.log_dt], w=[ldt])
        S(lambda e: e.activation(out=ldt[:, :], in_=ldt[:, :], func=AF.Exp), r=[ldt], w=[ldt])
        dtb = ldt[:, :].unsqueeze(2).to_broadcast([128, 16, 64])
        lrd = s("lrd", [128, 16, 64], F32)
        lid = s("lid", [128, 16, 64], F32)
        V(lambda e: e.tensor_tensor(out=lrd[:, :, :], in0=lre[:, :, :], in1=dtb, op=ALU.mult), r=[lre, ldt], w=[lrd])
        V(lambda e: e.tensor_tensor(out=lid[:, :, :], in0=lim[:, :, :], in1=dtb, op=ALU.mult), r=[lim, ldt], w=[lid])
        sp1 = s("sp1", [128, 1], F32)
        G(lambda e: e.iota(sp1[:, :], pattern=[[0, 1]], base=1, channel_multiplier=1, allow_small_or_imprecise_dtypes=True), w=[sp1])
        E = s("E", [128, 16, 64], F32)
        An = s("An", [128, 16, 64], F32)
        sA = s("sA", [128, 16, 64], F32)
        cA = s("cA", [128, 16, 64], F32)
        tmp = s("s5tmp", [128, 16, 64], F32)
        tmi = s("s5tmi", [128, 16, 64], I32)
        qm = s("qm", [128, 16, 64], F32)
        pm = s("pm", [128, 16, 64], F32)
        V(lambda e: e.tensor_scalar(out=E[:, :, :], in0=lrd[:, :, :], scalar1=sp1[:, 0:1], scalar2=None, op0=ALU.mult), r=[lrd, sp1], w=[E])
        V(lambda e: e.tensor_scalar(out=An[:, :, :], in0=lid[:, :, :], scalar1=sp1[:, 0:1], scalar2=None, op0=ALU.mult), r=[lid, sp1], w=[An])
        self.sincos(An, sA, cA, tmp, tmi, None)
        S(lambda e: e.activation(out=qm[:, :, :], in_=E[:, :, :], func=AF.Exp), r=[E], w=[qm])
        S(lambda e: e.activation(out=pm[:, :, :], in_=E[:, :, :], func=AF.Exp, scale=-1.0), r=[E], w=[pm])
        V(lambda e: e.tensor_tensor(out=self.Qre[:, :, :], in0=qm[:, :, :], in1=cA[:, :, :], op=ALU.mult), r=[qm, cA], w=[self.Qre])
        V(lambda e: e.tensor_tensor(out=self.QsT[:, :, 1, :], in0=qm[:, :, :], in1=sA[:, :, :], op=ALU.mult), r=[qm, sA], w=[self.QsT])
        V(lambda e: e.tensor_scalar(out=self.QsT[:, :, 0, :], in0=self.QsT[:, :, 1, :], scalar1=-1.0, scalar2=None, op0=ALU.mult), r=[self.QsT], w=[self.QsT])
        V(lambda e: e.tensor_tensor(out=self.Pre[:, :, :], in0=pm[:, :, :], in1=cA[:, :, :], op=ALU.mult), r=[pm, cA], w=[self.Pre])
        V(lambda e: e.tensor_tensor(out=self.PsT[:, :, 0, :], in0=pm[:, :, :], in1=sA[:, :, :], op=ALU.mult), r=[pm, sA], w=[self.PsT])
        V(lambda e: e.tensor_scalar(out=self.PsT[:, :, 1, :], in0=self.PsT[:, :, 0, :], scalar1=-1.0, scalar2=None, op0=ALU.mult), r=[self.PsT], w=[self.PsT])
        self.sincos(lid, sA, cA, tmp, tmi, None)
        S(lambda e: e.activation(out=qm[:, :, :], in_=lrd[:, :, :], func=AF.Exp), r=[lrd], w=[qm])
        nr, ni = E, An
        V(lambda e: e.tensor_tensor(out=nr[:, :, :], in0=qm[:, :, :], in1=cA[:, :, :], op=ALU.mult), r=[qm, cA], w=[nr])
        V(lambda e: e.tensor_scalar(out=nr[:, :, :], in0=nr[:, :, :], scalar1=-1.0, scalar2=None, op0=ALU.add), r=[nr], w=[nr])
        V(lambda e: e.tensor_tensor(out=ni[:, :, :], in0=qm[:, :, :], in1=sA[:, :, :], op=ALU.mult), r=[qm, sA], w=[ni])
        m2 = pm
        V(lambda e: e.tensor_tensor(out=m2[:, :, :], in0=lre[:, :, :], in1=lre[:, :, :], op=ALU.mult), r=[lre], w=[m2])
        V(lambda e: e.tensor_tensor(out=tmp[:, :, :], in0=lim[:, :, :], in1=lim[:, :, :], op=ALU.mult), r=[lim], w=[tmp])
        V(lambda e: e.tensor_tensor(out=m2[:, :, :], in0=m2[:, :, :], in1=tmp[:, :, :], op=ALU.add), r=[m2, tmp], w=[m2])
        V(lambda e: e.reciprocal(out=m2[:, :, :], in_=m2[:, :, :]), r=[m2], w=[m2])
        fre, fim = sA, cA
        t1 = s("ft1", [128, 16, 64], F32)
        t2 = s("ft2", [128, 16, 64], F32)
        V(lambda e: e.tensor_tensor(out=t1[:, :, :], in0=nr[:, :, :], in1=lre[:, :, :], op=ALU.mult), r=[nr, lre], w=[t1])
        V(lambda e: e.tensor_tensor(out=t2[:, :, :], in0=ni[:, :, :], in1=lim[:, :, :], op=ALU.mult), r=[ni, lim], w=[t2])
        V(lambda e: e.tensor_tensor(out=t1[:, :, :], in0=t1[:, :, :], in1=t2[:, :, :], op=ALU.add), r=[t1, t2], w=[t1])
        V(lambda e: e.tensor_tensor(out=fre[:, :, :], in0=t1[:, :, :], in1=m2[:, :, :], op=ALU.mult), r=[t1, m2], w=[fre])
        V(lambda e: e.tensor_tensor(out=t1[:, :, :], in0=ni[:, :, :], in1=lre[:, :, :], op=ALU.mult), r=[ni, lre], w=[t1])
        V(lambda e: e.tensor_tensor(out=t2[:, :, :], in0=nr[:, :, :], in1=lim[:, :, :], op=ALU.mult), r=[nr, lim], w=[t2])
        V(lambda e: e.tensor_tensor(out=t1[:, :, :], in0=t1[:, :, :], in1=t2[:, :, :], op=ALU.subtract), r=[t1, t2], w=[t1])
        V(lambda e: e.tensor_tensor(out=fim[:, :, :], in0=t1[:, :, :], in1=m2[:, :, :], op=ALU.mult), r=[t1, m2], w=[fim])
        bT = s("bTt", [128, 2, 2, 64], F32)
        self.dma("sp", bT[:, :, :, :], self.ssm_bT[l].rearrange("r p k n -> p r k n"), r=[self.ssm_bT], w=[bT])
        bm = s("bmask", [128, 8], F32)
        one8 = s("one8", [128, 8], F32)
        G(lambda e: e.memset(one8[:, :], 1.0), w=[one8])
        G(lambda e: e.affine_select(out=bm[:, :], in_=one8[:, :], pattern=[[-16, 8]], compare_op=ALU.is_ge, fill=0.0, base=0, channel_multiplier=1),
          r=[one8], w=[bm])
        G(lambda e: e.affine_select(out=bm[:, :], in_=bm[:, :], pattern=[[16, 8]], compare_op=ALU.is_ge, fill=0.0, base=15, channel_multiplier=-1),
          r=[bm], w=[bm])
        bmb = bm[:, :].unsqueeze(2).to_broadcast([128, 8, 64])
        for kc in range(2):
            fr = fre[:, kc * 8:(kc + 1) * 8, :]
            fi = fim[:, kc * 8:(kc + 1) * 8, :]
            bre = bT[:, 0, kc, :].unsqueeze(1).to_broadcast([128, 8, 64])
            bim = bT[:, 1, kc, :].unsqueeze(1).to_broadcast([128, 8, 64])
            a1, a2 = t1[:, 0:8, :], t2[:, 0:8, :]
            V(lambda e, fr=fr, bre=bre: e.tensor_tensor(out=a1, in0=fr, in1=bre, op=ALU.mult), r=[fre, bT], w=[t1])
            V(lambda e, fi=fi, bim=bim: e.tensor_tensor(out=a2, in0=fi, in1=bim, op=ALU.mult), r=[fim, bT], w=[t2])
            V(lambda e: e.tensor_tensor(out=a1, in0=a1, in1=a2, op=ALU.subtract), r=[t1, t2], w=[t1])
            V(lambda e, kc=kc: e.tensor_tensor(out=self.Bblk[:, kc, :, 0, :], in0=a1, in1=bmb, op=ALU.mult), r=[t1, bm], w=[self.Bblk])
            V(lambda e, fr=fr, bim=bim: e.tensor_tensor(out=a1, in0=fr, in1=bim, op=ALU.mult), r=[fre, bT], w=[t1])
            V(lambda e, fi=fi, bre=bre: e.tensor_tensor(out=a2, in0=fi, in1=bre, op=ALU.mult), r=[fim, bT], w=[t2])
            V(lambda e: e.tensor_tensor(out=a1, in0=a1, in1=a2, op=ALU.add), r=[t1, t2], w=[t1])
            V(lambda e, kc=kc: e.tensor_tensor(out=self.Bblk[:, kc, :, 1, :], in0=a1, in1=bmb, op=ALU.mult), r=[t1, bm], w=[self.Bblk])
        cTs = s("cTs", [128, 16, 16], F32)
        self.dma("sp", cTs[:, :, :], self.ssm_cT[l].rearrange("g p c -> p g c"), r=[self.ssm_cT], w=[cTs])
        sg = s("sgn", [128, 1], F32)
        G(lambda e: e.memset(sg[0:64, :], 1.0), w=[sg])
        G(lambda e: e.memset(sg[64:128, :], -1.0), w=[sg])
        G(lambda e: e.memset(self.Cblk[:, :, :], 0.0), w=[self.Cblk])
        for g in range(16):
            c0 = (g % 8) * 16
            S(lambda e, g=g, c0=c0: e.activation(out=self.Cblk[:, g, c0:c0 + 16], in_=cTs[:, g, :], func=AF.Identity, scale=sg[:, 0:1]),
              r=[cTs, sg, self.Cblk], w=[self.Cblk])
        onesb = s("onesb", [128, 128], F32)
        G(lambda e: e.memset(onesb[:, :], 1.0), w=[onesb])
        G(lambda e: e.affine_select(out=onesb[:, :], in_=onesb[:, :], pattern=[[1, 128]], compare_op=ALU.is_ge, fill=0.0, base=0, channel_multiplier=-1),
          r=[onesb], w=[onesb])
        G(lambda e: e.tensor_copy(out=self.TriT[:, :], in_=onesb[:, :]), r=[onesb], w=[self.TriT])
        G(lambda e: e.memset(self.ones1[:, :], 1.0), w=[self.ones1])
        self.dma("sp", self.dTt[:, :], self.ssm_dT[l], r=[self.ssm_dT], w=[self.dTt])
        self.dma("sp", self.glub[:, :], self.glu_bT[l], r=[self.glu_bT], w=[self.glub])
        V(lambda e: e.tensor_scalar(out=self.glubh[:, :], in0=self.glub[:, :], scalar1=0.5, scalar2=None, op0=ALU.mult), r=[self.glub], w=[self.glubh])
        self.dma("pool", self.gluw[:, :, :], self.glu_w[l].rearrange("(k p) n -> p k n", p=128), r=[self.glu_w], w=[self.gluw])
        self.pop()

    def s5_chunk(self, l, j):
        B = self.bank
        V, S, G = self.V, self.S, self.G
        hT = self.hT[j % 2]
        uT = self.uT[j % 2]
        co = self.co[j % 2]
        ps_s = B[7]
        for cc in range(2):
            for k in range(8):
                self.mm(ps_s[:, cc * 128:(cc + 1) * 128], self.win[:, k, 2048 + cc * 128:2048 + (cc + 1) * 128], hT[:, k, :], r=[self.win, hT], w=[ps_s],
                        start=(k == 0), stop=(k == 7), signal=(k == 7 and cc == 1))
        S(lambda e: e.copy(out=uT[:, :, :], in_=ps_s[:, 0:256].rearrange("p (c t) -> p c t", c=2)), r=[ps_s], w=[uT])
        yps = B[7]
        for h in range(2):
            bu = [B[1], B[2]]
            zz = [B[3], B[4]]
            g0 = h * 8
            for q in range(2):
                self.mm(bu[q][:, :], uT[:, h, :], self.Bblk[:, h, q * 4:(q + 1) * 4, :, :].rearrange("p g r n -> p (g r n)"), r=[uT, self.Bblk], w=[bu[q]],
                        start=True, stop=True)
            t1, t2, vv = self.s5t1, self.s5t2, self.s5v
            for q in range(2):
                gs = slice(g0 + q * 4, g0 + q * 4 + 4)
                bu4 = bu[q][:, :].rearrange("p (g r n) -> p g r n", g=4, r=2)
                pc = self.Pre[:, gs, :].unsqueeze(2).to_broadcast([128, 4, 2, 64])
                V(lambda e, q=q, bu4=bu4, pc=pc: e.tensor_tensor(out=t1[:, q * 4:(q + 1) * 4, :, :], in0=bu4, in1=pc, op=ALU.mult), r=[bu[q], self.Pre], w=[t1], x=[t1])
                V(lambda e, q=q, bu4=bu4, gs=gs: e.tensor_tensor(out=t2[:, q * 4:(q + 1) * 4, :, :], in0=bu4[:, :, ::-1, :], in1=self.PsT[:, gs, :, :], op=ALU.mult),
                  r=[bu[q], self.PsT], w=[t2], x=[t2])
            G(lambda e: e.tensor_tensor(out=vv[:, :], in0=t1[:, :, :, :].rearrange("p g r n -> p (g r n)"), in1=t2[:, :, :, :].rearrange("p g r n -> p (g r n)"), op=ALU.add),
              r=[t1, t2], w=[vv])
            for q in range(2):
                self.mm(zz[q][:, :], self.TriT[:, :], vv[:, q * 512:(q + 1) * 512], r=[self.TriT, vv], w=[zz[q]], start=True, stop=False)
                self.mm(zz[q][:, :], self.ones1[:, :], self.x0row[h][:, q * 512:(q + 1) * 512], r=[self.ones1, self.x0row[h]], w=[zz[q]], start=False, stop=True)
            xs = self.s5x[h]
            for q in range(2):
                gs = slice(g0 + q * 4, g0 + q * 4 + 4)
                z4 = zz[q][:, :].rearrange("p (g r n) -> p g r n", g=4, r=2)
                qc = self.Qre[:, gs, :].unsqueeze(2).to_broadcast([128, 4, 2, 64])
                V(lambda e, q=q, z4=z4, qc=qc: e.tensor_tensor(out=t1[:, q * 4:(q + 1) * 4, :, :], in0=z4, in1=qc, op=ALU.mult), r=[zz[q], self.Qre], w=[t1], x=[t1])
                V(lambda e, q=q, z4=z4, gs=gs: e.tensor_tensor(out=t2[:, q * 4:(q + 1) * 4, :, :], in0=z4[:, :, ::-1, :], in1=self.QsT[:, gs, :, :], op=ALU.mult),
                  r=[zz[q], self.QsT], w=[t2], x=[t2])
            G(lambda e, xs=xs: e.tensor_tensor(out=xs[:, :, :].rearrange("p g m -> p (g m)"), in0=t1[:, :, :, :].rearrange("p g r n -> p (g r n)"),
                                        in1=t2[:, :, :, :].rearrange("p g r n -> p (g r n)"), op=ALU.add), r=[t1, t2], w=[xs])
            self.dma("act", self.x0row[h][0:1, :], xs[127:128, :, :].rearrange("p g m -> p (g m)"), r=[xs], w=[self.x0row[h]])
            pxt = B[0]
            pxb = pxt[:, :].bitcast(BF16).rearrange("p (g t) -> p g t", g=8)
            for g in range(8):
                self.tr(pxb[:, g, :], xs[:, g, :], self.identb[:, :], r=[xs, self.identb], w=[pxt], signal=(g == 7))
            V(lambda e, pxb=pxb: e.tensor_copy(out=self.s5xT[:, :, :], in_=pxb), r=[pxt], w=[self.s5xT])
            for g in range(8):
                self.mm(yps[:, 256 + h * 128:256 + (h + 1) * 128], self.Cblk[:, g0 + g, :], self.s5xT[:, g, :], r=[self.Cblk, self.s5xT], w=[yps],
                        start=(g == 0), stop=(g == 7))
        for cc in range(2):
            V(lambda e, cc=cc: e.scalar_tensor_tensor(out=self.yf[:, cc, :], in0=uT[:, cc, :], scalar=self.dTt[:, cc:cc + 1], in1=yps[:, 256 + cc * 128:256 + (cc + 1) * 128],
                                                      op0=ALU.mult, op1=ALU.add), r=[uT, self.dTt, yps], w=[self.yf], x=[self.yf])
        S(lambda e: e.activation(out=self.yg[:, :, :], in_=self.yf[:, :, :], func=AF.Gelu_apprx_tanh), r=[self.yf], w=[self.yg])
        gps = B[5]
        for c2 in range(2):
            for cc in range(2):
                self.mm(gps[:, c2 * 128:(c2 + 1) * 128], self.gluw[:, cc, c2 * 128:(c2 + 1) * 128], self.yg[:, cc, :], r=[self.gluw, self.yg], w=[gps],
                        start=(cc == 0), stop=(cc == 1))
        for c2 in range(2):
            S(lambda e, c2=c2: e.activation(out=self.sgm[:, c2, :], in_=gps[:, c2 * 128:(c2 + 1) * 128], func=AF.Sigmoid, bias=self.glub[:, c2:c2 + 1], scale=1.0),
              r=[gps, self.glub], w=[self.sgm])
        V(lambda e: e.tensor_tensor(out=co[:, :, :], in0=self.yg[:, :, :], in1=self.sgm[:, :, :], op=ALU.mult), r=[self.yg, self.sgm], w=[co])
        self.dma("sp", self.coutT[:, j * 128:(j + 1) * 128].rearrange("(c p) t -> p c t", p=128), co[:, :, :], r=[co], w=[self.coutT])

    def half(self, bank, lo):
        t = Tl(bank.h, bank.b.name + ("lo" if lo else "hi"))
        return t

    def p1v2_alloc(self):
        s = self.sb
        self.xt = [s(f"xt{i}", [128, D], F32) for i in range(2)]
        self.xn = [s(f"xn{i}", [128, D], BF16) for i in range(2)]
        self.st = [s(f"st{i}", [128, 24], F32) for i in range(2)]
        self.st2 = [s(f"stb{i}", [128, 24], F32) for i in range(2)]
        self.hT = [s(f"hT{i}", [128, 8, 128], BF16) for i in range(2)]
        self.gl = [s(f"gl{i}", [128, 512], F32) for i in range(2)]
        self.qbd = [s(f"qbd{i}", [128, 2, 512], BF16) for i in range(2)]
        self.vp = [s(f"vp{i}", [128, 8, 65], BF16) for i in range(2)]
        for i in range(2):
            self.G(lambda e, i=i: e.memset(self.vp[i][:, :, :], 1.0), w=[self.vp[i]])
        self.uT = [s(f"uT{i}", [128, 2, 128], BF16) for i in range(2)]
        self.vnb = s("vnb", [128, 256], F32)
        self.vb = s("vb", [128, 256], BF16)
        self.aout = s("aout", [128, 256], BF16)
        self.qb = s("qb", [128, 2, 512], BF16)
        self.rt = s("rt", [128, 4, 8, 8], F32)
        self.uD = [s(f"uD{i}", [128, 2, 128], F32) for i in range(3)]
        self.p1t = [s(f"p1t{i}", [128, 4, 2, 64], BF16) for i in range(2)]
        self.p2t = [s(f"p2t{i}", [128, 4, 2, 64], BF16) for i in range(2)]
        self.q1 = [s(f"q1_{i}", [128, 4, 2, 64], F32) for i in range(2)]
        self.q2 = [s(f"q2_{i}", [128, 4, 2, 64], F32) for i in range(2)]
        self.qv = [s(f"qv{i}", [128, 512], BF16) for i in range(2)]
        self.qx = [s(f"qx{i}", [128, 4, 128], BF16) for i in range(2)]
        self.qxT = [s(f"qxT{i}", [128, 4, 128], BF16) for i in range(3)]
        self.x0q = [s(f"x0q{i}", [1, 512], BF16) for i in range(4)]
        for i in range(4):
            self.G(lambda e, i=i: e.memset(self.x0q[i][:, :], 0.0), w=[self.x0q[i]])
        self.yf = s("yf2", [128, 2, 128], F32)
        self.yg = s("yg2", [128, 2, 128], BF16)
        self.sgm = s("sgm2", [128, 2, 128], F32)
        self.co = [s(f"co2_{i}", [128, 2, 128], BF16) for i in range(2)]
        B = self.bank
        self.b5lo, self.b5hi = Tl(B[5].h, "b5lo"), Tl(B[5].h, "b5hi")
        self.b6lo, self.b6hi = Tl(B[6].h, "b6lo"), Tl(B[6].h, "b6hi")
        self.b7lo, self.b7hi = Tl(B[7].h, "b7lo"), Tl(B[7].h, "b7hi")

    def p1_front_ln(self, l, j, x_src):
        if j >= NCH:
            return
        i2 = j % 2
        xt, xn, st = self.xt[i2], self.xn[i2], self.st[i2]
        S = self.S
        if j == 0:
            self.dma("sp", xt[:, :], x_src[0:128, :], r=[x_src], w=[xt])
            xt1 = self.xt[1]
            self.dma("sp", xt1[:, :], x_src[128:256, :], r=[x_src], w=[xt1])
        self.ln_stats(xt, lambda a, b: xt[:, a:b], D, st)
        S(lambda e: e.activation(out=xn[:, :], in_=xt[:, :], func=AF.Identity, bias=st[:, 4:5], scale=st[:, 3:4]), r=[xt, st], w=[xn])
        if j + 2 < NCH:
            self.dma("sp", xt[:, :], x_src[(j + 2) * 128:(j + 3) * 128, :], r=[x_src], w=[xt])

    def p1_front_a(self, l, j, x_src):
        if j >= NCH:
            return
        i2 = j % 2
        xn, hT = self.xn[i2], self.hT[i2]
        B = self.bank
        S = self.S
        pT = B[0]
        pTb = pT[:, :].bitcast(BF16).rearrange("p (k t) -> p k t", k=8)
        for k in range(8):
            self.tr(pTb[:, k, :], xn[:, k * 128:(k + 1) * 128], self.identb[:, :], r=[xn, self.identb], w=[pT], signal=(k == 7))
        for k in range(8):
            S(lambda e, k=k: e.activation(out=hT[:, k, :], in_=pTb[:, k, :], func=AF.Identity, scale=self.modp[:, 1, k:k + 1], bias=self.modp[:, 0, k:k + 1]),
              r=[pT, self.modp], w=[hT], x=[hT])

    def p1_front_s(self, l, j):
        if j >= NCH:
            return
        i2 = j % 2
        hT, uT = self.hT[i2], self.uT[i2]
        S = self.S
        ps_s = self.b6lo
        for cc in range(2):
            for k in range(8):
                self.mm(ps_s[:, cc * 128:(cc + 1) * 128], self.win[:, k, 2048 + cc * 128:2048 + (cc + 1) * 128], hT[:, k, :], r=[self.win, hT], w=[ps_s],
                        start=(k == 0), stop=(k == 7), signal=(k == 7 and cc == 1))
        S(lambda e: e.copy(out=uT[:, :, :], in_=ps_s[:, 0:256].rearrange("p (c t) -> p c t", c=2)), r=[ps_s], w=[uT])
        uD = self.uD[j % 3]
        for cc in range(2):
            S(lambda e, cc=cc: e.activation(out=uD[:, cc, :], in_=ps_s[:, cc * 128:(cc + 1) * 128], func=AF.Identity, scale=self.dTt[:, cc:cc + 1]), r=[ps_s, self.dTt], w=[uD], x=[uD])

    def p1_front_p(self, l, j, which):
        if j >= NCH:
            return
        i2 = j % 2
        hT, gl, qf, vp = self.hT[i2], self.gl[i2], self.qbd[i2], self.vp[i2]
        B = self.bank
        S = self.S

        def proj(bank, c0):
            for k in range(8):
                self.mm(bank[:, :], hT[:, k, :], self.win[:, k, c0:c0 + 512], r=[hT, self.win], w=[bank], start=(k == 0), stop=(k == 7))
        if which == 0:
            proj(B[1], 0)
            S(lambda e: e.activation(out=gl[:, :], in_=B[1][:, :], func=AF.Gelu_apprx_tanh), r=[B[1]], w=[gl])
        elif which == 1:
            proj(B[2], 512)
            S(lambda e: e.copy(out=qf[:, 0, :], in_=B[2][:, :]), r=[B[2]], w=[qf])
        elif which == 2:
            proj(B[1], 1024)
            S(lambda e: e.copy(out=qf[:, 1, :], in_=B[1][:, :]), r=[B[1]], w=[qf], x=[qf])
        else:
            proj(B[2], 1536)
            S(lambda e: e.copy(out=vp[:, :, 0:64], in_=B[2][:, :].rearrange("p (h e) -> p h e", e=64)), r=[B[2]], w=[vp])

    def p1_front(self, l, j, x_src):
        self.p1_front_a(l, j, x_src)
        self.p1_front_s(l, j)
        for w_ in range(4):
            self.p1_front_p(l, j, w_)

    def p1_back(self, l, j):
        if j < 0:
            return
        i2 = j % 2
        st2 = self.st2[i2]
        gl, qf, vp, uT = self.gl[i2], self.qbd[i2], self.vp[i2], self.uT[i2]
        vnb, vb, aout, qb, rt = self.vnb, self.vb, self.aout, self.qbd[i2], self.rt
        B = self.bank
        V, S, G = self.V, self.S, self.G
        rows = slice(j * 128, (j + 1) * 128)
        self.ln_stats(gl, lambda a, b: gl[:, 256 + a:256 + b], 256, st2)
        V(lambda e: e.tensor_scalar(out=vnb[:, :], in0=gl[:, 256:512], scalar1=st2[:, 3:4], scalar2=st2[:, 4:5], op0=ALU.mult, op1=ALU.add),
          r=[gl, st2], w=[vnb])
        V(lambda e: e.tensor_tensor(out=vnb[:, :], in0=vnb[:, :], in1=self.glng[:, :], op=ALU.mult), r=[vnb, self.glng], w=[vnb], x=[vnb])
        V(lambda e: e.tensor_tensor(out=vb[:, :], in0=vnb[:, :], in1=self.glnb[:, :], op=ALU.add), r=[vnb, self.glnb], w=[vb], x=[vnb])
        psv = self.b5lo
        for h in range(4):
            self.mm(psv[:, h * 64:(h + 1) * 64], self.wsT[:, h, :], vb[:, h * 64:(h + 1) * 64], r=[self.wsT, vb], w=[psv], start=True, stop=True,
                    signal=(h == 3))
        for h in range(4):
            V(lambda e, h=h: e.scalar_tensor_tensor(out=aout[:, h * 64:(h + 1) * 64], in0=psv[:, h * 64:(h + 1) * 64], scalar=self.gbs[:, h:h + 1],
                                                    in1=gl[:, h * 64:(h + 1) * 64], op0=ALU.add, op1=ALU.mult), r=[psv, self.gbs, gl], w=[aout], x=[aout])
        self.dma("sp", self.mixtok[rows, 0:256], aout[:, :], r=[aout], w=[self.mixtok])
        q4 = qf[:, :, :].rearrange("p a (h e) -> p (a h) e", e=64)
        o4 = qb[:, :, :].rearrange("p a (h e) -> p (a h) e", e=64)
        for a in range(2):
            xa1, xa2 = q4[:, a * 8:(a + 1) * 8, 0:8], q4[:, a * 8:(a + 1) * 8, 8:16]
            cb = self.cs[:, j, :].unsqueeze(1).to_broadcast([128, 8, 8])
            sb_ = self.sn[:, j, :].unsqueeze(1).to_broadcast([128, 8, 8])
            oa = o4[:, a * 8:(a + 1) * 8, :]
            G(lambda e, xa1=xa1, cb=cb: e.tensor_tensor(out=rt[:, 0, :, :], in0=xa1, in1=cb, op=ALU.mult), r=[qf, self.cs], w=[rt])
            G(lambda e, xa2=xa2, sb_=sb_: e.tensor_tensor(out=rt[:, 1, :, :], in0=xa2, in1=sb_, op=ALU.mult), r=[qf, self.sn], w=[rt])
            G(lambda e, xa2=xa2, cb=cb: e.tensor_tensor(out=rt[:, 2, :, :], in0=xa2, in1=cb, op=ALU.mult), r=[qf, self.cs], w=[rt])
            G(lambda e, xa1=xa1, sb_=sb_: e.tensor_tensor(out=rt[:, 3, :, :], in0=xa1, in1=sb_, op=ALU.mult), r=[qf, self.sn], w=[rt])
            G(lambda e, oa=oa: e.tensor_tensor(out=oa[:, :, 0:8], in0=rt[:, 0, :, :], in1=rt[:, 1, :, :], op=ALU.subtract), r=[rt], w=[qb])
            G(lambda e, oa=oa: e.tensor_tensor(out=oa[:, :, 8:16], in0=rt[:, 2, :, :], in1=rt[:, 3, :, :], op=ALU.add), r=[rt], w=[qb])
        pq = B[7]
        pqb = pq[:, :].bitcast(BF16).rearrange("p (a k t) -> p a k t", a=2, k=4)
        for a in range(2):
            for k in range(4):
                self.tr(pqb[:, a, k, :], qb[:, a, k * 128:(k + 1) * 128], self.identb[:, :], r=[qb, self.identb], w=[pq], signal=(a == 1 and k == 3))
        S(lambda e: e.copy(out=self.QT[:, :, j * 128:(j + 1) * 128], in_=pqb[:, 0, :, :]), r=[pq], w=[self.QT])
        S(lambda e: e.copy(out=self.KT[:, :, j * 128:(j + 1) * 128], in_=pqb[:, 1, :, :]), r=[pq], w=[self.KT])
        self.dma("sp", self.vd[rows, :], vp[:, :, :].rearrange("p h e -> p (h e)"), r=[vp], w=[self.vd])

    def s5A(self, i):
        if i < 0 or i >= 4 * NCH:
            return
        j, q = divmod(i, 4)
        kc = q // 2
        gs = slice(q * 4, q * 4 + 4)
        uT = self.uT[j % 2]
        bu = self.bank[3]
        t1, t2 = self.p1t[i % 2], self.p2t[i % 2]
        self.mm(bu[:, :], uT[:, kc, :], self.Bblk[:, kc, (q % 2) * 4:(q % 2) * 4 + 4, :, :].rearrange("p g r n -> p (g r n)"), r=[uT, self.Bblk], w=[bu],
                start=True, stop=True)
        bu4 = bu[:, :].rearrange("p (g r n) -> p g r n", g=4, r=2)
        pc = self.Pre[:, gs, :].unsqueeze(2).to_broadcast([128, 4, 2, 64])
        self.V(lambda e: e.tensor_tensor(out=t1[:, :, :, :], in0=bu4, in1=pc, op=ALU.mult), r=[bu, self.Pre], w=[t1])
        self.V(lambda e: e.tensor_tensor(out=t2[:, :, :, :], in0=bu4[:, :, ::-1, :], in1=self.PsT[:, gs, :, :], op=ALU.mult), r=[bu, self.PsT], w=[t2])

    def s5B(self, i):
        if i < 0 or i >= 4 * NCH:
            return
        j, q = divmod(i, 4)
        gs = slice(q * 4, q * 4 + 4)
        zz = self.bank[4]
        t1, t2 = self.p1t[i % 2], self.p2t[i % 2]
        u1, u2, xs = self.q1[i % 2], self.q2[i % 2], self.qx[i % 2]
        f = lambda t: t[:, :, :, :].rearrange("p g r n -> p (g r n)")
        self.mm(zz[:, :], self.TriT[:, :], f(t1), r=[self.TriT, t1], w=[zz], start=True, stop=False)
        self.mm(zz[:, :], self.TriT[:, :], f(t2), r=[self.TriT, t2], w=[zz], start=False, stop=False)
        self.mm(zz[:, :], self.ones1[:, :], self.x0q[q][:, :], r=[self.ones1, self.x0q[q]], w=[zz], start=False, stop=True)
        z4 = zz[:, :].rearrange("p (g r n) -> p g r n", g=4, r=2)
        qc = self.Qre[:, gs, :].unsqueeze(2).to_broadcast([128, 4, 2, 64])
        self.V(lambda e: e.tensor_tensor(out=u1[:, :, :, :], in0=z4, in1=qc, op=ALU.mult), r=[zz, self.Qre], w=[u1])
        self.V(lambda e: e.tensor_tensor(out=u2[:, :, :, :], in0=z4[:, :, ::-1, :], in1=self.QsT[:, gs, :, :], op=ALU.mult), r=[zz, self.QsT], w=[u2])
        self.G(lambda e: e.tensor_tensor(out=xs[:, :, :].rearrange("p g m -> p (g m)"), in0=f(u1), in1=f(u2), op=ALU.add), r=[u1, u2], w=[xs])
        self.dma("pool", self.x0q[q][0:1, :], xs[127:128, :, :].rearrange("p g m -> p (g m)"), r=[xs], w=[self.x0q[q]])

    def s5C(self, i):
        if i < 0 or i >= 4 * NCH:
            return
        xs, xT = self.qx[i % 2], self.qxT[i % 3]
        pxt = self.b6hi
        pxb = pxt[:, 256:512].bitcast(BF16).rearrange("p (g t) -> p g t", g=4)
        for g in range(4):
            self.tr(pxb[:, g, :], xs[:, g, :], self.identb[:, :], r=[xs, self.identb], w=[pxt], signal=(g == 3))
        self.S(lambda e: e.copy(out=xT[:, :, :], in_=pxb), r=[pxt], w=[xT])

    def s5D(self, i):
        if i < 0 or i >= 4 * NCH:
            return
        j, q = divmod(i, 4)
        kc = q // 2
        xT = self.qxT[i % 3]
        yps = self.b5hi
        for g in range(4):
            self.mm(yps[:, 256 + kc * 128:256 + (kc + 1) * 128], self.Cblk[:, q * 4 + g, :], xT[:, g, :], r=[self.Cblk, xT], w=[yps],
                    start=(q % 2 == 0 and g == 0), stop=(q % 2 == 1 and g == 3))

    def s5_step(self, i):
        self.s5A(i + 2)
        self.s5B(i + 1)
        self.s5C(i)
        if i % 2 == 0:
            self.s5D(i - 2)
            self.s5D(i - 1)

    def s5_tail(self, j):
        if j < 0 or j >= NCH:
            return
        V, S, G = self.V, self.S, self.G
        i2 = j % 2
        uT = self.uT[i2]
        yps = self.b5hi
        co = self.co[i2]
        for cc in range(2):
            V(lambda e, cc=cc: e.scalar_tensor_tensor(out=self.yf[:, cc, :], in0=self.uD[j % 3][:, cc, :], scalar=1.0, in1=yps[:, 256 + cc * 128:256 + (cc + 1) * 128],
                                                      op0=ALU.mult, op1=ALU.add), r=[self.uD[j % 3], yps], w=[self.yf], x=[self.yf])
        S(lambda e: e.activation(out=self.yg[:, :, :], in_=self.yf[:, :, :], func=AF.Gelu_apprx_tanh), r=[self.yf], w=[self.yg])
        gps = self.b5lo
        for c2 in range(2):
            for cc in range(2):
                self.mm(gps[:, c2 * 128:(c2 + 1) * 128], self.gluw[:, cc, c2 * 128:(c2 + 1) * 128], self.yg[:, cc, :], r=[self.gluw, self.yg], w=[gps],
                        start=(cc == 0), stop=(cc == 1))
        for c2 in range(2):
            S(lambda e, c2=c2: e.activation(out=self.sgm[:, c2, :], in_=gps[:, c2 * 128:(c2 + 1) * 128], func=AF.Tanh, bias=self.glubh[:, c2:c2 + 1], scale=0.5),
              r=[gps, self.glubh], w=[self.sgm])
        V(lambda e: e.scalar_tensor_tensor(out=self.sgm[:, :, :], in0=self.sgm[:, :, :], scalar=1.0, in1=self.yg[:, :, :], op0=ALU.add, op1=ALU.mult),
          r=[self.sgm, self.yg], w=[self.sgm])
        V(lambda e: e.tensor_scalar(out=co[:, :, :], in0=self.sgm[:, :, :], scalar1=0.5, scalar2=None, op0=ALU.mult), r=[self.sgm], w=[co])
        self.dma("sp", self.coutT[:, j * 128:(j + 1) * 128].rearrange("(c p) t -> p c t", p=128), co[:, :, :], r=[co], w=[self.coutT])

    def p1_all(self, l, x_src):
        self.p1_front_ln(l, 0, x_src)
        self.p1_front_ln(l, 1, x_src)
        self.p1_front(l, 0, x_src)
        self.s5A(0); self.s5A(1); self.s5B(0)
        for j in range(NCH):
            self.p1_front_ln(l, j + 2, x_src)
            self.p1_front(l, j + 1, x_src)
            self.p1_back(l, j)
            for q in range(4):
                self.s5_step(4 * j + q)
                if q == 0:
                    self.s5_tail(j - 1)
        self.s5_step(4 * NCH)
        self.s5_tail(NCH - 1)
        assert (4 * NCH) % 2 == 0

    CAP = 896
    NSLOT = 16 * 896 + 128
    GROUPS = ((0, 4), (512, 3))

    def p3_io(self):
        i = self.inp
        self.w_out = i("w_out", [2, D, D])
        self.ln1_g = i("ln1_g", [2, D]); self.ln1_b = i("ln1_b", [2, D])
        self.ln2_g = i("ln2_g", [2, D]); self.ln2_b = i("ln2_b", [2, D])
        self.router_w = i("router_w", [D, 16])
        self.router_bias = i("router_bias", [16])
        self.x1d = self.scratch("x1d", [T, D], F32)
        self.xmid = self.scratch("xmid", [T, D], F32)
        self.h2slots = self.scratch("h2slots", [self.NSLOT, D], BF16)
        self.oslots = self.scratch("oslots", [self.NSLOT, D], F32)

    def route_alloc(self):
        s = self.sb
        self.slotA = s("slotA", [128, NCH], I32)
        self.slotB = s("slotB", [128, NCH], I32)
        self.gAB = s("gAB", [128, 2, NCH], F32)

    def p3_alloc(self, l):
        s = self.sb
        self.wout = s("wout", [128, 8, D], BF16)
        for k in range(8):
            self.dma("pool", self.wout[:, k, :], self.w_out[l, k * 128:(k + 1) * 128, :], r=[self.w_out], w=[self.wout])
        self.lng = s("lng", [128, D], F32); self.lnb = s("lnb", [128, D], F32)
        self.dma("sp", self.lng[:, :], self.ln1_g[l].partition_broadcast(128), r=[self.ln1_g], w=[self.lng])
        self.dma("sp", self.lnb[:, :], self.ln1_b[l].partition_broadcast(128), r=[self.ln1_b], w=[self.lnb])
        self.rw = s("rw", [128, 8, 16], F32)
        self.dma("sp", self.rw[:, :, :], self.router_w[:, :].rearrange("(k p) e -> p k e", p=128), r=[self.router_w], w=[self.rw])
        self.rbias = s("rbias", [128, 16], F32)
        self.dma("sp", self.rbias[:, :], self.router_bias[:].partition_broadcast(128), r=[self.router_bias], w=[self.rbias])
        self.o3 = [s(f"o3_{i}", [128, 3, 520], F32) for i in range(2)]
        self.rec = s("rec", [128, 8], F32)
        self.mt = [s(f"mt{i}", [128, D], BF16) for i in range(2)]
        self.mixT = [s(f"mixT{i}", [128, 8, 128], BF16) for i in range(2)]
        self.xres = [s(f"xres{i}", [128, D], F32) for i in range(2)]
        self.yy = s("yy", [128, D], F32)
        self.x1t = [s(f"x1t{i}", [128, D], F32) for i in range(3)]
        self.cT = [s(f"cT{i}", [128, 2, 128], BF16) for i in range(2)]
        self.st3b = s("st3b", [128, 24], F32)
        self.h2f = s("h2f", [128, D], F32)
        self.h2f2 = [self.h2f, s("h2fb", [128, D], F32)]
        self.h2all = s("h2all", [128, NCH, D], BF16)
        self.h2c = [Tl(self.h2all.h, f"h2c{j}") for j in range(NCH)]
        self.scall = s("scall", [128, NCH, 16], F32)
        self.h2T = s("h2T", [128, 8, 128], F32)
        self.st3 = s("st3", [128, 24], F32)
        self.trs = s("trs", [128, 128], F32)
        self.eoff = s("eoff", [128, 16], F32)
        self.trashp = s("trashp", [128, 1], F32)
        self.ones16 = s("ones16", [128, 16], F32)
        G = self.G
        G(lambda e: e.memset(self.ones16[:, :], 1.0), w=[self.ones16])
        G(lambda e: e.affine_select(out=self.trs[:, :], in_=self.onesf[:, :], pattern=[[1, 128]], compare_op=ALU.is_ge, fill=0.0, base=-1, channel_multiplier=-1),
          r=[self.onesf], w=[self.trs])
        G(lambda e: e.iota(self.eoff[:, :], pattern=[[self.CAP, 16]], base=0, channel_multiplier=0, allow_small_or_imprecise_dtypes=True), w=[self.eoff])
        G(lambda e: e.iota(self.trashp[:, :], pattern=[[0, 1]], base=16 * self.CAP, channel_multiplier=1, allow_small_or_imprecise_dtypes=True), w=[self.trashp])

    def p3_L1(self, k):
        if k >= NCH:
            return
        rws = slice(k * 128, (k + 1) * 128)
        o3_, mt_ = self.o3[k % 2], self.mt[k % 2]
        for bi in range(3):
            self.dma("sp", o3_[:, bi, :], self.obr[bi][rws, :], r=[self.obr[bi]], w=[o3_])
        self.dma("sp", mt_[:, 0:256], self.mixtok[rws, 0:256], r=[self.mixtok], w=[mt_])

    def p3_L2(self, k, x_src):
        if k >= NCH:
            return
        rws = slice(k * 128, (k + 1) * 128)
        self.dma("sp", self.cT[k % 2][:, :, :], self.coutT[:, rws].rearrange("(c p) t -> p c t", p=128), r=[self.coutT], w=[self.cT[k % 2]])
        self.dma("sp", self.xres[k % 2][:, :], x_src[rws, :], r=[x_src], w=[self.xres[k % 2]])

    def p3_S1(self, j):
        if j >= NCH or j < 0:
            return
        B = self.bank
        V, S, G = self.V, self.S, self.G
        o3, rec, mt = self.o3[j % 2], self.rec, self.mt[j % 2]
        mixT = self.mixT[j % 2]
        V(lambda e: e.tensor_tensor(out=o3[:, 0, :], in0=o3[:, 0, :], in1=o3[:, 1, :], op=ALU.add), r=[o3], w=[o3], x=[o3])
        V(lambda e: e.tensor_tensor(out=o3[:, 0, :], in0=o3[:, 0, :], in1=o3[:, 2, :], op=ALU.add), r=[o3], w=[o3], x=[o3])
        o8 = o3[:, 0, :].rearrange("p (h e) -> p h e", e=65)
        V(lambda e: e.reciprocal(out=rec[:, :], in_=o8[:, :, 64]), r=[o3], w=[rec])
        V(lambda e: e.tensor_tensor(out=mt[:, 256:768].rearrange("p (h e) -> p h e", e=64), in0=o8[:, :, 0:64], in1=rec[:, :].unsqueeze(2).to_broadcast([128, 8, 64]),
                                    op=ALU.mult), r=[o3, rec], w=[mt])
        pT = B[0]
        pTb = pT[:, :].bitcast(BF16).rearrange("p (k t) -> p k t", k=8)
        for k in range(6):
            self.tr(pTb[:, k, :], mt[:, k * 128:(k + 1) * 128], self.identb[:, :], r=[mt, self.identb], w=[pT], signal=(k == 5))
        S(lambda e: e.copy(out=mixT[:, 0:6, :], in_=pTb[:, 0:6, :]), r=[pT], w=[mixT])

    def p3_S2(self, j):
        if j >= NCH or j < 0:
            return
        B = self.bank
        V, S, G = self.V, self.S, self.G
        rows = slice(j * 128, (j + 1) * 128)
        mixT, cT, xres = self.mixT[j % 2], self.cT[j % 2], self.xres[j % 2]
        wb = [B[1], B[2]] if j % 2 == 0 else [B[6], B[7]]
        for nb in range(2):
            for k in range(8):
                lhs = mixT[:, k, :] if k < 6 else cT[:, k - 6, :]
                self.mm(wb[nb][:, :], lhs, self.wout[:, k, nb * 512:(nb + 1) * 512], r=[mixT, cT, self.wout], w=[wb[nb]], start=(k == 0), stop=(k == 7))
        yy = self.yy
        for nb in range(2):
            cs_ = slice(nb * 512, (nb + 1) * 512)
            V(lambda e, nb=nb, cs_=cs_: e.tensor_tensor(out=yy[:, cs_], in0=wb[nb][:, :], in1=self.opg[:, 0, cs_], op=ALU.mult), r=[wb[nb], self.opg], w=[yy], x=[yy])
        V(lambda e: e.scalar_tensor_tensor(out=yy[:, :], in0=xres[:, :], scalar=float(ALPHA), in1=yy[:, :], op0=ALU.mult, op1=ALU.add), r=[xres, yy], w=[yy], x=[yy])
        self.ln_stats(yy, lambda a, b: yy[:, a:b], D, self.st3, act=True)
        x1 = self.x1t[j % 3]
        S(lambda e: e.activation(out=x1[:, :], in_=yy[:, :], func=AF.Identity, bias=self.st3[:, 4:5], scale=self.st3[:, 3:4]), r=[yy, self.st3], w=[x1])

    def p3_S2b(self, j):
        if j >= NCH or j < 0:
            return
        V = self.V
        rows = slice(j * 128, (j + 1) * 128)
        x1 = self.x1t[j % 3]
        V(lambda e: e.tensor_tensor(out=x1[:, :], in0=x1[:, :], in1=self.lng[:, :], op=ALU.mult), r=[x1, self.lng], w=[x1])
        V(lambda e: e.tensor_tensor(out=x1[:, :], in0=x1[:, :], in1=self.lnb[:, :], op=ALU.add), r=[x1, self.lnb], w=[x1], x=[x1])
        self.dma("sp", self.x1d[rows, :], x1[:, :], r=[x1], w=[self.x1d])

    def p3_S3a(self, j):
        if j >= NCH or j < 0:
            return
        S = self.S
        x1 = self.x1t[j % 3]
        h2f = self.h2f2[j % 2]
        self.ln_stats(x1, lambda a, b: x1[:, a:b], D, self.st3b, act=True)
        S(lambda e: e.activation(out=h2f[:, :], in_=x1[:, :], func=AF.Identity, bias=self.st3b[:, 4:5], scale=self.st3b[:, 3:4]), r=[x1, self.st3b], w=[h2f])

    def p3_S3b(self, j):
        if j >= NCH or j < 0:
            return
        B = self.bank
        V, S, G = self.V, self.S, self.G
        h2f = self.h2f2[j % 2]
        V(lambda e: e.tensor_tensor(out=h2f[:, :], in0=h2f[:, :], in1=self.opg2[:, 1, :], op=ALU.mult), r=[h2f, self.opg2], w=[h2f])
        V(lambda e: e.tensor_tensor(out=h2f[:, :], in0=h2f[:, :], in1=self.opg2[:, 0, :], op=ALU.add), r=[h2f, self.opg2], w=[h2f], x=[h2f])
        S(lambda e: e.copy(out=self.h2all[:, j, :], in_=h2f[:, :]), r=[h2f], w=[self.h2c[j]])
        for half in range(2):
            pt = B[3 + half]
            for k in range(4):
                self.tr(pt[:, k * 128:(k + 1) * 128], h2f[:, (half * 4 + k) * 128:(half * 4 + k + 1) * 128], self.identf[:, :], r=[h2f, self.identf], w=[pt], signal=(k == 3))
            S(lambda e, half=half, pt=pt: e.copy(out=self.h2T[:, half * 4:(half + 1) * 4, :], in_=pt[:, :].rearrange("p (k t) -> p k t", k=4)), r=[pt], w=[self.h2T])
        lg = B[5]
        for k in range(8):
            self.mm(lg[:, 0:16], self.h2T[:, k, :], self.rw[:, k, :], r=[self.h2T, self.rw], w=[lg], start=(k == 0), stop=(k == 7))
        S(lambda e: e.copy(out=self.scall[:, j, :], in_=lg[:, 0:16]), r=[lg], w=[self.scall])
        if (j + 1) % self.RSEG == 0:
            self.p3_route(j + 1 - self.RSEG)

    def p3_all(self, l, x_src):
        self.p3_L1(0)
        for j in range(-2, NCH + 2):
            self.p3_L1(j + 3)
            self.p3_L2(j + 2, x_src)
            self.p3_S1(j + 2)
            self.p3_S2(j + 1)
            self.p3_S2b(j)
            self.p3_S3a(j - 1)
            self.p3_S3b(j - 2)

    RSEG = 8

    def p3_route_alloc(self):
        s = self.sb
        NJ = self.RSEG
        mk = lambda n: s(n, [128, NJ, 16], F32)
        t = {}
        t["big"] = [mk(f"r_{i}") for i in range(14)]
        t["g4"] = [s(f"r4_{i}", [128, NJ, 4], F32) for i in range(4)]
        t["g1"] = [s(f"r1_{i}", [128, NJ], F32) for i in range(3)]
        t["mask_e0"] = mk("mask_e0")
        t["mask_j0"] = s("mask_j0", [128, 16, NJ], F32)
        G = self.G
        G(lambda e: e.memset(t["mask_e0"][:, :, :], 1.0), w=[t["mask_e0"]])
        G(lambda e: e.memset(t["mask_e0"][:, :, 0:1], 0.0), w=[t["mask_e0"]])
        G(lambda e: e.memset(t["mask_j0"][:, :, :], 1.0), w=[t["mask_j0"]])
        G(lambda e: e.memset(t["mask_j0"][:, :, 0:1], 0.0), w=[t["mask_j0"]])
        self.carry = s("carry", [128, 16], F32)
        G(lambda e: e.memset(self.carry[:, :], 0.0), w=[self.carry])
        self._rt = t

    def p3_route(self, j0):
        B = self.bank
        V, S, G = self.V, self.S, self.G
        NJ = self.RSEG
        t = self._rt
        sel, eq, msk, top2, chosen, gw, tmp, cum, pos, valid, slotv, baseT, totT, sc = t["big"]
        m1, m2, gs, gsel = t["g4"]
        gmax, gsum, t32 = t["g1"]
        mask_e0, mask_j0 = t["mask_e0"], t["mask_j0"]
        f2 = lambda t_: t_[:, :, :].rearrange("p j e -> p (j e)")
        S(lambda e: e.activation(out=f2(sc), in_=self.scall[:, j0:j0 + NJ, :].rearrange("p j e -> p (j e)"), func=AF.Sigmoid), r=[self.scall], w=[sc])
        g4 = lambda t: t[:, :, :].rearrange("p j (g i) -> p (j g) i", i=4)
        b4 = lambda t: t[:, :, :].rearrange("p j g -> p (j g)").unsqueeze(2).to_broadcast([128, NJ * 4, 4])
        V(lambda e: e.tensor_tensor(out=sel[:, :, :], in0=sc[:, :, :], in1=self.rbias[:, :].unsqueeze(1).to_broadcast([128, NJ, 16]), op=ALU.add), r=[sc, self.rbias], w=[sel])
        V(lambda e: e.tensor_reduce(out=m1[:, :, :].rearrange("p j g -> p (j g)"), in_=g4(sel), axis=AX.X, op=ALU.max), r=[sel], w=[m1])
        V(lambda e: e.tensor_tensor(out=g4(eq), in0=g4(sel), in1=b4(m1), op=ALU.is_equal), r=[sel, m1], w=[eq])
        V(lambda e: e.scalar_tensor_tensor(out=f2(msk), in0=f2(eq), scalar=-1e9, in1=f2(sel), op0=ALU.mult, op1=ALU.add), r=[eq, sel], w=[msk])
        V(lambda e: e.tensor_reduce(out=m2[:, :, :].rearrange("p j g -> p (j g)"), in_=g4(msk), axis=AX.X, op=ALU.max), r=[msk], w=[m2])
        V(lambda e: e.tensor_tensor(out=gs[:, :, :], in0=m1[:, :, :], in1=m2[:, :, :], op=ALU.add), r=[m1, m2], w=[gs])
        V(lambda e: e.tensor_reduce(out=gmax[:, :], in_=gs[:, :, :], axis=AX.X, op=ALU.max), r=[gs], w=[gmax])
        V(lambda e: e.tensor_tensor(out=gsel[:, :, :], in0=gs[:, :, :], in1=gmax[:, :].unsqueeze(2).to_broadcast([128, NJ, 4]), op=ALU.is_equal), r=[gs, gmax], w=[gsel])
        V(lambda e: e.tensor_tensor(out=g4(top2), in0=g4(sel), in1=b4(m2), op=ALU.is_ge), r=[sel, m2], w=[top2])
        V(lambda e: e.tensor_tensor(out=g4(chosen), in0=g4(top2), in1=b4(gsel), op=ALU.mult), r=[top2, gsel], w=[chosen])
        V(lambda e: e.tensor_tensor(out=gw[:, :, :], in0=chosen[:, :, :], in1=sc[:, :, :], op=ALU.mult), r=[chosen, sc], w=[gw])
        V(lambda e: e.tensor_reduce(out=gsum[:, :], in_=gw[:, :, :], axis=AX.X, op=ALU.add), r=[gw], w=[gsum])
        V(lambda e: e.reciprocal(out=gsum[:, :], in_=gsum[:, :]), r=[gsum], w=[gsum])
        V(lambda e: e.tensor_tensor(out=gw[:, :, :], in0=gw[:, :, :], in1=gsum[:, :].unsqueeze(2).to_broadcast([128, NJ, 16]), op=ALU.mult), r=[gw, gsum], w=[gw])
        cbk = B[5]
        W = NJ * 16
        self.mm(cbk[:, 128:128 + W], self.trs[:, :], f2(chosen), r=[self.trs, chosen], w=[cbk], start=True, stop=True)
        self.mm(cbk[:, 256:256 + W], self.onesf[:, :], f2(chosen), r=[self.onesf, chosen], w=[cbk], start=True, stop=True)
        V(lambda e: e.tensor_copy(out=f2(totT).rearrange("p (e j) -> p e j", e=16),
                                  in_=cbk[:, 256:256 + W].rearrange("p (j e) -> p e j", e=16)), r=[cbk], w=[totT])
        V(lambda e: e.tensor_tensor_scan(out=f2(baseT), data0=mask_j0[:, :, :].rearrange("p e j -> p (e j)"), data1=f2(totT), initial=0.0, op0=ALU.mult, op1=ALU.add),
          r=[mask_j0, totT], w=[baseT])
        V(lambda e: e.tensor_tensor(out=f2(baseT), in0=f2(baseT), in1=f2(totT), op=ALU.subtract), r=[baseT, totT], w=[baseT])
        bT3 = f2(baseT).rearrange("p (e j) -> p e j", e=16)
        tT3 = f2(totT).rearrange("p (e j) -> p e j", e=16)
        V(lambda e: e.tensor_tensor(out=bT3, in0=bT3, in1=self.carry[:, :].unsqueeze(2).to_broadcast([128, 16, NJ]), op=ALU.add), r=[baseT, self.carry], w=[baseT])
        V(lambda e: e.tensor_tensor(out=self.carry[:, :], in0=bT3[:, :, NJ - 1], in1=tT3[:, :, NJ - 1], op=ALU.add), r=[baseT, totT], w=[self.carry])
        V(lambda e: e.tensor_tensor(out=pos[:, :, :], in0=cbk[:, 128:128 + W].rearrange("p (j e) -> p j e", e=16),
                                    in1=f2(baseT).rearrange("p (e j) -> p j e", e=16), op=ALU.add), r=[cbk, baseT], w=[pos])
        V(lambda e: e.tensor_scalar(out=f2(valid), in0=f2(pos), scalar1=float(self.CAP), scalar2=None, op0=ALU.is_lt), r=[pos], w=[valid])
        V(lambda e: e.tensor_tensor(out=slotv[:, :, :], in0=pos[:, :, :], in1=self.eoff[:, :].unsqueeze(1).to_broadcast([128, NJ, 16]), op=ALU.add), r=[pos, self.eoff], w=[slotv])
        V(lambda e: e.tensor_scalar(out=f2(slotv), in0=f2(slotv), scalar1=self.trashp[:, 0:1], scalar2=None, op0=ALU.subtract), r=[slotv, self.trashp], w=[slotv])
        V(lambda e: e.tensor_tensor(out=f2(slotv), in0=f2(slotv), in1=f2(valid), op=ALU.mult), r=[slotv, valid], w=[slotv])
        V(lambda e: e.tensor_scalar(out=f2(slotv), in0=f2(slotv), scalar1=self.trashp[:, 0:1], scalar2=None, op0=ALU.add), r=[slotv, self.trashp], w=[slotv])
        V(lambda e: e.tensor_tensor(out=f2(gw), in0=f2(gw), in1=f2(valid), op=ALU.mult), r=[gw, valid], w=[gw])
        V(lambda e: e.tensor_tensor_scan(out=f2(cum), data0=f2(mask_e0), data1=f2(chosen), initial=0.0, op0=ALU.mult, op1=ALU.add), r=[mask_e0, chosen], w=[cum])
        for which, dsti in ((1.0, self.slotA), (2.0, self.slotB)):
            wi = int(which) - 1
            V(lambda e, which=which: e.tensor_scalar(out=f2(tmp), in0=f2(cum), scalar1=float(which), scalar2=None, op0=ALU.is_equal), r=[cum], w=[tmp])
            V(lambda e: e.tensor_tensor(out=f2(tmp), in0=f2(tmp), in1=f2(chosen), op=ALU.mult), r=[tmp, chosen], w=[tmp])
            V(lambda e: e.tensor_tensor(out=f2(eq), in0=f2(tmp), in1=f2(gw), op=ALU.mult), r=[tmp, gw], w=[eq])
            V(lambda e, wi=wi: e.tensor_reduce(out=self.gAB[:, wi, j0:j0 + NJ], in_=eq[:, :, :], axis=AX.X, op=ALU.add), r=[eq], w=[self.gAB])
            V(lambda e: e.tensor_tensor(out=f2(tmp), in0=f2(tmp), in1=f2(slotv), op=ALU.mult), r=[tmp, slotv], w=[tmp])
            V(lambda e: e.tensor_reduce(out=t32[:, :], in_=tmp[:, :, :], axis=AX.X, op=ALU.add), r=[tmp], w=[t32])
            V(lambda e: e.tensor_scalar(out=t32[:, :], in0=t32[:, :], scalar1=0.0, scalar2=float(self.NSLOT - 1), op0=ALU.max, op1=ALU.min), r=[t32], w=[t32])
            V(lambda e, dsti=dsti: e.tensor_copy(out=dsti[:, j0:j0 + NJ], in_=t32[:, :]), r=[t32], w=[dsti])
        for j in range(j0, j0 + NJ):
            for dsti in (self.slotA, self.slotB):
                self.cx.dma("pool", None, None, reads=[self.h2c[j].b, dsti.b], writes=[],
                            fn=lambda e, dsti=dsti, j=j: e.indirect_dma_start(out=self.h2slots[:, :], out_offset=bass.IndirectOffsetOnAxis(ap=dsti[:, j:j + 1], axis=0),
                                                                              in_=self.h2all[:, j, :], in_offset=None))

    def p4_io(self):
        i = self.inp
        self.w_gate = i("exp_w_gate", [2, 16, D, 512])
        self.w_up = i("exp_w_up", [2, 16, D, 512])
        self.w_down = i("exp_w_down", [2, 16, 512, D])

    def zero_slots(self):
        self.push()
        zt = self.sb("zt", [128, D], F32)
        self.G(lambda e: e.memset(zt[:, :], 0.0), w=[zt])
        self.dma("sp", self.oslots[16 * self.CAP:16 * self.CAP + 128, :], zt[:, :], r=[zt], w=[self.oslots])
        self.pop()

    def p4_experts(self, l):
        B = self.bank
        V, S, G = self.V, self.S, self.G
        s = self.sb
        wg = [s(f"wg{i}", [128, 8, 512], BF16) for i in range(2)]
        wu = [s(f"wu{i}", [128, 8, 512], BF16) for i in range(2)]
        wd = [s(f"wd{i}", [128, 4, D], BF16) for i in range(2)]
        rt = [s(f"rtok{i}", [128, 4, D], BF16) for i in range(2)]
        rT = s("rT", [128, 8, 512], BF16)
        sil = [s(f"sil{i}", [128, 512], BF16) for i in range(2)]
        hidT = s("hidT", [128, 4, 512], BF16)
        osb = [s(f"eosb{i}", [128, D], F32) for i in range(2)]

        def load_w(e):
            i = e % 2
            self.dma("pool", wg[i][:, :, :], self.w_gate[l, e].rearrange("(k p) f -> p k f", p=128), r=[self.w_gate], w=[wg[i]])
            self.dma("pool", wu[i][:, :, :], self.w_up[l, e].rearrange("(k p) f -> p k f", p=128), r=[self.w_up], w=[wu[i]])
            self.dma("pool", wd[i][:, :, :], self.w_down[l, e].rearrange("(k p) f -> p k f", p=128), r=[self.w_down], w=[wd[i]])

        glist = [(e, off, nb) for e in range(16) for (off, nb) in self.GROUPS]
        load_w(0)
        ob = 0

        def load_rows(gi):
            e, off, nb = glist[gi]
            r0 = e * self.CAP + off
            rtk = rt[gi % 2]
            self.dma("sp", rtk[:, 0:nb, :], self.h2slots[r0:r0 + nb * 128, :].rearrange("(b p) d -> p b d", p=128), r=[self.h2slots], w=[rtk])
        load_rows(0)
        for gi, (e, off, nb) in enumerate(glist):
            if off == 0 and e + 1 < 16:
                load_w(e + 1)
            i = e % 2
            r0 = e * self.CAP + off
            N = nb * 128
            rtk = rt[gi % 2]
            if gi + 1 < len(glist):
                load_rows(gi + 1)
            for blk in range(nb):
                pT = B[blk % 2]
                pTb = pT[:, :].bitcast(BF16).rearrange("p (k t) -> p k t", k=8)
                for k in range(8):
                    self.tr(pTb[:, k, :], rtk[:, blk, k * 128:(k + 1) * 128], self.identb[:, :], r=[rtk, self.identb], w=[pT], signal=(k == 7))
                if blk % 2 == 0:
                    V(lambda e_, blk=blk, pTb=pTb: e_.tensor_copy(out=rT[:, :, blk * 128:(blk + 1) * 128], in_=pTb), r=[pT], w=[rT])
                else:
                    S(lambda e_, blk=blk, pTb=pTb: e_.copy(out=rT[:, :, blk * 128:(blk + 1) * 128], in_=pTb), r=[pT], w=[rT])
            for fc in range(4):
                pg, pu = B[2 + 2 * (fc % 2)], B[3 + 2 * (fc % 2)]
                for k in range(8):
                    self.mm(pg[:, 0:N], wg[i][:, k, fc * 128:(fc + 1) * 128], rT[:, k, 0:N], r=[wg[i], rT], w=[pg], start=(k == 0), stop=(k == 7))
                for k in range(8):
                    self.mm(pu[:, 0:N], wu[i][:, k, fc * 128:(fc + 1) * 128], rT[:, k, 0:N], r=[wu[i], rT], w=[pu], start=(k == 0), stop=(k == 7))
                sl = sil[fc % 2]
                S(lambda e_, sl=sl, pg=pg, N=N: e_.activation(out=sl[:, 0:N], in_=pg[:, 0:N], func=AF.Silu), r=[pg], w=[sl])
                V(lambda e_, sl=sl, pu=pu, fc=fc, N=N: e_.tensor_tensor(out=hidT[:, fc, 0:N], in0=pu[:, 0:N], in1=sl[:, 0:N], op=ALU.mult), r=[pu, sl], w=[hidT])
            for blk in range(nb):
                o = osb[ob % 2]
                ob += 1
                for half in range(2):
                    pd = B[6 + half]
                    for fc in range(4):
                        self.mm(pd[:, :], hidT[:, fc, blk * 128:(blk + 1) * 128], wd[i][:, fc, half * 512:(half + 1) * 512], r=[hidT, wd[i]], w=[pd],
                                start=(fc == 0), stop=(fc == 3))
                    V(lambda e_, o=o, pd=pd, half=half: e_.tensor_tensor(out=o[:, half * 512:(half + 1) * 512], in0=pd[:, :],
                                                                         in1=self.opg[:, 1, half * 512:(half + 1) * 512], op=ALU.mult), r=[pd, self.opg], w=[o], x=[o])
                self.dma("sp", self.oslots[r0 + blk * 128:r0 + (blk + 1) * 128, :], o[:, :], r=[o], w=[])

    def p5_alloc(self, l):
        s = self.sb
        self.lng2 = s("lng2", [128, D], F32); self.lnb2 = s("lnb2", [128, D], F32)
        self.dma("sp", self.lng2[:, :], self.ln2_g[l].partition_broadcast(128), r=[self.ln2_g], w=[self.lng2])
        self.dma("sp", self.lnb2[:, :], self.ln2_b[l].partition_broadcast(128), r=[self.ln2_b], w=[self.lnb2])
        self.rA = [s(f"rA{i}", [128, D], F32) for i in range(2)]
        self.rB = [s(f"rB{i}", [128, D], F32) for i in range(2)]
        self.x1r = [s(f"x1r{i}", [128, D], F32) for i in range(2)]
        self.x2t = [s(f"x2t{i}", [128, D], F32) for i in range(2)]
        self.st5 = s("st5", [128, 24], F32)
        self.y5 = [s(f"y5_{i}", [128, D], F32) for i in range(2)]

    def p5_loads(self, jj):
        if jj >= NCH:
            return
        rA_, rB_, x1r_ = self.rA[jj % 2], self.rB[jj % 2], self.x1r[jj % 2]
        self.cx.dma("pool", None, None, reads=[self.oslots.b, self.slotA.b], writes=[rA_.b],
                    fn=lambda e: e.indirect_dma_start(out=rA_[:, :], out_offset=None, in_=self.oslots[:, :],
                                                      in_offset=bass.IndirectOffsetOnAxis(ap=self.slotA[:, jj:jj + 1], axis=0)))
        self.cx.dma("pool", None, None, reads=[self.oslots.b, self.slotB.b], writes=[rB_.b],
                    fn=lambda e: e.indirect_dma_start(out=rB_[:, :], out_offset=None, in_=self.oslots[:, :],
                                                      in_offset=bass.IndirectOffsetOnAxis(ap=self.slotB[:, jj:jj + 1], axis=0)))
        self.dma("sp", x1r_[:, :], self.x1d[jj * 128:(jj + 1) * 128, :], r=[self.x1d], w=[x1r_])

    def p5_S1(self, j):
        if j >= NCH:
            return
        V, S, G = self.V, self.S, self.G
        rA, rB, x1r, y5 = self.rA[j % 2], self.rB[j % 2], self.x1r[j % 2], self.y5[j % 2]
        S(lambda e: e.activation(out=rA[:, :], in_=rA[:, :], func=AF.Identity, scale=self.gAB[:, 0, j:j + 1]), r=[rA, self.gAB], w=[rA])
        V(lambda e: e.scalar_tensor_tensor(out=rA[:, :], in0=rB[:, :], scalar=self.gAB[:, 1, j:j + 1], in1=rA[:, :], op0=ALU.mult, op1=ALU.add), r=[rB, rA, self.gAB], w=[rA])
        V(lambda e: e.scalar_tensor_tensor(out=y5[:, :], in0=x1r[:, :], scalar=float(ALPHA), in1=rA[:, :], op0=ALU.mult, op1=ALU.add), r=[x1r, rA], w=[y5], x=[rA])

    def p5_S2(self, j, dst):
        if j >= NCH or j < 0:
            return
        S = self.S
        y5, x2 = self.y5[j % 2], self.x2t[j % 2]
        self.ln_stats(y5, lambda a, b: y5[:, a:b], D, self.st5, act=True)
        S(lambda e: e.activation(out=x2[:, :], in_=y5[:, :], func=AF.Identity, bias=self.st5[:, 4:5], scale=self.st5[:, 3:4]), r=[y5, self.st5], w=[x2])

    def p5_S3(self, j, dst):
        if j >= NCH or j < 0:
            return
        V, G = self.V, self.G
        rows = slice(j * 128, (j + 1) * 128)
        x2 = self.x2t[j % 2]
        V(lambda e: e.tensor_tensor(out=x2[:, 0:512], in0=x2[:, 0:512], in1=self.lng2[:, 0:512], op=ALU.mult), r=[x2, self.lng2], w=[x2])
        G(lambda e: e.tensor_tensor(out=x2[:, 512:1024], in0=x2[:, 512:1024], in1=self.lng2[:, 512:1024], op=ALU.mult), r=[x2, self.lng2], w=[x2])
        V(lambda e: e.tensor_tensor(out=x2[:, 0:512], in0=x2[:, 0:512], in1=self.lnb2[:, 0:512], op=ALU.add), r=[x2, self.lnb2], w=[x2])
        G(lambda e: e.tensor_tensor(out=x2[:, 512:1024], in0=x2[:, 512:1024], in1=self.lnb2[:, 512:1024], op=ALU.add), r=[x2, self.lnb2], w=[x2])
        self.dma("sp", dst[rows, :], x2[:, :], r=[x2], w=[dst])

    def p5_all(self, l, dst):
        self.p5_loads(0); self.p5_loads(1)
        self.p5_S1(0)
        self.p5_loads(2)
        self.p5_S1(1)
        self.p5_S2(0, dst)
        for j in range(NCH):
            self.p5_loads(j + 3)
            self.p5_S1(j + 2)
            self.p5_S2(j + 1, dst)
            self.p5_S3(j, dst)

    def build(self):
        self.declare_io(); self.s5_io(); self.p3_io(); self.p4_io()
        self.setup()
        x_src = self.x_in
        for l in range(self.nlayers):
            dst = self.out if l == self.nlayers - 1 else self.xmid
            self.push()
            self.layer_alloc(); self.route_alloc(); self.layer_prep(l, 0)
            self.zero_slots()
            self.push(); self.qk_alloc()
            self.push(); self.s5_alloc(); self.s5_prep(l); self.load_win(l); self.p1v2_alloc()
            self.p1_all(l, x_src)
            self.pop()
            self.push(); self.p2_alloc(); self.p2_attention(); self.pop()
            self.pop()
            self.push()
            self.opg = self.sb("opg", [128, 2, 1024], F32)
            self.opg2 = self.sb("opg2", [128, 2, 1024], F32)
            self.layer_prep(l, 1)
            self.push(); self.p3_alloc(l)
            self.p3_route_alloc()
            self.p3_all(l, x_src)
            self.pop()
            self.push(); self.p4_experts(l); self.pop()
            self.push(); self.p5_alloc(l)
            self.p5_all(l, dst)
            self.pop()
            self.pop()
            self.pop()
            x_src = dst
        if getattr(self, "dbg_hook", None):
            self.dbg_hook(self)
        self.finish()


def make_inputs(inp, b):
    c = np.ascontiguousarray
    f = lambda k: np.asarray(inp[k])
    br, bi = f("ssm_b_re"), f("ssm_b_im")
    def bl(a):
        L = a.shape[0]
        return a.reshape(L, 2, 8, 64, 16).transpose(0, 2, 4, 1, 3).reshape(L, 128, 2, 64)
    bT = np.stack([bl(br), bl(bi)], axis=1)
    cr, ci = f("ssm_c_re"), f("ssm_c_im")
    cT = np.concatenate([cr.transpose(0, 1, 3, 2), ci.transpose(0, 1, 3, 2)], axis=2)
    L = br.shape[0]
    d = {
        "x": c(f("x")[b]), "ccol": c(f("c")[b].reshape(8, 128).T), "pos": c(f("positions")[b].reshape(32, 128).T),
        "ada_w": f("ada_w"), "ada_b": f("ada_b"), "w_in": f("w_in"), "gm_ln_g": f("gm_ln_g"), "gm_ln_b": f("gm_ln_b"),
        "gm_ws": f("gm_ws"), "gm_bsT": c(f("gm_bs").transpose(0, 2, 1)),
        "lam_re": c(f("ssm_lam_re").reshape(L, 1024)), "lam_im": c(f("ssm_lam_im").reshape(L, 1024)), "log_dt": f("ssm_log_dt"),
        "ssm_bT": c(bT), "ssm_cT": c(cT), "ssm_dT": c(f("ssm_d").reshape(L, 2, 128).transpose(0, 2, 1)),
        "glu_w": f("glu_w"), "glu_bT": c(f("glu_b").reshape(L, 2, 128).transpose(0, 2, 1)),
        "w_out": f("w_out"), "ln1_g": f("ln1_g"), "ln1_b": f("ln1_b"), "ln2_g": f("ln2_g"), "ln2_b": f("ln2_b"),
        "router_w": f("router_w"), "router_bias": f("router_bias"),
        "exp_w_gate": f("exp_w_gate"), "exp_w_up": f("exp_w_up"), "exp_w_down": f("exp_w_down"),
    }
    return d


_CACHE = {}


def kernel(**inputs):
    n = 8
    if "nc" not in _CACHE:
        nc = bass.Bass("TRN2", target_bir_lowering=False)
        kb = KB(nc)
        kb.build()
        _CACHE["nc"] = nc
        _CACHE["names"] = kb.in_names
    nc = _CACHE["nc"]
    names = _CACHE["names"]
    in_maps = []
    for b in range(n):
        im = make_inputs(inputs, b)
        in_maps.append({k: v for k, v in im.items() if k in names})
    res = run_bass_kernel_spmd(nc, in_maps, core_ids=list(range(n)))
    out = np.stack([np.asarray(r["out"]) for r in res.results], axis=0)
    return out.astype(np.float32)
```

```python
import numpy as np
import concourse.bass as bass
import concourse.mybir as mybir

F32 = mybir.dt.float32
BF16 = mybir.dt.bfloat16
I32 = mybir.dt.int32
U32 = mybir.dt.uint32
AF = mybir.ActivationFunctionType
ALU = mybir.AluOpType
AX = mybir.AxisListType


RELAXED = ()
ALLOW_RELAX = True


class Buf:
    __slots__ = ("w", "r", "name")

    def __init__(self, name=""):
        self.w = None
        self.r = []
        self.name = name


class Ctx:
    def __init__(self, nc, strict_same=False):
        self.nc = nc
        self.strict_same = strict_same
        self.relaxed = set(RELAXED)
        self.engs = {"pe": nc.tensor, "act": nc.scalar, "dve": nc.vector, "pool": nc.gpsimd, "sp": nc.sync}
        self.sem = {}
        self.cnt = {}
        for e in ("pe", "act", "dve", "pool"):
            self.sem[e] = nc.alloc_semaphore("s_" + e)
            self.cnt[e] = 0
        self.dq = {}
        for q, n in (("sp", 10), ("act", 4), ("pool", 8)):
            self.dq[q] = {"sems": [nc.alloc_semaphore(f"d_{q}{i}") for i in range(n)], "vals": [0] * n, "k": 0}
        self.waited = {}
        self.nbuf = 0
        self.out_events = []

    def buf(self, name=""):
        return Buf(name)

    def _wait(self, eng, ev):
        sem, val = ev
        key = (eng, id(sem))
        if self.waited.get(key, 0) >= val:
            return
        self.engs[eng].wait_ge(sem, val)
        self.waited[key] = val

    def _deps(self, eng, reads, writes, relax=()):
        own = self.sem.get(eng)
        rl = set(id(b) for b in relax) if ALLOW_RELAX else set()

        def chk(b, ev):
            if ev[0] is own and (eng == "pe" or id(b) in rl):
                return
            self._wait(eng, ev)
        for b in reads:
            if b.w is not None:
                chk(b, b.w)
        for b in writes:
            if b.w is not None:
                chk(b, b.w)
            for ev in b.r:
                chk(b, ev)

    def _commit(self, ev, reads, writes):
        for b in writes:
            b.w = ev
            b.r = []
        for b in reads:
            b.r.append(ev)
            if len(b.r) > 24:
                b.r = b.r[-24:]

    def op(self, eng, fn, reads=(), writes=(), signal=True, relax=()):
        self._deps(eng, reads, writes, relax)
        inst = fn(self.engs[eng])
        if signal:
            self.cnt[eng] += 1
            inst.then_inc(self.sem[eng], 1)
            ev = (self.sem[eng], self.cnt[eng])
        else:
            ev = (self.sem[eng], self.cnt[eng] + 1)
        self._commit(ev, reads, writes)
        return ev

    def dma(self, q, out, in_, reads=(), writes=(), fn=None, **kw):
        d = self.dq[q]
        i = d["k"] % len(d["sems"])
        d["k"] += 1
        sem = d["sems"][i]
        self._deps(q, reads, writes)
        if d["vals"][i] > 0:
            self._wait(q, (sem, d["vals"][i]))
        if fn is None:
            inst = self.engs[q].dma_start(out=out, in_=in_, **kw)
        else:
            inst = fn(self.engs[q])
        d["vals"][i] += 16
        inst.then_inc(sem, 16)
        ev = (sem, d["vals"][i])
        self._commit(ev, reads, writes)
        return ev

    def barrier(self):
        evs = [(self.sem[e], self.cnt[e]) for e in self.sem if self.cnt[e] > 0]
        for q, d in self.dq.items():
            for sem, v in zip(d["sems"], d["vals"]):
                if v > 0:
                    evs.append((sem, v))
        for eng in ("pe", "act", "dve", "pool", "sp"):
            own = self.sem.get(eng)
            for ev in evs:
                self._wait(eng, ev)

    def finish(self, bufs):
        for b in bufs:
            if b.w is not None:
                self._wait("sp", b.w)
            for ev in b.r:
                self._wait("sp", ev)

from concourse.bass_utils import run_bass_kernel_spmd
import math
import contextlib

T = 4096
D = 1024
NCH = 32
PW = 2304
EPS = 1e-5
ALPHA = (2.0 * 2) ** 0.25
TWO_PI = 2.0 * math.pi
ROPE_THETA = 500000.0


class Tl:
    def __init__(self, h, name=""):
        self.h = h
        self.b = Buf(name)

    def __getitem__(self, k):
        return self.h[k]


class KB:
    def __init__(self, nc, nlayers=2, dbg=(), stop_after=None):
        self.nc = nc
        self.cx = Ctx(nc)
        self.dbg = set(dbg)
        self.stop_after = stop_after
        self.nlayers = nlayers
        self.outs = []
        self.stk = [contextlib.ExitStack()]
        self.nps = 0

    def inp(self, name, shape, dt=F32):
        self.in_names = getattr(self, "in_names", set())
        self.in_names.add(name)
        return Tl(self.nc.dram_tensor(name, list(shape), dt, kind="ExternalInput").ap(), name)

    def outp(self, name, shape, dt=F32):
        t = Tl(self.nc.dram_tensor(name, list(shape), dt, kind="ExternalOutput").ap(), name)
        self.outs.append(t)
        return t

    def scratch(self, name, shape, dt):
        return Tl(self.nc.dram_tensor(name, list(shape), dt, kind="Internal").ap(), name)

    def sb(self, name, shape, dt):
        self.nsb = getattr(self, "nsb", 0) + 1
        h = self.stk[-1].enter_context(self.nc.sbuf_tensor(f"{name}_{self.nsb}", list(shape), dt))
        return Tl(h, name)

    def push(self):
        self.stk.append(contextlib.ExitStack())

    def pop(self):
        self.cx.barrier()
        self.stk.pop().close()

    def ps(self, name, shape, dt=F32):
        return Tl(self.nc.alloc_psum_tensor(name, list(shape), dt), name)

    def _rw(self, r, w):
        return [t.b for t in r], [t.b for t in w]

    def V(self, fn, r=(), w=(), x=()):
        r, w = self._rw(r, w)
        return self.cx.op("dve", fn, r, w, relax=[t.b for t in x])

    def S(self, fn, r=(), w=(), x=()):
        r, w = self._rw(r, w)
        return self.cx.op("act", fn, r, w, relax=[t.b for t in x])

    def G(self, fn, r=(), w=(), x=()):
        r, w = self._rw(r, w)
        return self.cx.op("pool", fn, r, w, relax=[t.b for t in x])

    def P(self, fn, r=(), w=(), signal=True):
        r, w = self._rw(r, w)
        return self.cx.op("pe", fn, r, w, signal=signal)

    def dma(self, q, out, in_, r=(), w=(), **kw):
        r, w = self._rw(r, w)
        return self.cx.dma(q, out, in_, r, w, **kw)

    def mm(self, out, lhsT, rhs, r, w, start, stop, signal=None):
        if signal is None:
            signal = stop
        return self.P(lambda e: e.matmul(out, lhsT, rhs, start=start, stop=stop), r, w, signal=signal)

    def tr(self, out, in_, ident, r, w, signal=True):
        return self.P(lambda e: e.transpose(out, in_, ident), r, w, signal=signal)

    def declare_io(self):
        i = self.inp
        self.x_in = i("x", [T, D])
        self.ccol = i("ccol", [128, 8])
        self.pos = i("pos", [128, NCH], I32)
        self.ada_w = i("ada_w", [2, D, 6 * D])
        self.ada_b = i("ada_b", [2, 6 * D])
        self.w_in = i("w_in", [2, D, PW])
        self.gm_ln_g = i("gm_ln_g", [2, 256])
        self.gm_ln_b = i("gm_ln_b", [2, 256])
        self.gm_ws = i("gm_ws", [2, 4, 128, 128])
        self.gm_bsT = i("gm_bsT", [2, 128, 4])
        self.out = self.outp("out", [T, D])
        self.mixtok = self.scratch("mixtok", [T, 1024], BF16)
        self.vd = self.scratch("vd", [T, 520], BF16)

    def consts(self):
        nc = self.nc
        self.identb = self.sb("identb", [128, 128], BF16)
        self.identf = self.sb("identf", [128, 128], F32)
        self.onesf = self.sb("onesf", [128, 128], F32)
        self.eps_t = self.sb("eps_t", [128, 1], F32)
        self.G(lambda e: e.memset(self.onesf[:, :], 1.0), w=[self.onesf])
        self.G(lambda e: e.memset(self.eps_t[:, :], EPS), w=[self.eps_t])
        self.mhalf = self.sb("mhalf", [128, 1], F32)
        self.G(lambda e: e.memset(self.mhalf[:, :], -0.5), w=[self.mhalf])
        self.G(lambda e: e.affine_select(out=self.identf[:, :], in_=self.onesf[:, :], pattern=[[-1, 128]],
                                         compare_op=ALU.is_equal, fill=0.0, base=0, channel_multiplier=1),
               r=[self.onesf], w=[self.identf])
        self.G(lambda e: e.tensor_copy(out=self.identb[:, :], in_=self.identf[:, :]), r=[self.identf], w=[self.identb])
        self.posf = self.sb("posf", [128, NCH], F32)
        self.posi = self.sb("posi", [128, NCH], I32)
        self.dma("sp", self.posi[:, :], self.pos[:, :], r=[self.pos], w=[self.posi])
        self.V(lambda e: e.tensor_copy(out=self.posf[:, :], in_=self.posi[:, :]), r=[self.posi], w=[self.posf])
        self.cs = self.sb("cs", [128, NCH, 8], F32)
        self.sn = self.sb("sn", [128, NCH, 8], F32)
        self.push()
        ang = self.sb("ang", [128, NCH, 8], F32)
        tmp = self.sb("angt", [128, NCH, 8], F32)
        tmi = self.sb("angi", [128, NCH, 8], I32)
        for j in range(8):
            fr = ROPE_THETA ** (-(j * 2.0) / 16.0)
            self.V(lambda e, j=j, fr=fr: e.tensor_scalar(out=ang[:, :, j], in0=self.posf[:, :], scalar1=float(fr), scalar2=None, op0=ALU.mult),
                   r=[self.posf], w=[ang])
        self.sincos(ang, self.sn, self.cs, tmp, tmi, [128, NCH * 8])
        self.pop()

    def _flat(self, t):
        ap = t[:]
        if len(ap.shape) == 2:
            return ap
        names = " ".join(f"a{i}" for i in range(len(ap.shape) - 1))
        return ap.rearrange(f"p {names} -> p ({names})")

    def range_reduce(self, src, dst, tmp, tmi, shift):
        s, d, t, ti = self._flat(src), self._flat(dst), self._flat(tmp), self._flat(tmi)
        self.V(lambda e: e.tensor_scalar(out=t, in0=s, scalar1=float(shift), scalar2=float(1.0 / TWO_PI), op0=ALU.add, op1=ALU.mult),
               r=[src], w=[tmp])
        self.V(lambda e: e.tensor_copy(out=ti, in_=t), r=[tmp], w=[tmi])
        self.V(lambda e: e.tensor_copy(out=t, in_=ti), r=[tmi], w=[tmp])
        self.V(lambda e: e.tensor_scalar(out=t, in0=t, scalar1=float(-TWO_PI), scalar2=float(shift), op0=ALU.mult, op1=ALU.add),
               r=[tmp], w=[tmp])
        self.V(lambda e: e.tensor_tensor(out=d, in0=t, in1=s, op=ALU.add), r=[tmp, src], w=[dst])
        self.V(lambda e: e.tensor_scalar(out=t, in0=d, scalar1=float(math.pi), scalar2=float(-TWO_PI), op0=ALU.is_gt, op1=ALU.mult),
               r=[dst], w=[tmp])
        self.V(lambda e: e.tensor_tensor(out=d, in0=d, in1=t, op=ALU.add), r=[tmp, dst], w=[dst])
        self.V(lambda e: e.tensor_scalar(out=t, in0=d, scalar1=float(-math.pi), scalar2=float(TWO_PI), op0=ALU.is_lt, op1=ALU.mult),
               r=[dst], w=[tmp])
        self.V(lambda e: e.tensor_tensor(out=d, in0=d, in1=t, op=ALU.add), r=[tmp, dst], w=[dst])
        self.V(lambda e: e.tensor_scalar(out=d, in0=d, scalar1=float(math.pi), scalar2=float(-math.pi), op0=ALU.min, op1=ALU.max),
               r=[dst], w=[dst])

    def sincos(self, ang, sn, cs, tmp, tmi, shape):
        self.range_reduce(ang, sn, tmp, tmi, 0.0)
        self.S(lambda e: e.activation(out=self._flat(sn), in_=self._flat(sn), func=AF.Sin), r=[sn], w=[sn])
        self.range_reduce(ang, cs, tmp, tmi, math.pi / 2)
        self.S(lambda e: e.activation(out=self._flat(cs), in_=self._flat(cs), func=AF.Sin), r=[cs], w=[cs])

    def setup(self):
        self.bank = [self.ps(f"bank{i}", [128, 512], F32) for i in range(8)]
        self.consts()

    def layer_alloc(self):
        self.modp = self.sb("modp", [128, 4, 8], F32)

        self.wsT = self.sb("wsT", [128, 4, 128], BF16)
        self.gbs = self.sb("gbs", [128, 4], F32)
        self.glng = self.sb("glng", [128, 256], F32)
        self.glnb = self.sb("glnb", [128, 256], F32)

    def prep_alloc(self):
        self.adaw = [self.sb(f"adaw{i}", [128, 8, 512], F32) for i in range(2)]
        self.adab = [self.sb(f"adab{i}", [128, 512], F32) for i in range(2)]
        self.modc = [self.sb(f"modc{i}", [128, 512], F32) for i in range(2)]
        self.wtmp = self.sb("wtmp", [128, 4, 128], F32)
        ccs = self.sb("ccs", [128, 8], F32)
        self.dma("sp", ccs[:, :], self.ccol[:, :], r=[self.ccol], w=[ccs])
        self.S(lambda e: e.activation(out=ccs[:, :], in_=ccs[:, :], func=AF.Silu), r=[ccs], w=[ccs])
        self.condrep = self.sb("condrep", [128, 8, 128], F32)
        self.V(lambda e: e.tensor_copy(out=self.condrep[:, :, :], in_=ccs[:, :].unsqueeze(2).to_broadcast([128, 8, 128])),
               r=[ccs], w=[self.condrep])


    def load_win(self, l):
        self.win = self.sb("win", [128, 8, PW], BF16)
        for k in range(8):
            self.dma("pool", self.win[:, k, :], self.w_in[l, k * 128:(k + 1) * 128, :], r=[self.w_in], w=[self.win])

    def layer_prep(self, l, part=0):
        self.push()
        self.prep_alloc()
        pb = self.bank[7]
        pt = self.bank[6]
        for n in range(12):
            if (part == 0) != (n // 2 in (0, 1)):
                continue
            aw = self.adaw[n % 2]
            ab = self.adab[n % 2]
            mc = self.modc[n % 2]
            self.dma("sp", aw[:, :, :], self.ada_w[l, :, n * 512:(n + 1) * 512].rearrange("(k p) n -> p k n", p=128),
                     r=[self.ada_w], w=[aw])
            self.dma("sp", ab[:, :], self.ada_b[l, n * 512:(n + 1) * 512].partition_broadcast(128), r=[self.ada_b], w=[ab])
            for k in range(8):
                self.mm(pb[:, :], self.condrep[:, k, :], aw[:, k, :], r=[self.condrep, aw], w=[pb], start=(k == 0), stop=(k == 7))
            which, half = n // 2, n % 2
            if which in (2, 5, 3, 4):
                tgt = self.opg if which in (2, 5) else self.opg2
                gi = {2: 0, 5: 1, 3: 0, 4: 1}[which]
                dst = tgt[:, gi, half * 512:(half + 1) * 512]
                self.V(lambda e, dst=dst: e.tensor_tensor(out=dst, in0=pb[:, :], in1=ab[:, :], op=ALU.add), r=[pb, ab], w=[tgt])
                if which != 3:
                    self.V(lambda e, dst=dst: e.tensor_scalar(out=dst, in0=dst, scalar1=1.0, scalar2=None, op0=ALU.add), r=[tgt], w=[tgt])
            else:
                slot = {0: 0, 1: 1}[which]
                self.V(lambda e: e.tensor_tensor(out=mc[:, :], in0=pb[:, :], in1=ab[:, :], op=ALU.add), r=[pb, ab], w=[mc])
                for b4 in range(4):
                    self.tr(pt[:, b4 * 128:(b4 + 1) * 128], mc[:, b4 * 128:(b4 + 1) * 128], self.identf[:, :], r=[mc, self.identf], w=[pt])
                addc = 1.0 if slot in (1, 3) else 0.0
                for b4 in range(4):
                    self.V(lambda e, b4=b4: e.tensor_scalar(out=self.modp[:, slot, half * 4 + b4:half * 4 + b4 + 1],
                                                            in0=pt[:, b4 * 128:b4 * 128 + 1], scalar1=float(addc), scalar2=None, op0=ALU.add),
                           r=[pt], w=[self.modp])
        if part == 0:
            self.dma("sp", self.wtmp[:, :, :], self.gm_ws[l].rearrange("h t s -> t h s"), r=[self.gm_ws], w=[self.wtmp])
            self.G(lambda e: e.affine_select(out=self.wtmp[:, :, :], in_=self.wtmp[:, :, :], pattern=[[0, 4], [-1, 128]],
                                             compare_op=ALU.is_ge, fill=0.0, base=0, channel_multiplier=1), r=[self.wtmp], w=[self.wtmp])
            for h in range(4):
                self.tr(pt[:, h * 128:(h + 1) * 128], self.wtmp[:, h, :], self.identf[:, :], r=[self.wtmp, self.identf], w=[pt])
            self.V(lambda e: e.tensor_copy(out=self.wsT[:, :, :], in_=pt[:, :].rearrange("p (h t) -> p h t", h=4)), r=[pt], w=[self.wsT])
            self.dma("sp", self.gbs[:, :], self.gm_bsT[l], r=[self.gm_bsT], w=[self.gbs])
            self.dma("sp", self.glng[:, :], self.gm_ln_g[l].partition_broadcast(128), r=[self.gm_ln_g], w=[self.glng])

            self.dma("sp", self.glnb[:, :], self.gm_ln_b[l].partition_broadcast(128), r=[self.gm_ln_b], w=[self.glnb])
        self.pop()

    def ln_stats(self, src, src_ap_fn, n, st, act=False):
        if act:
            return self.ln_stats_act(src, src_ap_fn, n, st)
        nchk = (n + 511) // 512
        w = n // nchk
        for i in range(nchk):
            self.V(lambda e, i=i: e.bn_stats(out=st[:, 8 + i * 6:8 + (i + 1) * 6], in_=src_ap_fn(i * w, (i + 1) * w)), r=[src], w=[st])
        self.V(lambda e: e.bn_aggr(out=st[:, 0:2], in_=st[:, 8:8 + 6 * nchk]), r=[st], w=[st])
        self.V(lambda e: e.tensor_scalar(out=st[:, 2:3], in0=st[:, 1:2], scalar1=float(EPS), scalar2=None, op0=ALU.add), r=[st], w=[st])
        self.G(lambda e: e.tensor_tensor(out=st[:, 3:4], in0=st[:, 2:3], in1=self.mhalf[:, 0:1], op=ALU.pow), r=[st, self.mhalf], w=[st])
        self.V(lambda e: e.tensor_scalar(out=st[:, 4:5], in0=st[:, 0:1], scalar1=-1.0, scalar2=st[:, 3:4], op0=ALU.mult, op1=ALU.mult), r=[st], w=[st])

    def ln_stats_act(self, src, src_ap_fn, n, st):
        nchk = (n + 511) // 512
        w = n // nchk
        for i in range(nchk):
            self.V(lambda e, i=i: e.bn_stats(out=st[:, 8 + i * 6:8 + (i + 1) * 6], in_=src_ap_fn(i * w, (i + 1) * w)), r=[src], w=[st])
        self.V(lambda e: e.bn_aggr(out=st[:, 0:2], in_=st[:, 8:8 + 6 * nchk]), r=[st], w=[st])
        self.S(lambda e: e.activation(out=st[:, 2:3], in_=st[:, 1:2], func=AF.Sqrt, bias=self.eps_t[:, 0:1], scale=1.0), r=[st, self.eps_t], w=[st])
        self.V(lambda e: e.reciprocal(out=st[:, 3:4], in_=st[:, 2:3]), r=[st], w=[st])
        self.V(lambda e: e.tensor_scalar(out=st[:, 4:5], in0=st[:, 0:1], scalar1=-1.0, scalar2=st[:, 3:4], op0=ALU.mult, op1=ALU.mult), r=[st], w=[st])

    def qk_alloc(self):
        self.QT = self.sb("QT", [128, 4, T], BF16)
        self.KT = self.sb("KT", [128, 4, T], BF16)

    def p1_alloc(self):
        s = self.sb
        self.xt = [s(f"xt{i}", [128, D], F32) for i in range(2)]
        self.xn = [s(f"xn{i}", [128, D], BF16) for i in range(2)]
        self.st = [s(f"st{i}", [128, 24], F32) for i in range(2)]
        self.st2 = [s(f"stb{i}", [128, 24], F32) for i in range(2)]
        self.hT = [s(f"hT{i}", [128, 8, 128], BF16) for i in range(2)]
        self.gl = [s(f"gl{i}", [128, 512], F32) for i in range(1)] * 2
        self.vnb = [s(f"vnb{i}", [128, 256], F32) for i in range(1)] * 2
        self.vb = [s(f"vb{i}", [128, 256], BF16) for i in range(1)] * 2
        self.aout = [s(f"aout{i}", [128, 256], BF16) for i in range(1)] * 2
        self.qf = [s(f"qf{i}", [128, 2, 512], F32) for i in range(1)] * 2
        self.qb = [s(f"qb{i}", [128, 2, 512], BF16) for i in range(1)] * 2
        self.rt = [s(f"rt{i}", [128, 4, 8, 8], F32) for i in range(1)] * 2
        self.vp = [s(f"vp{i}", [128, 8, 65], BF16) for i in range(1)] * 2
        for i in range(1):
            self.G(lambda e, i=i: e.memset(self.vp[i][:, :, :], 1.0), w=[self.vp[i]])

    def p1_chunk(self, l, j, x_src):
        i2 = j % 2
        xt, xn, st, st2, hT = self.xt[i2], self.xn[i2], self.st[i2], self.st2[i2], self.hT[i2]
        gl, vnb, vb, aout, qf, qb, rt, vp = self.gl[i2], self.vnb[i2], self.vb[i2], self.aout[i2], self.qf[i2], self.qb[i2], self.rt[i2], self.vp[i2]
        B = self.bank
        rows = slice(j * 128, (j + 1) * 128)
        if j == 0:
            self.dma("sp", xt[:, :], x_src[rows, :], r=[x_src], w=[xt])
        if j + 1 < NCH:
            xtn = self.xt[(j + 1) % 2]
            self.dma("sp", xtn[:, :], x_src[(j + 1) * 128:(j + 2) * 128, :], r=[x_src], w=[xtn])
        self.ln_stats(xt, lambda a, b: xt[:, a:b], D, st)
        self.S(lambda e: e.activation(out=xn[:, :], in_=xt[:, :], func=AF.Identity, bias=st[:, 4:5], scale=st[:, 3:4]), r=[xt, st], w=[xn])
        pT = B[0]
        pTb = pT[:, :].bitcast(BF16).rearrange("p (k t) -> p k t", k=8)
        for k in range(8):
            self.tr(pTb[:, k, :], xn[:, k * 128:(k + 1) * 128], self.identb[:, :], r=[xn, self.identb], w=[pT], signal=(k == 7))
        for k in range(8):
            self.V(lambda e, k=k: e.tensor_scalar(out=hT[:, k, :], in0=pTb[:, k, :], scalar1=self.modp[:, 1, k:k + 1], scalar2=self.modp[:, 0, k:k + 1],
                                                  op0=ALU.mult, op1=ALU.add), r=[pT, self.modp], w=[hT], x=[hT])
        for bi, c0 in ((1, 0), (2, 512), (3, 1024), (4, 1536)):
            for k in range(8):
                self.mm(B[bi][:, :], hT[:, k, :], self.win[:, k, c0:c0 + 512], r=[hT, self.win], w=[B[bi]], start=(k == 0), stop=(k == 7))
        self.S(lambda e: e.activation(out=gl[:, :], in_=B[1][:, :], func=AF.Gelu_apprx_tanh), r=[B[1]], w=[gl])
        self.ln_stats(gl, lambda a, b: gl[:, 256 + a:256 + b], 256, st2)
        self.V(lambda e: e.tensor_scalar(out=vnb[:, :], in0=gl[:, 256:512], scalar1=st2[:, 3:4], scalar2=st2[:, 4:5], op0=ALU.mult, op1=ALU.add),
               r=[gl, st2], w=[vnb])
        self.V(lambda e: e.tensor_tensor(out=vnb[:, :], in0=vnb[:, :], in1=self.glng[:, :], op=ALU.mult), r=[vnb, self.glng], w=[vnb], x=[vnb])
        self.V(lambda e: e.tensor_tensor(out=vb[:, :], in0=vnb[:, :], in1=self.glnb[:, :], op=ALU.add), r=[vnb, self.glnb], w=[vb], x=[vnb])
        psv = B[5]
        for h in range(4):
            self.mm(psv[:, h * 64:(h + 1) * 64], self.wsT[:, h, :], vb[:, h * 64:(h + 1) * 64], r=[self.wsT, vb], w=[psv], start=True, stop=True,
                    signal=(h == 3))
        for h in range(4):
            self.V(lambda e, h=h: e.scalar_tensor_tensor(out=aout[:, h * 64:(h + 1) * 64], in0=psv[:, h * 64:(h + 1) * 64], scalar=self.gbs[:, h:h + 1],
                                                         in1=gl[:, h * 64:(h + 1) * 64], op0=ALU.add, op1=ALU.mult), r=[psv, self.gbs, gl], w=[aout], x=[aout])
        self.dma("sp", self.mixtok[rows, 0:256], aout[:, :], r=[aout], w=[self.mixtok])
        self.S(lambda e: e.copy(out=qf[:, 0, :], in_=B[2][:, :]), r=[B[2]], w=[qf])
        self.S(lambda e: e.copy(out=qf[:, 1, :], in_=B[3][:, :]), r=[B[3]], w=[qf], x=[qf])
        q4 = qf[:, :, :].rearrange("p a (h e) -> p (a h) e", e=64)
        o4 = qb[:, :, :].rearrange("p a (h e) -> p (a h) e", e=64)
        for a in range(2):
            xa1, xa2 = q4[:, a * 8:(a + 1) * 8, 0:8], q4[:, a * 8:(a + 1) * 8, 8:16]
            cb = self.cs[:, j, :].unsqueeze(1).to_broadcast([128, 8, 8])
            sb_ = self.sn[:, j, :].unsqueeze(1).to_broadcast([128, 8, 8])
            oa = o4[:, a * 8:(a + 1) * 8, :]
            self.G(lambda e, xa1=xa1, cb=cb: e.tensor_tensor(out=rt[:, 0, :, :], in0=xa1, in1=cb, op=ALU.mult), r=[qf, self.cs], w=[rt])
            self.G(lambda e, xa2=xa2, sb_=sb_: e.tensor_tensor(out=rt[:, 1, :, :], in0=xa2, in1=sb_, op=ALU.mult), r=[qf, self.sn], w=[rt])
            self.G(lambda e, xa2=xa2, cb=cb: e.tensor_tensor(out=rt[:, 2, :, :], in0=xa2, in1=cb, op=ALU.mult), r=[qf, self.cs], w=[rt])
            self.G(lambda e, xa1=xa1, sb_=sb_: e.tensor_tensor(out=rt[:, 3, :, :], in0=xa1, in1=sb_, op=ALU.mult), r=[qf, self.sn], w=[rt])
            self.G(lambda e, oa=oa: e.tensor_tensor(out=oa[:, :, 0:8], in0=rt[:, 0, :, :], in1=rt[:, 1, :, :], op=ALU.subtract), r=[rt], w=[qb])
            self.G(lambda e, oa=oa: e.tensor_tensor(out=oa[:, :, 8:16], in0=rt[:, 2, :, :], in1=rt[:, 3, :, :], op=ALU.add), r=[rt], w=[qb])
            self.G(lambda e, oa=oa, a=a: e.tensor_copy(out=oa[:, :, 16:64], in_=q4[:, a * 8:(a + 1) * 8, 16:64]), r=[qf], w=[qb])
        pq = B[6]
        pqb = pq[:, :].bitcast(BF16).rearrange("p (a k t) -> p a k t", a=2, k=4)
        for a in range(2):
            for k in range(4):
                self.tr(pqb[:, a, k, :], qb[:, a, k * 128:(k + 1) * 128], self.identb[:, :], r=[qb, self.identb], w=[pq], signal=(a == 1 and k == 3))
        self.S(lambda e: e.copy(out=self.QT[:, :, j * 128:(j + 1) * 128], in_=pqb[:, 0, :, :]), r=[pq], w=[self.QT])
        self.S(lambda e: e.copy(out=self.KT[:, :, j * 128:(j + 1) * 128], in_=pqb[:, 1, :, :]), r=[pq], w=[self.KT])
        self.S(lambda e: e.copy(out=vp[:, :, 0:64], in_=B[4][:, :].rearrange("p (h e) -> p h e", e=64)), r=[B[4]], w=[vp])
        self.dma("sp", self.vd[rows, :], vp[:, :, :].rearrange("p h e -> p (h e)"), r=[vp], w=[self.vd])

    def finish(self):
        bufs = [t.b for t in self.outs]
        self.cx.finish(bufs)

    def p2_alloc(self):
        s = self.sb
        self.vbr = [s(f"vbr{i}", [128, 32, 520], BF16) for i in range(2)]
        self.negm = s("negm", [128, 256], BF16)
        negf = s("negf", [128, 256], F32)
        zf = s("zf", [128, 256], F32)
        self.G(lambda e: e.memset(zf[:, :], 0.0), w=[zf])
        self.G(lambda e: e.affine_select(out=negf[:, 0:128], in_=zf[:, 0:128], pattern=[[-1, 128]], compare_op=ALU.is_ge, fill=-30000.0,
                                         base=0, channel_multiplier=1), r=[zf], w=[negf])
        self.G(lambda e: e.affine_select(out=negf[:, 128:256], in_=zf[:, 128:256], pattern=[[1, 128]], compare_op=ALU.is_ge, fill=-30000.0,
                                         base=0, channel_multiplier=-1), r=[zf], w=[negf])
        self.G(lambda e: e.tensor_copy(out=self.negm[:, :], in_=negf[:, :]), r=[negf], w=[self.negm])
        self.m01 = s("m01", [128, 256], BF16)
        onef = s("onef2", [128, 256], F32)
        self.G(lambda e: e.memset(onef[:, :], 1.0), w=[onef])
        self.G(lambda e: e.affine_select(out=onef[:, 0:128], in_=onef[:, 0:128], pattern=[[-1, 128]], compare_op=ALU.is_ge, fill=0.0,
                                         base=0, channel_multiplier=1), r=[onef], w=[onef])
        self.G(lambda e: e.affine_select(out=onef[:, 128:256], in_=onef[:, 128:256], pattern=[[1, 128]], compare_op=ALU.is_ge, fill=0.0,
                                         base=0, channel_multiplier=-1), r=[onef], w=[onef])
        self.G(lambda e: e.tensor_copy(out=self.m01[:, :], in_=onef[:, :]), r=[onef], w=[self.m01])
        self.pexp = [s(f"pexp{i}", [128, 256], BF16) for i in range(6)]
        self.osb = [s(f"osb{i}", [128, 520], F32) for i in range(2)]

    def p2_attention(self):
        B = self.bank
        hb = 0
        self._hb = 0
        dils = (1, 4, 16)

        def load_vb(bi):
            d = dils[bi]
            vb_ = self.vbr[bi % 2]
            src = self.vd[:, :].rearrange("(n l r) c -> l n r c", l=128, r=d)
            for n in range(T // (128 * d)):
                self.dma("sp", vb_[:, n * d:(n + 1) * d, :], src[:, n, :, :], r=[self.vd], w=[vb_])
        load_vb(0)
        load_vb(1)
        for bi, d in enumerate(dils):
            seg = 128 * d
            nseg = T // seg
            vb = self.vbr[bi % 2]
            if bi == 1:
                load_vb(2)
            odst = self.obr[bi][:, :].rearrange("(n l r) c -> l n r c", l=128, r=d)
            blk = 0
            for n in range(nseg):
                for r_ in range(d):
                    cols = slice(n * seg + r_, (n + 1) * seg, d)
                    pcols = slice((n - 1) * seg + r_, n * seg, d)
                    bcur = n * d + r_
                    bprev = (n - 1) * d + r_
                    po = [B[6], B[7]]
                    osb = self.osb[blk % 2]
                    def scores(h):
                        nonlocal hb
                        hp, p0 = h // 2, (h % 2) * 64
                        ps = B[hb % 6]
                        o0 = 0
                        pe_ = self.pexp[hb % 6]
                        hb += 1
                        c0 = 0 if n > 0 else 128
                        if n > 0:
                            self.mm(ps[:, o0:o0 + 128], self.KT[p0:p0 + 64, hp, pcols], self.QT[p0:p0 + 64, hp, cols], r=[self.KT, self.QT], w=[ps],
                                    start=True, stop=True, signal=False)
                        self.mm(ps[:, o0 + 128:o0 + 256], self.KT[p0:p0 + 64, hp, cols], self.QT[p0:p0 + 64, hp, cols], r=[self.KT, self.QT], w=[ps],
                                start=True, stop=True)
                        self.S(lambda e, pe_=pe_, ps=ps, c0=c0, o0=o0: e.activation(out=pe_[:, c0:256], in_=ps[:, o0 + c0:o0 + 256], func=AF.Exp, scale=0.125),
                               r=[ps], w=[pe_])
                        mk = self.V if (h % 2 == 0) else self.G
                        mk(lambda e, pe_=pe_, c0=c0: e.tensor_tensor(out=pe_[:, c0:256], in0=pe_[:, c0:256], in1=self.m01[:, c0:256], op=ALU.mult),
                           r=[pe_, self.m01], w=[pe_])
                        return pe_

                    def pv(h, pe_):
                        pob = po[h // 4]
                        oc = slice((h % 4) * 65, (h % 4) * 65 + 65)
                        if n > 0:
                            self.mm(pob[:, oc], pe_[:, 0:128], vb[:, bprev, h * 65:(h + 1) * 65], r=[pe_, vb], w=[pob], start=True, stop=False)
                        self.mm(pob[:, oc], pe_[:, 128:256], vb[:, bcur, h * 65:(h + 1) * 65], r=[pe_, vb], w=[pob], start=(n == 0), stop=True)
                    pend = []
                    for h in range(8):
                        pend.append((h, scores(h)))
                        if len(pend) > 4:
                            pv(*pend.pop(0))
                    while pend:
                        pv(*pend.pop(0))
                    self.V(lambda e, osb=osb, po=po: e.tensor_copy(out=osb[:, 0:260], in_=po[0][:, 0:260]), r=[po[0]], w=[osb])
                    self.V(lambda e, osb=osb, po=po: e.tensor_copy(out=osb[:, 260:520], in_=po[1][:, 0:260]), r=[po[1]], w=[osb])
                    self.dma("sp", odst[:, n, r_, :], osb[:, :], r=[osb], w=[self.obr[bi]])
                    blk += 1

    def s5_io(self):
        i = self.inp
        self.lam_re = i("lam_re", [2, 1024])
        self.lam_im = i("lam_im", [2, 1024])
        self.log_dt = i("log_dt", [2, 16])
        self.ssm_bT = i("ssm_bT", [2, 2, 128, 2, 64])
        self.ssm_cT = i("ssm_cT", [2, 16, 128, 16])
        self.ssm_dT = i("ssm_dT", [2, 128, 2])
        self.glu_w = i("glu_w", [2, 256, 256])
        self.glu_bT = i("glu_bT", [2, 128, 2])
        self.coutT = self.scratch("coutT", [256, T], BF16)
        self.obr = [self.scratch(f"obr{i}", [T, 520], F32) for i in range(3)]

    def s5_alloc(self):
        s = self.sb
        self.Bblk = s("Bblk", [128, 2, 8, 2, 64], BF16)
        self.Cblk = s("Cblk", [128, 16, 128], BF16)
        self.Pre = s("Pre", [128, 16, 64], F32)
        self.PsT = s("PsT", [128, 16, 2, 64], F32)
        self.Qre = s("Qre", [128, 16, 64], F32)
        self.QsT = s("QsT", [128, 16, 2, 64], F32)
        self.glubh = s("glubh", [128, 2], F32)
        self.TriT = s("TriT", [128, 128], BF16)
        self.ones1 = s("ones1", [1, 128], BF16)
        self.dTt = s("dTt", [128, 2], F32)
        self.gluw = s("gluw", [128, 2, 256], BF16)
        self.glub = s("glub", [128, 2], F32)

    def s5_prep(self, l):
        s = self.sb
        V, S, G = self.V, self.S, self.G
        self.push()
        lre = s("lre", [128, 16, 64], F32)
        lim = s("lim", [128, 16, 64], F32)
        ldt = s("ldt", [128, 16], F32)
        self.dma("sp", lre[:, :, :].rearrange("p g n -> p (g n)"), self.lam_re[l].partition_broadcast(128), r=[self.lam_re], w=[lre])
        self.dma("sp", lim[:, :, :].rearrange("p g n -> p (g n)"), self.lam_im[l].partition_broadcast(128), r=[self.lam_im], w=[lim])
        self.dma("sp", ldt[:, :], self.log_dt[l].partition_broadcast(128), r=[self.log_dt], w=[ldt])
        S(lambda e: e.activation(out=ldt[:, :], in_=ldt[:, :], func=AF.Exp), r=[ldt], w=[ldt])
        dtb = ldt[:, :].unsqueeze(2).to_broadcast([128, 16, 64])
        lrd = s("lrd", [128, 16, 64], F32)
        lid = s("lid", [128, 16, 64], F32)
        V(lambda e: e.tensor_tensor(out=lrd[:, :, :], in0=lre[:, :, :], in1=dtb, op=ALU.mult), r=[lre, ldt], w=[lrd])
        V(lambda e: e.tensor_tensor(out=lid[:, :, :], in0=lim[:, :, :], in1=dtb, op=ALU.mult), r=[lim, ldt], w=[lid])
        sp1 = s("sp1", [128, 1], F32)
        G(lambda e: e.iota(sp1[:, :], pattern=[[0, 1]], base=1, channel_multiplier=1, allow_small_or_imprecise_dtypes=True), w=[sp1])
        E = s("E", [128, 16, 64], F32)
        An = s("An", [128, 16, 64], F32)
        sA = s("sA", [128, 16, 64], F32)
        cA = s("cA", [128, 16, 64], F32)
        tmp = s("s5tmp", [128, 16, 64], F32)
        tmi = s("s5tmi", [128, 16, 64], I32)
        qm = s("qm", [128, 16, 64], F32)
        pm = s("pm", [128, 16, 64], F32)
        V(lambda e: e.tensor_scalar(out=E[:, :, :], in0=lrd[:, :, :], scalar1=sp1[:, 0:1], scalar2=None, op0=ALU.mult), r=[lrd, sp1], w=[E])
        V(lambda e: e.tensor_scalar(out=An[:, :, :], in0=lid[:, :, :], scalar1=sp1[:, 0:1], scalar2=None, op0=ALU.mult), r=[lid, sp1], w=[An])
        self.sincos(An, sA, cA, tmp, tmi, None)
        S(lambda e: e.activation(out=qm[:, :, :], in_=E[:, :, :], func=AF.Exp), r=[E], w=[qm])
        S(lambda e: e.activation(out=pm[:, :, :], in_=E[:, :, :], func=AF.Exp, scale=-1.0), r=[E], w=[pm])
        V(lambda e: e.tensor_tensor(out=self.Qre[:, :, :], in0=qm[:, :, :], in1=cA[:, :, :], op=ALU.mult), r=[qm, cA], w=[self.Qre])
        V(lambda e: e.tensor_tensor(out=self.QsT[:, :, 1, :], in0=qm[:, :, :], in1=sA[:, :, :], op=ALU.mult), r=[qm, sA], w=[self.QsT])
        V(lambda e: e.tensor_scalar(out=self.QsT[:, :, 0, :], in0=self.QsT[:, :, 1, :], scalar1=-1.0, scalar2=None, op0=ALU.mult), r=[self.QsT], w=[self.QsT])
        V(lambda e: e.tensor_tensor(out=self.Pre[:, :, :], in0=pm[:, :, :], in1=cA[:, :, :], op=ALU.mult), r=[pm, cA], w=[self.Pre])
        V(lambda e: e.tensor_tensor(out=self.PsT[:, :, 0, :], in0=pm[:, :, :], in1=sA[:, :, :], op=ALU.mult), r=[pm, sA], w=[self.PsT])
        V(lambda e: e.tensor_scalar(out=self.PsT[:, :, 1, :], in0=self.PsT[:, :, 0, :], scalar1=-1.0, scalar2=None, op0=ALU.mult), r=[self.PsT], w=[self.PsT])
        self.sincos(lid, sA, cA, tmp, tmi, None)
        S(lambda e: e.activation(out=qm[:, :, :], in_=lrd[:, :, :], func=AF.Exp), r=[lrd], w=[qm])
        nr, ni = E, An
        V(lambda e: e.tensor_tensor(out=nr[:, :, :], in0=qm[:, :, :], in1=cA[:, :, :], op=ALU.mult), r=[qm, cA], w=[nr])
        V(lambda e: e.tensor_scalar(out=nr[:, :, :], in0=nr[:, :, :], scalar1=-1.0, scalar2=None, op0=ALU.add), r=[nr], w=[nr])
        V(lambda e: e.tensor_tensor(out=ni[:, :, :], in0=qm[:, :, :], in1=sA[:, :, :], op=ALU.mult), r=[qm, sA], w=[ni])
        m2 = pm
        V(lambda e: e.tensor_tensor(out=m2[:, :, :], in0=lre[:, :, :], in1=lre[:, :, :], op=ALU.mult), r=[lre], w=[m2])
        V(lambda e: e.tensor_tensor(out=tmp[:, :, :], in0=lim[:, :, :], in1=lim[:, :, :], op=ALU.mult), r=[lim], w=[tmp])
        V(lambda e: e.tensor_tensor(out=m2[:, :, :], in0=m2[:, :, :], in1=tmp[:, :, :], op=ALU.add), r=[m2, tmp], w=[m2])
        V(lambda e: e.reciprocal(out=m2[:, :, :], in_=m2[:, :, :]), r=[m2], w=[m2])
        fre, fim = sA, cA
        t1 = s("ft1", [128, 16, 64], F32)
        t2 = s("ft2", [128, 16, 64], F32)
        V(lambda e: e.tensor_tensor(out=t1[:, :, :], in0=nr[:, :, :], in1=lre[:, :, :], op=ALU.mult), r=[nr, lre], w=[t1])
        V(lambda e: e.tensor_tensor(out=t2[:, :, :], in0=ni[:, :, :], in1=lim[:, :, :], op=ALU.mult), r=[ni, lim], w=[t2])
        V(lambda e: e.tensor_tensor(out=t1[:, :, :], in0=t1[:, :, :], in1=t2[:, :, :], op=ALU.add), r=[t1, t2], w=[t1])
        V(lambda e: e.tensor_tensor(out=fre[:, :, :], in0=t1[:, :, :], in1=m2[:, :, :], op=ALU.mult), r=[t1, m2], w=[fre])
        V(lambda e: e.tensor_tensor(out=t1[:, :, :], in0=ni[:, :, :], in1=lre[:, :, :], op=ALU.mult), r=[ni, lre], w=[t1])
        V(lambda e: e.tensor_tensor(out=t2[:, :, :], in0=nr[:, :, :], in1=lim[:, :, :], op=ALU.mult), r=[nr, lim], w=[t2])
        V(lambda e: e.tensor_tensor(out=t1[:, :, :], in0=t1[:, :, :], in1=t2[:, :, :], op=ALU.subtract), r=[t1, t2], w=[t1])
        V(lambda e: e.tensor_tensor(out=fim[:, :, :], in0=t1[:, :, :], in1=m2[:, :, :], op=ALU.mult), r=[t1, m2], w=[fim])
        bT = s("bTt", [128, 2, 2, 64], F32)
        self.dma("sp", bT[:, :, :, :], self.ssm_bT[l].rearrange("r p k n -> p r k n"), r=[self.ssm_bT], w=[bT])
        bm = s("bmask", [128, 8], F32)
        one8 = s("one8", [128, 8], F32)
        G(lambda e: e.memset(one8[:, :], 1.0), w=[one8])
        G(lambda e: e.affine_select(out=bm[:, :], in_=one8[:, :], pattern=[[-16, 8]], compare_op=ALU.is_ge, fill=0.0, base=0, channel_multiplier=1),
          r=[one8], w=[bm])
        G(lambda e: e.affine_select(out=bm[:, :], in_=bm[:, :], pattern=[[16, 8]], compare_op=ALU.is_ge, fill=0.0, base=15, channel_multiplier=-1),
          r=[bm], w=[bm])
        bmb = bm[:, :].unsqueeze(2).to_broadcast([128, 8, 64])
        for kc in range(2):
            fr = fre[:, kc * 8:(kc + 1) * 8, :]
            fi = fim[:, kc * 8:(kc + 1) * 8, :]
            bre = bT[:, 0, kc, :].unsqueeze(1).to_broadcast([128, 8, 64])
            bim = bT[:, 1, kc, :].unsqueeze(1).to_broadcast([128, 8, 64])
            a1, a2 = t1[:, 0:8, :], t2[:, 0:8, :]
            V(lambda e, fr=fr, bre=bre: e.tensor_tensor(out=a1, in0=fr, in1=bre, op=ALU.mult), r=[fre, bT], w=[t1])
            V(lambda e, fi=fi, bim=bim: e.tensor_tensor(out=a2, in0=fi, in1=bim, op=ALU.mult), r=[fim, bT], w=[t2])
            V(lambda e: e.tensor_tensor(out=a1, in0=a1, in1=a2, op=ALU.subtract), r=[t1, t2], w=[t1])
            V(lambda e, kc=kc: e.tensor_tensor(out=self.Bblk[:, kc, :, 0, :], in0=a1, in1=bmb, op=ALU.mult), r=[t1, bm], w=[self.Bblk])
            V(lambda e, fr=fr, bim=bim: e.tensor_tensor(out=a1, in0=fr, in1=bim, op=ALU.mult), r=[fre, bT], w=[t1])
            V(lambda e, fi=fi, bre=bre: e.tensor_tensor(out=a2, in0=fi, in1=bre, op=ALU.mult), r=[fim, bT], w=[t2])
            V(lambda e: e.tensor_tensor(out=a1, in0=a1, in1=a2, op=ALU.add), r=[t1, t2], w=[t1])
            V(lambda e, kc=kc: e.tensor_tensor(out=self.Bblk[:, kc, :, 1, :], in0=a1, in1=bmb, op=ALU.mult), r=[t1, bm], w=[self.Bblk])
        cTs = s("cTs", [128, 16, 16], F32)
        self.dma("sp", cTs[:, :, :], self.ssm_cT[l].rearrange("g p c -> p g c"), r=[self.ssm_cT], w=[cTs])
        sg = s("sgn", [128, 1], F32)
        G(lambda e: e.memset(sg[0:64, :], 1.0), w=[sg])
        G(lambda e: e.memset(sg[64:128, :], -1.0), w=[sg])
        G(lambda e: e.memset(self.Cblk[:, :, :], 0.0), w=[self.Cblk])
        for g in range(16):
            c0 = (g % 8) * 16
            S(lambda e, g=g, c0=c0: e.activation(out=self.Cblk[:, g, c0:c0 + 16], in_=cTs[:, g, :], func=AF.Identity, scale=sg[:, 0:1]),
              r=[cTs, sg, self.Cblk], w=[self.Cblk])
        onesb = s("onesb", [128, 128], F32)
        G(lambda e: e.memset(onesb[:, :], 1.0), w=[onesb])
        G(lambda e: e.affine_select(out=onesb[:, :], in_=onesb[:, :], pattern=[[1, 128]], compare_op=ALU.is_ge, fill=0.0, base=0, channel_multiplier=-1),
          r=[onesb], w=[onesb])
        G(lambda e: e.tensor_copy(out=self.TriT[:, :], in_=onesb[:, :]), r=[onesb], w=[self.TriT])
        G(lambda e: e.memset(self.ones1[:, :], 1.0), w=[self.ones1])
        self.dma("sp", self.dTt[:, :], self.ssm_dT[l], r=[self.ssm_dT], w=[self.dTt])
        self.dma("sp", self.glub[:, :], self.glu_bT[l], r=[self.glu_bT], w=[self.glub])
        V(lambda e: e.tensor_scalar(out=self.glubh[:, :], in0=self.glub[:, :], scalar1=0.5, scalar2=None, op0=ALU.mult), r=[self.glub], w=[self.glubh])
        self.dma("pool", self.gluw[:, :, :], self.glu_w[l].rearrange("(k p) n -> p k n", p=128), r=[self.glu_w], w=[self.gluw])
        self.pop()

    def s5_chunk(self, l, j):
        B = self.bank
        V, S, G = self.V, self.S, self.G
        hT = self.hT[j % 2]
        uT = self.uT[j % 2]
        co = self.co[j % 2]
        ps_s = B[7]
        for cc in range(2):
            for k in range(8):
                self.mm(ps_s[:, cc * 128:(cc + 1) * 128], self.win[:, k, 2048 + cc * 128:2048 + (cc + 1) * 128], hT[:, k, :], r=[self.win, hT], w=[ps_s],
                        start=(k == 0), stop=(k == 7), signal=(k == 7 and cc == 1))
        S(lambda e: e.copy(out=uT[:, :, :], in_=ps_s[:, 0:256].rearrange("p (c t) -> p c t", c=2)), r=[ps_s], w=[uT])
        yps = B[7]
        for h in range(2):
            bu = [B[1], B[2]]
            zz = [B[3], B[4]]
            g0 = h * 8
            for q in range(2):
                self.mm(bu[q][:, :], uT[:, h, :], self.Bblk[:, h, q * 4:(q + 1) * 4, :, :].rearrange("p g r n -> p (g r n)"), r=[uT, self.Bblk], w=[bu[q]],
                        start=True, stop=True)
            t1, t2, vv = self.s5t1, self.s5t2, self.s5v
            for q in range(2):
                gs = slice(g0 + q * 4, g0 + q * 4 + 4)
                bu4 = bu[q][:, :].rearrange("p (g r n) -> p g r n", g=4, r=2)
                pc = self.Pre[:, gs, :].unsqueeze(2).to_broadcast([128, 4, 2, 64])
                V(lambda e, q=q, bu4=bu4, pc=pc: e.tensor_tensor(out=t1[:, q * 4:(q + 1) * 4, :, :], in0=bu4, in1=pc, op=ALU.mult), r=[bu[q], self.Pre], w=[t1], x=[t1])
                V(lambda e, q=q, bu4=bu4, gs=gs: e.tensor_tensor(out=t2[:, q * 4:(q + 1) * 4, :, :], in0=bu4[:, :, ::-1, :], in1=self.PsT[:, gs, :, :], op=ALU.mult),
                  r=[bu[q], self.PsT], w=[t2], x=[t2])
            G(lambda e: e.tensor_tensor(out=vv[:, :], in0=t1[:, :, :, :].rearrange("p g r n -> p (g r n)"), in1=t2[:, :, :, :].rearrange("p g r n -> p (g r n)"), op=ALU.add),
              r=[t1, t2], w=[vv])
            for q in range(2):
                self.mm(zz[q][:, :], self.TriT[:, :], vv[:, q * 512:(q + 1) * 512], r=[self.TriT, vv], w=[zz[q]], start=True, stop=False)
                self.mm(zz[q][:, :], self.ones1[:, :], self.x0row[h][:, q * 512:(q + 1) * 512], r=[self.ones1, self.x0row[h]], w=[zz[q]], start=False, stop=True)
            xs = self.s5x[h]
            for q in range(2):
                gs = slice(g0 + q * 4, g0 + q * 4 + 4)
                z4 = zz[q][:, :].rearrange("p (g r n) -> p g r n", g=4, r=2)
                qc = self.Qre[:, gs, :].unsqueeze(2).to_broadcast([128, 4, 2, 64])
                V(lambda e, q=q, z4=z4, qc=qc: e.tensor_tensor(out=t1[:, q * 4:(q + 1) * 4, :, :], in0=z4, in1=qc, op=ALU.mult), r=[zz[q], self.Qre], w=[t1], x=[t1])
                V(lambda e, q=q, z4=z4, gs=gs: e.tensor_tensor(out=t2[:, q * 4:(q + 1) * 4, :, :], in0=z4[:, :, ::-1, :], in1=self.QsT[:, gs, :, :], op=ALU.mult),
                  r=[zz[q], self.QsT], w=[t2], x=[t2])
            G(lambda e, xs=xs: e.tensor_tensor(out=xs[:, :, :].rearrange("p g m -> p (g m)"), in0=t1[:, :, :, :].rearrange("p g r n -> p (g r n)"),
                                        in1=t2[:, :, :, :].rearrange("p g r n -> p (g r n)"), op=ALU.add), r=[t1, t2], w=[xs])
            self.dma("act", self.x0row[h][0:1, :], xs[127:128, :, :].rearrange("p g m -> p (g m)"), r=[xs], w=[self.x0row[h]])
            pxt = B[0]
            pxb = pxt[:, :].bitcast(BF16).rearrange("p (g t) -> p g t", g=8)
            for g in range(8):
                self.tr(pxb[:, g, :], xs[:, g, :], self.identb[:, :], r=[xs, self.identb], w=[pxt], signal=(g == 7))
            V(lambda e, pxb=pxb: e.tensor_copy(out=self.s5xT[:, :, :], in_=pxb), r=[pxt], w=[self.s5xT])
            for g in range(8):
                self.mm(yps[:, 256 + h * 128:256 + (h + 1) * 128], self.Cblk[:, g0 + g, :], self.s5xT[:, g, :], r=[self.Cblk, self.s5xT], w=[yps],
                        start=(g == 0), stop=(g == 7))
        for cc in range(2):
            V(lambda e, cc=cc: e.scalar_tensor_tensor(out=self.yf[:, cc, :], in0=uT[:, cc, :], scalar=self.dTt[:, cc:cc + 1], in1=yps[:, 256 + cc * 128:256 + (cc + 1) * 128],
                                                      op0=ALU.mult, op1=ALU.add), r=[uT, self.dTt, yps], w=[self.yf], x=[self.yf])
        S(lambda e: e.activation(out=self.yg[:, :, :], in_=self.yf[:, :, :], func=AF.Gelu_apprx_tanh), r=[self.yf], w=[self.yg])
        gps = B[5]
        for c2 in range(2):
            for cc in range(2):
                self.mm(gps[:, c2 * 128:(c2 + 1) * 128], self.gluw[:, cc, c2 * 128:(c2 + 1) * 128], self.yg[:, cc, :], r=[self.gluw, self.yg], w=[gps],
                        start=(cc == 0), stop=(cc == 1))
        for c2 in range(2):
            S(lambda e, c2=c2: e.activation(out=self.sgm[:, c2, :], in_=gps[:, c2 * 128:(c2 + 1) * 128], func=AF.Sigmoid, bias=self.glub[:, c2:c2 + 1], scale=1.0),
              r=[gps, self.glub], w=[self.sgm])
        V(lambda e: e.tensor_tensor(out=co[:, :, :], in0=self.yg[:, :, :], in1=self.sgm[:, :, :], op=ALU.mult), r=[self.yg, self.sgm], w=[co])
        self.dma("sp", self.coutT[:, j * 128:(j + 1) * 128].rearrange("(c p) t -> p c t", p=128), co[:, :, :], r=[co], w=[self.coutT])

    def half(self, bank, lo):
        t = Tl(bank.h, bank.b.name + ("lo" if lo else "hi"))
        return t

    def p1v2_alloc(self):
        s = self.sb
        self.xt = [s(f"xt{i}", [128, D], F32) for i in range(2)]
        self.xn = [s(f"xn{i}", [128, D], BF16) for i in range(2)]
        self.st = [s(f"st{i}", [128, 24], F32) for i in range(2)]
        self.st2 = [s(f"stb{i}", [128, 24], F32) for i in range(2)]
        self.hT = [s(f"hT{i}", [128, 8, 128], BF16) for i in range(2)]
        self.gl = [s(f"gl{i}", [128, 512], F32) for i in range(2)]
        self.qbd = [s(f"qbd{i}", [128, 2, 512], BF16) for i in range(2)]
        self.vp = [s(f"vp{i}", [128, 8, 65], BF16) for i in range(2)]
        for i in range(2):
            self.G(lambda e, i=i: e.memset(self.vp[i][:, :, :], 1.0), w=[self.vp[i]])
        self.uT = [s(f"uT{i}", [128, 2, 128], BF16) for i in range(2)]
        self.vnb = s("vnb", [128, 256], F32)
        self.vb = s("vb", [128, 256], BF16)
        self.aout = s("aout", [128, 256], BF16)
        self.qb = s("qb", [128, 2, 512], BF16)
        self.rt = s("rt", [128, 4, 8, 8], F32)
        self.uD = [s(f"uD{i}", [128, 2, 128], F32) for i in range(3)]
        self.p1t = [s(f"p1t{i}", [128, 4, 2, 64], BF16) for i in range(2)]
        self.p2t = [s(f"p2t{i}", [128, 4, 2, 64], BF16) for i in range(2)]
        self.q1 = [s(f"q1_{i}", [128, 4, 2, 64], F32) for i in range(2)]
        self.q2 = [s(f"q2_{i}", [128, 4, 2, 64], F32) for i in range(2)]
        self.qv = [s(f"qv{i}", [128, 512], BF16) for i in range(2)]
        self.qx = [s(f"qx{i}", [128, 4, 128], BF16) for i in range(2)]
        self.qxT = [s(f"qxT{i}", [128, 4, 128], BF16) for i in range(3)]
        self.x0q = [s(f"x0q{i}", [1, 512], BF16) for i in range(4)]
        for i in range(4):
            self.G(lambda e, i=i: e.memset(self.x0q[i][:, :], 0.0), w=[self.x0q[i]])
        self.yf = s("yf2", [128, 2, 128], F32)
        self.yg = s("yg2", [128, 2, 128], BF16)
        self.sgm = s("sgm2", [128, 2, 128], F32)
        self.co = [s(f"co2_{i}", [128, 2, 128], BF16) for i in range(2)]
        B = self.bank
        self.b5lo, self.b5hi = Tl(B[5].h, "b5lo"), Tl(B[5].h, "b5hi")
        self.b6lo, self.b6hi = Tl(B[6].h, "b6lo"), Tl(B[6].h, "b6hi")
        self.b7lo, self.b7hi = Tl(B[7].h, "b7lo"), Tl(B[7].h, "b7hi")

    def p1_front_ln(self, l, j, x_src):
        if j >= NCH:
            return
        i2 = j % 2
        xt, xn, st = self.xt[i2], self.xn[i2], self.st[i2]
        S = self.S
        if j == 0:
            self.dma("sp", xt[:, :], x_src[0:128, :], r=[x_src], w=[xt])
            xt1 = self.xt[1]
            self.dma("sp", xt1[:, :], x_src[128:256, :], r=[x_src], w=[xt1])
        self.ln_stats(xt, lambda a, b: xt[:, a:b], D, st)
        S(lambda e: e.activation(out=xn[:, :], in_=xt[:, :], func=AF.Identity, bias=st[:, 4:5], scale=st[:, 3:4]), r=[xt, st], w=[xn])
        if j + 2 < NCH:
            self.dma("sp", xt[:, :], x_src[(j + 2) * 128:(j + 3) * 128, :], r=[x_src], w=[xt])

    def p1_front_a(self, l, j, x_src):
        if j >= NCH:
            return
        i2 = j % 2
        xn, hT = self.xn[i2], self.hT[i2]
        B = self.bank
        S = self.S
        pT = B[0]
        pTb = pT[:, :].bitcast(BF16).rearrange("p (k t) -> p k t", k=8)
        for k in range(8):
            self.tr(pTb[:, k, :], xn[:, k * 128:(k + 1) * 128], self.identb[:, :], r=[xn, self.identb], w=[pT], signal=(k == 7))
        for k in range(8):
            S(lambda e, k=k: e.activation(out=hT[:, k, :], in_=pTb[:, k, :], func=AF.Identity, scale=self.modp[:, 1, k:k + 1], bias=self.modp[:, 0, k:k + 1]),
              r=[pT, self.modp], w=[hT], x=[hT])

    def p1_front_s(self, l, j):
        if j >= NCH:
            return
        i2 = j % 2
        hT, uT = self.hT[i2], self.uT[i2]
        S = self.S
        ps_s = self.b6lo
        for cc in range(2):
            for k in range(8):
                self.mm(ps_s[:, cc * 128:(cc + 1) * 128], self.win[:, k, 2048 + cc * 128:2048 + (cc + 1) * 128], hT[:, k, :], r=[self.win, hT], w=[ps_s],
                        start=(k == 0), stop=(k == 7), signal=(k == 7 and cc == 1))
        S(lambda e: e.copy(out=uT[:, :, :], in_=ps_s[:, 0:256].rearrange("p (c t) -> p c t", c=2)), r=[ps_s], w=[uT])
        uD = self.uD[j % 3]
        for cc in range(2):
            S(lambda e, cc=cc: e.activation(out=uD[:, cc, :], in_=ps_s[:, cc * 128:(cc + 1) * 128], func=AF.Identity, scale=self.dTt[:, cc:cc + 1]), r=[ps_s, self.dTt], w=[uD], x=[uD])

    def p1_front_p(self, l, j, which):
        if j >= NCH:
            return
        i2 = j % 2
        hT, gl, qf, vp = self.hT[i2], self.gl[i2], self.qbd[i2], self.vp[i2]
        B = self.bank
        S = self.S

        def proj(bank, c0):
            for k in range(8):
                self.mm(bank[:, :], hT[:, k, :], self.win[:, k, c0:c0 + 512], r=[hT, self.win], w=[bank], start=(k == 0), stop=(k == 7))
        if which == 0:
            proj(B[1], 0)
            S(lambda e: e.activation(out=gl[:, :], in_=B[1][:, :], func=AF.Gelu_apprx_tanh), r=[B[1]], w=[gl])
        elif which == 1:
            proj(B[2], 512)
            S(lambda e: e.copy(out=qf[:, 0, :], in_=B[2][:, :]), r=[B[2]], w=[qf])
        elif which == 2:
            proj(B[1], 1024)
            S(lambda e: e.copy(out=qf[:, 1, :], in_=B[1][:, :]), r=[B[1]], w=[qf], x=[qf])
        else:
            proj(B[2], 1536)
            S(lambda e: e.copy(out=vp[:, :, 0:64], in_=B[2][:, :].rearrange("p (h e) -> p h e", e=64)), r=[B[2]], w=[vp])

    def p1_front(self, l, j, x_src):
        self.p1_front_a(l, j, x_src)
        self.p1_front_s(l, j)
        for w_ in range(4):
            self.p1_front_p(l, j, w_)

    def p1_back(self, l, j):
        if j < 0:
            return
        i2 = j % 2
        st2 = self.st2[i2]
        gl, qf, vp, uT = self.gl[i2], self.qbd[i2], self.vp[i2], self.uT[i2]
        vnb, vb, aout, qb, rt = self.vnb, self.vb, self.aout, self.qbd[i2], self.rt
        B = self.bank
        V, S, G = self.V, self.S, self.G
        rows = slice(j * 128, (j + 1) * 128)
        self.ln_stats(gl, lambda a, b: gl[:, 256 + a:256 + b], 256, st2)
        V(lambda e: e.tensor_scalar(out=vnb[:, :], in0=gl[:, 256:512], scalar1=st2[:, 3:4], scalar2=st2[:, 4:5], op0=ALU.mult, op1=ALU.add),
          r=[gl, st2], w=[vnb])
        V(lambda e: e.tensor_tensor(out=vnb[:, :], in0=vnb[:, :], in1=self.glng[:, :], op=ALU.mult), r=[vnb, self.glng], w=[vnb], x=[vnb])
        V(lambda e: e.tensor_tensor(out=vb[:, :], in0=vnb[:, :], in1=self.glnb[:, :], op=ALU.add), r=[vnb, self.glnb], w=[vb], x=[vnb])
        psv = self.b5lo
        for h in range(4):
            self.mm(psv[:, h * 64:(h + 1) * 64], self.wsT[:, h, :], vb[:, h * 64:(h + 1) * 64], r=[self.wsT, vb], w=[psv], start=True, stop=True,
                    signal=(h == 3))
        for h in range(4):
            V(lambda e, h=h: e.scalar_tensor_tensor(out=aout[:, h * 64:(h + 1) * 64], in0=psv[:, h * 64:(h + 1) * 64], scalar=self.gbs[:, h:h + 1],
                                                    in1=gl[:, h * 64:(h + 1) * 64], op0=ALU.add, op1=ALU.mult), r=[psv, self.gbs, gl], w=[aout], x=[aout])
        self.dma("sp", self.mixtok[rows, 0:256], aout[:, :], r=[aout], w=[self.mixtok])
        q4 = qf[:, :, :].rearrange("p a (h e) -> p (a h) e", e=64)
        o4 = qb[:, :, :].rearrange("p a (h e) -> p (a h) e", e=64)
        for a in range(2):
            xa1, xa2 = q4[:, a * 8:(a + 1) * 8, 0:8], q4[:, a * 8:(a + 1) * 8, 8:16]
            cb = self.cs[:, j, :].unsqueeze(1).to_broadcast([128, 8, 8])
            sb_ = self.sn[:, j, :].unsqueeze(1).to_broadcast([128, 8, 8])
            oa = o4[:, a * 8:(a + 1) * 8, :]
            G(lambda e, xa1=xa1, cb=cb: e.tensor_tensor(out=rt[:, 0, :, :], in0=xa1, in1=cb, op=ALU.mult), r=[qf, self.cs], w=[rt])
            G(lambda e, xa2=xa2, sb_=sb_: e.tensor_tensor(out=rt[:, 1, :, :], in0=xa2, in1=sb_, op=ALU.mult), r=[qf, self.sn], w=[rt])
            G(lambda e, xa2=xa2, cb=cb: e.tensor_tensor(out=rt[:, 2, :, :], in0=xa2, in1=cb, op=ALU.mult), r=[qf, self.cs], w=[rt])
            G(lambda e, xa1=xa1, sb_=sb_: e.tensor_tensor(out=rt[:, 3, :, :], in0=xa1, in1=sb_, op=ALU.mult), r=[qf, self.sn], w=[rt])
            G(lambda e, oa=oa: e.tensor_tensor(out=oa[:, :, 0:8], in0=rt[:, 0, :, :], in1=rt[:, 1, :, :], op=ALU.subtract), r=[rt], w=[qb])
            G(lambda e, oa=oa: e.tensor_tensor(out=oa[:, :, 8:16], in0=rt[:, 2, :, :], in1=rt[:, 3, :, :], op=ALU.add), r=[rt], w=[qb])
        pq = B[7]
        pqb = pq[:, :].bitcast(BF16).rearrange("p (a k t) -> p a k t", a=2, k=4)
        for a in range(2):
            for k in range(4):
                self.tr(pqb[:, a, k, :], qb[:, a, k * 128:(k + 1) * 128], self.identb[:, :], r=[qb, self.identb], w=[pq], signal=(a == 1 and k == 3))
        S(lambda e: e.copy(out=self.QT[:, :, j * 128:(j + 1) * 128], in_=pqb[:, 0, :, :]), r=[pq], w=[self.QT])
        S(lambda e: e.copy(out=self.KT[:, :, j * 128:(j + 1) * 128], in_=pqb[:, 1, :, :]), r=[pq], w=[self.KT])
        self.dma("sp", self.vd[rows, :], vp[:, :, :].rearrange("p h e -> p (h e)"), r=[vp], w=[self.vd])

    def s5A(self, i):
        if i < 0 or i >= 4 * NCH:
            return
        j, q = divmod(i, 4)
        kc = q // 2
        gs = slice(q * 4, q * 4 + 4)
        uT = self.uT[j % 2]
        bu = self.bank[3]
        t1, t2 = self.p1t[i % 2], self.p2t[i % 2]
        self.mm(bu[:, :], uT[:, kc, :], self.Bblk[:, kc, (q % 2) * 4:(q % 2) * 4 + 4, :, :].rearrange("p g r n -> p (g r n)"), r=[uT, self.Bblk], w=[bu],
                start=True, stop=True)
        bu4 = bu[:, :].rearrange("p (g r n) -> p g r n", g=4, r=2)
        pc = self.Pre[:, gs, :].unsqueeze(2).to_broadcast([128, 4, 2, 64])
        self.V(lambda e: e.tensor_tensor(out=t1[:, :, :, :], in0=bu4, in1=pc, op=ALU.mult), r=[bu, self.Pre], w=[t1])
        self.V(lambda e: e.tensor_tensor(out=t2[:, :, :, :], in0=bu4[:, :, ::-1, :], in1=self.PsT[:, gs, :, :], op=ALU.mult), r=[bu, self.PsT], w=[t2])

    def s5B(self, i):
        if i < 0 or i >= 4 * NCH:
            return
        j, q = divmod(i, 4)
        gs = slice(q * 4, q * 4 + 4)
        zz = self.bank[4]
        t1, t2 = self.p1t[i % 2], self.p2t[i % 2]
        u1, u2, xs = self.q1[i % 2], self.q2[i % 2], self.qx[i % 2]
        f = lambda t: t[:, :, :, :].rearrange("p g r n -> p (g r n)")
        self.mm(zz[:, :], self.TriT[:, :], f(t1), r=[self.TriT, t1], w=[zz], start=True, stop=False)
        self.mm(zz[:, :], self.TriT[:, :], f(t2), r=[self.TriT, t2], w=[zz], start=False, stop=False)
        self.mm(zz[:, :], self.ones1[:, :], self.x0q[q][:, :], r=[self.ones1, self.x0q[q]], w=[zz], start=False, stop=True)
        z4 = zz[:, :].rearrange("p (g r n) -> p g r n", g=4, r=2)
        qc = self.Qre[:, gs, :].unsqueeze(2).to_broadcast([128, 4, 2, 64])
        self.V(lambda e: e.tensor_tensor(out=u1[:, :, :, :], in0=z4, in1=qc, op=ALU.mult), r=[zz, self.Qre], w=[u1])
        self.V(lambda e: e.tensor_tensor(out=u2[:, :, :, :], in0=z4[:, :, ::-1, :], in1=self.QsT[:, gs, :, :], op=ALU.mult), r=[zz, self.QsT], w=[u2])
        self.G(lambda e: e.tensor_tensor(out=xs[:, :, :].rearrange("p g m -> p (g m)"), in0=f(u1), in1=f(u2), op=ALU.add), r=[u1, u2], w=[xs])
        self.dma("pool", self.x0q[q][0:1, :], xs[127:128, :, :].rearrange("p g m -> p (g m)"), r=[xs], w=[self.x0q[q]])

    def s5C(self, i):
        if i < 0 or i >= 4 * NCH:
            return
        xs, xT = self.qx[i % 2], self.qxT[i % 3]
        pxt = self.b6hi
        pxb = pxt[:, 256:512].bitcast(BF16).rearrange("p (g t) -> p g t", g=4)
        for g in range(4):
            self.tr(pxb[:, g, :], xs[:, g, :], self.identb[:, :], r=[xs, self.identb], w=[pxt], signal=(g == 3))
        self.S(lambda e: e.copy(out=xT[:, :, :], in_=pxb), r=[pxt], w=[xT])

    def s5D(self, i):
        if i < 0 or i >= 4 * NCH:
            return
        j, q = divmod(i, 4)
        kc = q // 2
        xT = self.qxT[i % 3]
        yps = self.b5hi
        for g in range(4):
            self.mm(yps[:, 256 + kc * 128:256 + (kc + 1) * 128], self.Cblk[:, q * 4 + g, :], xT[:, g, :], r=[self.Cblk, xT], w=[yps],
                    start=(q % 2 == 0 and g == 0), stop=(q % 2 == 1 and g == 3))

    def s5_step(self, i):
        self.s5A(i + 2)
        self.s5B(i + 1)
        self.s5C(i)
        if i % 2 == 0:
            self.s5D(i - 2)
            self.s5D(i - 1)

    def s5_tail(self, j):
        if j < 0 or j >= NCH:
            return
        V, S, G = self.V, self.S, self.G
        i2 = j % 2
        uT = self.uT[i2]
        yps = self.b5hi
        co = self.co[i2]
        for cc in range(2):
            V(lambda e, cc=cc: e.scalar_tensor_tensor(out=self.yf[:, cc, :], in0=self.uD[j % 3][:, cc, :], scalar=1.0, in1=yps[:, 256 + cc * 128:256 + (cc + 1) * 128],
                                                      op0=ALU.mult, op1=ALU.add), r=[self.uD[j % 3], yps], w=[self.yf], x=[self.yf])
        S(lambda e: e.activation(out=self.yg[:, :, :], in_=self.yf[:, :, :], func=AF.Gelu_apprx_tanh), r=[self.yf], w=[self.yg])
        gps = self.b5lo
        for c2 in range(2):
            for cc in range(2):
                self.mm(gps[:, c2 * 128:(c2 + 1) * 128], self.gluw[:, cc, c2 * 128:(c2 + 1) * 128], self.yg[:, cc, :], r=[self.gluw, self.yg], w=[gps],
                        start=(cc == 0), stop=(cc == 1))
        for c2 in range(2):
            S(lambda e, c2=c2: e.activation(out=self.sgm[:, c2, :], in_=gps[:, c2 * 128:(c2 + 1) * 128], func=AF.Tanh, bias=self.glubh[:, c2:c2 + 1], scale=0.5),
              r=[gps, self.glubh], w=[self.sgm])
        V(lambda e: e.scalar_tensor_tensor(out=self.sgm[:, :, :], in0=self.sgm[:, :, :], scalar=1.0, in1=self.yg[:, :, :], op0=ALU.add, op1=ALU.mult),
          r=[self.sgm, self.yg], w=[self.sgm])
        V(lambda e: e.tensor_scalar(out=co[:, :, :], in0=self.sgm[:, :, :], scalar1=0.5, scalar2=None, op0=ALU.mult), r=[self.sgm], w=[co])
        self.dma("sp", self.coutT[:, j * 128:(j + 1) * 128].rearrange("(c p) t -> p c t", p=128), co[:, :, :], r=[co], w=[self.coutT])

    def p1_all(self, l, x_src):
        self.p1_front_ln(l, 0, x_src)
        self.p1_front_ln(l, 1, x_src)
        self.p1_front(l, 0, x_src)
        self.s5A(0); self.s5A(1); self.s5B(0)
        for j in range(NCH):
            self.p1_front_ln(l, j + 2, x_src)
            self.p1_front(l, j + 1, x_src)
            self.p1_back(l, j)
            for q in range(4):
                self.s5_step(4 * j + q)
                if q == 0:
                    self.s5_tail(j - 1)
        self.s5_step(4 * NCH)
        self.s5_tail(NCH - 1)
        assert (4 * NCH) % 2 == 0

    CAP = 896
    NSLOT = 16 * 896 + 128
    GROUPS = ((0, 4), (512, 3))

    def p3_io(self):
        i = self.inp
        self.w_out = i("w_out", [2, D, D])
        self.ln1_g = i("ln1_g", [2, D]); self.ln1_b = i("ln1_b", [2, D])
        self.ln2_g = i("ln2_g", [2, D]); self.ln2_b = i("ln2_b", [2, D])
        self.router_w = i("router_w", [D, 16])
        self.router_bias = i("router_bias", [16])
        self.x1d = self.scratch("x1d", [T, D], F32)
        self.xmid = self.scratch("xmid", [T, D], F32)
        self.h2slots = self.scratch("h2slots", [self.NSLOT, D], BF16)
        self.oslots = self.scratch("oslots", [self.NSLOT, D], F32)

    def route_alloc(self):
        s = self.sb
        self.slotA = s("slotA", [128, NCH], I32)
        self.slotB = s("slotB", [128, NCH], I32)
        self.gAB = s("gAB", [128, 2, NCH], F32)

    def p3_alloc(self, l):
        s = self.sb
        self.wout = s("wout", [128, 8, D], BF16)
        for k in range(8):
            self.dma("pool", self.wout[:, k, :], self.w_out[l, k * 128:(k + 1) * 128, :], r=[self.w_out], w=[self.wout])
        self.lng = s("lng", [128, D], F32); self.lnb = s("lnb", [128, D], F32)
        self.dma("sp", self.lng[:, :], self.ln1_g[l].partition_broadcast(128), r=[self.ln1_g], w=[self.lng])
        self.dma("sp", self.lnb[:, :], self.ln1_b[l].partition_broadcast(128), r=[self.ln1_b], w=[self.lnb])
        self.rw = s("rw", [128, 8, 16], F32)
        self.dma("sp", self.rw[:, :, :], self.router_w[:, :].rearrange("(k p) e -> p k e", p=128), r=[self.router_w], w=[self.rw])
        self.rbias = s("rbias", [128, 16], F32)
        self.dma("sp", self.rbias[:, :], self.router_bias[:].partition_broadcast(128), r=[self.router_bias], w=[self.rbias])
        self.o3 = [s(f"o3_{i}", [128, 3, 520], F32) for i in range(2)]
        self.rec = s("rec", [128, 8], F32)
        self.mt = [s(f"mt{i}", [128, D], BF16) for i in range(2)]
        self.mixT = [s(f"mixT{i}", [128, 8, 128], BF16) for i in range(2)]
        self.xres = [s(f"xres{i}", [128, D], F32) for i in range(2)]
        self.yy = s("yy", [128, D], F32)
        self.x1t = [s(f"x1t{i}", [128, D], F32) for i in range(3)]
        self.cT = [s(f"cT{i}", [128, 2, 128], BF16) for i in range(2)]
        self.st3b = s("st3b", [128, 24], F32)
        self.h2f = s("h2f", [128, D], F32)
        self.h2f2 = [self.h2f, s("h2fb", [128, D], F32)]
        self.h2all = s("h2all", [128, NCH, D], BF16)
        self.h2c = [Tl(self.h2all.h, f"h2c{j}") for j in range(NCH)]
        self.scall = s("scall", [128, NCH, 16], F32)
        self.h2T = s("h2T", [128, 8, 128], F32)
        self.st3 = s("st3", [128, 24], F32)
        self.trs = s("trs", [128, 128], F32)
        self.eoff = s("eoff", [128, 16], F32)
        self.trashp = s("trashp", [128, 1], F32)
        self.ones16 = s("ones16", [128, 16], F32)
        G = self.G
        G(lambda e: e.memset(self.ones16[:, :], 1.0), w=[self.ones16])
        G(lambda e: e.affine_select(out=self.trs[:, :], in_=self.onesf[:, :], pattern=[[1, 128]], compare_op=ALU.is_ge, fill=0.0, base=-1, channel_multiplier=-1),
          r=[self.onesf], w=[self.trs])
        G(lambda e: e.iota(self.eoff[:, :], pattern=[[self.CAP, 16]], base=0, channel_multiplier=0, allow_small_or_imprecise_dtypes=True), w=[self.eoff])
        G(lambda e: e.iota(self.trashp[:, :], pattern=[[0, 1]], base=16 * self.CAP, channel_multiplier=1, allow_small_or_imprecise_dtypes=True), w=[self.trashp])

    def p3_L1(self, k):
        if k >= NCH:
            return
        rws = slice(k * 128, (k + 1) * 128)
        o3_, mt_ = self.o3[k % 2], self.mt[k % 2]
        for bi in range(3):
            self.dma("sp", o3_[:, bi, :], self.obr[bi][rws, :], r=[self.obr[bi]], w=[o3_])
        self.dma("sp", mt_[:, 0:256], self.mixtok[rws, 0:256], r=[self.mixtok], w=[mt_])

    def p3_L2(self, k, x_src):
        if k >= NCH:
            return
        rws = slice(k * 128, (k + 1) * 128)
        self.dma("sp", self.cT[k % 2][:, :, :], self.coutT[:, rws].rearrange("(c p) t -> p c t", p=128), r=[self.coutT], w=[self.cT[k % 2]])
        self.dma("sp", self.xres[k % 2][:, :], x_src[rws, :], r=[x_src], w=[self.xres[k % 2]])

    def p3_S1(self, j):
        if j >= NCH or j < 0:
            return
        B = self.bank
        V, S, G = self.V, self.S, self.G
        o3, rec, mt = self.o3[j % 2], self.rec, self.mt[j % 2]
        mixT = self.mixT[j % 2]
        V(lambda e: e.tensor_tensor(out=o3[:, 0, :], in0=o3[:, 0, :], in1=o3[:, 1, :], op=ALU.add), r=[o3], w=[o3], x=[o3])
        V(lambda e: e.tensor_tensor(out=o3[:, 0, :], in0=o3[:, 0, :], in1=o3[:, 2, :], op=ALU.add), r=[o3], w=[o3], x=[o3])
        o8 = o3[:, 0, :].rearrange("p (h e) -> p h e", e=65)
        V(lambda e: e.reciprocal(out=rec[:, :], in_=o8[:, :, 64]), r=[o3], w=[rec])
        V(lambda e: e.tensor_tensor(out=mt[:, 256:768].rearrange("p (h e) -> p h e", e=64), in0=o8[:, :, 0:64], in1=rec[:, :].unsqueeze(2).to_broadcast([128, 8, 64]),
                                    op=ALU.mult), r=[o3, rec], w=[mt])
        pT = B[0]
        pTb = pT[:, :].bitcast(BF16).rearrange("p (k t) -> p k t", k=8)
        for k in range(6):
            self.tr(pTb[:, k, :], mt[:, k * 128:(k + 1) * 128], self.identb[:, :], r=[mt, self.identb], w=[pT], signal=(k == 5))
        S(lambda e: e.copy(out=mixT[:, 0:6, :], in_=pTb[:, 0:6, :]), r=[pT], w=[mixT])

    def p3_S2(self, j):
        if j >= NCH or j < 0:
            return
        B = self.bank
        V, S, G = self.V, self.S, self.G
        rows = slice(j * 128, (j + 1) * 128)
        mixT, cT, xres = self.mixT[j % 2], self.cT[j % 2], self.xres[j % 2]
        wb = [B[1], B[2]] if j % 2 == 0 else [B[6], B[7]]
        for nb in range(2):
            for k in range(8):
                lhs = mixT[:, k, :] if k < 6 else cT[:, k - 6, :]
                self.mm(wb[nb][:, :], lhs, self.wout[:, k, nb * 512:(nb + 1) * 512], r=[mixT, cT, self.wout], w=[wb[nb]], start=(k == 0), stop=(k == 7))
        yy = self.yy
        for nb in range(2):
            cs_ = slice(nb * 512, (nb + 1) * 512)
            V(lambda e, nb=nb, cs_=cs_: e.tensor_tensor(out=yy[:, cs_], in0=wb[nb][:, :], in1=self.opg[:, 0, cs_], op=ALU.mult), r=[wb[nb], self.opg], w=[yy], x=[yy])
        V(lambda e: e.scalar_tensor_tensor(out=yy[:, :], in0=xres[:, :], scalar=float(ALPHA), in1=yy[:, :], op0=ALU.mult, op1=ALU.add), r=[xres, yy], w=[yy], x=[yy])
        self.ln_stats(yy, lambda a, b: yy[:, a:b], D, self.st3, act=True)
        x1 = self.x1t[j % 3]
        S(lambda e: e.activation(out=x1[:, :], in_=yy[:, :], func=AF.Identity, bias=self.st3[:, 4:5], scale=self.st3[:, 3:4]), r=[yy, self.st3], w=[x1])

    def p3_S2b(self, j):
        if j >= NCH or j < 0:
            return
        V = self.V
        rows = slice(j * 128, (j + 1) * 128)
        x1 = self.x1t[j % 3]
        V(lambda e: e.tensor_tensor(out=x1[:, :], in0=x1[:, :], in1=self.lng[:, :], op=ALU.mult), r=[x1, self.lng], w=[x1])
        V(lambda e: e.tensor_tensor(out=x1[:, :], in0=x1[:, :], in1=self.lnb[:, :], op=ALU.add), r=[x1, self.lnb], w=[x1], x=[x1])
        self.dma("sp", self.x1d[rows, :], x1[:, :], r=[x1], w=[self.x1d])

    def p3_S3a(self, j):
        if j >= NCH or j < 0:
            return
        S = self.S
        x1 = self.x1t[j % 3]
        h2f = self.h2f2[j % 2]
        self.ln_stats(x1, lambda a, b: x1[:, a:b], D, self.st3b, act=True)
        S(lambda e: e.activation(out=h2f[:, :], in_=x1[:, :], func=AF.Identity, bias=self.st3b[:, 4:5], scale=self.st3b[:, 3:4]), r=[x1, self.st3b], w=[h2f])

    def p3_S3b(self, j):
        if j >= NCH or j < 0:
            return
        B = self.bank
        V, S, G = self.V, self.S, self.G
        h2f = self.h2f2[j % 2]
        V(lambda e: e.tensor_tensor(out=h2f[:, :], in0=h2f[:, :], in1=self.opg2[:, 1, :], op=ALU.mult), r=[h2f, self.opg2], w=[h2f])
        V(lambda e: e.tensor_tensor(out=h2f[:, :], in0=h2f[:, :], in1=self.opg2[:, 0, :], op=ALU.add), r=[h2f, self.opg2], w=[h2f], x=[h2f])
        S(lambda e: e.copy(out=self.h2all[:, j, :], in_=h2f[:, :]), r=[h2f], w=[self.h2c[j]])
        for half in range(2):
            pt = B[3 + half]
            for k in range(4):
                self.tr(pt[:, k * 128:(k + 1) * 128], h2f[:, (half * 4 + k) * 128:(half * 4 + k + 1) * 128], self.identf[:, :], r=[h2f, self.identf], w=[pt], signal=(k == 3))
            S(lambda e, half=half, pt=pt: e.copy(out=self.h2T[:, half * 4:(half + 1) * 4, :], in_=pt[:, :].rearrange("p (k t) -> p k t", k=4)), r=[pt], w=[self.h2T])
        lg = B[5]
        for k in range(8):
            self.mm(lg[:, 0:16], self.h2T[:, k, :], self.rw[:, k, :], r=[self.h2T, self.rw], w=[lg], start=(k == 0), stop=(k == 7))
        S(lambda e: e.copy(out=self.scall[:, j, :], in_=lg[:, 0:16]), r=[lg], w=[self.scall])
        if (j + 1) % self.RSEG == 0:
            self.p3_route(j + 1 - self.RSEG)

    def p3_all(self, l, x_src):
        self.p3_L1(0)
        for j in range(-2, NCH + 2):
            self.p3_L1(j + 3)
            self.p3_L2(j + 2, x_src)
            self.p3_S1(j + 2)
            self.p3_S2(j + 1)
            self.p3_S2b(j)
            self.p3_S3a(j - 1)
            self.p3_S3b(j - 2)

    RSEG = 8

    def p3_route_alloc(self):
        s = self.sb
        NJ = self.RSEG
        mk = lambda n: s(n, [128, NJ, 16], F32)
        t = {}
        t["big"] = [mk(f"r_{i}") for i in range(14)]
        t["g4"] = [s(f"r4_{i}", [128, NJ, 4], F32) for i in range(4)]
        t["g1"] = [s(f"r1_{i}", [128, NJ], F32) for i in range(3)]
        t["mask_e0"] = mk("mask_e0")
        t["mask_j0"] = s("mask_j0", [128, 16, NJ], F32)
        G = self.G
        G(lambda e: e.memset(t["mask_e0"][:, :, :], 1.0), w=[t["mask_e0"]])
        G(lambda e: e.memset(t["mask_e0"][:, :, 0:1], 0.0), w=[t["mask_e0"]])
        G(lambda e: e.memset(t["mask_j0"][:, :, :], 1.0), w=[t["mask_j0"]])
        G(lambda e: e.memset(t["mask_j0"][:, :, 0:1], 0.0), w=[t["mask_j0"]])
        self.carry = s("carry", [128, 16], F32)
        G(lambda e: e.memset(self.carry[:, :], 0.0), w=[self.carry])
        self._rt = t

    def p3_route(self, j0):
        B = self.bank
        V, S, G = self.V, self.S, self.G
        NJ = self.RSEG
        t = self._rt
        sel, eq, msk, top2, chosen, gw, tmp, cum, pos, valid, slotv, baseT, totT, sc = t["big"]
        m1, m2, gs, gsel = t["g4"]
        gmax, gsum, t32 = t["g1"]
        mask_e0, mask_j0 = t["mask_e0"], t["mask_j0"]
        f2 = lambda t_: t_[:, :, :].rearrange("p j e -> p (j e)")
        S(lambda e: e.activation(out=f2(sc), in_=self.scall[:, j0:j0 + NJ, :].rearrange("p j e -> p (j e)"), func=AF.Sigmoid), r=[self.scall], w=[sc])
        g4 = lambda t: t[:, :, :].rearrange("p j (g i) -> p (j g) i", i=4)
        b4 = lambda t: t[:, :, :].rearrange("p j g -> p (j g)").unsqueeze(2).to_broadcast([128, NJ * 4, 4])
        V(lambda e: e.tensor_tensor(out=sel[:, :, :], in0=sc[:, :, :], in1=self.rbias[:, :].unsqueeze(1).to_broadcast([128, NJ, 16]), op=ALU.add), r=[sc, self.rbias], w=[sel])
        V(lambda e: e.tensor_reduce(out=m1[:, :, :].rearrange("p j g -> p (j g)"), in_=g4(sel), axis=AX.X, op=ALU.max), r=[sel], w=[m1])
        V(lambda e: e.tensor_tensor(out=g4(eq), in0=g4(sel), in1=b4(m1), op=ALU.is_equal), r=[sel, m1], w=[eq])
        V(lambda e: e.scalar_tensor_tensor(out=f2(msk), in0=f2(eq), scalar=-1e9, in1=f2(sel), op0=ALU.mult, op1=ALU.add), r=[eq, sel], w=[msk])
        V(lambda e: e.tensor_reduce(out=m2[:, :, :].rearrange("p j g -> p (j g)"), in_=g4(msk), axis=AX.X, op=ALU.max), r=[msk], w=[m2])
        V(lambda e: e.tensor_tensor(out=gs[:, :, :], in0=m1[:, :, :], in1=m2[:, :, :], op=ALU.add), r=[m1, m2], w=[gs])
        V(lambda e: e.tensor_reduce(out=gmax[:, :], in_=gs[:, :, :], axis=AX.X, op=ALU.max), r=[gs], w=[gmax])
        V(lambda e: e.tensor_tensor(out=gsel[:, :, :], in0=gs[:, :, :], in1=gmax[:, :].unsqueeze(2).to_broadcast([128, NJ, 4]), op=ALU.is_equal), r=[gs, gmax], w=[gsel])
        V(lambda e: e.tensor_tensor(out=g4(top2), in0=g4(sel), in1=b4(m2), op=ALU.is_ge), r=[sel, m2], w=[top2])
        V(lambda e: e.tensor_tensor(out=g4(chosen), in0=g4(top2), in1=b4(gsel), op=ALU.mult), r=[top2, gsel], w=[chosen])
        V(lambda e: e.tensor_tensor(out=gw[:, :, :], in0=chosen[:, :, :], in1=sc[:, :, :], op=ALU.mult), r=[chosen, sc], w=[gw])
        V(lambda e: e.tensor_reduce(out=gsum[:, :], in_=gw[:, :, :], axis=AX.X, op=ALU.add), r=[gw], w=[gsum])
        V(lambda e: e.reciprocal(out=gsum[:, :], in_=gsum[:, :]), r=[gsum], w=[gsum])
        V(lambda e: e.tensor_tensor(out=gw[:, :, :], in0=gw[:, :, :], in1=gsum[:, :].unsqueeze(2).to_broadcast([128, NJ, 16]), op=ALU.mult), r=[gw, gsum], w=[gw])
        cbk = B[5]
        W = NJ * 16
        self.mm(cbk[:, 128:128 + W], self.trs[:, :], f2(chosen), r=[self.trs, chosen], w=[cbk], start=True, stop=True)
        self.mm(cbk[:, 256:256 + W], self.onesf[:, :], f2(chosen), r=[self.onesf, chosen], w=[cbk], start=True, stop=True)
        V(lambda e: e.tensor_copy(out=f2(totT).rearrange("p (e j) -> p e j", e=16),
                                  in_=cbk[:, 256:256 + W].rearrange("p (j e) -> p e j", e=16)), r=[cbk], w=[totT])
        V(lambda e: e.tensor_tensor_scan(out=f2(baseT), data0=mask_j0[:, :, :].rearrange("p e j -> p (e j)"), data1=f2(totT), initial=0.0, op0=ALU.mult, op1=ALU.add),
          r=[mask_j0, totT], w=[baseT])
        V(lambda e: e.tensor_tensor(out=f2(baseT), in0=f2(baseT), in1=f2(totT), op=ALU.subtract), r=[baseT, totT], w=[baseT])
        bT3 = f2(baseT).rearrange("p (e j) -> p e j", e=16)
        tT3 = f2(totT).rearrange("p (e j) -> p e j", e=16)
        V(lambda e: e.tensor_tensor(out=bT3, in0=bT3, in1=self.carry[:, :].unsqueeze(2).to_broadcast([128, 16, NJ]), op=ALU.add), r=[baseT, self.carry], w=[baseT])
        V(lambda e: e.tensor_tensor(out=self.carry[:, :], in0=bT3[:, :, NJ - 1], in1=tT3[:, :, NJ - 1], op=ALU.add), r=[baseT, totT], w=[self.carry])
        V(lambda e: e.tensor_tensor(out=pos[:, :, :], in0=cbk[:, 128:128 + W].rearrange("p (j e) -> p j e", e=16),
                                    in1=f2(baseT).rearrange("p (e j) -> p j e", e=16), op=ALU.add), r=[cbk, baseT], w=[pos])
        V(lambda e: e.tensor_scalar(out=f2(valid), in0=f2(pos), scalar1=float(self.CAP), scalar2=None, op0=ALU.is_lt), r=[pos], w=[valid])
        V(lambda e: e.tensor_tensor(out=slotv[:, :, :], in0=pos[:, :, :], in1=self.eoff[:, :].unsqueeze(1).to_broadcast([128, NJ, 16]), op=ALU.add), r=[pos, self.eoff], w=[slotv])
        V(lambda e: e.tensor_scalar(out=f2(slotv), in0=f2(slotv), scalar1=self.trashp[:, 0:1], scalar2=None, op0=ALU.subtract), r=[slotv, self.trashp], w=[slotv])
        V(lambda e: e.tensor_tensor(out=f2(slotv), in0=f2(slotv), in1=f2(valid), op=ALU.mult), r=[slotv, valid], w=[slotv])
        V(lambda e: e.tensor_scalar(out=f2(slotv), in0=f2(slotv), scalar1=self.trashp[:, 0:1], scalar2=None, op0=ALU.add), r=[slotv, self.trashp], w=[slotv])
        V(lambda e: e.tensor_tensor(out=f2(gw), in0=f2(gw), in1=f2(valid), op=ALU.mult), r=[gw, valid], w=[gw])
        V(lambda e: e.tensor_tensor_scan(out=f2(cum), data0=f2(mask_e0), data1=f2(chosen), initial=0.0, op0=ALU.mult, op1=ALU.add), r=[mask_e0, chosen], w=[cum])
        for which, dsti in ((1.0, self.slotA), (2.0, self.slotB)):
            wi = int(which) - 1
            V(lambda e, which=which: e.tensor_scalar(out=f2(tmp), in0=f2(cum), scalar1=float(which), scalar2=None, op0=ALU.is_equal), r=[cum], w=[tmp])
            V(lambda e: e.tensor_tensor(out=f2(tmp), in0=f2(tmp), in1=f2(chosen), op=ALU.mult), r=[tmp, chosen], w=[tmp])
            V(lambda e: e.tensor_tensor(out=f2(eq), in0=f2(tmp), in1=f2(gw), op=ALU.mult), r=[tmp, gw], w=[eq])
            V(lambda e, wi=wi: e.tensor_reduce(out=self.gAB[:, wi, j0:j0 + NJ], in_=eq[:, :, :], axis=AX.X, op=ALU.add), r=[eq], w=[self.gAB])
            V(lambda e: e.tensor_tensor(out=f2(tmp), in0=f2(tmp), in1=f2(slotv), op=ALU.mult), r=[tmp, slotv], w=[tmp])
            V(lambda e: e.tensor_reduce(out=t32[:, :], in_=tmp[:, :, :], axis=AX.X, op=ALU.add), r=[tmp], w=[t32])
            V(lambda e: e.tensor_scalar(out=t32[:, :], in0=t32[:, :], scalar1=0.0, scalar2=float(self.NSLOT - 1), op0=ALU.max, op1=ALU.min), r=[t32], w=[t32])
            V(lambda e, dsti=dsti: e.tensor_copy(out=dsti[:, j0:j0 + NJ], in_=t32[:, :]), r=[t32], w=[dsti])
        for j in range(j0, j0 + NJ):
            for dsti in (self.slotA, self.slotB):
                self.cx.dma("pool", None, None, reads=[self.h2c[j].b, dsti.b], writes=[],
                            fn=lambda e, dsti=dsti, j=j: e.indirect_dma_start(out=self.h2slots[:, :], out_offset=bass.IndirectOffsetOnAxis(ap=dsti[:, j:j + 1], axis=0),
                                                                              in_=self.h2all[:, j, :], in_offset=None))

    def p4_io(self):
        i = self.inp
        self.w_gate = i("exp_w_gate", [2, 16, D, 512])
        self.w_up = i("exp_w_up", [2, 16, D, 512])
        self.w_down = i("exp_w_down", [2, 16, 512, D])

    def zero_slots(self):
        self.push()
        zt = self.sb("zt", [128, D], F32)
        self.G(lambda e: e.memset(zt[:, :], 0.0), w=[zt])
        self.dma("sp", self.oslots[16 * self.CAP:16 * self.CAP + 128, :], zt[:, :], r=[zt], w=[self.oslots])
        self.pop()

    def p4_experts(self, l):
        B = self.bank
        V, S, G = self.V, self.S, self.G
        s = self.sb
        wg = [s(f"wg{i}", [128, 8, 512], BF16) for i in range(2)]
        wu = [s(f"wu{i}", [128, 8, 512], BF16) for i in range(2)]
        wd = [s(f"wd{i}", [128, 4, D], BF16) for i in range(2)]
        rt = [s(f"rtok{i}", [128, 4, D], BF16) for i in range(2)]
        rT = s("rT", [128, 8, 512], BF16)
        sil = [s(f"sil{i}", [128, 512], BF16) for i in range(2)]
        hidT = s("hidT", [128, 4, 512], BF16)
        osb = [s(f"eosb{i}", [128, D], F32) for i in range(2)]

        def load_w(e):
            i = e % 2
            self.dma("pool", wg[i][:, :, :], self.w_gate[l, e].rearrange("(k p) f -> p k f", p=128), r=[self.w_gate], w=[wg[i]])
            self.dma("pool", wu[i][:, :, :], self.w_up[l, e].rearrange("(k p) f -> p k f", p=128), r=[self.w_up], w=[wu[i]])
            self.dma("pool", wd[i][:, :, :], self.w_down[l, e].rearrange("(k p) f -> p k f", p=128), r=[self.w_down], w=[wd[i]])

        glist = [(e, off, nb) for e in range(16) for (off, nb) in self.GROUPS]
        load_w(0)
        ob = 0

        def load_rows(gi):
            e, off, nb = glist[gi]
            r0 = e * self.CAP + off
            rtk = rt[gi % 2]
            self.dma("sp", rtk[:, 0:nb, :], self.h2slots[r0:r0 + nb * 128, :].rearrange("(b p) d -> p b d", p=128), r=[self.h2slots], w=[rtk])
        load_rows(0)
        for gi, (e, off, nb) in enumerate(glist):
            if off == 0 and e + 1 < 16:
                load_w(e + 1)
            i = e % 2
            r0 = e * self.CAP + off
            N = nb * 128
            rtk = rt[gi % 2]
            if gi + 1 < len(glist):
                load_rows(gi + 1)
            for blk in range(nb):
                pT = B[blk % 2]
                pTb = pT[:, :].bitcast(BF16).rearrange("p (k t) -> p k t", k=8)
                for k in range(8):
                    self.tr(pTb[:, k, :], rtk[:, blk, k * 128:(k + 1) * 128], self.identb[:, :], r=[rtk, self.identb], w=[pT], signal=(k == 7))
                if blk % 2 == 0:
                    V(lambda e_, blk=blk, pTb=pTb: e_.tensor_copy(out=rT[:, :, blk * 128:(blk + 1) * 128], in_=pTb), r=[pT], w=[rT])
                else:
                    S(lambda e_, blk=blk, pTb=pTb: e_.copy(out=rT[:, :, blk * 128:(blk + 1) * 128], in_=pTb), r=[pT], w=[rT])
            for fc in range(4):
                pg, pu = B[2 + 2 * (fc % 2)], B[3 + 2 * (fc % 2)]
                for k in range(8):
                    self.mm(pg[:, 0:N], wg[i][:, k, fc * 128:(fc + 1) * 128], rT[:, k, 0:N], r=[wg[i], rT], w=[pg], start=(k == 0), stop=(k == 7))
                for k in range(8):
                    self.mm(pu[:, 0:N], wu[i][:, k, fc * 128:(fc + 1) * 128], rT[:, k, 0:N], r=[wu[i], rT], w=[pu], start=(k == 0), stop=(k == 7))
                sl = sil[fc % 2]
                S(lambda e_, sl=sl, pg=pg, N=N: e_.activation(out=sl[:, 0:N], in_=pg[:, 0:N], func=AF.Silu), r=[pg], w=[sl])
                V(lambda e_, sl=sl, pu=pu, fc=fc, N=N: e_.tensor_tensor(out=hidT[:, fc, 0:N], in0=pu[:, 0:N], in1=sl[:, 0:N], op=ALU.mult), r=[pu, sl], w=[hidT])
            for blk in range(nb):
                o = osb[ob % 2]
                ob += 1
                for half in range(2):
                    pd = B[6 + half]
                    for fc in range(4):
                        self.mm(pd[:, :], hidT[:, fc, blk * 128:(blk + 1) * 128], wd[i][:, fc, half * 512:(half + 1) * 512], r=[hidT, wd[i]], w=[pd],
                                start=(fc == 0), stop=(fc == 3))
                    V(lambda e_, o=o, pd=pd, half=half: e_.tensor_tensor(out=o[:, half * 512:(half + 1) * 512], in0=pd[:, :],
                                                                         in1=self.opg[:, 1, half * 512:(half + 1) * 512], op=ALU.mult), r=[pd, self.opg], w=[o], x=[o])
                self.dma("sp", self.oslots[r0 + blk * 128:r0 + (blk + 1) * 128, :], o[:, :], r=[o], w=[])

    def p5_alloc(self, l):
        s = self.sb
        self.lng2 = s("lng2", [128, D], F32); self.lnb2 = s("lnb2", [128, D], F32)
        self.dma("sp", self.lng2[:, :], self.ln2_g[l].partition_broadcast(128), r=[self.ln2_g], w=[self.lng2])
        self.dma("sp", self.lnb2[:, :], self.ln2_b[l].partition_broadcast(128), r=[self.ln2_b], w=[self.lnb2])
        self.rA = [s(f"rA{i}", [128, D], F32) for i in range(2)]
        self.rB = [s(f"rB{i}", [128, D], F32) for i in range(2)]
        self.x1r = [s(f"x1r{i}", [128, D], F32) for i in range(2)]
        self.x2t = [s(f"x2t{i}", [128, D], F32) for i in range(2)]
        self.st5 = s("st5", [128, 24], F32)
        self.y5 = [s(f"y5_{i}", [128, D], F32) for i in range(2)]

    def p5_loads(self, jj):
        if jj >= NCH:
            return
        rA_, rB_, x1r_ = self.rA[jj % 2], self.rB[jj % 2], self.x1r[jj % 2]
        self.cx.dma("pool", None, None, reads=[self.oslots.b, self.slotA.b], writes=[rA_.b],
                    fn=lambda e: e.indirect_dma_start(out=rA_[:, :], out_offset=None, in_=self.oslots[:, :],
                                                      in_offset=bass.IndirectOffsetOnAxis(ap=self.slotA[:, jj:jj + 1], axis=0)))
        self.cx.dma("pool", None, None, reads=[self.oslots.b, self.slotB.b], writes=[rB_.b],
                    fn=lambda e: e.indirect_dma_start(out=rB_[:, :], out_offset=None, in_=self.oslots[:, :],
                                                      in_offset=bass.IndirectOffsetOnAxis(ap=self.slotB[:, jj:jj + 1], axis=0)))
        self.dma("sp", x1r_[:, :], self.x1d[jj * 128:(jj + 1) * 128, :], r=[self.x1d], w=[x1r_])

    def p5_S1(self, j):
        if j >= NCH:
            return
        V, S, G = self.V, self.S, self.G
        rA, rB, x1r, y5 = self.rA[j % 2], self.rB[j % 2], self.x1r[j % 2], self.y5[j % 2]
        S(lambda e: e.activation(out=rA[:, :], in_=rA[:, :], func=AF.Identity, scale=self.gAB[:, 0, j:j + 1]), r=[rA, self.gAB], w=[rA])
        V(lambda e: e.scalar_tensor_tensor(out=rA[:, :], in0=rB[:, :], scalar=self.gAB[:, 1, j:j + 1], in1=rA[:, :], op0=ALU.mult, op1=ALU.add), r=[rB, rA, self.gAB], w=[rA])
        V(lambda e: e.scalar_tensor_tensor(out=y5[:, :], in0=x1r[:, :], scalar=float(ALPHA), in1=rA[:, :], op0=ALU.mult, op1=ALU.add), r=[x1r, rA], w=[y5], x=[rA])

    def p5_S2(self, j, dst):
        if j >= NCH or j < 0:
            return
        S = self.S
        y5, x2 = self.y5[j % 2], self.x2t[j % 2]
        self.ln_stats(y5, lambda a, b: y5[:, a:b], D, self.st5, act=True)
        S(lambda e: e.activation(out=x2[:, :], in_=y5[:, :], func=AF.Identity, bias=self.st5[:, 4:5], scale=self.st5[:, 3:4]), r=[y5, self.st5], w=[x2])

    def p5_S3(self, j, dst):
        if j >= NCH or j < 0:
            return
        V, G = self.V, self.G
        rows = slice(j * 128, (j + 1) * 128)
        x2 = self.x2t[j % 2]
        V(lambda e: e.tensor_tensor(out=x2[:, :], in0=x2[:, :], in1=self.lng2[:, :], op=ALU.mult), r=[x2, self.lng2], w=[x2])
        V(lambda e: e.tensor_tensor(out=x2[:, :], in0=x2[:, :], in1=self.lnb2[:, :], op=ALU.add), r=[x2, self.lnb2], w=[x2], x=[x2])
        self.dma("sp", dst[rows, :], x2[:, :], r=[x2], w=[dst])

    def p5_all(self, l, dst):
        self.p5_loads(0); self.p5_loads(1)
        self.p5_S1(0)
        self.p5_loads(2)
        self.p5_S1(1)
        self.p5_S2(0, dst)
        for j in range(NCH):
            self.p5_loads(j + 3)
            self.p5_S1(j + 2)
            self.p5_S2(j + 1, dst)
            self.p5_S3(j, dst)

    def build(self):
        self.declare_io(); self.s5_io(); self.p3_io(); self.p4_io()
        self.setup()
        x_src = self.x_in
        for l in range(self.nlayers):
            dst = self.out if l == self.nlayers - 1 else self.xmid
            self.push()
            self.layer_alloc(); self.route_alloc(); self.layer_prep(l, 0)
            self.zero_slots()
            self.push(); self.qk_alloc()
            self.push(); self.s5_alloc(); self.s5_prep(l); self.load_win(l); self.p1v2_alloc()
            self.p1_all(l, x_src)
            self.pop()
            self.push(); self.p2_alloc(); self.p2_attention(); self.pop()
            self.pop()
            self.push()
            self.opg = self.sb("opg", [128, 2, 1024], F32)
            self.opg2 = self.sb("opg2", [128, 2, 1024], F32)
            self.layer_prep(l, 1)
            self.push(); self.p3_alloc(l)
            self.p3_route_alloc()
            self.p3_all(l, x_src)
            self.pop()
            self.push(); self.p4_experts(l); self.pop()
            self.push(); self.p5_alloc(l)
            self.p5_all(l, dst)
            self.pop()
            self.pop()
            self.pop()
            x_src = dst
        if getattr(self, "dbg_hook", None):
            self.dbg_hook(self)
        self.finish()


def make_inputs(inp, b):
    c = np.ascontiguousarray
    f = lambda k: np.asarray(inp[k])
    br, bi = f("ssm_b_re"), f("ssm_b_im")
    def bl(a):
        L = a.shape[0]
        return a.reshape(L, 2, 8, 64, 16).transpose(0, 2, 4, 1, 3).reshape(L, 128, 2, 64)
    bT = np.stack([bl(br), bl(bi)], axis=1)
    cr, ci = f("ssm_c_re"), f("ssm_c_im")
    cT = np.concatenate([cr.transpose(0, 1, 3, 2), ci.transpose(0, 1, 3, 2)], axis=2)
    L = br.shape[0]
    d = {
        "x": c(f("x")[b]), "ccol": c(f("c")[b].reshape(8, 128).T), "pos": c(f("positions")[b].reshape(32, 128).T),
        "ada_w": f("ada_w"), "ada_b": f("ada_b"), "w_in": f("w_in"), "gm_ln_g": f("gm_ln_g"), "gm_ln_b": f("gm_ln_b"),
        "gm_ws": f("gm_ws"), "gm_bsT": c(f("gm_bs").transpose(0, 2, 1)),
        "lam_re": c(f("ssm_lam_re").reshape(L, 1024)), "lam_im": c(f("ssm_lam_im").reshape(L, 1024)), "log_dt": f("ssm_log_dt"),
        "ssm_bT": c(bT), "ssm_cT": c(cT), "ssm_dT": c(f("ssm_d").reshape(L, 2, 128).transpose(0, 2, 1)),
        "glu_w": f("glu_w"), "glu_bT": c(f("glu_b").reshape(L, 2, 128).transpose(0, 2, 1)),
        "w_out": f("w_out"), "ln1_g": f("ln1_g"), "ln1_b": f("ln1_b"), "ln2_g": f("ln2_g"), "ln2_b": f("ln2_b"),
        "router_w": f("router_w"), "router_bias": f("router_bias"),
        "exp_w_gate": f("exp_w_gate"), "exp_w_up": f("exp_w_up"), "exp_w_down": f("exp_w_down"),
    }
    return d


_CACHE = {}


def kernel(**inputs):
    n = 8
    if "nc" not in _CACHE:
        nc = bass.Bass("TRN2", target_bir_lowering=False)
        kb = KB(nc)
        kb.build()
        _CACHE["nc"] = nc
        _CACHE["names"] = kb.in_names
    nc = _CACHE["nc"]
    names = _CACHE["names"]
    in_maps = []
    for b in range(n):
        im = make_inputs(inputs, b)
        in_maps.append({k: v for k, v in im.items() if k in names})
    res = run_bass_kernel_spmd(nc, in_maps, core_ids=list(range(n)))
    out = np.stack([np.asarray(r["out"]) for r in res.results], axis=0)
    return out.astype(np.float32)
```

```python
import numpy as np
import concourse.bass as bass
import concourse.mybir as mybir

F32 = mybir.dt.float32
BF16 = mybir.dt.bfloat16
I32 = mybir.dt.int32
U32 = mybir.dt.uint32
AF = mybir.ActivationFunctionType
ALU = mybir.AluOpType
AX = mybir.AxisListType


RELAXED = ()
ALLOW_RELAX = True


class Buf:
    __slots__ = ("w", "r", "name")

    def __init__(self, name=""):
        self.w = None
        self.r = []
        self.name = name


class Ctx:
    def __init__(self, nc, strict_same=False):
        self.nc = nc
        self.strict_same = strict_same
        self.relaxed = set(RELAXED)
        self.engs = {"pe": nc.tensor, "act": nc.scalar, "dve": nc.vector, "pool": nc.gpsimd, "sp": nc.sync}
        self.sem = {}
        self.cnt = {}
        for e in ("pe", "act", "dve", "pool"):
            self.sem[e] = nc.alloc_semaphore("s_" + e)
            self.cnt[e] = 0
        self.dq = {}
        for q, n in (("sp", 10), ("act", 4), ("pool", 8)):
            self.dq[q] = {"sems": [nc.alloc_semaphore(f"d_{q}{i}") for i in range(n)], "vals": [0] * n, "k": 0}
        self.waited = {}
        self.nbuf = 0
        self.out_events = []

    def buf(self, name=""):
        return Buf(name)

    def _wait(self, eng, ev):
        sem, val = ev
        key = (eng, id(sem))
        if self.waited.get(key, 0) >= val:
            return
        self.engs[eng].wait_ge(sem, val)
        self.waited[key] = val

    def _deps(self, eng, reads, writes, relax=()):
        own = self.sem.get(eng)
        rl = set(id(b) for b in relax) if ALLOW_RELAX else set()

        def chk(b, ev):
            if ev[0] is own and (eng == "pe" or id(b) in rl):
                return
            self._wait(eng, ev)
        for b in reads:
            if b.w is not None:
                chk(b, b.w)
        for b in writes:
            if b.w is not None:
                chk(b, b.w)
            for ev in b.r:
                chk(b, ev)

    def _commit(self, ev, reads, writes):
        for b in writes:
            b.w = ev
            b.r = []
        for b in reads:
            b.r.append(ev)
            if len(b.r) > 24:
                b.r = b.r[-24:]

    def op(self, eng, fn, reads=(), writes=(), signal=True, relax=()):
        self._deps(eng, reads, writes, relax)
        inst = fn(self.engs[eng])
        if signal:
            self.cnt[eng] += 1
            inst.then_inc(self.sem[eng], 1)
            ev = (self.sem[eng], self.cnt[eng])
        else:
            ev = (self.sem[eng], self.cnt[eng] + 1)
        self._commit(ev, reads, writes)
        return ev

    def dma(self, q, out, in_, reads=(), writes=(), fn=None, **kw):
        d = self.dq[q]
        i = d["k"] % len(d["sems"])
        d["k"] += 1
        sem = d["sems"][i]
        self._deps(q, reads, writes)
        if d["vals"][i] > 0:
            self._wait(q, (sem, d["vals"][i]))
        if fn is None:
            inst = self.engs[q].dma_start(out=out, in_=in_, **kw)
        else:
            inst = fn(self.engs[q])
        d["vals"][i] += 16
        inst.then_inc(sem, 16)
        ev = (sem, d["vals"][i])
        self._commit(ev, reads, writes)
        return ev

    def barrier(self):
        evs = [(self.sem[e], self.cnt[e]) for e in self.sem if self.cnt[e] > 0]
        for q, d in self.dq.items():
            for sem, v in zip(d["sems"], d["vals"]):
                if v > 0:
                    evs.append((sem, v))
        for eng in ("pe", "act", "dve", "pool", "sp"):
            own = self.sem.get(eng)
            for ev in evs:
                self._wait(eng, ev)

    def finish(self, bufs):
        for b in bufs:
            if b.w is not None:
                self._wait("sp", b.w)
            for ev in b.r:
                self._wait("sp", ev)

from concourse.bass_utils import run_bass_kernel_spmd
import math
import contextlib

T = 4096
D = 1024
NCH = 32
PW = 2304
EPS = 1e-5
ALPHA = (2.0 * 2) ** 0.25
TWO_PI = 2.0 * math.pi
ROPE_THETA = 500000.0


class Tl:
    def __init__(self, h, name=""):
        self.h = h
        self.b = Buf(name)

    def __getitem__(self, k):
        return self.h[k]


class KB:
    def __init__(self, nc, nlayers=2, dbg=(), stop_after=None):
        self.nc = nc
        self.cx = Ctx(nc)
        self.dbg = set(dbg)
        self.stop_after = stop_after
        self.nlayers = nlayers
        self.outs = []
        self.stk = [contextlib.ExitStack()]
        self.nps = 0

    def inp(self, name, shape, dt=F32):
        self.in_names = getattr(self, "in_names", set())
        self.in_names.add(name)
        return Tl(self.nc.dram_tensor(name, list(shape), dt, kind="ExternalInput").ap(), name)

    def outp(self, name, shape, dt=F32):
        t = Tl(self.nc.dram_tensor(name, list(shape), dt, kind="ExternalOutput").ap(), name)
        self.outs.append(t)
        return t

    def scratch(self, name, shape, dt):
        return Tl(self.nc.dram_tensor(name, list(shape), dt, kind="Internal").ap(), name)

    def sb(self, name, shape, dt):
        self.nsb = getattr(self, "nsb", 0) + 1
        h = self.stk[-1].enter_context(self.nc.sbuf_tensor(f"{name}_{self.nsb}", list(shape), dt))
        return Tl(h, name)

    def push(self):
        self.stk.append(contextlib.ExitStack())

    def pop(self):
        self.cx.barrier()
        self.stk.pop().close()

    def ps(self, name, shape, dt=F32):
        return Tl(self.nc.alloc_psum_tensor(name, list(shape), dt), name)

    def _rw(self, r, w):
        return [t.b for t in r], [t.b for t in w]

    def V(self, fn, r=(), w=(), x=()):
        r, w = self._rw(r, w)
        return self.cx.op("dve", fn, r, w, relax=[t.b for t in x])

    def S(self, fn, r=(), w=(), x=()):
        r, w = self._rw(r, w)
        return self.cx.op("act", fn, r, w, relax=[t.b for t in x])

    def G(self, fn, r=(), w=(), x=()):
        r, w = self._rw(r, w)
        return self.cx.op("pool", fn, r, w, relax=[t.b for t in x])

    def P(self, fn, r=(), w=(), signal=True):
        r, w = self._rw(r, w)
        return self.cx.op("pe", fn, r, w, signal=signal)

    def dma(self, q, out, in_, r=(), w=(), **kw):
        r, w = self._rw(r, w)
        return self.cx.dma(q, out, in_, r, w, **kw)

    def mm(self, out, lhsT, rhs, r, w, start, stop, signal=None):
        if signal is None:
            signal = stop
        return self.P(lambda e: e.matmul(out, lhsT, rhs, start=start, stop=stop), r, w, signal=signal)

    def tr(self, out, in_, ident, r, w, signal=True):
        return self.P(lambda e: e.transpose(out, in_, ident), r, w, signal=signal)

    def declare_io(self):
        i = self.inp
        self.x_in = i("x", [T, D])
        self.ccol = i("ccol", [128, 8])
        self.pos = i("pos", [128, NCH], I32)
        self.ada_w = i("ada_w", [2, D, 6 * D])
        self.ada_b = i("ada_b", [2, 6 * D])
        self.w_in = i("w_in", [2, D, PW])
        self.gm_ln_g = i("gm_ln_g", [2, 256])
        self.gm_ln_b = i("gm_ln_b", [2, 256])
        self.gm_ws = i("gm_ws", [2, 4, 128, 128])
        self.gm_bsT = i("gm_bsT", [2, 128, 4])
        self.out = self.outp("out", [T, D])
        self.mixtok = self.scratch("mixtok", [T, 1024], BF16)
        self.vd = self.scratch("vd", [T, 520], BF16)

    def consts(self):
        nc = self.nc
        self.identb = self.sb("identb", [128, 128], BF16)
        self.identf = self.sb("identf", [128, 128], F32)
        self.onesf = self.sb("onesf", [128, 128], F32)
        self.eps_t = self.sb("eps_t", [128, 1], F32)
        self.G(lambda e: e.memset(self.onesf[:, :], 1.0), w=[self.onesf])
        self.G(lambda e: e.memset(self.eps_t[:, :], EPS), w=[self.eps_t])
        self.mhalf = self.sb("mhalf", [128, 1], F32)
        self.G(lambda e: e.memset(self.mhalf[:, :], -0.5), w=[self.mhalf])
        self.G(lambda e: e.affine_select(out=self.identf[:, :], in_=self.onesf[:, :], pattern=[[-1, 128]],
                                         compare_op=ALU.is_equal, fill=0.0, base=0, channel_multiplier=1),
               r=[self.onesf], w=[self.identf])
        self.G(lambda e: e.tensor_copy(out=self.identb[:, :], in_=self.identf[:, :]), r=[self.identf], w=[self.identb])
        self.posf = self.sb("posf", [128, NCH], F32)
        self.posi = self.sb("posi", [128, NCH], I32)
        self.dma("sp", self.posi[:, :], self.pos[:, :], r=[self.pos], w=[self.posi])
        self.V(lambda e: e.tensor_copy(out=self.posf[:, :], in_=self.posi[:, :]), r=[self.posi], w=[self.posf])
        self.cs = self.sb("cs", [128, NCH, 8], F32)
        self.sn = self.sb("sn", [128, NCH, 8], F32)
        self.push()
        ang = self.sb("ang", [128, NCH, 8], F32)
        tmp = self.sb("angt", [128, NCH, 8], F32)
        tmi = self.sb("angi", [128, NCH, 8], I32)
        for j in range(8):
            fr = ROPE_THETA ** (-(j * 2.0) / 16.0)
            self.V(lambda e, j=j, fr=fr: e.tensor_scalar(out=ang[:, :, j], in0=self.posf[:, :], scalar1=float(fr), scalar2=None, op0=ALU.mult),
                   r=[self.posf], w=[ang])
        self.sincos(ang, self.sn, self.cs, tmp, tmi, [128, NCH * 8])
        self.pop()

    def _flat(self, t):
        ap = t[:]
        if len(ap.shape) == 2:
            return ap
        names = " ".join(f"a{i}" for i in range(len(ap.shape) - 1))
        return ap.rearrange(f"p {names} -> p ({names})")

    def range_reduce(self, src, dst, tmp, tmi, shift):
        s, d, t, ti = self._flat(src), self._flat(dst), self._flat(tmp), self._flat(tmi)
        self.V(lambda e: e.tensor_scalar(out=t, in0=s, scalar1=float(shift), scalar2=float(1.0 / TWO_PI), op0=ALU.add, op1=ALU.mult),
               r=[src], w=[tmp])
        self.V(lambda e: e.tensor_copy(out=ti, in_=t), r=[tmp], w=[tmi])
        self.V(lambda e: e.tensor_copy(out=t, in_=ti), r=[tmi], w=[tmp])
        self.V(lambda e: e.tensor_scalar(out=t, in0=t, scalar1=float(-TWO_PI), scalar2=float(shift), op0=ALU.mult, op1=ALU.add),
               r=[tmp], w=[tmp])
        self.V(lambda e: e.tensor_tensor(out=d, in0=t, in1=s, op=ALU.add), r=[tmp, src], w=[dst])
        self.V(lambda e: e.tensor_scalar(out=t, in0=d, scalar1=float(math.pi), scalar2=float(-TWO_PI), op0=ALU.is_gt, op1=ALU.mult),
               r=[dst], w=[tmp])
        self.V(lambda e: e.tensor_tensor(out=d, in0=d, in1=t, op=ALU.add), r=[tmp, dst], w=[dst])
        self.V(lambda e: e.tensor_scalar(out=t, in0=d, scalar1=float(-math.pi), scalar2=float(TWO_PI), op0=ALU.is_lt, op1=ALU.mult),
               r=[dst], w=[tmp])
        self.V(lambda e: e.tensor_tensor(out=d, in0=d, in1=t, op=ALU.add), r=[tmp, dst], w=[dst])
        self.V(lambda e: e.tensor_scalar(out=d, in0=d, scalar1=float(math.pi), scalar2=float(-math.pi), op0=ALU.min, op1=ALU.max),
               r=[dst], w=[dst])

    def sincos(self, ang, sn, cs, tmp, tmi, shape):
        self.range_reduce(ang, sn, tmp, tmi, 0.0)
        self.S(lambda e: e.activation(out=self._flat(sn), in_=self._flat(sn), func=AF.Sin), r=[sn], w=[sn])
        self.range_reduce(ang, cs, tmp, tmi, math.pi / 2)
        self.S(lambda e: e.activation(out=self._flat(cs), in_=self._flat(cs), func=AF.Sin), r=[cs], w=[cs])

    def setup(self):
        self.bank = [self.ps(f"bank{i}", [128, 512], F32) for i in range(8)]
        self.consts()

    def layer_alloc(self):
        self.modp = self.sb("modp", [128, 4, 8], F32)

        self.wsT = self.sb("wsT", [128, 4, 128], BF16)
        self.gbs = self.sb("gbs", [128, 4], F32)
        self.glng = self.sb("glng", [128, 256], F32)
        self.glnb = self.sb("glnb", [128, 256], F32)

    def prep_alloc(self):
        self.adaw = [self.sb(f"adaw{i}", [128, 8, 512], F32) for i in range(2)]
        self.adab = [self.sb(f"adab{i}", [128, 512], F32) for i in range(2)]
        self.modc = [self.sb(f"modc{i}", [128, 512], F32) for i in range(2)]
        self.wtmp = self.sb("wtmp", [128, 4, 128], F32)
        ccs = self.sb("ccs", [128, 8], F32)
        self.dma("sp", ccs[:, :], self.ccol[:, :], r=[self.ccol], w=[ccs])
        self.S(lambda e: e.activation(out=ccs[:, :], in_=ccs[:, :], func=AF.Silu), r=[ccs], w=[ccs])
        self.condrep = self.sb("condrep", [128, 8, 128], F32)
        self.V(lambda e: e.tensor_copy(out=self.condrep[:, :, :], in_=ccs[:, :].unsqueeze(2).to_broadcast([128, 8, 128])),
               r=[ccs], w=[self.condrep])


    def load_win(self, l):
        self.win = self.sb("win", [128, 8, PW], BF16)
        for k in range(8):
            self.dma("pool", self.win[:, k, :], self.w_in[l, k * 128:(k + 1) * 128, :], r=[self.w_in], w=[self.win])

    def layer_prep(self, l, part=0):
        self.push()
        self.prep_alloc()
        pb = self.bank[7]
        pt = self.bank[6]
        for n in range(12):
            if (part == 0) != (n // 2 in (0, 1)):
                continue
            aw = self.adaw[n % 2]
            ab = self.adab[n % 2]
            mc = self.modc[n % 2]
            self.dma("sp", aw[:, :, :], self.ada_w[l, :, n * 512:(n + 1) * 512].rearrange("(k p) n -> p k n", p=128),
                     r=[self.ada_w], w=[aw])
            self.dma("sp", ab[:, :], self.ada_b[l, n * 512:(n + 1) * 512].partition_broadcast(128), r=[self.ada_b], w=[ab])
            for k in range(8):
                self.mm(pb[:, :], self.condrep[:, k, :], aw[:, k, :], r=[self.condrep, aw], w=[pb], start=(k == 0), stop=(k == 7))
            which, half = n // 2, n % 2
            if which in (2, 5, 3, 4):
                tgt = self.opg if which in (2, 5) else self.opg2
                gi = {2: 0, 5: 1, 3: 0, 4: 1}[which]
                dst = tgt[:, gi, half * 512:(half + 1) * 512]
                self.V(lambda e, dst=dst: e.tensor_tensor(out=dst, in0=pb[:, :], in1=ab[:, :], op=ALU.add), r=[pb, ab], w=[tgt])
                if which != 3:
                    self.V(lambda e, dst=dst: e.tensor_scalar(out=dst, in0=dst, scalar1=1.0, scalar2=None, op0=ALU.add), r=[tgt], w=[tgt])
            else:
                slot = {0: 0, 1: 1}[which]
                self.V(lambda e: e.tensor_tensor(out=mc[:, :], in0=pb[:, :], in1=ab[:, :], op=ALU.add), r=[pb, ab], w=[mc])
                for b4 in range(4):
                    self.tr(pt[:, b4 * 128:(b4 + 1) * 128], mc[:, b4 * 128:(b4 + 1) * 128], self.identf[:, :], r=[mc, self.identf], w=[pt])
                addc = 1.0 if slot in (1, 3) else 0.0
                for b4 in range(4):
                    self.V(lambda e, b4=b4: e.tensor_scalar(out=self.modp[:, slot, half * 4 + b4:half * 4 + b4 + 1],
                                                            in0=pt[:, b4 * 128:b4 * 128 + 1], scalar1=float(addc), scalar2=None, op0=ALU.add),
                           r=[pt], w=[self.modp])
        if part == 0:
            self.dma("sp", self.wtmp[:, :, :], self.gm_ws[l].rearrange("h t s -> t h s"), r=[self.gm_ws], w=[self.wtmp])
            self.G(lambda e: e.affine_select(out=self.wtmp[:, :, :], in_=self.wtmp[:, :, :], pattern=[[0, 4], [-1, 128]],
                                             compare_op=ALU.is_ge, fill=0.0, base=0, channel_multiplier=1), r=[self.wtmp], w=[self.wtmp])
            for h in range(4):
                self.tr(pt[:, h * 128:(h + 1) * 128], self.wtmp[:, h, :], self.identf[:, :], r=[self.wtmp, self.identf], w=[pt])
            self.V(lambda e: e.tensor_copy(out=self.wsT[:, :, :], in_=pt[:, :].rearrange("p (h t) -> p h t", h=4)), r=[pt], w=[self.wsT])
            self.dma("sp", self.gbs[:, :], self.gm_bsT[l], r=[self.gm_bsT], w=[self.gbs])
            self.dma("sp", self.glng[:, :], self.gm_ln_g[l].partition_broadcast(128), r=[self.gm_ln_g], w=[self.glng])

            self.dma("sp", self.glnb[:, :], self.gm_ln_b[l].partition_broadcast(128), r=[self.gm_ln_b], w=[self.glnb])
        self.pop()

    def ln_stats(self, src, src_ap_fn, n, st, act=False):
        if act:
            return self.ln_stats_act(src, src_ap_fn, n, st)
        nchk = (n + 511) // 512
        w = n // nchk
        for i in range(nchk):
            self.V(lambda e, i=i: e.bn_stats(out=st[:, 8 + i * 6:8 + (i + 1) * 6], in_=src_ap_fn(i * w, (i + 1) * w)), r=[src], w=[st])
        self.V(lambda e: e.bn_aggr(out=st[:, 0:2], in_=st[:, 8:8 + 6 * nchk]), r=[st], w=[st])
        self.V(lambda e: e.tensor_scalar(out=st[:, 2:3], in0=st[:, 1:2], scalar1=float(EPS), scalar2=None, op0=ALU.add), r=[st], w=[st])
        self.G(lambda e: e.tensor_tensor(out=st[:, 3:4], in0=st[:, 2:3], in1=self.mhalf[:, 0:1], op=ALU.pow), r=[st, self.mhalf], w=[st])
        self.V(lambda e: e.tensor_scalar(out=st[:, 4:5], in0=st[:, 0:1], scalar1=-1.0, scalar2=st[:, 3:4], op0=ALU.mult, op1=ALU.mult), r=[st], w=[st])

    def ln_stats_act(self, src, src_ap_fn, n, st):
        nchk = (n + 511) // 512
        w = n // nchk
        for i in range(nchk):
            self.V(lambda e, i=i: e.bn_stats(out=st[:, 8 + i * 6:8 + (i + 1) * 6], in_=src_ap_fn(i * w, (i + 1) * w)), r=[src], w=[st])
        self.V(lambda e: e.bn_aggr(out=st[:, 0:2], in_=st[:, 8:8 + 6 * nchk]), r=[st], w=[st])
        self.S(lambda e: e.activation(out=st[:, 2:3], in_=st[:, 1:2], func=AF.Sqrt, bias=self.eps_t[:, 0:1], scale=1.0), r=[st, self.eps_t], w=[st])
        self.V(lambda e: e.reciprocal(out=st[:, 3:4], in_=st[:, 2:3]), r=[st], w=[st])
        self.V(lambda e: e.tensor_scalar(out=st[:, 4:5], in0=st[:, 0:1], scalar1=-1.0, scalar2=st[:, 3:4], op0=ALU.mult, op1=ALU.mult), r=[st], w=[st])

    def qk_alloc(self):
        self.QT = self.sb("QT", [128, 4, T], BF16)
        self.KT = self.sb("KT", [128, 4, T], BF16)

    def p1_alloc(self):
        s = self.sb
        self.xt = [s(f"xt{i}", [128, D], F32) for i in range(2)]
        self.xn = [s(f"xn{i}", [128, D], BF16) for i in range(2)]
        self.st = [s(f"st{i}", [128, 24], F32) for i in range(2)]
        self.st2 = [s(f"stb{i}", [128, 24], F32) for i in range(2)]
        self.hT = [s(f"hT{i}", [128, 8, 128], BF16) for i in range(2)]
        self.gl = [s(f"gl{i}", [128, 512], F32) for i in range(1)] * 2
        self.vnb = [s(f"vnb{i}", [128, 256], F32) for i in range(1)] * 2
        self.vb = [s(f"vb{i}", [128, 256], BF16) for i in range(1)] * 2
        self.aout = [s(f"aout{i}", [128, 256], BF16) for i in range(1)] * 2
        self.qf = [s(f"qf{i}", [128, 2, 512], F32) for i in range(1)] * 2
        self.qb = [s(f"qb{i}", [128, 2, 512], BF16) for i in range(1)] * 2
        self.rt = [s(f"rt{i}", [128, 4, 8, 8], F32) for i in range(1)] * 2
        self.vp = [s(f"vp{i}", [128, 8, 65], BF16) for i in range(1)] * 2
        for i in range(1):
            self.G(lambda e, i=i: e.memset(self.vp[i][:, :, :], 1.0), w=[self.vp[i]])

    def p1_chunk(self, l, j, x_src):
        i2 = j % 2
        xt, xn, st, st2, hT = self.xt[i2], self.xn[i2], self.st[i2], self.st2[i2], self.hT[i2]
        gl, vnb, vb, aout, qf, qb, rt, vp = self.gl[i2], self.vnb[i2], self.vb[i2], self.aout[i2], self.qf[i2], self.qb[i2], self.rt[i2], self.vp[i2]
        B = self.bank
        rows = slice(j * 128, (j + 1) * 128)
        if j == 0:
            self.dma("sp", xt[:, :], x_src[rows, :], r=[x_src], w=[xt])
        if j + 1 < NCH:
            xtn = self.xt[(j + 1) % 2]
            self.dma("sp", xtn[:, :], x_src[(j + 1) * 128:(j + 2) * 128, :], r=[x_src], w=[xtn])
        self.ln_stats(xt, lambda a, b: xt[:, a:b], D, st)
        self.S(lambda e: e.activation(out=xn[:, :], in_=xt[:, :], func=AF.Identity, bias=st[:, 4:5], scale=st[:, 3:4]), r=[xt, st], w=[xn])
        pT = B[0]
        pTb = pT[:, :].bitcast(BF16).rearrange("p (k t) -> p k t", k=8)
        for k in range(8):
            self.tr(pTb[:, k, :], xn[:, k * 128:(k + 1) * 128], self.identb[:, :], r=[xn, self.identb], w=[pT], signal=(k == 7))
        for k in range(8):
            self.V(lambda e, k=k: e.tensor_scalar(out=hT[:, k, :], in0=pTb[:, k, :], scalar1=self.modp[:, 1, k:k + 1], scalar2=self.modp[:, 0, k:k + 1],
                                                  op0=ALU.mult, op1=ALU.add), r=[pT, self.modp], w=[hT], x=[hT])
        for bi, c0 in ((1, 0), (2, 512), (3, 1024), (4, 1536)):
            for k in range(8):
                self.mm(B[bi][:, :], hT[:, k, :], self.win[:, k, c0:c0 + 512], r=[hT, self.win], w=[B[bi]], start=(k == 0), stop=(k == 7))
        self.S(lambda e: e.activation(out=gl[:, :], in_=B[1][:, :], func=AF.Gelu_apprx_tanh), r=[B[1]], w=[gl])
        self.ln_stats(gl, lambda a, b: gl[:, 256 + a:256 + b], 256, st2)
        self.V(lambda e: e.tensor_scalar(out=vnb[:, :], in0=gl[:, 256:512], scalar1=st2[:, 3:4], scalar2=st2[:, 4:5], op0=ALU.mult, op1=ALU.add),
               r=[gl, st2], w=[vnb])
        self.V(lambda e: e.tensor_tensor(out=vnb[:, :], in0=vnb[:, :], in1=self.glng[:, :], op=ALU.mult), r=[vnb, self.glng], w=[vnb], x=[vnb])
        self.V(lambda e: e.tensor_tensor(out=vb[:, :], in0=vnb[:, :], in1=self.glnb[:, :], op=ALU.add), r=[vnb, self.glnb], w=[vb], x=[vnb])
        psv = B[5]
        for h in range(4):
            self.mm(psv[:, h * 64:(h + 1) * 64], self.wsT[:, h, :], vb[:, h * 64:(h + 1) * 64], r=[self.wsT, vb], w=[psv], start=True, stop=True,
                    signal=(h == 3))
        for h in range(4):
            self.V(lambda e, h=h: e.scalar_tensor_tensor(out=aout[:, h * 64:(h + 1) * 64], in0=psv[:, h * 64:(h + 1) * 64], scalar=self.gbs[:, h:h + 1],
                                                         in1=gl[:, h * 64:(h + 1) * 64], op0=ALU.add, op1=ALU.mult), r=[psv, self.gbs, gl], w=[aout], x=[aout])
        self.dma("sp", self.mixtok[rows, 0:256], aout[:, :], r=[aout], w=[self.mixtok])
        self.S(lambda e: e.copy(out=qf[:, 0, :], in_=B[2][:, :]), r=[B[2]], w=[qf])
        self.S(lambda e: e.copy(out=qf[:, 1, :], in_=B[3][:, :]), r=[B[3]], w=[qf], x=[qf])
        q4 = qf[:, :, :].rearrange("p a (h e) -> p (a h) e", e=64)
        o4 = qb[:, :, :].rearrange("p a (h e) -> p (a h) e", e=64)
        for a in range(2):
            xa1, xa2 = q4[:, a * 8:(a + 1) * 8, 0:8], q4[:, a * 8:(a + 1) * 8, 8:16]
            cb = self.cs[:, j, :].unsqueeze(1).to_broadcast([128, 8, 8])
            sb_ = self.sn[:, j, :].unsqueeze(1).to_broadcast([128, 8, 8])
            oa = o4[:, a * 8:(a + 1) * 8, :]
            self.G(lambda e, xa1=xa1, cb=cb: e.tensor_tensor(out=rt[:, 0, :, :], in0=xa1, in1=cb, op=ALU.mult), r=[qf, self.cs], w=[rt])
            self.G(lambda e, xa2=xa2, sb_=sb_: e.tensor_tensor(out=rt[:, 1, :, :], in0=xa2, in1=sb_, op=ALU.mult), r=[qf, self.sn], w=[rt])
            self.G(lambda e, xa2=xa2, cb=cb: e.tensor_tensor(out=rt[:, 2, :, :], in0=xa2, in1=cb, op=ALU.mult), r=[qf, self.cs], w=[rt])
            self.G(lambda e, xa1=xa1, sb_=sb_: e.tensor_tensor(out=rt[:, 3, :, :], in0=xa1, in1=sb_, op=ALU.mult), r=[qf, self.sn], w=[rt])
            self.G(lambda e, oa=oa: e.tensor_tensor(out=oa[:, :, 0:8], in0=rt[:, 0, :, :], in1=rt[:, 1, :, :], op=ALU.subtract), r=[rt], w=[qb])
            self.G(lambda e, oa=oa: e.tensor_tensor(out=oa[:, :, 8:16], in0=rt[:, 2, :, :], in1=rt[:, 3, :, :], op=ALU.add), r=[rt], w=[qb])
            self.G(lambda e, oa=oa, a=a: e.tensor_copy(out=oa[:, :, 16:64], in_=q4[:, a * 8:(a + 1) * 8, 16:64]), r=[qf], w=[qb])
        pq = B[6]
        pqb = pq[:, :].bitcast(BF16).rearrange("p (a k t) -> p a k t", a=2, k=4)
        for a in range(2):
            for k in range(4):
                self.tr(pqb[:, a, k, :], qb[:, a, k * 128:(k + 1) * 128], self.identb[:, :], r=[qb, self.identb], w=[pq], signal=(a == 1 and k == 3))
        self.S(lambda e: e.copy(out=self.QT[:, :, j * 128:(j + 1) * 128], in_=pqb[:, 0, :, :]), r=[pq], w=[self.QT])
        self.S(lambda e: e.copy(out=self.KT[:, :, j * 128:(j + 1) * 128], in_=pqb[:, 1, :, :]), r=[pq], w=[self.KT])
        self.S(lambda e: e.copy(out=vp[:, :, 0:64], in_=B[4][:, :].rearrange("p (h e) -> p h e", e=64)), r=[B[4]], w=[vp])
        self.dma("sp", self.vd[rows, :], vp[:, :, :].rearrange("p h e -> p (h e)"), r=[vp], w=[self.vd])

    def finish(self):
        bufs = [t.b for t in self.outs]
        self.cx.finish(bufs)

    def p2_alloc(self):
        s = self.sb
        self.vbr = [s(f"vbr{i}", [128, 32, 520], BF16) for i in range(2)]
        self.negm = s("negm", [128, 256], BF16)
        negf = s("negf", [128, 256], F32)
        zf = s("zf", [128, 256], F32)
        self.G(lambda e: e.memset(zf[:, :], 0.0), w=[zf])
        self.G(lambda e: e.affine_select(out=negf[:, 0:128], in_=zf[:, 0:128], pattern=[[-1, 128]], compare_op=ALU.is_ge, fill=-30000.0,
                                         base=0, channel_multiplier=1), r=[zf], w=[negf])
        self.G(lambda e: e.affine_select(out=negf[:, 128:256], in_=zf[:, 128:256], pattern=[[1, 128]], compare_op=ALU.is_ge, fill=-30000.0,
                                         base=0, channel_multiplier=-1), r=[zf], w=[negf])
        self.G(lambda e: e.tensor_copy(out=self.negm[:, :], in_=negf[:, :]), r=[negf], w=[self.negm])
        self.m01 = s("m01", [128, 256], BF16)
        onef = s("onef2", [128, 256], F32)
        self.G(lambda e: e.memset(onef[:, :], 1.0), w=[onef])
        self.G(lambda e: e.affine_select(out=onef[:, 0:128], in_=onef[:, 0:128], pattern=[[-1, 128]], compare_op=ALU.is_ge, fill=0.0,
                                         base=0, channel_multiplier=1), r=[onef], w=[onef])
        self.G(lambda e: e.affine_select(out=onef[:, 128:256], in_=onef[:, 128:256], pattern=[[1, 128]], compare_op=ALU.is_ge, fill=0.0,
                                         base=0, channel_multiplier=-1), r=[onef], w=[onef])
        self.G(lambda e: e.tensor_copy(out=self.m01[:, :], in_=onef[:, :]), r=[onef], w=[self.m01])
        self.pexp = [s(f"pexp{i}", [128, 256], BF16) for i in range(6)]
        self.osb = [s(f"osb{i}", [128, 520], F32) for i in range(2)]

    def p2_attention(self):
        B = self.bank
        hb = 0
        self._hb = 0
        dils = (1, 4, 16)

        def load_vb(bi):
            d = dils[bi]
            vb_ = self.vbr[bi % 2]
            src = self.vd[:, :].rearrange("(n l r) c -> l n r c", l=128, r=d)
            for n in range(T // (128 * d)):
                self.dma("sp", vb_[:, n * d:(n + 1) * d, :], src[:, n, :, :], r=[self.vd], w=[vb_])
        load_vb(0)
        load_vb(1)
        for bi, d in enumerate(dils):
            seg = 128 * d
            nseg = T // seg
            vb = self.vbr[bi % 2]
            if bi == 1:
                load_vb(2)
            odst = self.obr[bi][:, :].rearrange("(n l r) c -> l n r c", l=128, r=d)
            blk = 0
            for n in range(nseg):
                for r_ in range(d):
                    cols = slice(n * seg + r_, (n + 1) * seg, d)
                    pcols = slice((n - 1) * seg + r_, n * seg, d)
                    bcur = n * d + r_
                    bprev = (n - 1) * d + r_
                    po = [B[6], B[7]]
                    osb = self.osb[blk % 2]
                    def scores(h):
                        nonlocal hb
                        hp, p0 = h // 2, (h % 2) * 64
                        ps = B[hb % 6]
                        o0 = 0
                        pe_ = self.pexp[hb % 6]
                        hb += 1
                        c0 = 0 if n > 0 else 128
                        if n > 0:
                            self.mm(ps[:, o0:o0 + 128], self.KT[p0:p0 + 64, hp, pcols], self.QT[p0:p0 + 64, hp, cols], r=[self.KT, self.QT], w=[ps],
                                    start=True, stop=True, signal=False)
                        self.mm(ps[:, o0 + 128:o0 + 256], self.KT[p0:p0 + 64, hp, cols], self.QT[p0:p0 + 64, hp, cols], r=[self.KT, self.QT], w=[ps],
                                start=True, stop=True)
                        self.S(lambda e, pe_=pe_, ps=ps, c0=c0, o0=o0: e.activation(out=pe_[:, c0:256], in_=ps[:, o0 + c0:o0 + 256], func=AF.Exp, scale=0.125),
                               r=[ps], w=[pe_])
                        mk = self.V if (h % 2 == 0) else self.G
                        mk(lambda e, pe_=pe_, c0=c0: e.tensor_tensor(out=pe_[:, c0:256], in0=pe_[:, c0:256], in1=self.m01[:, c0:256], op=ALU.mult),
                           r=[pe_, self.m01], w=[pe_])
                        return pe_

                    def pv(h, pe_):
                        pob = po[h // 4]
                        oc = slice((h % 4) * 65, (h % 4) * 65 + 65)
                        if n > 0:
                            self.mm(pob[:, oc], pe_[:, 0:128], vb[:, bprev, h * 65:(h + 1) * 65], r=[pe_, vb], w=[pob], start=True, stop=False)
                        self.mm(pob[:, oc], pe_[:, 128:256], vb[:, bcur, h * 65:(h + 1) * 65], r=[pe_, vb], w=[pob], start=(n == 0), stop=True)
                    pend = []
                    for h in range(8):
                        pend.append((h, scores(h)))
                        if len(pend) > 4:
                            pv(*pend.pop(0))
                    while pend:
                        pv(*pend.pop(0))
                    self.V(lambda e, osb=osb, po=po: e.tensor_copy(out=osb[:, 0:260], in_=po[0][:, 0:260]), r=[po[0]], w=[osb])
                    self.V(lambda e, osb=osb, po=po: e.tensor_copy(out=osb[:, 260:520], in_=po[1][:, 0:260]), r=[po[1]], w=[osb])
                    self.dma("sp", odst[:, n, r_, :], osb[:, :], r=[osb], w=[self.obr[bi]])
                    blk += 1

    def s5_io(self):
        i = self.inp
        self.lam_re = i("lam_re", [2, 1024])
        self.lam_im = i("lam_im", [2, 1024])
        self.log_dt = i("log_dt", [2, 16])
        self.ssm_bT = i("ssm_bT", [2, 2, 128, 2, 64])
        self.ssm_cT = i("ssm_cT", [2, 16, 128, 16])
        self.ssm_dT = i("ssm_dT", [2, 128, 2])
        self.glu_w = i("glu_w", [2, 256, 256])
        self.glu_bT = i("glu_bT", [2, 128, 2])
        self.coutT = self.scratch("coutT", [256, T], BF16)
        self.obr = [self.scratch(f"obr{i}", [T, 520], F32) for i in range(3)]

    def s5_alloc(self):
        s = self.sb
        self.Bblk = s("Bblk", [128, 2, 8, 2, 64], BF16)
        self.Cblk = s("Cblk", [128, 16, 128], BF16)
        self.Pre = s("Pre", [128, 16, 64], F32)
        self.PsT = s("PsT", [128, 16, 2, 64], F32)
        self.Qre = s("Qre", [128, 16, 64], F32)
        self.QsT = s("QsT", [128, 16, 2, 64], F32)
        self.glubh = s("glubh", [128, 2], F32)
        self.TriT = s("TriT", [128, 128], BF16)
        self.ones1 = s("ones1", [1, 128], BF16)
        self.dTt = s("dTt", [128, 2], F32)
        self.gluw = s("gluw", [128, 2, 256], BF16)
        self.glub = s("glub", [128, 2], F32)

    def s5_prep(self, l):
        s = self.sb
        V, S, G = self.V, self.S, self.G
        self.push()
        lre = s("lre", [128, 16, 64], F32)
        lim = s("lim", [128, 16, 64], F32)
        ldt = s("ldt", [128, 16], F32)
        self.dma("sp", lre[:, :, :].rearrange("p g n -> p (g n)"), self.lam_re[l].partition_broadcast(128), r=[self.lam_re], w=[lre])
        self.dma("sp", lim[:, :, :].rearrange("p g n -> p (g n)"), self.lam_im[l].partition_broadcast(128), r=[self.lam_im], w=[lim])
        self.dma("sp", ldt[:, :], self.log_dt[l].partition_broadcast(128), r=[self.log_dt], w=[ldt])
        S(lambda e: e.activation(out=ldt[:, :], in_=ldt[:, :], func=AF.Exp), r=[ldt], w=[ldt])
        dtb = ldt[:, :].unsqueeze(2).to_broadcast([128, 16, 64])
        lrd = s("lrd", [128, 16, 64], F32)
        lid = s("lid", [128, 16, 64], F32)
        V(lambda e: e.tensor_tensor(out=lrd[:, :, :], in0=lre[:, :, :], in1=dtb, op=ALU.mult), r=[lre, ldt], w=[lrd])
        V(lambda e: e.tensor_tensor(out=lid[:, :, :], in0=lim[:, :, :], in1=dtb, op=ALU.mult), r=[lim, ldt], w=[lid])
        sp1 = s("sp1", [128, 1], F32)
        G(lambda e: e.iota(sp1[:, :], pattern=[[0, 1]], base=1, channel_multiplier=1, allow_small_or_imprecise_dtypes=True), w=[sp1])
        E = s("E", [128, 16, 64], F32)
        An = s("An", [128, 16, 64], F32)
        sA = s("sA", [128, 16, 64], F32)
        cA = s("cA", [128, 16, 64], F32)
        tmp = s("s5tmp", [128, 16, 64], F32)
        tmi = s("s5tmi", [128, 16, 64], I32)
        qm = s("qm", [128, 16, 64], F32)
        pm = s("pm", [128, 16, 64], F32)
        V(lambda e: e.tensor_scalar(out=E[:, :, :], in0=lrd[:, :, :], scalar1=sp1[:, 0:1], scalar2=None, op0=ALU.mult), r=[lrd, sp1], w=[E])
        V(lambda e: e.tensor_scalar(out=An[:, :, :], in0=lid[:, :, :], scalar1=sp1[:, 0:1], scalar2=None, op0=ALU.mult), r=[lid, sp1], w=[An])
        self.sincos(An, sA, cA, tmp, tmi, None)
        S(lambda e: e.activation(out=qm[:, :, :], in_=E[:, :, :], func=AF.Exp), r=[E], w=[qm])
        S(lambda e: e.activation(out=pm[:, :, :], in_=E[:, :, :], func=AF.Exp, scale=-1.0), r=[E], w=[pm])
        V(lambda e: e.tensor_tensor(out=self.Qre[:, :, :], in0=qm[:, :, :], in1=cA[:, :, :], op=ALU.mult), r=[qm, cA], w=[self.Qre])
        V(lambda e: e.tensor_tensor(out=self.QsT[:, :, 1, :], in0=qm[:, :, :], in1=sA[:, :, :], op=ALU.mult), r=[qm, sA], w=[self.QsT])
        V(lambda e: e.tensor_scalar(out=self.QsT[:, :, 0, :], in0=self.QsT[:, :, 1, :], scalar1=-1.0, scalar2=None, op0=ALU.mult), r=[self.QsT], w=[self.QsT])
        V(lambda e: e.tensor_tensor(out=self.Pre[:, :, :], in0=pm[:, :, :], in1=cA[:, :, :], op=ALU.mult), r=[pm, cA], w=[self.Pre])
        V(lambda e: e.tensor_tensor(out=self.PsT[:, :, 0, :], in0=pm[:, :, :], in1=sA[:, :, :], op=ALU.mult), r=[pm, sA], w=[self.PsT])
        V(lambda e: e.tensor_scalar(out=self.PsT[:, :, 1, :], in0=self.PsT[:, :, 0, :], scalar1=-1.0, scalar2=None, op0=ALU.mult), r=[self.PsT], w=[self.PsT])
        self.sincos(lid, sA, cA, tmp, tmi, None)
        S(lambda e: e.activation(out=qm[:, :, :], in_=lrd[:, :, :], func=AF.Exp), r=[lrd], w=[qm])
        nr, ni = E, An
        V(lambda e: e.tensor_tensor(out=nr[:, :, :], in0=qm[:, :, :], in1=cA[:, :, :], op=ALU.mult), r=[qm, cA], w=[nr])
        V(lambda e: e.tensor_scalar(out=nr[:, :, :], in0=nr[:, :, :], scalar1=-1.0, scalar2=None, op0=ALU.add), r=[nr], w=[nr])
        V(lambda e: e.tensor_tensor(out=ni[:, :, :], in0=qm[:, :, :], in1=sA[:, :, :], op=ALU.mult), r=[qm, sA], w=[ni])
        m2 = pm
        V(lambda e: e.tensor_tensor(out=m2[:, :, :], in0=lre[:, :, :], in1=lre[:, :, :], op=ALU.mult), r=[lre], w=[m2])
        V(lambda e: e.tensor_tensor(out=tmp[:, :, :], in0=lim[:, :, :], in1=lim[:, :, :], op=ALU.mult), r=[lim], w=[tmp])
        V(lambda e: e.tensor_tensor(out=m2[:, :, :], in0=m2[:, :, :], in1=tmp[:, :, :], op=ALU.add), r=[m2, tmp], w=[m2])
        V(lambda e: e.reciprocal(out=m2[:, :, :], in_=m2[:, :, :]), r=[m2], w=[m2])
        fre, fim = sA, cA
        t1 = s("ft1", [128, 16, 64], F32)
        t2 = s("ft2", [128, 16, 64], F32)
        V(lambda e: e.tensor_tensor(out=t1[:, :, :], in0=nr[:, :, :], in1=lre[:, :, :], op=ALU.mult), r=[nr, lre], w=[t1])
        V(lambda e: e.tensor_tensor(out=t2[:, :, :], in0=ni[:, :, :], in1=lim[:, :, :], op=ALU.mult), r=[ni, lim], w=[t2])
        V(lambda e: e.tensor_tensor(out=t1[:, :, :], in0=t1[:, :, :], in1=t2[:, :, :], op=ALU.add), r=[t1, t2], w=[t1])
        V(lambda e: e.tensor_tensor(out=fre[:, :, :], in0=t1[:, :, :], in1=m2[:, :, :], op=ALU.mult), r=[t1, m2], w=[fre])
        V(lambda e: e.tensor_tensor(out=t1[:, :, :], in0=ni[:, :, :], in1=lre[:, :, :], op=ALU.mult), r=[ni, lre], w=[t1])
        V(lambda e: e.tensor_tensor(out=t2[:, :, :], in0=nr[:, :, :], in1=lim[:, :, :], op=ALU.mult), r=[nr, lim], w=[t2])
        V(lambda e: e.tensor_tensor(out=t1[:, :, :], in0=t1[:, :, :], in1=t2[:, :, :], op=ALU.subtract), r=[t1, t2], w=[t1])
        V(lambda e: e.tensor_tensor(out=fim[:, :, :], in0=t1[:, :, :], in1=m2[:, :, :], op=ALU.mult), r=[t1, m2], w=[fim])
        bT = s("bTt", [128, 2, 2, 64], F32)
        self.dma("sp", bT[:, :, :, :], self.ssm_bT[l].rearrange("r p k n -> p r k n"), r=[self.ssm_bT], w=[bT])
        bm = s("bmask", [128, 8], F32)
        one8 = s("one8", [128, 8], F32)
        G(lambda e: e.memset(one8[:, :], 1.0), w=[one8])
        G(lambda e: e.affine_select(out=bm[:, :], in_=one8[:, :], pattern=[[-16, 8]], compare_op=ALU.is_ge, fill=0.0, base=0, channel_multiplier=1),
          r=[one8], w=[bm])
        G(lambda e: e.affine_select(out=bm[:, :], in_=bm[:, :], pattern=[[16, 8]], compare_op=ALU.is_ge, fill=0.0, base=15, channel_multiplier=-1),
          r=[bm], w=[bm])
        bmb = bm[:, :].unsqueeze(2).to_broadcast([128, 8, 64])
        for kc in range(2):
            fr = fre[:, kc * 8:(kc + 1) * 8, :]
            fi = fim[:, kc * 8:(kc + 1) * 8, :]
            bre = bT[:, 0, kc, :].unsqueeze(1).to_broadcast([128, 8, 64])
            bim = bT[:, 1, kc, :].unsqueeze(1).to_broadcast([128, 8, 64])
            a1, a2 = t1[:, 0:8, :], t2[:, 0:8, :]
            V(lambda e, fr=fr, bre=bre: e.tensor_tensor(out=a1, in0=fr, in1=bre, op=ALU.mult), r=[fre, bT], w=[t1])
            V(lambda e, fi=fi, bim=bim: e.tensor_tensor(out=a2, in0=fi, in1=bim, op=ALU.mult), r=[fim, bT], w=[t2])
            V(lambda e: e.tensor_tensor(out=a1, in0=a1, in1=a2, op=ALU.subtract), r=[t1, t2], w=[t1])
            V(lambda e, kc=kc: e.tensor_tensor(out=self.Bblk[:, kc, :, 0, :], in0=a1, in1=bmb, op=ALU.mult), r=[t1, bm], w=[self.Bblk])
            V(lambda e, fr=fr, bim=bim: e.tensor_tensor(out=a1, in0=fr, in1=bim, op=ALU.mult), r=[fre, bT], w=[t1])
            V(lambda e, fi=fi, bre=bre: e.tensor_tensor(out=a2, in0=fi, in1=bre, op=ALU.mult), r=[fim, bT], w=[t2])
            V(lambda e: e.tensor_tensor(out=a1, in0=a1, in1=a2, op=ALU.add), r=[t1, t2], w=[t1])
            V(lambda e, kc=kc: e.tensor_tensor(out=self.Bblk[:, kc, :, 1, :], in0=a1, in1=bmb, op=ALU.mult), r=[t1, bm], w=[self.Bblk])
        cTs = s("cTs", [128, 16, 16], F32)
        self.dma("sp", cTs[:, :, :], self.ssm_cT[l].rearrange("g p c -> p g c"), r=[self.ssm_cT], w=[cTs])
        sg = s("sgn", [128, 1], F32)
        G(lambda e: e.memset(sg[0:64, :], 1.0), w=[sg])
        G(lambda e: e.memset(sg[64:128, :], -1.0), w=[sg])
        G(lambda e: e.memset(self.Cblk[:, :, :], 0.0), w=[self.Cblk])
        for g in range(16):
            c0 = (g % 8) * 16
            S(lambda e, g=g, c0=c0: e.activation(out=self.Cblk[:, g, c0:c0 + 16], in_=cTs[:, g, :], func=AF.Identity, scale=sg[:, 0:1]),
              r=[cTs, sg, self.Cblk], w=[self.Cblk])
        onesb = s("onesb", [128, 128], F32)
        G(lambda e: e.memset(onesb[:, :], 1.0), w=[onesb])
        G(lambda e: e.affine_select(out=onesb[:, :], in_=onesb[:, :], pattern=[[1, 128]], compare_op=ALU.is_ge, fill=0.0, base=0, channel_multiplier=-1),
          r=[onesb], w=[onesb])
        G(lambda e: e.tensor_copy(out=self.TriT[:, :], in_=onesb[:, :]), r=[onesb], w=[self.TriT])
        G(lambda e: e.memset(self.ones1[:, :], 1.0), w=[self.ones1])
        self.dma("sp", self.dTt[:, :], self.ssm_dT[l], r=[self.ssm_dT], w=[self.dTt])
        self.dma("sp", self.glub[:, :], self.glu_bT[l], r=[self.glu_bT], w=[self.glub])
        V(lambda e: e.tensor_scalar(out=self.glubh[:, :], in0=self.glub[:, :], scalar1=0.5, scalar2=None, op0=ALU.mult), r=[self.glub], w=[self.glubh])
        self.dma("pool", self.gluw[:, :, :], self.glu_w[l].rearrange("(k p) n -> p k n", p=128), r=[self.glu_w], w=[self.gluw])
        self.pop()

    def s5_chunk(self, l, j):
        B = self.bank
        V, S, G = self.V, self.S, self.G
        hT = self.hT[j % 2]
        uT = self.uT[j % 2]
        co = self.co[j % 2]
        ps_s = B[7]
        for cc in range(2):
            for k in range(8):
                self.mm(ps_s[:, cc * 128:(cc + 1) * 128], self.win[:, k, 2048 + cc * 128:2048 + (cc + 1) * 128], hT[:, k, :], r=[self.win, hT], w=[ps_s],
                        start=(k == 0), stop=(k == 7), signal=(k == 7 and cc == 1))
        S(lambda e: e.copy(out=uT[:, :, :], in_=ps_s[:, 0:256].rearrange("p (c t) -> p c t", c=2)), r=[ps_s], w=[uT])
        yps = B[7]
        for h in range(2):
            bu = [B[1], B[2]]
            zz = [B[3], B[4]]
            g0 = h * 8
            for q in range(2):
                self.mm(bu[q][:, :], uT[:, h, :], self.Bblk[:, h, q * 4:(q + 1) * 4, :, :].rearrange("p g r n -> p (g r n)"), r=[uT, self.Bblk], w=[bu[q]],
                        start=True, stop=True)
            t1, t2, vv = self.s5t1, self.s5t2, self.s5v
            for q in range(2):
                gs = slice(g0 + q * 4, g0 + q * 4 + 4)
                bu4 = bu[q][:, :].rearrange("p (g r n) -> p g r n", g=4, r=2)
                pc = self.Pre[:, gs, :].unsqueeze(2).to_broadcast([128, 4, 2, 64])
                V(lambda e, q=q, bu4=bu4, pc=pc: e.tensor_tensor(out=t1[:, q * 4:(q + 1) * 4, :, :], in0=bu4, in1=pc, op=ALU.mult), r=[bu[q], self.Pre], w=[t1], x=[t1])
                V(lambda e, q=q, bu4=bu4, gs=gs: e.tensor_tensor(out=t2[:, q * 4:(q + 1) * 4, :, :], in0=bu4[:, :, ::-1, :], in1=self.PsT[:, gs, :, :], op=ALU.mult),
                  r=[bu[q], self.PsT], w=[t2], x=[t2])
            G(lambda e: e.tensor_tensor(out=vv[:, :], in0=t1[:, :, :, :].rearrange("p g r n -> p (g r n)"), in1=t2[:, :, :, :].rearrange("p g r n -> p (g r n)"), op=ALU.add),
              r=[t1, t2], w=[vv])
            for q in range(2):
                self.mm(zz[q][:, :], self.TriT[:, :], vv[:, q * 512:(q + 1) * 512], r=[self.TriT, vv], w=[zz[q]], start=True, stop=False)
                self.mm(zz[q][:, :], self.ones1[:, :], self.x0row[h][:, q * 512:(q + 1) * 512], r=[self.ones1, self.x0row[h]], w=[zz[q]], start=False, stop=True)
            xs = self.s5x[h]
            for q in range(2):
                gs = slice(g0 + q * 4, g0 + q * 4 + 4)
                z4 = zz[q][:, :].rearrange("p (g r n) -> p g r n", g=4, r=2)
                qc = self.Qre[:, gs, :].unsqueeze(2).to_broadcast([128, 4, 2, 64])
                V(lambda e, q=q, z4=z4, qc=qc: e.tensor_tensor(out=t1[:, q * 4:(q + 1) * 4, :, :], in0=z4, in1=qc, op=ALU.mult), r=[zz[q], self.Qre], w=[t1], x=[t1])
                V(lambda e, q=q, z4=z4, gs=gs: e.tensor_tensor(out=t2[:, q * 4:(q + 1) * 4, :, :], in0=z4[:, :, ::-1, :], in1=self.QsT[:, gs, :, :], op=ALU.mult),
                  r=[zz[q], self.QsT], w=[t2], x=[t2])
            G(lambda e, xs=xs: e.tensor_tensor(out=xs[:, :, :].rearrange("p g m -> p (g m)"), in0=t1[:, :, :, :].rearrange("p g r n -> p (g r n)"),
                                        in1=t2[:, :, :, :].rearrange("p g r n -> p (g r n)"), op=ALU.add), r=[t1, t2], w=[xs])
            self.dma("act", self.x0row[h][0:1, :], xs[127:128, :, :].rearrange("p g m -> p (g m)"), r=[xs], w=[self.x0row[h]])
            pxt = B[0]
            pxb = pxt[:, :].bitcast(BF16).rearrange("p (g t) -> p g t", g=8)
            for g in range(8):
                self.tr(pxb[:, g, :], xs[:, g, :], self.identb[:, :], r=[xs, self.identb], w=[pxt], signal=(g == 7))
            V(lambda e, pxb=pxb: e.tensor_copy(out=self.s5xT[:, :, :], in_=pxb), r=[pxt], w=[self.s5xT])
            for g in range(8):
                self.mm(yps[:, 256 + h * 128:256 + (h + 1) * 128], self.Cblk[:, g0 + g, :], self.s5xT[:, g, :], r=[self.Cblk, self.s5xT], w=[yps],
                        start=(g == 0), stop=(g == 7))
        for cc in range(2):
            V(lambda e, cc=cc: e.scalar_tensor_tensor(out=self.yf[:, cc, :], in0=uT[:, cc, :], scalar=self.dTt[:, cc:cc + 1], in1=yps[:, 256 + cc * 128:256 + (cc + 1) * 128],
                                                      op0=ALU.mult, op1=ALU.add), r=[uT, self.dTt, yps], w=[self.yf], x=[self.yf])
        S(lambda e: e.activation(out=self.yg[:, :, :], in_=self.yf[:, :, :], func=AF.Gelu_apprx_tanh), r=[self.yf], w=[self.yg])
        gps = B[5]
        for c2 in range(2):
            for cc in range(2):
                self.mm(gps[:, c2 * 128:(c2 + 1) * 128], self.gluw[:, cc, c2 * 128:(c2 + 1) * 128], self.yg[:, cc, :], r=[self.gluw, self.yg], w=[gps],
                        start=(cc == 0), stop=(cc == 1))
        for c2 in range(2):
            S(lambda e, c2=c2: e.activation(out=self.sgm[:, c2, :], in_=gps[:, c2 * 128:(c2 + 1) * 128], func=AF.Sigmoid, bias=self.glub[:, c2:c2 + 1], scale=1.0),
              r=[gps, self.glub], w=[self.sgm])
        V(lambda e: e.tensor_tensor(out=co[:, :, :], in0=self.yg[:, :, :], in1=self.sgm[:, :, :], op=ALU.mult), r=[self.yg, self.sgm], w=[co])
        self.dma("sp", self.coutT[:, j * 128:(j + 1) * 128].rearrange("(c p) t -> p c t", p=128), co[:, :, :], r=[co], w=[self.coutT])

    def half(self, bank, lo):
        t = Tl(bank.h, bank.b.name + ("lo" if lo else "hi"))
        return t

    def p1v2_alloc(self):
        s = self.sb
        self.xt = [s(f"xt{i}", [128, D], F32) for i in range(2)]
        self.xn = [s(f"xn{i}", [128, D], BF16) for i in range(2)]
        self.st = [s(f"st{i}", [128, 24], F32) for i in range(2)]
        self.st2 = [s(f"stb{i}", [128, 24], F32) for i in range(2)]
        self.hT = [s(f"hT{i}", [128, 8, 128], BF16) for i in range(2)]
        self.gl = [s(f"gl{i}", [128, 512], F32) for i in range(2)]
        self.qbd = [s(f"qbd{i}", [128, 2, 512], BF16) for i in range(2)]
        self.vp = [s(f"vp{i}", [128, 8, 65], BF16) for i in range(2)]
        for i in range(2):
            self.G(lambda e, i=i: e.memset(self.vp[i][:, :, :], 1.0), w=[self.vp[i]])
        self.uT = [s(f"uT{i}", [128, 2, 128], BF16) for i in range(2)]
        self.vnb = s("vnb", [128, 256], F32)
        self.vb = s("vb", [128, 256], BF16)
        self.aout = s("aout", [128, 256], BF16)
        self.qb = s("qb", [128, 2, 512], BF16)
        self.rt = s("rt", [128, 4, 8, 8], F32)
        self.uD = [s(f"uD{i}", [128, 2, 128], F32) for i in range(3)]
        self.p1t = [s(f"p1t{i}", [128, 4, 2, 64], BF16) for i in range(2)]
        self.p2t = [s(f"p2t{i}", [128, 4, 2, 64], BF16) for i in range(2)]
        self.q1 = [s(f"q1_{i}", [128, 4, 2, 64], F32) for i in range(2)]
        self.q2 = [s(f"q2_{i}", [128, 4, 2, 64], F32) for i in range(2)]
        self.qv = [s(f"qv{i}", [128, 512], BF16) for i in range(2)]
        self.qx = [s(f"qx{i}", [128, 4, 128], BF16) for i in range(2)]
        self.qxT = [s(f"qxT{i}", [128, 4, 128], BF16) for i in range(3)]
        self.x0q = [s(f"x0q{i}", [1, 512], BF16) for i in range(4)]
        for i in range(4):
            self.G(lambda e, i=i: e.memset(self.x0q[i][:, :], 0.0), w=[self.x0q[i]])
        self.yf = s("yf2", [128, 2, 128], F32)
        self.yg = s("yg2", [128, 2, 128], BF16)
        self.sgm = s("sgm2", [128, 2, 128], F32)
        self.co = [s(f"co2_{i}", [128, 2, 128], BF16) for i in range(2)]
        B = self.bank
        self.b5lo, self.b5hi = Tl(B[5].h, "b5lo"), Tl(B[5].h, "b5hi")
        self.b6lo, self.b6hi = Tl(B[6].h, "b6lo"), Tl(B[6].h, "b6hi")
        self.b7lo, self.b7hi = Tl(B[7].h, "b7lo"), Tl(B[7].h, "b7hi")

    def p1_front_ln(self, l, j, x_src):
        if j >= NCH:
            return
        i2 = j % 2
        xt, xn, st = self.xt[i2], self.xn[i2], self.st[i2]
        S = self.S
        if j == 0:
            self.dma("sp", xt[:, :], x_src[0:128, :], r=[x_src], w=[xt])
            xt1 = self.xt[1]
            self.dma("sp", xt1[:, :], x_src[128:256, :], r=[x_src], w=[xt1])
        self.ln_stats(xt, lambda a, b: xt[:, a:b], D, st)
        S(lambda e: e.activation(out=xn[:, :], in_=xt[:, :], func=AF.Identity, bias=st[:, 4:5], scale=st[:, 3:4]), r=[xt, st], w=[xn])
        if j + 2 < NCH:
            self.dma("sp", xt[:, :], x_src[(j + 2) * 128:(j + 3) * 128, :], r=[x_src], w=[xt])

    def p1_front_a(self, l, j, x_src):
        if j >= NCH:
            return
        i2 = j % 2
        xn, hT = self.xn[i2], self.hT[i2]
        B = self.bank
        S = self.S
        pT = B[0]
        pTb = pT[:, :].bitcast(BF16).rearrange("p (k t) -> p k t", k=8)
        for k in range(8):
            self.tr(pTb[:, k, :], xn[:, k * 128:(k + 1) * 128], self.identb[:, :], r=[xn, self.identb], w=[pT], signal=(k == 7))
        for k in range(8):
            S(lambda e, k=k: e.activation(out=hT[:, k, :], in_=pTb[:, k, :], func=AF.Identity, scale=self.modp[:, 1, k:k + 1], bias=self.modp[:, 0, k:k + 1]),
              r=[pT, self.modp], w=[hT], x=[hT])

    def p1_front_s(self, l, j):
        if j >= NCH:
            return
        i2 = j % 2
        hT, uT = self.hT[i2], self.uT[i2]
        S = self.S
        ps_s = self.b6lo
        for cc in range(2):
            for k in range(8):
                self.mm(ps_s[:, cc * 128:(cc + 1) * 128], self.win[:, k, 2048 + cc * 128:2048 + (cc + 1) * 128], hT[:, k, :], r=[self.win, hT], w=[ps_s],
                        start=(k == 0), stop=(k == 7), signal=(k == 7 and cc == 1))
        S(lambda e: e.copy(out=uT[:, :, :], in_=ps_s[:, 0:256].rearrange("p (c t) -> p c t", c=2)), r=[ps_s], w=[uT])
        uD = self.uD[j % 3]
        for cc in range(2):
            S(lambda e, cc=cc: e.activation(out=uD[:, cc, :], in_=ps_s[:, cc * 128:(cc + 1) * 128], func=AF.Identity, scale=self.dTt[:, cc:cc + 1]), r=[ps_s, self.dTt], w=[uD], x=[uD])

    def p1_front_p(self, l, j, which):
        if j >= NCH:
            return
        i2 = j % 2
        hT, gl, qf, vp = self.hT[i2], self.gl[i2], self.qbd[i2], self.vp[i2]
        B = self.bank
        S = self.S

        def proj(bank, c0):
            for k in range(8):
                self.mm(bank[:, :], hT[:, k, :], self.win[:, k, c0:c0 + 512], r=[hT, self.win], w=[bank], start=(k == 0), stop=(k == 7))
        if which == 0:
            proj(B[1], 0)
            S(lambda e: e.activation(out=gl[:, :], in_=B[1][:, :], func=AF.Gelu_apprx_tanh), r=[B[1]], w=[gl])
        elif which == 1:
            proj(B[2], 512)
            S(lambda e: e.copy(out=qf[:, 0, :], in_=B[2][:, :]), r=[B[2]], w=[qf])
        elif which == 2:
            proj(B[1], 1024)
            S(lambda e: e.copy(out=qf[:, 1, :], in_=B[1][:, :]), r=[B[1]], w=[qf], x=[qf])
        else:
            proj(B[2], 1536)
            S(lambda e: e.copy(out=vp[:, :, 0:64], in_=B[2][:, :].rearrange("p (h e) -> p h e", e=64)), r=[B[2]], w=[vp])

    def p1_front(self, l, j, x_src):
        self.p1_front_a(l, j, x_src)
        self.p1_front_s(l, j)
        for w_ in range(4):
            self.p1_front_p(l, j, w_)

    def p1_back(self, l, j):
        if j < 0:
            return
        i2 = j % 2
        st2 = self.st2[i2]
        gl, qf, vp, uT = self.gl[i2], self.qbd[i2], self.vp[i2], self.uT[i2]
        vnb, vb, aout, qb, rt = self.vnb, self.vb, self.aout, self.qbd[i2], self.rt
        B = self.bank
        V, S, G = self.V, self.S, self.G
        rows = slice(j * 128, (j + 1) * 128)
        self.ln_stats(gl, lambda a, b: gl[:, 256 + a:256 + b], 256, st2)
        V(lambda e: e.tensor_scalar(out=vnb[:, :], in0=gl[:, 256:512], scalar1=st2[:, 3:4], scalar2=st2[:, 4:5], op0=ALU.mult, op1=ALU.add),
          r=[gl, st2], w=[vnb])
        V(lambda e: e.tensor_tensor(out=vnb[:, :], in0=vnb[:, :], in1=self.glng[:, :], op=ALU.mult), r=[vnb, self.glng], w=[vnb], x=[vnb])
        V(lambda e: e.tensor_tensor(out=vb[:, :], in0=vnb[:, :], in1=self.glnb[:, :], op=ALU.add), r=[vnb, self.glnb], w=[vb], x=[vnb])
        psv = self.b5lo
        for h in range(4):
            self.mm(psv[:, h * 64:(h + 1) * 64], self.wsT[:, h, :], vb[:, h * 64:(h + 1) * 64], r=[self.wsT, vb], w=[psv], start=True, stop=True,
                    signal=(h == 3))
        for h in range(4):
            V(lambda e, h=h: e.scalar_tensor_tensor(out=aout[:, h * 64:(h + 1) * 64], in0=psv[:, h * 64:(h + 1) * 64], scalar=self.gbs[:, h:h + 1],
                                                    in1=gl[:, h * 64:(h + 1) * 64], op0=ALU.add, op1=ALU.mult), r=[psv, self.gbs, gl], w=[aout], x=[aout])
        self.dma("sp", self.mixtok[rows, 0:256], aout[:, :], r=[aout], w=[self.mixtok])
        q4 = qf[:, :, :].rearrange("p a (h e) -> p (a h) e", e=64)
        o4 = qb[:, :, :].rearrange("p a (h e) -> p (a h) e", e=64)
        for a in range(2):
            xa1, xa2 = q4[:, a * 8:(a + 1) * 8, 0:8], q4[:, a * 8:(a + 1) * 8, 8:16]
            cb = self.cs[:, j, :].unsqueeze(1).to_broadcast([128, 8, 8])
            sb_ = self.sn[:, j, :].unsqueeze(1).to_broadcast([128, 8, 8])
            oa = o4[:, a * 8:(a + 1) * 8, :]
            G(lambda e, xa1=xa1, cb=cb: e.tensor_tensor(out=rt[:, 0, :, :], in0=xa1, in1=cb, op=ALU.mult), r=[qf, self.cs], w=[rt])
            G(lambda e, xa2=xa2, sb_=sb_: e.tensor_tensor(out=rt[:, 1, :, :], in0=xa2, in1=sb_, op=ALU.mult), r=[qf, self.sn], w=[rt])
            G(lambda e, xa2=xa2, cb=cb: e.tensor_tensor(out=rt[:, 2, :, :], in0=xa2, in1=cb, op=ALU.mult), r=[qf, self.cs], w=[rt])
            G(lambda e, xa1=xa1, sb_=sb_: e.tensor_tensor(out=rt[:, 3, :, :], in0=xa1, in1=sb_, op=ALU.mult), r=[qf, self.sn], w=[rt])
            G(lambda e, oa=oa: e.tensor_tensor(out=oa[:, :, 0:8], in0=rt[:, 0, :, :], in1=rt[:, 1, :, :], op=ALU.subtract), r=[rt], w=[qb])
            G(lambda e, oa=oa: e.tensor_tensor(out=oa[:, :, 8:16], in0=rt[:, 2, :, :], in1=rt[:, 3, :, :], op=ALU.add), r=[rt], w=[qb])
        pq = B[7]
        pqb = pq[:, :].bitcast(BF16).rearrange("p (a k t) -> p a k t", a=2, k=4)
        for a in range(2):
            for k in range(4):
                self.tr(pqb[:, a, k, :], qb[:, a, k * 128:(k + 1) * 128], self.identb[:, :], r=[qb, self.identb], w=[pq], signal=(a == 1 and k == 3))
        S(lambda e: e.copy(out=self.QT[:, :, j * 128:(j + 1) * 128], in_=pqb[:, 0, :, :]), r=[pq], w=[self.QT])
        S(lambda e: e.copy(out=self.KT[:, :, j * 128:(j + 1) * 128], in_=pqb[:, 1, :, :]), r=[pq], w=[self.KT])
        self.dma("sp", self.vd[rows, :], vp[:, :, :].rearrange("p h e -> p (h e)"), r=[vp], w=[self.vd])

    def s5A(self, i):
        if i < 0 or i >= 4 * NCH:
            return
        j, q = divmod(i, 4)
        kc = q // 2
        gs = slice(q * 4, q * 4 + 4)
        uT = self.uT[j % 2]
        bu = self.bank[3]
        t1, t2 = self.p1t[i % 2], self.p2t[i % 2]
        self.mm(bu[:, :], uT[:, kc, :], self.Bblk[:, kc, (q % 2) * 4:(q % 2) * 4 + 4, :, :].rearrange("p g r n -> p (g r n)"), r=[uT, self.Bblk], w=[bu],
                start=True, stop=True)
        bu4 = bu[:, :].rearrange("p (g r n) -> p g r n", g=4, r=2)
        pc = self.Pre[:, gs, :].unsqueeze(2).to_broadcast([128, 4, 2, 64])
        self.V(lambda e: e.tensor_tensor(out=t1[:, :, :, :], in0=bu4, in1=pc, op=ALU.mult), r=[bu, self.Pre], w=[t1])
        self.V(lambda e: e.tensor_tensor(out=t2[:, :, :, :], in0=bu4[:, :, ::-1, :], in1=self.PsT[:, gs, :, :], op=ALU.mult), r=[bu, self.PsT], w=[t2])

    def s5B(self, i):
        if i < 0 or i >= 4 * NCH:
            return
        j, q = divmod(i, 4)
        gs = slice(q * 4, q * 4 + 4)
        zz = self.bank[4]
        t1, t2 = self.p1t[i % 2], self.p2t[i % 2]
        u1, u2, xs = self.q1[i % 2], self.q2[i % 2], self.qx[i % 2]
        f = lambda t: t[:, :, :, :].rearrange("p g r n -> p (g r n)")
        self.mm(zz[:, :], self.TriT[:, :], f(t1), r=[self.TriT, t1], w=[zz], start=True, stop=False)
        self.mm(zz[:, :], self.TriT[:, :], f(t2), r=[self.TriT, t2], w=[zz], start=False, stop=False)
        self.mm(zz[:, :], self.ones1[:, :], self.x0q[q][:, :], r=[self.ones1, self.x0q[q]], w=[zz], start=False, stop=True)
        z4 = zz[:, :].rearrange("p (g r n) -> p g r n", g=4, r=2)
        qc = self.Qre[:, gs, :].unsqueeze(2).to_broadcast([128, 4, 2, 64])
        self.V(lambda e: e.tensor_tensor(out=u1[:, :, :, :], in0=z4, in1=qc, op=ALU.mult), r=[zz, self.Qre], w=[u1])
        self.V(lambda e: e.tensor_tensor(out=u2[:, :, :, :], in0=z4[:, :, ::-1, :], in1=self.QsT[:, gs, :, :], op=ALU.mult), r=[zz, self.QsT], w=[u2])
        self.G(lambda e: e.tensor_tensor(out=xs[:, :, :].rearrange("p g m -> p (g m)"), in0=f(u1), in1=f(u2), op=ALU.add), r=[u1, u2], w=[xs])
        self.dma("pool", self.x0q[q][0:1, :], xs[127:128, :, :].rearrange("p g m -> p (g m)"), r=[xs], w=[self.x0q[q]])

    def s5C(self, i):
        if i < 0 or i >= 4 * NCH:
            return
        xs, xT = self.qx[i % 2], self.qxT[i % 3]
        pxt = self.b6hi
        pxb = pxt[:, 256:512].bitcast(BF16).rearrange("p (g t) -> p g t", g=4)
        for g in range(4):
            self.tr(pxb[:, g, :], xs[:, g, :], self.identb[:, :], r=[xs, self.identb], w=[pxt], signal=(g == 3))
        self.S(lambda e: e.copy(out=xT[:, :, :], in_=pxb), r=[pxt], w=[xT])

    def s5D(self, i):
        if i < 0 or i >= 4 * NCH:
            return
        j, q = divmod(i, 4)
        kc = q // 2
        xT = self.qxT[i % 3]
        yps = self.b5hi
        for g in range(4):
            self.mm(yps[:, 256 + kc * 128:256 + (kc + 1) * 128], self.Cblk[:, q * 4 + g, :], xT[:, g, :], r=[self.Cblk, xT], w=[yps],
                    start=(q % 2 == 0 and g == 0), stop=(q % 2 == 1 and g == 3))

    def s5_step(self, i):
        self.s5A(i + 2)
        self.s5B(i + 1)
        self.s5C(i)
        if i % 2 == 0:
            self.s5D(i - 2)
            self.s5D(i - 1)

    def s5_tail(self, j):
        if j < 0 or j >= NCH:
            return
        V, S, G = self.V, self.S, self.G
        i2 = j % 2
        uT = self.uT[i2]
        yps = self.b5hi
        co = self.co[i2]
        for cc in range(2):
            V(lambda e, cc=cc: e.scalar_tensor_tensor(out=self.yf[:, cc, :], in0=self.uD[j % 3][:, cc, :], scalar=1.0, in1=yps[:, 256 + cc * 128:256 + (cc + 1) * 128],
                                                      op0=ALU.mult, op1=ALU.add), r=[self.uD[j % 3], yps], w=[self.yf], x=[self.yf])
        S(lambda e: e.activation(out=self.yg[:, :, :], in_=self.yf[:, :, :], func=AF.Gelu_apprx_tanh), r=[self.yf], w=[self.yg])
        gps = self.b5lo
        for c2 in range(2):
            for cc in range(2):
                self.mm(gps[:, c2 * 128:(c2 + 1) * 128], self.gluw[:, cc, c2 * 128:(c2 + 1) * 128], self.yg[:, cc, :], r=[self.gluw, self.yg], w=[gps],
                        start=(cc == 0), stop=(cc == 1))
        for c2 in range(2):
            S(lambda e, c2=c2: e.activation(out=self.sgm[:, c2, :], in_=gps[:, c2 * 128:(c2 + 1) * 128], func=AF.Tanh, bias=self.glubh[:, c2:c2 + 1], scale=0.5),
              r=[gps, self.glubh], w=[self.sgm])
        V(lambda e: e.scalar_tensor_tensor(out=self.sgm[:, :, :], in0=self.sgm[:, :, :], scalar=1.0, in1=self.yg[:, :, :], op0=ALU.add, op1=ALU.mult),
          r=[self.sgm, self.yg], w=[self.sgm])
        V(lambda e: e.tensor_scalar(out=co[:, :, :], in0=self.sgm[:, :, :], scalar1=0.5, scalar2=None, op0=ALU.mult), r=[self.sgm], w=[co])
        self.dma("sp", self.coutT[:, j * 128:(j + 1) * 128].rearrange("(c p) t -> p c t", p=128), co[:, :, :], r=[co], w=[self.coutT])

    def p1_all(self, l, x_src):
        self.p1_front_ln(l, 0, x_src)
        self.p1_front_ln(l, 1, x_src)
        self.p1_front(l, 0, x_src)
        self.s5A(0); self.s5A(1); self.s5B(0)
        for j in range(NCH):
            self.p1_front_ln(l, j + 2, x_src)
            self.p1_front(l, j + 1, x_src)
            self.p1_back(l, j)
            for q in range(4):
                self.s5_step(4 * j + q)
                if q == 0:
                    self.s5_tail(j - 1)
        self.s5_step(4 * NCH)
        self.s5_tail(NCH - 1)
        assert (4 * NCH) % 2 == 0

    CAP = 896
    NSLOT = 16 * 896 + 128
    GROUPS = ((0, 4), (512, 3))

    def p3_io(self):
        i = self.inp
        self.w_out = i("w_out", [2, D, D])
        self.ln1_g = i("ln1_g", [2, D]); self.ln1_b = i("ln1_b", [2, D])
        self.ln2_g = i("ln2_g", [2, D]); self.ln2_b = i("ln2_b", [2, D])
        self.router_w = i("router_w", [D, 16])
        self.router_bias = i("router_bias", [16])
        self.x1d = self.scratch("x1d", [T, D], F32)
        self.xmid = self.scratch("xmid", [T, D], F32)
        self.h2slots = self.scratch("h2slots", [self.NSLOT, D], BF16)
        self.oslots = self.scratch("oslots", [self.NSLOT, D], F32)

    def route_alloc(self):
        s = self.sb
        self.slotA = s("slotA", [128, NCH], I32)
        self.slotB = s("slotB", [128, NCH], I32)
        self.gAB = s("gAB", [128, 2, NCH], F32)

    def p3_alloc(self, l):
        s = self.sb
        self.wout = s("wout", [128, 8, D], BF16)
        for k in range(8):
            self.dma("pool", self.wout[:, k, :], self.w_out[l, k * 128:(k + 1) * 128, :], r=[self.w_out], w=[self.wout])
        self.lng = s("lng", [128, D], F32); self.lnb = s("lnb", [128, D], F32)
        self.dma("sp", self.lng[:, :], self.ln1_g[l].partition_broadcast(128), r=[self.ln1_g], w=[self.lng])
        self.dma("sp", self.lnb[:, :], self.ln1_b[l].partition_broadcast(128), r=[self.ln1_b], w=[self.lnb])
        self.rw = s("rw", [128, 8, 16], F32)
        self.dma("sp", self.rw[:, :, :], self.router_w[:, :].rearrange("(k p) e -> p k e", p=128), r=[self.router_w], w=[self.rw])
        self.rbias = s("rbias", [128, 16], F32)
        self.dma("sp", self.rbias[:, :], self.router_bias[:].partition_broadcast(128), r=[self.router_bias], w=[self.rbias])
        self.o3 = [s(f"o3_{i}", [128, 3, 520], F32) for i in range(2)]
        self.rec = s("rec", [128, 8], F32)
        self.mt = [s(f"mt{i}", [128, D], BF16) for i in range(2)]
        self.mixT = [s(f"mixT{i}", [128, 8, 128], BF16) for i in range(2)]
        self.xres = [s(f"xres{i}", [128, D], F32) for i in range(2)]
        self.yy = s("yy", [128, D], F32)
        self.x1t = [s(f"x1t{i}", [128, D], F32) for i in range(3)]
        self.cT = [s(f"cT{i}", [128, 2, 128], BF16) for i in range(2)]
        self.st3b = s("st3b", [128, 24], F32)
        self.h2f = s("h2f", [128, D], F32)
        self.h2f2 = [self.h2f, s("h2fb", [128, D], F32)]
        self.h2all = s("h2all", [128, NCH, D], BF16)
        self.h2c = [Tl(self.h2all.h, f"h2c{j}") for j in range(NCH)]
        self.scall = s("scall", [128, NCH, 16], F32)
        self.h2T = s("h2T", [128, 8, 128], F32)
        self.st3 = s("st3", [128, 24], F32)
        self.trs = s("trs", [128, 128], F32)
        self.eoff = s("eoff", [128, 16], F32)
        self.trashp = s("trashp", [128, 1], F32)
        self.ones16 = s("ones16", [128, 16], F32)
        G = self.G
        G(lambda e: e.memset(self.ones16[:, :], 1.0), w=[self.ones16])
        G(lambda e: e.affine_select(out=self.trs[:, :], in_=self.onesf[:, :], pattern=[[1, 128]], compare_op=ALU.is_ge, fill=0.0, base=-1, channel_multiplier=-1),
          r=[self.onesf], w=[self.trs])
        G(lambda e: e.iota(self.eoff[:, :], pattern=[[self.CAP, 16]], base=0, channel_multiplier=0, allow_small_or_imprecise_dtypes=True), w=[self.eoff])
        G(lambda e: e.iota(self.trashp[:, :], pattern=[[0, 1]], base=16 * self.CAP, channel_multiplier=1, allow_small_or_imprecise_dtypes=True), w=[self.trashp])

    def p3_L1(self, k):
        if k >= NCH:
            return
        rws = slice(k * 128, (k + 1) * 128)
        o3_, mt_ = self.o3[k % 2], self.mt[k % 2]
        for bi in range(3):
            self.dma("sp", o3_[:, bi, :], self.obr[bi][rws, :], r=[self.obr[bi]], w=[o3_])
        self.dma("sp", mt_[:, 0:256], self.mixtok[rws, 0:256], r=[self.mixtok], w=[mt_])

    def p3_L2(self, k, x_src):
        if k >= NCH:
            return
        rws = slice(k * 128, (k + 1) * 128)
        self.dma("sp", self.cT[k % 2][:, :, :], self.coutT[:, rws].rearrange("(c p) t -> p c t", p=128), r=[self.coutT], w=[self.cT[k % 2]])
        self.dma("sp", self.xres[k % 2][:, :], x_src[rws, :], r=[x_src], w=[self.xres[k % 2]])

    def p3_S1(self, j):
        if j >= NCH or j < 0:
            return
        B = self.bank
        V, S, G = self.V, self.S, self.G
        o3, rec, mt = self.o3[j % 2], self.rec, self.mt[j % 2]
        mixT = self.mixT[j % 2]
        V(lambda e: e.tensor_tensor(out=o3[:, 0, :], in0=o3[:, 0, :], in1=o3[:, 1, :], op=ALU.add), r=[o3], w=[o3], x=[o3])
        V(lambda e: e.tensor_tensor(out=o3[:, 0, :], in0=o3[:, 0, :], in1=o3[:, 2, :], op=ALU.add), r=[o3], w=[o3], x=[o3])
        o8 = o3[:, 0, :].rearrange("p (h e) -> p h e", e=65)
        V(lambda e: e.reciprocal(out=rec[:, :], in_=o8[:, :, 64]), r=[o3], w=[rec])
        V(lambda e: e.tensor_tensor(out=mt[:, 256:768].rearrange("p (h e) -> p h e", e=64), in0=o8[:, :, 0:64], in1=rec[:, :].unsqueeze(2).to_broadcast([128, 8, 64]),
                                    op=ALU.mult), r=[o3, rec], w=[mt])
        pT = B[0]
        pTb = pT[:, :].bitcast(BF16).rearrange("p (k t) -> p k t", k=8)
        for k in range(6):
            self.tr(pTb[:, k, :], mt[:, k * 128:(k + 1) * 128], self.identb[:, :], r=[mt, self.identb], w=[pT], signal=(k == 5))
        S(lambda e: e.copy(out=mixT[:, 0:6, :], in_=pTb[:, 0:6, :]), r=[pT], w=[mixT])

    def p3_S2(self, j):
        if j >= NCH or j < 0:
            return
        B = self.bank
        V, S, G = self.V, self.S, self.G
        rows = slice(j * 128, (j + 1) * 128)
        mixT, cT, xres = self.mixT[j % 2], self.cT[j % 2], self.xres[j % 2]
        wb = [B[1], B[2]] if j % 2 == 0 else [B[6], B[7]]
        for nb in range(2):
            for k in range(8):
                lhs = mixT[:, k, :] if k < 6 else cT[:, k - 6, :]
                self.mm(wb[nb][:, :], lhs, self.wout[:, k, nb * 512:(nb + 1) * 512], r=[mixT, cT, self.wout], w=[wb[nb]], start=(k == 0), stop=(k == 7))
        yy = self.yy
        for nb in range(2):
            cs_ = slice(nb * 512, (nb + 1) * 512)
            V(lambda e, nb=nb, cs_=cs_: e.tensor_tensor(out=yy[:, cs_], in0=wb[nb][:, :], in1=self.opg[:, 0, cs_], op=ALU.mult), r=[wb[nb], self.opg], w=[yy], x=[yy])
        V(lambda e: e.scalar_tensor_tensor(out=yy[:, :], in0=xres[:, :], scalar=float(ALPHA), in1=yy[:, :], op0=ALU.mult, op1=ALU.add), r=[xres, yy], w=[yy], x=[yy])
        self.ln_stats(yy, lambda a, b: yy[:, a:b], D, self.st3, act=True)
        x1 = self.x1t[j % 3]
        S(lambda e: e.activation(out=x1[:, :], in_=yy[:, :], func=AF.Identity, bias=self.st3[:, 4:5], scale=self.st3[:, 3:4]), r=[yy, self.st3], w=[x1])

    def p3_S2b(self, j):
        if j >= NCH or j < 0:
            return
        V = self.V
        rows = slice(j * 128, (j + 1) * 128)
        x1 = self.x1t[j % 3]
        V(lambda e: e.tensor_tensor(out=x1[:, :], in0=x1[:, :], in1=self.lng[:, :], op=ALU.mult), r=[x1, self.lng], w=[x1])
        V(lambda e: e.tensor_tensor(out=x1[:, :], in0=x1[:, :], in1=self.lnb[:, :], op=ALU.add), r=[x1, self.lnb], w=[x1], x=[x1])
        self.dma("sp", self.x1d[rows, :], x1[:, :], r=[x1], w=[self.x1d])

    def p3_S3a(self, j):
        if j >= NCH or j < 0:
            return
        S = self.S
        x1 = self.x1t[j % 3]
        h2f = self.h2f2[j % 2]
        self.ln_stats(x1, lambda a, b: x1[:, a:b], D, self.st3b, act=True)
        S(lambda e: e.activation(out=h2f[:, :], in_=x1[:, :], func=AF.Identity, bias=self.st3b[:, 4:5], scale=self.st3b[:, 3:4]), r=[x1, self.st3b], w=[h2f])

    def p3_S3b(self, j):
        if j >= NCH or j < 0:
            return
        B = self.bank
        V, S, G = self.V, self.S, self.G
        h2f = self.h2f2[j % 2]
        V(lambda e: e.tensor_tensor(out=h2f[:, :], in0=h2f[:, :], in1=self.opg2[:, 1, :], op=ALU.mult), r=[h2f, self.opg2], w=[h2f])
        V(lambda e: e.tensor_tensor(out=h2f[:, :], in0=h2f[:, :], in1=self.opg2[:, 0, :], op=ALU.add), r=[h2f, self.opg2], w=[h2f], x=[h2f])
        S(lambda e: e.copy(out=self.h2all[:, j, :], in_=h2f[:, :]), r=[h2f], w=[self.h2c[j]])
        for half in range(2):
            pt = B[3 + half]
            for k in range(4):
                self.tr(pt[:, k * 128:(k + 1) * 128], h2f[:, (half * 4 + k) * 128:(half * 4 + k + 1) * 128], self.identf[:, :], r=[h2f, self.identf], w=[pt], signal=(k == 3))
            S(lambda e, half=half, pt=pt: e.copy(out=self.h2T[:, half * 4:(half + 1) * 4, :], in_=pt[:, :].rearrange("p (k t) -> p k t", k=4)), r=[pt], w=[self.h2T])
        lg = B[5]
        for k in range(8):
            self.mm(lg[:, 0:16], self.h2T[:, k, :], self.rw[:, k, :], r=[self.h2T, self.rw], w=[lg], start=(k == 0), stop=(k == 7))
        S(lambda e: e.copy(out=self.scall[:, j, :], in_=lg[:, 0:16]), r=[lg], w=[self.scall])
        if (j + 1) % self.RSEG == 0:
            self.p3_route(j + 1 - self.RSEG)

    def p3_all(self, l, x_src):
        self.p3_L1(0)
        for j in range(-2, NCH + 2):
            self.p3_L1(j + 3)
            self.p3_L2(j + 2, x_src)
            self.p3_S1(j + 2)
            self.p3_S2(j + 1)
            self.p3_S2b(j)
            self.p3_S3a(j - 1)
            self.p3_S3b(j - 2)

    RSEG = 8

    def p3_route_alloc(self):
        s = self.sb
        NJ = self.RSEG
        mk = lambda n: s(n, [128, NJ, 16], F32)
        t = {}
        t["big"] = [mk(f"r_{i}") for i in range(14)]
        t["g4"] = [s(f"r4_{i}", [128, NJ, 4], F32) for i in range(4)]
        t["g1"] = [s(f"r1_{i}", [128, NJ], F32) for i in range(3)]
        t["mask_e0"] = mk("mask_e0")
        t["mask_j0"] = s("mask_j0", [128, 16, NJ], F32)
        G = self.G
        G(lambda e: e.memset(t["mask_e0"][:, :, :], 1.0), w=[t["mask_e0"]])
        G(lambda e: e.memset(t["mask_e0"][:, :, 0:1], 0.0), w=[t["mask_e0"]])
        G(lambda e: e.memset(t["mask_j0"][:, :, :], 1.0), w=[t["mask_j0"]])
        G(lambda e: e.memset(t["mask_j0"][:, :, 0:1], 0.0), w=[t["mask_j0"]])
        self.carry = s("carry", [128, 16], F32)
        G(lambda e: e.memset(self.carry[:, :], 0.0), w=[self.carry])
        self._rt = t

    def p3_route(self, j0):
        B = self.bank
        V, S, G = self.V, self.S, self.G
        NJ = self.RSEG
        t = self._rt
        sel, eq, msk, top2, chosen, gw, tmp, cum, pos, valid, slotv, baseT, totT, sc = t["big"]
        m1, m2, gs, gsel = t["g4"]
        gmax, gsum, t32 = t["g1"]
        mask_e0, mask_j0 = t["mask_e0"], t["mask_j0"]
        f2 = lambda t_: t_[:, :, :].rearrange("p j e -> p (j e)")
        S(lambda e: e.activation(out=f2(sc), in_=self.scall[:, j0:j0 + NJ, :].rearrange("p j e -> p (j e)"), func=AF.Sigmoid), r=[self.scall], w=[sc])
        g4 = lambda t: t[:, :, :].rearrange("p j (g i) -> p (j g) i", i=4)
        b4 = lambda t: t[:, :, :].rearrange("p j g -> p (j g)").unsqueeze(2).to_broadcast([128, NJ * 4, 4])
        V(lambda e: e.tensor_tensor(out=sel[:, :, :], in0=sc[:, :, :], in1=self.rbias[:, :].unsqueeze(1).to_broadcast([128, NJ, 16]), op=ALU.add), r=[sc, self.rbias], w=[sel])
        V(lambda e: e.tensor_reduce(out=m1[:, :, :].rearrange("p j g -> p (j g)"), in_=g4(sel), axis=AX.X, op=ALU.max), r=[sel], w=[m1])
        V(lambda e: e.tensor_tensor(out=g4(eq), in0=g4(sel), in1=b4(m1), op=ALU.is_equal), r=[sel, m1], w=[eq])
        V(lambda e: e.scalar_tensor_tensor(out=f2(msk), in0=f2(eq), scalar=-1e9, in1=f2(sel), op0=ALU.mult, op1=ALU.add), r=[eq, sel], w=[msk])
        V(lambda e: e.tensor_reduce(out=m2[:, :, :].rearrange("p j g -> p (j g)"), in_=g4(msk), axis=AX.X, op=ALU.max), r=[msk], w=[m2])
        V(lambda e: e.tensor_tensor(out=gs[:, :, :], in0=m1[:, :, :], in1=m2[:, :, :], op=ALU.add), r=[m1, m2], w=[gs])
        V(lambda e: e.tensor_reduce(out=gmax[:, :], in_=gs[:, :, :], axis=AX.X, op=ALU.max), r=[gs], w=[gmax])
        V(lambda e: e.tensor_tensor(out=gsel[:, :, :], in0=gs[:, :, :], in1=gmax[:, :].unsqueeze(2).to_broadcast([128, NJ, 4]), op=ALU.is_equal), r=[gs, gmax], w=[gsel])
        V(lambda e: e.tensor_tensor(out=g4(top2), in0=g4(sel), in1=b4(m2), op=ALU.is_ge), r=[sel, m2], w=[top2])
        V(lambda e: e.tensor_tensor(out=g4(chosen), in0=g4(top2), in1=b4(gsel), op=ALU.mult), r=[top2, gsel], w=[chosen])
        V(lambda e: e.tensor_tensor(out=gw[:, :, :], in0=chosen[:, :, :], in1=sc[:, :, :], op=ALU.mult), r=[chosen, sc], w=[gw])
        V(lambda e: e.tensor_reduce(out=gsum[:, :], in_=gw[:, :, :], axis=AX.X, op=ALU.add), r=[gw], w=[gsum])
        V(lambda e: e.reciprocal(out=gsum[:, :], in_=gsum[:, :]), r=[gsum], w=[gsum])
        V(lambda e: e.tensor_tensor(out=gw[:, :, :], in0=gw[:, :, :], in1=gsum[:, :].unsqueeze(2).to_broadcast([128, NJ, 16]), op=ALU.mult), r=[gw, gsum], w=[gw])
        cbk = B[5]
        W = NJ * 16
        self.mm(cbk[:, 128:128 + W], self.trs[:, :], f2(chosen), r=[self.trs, chosen], w=[cbk], start=True, stop=True)
        self.mm(cbk[:, 256:256 + W], self.onesf[:, :], f2(chosen), r=[self.onesf, chosen], w=[cbk], start=True, stop=True)
        V(lambda e: e.tensor_copy(out=f2(totT).rearrange("p (e j) -> p e j", e=16),
                                  in_=cbk[:, 256:256 + W].rearrange("p (j e) -> p e j", e=16)), r=[cbk], w=[totT])
        V(lambda e: e.tensor_tensor_scan(out=f2(baseT), data0=mask_j0[:, :, :].rearrange("p e j -> p (e j)"), data1=f2(totT), initial=0.0, op0=ALU.mult, op1=ALU.add),
          r=[mask_j0, totT], w=[baseT])
        V(lambda e: e.tensor_tensor(out=f2(baseT), in0=f2(baseT), in1=f2(totT), op=ALU.subtract), r=[baseT, totT], w=[baseT])
        bT3 = f2(baseT).rearrange("p (e j) -> p e j", e=16)
        tT3 = f2(totT).rearrange("p (e j) -> p e j", e=16)
        V(lambda e: e.tensor_tensor(out=bT3, in0=bT3, in1=self.carry[:, :].unsqueeze(2).to_broadcast([128, 16, NJ]), op=ALU.add), r=[baseT, self.carry], w=[baseT])
        V(lambda e: e.tensor_tensor(out=self.carry[:, :], in0=bT3[:, :, NJ - 1], in1=tT3[:, :, NJ - 1], op=ALU.add), r=[baseT, totT], w=[self.carry])
        V(lambda e: e.tensor_tensor(out=pos[:, :, :], in0=cbk[:, 128:128 + W].rearrange("p (j e) -> p j e", e=16),
                                    in1=f2(baseT).rearrange("p (e j) -> p j e", e=16), op=ALU.add), r=[cbk, baseT], w=[pos])
        V(lambda e: e.tensor_scalar(out=f2(valid), in0=f2(pos), scalar1=float(self.CAP), scalar2=None, op0=ALU.is_lt), r=[pos], w=[valid])
        V(lambda e: e.tensor_tensor(out=slotv[:, :, :], in0=pos[:, :, :], in1=self.eoff[:, :].unsqueeze(1).to_broadcast([128, NJ, 16]), op=ALU.add), r=[pos, self.eoff], w=[slotv])
        V(lambda e: e.tensor_scalar(out=f2(slotv), in0=f2(slotv), scalar1=self.trashp[:, 0:1], scalar2=None, op0=ALU.subtract), r=[slotv, self.trashp], w=[slotv])
        V(lambda e: e.tensor_tensor(out=f2(slotv), in0=f2(slotv), in1=f2(valid), op=ALU.mult), r=[slotv, valid], w=[slotv])
        V(lambda e: e.tensor_scalar(out=f2(slotv), in0=f2(slotv), scalar1=self.trashp[:, 0:1], scalar2=None, op0=ALU.add), r=[slotv, self.trashp], w=[slotv])
        V(lambda e: e.tensor_tensor(out=f2(gw), in0=f2(gw), in1=f2(valid), op=ALU.mult), r=[gw, valid], w=[gw])
        V(lambda e: e.tensor_tensor_scan(out=f2(cum), data0=f2(mask_e0), data1=f2(chosen), initial=0.0, op0=ALU.mult, op1=ALU.add), r=[mask_e0, chosen], w=[cum])
        for which, dsti in ((1.0, self.slotA), (2.0, self.slotB)):
            wi = int(which) - 1
            V(lambda e, which=which: e.tensor_scalar(out=f2(tmp), in0=f2(cum), scalar1=float(which), scalar2=None, op0=ALU.is_equal), r=[cum], w=[tmp])
            V(lambda e: e.tensor_tensor(out=f2(tmp), in0=f2(tmp), in1=f2(chosen), op=ALU.mult), r=[tmp, chosen], w=[tmp])
            V(lambda e: e.tensor_tensor(out=f2(eq), in0=f2(tmp), in1=f2(gw), op=ALU.mult), r=[tmp, gw], w=[eq])
            V(lambda e, wi=wi: e.tensor_reduce(out=self.gAB[:, wi, j0:j0 + NJ], in_=eq[:, :, :], axis=AX.X, op=ALU.add), r=[eq], w=[self.gAB])
            V(lambda e: e.tensor_tensor(out=f2(tmp), in0=f2(tmp), in1=f2(slotv), op=ALU.mult), r=[tmp, slotv], w=[tmp])
            V(lambda e: e.tensor_reduce(out=t32[:, :], in_=tmp[:, :, :], axis=AX.X, op=ALU.add), r=[tmp], w=[t32])
            V(lambda e: e.tensor_scalar(out=t32[:, :], in0=t32[:, :], scalar1=0.0, scalar2=float(self.NSLOT - 1), op0=ALU.max, op1=ALU.min), r=[t32], w=[t32])
            V(lambda e, dsti=dsti: e.tensor_copy(out=dsti[:, j0:j0 + NJ], in_=t32[:, :]), r=[t32], w=[dsti])
        for j in range(j0, j0 + NJ):
            for dsti in (self.slotA, self.slotB):
                self.cx.dma("pool", None, None, reads=[self.h2c[j].b, dsti.b], writes=[],
                            fn=lambda e, dsti=dsti, j=j: e.indirect_dma_start(out=self.h2slots[:, :], out_offset=bass.IndirectOffsetOnAxis(ap=dsti[:, j:j + 1], axis=0),
                                                                              in_=self.h2all[:, j, :], in_offset=None))

    def p4_io(self):
        i = self.inp
        self.w_gate = i("exp_w_gate", [2, 16, D, 512])
        self.w_up = i("exp_w_up", [2, 16, D, 512])
        self.w_down = i("exp_w_down", [2, 16, 512, D])

    def zero_slots(self):
        self.push()
        zt = self.sb("zt", [128, D], F32)
        self.G(lambda e: e.memset(zt[:, :], 0.0), w=[zt])
        self.dma("sp", self.oslots[16 * self.CAP:16 * self.CAP + 128, :], zt[:, :], r=[zt], w=[self.oslots])
        self.pop()

    def p4_experts(self, l):
        B = self.bank
        V, S, G = self.V, self.S, self.G
        s = self.sb
        wg = [s(f"wg{i}", [128, 8, 512], BF16) for i in range(2)]
        wu = [s(f"wu{i}", [128, 8, 512], BF16) for i in range(2)]
        wd = [s(f"wd{i}", [128, 4, D], BF16) for i in range(2)]
        rt = [s(f"rtok{i}", [128, 4, D], BF16) for i in range(2)]
        rT = s("rT", [128, 8, 512], BF16)
        sil = [s(f"sil{i}", [128, 512], BF16) for i in range(2)]
        hidT = s("hidT", [128, 4, 512], BF16)
        osb = [s(f"eosb{i}", [128, D], F32) for i in range(2)]

        def load_w(e):
            i = e % 2
            self.dma("pool", wg[i][:, :, :], self.w_gate[l, e].rearrange("(k p) f -> p k f", p=128), r=[self.w_gate], w=[wg[i]])
            self.dma("pool", wu[i][:, :, :], self.w_up[l, e].rearrange("(k p) f -> p k f", p=128), r=[self.w_up], w=[wu[i]])
            self.dma("pool", wd[i][:, :, :], self.w_down[l, e].rearrange("(k p) f -> p k f", p=128), r=[self.w_down], w=[wd[i]])

        glist = [(e, off, nb) for e in range(16) for (off, nb) in self.GROUPS]
        load_w(0)
        ob = 0

        def load_rows(gi):
            e, off, nb = glist[gi]
            r0 = e * self.CAP + off
            rtk = rt[gi % 2]
            self.dma("sp", rtk[:, 0:nb, :], self.h2slots[r0:r0 + nb * 128, :].rearrange("(b p) d -> p b d", p=128), r=[self.h2slots], w=[rtk])
        load_rows(0)
        for gi, (e, off, nb) in enumerate(glist):
            if off == 0 and e + 1 < 16:
                load_w(e + 1)
            i = e % 2
            r0 = e * self.CAP + off
            N = nb * 128
            rtk = rt[gi % 2]
            if gi + 1 < len(glist):
                load_rows(gi + 1)
            for blk in range(nb):
                pT = B[blk % 2]
                pTb = pT[:, :].bitcast(BF16).rearrange("p (k t) -> p k t", k=8)
                for k in range(8):
                    self.tr(pTb[:, k, :], rtk[:, blk, k * 128:(k + 1) * 128], self.identb[:, :], r=[rtk, self.identb], w=[pT], signal=(k == 7))
                S(lambda e_, blk=blk, pTb=pTb: e_.copy(out=rT[:, :, blk * 128:(blk + 1) * 128], in_=pTb), r=[pT], w=[rT], x=[rT])
            for fc in range(4):
                pg, pu = B[2 + 2 * (fc % 2)], B[3 + 2 * (fc % 2)]
                for k in range(8):
                    self.mm(pg[:, 0:N], wg[i][:, k, fc * 128:(fc + 1) * 128], rT[:, k, 0:N], r=[wg[i], rT], w=[pg], start=(k == 0), stop=(k == 7))
                for k in range(8):
                    self.mm(pu[:, 0:N], wu[i][:, k, fc * 128:(fc + 1) * 128], rT[:, k, 0:N], r=[wu[i], rT], w=[pu], start=(k == 0), stop=(k == 7))
                sl = sil[fc % 2]
                S(lambda e_, sl=sl, pg=pg, N=N: e_.activation(out=sl[:, 0:N], in_=pg[:, 0:N], func=AF.Silu), r=[pg], w=[sl])
                V(lambda e_, sl=sl, pu=pu, fc=fc, N=N: e_.tensor_tensor(out=hidT[:, fc, 0:N], in0=pu[:, 0:N], in1=sl[:, 0:N], op=ALU.mult), r=[pu, sl], w=[hidT])
            for blk in range(nb):
                o = osb[ob % 2]
                ob += 1
                for half in range(2):
                    pd = B[6 + half]
                    for fc in range(4):
                        self.mm(pd[:, :], hidT[:, fc, blk * 128:(blk + 1) * 128], wd[i][:, fc, half * 512:(half + 1) * 512], r=[hidT, wd[i]], w=[pd],
                                start=(fc == 0), stop=(fc == 3))
                    V(lambda e_, o=o, pd=pd, half=half: e_.tensor_tensor(out=o[:, half * 512:(half + 1) * 512], in0=pd[:, :],
                                                                         in1=self.opg[:, 1, half * 512:(half + 1) * 512], op=ALU.mult), r=[pd, self.opg], w=[o], x=[o])
                self.dma("sp", self.oslots[r0 + blk * 128:r0 + (blk + 1) * 128, :], o[:, :], r=[o], w=[])

    def p5_alloc(self, l):
        s = self.sb
        self.lng2 = s("lng2", [128, D], F32); self.lnb2 = s("lnb2", [128, D], F32)
        self.dma("sp", self.lng2[:, :], self.ln2_g[l].partition_broadcast(128), r=[self.ln2_g], w=[self.lng2])
        self.dma("sp", self.lnb2[:, :], self.ln2_b[l].partition_broadcast(128), r=[self.ln2_b], w=[self.lnb2])
        self.rA = [s(f"rA{i}", [128, D], F32) for i in range(2)]
        self.rB = [s(f"rB{i}", [128, D], F32) for i in range(2)]
        self.x1r = [s(f"x1r{i}", [128, D], F32) for i in range(2)]
        self.x2t = [s(f"x2t{i}", [128, D], F32) for i in range(2)]
        self.st5 = s("st5", [128, 24], F32)
        self.y5 = [s(f"y5_{i}", [128, D], F32) for i in range(2)]

    def p5_loads(self, jj):
        if jj >= NCH:
            return
        rA_, rB_, x1r_ = self.rA[jj % 2], self.rB[jj % 2], self.x1r[jj % 2]
        self.cx.dma("pool", None, None, reads=[self.oslots.b, self.slotA.b], writes=[rA_.b],
                    fn=lambda e: e.indirect_dma_start(out=rA_[:, :], out_offset=None, in_=self.oslots[:, :],
                                                      in_offset=bass.IndirectOffsetOnAxis(ap=self.slotA[:, jj:jj + 1], axis=0)))
        self.cx.dma("pool", None, None, reads=[self.oslots.b, self.slotB.b], writes=[rB_.b],
                    fn=lambda e: e.indirect_dma_start(out=rB_[:, :], out_offset=None, in_=self.oslots[:, :],
                                                      in_offset=bass.IndirectOffsetOnAxis(ap=self.slotB[:, jj:jj + 1], axis=0)))
        self.dma("sp", x1r_[:, :], self.x1d[jj * 128:(jj + 1) * 128, :], r=[self.x1d], w=[x1r_])

    def p5_S1(self, j):
        if j >= NCH:
            return
        V, S, G = self.V, self.S, self.G
        rA, rB, x1r, y5 = self.rA[j % 2], self.rB[j % 2], self.x1r[j % 2], self.y5[j % 2]
        S(lambda e: e.activation(out=rA[:, :], in_=rA[:, :], func=AF.Identity, scale=self.gAB[:, 0, j:j + 1]), r=[rA, self.gAB], w=[rA])
        V(lambda e: e.scalar_tensor_tensor(out=rA[:, :], in0=rB[:, :], scalar=self.gAB[:, 1, j:j + 1], in1=rA[:, :], op0=ALU.mult, op1=ALU.add), r=[rB, rA, self.gAB], w=[rA])
        V(lambda e: e.scalar_tensor_tensor(out=y5[:, :], in0=x1r[:, :], scalar=float(ALPHA), in1=rA[:, :], op0=ALU.mult, op1=ALU.add), r=[x1r, rA], w=[y5], x=[rA])

    def p5_S2(self, j, dst):
        if j >= NCH or j < 0:
            return
        S = self.S
        y5, x2 = self.y5[j % 2], self.x2t[j % 2]
        self.ln_stats(y5, lambda a, b: y5[:, a:b], D, self.st5, act=True)
        S(lambda e: e.activation(out=x2[:, :], in_=y5[:, :], func=AF.Identity, bias=self.st5[:, 4:5], scale=self.st5[:, 3:4]), r=[y5, self.st5], w=[x2])

    def p5_S3(self, j, dst):
        if j >= NCH or j < 0:
            return
        V, G = self.V, self.G
        rows = slice(j * 128, (j + 1) * 128)
        x2 = self.x2t[j % 2]
        V(lambda e: e.tensor_tensor(out=x2[:, :], in0=x2[:, :], in1=self.lng2[:, :], op=ALU.mult), r=[x2, self.lng2], w=[x2])
        V(lambda e: e.tensor_tensor(out=x2[:, :], in0=x2[:, :], in1=self.lnb2[:, :], op=ALU.add), r=[x2, self.lnb2], w=[x2], x=[x2])
        self.dma("sp", dst[rows, :], x2[:, :], r=[x2], w=[dst])

    def p5_all(self, l, dst):
        self.p5_loads(0); self.p5_loads(1)
        self.p5_S1(0)
        self.p5_loads(2)
        self.p5_S1(1)
        self.p5_S2(0, dst)
        for j in range(NCH):
            self.p5_loads(j + 3)
            self.p5_S1(j + 2)
            self.p5_S2(j + 1, dst)
            self.p5_S3(j, dst)

    def build(self):
        self.declare_io(); self.s5_io(); self.p3_io(); self.p4_io()
        self.setup()
        x_src = self.x_in
        for l in range(self.nlayers):
            dst = self.out if l == self.nlayers - 1 else self.xmid
            self.push()
            self.layer_alloc(); self.route_alloc(); self.layer_prep(l, 0)
            self.zero_slots()
            self.push(); self.qk_alloc()
            self.push(); self.s5_alloc(); self.s5_prep(l); self.load_win(l); self.p1v2_alloc()
            self.p1_all(l, x_src)
            self.pop()
            self.push(); self.p2_alloc(); self.p2_attention(); self.pop()
            self.pop()
            self.push()
            self.opg = self.sb("opg", [128, 2, 1024], F32)
            self.opg2 = self.sb("opg2", [128, 2, 1024], F32)
            self.layer_prep(l, 1)
            self.push(); self.p3_alloc(l)
            self.p3_route_alloc()
            self.p3_all(l, x_src)
            self.pop()
            self.push(); self.p4_experts(l); self.pop()
            self.push(); self.p5_alloc(l)
            self.p5_all(l, dst)
            self.pop()
            self.pop()
            self.pop()
            x_src = dst
        if getattr(self, "dbg_hook", None):
            self.dbg_hook(self)
        self.finish()


def make_inputs(inp, b):
    c = np.ascontiguousarray
    f = lambda k: np.asarray(inp[k])
    br, bi = f("ssm_b_re"), f("ssm_b_im")
    def bl(a):
        L = a.shape[0]
        return a.reshape(L, 2, 8, 64, 16).transpose(0, 2, 4, 1, 3).reshape(L, 128, 2, 64)
    bT = np.stack([bl(br), bl(bi)], axis=1)
    cr, ci = f("ssm_c_re"), f("ssm_c_im")
    cT = np.concatenate([cr.transpose(0, 1, 3, 2), ci.transpose(0, 1, 3, 2)], axis=2)
    L = br.shape[0]
    d = {
        "x": c(f("x")[b]), "ccol": c(f("c")[b].reshape(8, 128).T), "pos": c(f("positions")[b].reshape(32, 128).T),
        "ada_w": f("ada_w"), "ada_b": f("ada_b"), "w_in": f("w_in"), "gm_ln_g": f("gm_ln_g"), "gm_ln_b": f("gm_ln_b"),
        "gm_ws": f("gm_ws"), "gm_bsT": c(f("gm_bs").transpose(0, 2, 1)),
        "lam_re": c(f("ssm_lam_re").reshape(L, 1024)), "lam_im": c(f("ssm_lam_im").reshape(L, 1024)), "log_dt": f("ssm_log_dt"),
        "ssm_bT": c(bT), "ssm_cT": c(cT), "ssm_dT": c(f("ssm_d").reshape(L, 2, 128).transpose(0, 2, 1)),
        "glu_w": f("glu_w"), "glu_bT": c(f("glu_b").reshape(L, 2, 128).transpose(0, 2, 1)),
        "w_out": f("w_out"), "ln1_g": f("ln1_g"), "ln1_b": f("ln1_b"), "ln2_g": f("ln2_g"), "ln2_b": f("ln2_b"),
        "router_w": f("router_w"), "router_bias": f("router_bias"),
        "exp_w_gate": f("exp_w_gate"), "exp_w_up": f("exp_w_up"), "exp_w_down": f("exp_w_down"),
    }
    return d


_CACHE = {}


def kernel(**inputs):
    n = 8
    if "nc" not in _CACHE:
        nc = bass.Bass("TRN2", target_bir_lowering=False)
        kb = KB(nc)
        kb.build()
        _CACHE["nc"] = nc
        _CACHE["names"] = kb.in_names
    nc = _CACHE["nc"]
    names = _CACHE["names"]
    in_maps = []
    for b in range(n):
        im = make_inputs(inputs, b)
        in_maps.append({k: v for k, v in im.items() if k in names})
    res = run_bass_kernel_spmd(nc, in_maps, core_ids=list(range(n)))
    out = np.stack([np.asarray(r["out"]) for r in res.results], axis=0)
    return out.astype(np.float32)
```

```python
import numpy as np
import concourse.bass as bass
import concourse.mybir as mybir

F32 = mybir.dt.float32
BF16 = mybir.dt.bfloat16
I32 = mybir.dt.int32
U32 = mybir.dt.uint32
AF = mybir.ActivationFunctionType
ALU = mybir.AluOpType
AX = mybir.AxisListType


RELAXED = ()
ALLOW_RELAX = True


class Buf:
    __slots__ = ("w", "r", "name")

    def __init__(self, name=""):
        self.w = None
        self.r = []
        self.name = name


class Ctx:
    def __init__(self, nc, strict_same=False):
        self.nc = nc
        self.strict_same = strict_same
        self.relaxed = set(RELAXED)
        self.engs = {"pe": nc.tensor, "act": nc.scalar, "dve": nc.vector, "pool": nc.gpsimd, "sp": nc.sync}
        self.sem = {}
        self.cnt = {}
        for e in ("pe", "act", "dve", "pool"):
            self.sem[e] = nc.alloc_semaphore("s_" + e)
            self.cnt[e] = 0
        self.dq = {}
        for q, n in (("sp", 10), ("act", 4), ("pool", 8)):
            self.dq[q] = {"sems": [nc.alloc_semaphore(f"d_{q}{i}") for i in range(n)], "vals": [0] * n, "k": 0}
        self.waited = {}
        self.nbuf = 0
        self.out_events = []

    def buf(self, name=""):
        return Buf(name)

    def _wait(self, eng, ev):
        sem, val = ev
        key = (eng, id(sem))
        if self.waited.get(key, 0) >= val:
            return
        self.engs[eng].wait_ge(sem, val)
        self.waited[key] = val

    def _deps(self, eng, reads, writes, relax=()):
        own = self.sem.get(eng)
        rl = set(id(b) for b in relax) if ALLOW_RELAX else set()

        def chk(b, ev):
            if ev[0] is own and (eng == "pe" or id(b) in rl):
                return
            self._wait(eng, ev)
        for b in reads:
            if b.w is not None:
                chk(b, b.w)
        for b in writes:
            if b.w is not None:
                chk(b, b.w)
            for ev in b.r:
                chk(b, ev)

    def _commit(self, ev, reads, writes):
        for b in writes:
            b.w = ev
            b.r = []
        for b in reads:
            b.r.append(ev)
            if len(b.r) > 24:
                b.r = b.r[-24:]

    def op(self, eng, fn, reads=(), writes=(), signal=True, relax=()):
        self._deps(eng, reads, writes, relax)
        inst = fn(self.engs[eng])
        if signal:
            self.cnt[eng] += 1
            inst.then_inc(self.sem[eng], 1)
            ev = (self.sem[eng], self.cnt[eng])
        else:
            ev = (self.sem[eng], self.cnt[eng] + 1)
        self._commit(ev, reads, writes)
        return ev

    def dma(self, q, out, in_, reads=(), writes=(), fn=None, **kw):
        d = self.dq[q]
        i = d["k"] % len(d["sems"])
        d["k"] += 1
        sem = d["sems"][i]
        self._deps(q, reads, writes)
        if d["vals"][i] > 0:
            self._wait(q, (sem, d["vals"][i]))
        if fn is None:
            inst = self.engs[q].dma_start(out=out, in_=in_, **kw)
        else:
            inst = fn(self.engs[q])
        d["vals"][i] += 16
        inst.then_inc(sem, 16)
        ev = (sem, d["vals"][i])
        self._commit(ev, reads, writes)
        return ev

    def barrier(self):
        evs = [(self.sem[e], self.cnt[e]) for e in self.sem if self.cnt[e] > 0]
        for q, d in self.dq.items():
            for sem, v in zip(d["sems"], d["vals"]):
                if v > 0:
                    evs.append((sem, v))
        for eng in ("pe", "act", "dve", "pool", "sp"):
            own = self.sem.get(eng)
            for ev in evs:
                self._wait(eng, ev)

    def finish(self, bufs):
        for b in bufs:
            if b.w is not None:
                self._wait("sp", b.w)
            for ev in b.r:
                self._wait("sp", ev)

from concourse.bass_utils import run_bass_kernel_spmd
import math
import contextlib

T = 4096
D = 1024
NCH = 32
PW = 2304
EPS = 1e-5
ALPHA = (2.0 * 2) ** 0.25
TWO_PI = 2.0 * math.pi
ROPE_THETA = 500000.0


class Tl:
    def __init__(self, h, name=""):
        self.h = h
        self.b = Buf(name)

    def __getitem__(self, k):
        return self.h[k]


class KB:
    def __init__(self, nc, nlayers=2, dbg=(), stop_after=None):
        self.nc = nc
        self.cx = Ctx(nc)
        self.dbg = set(dbg)
        self.stop_after = stop_after
        self.nlayers = nlayers
        self.outs = []
        self.stk = [contextlib.ExitStack()]
        self.nps = 0

    def inp(self, name, shape, dt=F32):
        self.in_names = getattr(self, "in_names", set())
        self.in_names.add(name)
        return Tl(self.nc.dram_tensor(name, list(shape), dt, kind="ExternalInput").ap(), name)

    def outp(self, name, shape, dt=F32):
        t = Tl(self.nc.dram_tensor(name, list(shape), dt, kind="ExternalOutput").ap(), name)
        self.outs.append(t)
        return t

    def scratch(self, name, shape, dt):
        return Tl(self.nc.dram_tensor(name, list(shape), dt, kind="Internal").ap(), name)

    def sb(self, name, shape, dt):
        self.nsb = getattr(self, "nsb", 0) + 1
        h = self.stk[-1].enter_context(self.nc.sbuf_tensor(f"{name}_{self.nsb}", list(shape), dt))
        return Tl(h, name)

    def push(self):
        self.stk.append(contextlib.ExitStack())

    def pop(self):
        self.cx.barrier()
        self.stk.pop().close()

    def ps(self, name, shape, dt=F32):
        return Tl(self.nc.alloc_psum_tensor(name, list(shape), dt), name)

    def _rw(self, r, w):
        return [t.b for t in r], [t.b for t in w]

    def V(self, fn, r=(), w=(), x=()):
        r, w = self._rw(r, w)
        return self.cx.op("dve", fn, r, w, relax=[t.b for t in x])

    def S(self, fn, r=(), w=(), x=()):
        r, w = self._rw(r, w)
        return self.cx.op("act", fn, r, w, relax=[t.b for t in x])

    def G(self, fn, r=(), w=(), x=()):
        r, w = self._rw(r, w)
        return self.cx.op("pool", fn, r, w, relax=[t.b for t in x])

    def P(self, fn, r=(), w=(), signal=True):
        r, w = self._rw(r, w)
        return self.cx.op("pe", fn, r, w, signal=signal)

    def dma(self, q, out, in_, r=(), w=(), **kw):
        r, w = self._rw(r, w)
        return self.cx.dma(q, out, in_, r, w, **kw)

    def mm(self, out, lhsT, rhs, r, w, start, stop, signal=None):
        if signal is None:
            signal = stop
        return self.P(lambda e: e.matmul(out, lhsT, rhs, start=start, stop=stop), r, w, signal=signal)

    def tr(self, out, in_, ident, r, w, signal=True):
        return self.P(lambda e: e.transpose(out, in_, ident), r, w, signal=signal)

    def declare_io(self):
        i = self.inp
        self.x_in = i("x", [T, D])
        self.ccol = i("ccol", [128, 8])
        self.pos = i("pos", [128, NCH], I32)
        self.ada_w = i("ada_w", [2, D, 6 * D])
        self.ada_b = i("ada_b", [2, 6 * D])
        self.w_in = i("w_in", [2, D, PW])
        self.gm_ln_g = i("gm_ln_g", [2, 256])
        self.gm_ln_b = i("gm_ln_b", [2, 256])
        self.gm_ws = i("gm_ws", [2, 4, 128, 128])
        self.gm_bsT = i("gm_bsT", [2, 128, 4])
        self.out = self.outp("out", [T, D])
        self.mixtok = self.scratch("mixtok", [T, 1024], BF16)
        self.vd = self.scratch("vd", [T, 520], BF16)

    def consts(self):
        nc = self.nc
        self.identb = self.sb("identb", [128, 128], BF16)
        self.identf = self.sb("identf", [128, 128], F32)
        self.onesf = self.sb("onesf", [128, 128], F32)
        self.eps_t = self.sb("eps_t", [128, 1], F32)
        self.G(lambda e: e.memset(self.onesf[:, :], 1.0), w=[self.onesf])
        self.G(lambda e: e.memset(self.eps_t[:, :], EPS), w=[self.eps_t])
        self.mhalf = self.sb("mhalf", [128, 1], F32)
        self.G(lambda e: e.memset(self.mhalf[:, :], -0.5), w=[self.mhalf])
        self.G(lambda e: e.affine_select(out=self.identf[:, :], in_=self.onesf[:, :], pattern=[[-1, 128]],
                                         compare_op=ALU.is_equal, fill=0.0, base=0, channel_multiplier=1),
               r=[self.onesf], w=[self.identf])
        self.G(lambda e: e.tensor_copy(out=self.identb[:, :], in_=self.identf[:, :]), r=[self.identf], w=[self.identb])
        self.posf = self.sb("posf", [128, NCH], F32)
        self.posi = self.sb("posi", [128, NCH], I32)
        self.dma("sp", self.posi[:, :], self.pos[:, :], r=[self.pos], w=[self.posi])
        self.V(lambda e: e.tensor_copy(out=self.posf[:, :], in_=self.posi[:, :]), r=[self.posi], w=[self.posf])
        self.cs = self.sb("cs", [128, NCH, 8], F32)
        self.sn = self.sb("sn", [128, NCH, 8], F32)
        self.push()
        ang = self.sb("ang", [128, NCH, 8], F32)
        tmp = self.sb("angt", [128, NCH, 8], F32)
        tmi = self.sb("angi", [128, NCH, 8], I32)
        for j in range(8):
            fr = ROPE_THETA ** (-(j * 2.0) / 16.0)
            self.V(lambda e, j=j, fr=fr: e.tensor_scalar(out=ang[:, :, j], in0=self.posf[:, :], scalar1=float(fr), scalar2=None, op0=ALU.mult),
                   r=[self.posf], w=[ang])
        self.sincos(ang, self.sn, self.cs, tmp, tmi, [128, NCH * 8])
        self.pop()

    def _flat(self, t):
        ap = t[:]
        if len(ap.shape) == 2:
            return ap
        names = " ".join(f"a{i}" for i in range(len(ap.shape) - 1))
        return ap.rearrange(f"p {names} -> p ({names})")

    def range_reduce(self, src, dst, tmp, tmi, shift):
        s, d, t, ti = self._flat(src), self._flat(dst), self._flat(tmp), self._flat(tmi)
        self.V(lambda e: e.tensor_scalar(out=t, in0=s, scalar1=float(shift), scalar2=float(1.0 / TWO_PI), op0=ALU.add, op1=ALU.mult),
               r=[src], w=[tmp])
        self.V(lambda e: e.tensor_copy(out=ti, in_=t), r=[tmp], w=[tmi])
        self.V(lambda e: e.tensor_copy(out=t, in_=ti), r=[tmi], w=[tmp])
        self.V(lambda e: e.tensor_scalar(out=t, in0=t, scalar1=float(-TWO_PI), scalar2=float(shift), op0=ALU.mult, op1=ALU.add),
               r=[tmp], w=[tmp])
        self.V(lambda e: e.tensor_tensor(out=d, in0=t, in1=s, op=ALU.add), r=[tmp, src], w=[dst])
        self.V(lambda e: e.tensor_scalar(out=t, in0=d, scalar1=float(math.pi), scalar2=float(-TWO_PI), op0=ALU.is_gt, op1=ALU.mult),
               r=[dst], w=[tmp])
        self.V(lambda e: e.tensor_tensor(out=d, in0=d, in1=t, op=ALU.add), r=[tmp, dst], w=[dst])
        self.V(lambda e: e.tensor_scalar(out=t, in0=d, scalar1=float(-math.pi), scalar2=float(TWO_PI), op0=ALU.is_lt, op1=ALU.mult),
               r=[dst], w=[tmp])
        self.V(lambda e: e.tensor_tensor(out=d, in0=d, in1=t, op=ALU.add), r=[tmp, dst], w=[dst])
        self.V(lambda e: e.tensor_scalar(out=d, in0=d, scalar1=float(math.pi), scalar2=float(-math.pi), op0=ALU.min, op1=ALU.max),
               r=[dst], w=[dst])

    def sincos(self, ang, sn, cs, tmp, tmi, shape):
        self.range_reduce(ang, sn, tmp, tmi, 0.0)
        self.S(lambda e: e.activation(out=self._flat(sn), in_=self._flat(sn), func=AF.Sin), r=[sn], w=[sn])
        self.range_reduce(ang, cs, tmp, tmi, math.pi / 2)
        self.S(lambda e: e.activation(out=self._flat(cs), in_=self._flat(cs), func=AF.Sin), r=[cs], w=[cs])

    def setup(self):
        self.bank = [self.ps(f"bank{i}", [128, 512], F32) for i in range(8)]
        self.consts()

    def layer_alloc(self):
        self.modp = self.sb("modp", [128, 4, 8], F32)

        self.wsT = self.sb("wsT", [128, 4, 128], BF16)
        self.gbs = self.sb("gbs", [128, 4], F32)
        self.glng = self.sb("glng", [128, 256], F32)
        self.glnb = self.sb("glnb", [128, 256], F32)

    def prep_alloc(self):
        self.adaw = [self.sb(f"adaw{i}", [128, 8, 512], F32) for i in range(2)]
        self.adab = [self.sb(f"adab{i}", [128, 512], F32) for i in range(2)]
        self.modc = [self.sb(f"modc{i}", [128, 512], F32) for i in range(2)]
        self.wtmp = self.sb("wtmp", [128, 4, 128], F32)
        ccs = self.sb("ccs", [128, 8], F32)
        self.dma("sp", ccs[:, :], self.ccol[:, :], r=[self.ccol], w=[ccs])
        self.S(lambda e: e.activation(out=ccs[:, :], in_=ccs[:, :], func=AF.Silu), r=[ccs], w=[ccs])
        self.condrep = self.sb("condrep", [128, 8, 128], F32)
        self.V(lambda e: e.tensor_copy(out=self.condrep[:, :, :], in_=ccs[:, :].unsqueeze(2).to_broadcast([128, 8, 128])),
               r=[ccs], w=[self.condrep])


    def load_win(self, l):
        self.win = self.sb("win", [128, 8, PW], BF16)
        for k in range(8):
            self.dma("pool", self.win[:, k, :], self.w_in[l, k * 128:(k + 1) * 128, :], r=[self.w_in], w=[self.win])

    def layer_prep(self, l, part=0):
        self.push()
        self.prep_alloc()
        pb = self.bank[7]
        pt = self.bank[6]
        for n in range(12):
            if (part == 0) != (n // 2 in (0, 1)):
                continue
            aw = self.adaw[n % 2]
            ab = self.adab[n % 2]
            mc = self.modc[n % 2]
            self.dma("sp", aw[:, :, :], self.ada_w[l, :, n * 512:(n + 1) * 512].rearrange("(k p) n -> p k n", p=128),
                     r=[self.ada_w], w=[aw])
            self.dma("sp", ab[:, :], self.ada_b[l, n * 512:(n + 1) * 512].partition_broadcast(128), r=[self.ada_b], w=[ab])
            for k in range(8):
                self.mm(pb[:, :], self.condrep[:, k, :], aw[:, k, :], r=[self.condrep, aw], w=[pb], start=(k == 0), stop=(k == 7))
            which, half = n // 2, n % 2
            if which in (2, 5, 3, 4):
                tgt = self.opg if which in (2, 5) else self.opg2
                gi = {2: 0, 5: 1, 3: 0, 4: 1}[which]
                dst = tgt[:, gi, half * 512:(half + 1) * 512]
                self.V(lambda e, dst=dst: e.tensor_tensor(out=dst, in0=pb[:, :], in1=ab[:, :], op=ALU.add), r=[pb, ab], w=[tgt])
                if which != 3:
                    self.V(lambda e, dst=dst: e.tensor_scalar(out=dst, in0=dst, scalar1=1.0, scalar2=None, op0=ALU.add), r=[tgt], w=[tgt])
            else:
                slot = {0: 0, 1: 1}[which]
                self.V(lambda e: e.tensor_tensor(out=mc[:, :], in0=pb[:, :], in1=ab[:, :], op=ALU.add), r=[pb, ab], w=[mc])
                for b4 in range(4):
                    self.tr(pt[:, b4 * 128:(b4 + 1) * 128], mc[:, b4 * 128:(b4 + 1) * 128], self.identf[:, :], r=[mc, self.identf], w=[pt])
                addc = 1.0 if slot in (1, 3) else 0.0
                for b4 in range(4):
                    self.V(lambda e, b4=b4: e.tensor_scalar(out=self.modp[:, slot, half * 4 + b4:half * 4 + b4 + 1],
                                                            in0=pt[:, b4 * 128:b4 * 128 + 1], scalar1=float(addc), scalar2=None, op0=ALU.add),
                           r=[pt], w=[self.modp])
        if part == 0:
            self.dma("sp", self.wtmp[:, :, :], self.gm_ws[l].rearrange("h t s -> t h s"), r=[self.gm_ws], w=[self.wtmp])
            self.G(lambda e: e.affine_select(out=self.wtmp[:, :, :], in_=self.wtmp[:, :, :], pattern=[[0, 4], [-1, 128]],
                                             compare_op=ALU.is_ge, fill=0.0, base=0, channel_multiplier=1), r=[self.wtmp], w=[self.wtmp])
            for h in range(4):
                self.tr(pt[:, h * 128:(h + 1) * 128], self.wtmp[:, h, :], self.identf[:, :], r=[self.wtmp, self.identf], w=[pt])
            self.V(lambda e: e.tensor_copy(out=self.wsT[:, :, :], in_=pt[:, :].rearrange("p (h t) -> p h t", h=4)), r=[pt], w=[self.wsT])
            self.dma("sp", self.gbs[:, :], self.gm_bsT[l], r=[self.gm_bsT], w=[self.gbs])
            self.dma("sp", self.glng[:, :], self.gm_ln_g[l].partition_broadcast(128), r=[self.gm_ln_g], w=[self.glng])

            self.dma("sp", self.glnb[:, :], self.gm_ln_b[l].partition_broadcast(128), r=[self.gm_ln_b], w=[self.glnb])
        self.pop()

    def ln_stats(self, src, src_ap_fn, n, st, act=False):
        if act:
            return self.ln_stats_act(src, src_ap_fn, n, st)
        nchk = (n + 511) // 512
        w = n // nchk
        for i in range(nchk):
            self.V(lambda e, i=i: e.bn_stats(out=st[:, 8 + i * 6:8 + (i + 1) * 6], in_=src_ap_fn(i * w, (i + 1) * w)), r=[src], w=[st])
        self.V(lambda e: e.bn_aggr(out=st[:, 0:2], in_=st[:, 8:8 + 6 * nchk]), r=[st], w=[st])
        self.V(lambda e: e.tensor_scalar(out=st[:, 2:3], in0=st[:, 1:2], scalar1=float(EPS), scalar2=None, op0=ALU.add), r=[st], w=[st])
        self.G(lambda e: e.tensor_tensor(out=st[:, 3:4], in0=st[:, 2:3], in1=self.mhalf[:, 0:1], op=ALU.pow), r=[st, self.mhalf], w=[st])
        self.V(lambda e: e.tensor_scalar(out=st[:, 4:5], in0=st[:, 0:1], scalar1=-1.0, scalar2=st[:, 3:4], op0=ALU.mult, op1=ALU.mult), r=[st], w=[st])

    def ln_stats_act(self, src, src_ap_fn, n, st):
        nchk = (n + 511) // 512
        w = n // nchk
        for i in range(nchk):
            self.V(lambda e, i=i: e.bn_stats(out=st[:, 8 + i * 6:8 + (i + 1) * 6], in_=src_ap_fn(i * w, (i + 1) * w)), r=[src], w=[st])
        self.V(lambda e: e.bn_aggr(out=st[:, 0:2], in_=st[:, 8:8 + 6 * nchk]), r=[st], w=[st])
        self.S(lambda e: e.activation(out=st[:, 2:3], in_=st[:, 1:2], func=AF.Sqrt, bias=self.eps_t[:, 0:1], scale=1.0), r=[st, self.eps_t], w=[st])
        self.V(lambda e: e.reciprocal(out=st[:, 3:4], in_=st[:, 2:3]), r=[st], w=[st])
        self.V(lambda e: e.tensor_scalar(out=st[:, 4:5], in0=st[:, 0:1], scalar1=-1.0, scalar2=st[:, 3:4], op0=ALU.mult, op1=ALU.mult), r=[st], w=[st])

    def qk_alloc(self):
        self.QT = self.sb("QT", [128, 4, T], BF16)
        self.KT = self.sb("KT", [128, 4, T], BF16)

    def p1_alloc(self):
        s = self.sb
        self.xt = [s(f"xt{i}", [128, D], F32) for i in range(2)]
        self.xn = [s(f"xn{i}", [128, D], BF16) for i in range(2)]
        self.st = [s(f"st{i}", [128, 24], F32) for i in range(2)]
        self.st2 = [s(f"stb{i}", [128, 24], F32) for i in range(2)]
        self.hT = [s(f"hT{i}", [128, 8, 128], BF16) for i in range(2)]
        self.gl = [s(f"gl{i}", [128, 512], F32) for i in range(1)] * 2
        self.vnb = [s(f"vnb{i}", [128, 256], F32) for i in range(1)] * 2
        self.vb = [s(f"vb{i}", [128, 256], BF16) for i in range(1)] * 2
        self.aout = [s(f"aout{i}", [128, 256], BF16) for i in range(1)] * 2
        self.qf = [s(f"qf{i}", [128, 2, 512], F32) for i in range(1)] * 2
        self.qb = [s(f"qb{i}", [128, 2, 512], BF16) for i in range(1)] * 2
        self.rt = [s(f"rt{i}", [128, 4, 8, 8], F32) for i in range(1)] * 2
        self.vp = [s(f"vp{i}", [128, 8, 65], BF16) for i in range(1)] * 2
        for i in range(1):
            self.G(lambda e, i=i: e.memset(self.vp[i][:, :, :], 1.0), w=[self.vp[i]])

    def p1_chunk(self, l, j, x_src):
        i2 = j % 2
        xt, xn, st, st2, hT = self.xt[i2], self.xn[i2], self.st[i2], self.st2[i2], self.hT[i2]
        gl, vnb, vb, aout, qf, qb, rt, vp = self.gl[i2], self.vnb[i2], self.vb[i2], self.aout[i2], self.qf[i2], self.qb[i2], self.rt[i2], self.vp[i2]
        B = self.bank
        rows = slice(j * 128, (j + 1) * 128)
        if j == 0:
            self.dma("sp", xt[:, :], x_src[rows, :], r=[x_src], w=[xt])
        if j + 1 < NCH:
            xtn = self.xt[(j + 1) % 2]
            self.dma("sp", xtn[:, :], x_src[(j + 1) * 128:(j + 2) * 128, :], r=[x_src], w=[xtn])
        self.ln_stats(xt, lambda a, b: xt[:, a:b], D, st)
        self.S(lambda e: e.activation(out=xn[:, :], in_=xt[:, :], func=AF.Identity, bias=st[:, 4:5], scale=st[:, 3:4]), r=[xt, st], w=[xn])
        pT = B[0]
        pTb = pT[:, :].bitcast(BF16).rearrange("p (k t) -> p k t", k=8)
        for k in range(8):
            self.tr(pTb[:, k, :], xn[:, k * 128:(k + 1) * 128], self.identb[:, :], r=[xn, self.identb], w=[pT], signal=(k == 7))
        for k in range(8):
            self.V(lambda e, k=k: e.tensor_scalar(out=hT[:, k, :], in0=pTb[:, k, :], scalar1=self.modp[:, 1, k:k + 1], scalar2=self.modp[:, 0, k:k + 1],
                                                  op0=ALU.mult, op1=ALU.add), r=[pT, self.modp], w=[hT], x=[hT])
        for bi, c0 in ((1, 0), (2, 512), (3, 1024), (4, 1536)):
            for k in range(8):
                self.mm(B[bi][:, :], hT[:, k, :], self.win[:, k, c0:c0 + 512], r=[hT, self.win], w=[B[bi]], start=(k == 0), stop=(k == 7))
        self.S(lambda e: e.activation(out=gl[:, :], in_=B[1][:, :], func=AF.Gelu_apprx_tanh), r=[B[1]], w=[gl])
        self.ln_stats(gl, lambda a, b: gl[:, 256 + a:256 + b], 256, st2)
        self.V(lambda e: e.tensor_scalar(out=vnb[:, :], in0=gl[:, 256:512], scalar1=st2[:, 3:4], scalar2=st2[:, 4:5], op0=ALU.mult, op1=ALU.add),
               r=[gl, st2], w=[vnb])
        self.V(lambda e: e.tensor_tensor(out=vnb[:, :], in0=vnb[:, :], in1=self.glng[:, :], op=ALU.mult), r=[vnb, self.glng], w=[vnb], x=[vnb])
        self.V(lambda e: e.tensor_tensor(out=vb[:, :], in0=vnb[:, :], in1=self.glnb[:, :], op=ALU.add), r=[vnb, self.glnb], w=[vb], x=[vnb])
        psv = B[5]
        for h in range(4):
            self.mm(psv[:, h * 64:(h + 1) * 64], self.wsT[:, h, :], vb[:, h * 64:(h + 1) * 64], r=[self.wsT, vb], w=[psv], start=True, stop=True,
                    signal=(h == 3))
        for h in range(4):
            self.V(lambda e, h=h: e.scalar_tensor_tensor(out=aout[:, h * 64:(h + 1) * 64], in0=psv[:, h * 64:(h + 1) * 64], scalar=self.gbs[:, h:h + 1],
                                                         in1=gl[:, h * 64:(h + 1) * 64], op0=ALU.add, op1=ALU.mult), r=[psv, self.gbs, gl], w=[aout], x=[aout])
        self.dma("sp", self.mixtok[rows, 0:256], aout[:, :], r=[aout], w=[self.mixtok])
        self.S(lambda e: e.copy(out=qf[:, 0, :], in_=B[2][:, :]), r=[B[2]], w=[qf])
        self.S(lambda e: e.copy(out=qf[:, 1, :], in_=B[3][:, :]), r=[B[3]], w=[qf], x=[qf])
        q4 = qf[:, :, :].rearrange("p a (h e) -> p (a h) e", e=64)
        o4 = qb[:, :, :].rearrange("p a (h e) -> p (a h) e", e=64)
        for a in range(2):
            xa1, xa2 = q4[:, a * 8:(a + 1) * 8, 0:8], q4[:, a * 8:(a + 1) * 8, 8:16]
            cb = self.cs[:, j, :].unsqueeze(1).to_broadcast([128, 8, 8])
            sb_ = self.sn[:, j, :].unsqueeze(1).to_broadcast([128, 8, 8])
            oa = o4[:, a * 8:(a + 1) * 8, :]
            self.G(lambda e, xa1=xa1, cb=cb: e.tensor_tensor(out=rt[:, 0, :, :], in0=xa1, in1=cb, op=ALU.mult), r=[qf, self.cs], w=[rt])
            self.G(lambda e, xa2=xa2, sb_=sb_: e.tensor_tensor(out=rt[:, 1, :, :], in0=xa2, in1=sb_, op=ALU.mult), r=[qf, self.sn], w=[rt])
            self.G(lambda e, xa2=xa2, cb=cb: e.tensor_tensor(out=rt[:, 2, :, :], in0=xa2, in1=cb, op=ALU.mult), r=[qf, self.cs], w=[rt])
            self.G(lambda e, xa1=xa1, sb_=sb_: e.tensor_tensor(out=rt[:, 3, :, :], in0=xa1, in1=sb_, op=ALU.mult), r=[qf, self.sn], w=[rt])
            self.G(lambda e, oa=oa: e.tensor_tensor(out=oa[:, :, 0:8], in0=rt[:, 0, :, :], in1=rt[:, 1, :, :], op=ALU.subtract), r=[rt], w=[qb])
            self.G(lambda e, oa=oa: e.tensor_tensor(out=oa[:, :, 8:16], in0=rt[:, 2, :, :], in1=rt[:, 3, :, :], op=ALU.add), r=[rt], w=[qb])
            self.G(lambda e, oa=oa, a=a: e.tensor_copy(out=oa[:, :, 16:64], in_=q4[:, a * 8:(a + 1) * 8, 16:64]), r=[qf], w=[qb])
        pq = B[6]
        pqb = pq[:, :].bitcast(BF16).rearrange("p (a k t) -> p a k t", a=2, k=4)
        for a in range(2):
            for k in range(4):
                self.tr(pqb[:, a, k, :], qb[:, a, k * 128:(k + 1) * 128], self.identb[:, :], r=[qb, self.identb], w=[pq], signal=(a == 1 and k == 3))
        self.S(lambda e: e.copy(out=self.QT[:, :, j * 128:(j + 1) * 128], in_=pqb[:, 0, :, :]), r=[pq], w=[self.QT])
        self.S(lambda e: e.copy(out=self.KT[:, :, j * 128:(j + 1) * 128], in_=pqb[:, 1, :, :]), r=[pq], w=[self.KT])
        self.S(lambda e: e.copy(out=vp[:, :, 0:64], in_=B[4][:, :].rearrange("p (h e) -> p h e", e=64)), r=[B[4]], w=[vp])
        self.dma("sp", self.vd[rows, :], vp[:, :, :].rearrange("p h e -> p (h e)"), r=[vp], w=[self.vd])

    def finish(self):
        bufs = [t.b for t in self.outs]
        self.cx.finish(bufs)

    def p2_alloc(self):
        s = self.sb
        self.vbr = [s(f"vbr{i}", [128, 32, 520], BF16) for i in range(2)]
        self.negm = s("negm", [128, 256], BF16)
        negf = s("negf", [128, 256], F32)
        zf = s("zf", [128, 256], F32)
        self.G(lambda e: e.memset(zf[:, :], 0.0), w=[zf])
        self.G(lambda e: e.affine_select(out=negf[:, 0:128], in_=zf[:, 0:128], pattern=[[-1, 128]], compare_op=ALU.is_ge, fill=-30000.0,
                                         base=0, channel_multiplier=1), r=[zf], w=[negf])
        self.G(lambda e: e.affine_select(out=negf[:, 128:256], in_=zf[:, 128:256], pattern=[[1, 128]], compare_op=ALU.is_ge, fill=-30000.0,
                                         base=0, channel_multiplier=-1), r=[zf], w=[negf])
        self.G(lambda e: e.tensor_copy(out=self.negm[:, :], in_=negf[:, :]), r=[negf], w=[self.negm])
        self.m01 = s("m01", [128, 256], BF16)
        onef = s("onef2", [128, 256], F32)
        self.G(lambda e: e.memset(onef[:, :], 1.0), w=[onef])
        self.G(lambda e: e.affine_select(out=onef[:, 0:128], in_=onef[:, 0:128], pattern=[[-1, 128]], compare_op=ALU.is_ge, fill=0.0,
                                         base=0, channel_multiplier=1), r=[onef], w=[onef])
        self.G(lambda e: e.affine_select(out=onef[:, 128:256], in_=onef[:, 128:256], pattern=[[1, 128]], compare_op=ALU.is_ge, fill=0.0,
                                         base=0, channel_multiplier=-1), r=[onef], w=[onef])
        self.G(lambda e: e.tensor_copy(out=self.m01[:, :], in_=onef[:, :]), r=[onef], w=[self.m01])
        self.pexp = [s(f"pexp{i}", [128, 256], BF16) for i in range(6)]
        self.osb = [s(f"osb{i}", [128, 520], F32) for i in range(2)]

    def p2_attention(self):
        B = self.bank
        hb = 0
        self._hb = 0
        dils = (1, 4, 16)

        def load_vb(bi):
            d = dils[bi]
            vb_ = self.vbr[bi % 2]
            src = self.vd[:, :].rearrange("(n l r) c -> l n r c", l=128, r=d)
            for n in range(T // (128 * d)):
                self.dma("sp", vb_[:, n * d:(n + 1) * d, :], src[:, n, :, :], r=[self.vd], w=[vb_])
        load_vb(0)
        load_vb(1)
        for bi, d in enumerate(dils):
            seg = 128 * d
            nseg = T // seg
            vb = self.vbr[bi % 2]
            if bi == 1:
                load_vb(2)
            odst = self.obr[bi][:, :].rearrange("(n l r) c -> l n r c", l=128, r=d)
            blk = 0
            for n in range(nseg):
                for r_ in range(d):
                    cols = slice(n * seg + r_, (n + 1) * seg, d)
                    pcols = slice((n - 1) * seg + r_, n * seg, d)
                    bcur = n * d + r_
                    bprev = (n - 1) * d + r_
                    po = [B[6], B[7]]
                    osb = self.osb[blk % 2]
                    def scores(h):
                        nonlocal hb
                        hp, p0 = h // 2, (h % 2) * 64
                        ps = B[hb % 6]
                        o0 = 0
                        pe_ = self.pexp[hb % 6]
                        hb += 1
                        c0 = 0 if n > 0 else 128
                        if n > 0:
                            self.mm(ps[:, o0:o0 + 128], self.KT[p0:p0 + 64, hp, pcols], self.QT[p0:p0 + 64, hp, cols], r=[self.KT, self.QT], w=[ps],
                                    start=True, stop=True, signal=False)
                        self.mm(ps[:, o0 + 128:o0 + 256], self.KT[p0:p0 + 64, hp, cols], self.QT[p0:p0 + 64, hp, cols], r=[self.KT, self.QT], w=[ps],
                                start=True, stop=True)
                        self.S(lambda e, pe_=pe_, ps=ps, c0=c0, o0=o0: e.activation(out=pe_[:, c0:256], in_=ps[:, o0 + c0:o0 + 256], func=AF.Exp, scale=0.125),
                               r=[ps], w=[pe_])
                        mk = self.V if (h % 2 == 0) else self.G
                        mk(lambda e, pe_=pe_, c0=c0: e.tensor_tensor(out=pe_[:, c0:256], in0=pe_[:, c0:256], in1=self.m01[:, c0:256], op=ALU.mult),
                           r=[pe_, self.m01], w=[pe_])
                        return pe_

                    def pv(h, pe_):
                        pob = po[h // 4]
                        oc = slice((h % 4) * 65, (h % 4) * 65 + 65)
                        if n > 0:
                            self.mm(pob[:, oc], pe_[:, 0:128], vb[:, bprev, h * 65:(h + 1) * 65], r=[pe_, vb], w=[pob], start=True, stop=False)
                        self.mm(pob[:, oc], pe_[:, 128:256], vb[:, bcur, h * 65:(h + 1) * 65], r=[pe_, vb], w=[pob], start=(n == 0), stop=True)
                    pend = []
                    for h in range(8):
                        pend.append((h, scores(h)))
                        if len(pend) > 4:
                            pv(*pend.pop(0))
                    while pend:
                        pv(*pend.pop(0))
                    self.V(lambda e, osb=osb, po=po: e.tensor_copy(out=osb[:, 0:260], in_=po[0][:, 0:260]), r=[po[0]], w=[osb])
                    self.V(lambda e, osb=osb, po=po: e.tensor_copy(out=osb[:, 260:520], in_=po[1][:, 0:260]), r=[po[1]], w=[osb])
                    self.dma("sp", odst[:, n, r_, :], osb[:, :], r=[osb], w=[self.obr[bi]])
                    blk += 1

    def s5_io(self):
        i = self.inp
        self.lam_re = i("lam_re", [2, 1024])
        self.lam_im = i("lam_im", [2, 1024])
        self.log_dt = i("log_dt", [2, 16])
        self.ssm_bT = i("ssm_bT", [2, 2, 128, 2, 64])
        self.ssm_cT = i("ssm_cT", [2, 16, 128, 16])
        self.ssm_dT = i("ssm_dT", [2, 128, 2])
        self.glu_w = i("glu_w", [2, 256, 256])
        self.glu_bT = i("glu_bT", [2, 128, 2])
        self.coutT = self.scratch("coutT", [256, T], BF16)
        self.obr = [self.scratch(f"obr{i}", [T, 520], F32) for i in range(3)]

    def s5_alloc(self):
        s = self.sb
        self.Bblk = s("Bblk", [128, 2, 8, 2, 64], BF16)
        self.Cblk = s("Cblk", [128, 16, 128], BF16)
        self.Pre = s("Pre", [128, 16, 64], F32)
        self.PsT = s("PsT", [128, 16, 2, 64], F32)
        self.Qre = s("Qre", [128, 16, 64], F32)
        self.QsT = s("QsT", [128, 16, 2, 64], F32)
        self.glubh = s("glubh", [128, 2], F32)
        self.TriT = s("TriT", [128, 128], BF16)
        self.ones1 = s("ones1", [1, 128], BF16)
        self.dTt = s("dTt", [128, 2], F32)
        self.gluw = s("gluw", [128, 2, 256], BF16)
        self.glub = s("glub", [128, 2], F32)

    def s5_prep(self, l):
        s = self.sb
        V, S, G = self.V, self.S, self.G
        self.push()
        lre = s("lre", [128, 16, 64], F32)
        lim = s("lim", [128, 16, 64], F32)
        ldt = s("ldt", [128, 16], F32)
        self.dma("sp", lre[:, :, :].rearrange("p g n -> p (g n)"), self.lam_re[l].partition_broadcast(128), r=[self.lam_re], w=[lre])
        self.dma("sp", lim[:, :, :].rearrange("p g n -> p (g n)"), self.lam_im[l].partition_broadcast(128), r=[self.lam_im], w=[lim])
        self.dma("sp", ldt[:, :], self.log_dt[l].partition_broadcast(128), r=[self.log_dt], w=[ldt])
        S(lambda e: e.activation(out=ldt[:, :], in_=ldt[:, :], func=AF.Exp), r=[ldt], w=[ldt])
        dtb = ldt[:, :].unsqueeze(2).to_broadcast([128, 16, 64])
        lrd = s("lrd", [128, 16, 64], F32)
        lid = s("lid", [128, 16, 64], F32)
        V(lambda e: e.tensor_tensor(out=lrd[:, :, :], in0=lre[:, :, :], in1=dtb, op=ALU.mult), r=[lre, ldt], w=[lrd])
        V(lambda e: e.tensor_tensor(out=lid[:, :, :], in0=lim[:, :, :], in1=dtb, op=ALU.mult), r=[lim, ldt], w=[lid])
        sp1 = s("sp1", [128, 1], F32)
        G(lambda e: e.iota(sp1[:, :], pattern=[[0, 1]], base=1, channel_multiplier=1, allow_small_or_imprecise_dtypes=True), w=[sp1])
        E = s("E", [128, 16, 64], F32)
        An = s("An", [128, 16, 64], F32)
        sA = s("sA", [128, 16, 64], F32)
        cA = s("cA", [128, 16, 64], F32)
        tmp = s("s5tmp", [128, 16, 64], F32)
        tmi = s("s5tmi", [128, 16, 64], I32)
        qm = s("qm", [128, 16, 64], F32)
        pm = s("pm", [128, 16, 64], F32)
        V(lambda e: e.tensor_scalar(out=E[:, :, :], in0=lrd[:, :, :], scalar1=sp1[:, 0:1], scalar2=None, op0=ALU.mult), r=[lrd, sp1], w=[E])
        V(lambda e: e.tensor_scalar(out=An[:, :, :], in0=lid[:, :, :], scalar1=sp1[:, 0:1], scalar2=None, op0=ALU.mult), r=[lid, sp1], w=[An])
        self.sincos(An, sA, cA, tmp, tmi, None)
        S(lambda e: e.activation(out=qm[:, :, :], in_=E[:, :, :], func=AF.Exp), r=[E], w=[qm])
        S(lambda e: e.activation(out=pm[:, :, :], in_=E[:, :, :], func=AF.Exp, scale=-1.0), r=[E], w=[pm])
        V(lambda e: e.tensor_tensor(out=self.Qre[:, :, :], in0=qm[:, :, :], in1=cA[:, :, :], op=ALU.mult), r=[qm, cA], w=[self.Qre])
        V(lambda e: e.tensor_tensor(out=self.QsT[:, :, 1, :], in0=qm[:, :, :], in1=sA[:, :, :], op=ALU.mult), r=[qm, sA], w=[self.QsT])
        V(lambda e: e.tensor_scalar(out=self.QsT[:, :, 0, :], in0=self.QsT[:, :, 1, :], scalar1=-1.0, scalar2=None, op0=ALU.mult), r=[self.QsT], w=[self.QsT])
        V(lambda e: e.tensor_tensor(out=self.Pre[:, :, :], in0=pm[:, :, :], in1=cA[:, :, :], op=ALU.mult), r=[pm, cA], w=[self.Pre])
        V(lambda e: e.tensor_tensor(out=self.PsT[:, :, 0, :], in0=pm[:, :, :], in1=sA[:, :, :], op=ALU.mult), r=[pm, sA], w=[self.PsT])
        V(lambda e: e.tensor_scalar(out=self.PsT[:, :, 1, :], in0=self.PsT[:, :, 0, :], scalar1=-1.0, scalar2=None, op0=ALU.mult), r=[self.PsT], w=[self.PsT])
        self.sincos(lid, sA, cA, tmp, tmi, None)
        S(lambda e: e.activation(out=qm[:, :, :], in_=lrd[:, :, :], func=AF.Exp), r=[lrd], w=[qm])
        nr, ni = E, An
        V(lambda e: e.tensor_tensor(out=nr[:, :, :], in0=qm[:, :, :], in1=cA[:, :, :], op=ALU.mult), r=[qm, cA], w=[nr])
        V(lambda e: e.tensor_scalar(out=nr[:, :, :], in0=nr[:, :, :], scalar1=-1.0, scalar2=None, op0=ALU.add), r=[nr], w=[nr])
        V(lambda e: e.tensor_tensor(out=ni[:, :, :], in0=qm[:, :, :], in1=sA[:, :, :], op=ALU.mult), r=[qm, sA], w=[ni])
        m2 = pm
        V(lambda e: e.tensor_tensor(out=m2[:, :, :], in0=lre[:, :, :], in1=lre[:, :, :], op=ALU.mult), r=[lre], w=[m2])
        V(lambda e: e.tensor_tensor(out=tmp[:, :, :], in0=lim[:, :, :], in1=lim[:, :, :], op=ALU.mult), r=[lim], w=[tmp])
        V(lambda e: e.tensor_tensor(out=m2[:, :, :], in0=m2[:, :, :], in1=tmp[:, :, :], op=ALU.add), r=[m2, tmp], w=[m2])
        V(lambda e: e.reciprocal(out=m2[:, :, :], in_=m2[:, :, :]), r=[m2], w=[m2])
        fre, fim = sA, cA
        t1 = s("ft1", [128, 16, 64], F32)
        t2 = s("ft2", [128, 16, 64], F32)
        V(lambda e: e.tensor_tensor(out=t1[:, :, :], in0=nr[:, :, :], in1=lre[:, :, :], op=ALU.mult), r=[nr, lre], w=[t1])
        V(lambda e: e.tensor_tensor(out=t2[:, :, :], in0=ni[:, :, :], in1=lim[:, :, :], op=ALU.mult), r=[ni, lim], w=[t2])
        V(lambda e: e.tensor_tensor(out=t1[:, :, :], in0=t1[:, :, :], in1=t2[:, :, :], op=ALU.add), r=[t1, t2], w=[t1])
        V(lambda e: e.tensor_tensor(out=fre[:, :, :], in0=t1[:, :, :], in1=m2[:, :, :], op=ALU.mult), r=[t1, m2], w=[fre])
        V(lambda e: e.tensor_tensor(out=t1[:, :, :], in0=ni[:, :, :], in1=lre[:, :, :], op=ALU.mult), r=[ni, lre], w=[t1])
        V(lambda e: e.tensor_tensor(out=t2[:, :, :], in0=nr[:, :, :], in1=lim[:, :, :], op=ALU.mult), r=[nr, lim], w=[t2])
        V(lambda e: e.tensor_tensor(out=t1[:, :, :], in0=t1[:, :, :], in1=t2[:, :, :], op=ALU.subtract), r=[t1, t2], w=[t1])
        V(lambda e: e.tensor_tensor(out=fim[:, :, :], in0=t1[:, :, :], in1=m2[:, :, :], op=ALU.mult), r=[t1, m2], w=[fim])
        bT = s("bTt", [128, 2, 2, 64], F32)
        self.dma("sp", bT[:, :, :, :], self.ssm_bT[l].rearrange("r p k n -> p r k n"), r=[self.ssm_bT], w=[bT])
        bm = s("bmask", [128, 8], F32)
        one8 = s("one8", [128, 8], F32)
        G(lambda e: e.memset(one8[:, :], 1.0), w=[one8])
        G(lambda e: e.affine_select(out=bm[:, :], in_=one8[:, :], pattern=[[-16, 8]], compare_op=ALU.is_ge, fill=0.0, base=0, channel_multiplier=1),
          r=[one8], w=[bm])
        G(lambda e: e.affine_select(out=bm[:, :], in_=bm[:, :], pattern=[[16, 8]], compare_op=ALU.is_ge, fill=0.0, base=15, channel_multiplier=-1),
          r=[bm], w=[bm])
        bmb = bm[:, :].unsqueeze(2).to_broadcast([128, 8, 64])
        for kc in range(2):
            fr = fre[:, kc * 8:(kc + 1) * 8, :]
            fi = fim[:, kc * 8:(kc + 1) * 8, :]
            bre = bT[:, 0, kc, :].unsqueeze(1).to_broadcast([128, 8, 64])
            bim = bT[:, 1, kc, :].unsqueeze(1).to_broadcast([128, 8, 64])
            a1, a2 = t1[:, 0:8, :], t2[:, 0:8, :]
            V(lambda e, fr=fr, bre=bre: e.tensor_tensor(out=a1, in0=fr, in1=bre, op=ALU.mult), r=[fre, bT], w=[t1])
            V(lambda e, fi=fi, bim=bim: e.tensor_tensor(out=a2, in0=fi, in1=bim, op=ALU.mult), r=[fim, bT], w=[t2])
            V(lambda e: e.tensor_tensor(out=a1, in0=a1, in1=a2, op=ALU.subtract), r=[t1, t2], w=[t1])
            V(lambda e, kc=kc: e.tensor_tensor(out=self.Bblk[:, kc, :, 0, :], in0=a1, in1=bmb, op=ALU.mult), r=[t1, bm], w=[self.Bblk])
            V(lambda e, fr=fr, bim=bim: e.tensor_tensor(out=a1, in0=fr, in1=bim, op=ALU.mult), r=[fre, bT], w=[t1])
            V(lambda e, fi=fi, bre=bre: e.tensor_tensor(out=a2, in0=fi, in1=bre, op=ALU.mult), r=[fim, bT], w=[t2])
            V(lambda e: e.tensor_tensor(out=a1, in0=a1, in1=a2, op=ALU.add), r=[t1, t2], w=[t1])
            V(lambda e, kc=kc: e.tensor_tensor(out=self.Bblk[:, kc, :, 1, :], in0=a1, in1=bmb, op=ALU.mult), r=[t1, bm], w=[self.Bblk])
        cTs = s("cTs", [128, 16, 16], F32)
        self.dma("sp", cTs[:, :, :], self.ssm_cT[l].rearrange("g p c -> p g c"), r=[self.ssm_cT], w=[cTs])
        sg = s("sgn", [128, 1], F32)
        G(lambda e: e.memset(sg[0:64, :], 1.0), w=[sg])
        G(lambda e: e.memset(sg[64:128, :], -1.0), w=[sg])
        G(lambda e: e.memset(self.Cblk[:, :, :], 0.0), w=[self.Cblk])
        for g in range(16):
            c0 = (g % 8) * 16
            S(lambda e, g=g, c0=c0: e.activation(out=self.Cblk[:, g, c0:c0 + 16], in_=cTs[:, g, :], func=AF.Identity, scale=sg[:, 0:1]),
              r=[cTs, sg, self.Cblk], w=[self.Cblk])
        onesb = s("onesb", [128, 128], F32)
        G(lambda e: e.memset(onesb[:, :], 1.0), w=[onesb])
        G(lambda e: e.affine_select(out=onesb[:, :], in_=onesb[:, :], pattern=[[1, 128]], compare_op=ALU.is_ge, fill=0.0, base=0, channel_multiplier=-1),
          r=[onesb], w=[onesb])
        G(lambda e: e.tensor_copy(out=self.TriT[:, :], in_=onesb[:, :]), r=[onesb], w=[self.TriT])
        G(lambda e: e.memset(self.ones1[:, :], 1.0), w=[self.ones1])
        self.dma("sp", self.dTt[:, :], self.ssm_dT[l], r=[self.ssm_dT], w=[self.dTt])
        self.dma("sp", self.glub[:, :], self.glu_bT[l], r=[self.glu_bT], w=[self.glub])
        V(lambda e: e.tensor_scalar(out=self.glubh[:, :], in0=self.glub[:, :], scalar1=0.5, scalar2=None, op0=ALU.mult), r=[self.glub], w=[self.glubh])
        self.dma("pool", self.gluw[:, :, :], self.glu_w[l].rearrange("(k p) n -> p k n", p=128), r=[self.glu_w], w=[self.gluw])
        self.pop()

    def s5_chunk(self, l, j):
        B = self.bank
        V, S, G = self.V, self.S, self.G
        hT = self.hT[j % 2]
        uT = self.uT[j % 2]
        co = self.co[j % 2]
        ps_s = B[7]
        for cc in range(2):
            for k in range(8):
                self.mm(ps_s[:, cc * 128:(cc + 1) * 128], self.win[:, k, 2048 + cc * 128:2048 + (cc + 1) * 128], hT[:, k, :], r=[self.win, hT], w=[ps_s],
                        start=(k == 0), stop=(k == 7), signal=(k == 7 and cc == 1))
        S(lambda e: e.copy(out=uT[:, :, :], in_=ps_s[:, 0:256].rearrange("p (c t) -> p c t", c=2)), r=[ps_s], w=[uT])
        yps = B[7]
        for h in range(2):
            bu = [B[1], B[2]]
            zz = [B[3], B[4]]
            g0 = h * 8
            for q in range(2):
                self.mm(bu[q][:, :], uT[:, h, :], self.Bblk[:, h, q * 4:(q + 1) * 4, :, :].rearrange("p g r n -> p (g r n)"), r=[uT, self.Bblk], w=[bu[q]],
                        start=True, stop=True)
            t1, t2, vv = self.s5t1, self.s5t2, self.s5v
            for q in range(2):
                gs = slice(g0 + q * 4, g0 + q * 4 + 4)
                bu4 = bu[q][:, :].rearrange("p (g r n) -> p g r n", g=4, r=2)
                pc = self.Pre[:, gs, :].unsqueeze(2).to_broadcast([128, 4, 2, 64])
                V(lambda e, q=q, bu4=bu4, pc=pc: e.tensor_tensor(out=t1[:, q * 4:(q + 1) * 4, :, :], in0=bu4, in1=pc, op=ALU.mult), r=[bu[q], self.Pre], w=[t1], x=[t1])
                V(lambda e, q=q, bu4=bu4, gs=gs: e.tensor_tensor(out=t2[:, q * 4:(q + 1) * 4, :, :], in0=bu4[:, :, ::-1, :], in1=self.PsT[:, gs, :, :], op=ALU.mult),
                  r=[bu[q], self.PsT], w=[t2], x=[t2])
            G(lambda e: e.tensor_tensor(out=vv[:, :], in0=t1[:, :, :, :].rearrange("p g r n -> p (g r n)"), in1=t2[:, :, :, :].rearrange("p g r n -> p (g r n)"), op=ALU.add),
              r=[t1, t2], w=[vv])
            for q in range(2):
                self.mm(zz[q][:, :], self.TriT[:, :], vv[:, q * 512:(q + 1) * 512], r=[self.TriT, vv], w=[zz[q]], start=True, stop=False)
                self.mm(zz[q][:, :], self.ones1[:, :], self.x0row[h][:, q * 512:(q + 1) * 512], r=[self.ones1, self.x0row[h]], w=[zz[q]], start=False, stop=True)
            xs = self.s5x[h]
            for q in range(2):
                gs = slice(g0 + q * 4, g0 + q * 4 + 4)
                z4 = zz[q][:, :].rearrange("p (g r n) -> p g r n", g=4, r=2)
                qc = self.Qre[:, gs, :].unsqueeze(2).to_broadcast([128, 4, 2, 64])
                V(lambda e, q=q, z4=z4, qc=qc: e.tensor_tensor(out=t1[:, q * 4:(q + 1) * 4, :, :], in0=z4, in1=qc, op=ALU.mult), r=[zz[q], self.Qre], w=[t1], x=[t1])
                V(lambda e, q=q, z4=z4, gs=gs: e.tensor_tensor(out=t2[:, q * 4:(q + 1) * 4, :, :], in0=z4[:, :, ::-1, :], in1=self.QsT[:, gs, :, :], op=ALU.mult),
                  r=[zz[q], self.QsT], w=[t2], x=[t2])
            G(lambda e, xs=xs: e.tensor_tensor(out=xs[:, :, :].rearrange("p g m -> p (g m)"), in0=t1[:, :, :, :].rearrange("p g r n -> p (g r n)"),
                                        in1=t2[:, :, :, :].rearrange("p g r n -> p (g r n)"), op=ALU.add), r=[t1, t2], w=[xs])
            self.dma("act", self.x0row[h][0:1, :], xs[127:128, :, :].rearrange("p g m -> p (g m)"), r=[xs], w=[self.x0row[h]])
            pxt = B[0]
            pxb = pxt[:, :].bitcast(BF16).rearrange("p (g t) -> p g t", g=8)
            for g in range(8):
                self.tr(pxb[:, g, :], xs[:, g, :], self.identb[:, :], r=[xs, self.identb], w=[pxt], signal=(g == 7))
            V(lambda e, pxb=pxb: e.tensor_copy(out=self.s5xT[:, :, :], in_=pxb), r=[pxt], w=[self.s5xT])
            for g in range(8):
                self.mm(yps[:, 256 + h * 128:256 + (h + 1) * 128], self.Cblk[:, g0 + g, :], self.s5xT[:, g, :], r=[self.Cblk, self.s5xT], w=[yps],
                        start=(g == 0), stop=(g == 7))
        for cc in range(2):
            V(lambda e, cc=cc: e.scalar_tensor_tensor(out=self.yf[:, cc, :], in0=uT[:, cc, :], scalar=self.dTt[:, cc:cc + 1], in1=yps[:, 256 + cc * 128:256 + (cc + 1) * 128],
                                                      op0=ALU.mult, op1=ALU.add), r=[uT, self.dTt, yps], w=[self.yf], x=[self.yf])
        S(lambda e: e.activation(out=self.yg[:, :, :], in_=self.yf[:, :, :], func=AF.Gelu_apprx_tanh), r=[self.yf], w=[self.yg])
        gps = B[5]
        for c2 in range(2):
            for cc in range(2):
                self.mm(gps[:, c2 * 128:(c2 + 1) * 128], self.gluw[:, cc, c2 * 128:(c2 + 1) * 128], self.yg[:, cc, :], r=[self.gluw, self.yg], w=[gps],
                        start=(cc == 0), stop=(cc == 1))
        for c2 in range(2):
            S(lambda e, c2=c2: e.activation(out=self.sgm[:, c2, :], in_=gps[:, c2 * 128:(c2 + 1) * 128], func=AF.Sigmoid, bias=self.glub[:, c2:c2 + 1], scale=1.0),
              r=[gps, self.glub], w=[self.sgm])
        V(lambda e: e.tensor_tensor(out=co[:, :, :], in0=self.yg[:, :, :], in1=self.sgm[:, :, :], op=ALU.mult), r=[self.yg, self.sgm], w=[co])
        self.dma("sp", self.coutT[:, j * 128:(j + 1) * 128].rearrange("(c p) t -> p c t", p=128), co[:, :, :], r=[co], w=[self.coutT])

    def half(self, bank, lo):
        t = Tl(bank.h, bank.b.name + ("lo" if lo else "hi"))
        return t

    def p1v2_alloc(self):
        s = self.sb
        self.xt = [s(f"xt{i}", [128, D], F32) for i in range(2)]
        self.xn = [s(f"xn{i}", [128, D], BF16) for i in range(2)]
        self.st = [s(f"st{i}", [128, 24], F32) for i in range(2)]
        self.st2 = [s(f"stb{i}", [128, 24], F32) for i in range(2)]
        self.hT = [s(f"hT{i}", [128, 8, 128], BF16) for i in range(2)]
        self.gl = [s(f"gl{i}", [128, 512], F32) for i in range(2)]
        self.qbd = [s(f"qbd{i}", [128, 2, 512], BF16) for i in range(2)]
        self.vp = [s(f"vp{i}", [128, 8, 65], BF16) for i in range(2)]
        for i in range(2):
            self.G(lambda e, i=i: e.memset(self.vp[i][:, :, :], 1.0), w=[self.vp[i]])
        self.uT = [s(f"uT{i}", [128, 2, 128], BF16) for i in range(2)]
        self.vnb = s("vnb", [128, 256], F32)
        self.vb = s("vb", [128, 256], BF16)
        self.aout = s("aout", [128, 256], BF16)
        self.qb = s("qb", [128, 2, 512], BF16)
        self.rt = s("rt", [128, 4, 8, 8], F32)
        self.uD = [s(f"uD{i}", [128, 2, 128], F32) for i in range(3)]
        self.p1t = [s(f"p1t{i}", [128, 4, 2, 64], BF16) for i in range(2)]
        self.p2t = [s(f"p2t{i}", [128, 4, 2, 64], BF16) for i in range(2)]
        self.q1 = [s(f"q1_{i}", [128, 4, 2, 64], F32) for i in range(2)]
        self.q2 = [s(f"q2_{i}", [128, 4, 2, 64], F32) for i in range(2)]
        self.qv = [s(f"qv{i}", [128, 512], BF16) for i in range(2)]
        self.qx = [s(f"qx{i}", [128, 4, 128], BF16) for i in range(2)]
        self.qxT = [s(f"qxT{i}", [128, 4, 128], BF16) for i in range(3)]
        self.x0q = [s(f"x0q{i}", [1, 512], BF16) for i in range(4)]
        for i in range(4):
            self.G(lambda e, i=i: e.memset(self.x0q[i][:, :], 0.0), w=[self.x0q[i]])
        self.yf = s("yf2", [128, 2, 128], F32)
        self.yg = s("yg2", [128, 2, 128], BF16)
        self.sgm = s("sgm2", [128, 2, 128], F32)
        self.co = [s(f"co2_{i}", [128, 2, 128], BF16) for i in range(2)]
        B = self.bank
        self.b5lo, self.b5hi = Tl(B[5].h, "b5lo"), Tl(B[5].h, "b5hi")
        self.b6lo, self.b6hi = Tl(B[6].h, "b6lo"), Tl(B[6].h, "b6hi")
        self.b7lo, self.b7hi = Tl(B[7].h, "b7lo"), Tl(B[7].h, "b7hi")

    def p1_front_ln(self, l, j, x_src):
        if j >= NCH:
            return
        i2 = j % 2
        xt, xn, st = self.xt[i2], self.xn[i2], self.st[i2]
        S = self.S
        if j == 0:
            self.dma("sp", xt[:, :], x_src[0:128, :], r=[x_src], w=[xt])
            xt1 = self.xt[1]
            self.dma("sp", xt1[:, :], x_src[128:256, :], r=[x_src], w=[xt1])
        self.ln_stats(xt, lambda a, b: xt[:, a:b], D, st)
        S(lambda e: e.activation(out=xn[:, :], in_=xt[:, :], func=AF.Identity, bias=st[:, 4:5], scale=st[:, 3:4]), r=[xt, st], w=[xn])
        if j + 2 < NCH:
            self.dma("sp", xt[:, :], x_src[(j + 2) * 128:(j + 3) * 128, :], r=[x_src], w=[xt])

    def p1_front_a(self, l, j, x_src):
        if j >= NCH:
            return
        i2 = j % 2
        xn, hT = self.xn[i2], self.hT[i2]
        B = self.bank
        S = self.S
        pT = B[0]
        pTb = pT[:, :].bitcast(BF16).rearrange("p (k t) -> p k t", k=8)
        for k in range(8):
            self.tr(pTb[:, k, :], xn[:, k * 128:(k + 1) * 128], self.identb[:, :], r=[xn, self.identb], w=[pT], signal=(k == 7))
        for k in range(8):
            S(lambda e, k=k: e.activation(out=hT[:, k, :], in_=pTb[:, k, :], func=AF.Identity, scale=self.modp[:, 1, k:k + 1], bias=self.modp[:, 0, k:k + 1]),
              r=[pT, self.modp], w=[hT], x=[hT])

    def p1_front_s(self, l, j):
        if j >= NCH:
            return
        i2 = j % 2
        hT, uT = self.hT[i2], self.uT[i2]
        S = self.S
        ps_s = self.b6lo
        for cc in range(2):
            for k in range(8):
                self.mm(ps_s[:, cc * 128:(cc + 1) * 128], self.win[:, k, 2048 + cc * 128:2048 + (cc + 1) * 128], hT[:, k, :], r=[self.win, hT], w=[ps_s],
                        start=(k == 0), stop=(k == 7), signal=(k == 7 and cc == 1))
        S(lambda e: e.copy(out=uT[:, :, :], in_=ps_s[:, 0:256].rearrange("p (c t) -> p c t", c=2)), r=[ps_s], w=[uT])
        uD = self.uD[j % 3]
        for cc in range(2):
            S(lambda e, cc=cc: e.activation(out=uD[:, cc, :], in_=ps_s[:, cc * 128:(cc + 1) * 128], func=AF.Identity, scale=self.dTt[:, cc:cc + 1]), r=[ps_s, self.dTt], w=[uD], x=[uD])

    def p1_front_p(self, l, j, which):
        if j >= NCH:
            return
        i2 = j % 2
        hT, gl, qf, vp = self.hT[i2], self.gl[i2], self.qbd[i2], self.vp[i2]
        B = self.bank
        S = self.S

        def proj(bank, c0):
            for k in range(8):
                self.mm(bank[:, :], hT[:, k, :], self.win[:, k, c0:c0 + 512], r=[hT, self.win], w=[bank], start=(k == 0), stop=(k == 7))
        if which == 0:
            proj(B[1], 0)
            S(lambda e: e.activation(out=gl[:, :], in_=B[1][:, :], func=AF.Gelu_apprx_tanh), r=[B[1]], w=[gl])
        elif which == 1:
            proj(B[2], 512)
            S(lambda e: e.copy(out=qf[:, 0, :], in_=B[2][:, :]), r=[B[2]], w=[qf])
        elif which == 2:
            proj(B[1], 1024)
            S(lambda e: e.copy(out=qf[:, 1, :], in_=B[1][:, :]), r=[B[1]], w=[qf], x=[qf])
        else:
            proj(B[2], 1536)
            S(lambda e: e.copy(out=vp[:, :, 0:64], in_=B[2][:, :].rearrange("p (h e) -> p h e", e=64)), r=[B[2]], w=[vp])

    def p1_front(self, l, j, x_src):
        self.p1_front_a(l, j, x_src)
        self.p1_front_s(l, j)
        for w_ in range(4):
            self.p1_front_p(l, j, w_)

    def p1_back(self, l, j):
        if j < 0:
            return
        i2 = j % 2
        st2 = self.st2[i2]
        gl, qf, vp, uT = self.gl[i2], self.qbd[i2], self.vp[i2], self.uT[i2]
        vnb, vb, aout, qb, rt = self.vnb, self.vb, self.aout, self.qbd[i2], self.rt
        B = self.bank
        V, S, G = self.V, self.S, self.G
        rows = slice(j * 128, (j + 1) * 128)
        self.ln_stats(gl, lambda a, b: gl[:, 256 + a:256 + b], 256, st2)
        V(lambda e: e.tensor_scalar(out=vnb[:, :], in0=gl[:, 256:512], scalar1=st2[:, 3:4], scalar2=st2[:, 4:5], op0=ALU.mult, op1=ALU.add),
          r=[gl, st2], w=[vnb])
        V(lambda e: e.tensor_tensor(out=vnb[:, :], in0=vnb[:, :], in1=self.glng[:, :], op=ALU.mult), r=[vnb, self.glng], w=[vnb], x=[vnb])
        V(lambda e: e.tensor_tensor(out=vb[:, :], in0=vnb[:, :], in1=self.glnb[:, :], op=ALU.add), r=[vnb, self.glnb], w=[vb], x=[vnb])
        psv = self.b5lo
        for h in range(4):
            self.mm(psv[:, h * 64:(h + 1) * 64], self.wsT[:, h, :], vb[:, h * 64:(h + 1) * 64], r=[self.wsT, vb], w=[psv], start=True, stop=True,
                    signal=(h == 3))
        for h in range(4):
            V(lambda e, h=h: e.scalar_tensor_tensor(out=aout[:, h * 64:(h + 1) * 64], in0=psv[:, h * 64:(h + 1) * 64], scalar=self.gbs[:, h:h + 1],
                                                    in1=gl[:, h * 64:(h + 1) * 64], op0=ALU.add, op1=ALU.mult), r=[psv, self.gbs, gl], w=[aout], x=[aout])
        self.dma("sp", self.mixtok[rows, 0:256], aout[:, :], r=[aout], w=[self.mixtok])
        q4 = qf[:, :, :].rearrange("p a (h e) -> p (a h) e", e=64)
        o4 = qb[:, :, :].rearrange("p a (h e) -> p (a h) e", e=64)
        for a in range(2):
            xa1, xa2 = q4[:, a * 8:(a + 1) * 8, 0:8], q4[:, a * 8:(a + 1) * 8, 8:16]
            cb = self.cs[:, j, :].unsqueeze(1).to_broadcast([128, 8, 8])
            sb_ = self.sn[:, j, :].unsqueeze(1).to_broadcast([128, 8, 8])
            oa = o4[:, a * 8:(a + 1) * 8, :]
            G(lambda e, xa1=xa1, cb=cb: e.tensor_tensor(out=rt[:, 0, :, :], in0=xa1, in1=cb, op=ALU.mult), r=[qf, self.cs], w=[rt])
            G(lambda e, xa2=xa2, sb_=sb_: e.tensor_tensor(out=rt[:, 1, :, :], in0=xa2, in1=sb_, op=ALU.mult), r=[qf, self.sn], w=[rt])
            G(lambda e, xa2=xa2, cb=cb: e.tensor_tensor(out=rt[:, 2, :, :], in0=xa2, in1=cb, op=ALU.mult), r=[qf, self.cs], w=[rt])
            G(lambda e, xa1=xa1, sb_=sb_: e.tensor_tensor(out=rt[:, 3, :, :], in0=xa1, in1=sb_, op=ALU.mult), r=[qf, self.sn], w=[rt])
            G(lambda e, oa=oa: e.tensor_tensor(out=oa[:, :, 0:8], in0=rt[:, 0, :, :], in1=rt[:, 1, :, :], op=ALU.subtract), r=[rt], w=[qb])
            G(lambda e, oa=oa: e.tensor_tensor(out=oa[:, :, 8:16], in0=rt[:, 2, :, :], in1=rt[:, 3, :, :], op=ALU.add), r=[rt], w=[qb])
        pq = B[7]
        pqb = pq[:, :].bitcast(BF16).rearrange("p (a k t) -> p a k t", a=2, k=4)
        for a in range(2):
            for k in range(4):
                self.tr(pqb[:, a, k, :], qb[:, a, k * 128:(k + 1) * 128], self.identb[:, :], r=[qb, self.identb], w=[pq], signal=(a == 1 and k == 3))
        S(lambda e: e.copy(out=self.QT[:, :, j * 128:(j + 1) * 128], in_=pqb[:, 0, :, :]), r=[pq], w=[self.QT])
        S(lambda e: e.copy(out=self.KT[:, :, j * 128:(j + 1) * 128], in_=pqb[:, 1, :, :]), r=[pq], w=[self.KT])
        self.dma("sp", self.vd[rows, :], vp[:, :, :].rearrange("p h e -> p (h e)"), r=[vp], w=[self.vd])

    def s5A(self, i):
        if i < 0 or i >= 4 * NCH:
            return
        j, q = divmod(i, 4)
        kc = q // 2
        gs = slice(q * 4, q * 4 + 4)
        uT = self.uT[j % 2]
        bu = self.bank[3]
        t1, t2 = self.p1t[i % 2], self.p2t[i % 2]
        self.mm(bu[:, :], uT[:, kc, :], self.Bblk[:, kc, (q % 2) * 4:(q % 2) * 4 + 4, :, :].rearrange("p g r n -> p (g r n)"), r=[uT, self.Bblk], w=[bu],
                start=True, stop=True)
        bu4 = bu[:, :].rearrange("p (g r n) -> p g r n", g=4, r=2)
        pc = self.Pre[:, gs, :].unsqueeze(2).to_broadcast([128, 4, 2, 64])
        self.V(lambda e: e.tensor_tensor(out=t1[:, :, :, :], in0=bu4, in1=pc, op=ALU.mult), r=[bu, self.Pre], w=[t1])
        self.V(lambda e: e.tensor_tensor(out=t2[:, :, :, :], in0=bu4[:, :, ::-1, :], in1=self.PsT[:, gs, :, :], op=ALU.mult), r=[bu, self.PsT], w=[t2])

    def s5B(self, i):
        if i < 0 or i >= 4 * NCH:
            return
        j, q = divmod(i, 4)
        gs = slice(q * 4, q * 4 + 4)
        zz = self.bank[4]
        t1, t2 = self.p1t[i % 2], self.p2t[i % 2]
        u1, u2, xs = self.q1[i % 2], self.q2[i % 2], self.qx[i % 2]
        f = lambda t: t[:, :, :, :].rearrange("p g r n -> p (g r n)")
        self.mm(zz[:, :], self.TriT[:, :], f(t1), r=[self.TriT, t1], w=[zz], start=True, stop=False)
        self.mm(zz[:, :], self.TriT[:, :], f(t2), r=[self.TriT, t2], w=[zz], start=False, stop=False)
        self.mm(zz[:, :], self.ones1[:, :], self.x0q[q][:, :], r=[self.ones1, self.x0q[q]], w=[zz], start=False, stop=True)
        z4 = zz[:, :].rearrange("p (g r n) -> p g r n", g=4, r=2)
        qc = self.Qre[:, gs, :].unsqueeze(2).to_broadcast([128, 4, 2, 64])
        self.V(lambda e: e.tensor_tensor(out=u1[:, :, :, :], in0=z4, in1=qc, op=ALU.mult), r=[zz, self.Qre], w=[u1])
        self.V(lambda e: e.tensor_tensor(out=u2[:, :, :, :], in0=z4[:, :, ::-1, :], in1=self.QsT[:, gs, :, :], op=ALU.mult), r=[zz, self.QsT], w=[u2])
        self.G(lambda e: e.tensor_tensor(out=xs[:, :, :].rearrange("p g m -> p (g m)"), in0=f(u1), in1=f(u2), op=ALU.add), r=[u1, u2], w=[xs])
        self.dma("pool", self.x0q[q][0:1, :], xs[127:128, :, :].rearrange("p g m -> p (g m)"), r=[xs], w=[self.x0q[q]])

    def s5C(self, i):
        if i < 0 or i >= 4 * NCH:
            return
        xs, xT = self.qx[i % 2], self.qxT[i % 3]
        pxt = self.b6hi
        pxb = pxt[:, 256:512].bitcast(BF16).rearrange("p (g t) -> p g t", g=4)
        for g in range(4):
            self.tr(pxb[:, g, :], xs[:, g, :], self.identb[:, :], r=[xs, self.identb], w=[pxt], signal=(g == 3))
        self.S(lambda e: e.copy(out=xT[:, :, :], in_=pxb), r=[pxt], w=[xT])

    def s5D(self, i):
        if i < 0 or i >= 4 * NCH:
            return
        j, q = divmod(i, 4)
        kc = q // 2
        xT = self.qxT[i % 3]
        yps = self.b5hi
        for g in range(4):
            self.mm(yps[:, 256 + kc * 128:256 + (kc + 1) * 128], self.Cblk[:, q * 4 + g, :], xT[:, g, :], r=[self.Cblk, xT], w=[yps],
                    start=(q % 2 == 0 and g == 0), stop=(q % 2 == 1 and g == 3))

    def s5_step(self, i):
        self.s5A(i + 2)
        self.s5B(i + 1)
        self.s5C(i)
        if i % 2 == 0:
            self.s5D(i - 2)
            self.s5D(i - 1)

    def s5_tail(self, j):
        if j < 0 or j >= NCH:
            return
        V, S, G = self.V, self.S, self.G
        i2 = j % 2
        uT = self.uT[i2]
        yps = self.b5hi
        co = self.co[i2]
        for cc in range(2):
            V(lambda e, cc=cc: e.scalar_tensor_tensor(out=self.yf[:, cc, :], in0=self.uD[j % 3][:, cc, :], scalar=1.0, in1=yps[:, 256 + cc * 128:256 + (cc + 1) * 128],
                                                      op0=ALU.mult, op1=ALU.add), r=[self.uD[j % 3], yps], w=[self.yf], x=[self.yf])
        S(lambda e: e.activation(out=self.yg[:, :, :], in_=self.yf[:, :, :], func=AF.Gelu_apprx_tanh), r=[self.yf], w=[self.yg])
        gps = self.b5lo
        for c2 in range(2):
            for cc in range(2):
                self.mm(gps[:, c2 * 128:(c2 + 1) * 128], self.gluw[:, cc, c2 * 128:(c2 + 1) * 128], self.yg[:, cc, :], r=[self.gluw, self.yg], w=[gps],
                        start=(cc == 0), stop=(cc == 1))
        for c2 in range(2):
            S(lambda e, c2=c2: e.activation(out=self.sgm[:, c2, :], in_=gps[:, c2 * 128:(c2 + 1) * 128], func=AF.Tanh, bias=self.glubh[:, c2:c2 + 1], scale=0.5),
              r=[gps, self.glubh], w=[self.sgm])
        V(lambda e: e.scalar_tensor_tensor(out=self.sgm[:, :, :], in0=self.sgm[:, :, :], scalar=1.0, in1=self.yg[:, :, :], op0=ALU.add, op1=ALU.mult),
          r=[self.sgm, self.yg], w=[self.sgm])
        V(lambda e: e.tensor_scalar(out=co[:, :, :], in0=self.sgm[:, :, :], scalar1=0.5, scalar2=None, op0=ALU.mult), r=[self.sgm], w=[co])
        self.dma("sp", self.coutT[:, j * 128:(j + 1) * 128].rearrange("(c p) t -> p c t", p=128), co[:, :, :], r=[co], w=[self.coutT])

    def p1_all(self, l, x_src):
        self.p1_front_ln(l, 0, x_src)
        self.p1_front_ln(l, 1, x_src)
        self.p1_front(l, 0, x_src)
        self.s5A(0); self.s5A(1); self.s5B(0)
        for j in range(NCH):
            self.p1_front_ln(l, j + 2, x_src)
            self.p1_front(l, j + 1, x_src)
            self.p1_back(l, j)
            for q in range(4):
                self.s5_step(4 * j + q)
                if q == 0:
                    self.s5_tail(j - 1)
        self.s5_step(4 * NCH)
        self.s5_tail(NCH - 1)
        assert (4 * NCH) % 2 == 0

    CAP = 896
    NSLOT = 16 * 896 + 128
    GROUPS = ((0, 4), (512, 3))

    def p3_io(self):
        i = self.inp
        self.w_out = i("w_out", [2, D, D])
        self.ln1_g = i("ln1_g", [2, D]); self.ln1_b = i("ln1_b", [2, D])
        self.ln2_g = i("ln2_g", [2, D]); self.ln2_b = i("ln2_b", [2, D])
        self.router_w = i("router_w", [D, 16])
        self.router_bias = i("router_bias", [16])
        self.x1d = self.scratch("x1d", [T, D], F32)
        self.xmid = self.scratch("xmid", [T, D], F32)
        self.h2slots = self.scratch("h2slots", [self.NSLOT, D], BF16)
        self.oslots = self.scratch("oslots", [self.NSLOT, D], F32)

    def route_alloc(self):
        s = self.sb
        self.slotA = s("slotA", [128, NCH], I32)
        self.slotB = s("slotB", [128, NCH], I32)
        self.gAB = s("gAB", [128, 2, NCH], F32)

    def p3_alloc(self, l):
        s = self.sb
        self.wout = s("wout", [128, 8, D], BF16)
        for k in range(8):
            self.dma("pool", self.wout[:, k, :], self.w_out[l, k * 128:(k + 1) * 128, :], r=[self.w_out], w=[self.wout])
        self.lng = s("lng", [128, D], F32); self.lnb = s("lnb", [128, D], F32)
        self.dma("sp", self.lng[:, :], self.ln1_g[l].partition_broadcast(128), r=[self.ln1_g], w=[self.lng])
        self.dma("sp", self.lnb[:, :], self.ln1_b[l].partition_broadcast(128), r=[self.ln1_b], w=[self.lnb])
        self.rw = s("rw", [128, 8, 16], F32)
        self.dma("sp", self.rw[:, :, :], self.router_w[:, :].rearrange("(k p) e -> p k e", p=128), r=[self.router_w], w=[self.rw])
        self.rbias = s("rbias", [128, 16], F32)
        self.dma("sp", self.rbias[:, :], self.router_bias[:].partition_broadcast(128), r=[self.router_bias], w=[self.rbias])
        self.o3 = [s(f"o3_{i}", [128, 3, 520], F32) for i in range(2)]
        self.rec = s("rec", [128, 8], F32)
        self.mt = [s(f"mt{i}", [128, D], BF16) for i in range(2)]
        self.mixT = [s(f"mixT{i}", [128, 8, 128], BF16) for i in range(2)]
        self.xres = [s(f"xres{i}", [128, D], F32) for i in range(2)]
        self.yy = s("yy", [128, D], F32)
        self.x1t = [s(f"x1t{i}", [128, D], F32) for i in range(3)]
        self.cT = [s(f"cT{i}", [128, 2, 128], BF16) for i in range(2)]
        self.st3b = s("st3b", [128, 24], F32)
        self.h2f = s("h2f", [128, D], F32)
        self.h2f2 = [self.h2f, s("h2fb", [128, D], F32)]
        self.h2all = s("h2all", [128, NCH, D], BF16)
        self.h2c = [Tl(self.h2all.h, f"h2c{j}") for j in range(NCH)]
        self.scall = s("scall", [128, NCH, 16], F32)
        self.h2T = s("h2T", [128, 8, 128], F32)
        self.st3 = s("st3", [128, 24], F32)
        self.trs = s("trs", [128, 128], F32)
        self.eoff = s("eoff", [128, 16], F32)
        self.trashp = s("trashp", [128, 1], F32)
        self.ones16 = s("ones16", [128, 16], F32)
        G = self.G
        G(lambda e: e.memset(self.ones16[:, :], 1.0), w=[self.ones16])
        G(lambda e: e.affine_select(out=self.trs[:, :], in_=self.onesf[:, :], pattern=[[1, 128]], compare_op=ALU.is_ge, fill=0.0, base=-1, channel_multiplier=-1),
          r=[self.onesf], w=[self.trs])
        G(lambda e: e.iota(self.eoff[:, :], pattern=[[self.CAP, 16]], base=0, channel_multiplier=0, allow_small_or_imprecise_dtypes=True), w=[self.eoff])
        G(lambda e: e.iota(self.trashp[:, :], pattern=[[0, 1]], base=16 * self.CAP, channel_multiplier=1, allow_small_or_imprecise_dtypes=True), w=[self.trashp])

    def p3_L1(self, k):
        if k >= NCH:
            return
        rws = slice(k * 128, (k + 1) * 128)
        o3_, mt_ = self.o3[k % 2], self.mt[k % 2]
        for bi in range(3):
            self.dma("sp", o3_[:, bi, :], self.obr[bi][rws, :], r=[self.obr[bi]], w=[o3_])
        self.dma("sp", mt_[:, 0:256], self.mixtok[rws, 0:256], r=[self.mixtok], w=[mt_])

    def p3_L2(self, k, x_src):
        if k >= NCH:
            return
        rws = slice(k * 128, (k + 1) * 128)
        self.dma("sp", self.cT[k % 2][:, :, :], self.coutT[:, rws].rearrange("(c p) t -> p c t", p=128), r=[self.coutT], w=[self.cT[k % 2]])
        self.dma("sp", self.xres[k % 2][:, :], x_src[rws, :], r=[x_src], w=[self.xres[k % 2]])

    def p3_S1(self, j):
        if j >= NCH or j < 0:
            return
        B = self.bank
        V, S, G = self.V, self.S, self.G
        o3, rec, mt = self.o3[j % 2], self.rec, self.mt[j % 2]
        mixT = self.mixT[j % 2]
        V(lambda e: e.tensor_tensor(out=o3[:, 0, :], in0=o3[:, 0, :], in1=o3[:, 1, :], op=ALU.add), r=[o3], w=[o3], x=[o3])
        V(lambda e: e.tensor_tensor(out=o3[:, 0, :], in0=o3[:, 0, :], in1=o3[:, 2, :], op=ALU.add), r=[o3], w=[o3], x=[o3])
        o8 = o3[:, 0, :].rearrange("p (h e) -> p h e", e=65)
        V(lambda e: e.reciprocal(out=rec[:, :], in_=o8[:, :, 64]), r=[o3], w=[rec])
        V(lambda e: e.tensor_tensor(out=mt[:, 256:768].rearrange("p (h e) -> p h e", e=64), in0=o8[:, :, 0:64], in1=rec[:, :].unsqueeze(2).to_broadcast([128, 8, 64]),
                                    op=ALU.mult), r=[o3, rec], w=[mt])
        pT = B[0]
        pTb = pT[:, :].bitcast(BF16).rearrange("p (k t) -> p k t", k=8)
        for k in range(6):
            self.tr(pTb[:, k, :], mt[:, k * 128:(k + 1) * 128], self.identb[:, :], r=[mt, self.identb], w=[pT], signal=(k == 5))
        S(lambda e: e.copy(out=mixT[:, 0:6, :], in_=pTb[:, 0:6, :]), r=[pT], w=[mixT])

    def p3_S2(self, j):
        if j >= NCH or j < 0:
            return
        B = self.bank
        V, S, G = self.V, self.S, self.G
        rows = slice(j * 128, (j + 1) * 128)
        mixT, cT, xres = self.mixT[j % 2], self.cT[j % 2], self.xres[j % 2]
        wb = [B[1], B[2]] if j % 2 == 0 else [B[6], B[7]]
        for nb in range(2):
            for k in range(8):
                lhs = mixT[:, k, :] if k < 6 else cT[:, k - 6, :]
                self.mm(wb[nb][:, :], lhs, self.wout[:, k, nb * 512:(nb + 1) * 512], r=[mixT, cT, self.wout], w=[wb[nb]], start=(k == 0), stop=(k == 7))
        yy = self.yy
        for nb in range(2):
            cs_ = slice(nb * 512, (nb + 1) * 512)
            V(lambda e, nb=nb, cs_=cs_: e.tensor_tensor(out=yy[:, cs_], in0=wb[nb][:, :], in1=self.opg[:, 0, cs_], op=ALU.mult), r=[wb[nb], self.opg], w=[yy], x=[yy])
        V(lambda e: e.scalar_tensor_tensor(out=yy[:, :], in0=xres[:, :], scalar=float(ALPHA), in1=yy[:, :], op0=ALU.mult, op1=ALU.add), r=[xres, yy], w=[yy], x=[yy])
        self.ln_stats(yy, lambda a, b: yy[:, a:b], D, self.st3, act=True)
        x1 = self.x1t[j % 3]
        S(lambda e: e.activation(out=x1[:, :], in_=yy[:, :], func=AF.Identity, bias=self.st3[:, 4:5], scale=self.st3[:, 3:4]), r=[yy, self.st3], w=[x1])

    def p3_S2b(self, j):
        if j >= NCH or j < 0:
            return
        V = self.V
        rows = slice(j * 128, (j + 1) * 128)
        x1 = self.x1t[j % 3]
        V(lambda e: e.tensor_tensor(out=x1[:, :], in0=x1[:, :], in1=self.lng[:, :], op=ALU.mult), r=[x1, self.lng], w=[x1])
        V(lambda e: e.tensor_tensor(out=x1[:, :], in0=x1[:, :], in1=self.lnb[:, :], op=ALU.add), r=[x1, self.lnb], w=[x1], x=[x1])
        self.dma("sp", self.x1d[rows, :], x1[:, :], r=[x1], w=[self.x1d])

    def p3_S3a(self, j):
        if j >= NCH or j < 0:
            return
        S = self.S
        x1 = self.x1t[j % 3]
        h2f = self.h2f2[j % 2]
        self.ln_stats(x1, lambda a, b: x1[:, a:b], D, self.st3b, act=True)
        S(lambda e: e.activation(out=h2f[:, :], in_=x1[:, :], func=AF.Identity, bias=self.st3b[:, 4:5], scale=self.st3b[:, 3:4]), r=[x1, self.st3b], w=[h2f])

    def p3_S3b(self, j):
        if j >= NCH or j < 0:
            return
        B = self.bank
        V, S, G = self.V, self.S, self.G
        h2f = self.h2f2[j % 2]
        V(lambda e: e.tensor_tensor(out=h2f[:, :], in0=h2f[:, :], in1=self.opg2[:, 1, :], op=ALU.mult), r=[h2f, self.opg2], w=[h2f])
        V(lambda e: e.tensor_tensor(out=h2f[:, :], in0=h2f[:, :], in1=self.opg2[:, 0, :], op=ALU.add), r=[h2f, self.opg2], w=[h2f], x=[h2f])
        S(lambda e: e.copy(out=self.h2all[:, j, :], in_=h2f[:, :]), r=[h2f], w=[self.h2c[j]])
        for half in range(2):
            pt = B[3 + half]
            for k in range(4):
                self.tr(pt[:, k * 128:(k + 1) * 128], h2f[:, (half * 4 + k) * 128:(half * 4 + k + 1) * 128], self.identf[:, :], r=[h2f, self.identf], w=[pt], signal=(k == 3))
            S(lambda e, half=half, pt=pt: e.copy(out=self.h2T[:, half * 4:(half + 1) * 4, :], in_=pt[:, :].rearrange("p (k t) -> p k t", k=4)), r=[pt], w=[self.h2T])
        lg = B[5]
        for k in range(8):
            self.mm(lg[:, 0:16], self.h2T[:, k, :], self.rw[:, k, :], r=[self.h2T, self.rw], w=[lg], start=(k == 0), stop=(k == 7))
        S(lambda e: e.copy(out=self.scall[:, j, :], in_=lg[:, 0:16]), r=[lg], w=[self.scall])
        if (j + 1) % self.RSEG == 0:
            self.p3_route(j + 1 - self.RSEG)

    def p3_all(self, l, x_src):
        self.p3_L1(0)
        for j in range(-2, NCH + 2):
            self.p3_L1(j + 3)
            self.p3_L2(j + 2, x_src)
            self.p3_S1(j + 2)
            self.p3_S2(j + 1)
            self.p3_S2b(j)
            self.p3_S3a(j - 1)
            self.p3_S3b(j - 2)

    RSEG = 8

    def p3_route_alloc(self):
        s = self.sb
        NJ = self.RSEG
        mk = lambda n: s(n, [128, NJ, 16], F32)
        t = {}
        t["big"] = [mk(f"r_{i}") for i in range(14)]
        t["g4"] = [s(f"r4_{i}", [128, NJ, 4], F32) for i in range(4)]
        t["g1"] = [s(f"r1_{i}", [128, NJ], F32) for i in range(3)]
        t["mask_e0"] = mk("mask_e0")
        t["mask_j0"] = s("mask_j0", [128, 16, NJ], F32)
        G = self.G
        G(lambda e: e.memset(t["mask_e0"][:, :, :], 1.0), w=[t["mask_e0"]])
        G(lambda e: e.memset(t["mask_e0"][:, :, 0:1], 0.0), w=[t["mask_e0"]])
        G(lambda e: e.memset(t["mask_j0"][:, :, :], 1.0), w=[t["mask_j0"]])
        G(lambda e: e.memset(t["mask_j0"][:, :, 0:1], 0.0), w=[t["mask_j0"]])
        self.carry = s("carry", [128, 16], F32)
        G(lambda e: e.memset(self.carry[:, :], 0.0), w=[self.carry])
        self._rt = t

    def p3_route(self, j0):
        B = self.bank
        V, S, G = self.V, self.S, self.G
        NJ = self.RSEG
        t = self._rt
        sel, eq, msk, top2, chosen, gw, tmp, cum, pos, valid, slotv, baseT, totT, sc = t["big"]
        m1, m2, gs, gsel = t["g4"]
        gmax, gsum, t32 = t["g1"]
        mask_e0, mask_j0 = t["mask_e0"], t["mask_j0"]
        f2 = lambda t_: t_[:, :, :].rearrange("p j e -> p (j e)")
        S(lambda e: e.activation(out=f2(sc), in_=self.scall[:, j0:j0 + NJ, :].rearrange("p j e -> p (j e)"), func=AF.Sigmoid), r=[self.scall], w=[sc])
        g4 = lambda t: t[:, :, :].rearrange("p j (g i) -> p (j g) i", i=4)
        b4 = lambda t: t[:, :, :].rearrange("p j g -> p (j g)").unsqueeze(2).to_broadcast([128, NJ * 4, 4])
        V(lambda e: e.tensor_tensor(out=sel[:, :, :], in0=sc[:, :, :], in1=self.rbias[:, :].unsqueeze(1).to_broadcast([128, NJ, 16]), op=ALU.add), r=[sc, self.rbias], w=[sel])
        V(lambda e: e.tensor_reduce(out=m1[:, :, :].rearrange("p j g -> p (j g)"), in_=g4(sel), axis=AX.X, op=ALU.max), r=[sel], w=[m1])
        V(lambda e: e.tensor_tensor(out=g4(eq), in0=g4(sel), in1=b4(m1), op=ALU.is_equal), r=[sel, m1], w=[eq])
        V(lambda e: e.scalar_tensor_tensor(out=f2(msk), in0=f2(eq), scalar=-1e9, in1=f2(sel), op0=ALU.mult, op1=ALU.add), r=[eq, sel], w=[msk])
        V(lambda e: e.tensor_reduce(out=m2[:, :, :].rearrange("p j g -> p (j g)"), in_=g4(msk), axis=AX.X, op=ALU.max), r=[msk], w=[m2])
        V(lambda e: e.tensor_tensor(out=gs[:, :, :], in0=m1[:, :, :], in1=m2[:, :, :], op=ALU.add), r=[m1, m2], w=[gs])
        V(lambda e: e.tensor_reduce(out=gmax[:, :], in_=gs[:, :, :], axis=AX.X, op=ALU.max), r=[gs], w=[gmax])
        V(lambda e: e.tensor_tensor(out=gsel[:, :, :], in0=gs[:, :, :], in1=gmax[:, :].unsqueeze(2).to_broadcast([128, NJ, 4]), op=ALU.is_equal), r=[gs, gmax], w=[gsel])
        V(lambda e: e.tensor_tensor(out=g4(top2), in0=g4(sel), in1=b4(m2), op=ALU.is_ge), r=[sel, m2], w=[top2])
        V(lambda e: e.tensor_tensor(out=g4(chosen), in0=g4(top2), in1=b4(gsel), op=ALU.mult), r=[top2, gsel], w=[chosen])
        V(lambda e: e.tensor_tensor(out=gw[:, :, :], in0=chosen[:, :, :], in1=sc[:, :, :], op=ALU.mult), r=[chosen, sc], w=[gw])
        V(lambda e: e.tensor_reduce(out=gsum[:, :], in_=gw[:, :, :], axis=AX.X, op=ALU.add), r=[gw], w=[gsum])
        V(lambda e: e.reciprocal(out=gsum[:, :], in_=gsum[:, :]), r=[gsum], w=[gsum])
        V(lambda e: e.tensor_tensor(out=gw[:, :, :], in0=gw[:, :, :], in1=gsum[:, :].unsqueeze(2).to_broadcast([128, NJ, 16]), op=ALU.mult), r=[gw, gsum], w=[gw])
        cbk = B[5]
        W = NJ * 16
        self.mm(cbk[:, 128:128 + W], self.trs[:, :], f2(chosen), r=[self.trs, chosen], w=[cbk], start=True, stop=True)
        self.mm(cbk[:, 256:256 + W], self.onesf[:, :], f2(chosen), r=[self.onesf, chosen], w=[cbk], start=True, stop=True)
        V(lambda e: e.tensor_copy(out=f2(totT).rearrange("p (e j) -> p e j", e=16),
                                  in_=cbk[:, 256:256 + W].rearrange("p (j e) -> p e j", e=16)), r=[cbk], w=[totT])
        V(lambda e: e.tensor_tensor_scan(out=f2(baseT), data0=mask_j0[:, :, :].rearrange("p e j -> p (e j)"), data1=f2(totT), initial=0.0, op0=ALU.mult, op1=ALU.add),
          r=[mask_j0, totT], w=[baseT])
        V(lambda e: e.tensor_tensor(out=f2(baseT), in0=f2(baseT), in1=f2(totT), op=ALU.subtract), r=[baseT, totT], w=[baseT])
        bT3 = f2(baseT).rearrange("p (e j) -> p e j", e=16)
        tT3 = f2(totT).rearrange("p (e j) -> p e j", e=16)
        V(lambda e: e.tensor_tensor(out=bT3, in0=bT3, in1=self.carry[:, :].unsqueeze(2).to_broadcast([128, 16, NJ]), op=ALU.add), r=[baseT, self.carry], w=[baseT])
        V(lambda e: e.tensor_tensor(out=self.carry[:, :], in0=bT3[:, :, NJ - 1], in1=tT3[:, :, NJ - 1], op=ALU.add), r=[baseT, totT], w=[self.carry])
        V(lambda e: e.tensor_tensor(out=pos[:, :, :], in0=cbk[:, 128:128 + W].rearrange("p (j e) -> p j e", e=16),
                                    in1=f2(baseT).rearrange("p (e j) -> p j e", e=16), op=ALU.add), r=[cbk, baseT], w=[pos])
        V(lambda e: e.tensor_scalar(out=f2(valid), in0=f2(pos), scalar1=float(self.CAP), scalar2=None, op0=ALU.is_lt), r=[pos], w=[valid])
        V(lambda e: e.tensor_tensor(out=slotv[:, :, :], in0=pos[:, :, :], in1=self.eoff[:, :].unsqueeze(1).to_broadcast([128, NJ, 16]), op=ALU.add), r=[pos, self.eoff], w=[slotv])
        V(lambda e: e.tensor_scalar(out=f2(slotv), in0=f2(slotv), scalar1=self.trashp[:, 0:1], scalar2=None, op0=ALU.subtract), r=[slotv, self.trashp], w=[slotv])
        V(lambda e: e.tensor_tensor(out=f2(slotv), in0=f2(slotv), in1=f2(valid), op=ALU.mult), r=[slotv, valid], w=[slotv])
        V(lambda e: e.tensor_scalar(out=f2(slotv), in0=f2(slotv), scalar1=self.trashp[:, 0:1], scalar2=None, op0=ALU.add), r=[slotv, self.trashp], w=[slotv])
        V(lambda e: e.tensor_tensor(out=f2(gw), in0=f2(gw), in1=f2(valid), op=ALU.mult), r=[gw, valid], w=[gw])
        V(lambda e: e.tensor_tensor_scan(out=f2(cum), data0=f2(mask_e0), data1=f2(chosen), initial=0.0, op0=ALU.mult, op1=ALU.add), r=[mask_e0, chosen], w=[cum])
        for which, dsti in ((1.0, self.slotA), (2.0, self.slotB)):
            wi = int(which) - 1
            V(lambda e, which=which: e.tensor_scalar(out=f2(tmp), in0=f2(cum), scalar1=float(which), scalar2=None, op0=ALU.is_equal), r=[cum], w=[tmp])
            V(lambda e: e.tensor_tensor(out=f2(tmp), in0=f2(tmp), in1=f2(chosen), op=ALU.mult), r=[tmp, chosen], w=[tmp])
            V(lambda e: e.tensor_tensor(out=f2(eq), in0=f2(tmp), in1=f2(gw), op=ALU.mult), r=[tmp, gw], w=[eq])
            V(lambda e, wi=wi: e.tensor_reduce(out=self.gAB[:, wi, j0:j0 + NJ], in_=eq[:, :, :], axis=AX.X, op=ALU.add), r=[eq], w=[self.gAB])
            V(lambda e: e.tensor_tensor(out=f2(tmp), in0=f2(tmp), in1=f2(slotv), op=ALU.mult), r=[tmp, slotv], w=[tmp])
            V(lambda e: e.tensor_reduce(out=t32[:, :], in_=tmp[:, :, :], axis=AX.X, op=ALU.add), r=[tmp], w=[t32])
            V(lambda e: e.tensor_scalar(out=t32[:, :], in0=t32[:, :], scalar1=0.0, scalar2=float(self.NSLOT - 1), op0=ALU.max, op1=ALU.min), r=[t32], w=[t32])
            V(lambda e, dsti=dsti: e.tensor_copy(out=dsti[:, j0:j0 + NJ], in_=t32[:, :]), r=[t32], w=[dsti])
        for j in range(j0, j0 + NJ):
            for dsti in (self.slotA, self.slotB):
                self.cx.dma("pool", None, None, reads=[self.h2c[j].b, dsti.b], writes=[],
                            fn=lambda e, dsti=dsti, j=j: e.indirect_dma_start(out=self.h2slots[:, :], out_offset=bass.IndirectOffsetOnAxis(ap=dsti[:, j:j + 1], axis=0),
                                                                              in_=self.h2all[:, j, :], in_offset=None))

    def p4_io(self):
        i = self.inp
        self.w_gate = i("exp_w_gate", [2, 16, D, 512])
        self.w_up = i("exp_w_up", [2, 16, D, 512])
        self.w_down = i("exp_w_down", [2, 16, 512, D])

    def zero_slots(self):
        self.push()
        zt = self.sb("zt", [128, D], F32)
        self.G(lambda e: e.memset(zt[:, :], 0.0), w=[zt])
        self.dma("sp", self.oslots[16 * self.CAP:16 * self.CAP + 128, :], zt[:, :], r=[zt], w=[self.oslots])
        self.pop()

    def p4_experts(self, l):
        B = self.bank
        V, S, G = self.V, self.S, self.G
        s = self.sb
        wg = [s(f"wg{i}", [128, 8, 512], BF16) for i in range(2)]
        wu = [s(f"wu{i}", [128, 8, 512], BF16) for i in range(2)]
        wd = [s(f"wd{i}", [128, 4, D], BF16) for i in range(2)]
        rt = [s(f"rtok{i}", [128, 4, D], BF16) for i in range(2)]
        rTd = [s(f"rT{i}", [128, 8, 512], BF16) for i in range(2)]
        sil = [s(f"sil{i}", [128, 512], BF16) for i in range(2)]
        hidT = s("hidT", [128, 4, 512], BF16)
        osb = [s(f"eosb{i}", [128, D], F32) for i in range(2)]

        def load_w(e):
            i = e % 2
            self.dma("pool", wg[i][:, :, :], self.w_gate[l, e].rearrange("(k p) f -> p k f", p=128), r=[self.w_gate], w=[wg[i]])
            self.dma("pool", wu[i][:, :, :], self.w_up[l, e].rearrange("(k p) f -> p k f", p=128), r=[self.w_up], w=[wu[i]])
            self.dma("pool", wd[i][:, :, :], self.w_down[l, e].rearrange("(k p) f -> p k f", p=128), r=[self.w_down], w=[wd[i]])

        glist = [(e, off, nb) for e in range(16) for (off, nb) in self.GROUPS]
        load_w(0)
        ob = 0

        def load_rows(gi):
            e, off, nb = glist[gi]
            r0 = e * self.CAP + off
            rtk = rt[gi % 2]
            self.dma("sp", rtk[:, 0:nb, :], self.h2slots[r0:r0 + nb * 128, :].rearrange("(b p) d -> p b d", p=128), r=[self.h2slots], w=[rtk])
        load_rows(0)

        def transposes(gi):
            e, off, nb = glist[gi]
            rtk, rT = rt[gi % 2], rTd[gi % 2]
            for blk in range(nb):
                pT = B[blk % 2]
                pTb = pT[:, :].bitcast(BF16).rearrange("p (k t) -> p k t", k=8)
                for k in range(8):
                    self.tr(pTb[:, k, :], rtk[:, blk, k * 128:(k + 1) * 128], self.identb[:, :], r=[rtk, self.identb], w=[pT], signal=(k == 7))
                S(lambda e_, blk=blk, pTb=pTb, rT=rT: e_.copy(out=rT[:, :, blk * 128:(blk + 1) * 128], in_=pTb), r=[pT], w=[rT], x=[rT])
        if len(glist) > 1:
            load_rows(1)
        transposes(0)
        for gi, (e, off, nb) in enumerate(glist):
            if off == 0 and e + 1 < 16:
                load_w(e + 1)
            i = e % 2
            r0 = e * self.CAP + off
            N = nb * 128
            rT = rTd[gi % 2]
            for fc in range(4):
                pg, pu = B[2 + 2 * (fc % 2)], B[3 + 2 * (fc % 2)]
                for k in range(8):
                    self.mm(pg[:, 0:N], wg[i][:, k, fc * 128:(fc + 1) * 128], rT[:, k, 0:N], r=[wg[i], rT], w=[pg], start=(k == 0), stop=(k == 7))
                for k in range(8):
                    self.mm(pu[:, 0:N], wu[i][:, k, fc * 128:(fc + 1) * 128], rT[:, k, 0:N], r=[wu[i], rT], w=[pu], start=(k == 0), stop=(k == 7))
                sl = sil[fc % 2]
                S(lambda e_, sl=sl, pg=pg, N=N: e_.activation(out=sl[:, 0:N], in_=pg[:, 0:N], func=AF.Silu), r=[pg], w=[sl])
                V(lambda e_, sl=sl, pu=pu, fc=fc, N=N: e_.tensor_tensor(out=hidT[:, fc, 0:N], in0=pu[:, 0:N], in1=sl[:, 0:N], op=ALU.mult), r=[pu, sl], w=[hidT])
            if gi + 1 < len(glist):
                transposes(gi + 1)
            if gi + 2 < len(glist):
                load_rows(gi + 2)
            for blk in range(nb):
                o = osb[ob % 2]
                ob += 1
                for half in range(2):
                    pd = B[6 + half]
                    for fc in range(4):
                        self.mm(pd[:, :], hidT[:, fc, blk * 128:(blk + 1) * 128], wd[i][:, fc, half * 512:(half + 1) * 512], r=[hidT, wd[i]], w=[pd],
                                start=(fc == 0), stop=(fc == 3))
                    V(lambda e_, o=o, pd=pd, half=half: e_.tensor_tensor(out=o[:, half * 512:(half + 1) * 512], in0=pd[:, :],
                                                                         in1=self.opg[:, 1, half * 512:(half + 1) * 512], op=ALU.mult), r=[pd, self.opg], w=[o], x=[o])
                self.dma("sp", self.oslots[r0 + blk * 128:r0 + (blk + 1) * 128, :], o[:, :], r=[o], w=[])

    def p5_alloc(self, l):
        s = self.sb
        self.lng2 = s("lng2", [128, D], F32); self.lnb2 = s("lnb2", [128, D], F32)
        self.dma("sp", self.lng2[:, :], self.ln2_g[l].partition_broadcast(128), r=[self.ln2_g], w=[self.lng2])
        self.dma("sp", self.lnb2[:, :], self.ln2_b[l].partition_broadcast(128), r=[self.ln2_b], w=[self.lnb2])
        self.rA = [s(f"rA{i}", [128, D], F32) for i in range(2)]
        self.rB = [s(f"rB{i}", [128, D], F32) for i in range(2)]
        self.x1r = [s(f"x1r{i}", [128, D], F32) for i in range(2)]
        self.x2t = [s(f"x2t{i}", [128, D], F32) for i in range(2)]
        self.st5 = s("st5", [128, 24], F32)
        self.y5 = [s(f"y5_{i}", [128, D], F32) for i in range(2)]

    def p5_loads(self, jj):
        if jj >= NCH:
            return
        rA_, rB_, x1r_ = self.rA[jj % 2], self.rB[jj % 2], self.x1r[jj % 2]
        self.cx.dma("pool", None, None, reads=[self.oslots.b, self.slotA.b], writes=[rA_.b],
                    fn=lambda e: e.indirect_dma_start(out=rA_[:, :], out_offset=None, in_=self.oslots[:, :],
                                                      in_offset=bass.IndirectOffsetOnAxis(ap=self.slotA[:, jj:jj + 1], axis=0)))
        self.cx.dma("pool", None, None, reads=[self.oslots.b, self.slotB.b], writes=[rB_.b],
                    fn=lambda e: e.indirect_dma_start(out=rB_[:, :], out_offset=None, in_=self.oslots[:, :],
                                                      in_offset=bass.IndirectOffsetOnAxis(ap=self.slotB[:, jj:jj + 1], axis=0)))
        self.dma("sp", x1r_[:, :], self.x1d[jj * 128:(jj + 1) * 128, :], r=[self.x1d], w=[x1r_])

    def p5_S1(self, j):
        if j >= NCH:
            return
        V, S, G = self.V, self.S, self.G
        rA, rB, x1r, y5 = self.rA[j % 2], self.rB[j % 2], self.x1r[j % 2], self.y5[j % 2]
        S(lambda e: e.activation(out=rA[:, :], in_=rA[:, :], func=AF.Identity, scale=self.gAB[:, 0, j:j + 1]), r=[rA, self.gAB], w=[rA])
        V(lambda e: e.scalar_tensor_tensor(out=rA[:, :], in0=rB[:, :], scalar=self.gAB[:, 1, j:j + 1], in1=rA[:, :], op0=ALU.mult, op1=ALU.add), r=[rB, rA, self.gAB], w=[rA])
        V(lambda e: e.scalar_tensor_tensor(out=y5[:, :], in0=x1r[:, :], scalar=float(ALPHA), in1=rA[:, :], op0=ALU.mult, op1=ALU.add), r=[x1r, rA], w=[y5], x=[rA])

    def p5_S2(self, j, dst):
        if j >= NCH or j < 0:
            return
        S = self.S
        y5, x2 = self.y5[j % 2], self.x2t[j % 2]
        self.ln_stats(y5, lambda a, b: y5[:, a:b], D, self.st5, act=True)
        S(lambda e: e.activation(out=x2[:, :], in_=y5[:, :], func=AF.Identity, bias=self.st5[:, 4:5], scale=self.st5[:, 3:4]), r=[y5, self.st5], w=[x2])

    def p5_S3(self, j, dst):
        if j >= NCH or j < 0:
            return
        V, G = self.V, self.G
        rows = slice(j * 128, (j + 1) * 128)
        x2 = self.x2t[j % 2]
        V(lambda e: e.tensor_tensor(out=x2[:, :], in0=x2[:, :], in1=self.lng2[:, :], op=ALU.mult), r=[x2, self.lng2], w=[x2])
        V(lambda e: e.tensor_tensor(out=x2[:, :], in0=x2[:, :], in1=self.lnb2[:, :], op=ALU.add), r=[x2, self.lnb2], w=[x2], x=[x2])
        self.dma("sp", dst[rows, :], x2[:, :], r=[x2], w=[dst])

    def p5_all(self, l, dst):
        self.p5_loads(0); self.p5_loads(1)
        self.p5_S1(0)
        self.p5_loads(2)
        self.p5_S1(1)
        self.p5_S2(0, dst)
        for j in range(NCH):
            self.p5_loads(j + 3)
            self.p5_S1(j + 2)
            self.p5_S2(j + 1, dst)
            self.p5_S3(j, dst)

    def build(self):
        self.declare_io(); self.s5_io(); self.p3_io(); self.p4_io()
        self.setup()
        x_src = self.x_in
        for l in range(self.nlayers):
            dst = self.out if l == self.nlayers - 1 else self.xmid
            self.push()
            self.layer_alloc(); self.route_alloc(); self.layer_prep(l, 0)
            self.zero_slots()
            self.push(); self.qk_alloc()
            self.push(); self.s5_alloc(); self.s5_prep(l); self.load_win(l); self.p1v2_alloc()
            self.p1_all(l, x_src)
            self.pop()
            self.push(); self.p2_alloc(); self.p2_attention(); self.pop()
            self.pop()
            self.push()
            self.opg = self.sb("opg", [128, 2, 1024], F32)
            self.opg2 = self.sb("opg2", [128, 2, 1024], F32)
            self.layer_prep(l, 1)
            self.push(); self.p3_alloc(l)
            self.p3_route_alloc()
            self.p3_all(l, x_src)
            self.pop()
            self.push(); self.p4_experts(l); self.pop()
            self.push(); self.p5_alloc(l)
            self.p5_all(l, dst)
            self.pop()
            self.pop()
            self.pop()
            x_src = dst
        if getattr(self, "dbg_hook", None):
            self.dbg_hook(self)
        self.finish()


def make_inputs(inp, b):
    c = np.ascontiguousarray
    f = lambda k: np.asarray(inp[k])
    br, bi = f("ssm_b_re"), f("ssm_b_im")
    def bl(a):
        L = a.shape[0]
        return a.reshape(L, 2, 8, 64, 16).transpose(0, 2, 4, 1, 3).reshape(L, 128, 2, 64)
    bT = np.stack([bl(br), bl(bi)], axis=1)
    cr, ci = f("ssm_c_re"), f("ssm_c_im")
    cT = np.concatenate([cr.transpose(0, 1, 3, 2), ci.transpose(0, 1, 3, 2)], axis=2)
    L = br.shape[0]
    d = {
        "x": c(f("x")[b]), "ccol": c(f("c")[b].reshape(8, 128).T), "pos": c(f("positions")[b].reshape(32, 128).T),
        "ada_w": f("ada_w"), "ada_b": f("ada_b"), "w_in": f("w_in"), "gm_ln_g": f("gm_ln_g"), "gm_ln_b": f("gm_ln_b"),
        "gm_ws": f("gm_ws"), "gm_bsT": c(f("gm_bs").transpose(0, 2, 1)),
        "lam_re": c(f("ssm_lam_re").reshape(L, 1024)), "lam_im": c(f("ssm_lam_im").reshape(L, 1024)), "log_dt": f("ssm_log_dt"),
        "ssm_bT": c(bT), "ssm_cT": c(cT), "ssm_dT": c(f("ssm_d").reshape(L, 2, 128).transpose(0, 2, 1)),
        "glu_w": f("glu_w"), "glu_bT": c(f("glu_b").reshape(L, 2, 128).transpose(0, 2, 1)),
        "w_out": f("w_out"), "ln1_g": f("ln1_g"), "ln1_b": f("ln1_b"), "ln2_g": f("ln2_g"), "ln2_b": f("ln2_b"),
        "router_w": f("router_w"), "router_bias": f("router_bias"),
        "exp_w_gate": f("exp_w_gate"), "exp_w_up": f("exp_w_up"), "exp_w_down": f("exp_w_down"),
    }
    return d


_CACHE = {}


def kernel(**inputs):
    n = 8
    if "nc" not in _CACHE:
        nc = bass.Bass("TRN2", target_bir_lowering=False)
        kb = KB(nc)
        kb.build()
        _CACHE["nc"] = nc
        _CACHE["names"] = kb.in_names
    nc = _CACHE["nc"]
    names = _CACHE["names"]
    in_maps = []
    for b in range(n):
        im = make_inputs(inputs, b)
        in_maps.append({k: v for k, v in im.items() if k in names})
    res = run_bass_kernel_spmd(nc, in_maps, core_ids=list(range(n)))
    out = np.stack([np.asarray(r["out"]) for r in res.results], axis=0)
    return out.astype(np.float32)
```
